# Optimizing a Trainium2 kernel written in Bass

```python
import math
import jax
import jax.numpy as jnp
from jax import lax
import numpy as np

D_MODEL = 1024
BATCH = 4
SEQ = 4096
DEPTH = 2

GRID_W = 64
CTX_LEN = 256
EPS = 1e-6
N_MOD = 6

MIX_WIDTH = D_MODEL
SGU_DIM = MIX_WIDTH // 4
SGU_HEADS = 4
SGU_HEAD_DIM = SGU_DIM // SGU_HEADS
SGU_CHUNK = 128

S5_DIM = MIX_WIDTH // 4
S5_GROUP = 16
S5_GROUPS = S5_DIM // S5_GROUP
S5_STATE = 64

GLA_DIM = MIX_WIDTH - SGU_DIM - S5_DIM
GLA_HEADS = 8
GLA_DV = GLA_DIM // GLA_HEADS
GLA_DK = GLA_DV // 2
GLA_KEY_DIM = GLA_HEADS * GLA_DK
GLA_RANK = 16
GLA_GATE_TEMP = 16.0
GLA_CHUNK = 64

PEER_HEADS = 8
PEER_NKEYS = 128
PEER_EXPERTS = PEER_NKEYS * PEER_NKEYS
PEER_QDIM = 256
PEER_HALF = PEER_QDIM // 2
PEER_TOPK = 16
PEER_TOKEN_BLOCK = 128

IN_SPLITS = (SGU_DIM, SGU_DIM, S5_DIM, GLA_KEY_DIM, GLA_KEY_DIM, GLA_DIM, GLA_DIM, GLA_RANK, GLA_RANK)
IN_WIDTH = sum(IN_SPLITS)

kernel_name = 'hybrid_sgu_s5_gla_peer_flow_block'


def rmsnorm(x, g=None):
    xf = x.astype(jnp.float32)
    y = xf * lax.rsqrt(jnp.mean(xf * xf, axis=-1, keepdims=True) + EPS)
    if g is not None:
        y = y * g.astype(jnp.float32)
    return y.astype(x.dtype)


def modulate(h, shift, scale):
    return h * (1 + scale) + shift


def split_cols(p):
    cuts = [int(i) for i in np.cumsum(IN_SPLITS)[:-1]]
    return jnp.split(p, cuts, axis=-1)


def flip_t(t):
    return jnp.flip(t, axis=1)


def to_col_major(t, rows):
    b, n = t.shape[0], t.shape[1]
    rest = t.shape[2:]
    return jnp.swapaxes(t.reshape(b, rows, GRID_W, *rest), 1, 2).reshape(b, n, *rest)


def from_col_major(t, rows):
    b, n = t.shape[0], t.shape[1]
    rest = t.shape[2:]
    return jnp.swapaxes(t.reshape(b, GRID_W, rows, *rest), 1, 2).reshape(b, n, *rest)


def sgu_mixer(u, v, w_s, b_s):
    bsz, length, _ = u.shape
    n = length // SGU_CHUNK
    u = jax.nn.gelu(u)
    v = rmsnorm(jax.nn.gelu(v).reshape(bsz, n, SGU_CHUNK, SGU_HEADS, SGU_HEAD_DIM))
    mixed = jnp.einsum('hts,bnshd->bnthd', w_s, v) + jnp.swapaxes(b_s, 0, 1)[:, :, None]
    return u * mixed.reshape(bsz, length, SGU_DIM)


def s5_discretise(lam_re, lam_im, b_re, b_im, log_step):
    f32 = jnp.float32
    lam_re = jnp.minimum(lam_re.astype(f32), -1e-4)
    lam_im = lam_im.astype(f32)
    dt = jnp.exp(log_step.astype(f32))[:, None]
    mag = jnp.exp(lam_re * dt)
    a_re = mag * jnp.cos(lam_im * dt)
    a_im = mag * jnp.sin(lam_im * dt)
    den = lam_re * lam_re + lam_im * lam_im
    f_re = ((a_re - 1.0) * lam_re + a_im * lam_im) / den
    f_im = (a_im * lam_re - (a_re - 1.0) * lam_im) / den
    b_re = b_re.astype(f32)
    b_im = b_im.astype(f32)
    bb_re = f_re[..., None] * b_re - f_im[..., None] * b_im
    bb_im = f_re[..., None] * b_im + f_im[..., None] * b_re
    return a_re, a_im, bb_re, bb_im


def complex_affine_combine(e1, e2):
    a1r, a1i, b1r, b1i = e1
    a2r, a2i, b2r, b2i = e2
    return (a2r * a1r - a2i * a1i, a2r * a1i + a2i * a1r,
            a2r * b1r - a2i * b1i + b2r, a2r * b1i + a2i * b1r + b2i)


def s5_scan(u, disc, h0):
    a_re, a_im, bb_re, bb_im = disc
    bu_re = jnp.einsum('blgh,gph->blgp', u, bb_re)
    bu_im = jnp.einsum('blgh,gph->blgp', u, bb_im)
    ar = jnp.broadcast_to(a_re, bu_re.shape)
    ai = jnp.broadcast_to(a_im, bu_re.shape)
    cr, ci, hr, hi = lax.associative_scan(complex_affine_combine, (ar, ai, bu_re, bu_im), axis=1)
    if h0 is not None:
        h0r = h0[0][:, None]
        h0i = h0[1][:, None]
        hr = hr + cr * h0r - ci * h0i
        hi = hi + cr * h0i + ci * h0r
    return hr, hi


def s5_readout(h, c_re, c_im):
    return jnp.einsum('blgp,ghp->blgh', h[0], c_re) - jnp.einsum('blgp,ghp->blgh', h[1], c_im)


def s5_glu(y, w_glu):
    y = y.reshape(y.shape[0], y.shape[1], S5_DIM)
    z = jax.nn.gelu(y) @ w_glu.astype(jnp.float32)
    za, zb = jnp.split(z, 2, axis=-1)
    return za * jax.nn.sigmoid(zb)


def s5_mixer(xc, xl, lam_re, lam_im, b_re, b_im, c_re, c_im, log_step, d_skip, w_glu, ctx_out):
    f32 = jnp.float32

    def groups(t):
        return t.astype(f32).reshape(t.shape[0], t.shape[1], S5_GROUPS, S5_GROUP)

    uc, ul = groups(xc), groups(xl)
    ys_c, ys_l = [], []
    for d in range(2):
        disc = s5_discretise(lam_re[d], lam_im[d], b_re[d], b_im[d], log_step[d])
        cre, cim = c_re[d].astype(f32), c_im[d].astype(f32)
        uc_d = uc if d == 0 else flip_t(uc)
        ul_d = ul if d == 0 else flip_t(ul)
        hc = s5_scan(uc_d, disc, None)
        hl = s5_scan(ul_d, disc, (hc[0][:, -1], hc[1][:, -1]))
        y_l = s5_readout(hl, cre, cim)
        ys_l.append(y_l if d == 0 else flip_t(y_l))
        if ctx_out:
            y_c = s5_readout(hc, cre, cim)
            ys_c.append(y_c if d == 0 else flip_t(y_c))
    dd = d_skip.astype(f32).reshape(S5_GROUPS, S5_GROUP)
    out_l = s5_glu(ys_l[0] + ys_l[1] + dd * ul, w_glu)
    out_c = s5_glu(ys_c[0] + ys_c[1] + dd * uc, w_glu) if ctx_out else None
    return out_l, out_c


def gla_scan(q, k, v, log_a, s0, need_out):
    bsz, length, nh, dk = q.shape
    dv = v.shape[-1]
    n = length // GLA_CHUNK

    def chunks(t):
        return t.reshape(bsz, n, GLA_CHUNK, nh, t.shape[-1])

    q, k, v, log_a = chunks(q), chunks(k), chunks(v), chunks(log_a)
    b = jnp.cumsum(log_a, axis=2)
    b_last = b[:, :, -1]
    chunk_kv = jnp.einsum('bnshk,bnshv->bnhkv', k * jnp.exp(b_last[:, :, None] - b), v)
    decay = jnp.exp(b_last)
    if s0 is None:
        s0 = jnp.zeros((bsz, nh, dk, dv), jnp.float32)

    def step(s, inp):
        dec, kv = inp
        return dec[..., None] * s + kv, s

    s_final, s_in = lax.scan(step, s0, (jnp.swapaxes(decay, 0, 1), jnp.swapaxes(chunk_kv, 0, 1)))
    if not need_out:
        return None, s_final
    s_in = jnp.swapaxes(s_in, 0, 1)
    o_inter = jnp.einsum('bnthk,bnhkv->bnthv', q * jnp.exp(b), s_in)
    b_ref = b[:, :, GLA_CHUNK // 2][:, :, None]
    scores = jnp.einsum('bnthk,bnshk->bnhts', q * jnp.exp(b - b_ref), k * jnp.exp(b_ref - b))
    mask = jnp.tril(jnp.ones((GLA_CHUNK, GLA_CHUNK), dtype=bool))
    scores = jnp.where(mask, scores, 0.0)
    o_intra = jnp.einsum('bnhts,bnshv->bnthv', scores, v)
    return (o_inter + o_intra).reshape(bsz, length, nh, dv), s_final


def gla_gates(z, w_up, b_up):
    f32 = jnp.float32
    za = z.astype(f32) @ w_up.astype(f32) + b_up.astype(f32)
    return (jax.nn.log_sigmoid(za) / GLA_GATE_TEMP).reshape(z.shape[0], z.shape[1], GLA_HEADS, GLA_DK)


def gla_out(o, g, norm_g):
    o = rmsnorm(o).reshape(o.shape[0], o.shape[1], GLA_DIM) * norm_g.astype(jnp.float32)
    return o * jax.nn.silu(g.astype(jnp.float32))


def gla_mixer(cols_c, cols_l, w_gate, b_gate, norm_g, rows, ctx_out):
    f32 = jnp.float32

    def prep(cols):
        q, k, v, g, zf, zb = cols
        q = q.astype(f32).reshape(q.shape[0], q.shape[1], GLA_HEADS, GLA_DK) * (GLA_DK ** -0.5)
        k = k.astype(f32).reshape(k.shape[0], k.shape[1], GLA_HEADS, GLA_DK)
        v = v.astype(f32).reshape(v.shape[0], v.shape[1], GLA_HEADS, GLA_DV)
        af = gla_gates(zf, w_gate[0], b_gate[0])
        ab = gla_gates(zb, w_gate[1], b_gate[1])
        return q, k, v, af, ab, g

    qc, kc, vc, afc, abc, gc = prep(cols_c)
    ql, kl, vl, afl, abl, gl = prep(cols_l)
    ql, kl, vl, afl, abl = [to_col_major(t, rows) for t in (ql, kl, vl, afl, abl)]
    oc_f, sc_f = gla_scan(qc, kc, vc, afc, None, ctx_out)
    oc_b, sc_b = gla_scan(flip_t(qc), flip_t(kc), flip_t(vc), flip_t(abc), None, ctx_out)
    ol_f, _ = gla_scan(ql, kl, vl, afl, sc_f, True)
    ol_b, _ = gla_scan(flip_t(ql), flip_t(kl), flip_t(vl), flip_t(abl), sc_b, True)
    out_l = gla_out(from_col_major(ol_f + flip_t(ol_b), rows), gl, norm_g)
    out_c = gla_out(oc_f + flip_t(oc_b), gc, norm_g) if ctx_out else None
    return out_l, out_c


def peer_ffn(h, w_query, sub_keys, expert_u, expert_v):
    bsz, length, dm = h.shape
    blocks = h.reshape(-1, PEER_TOKEN_BLOCK, dm)
    keys = sub_keys.astype(jnp.float32)

    def block(xb):
        q = (xb @ w_query).astype(jnp.float32).reshape(PEER_TOKEN_BLOCK, PEER_HEADS, 2, PEER_HALF)
        s = jnp.einsum('thpd,phkd->thpk', q, keys)
        s1, i1 = lax.top_k(s[:, :, 0], PEER_TOPK)
        s2, i2 = lax.top_k(s[:, :, 1], PEER_TOPK)
        cand_s = (s1[..., :, None] + s2[..., None, :]).reshape(PEER_TOKEN_BLOCK, PEER_HEADS, PEER_TOPK * PEER_TOPK)
        cand_i = (i1[..., :, None] * PEER_NKEYS + i2[..., None, :]).reshape(PEER_TOKEN_BLOCK, PEER_HEADS, PEER_TOPK * PEER_TOPK)
        top_s, pos = lax.top_k(cand_s, PEER_TOPK)
        idx = jnp.take_along_axis(cand_i, pos, axis=-1)
        gate = jax.nn.softmax(top_s, axis=-1)
        act = jax.nn.gelu(jnp.einsum('td,thkd->thk', xb, expert_u[idx]).astype(jnp.float32))
        return jnp.einsum('thk,thkd->td', (gate * act).astype(xb.dtype), expert_v[idx])

    return lax.map(block, blocks).reshape(bsz, length, dm)


def setup_inputs(seed: int = 0) -> dict:
    key = jax.random.key(seed)
    ks = iter(jax.random.split(key, 40))
    f32 = jnp.float32

    def nrm(shape, scale):
        return jax.random.normal(next(ks), shape, f32) * scale

    L, D = DEPTH, D_MODEL
    n_idx = jnp.arange(S5_STATE, dtype=f32)
    return {
        'x': nrm((BATCH, SEQ, D), 1.0),
        'c': nrm((BATCH, D), 1.0),
        'ctx': nrm((BATCH, CTX_LEN, D), 1.0),
        'c_ctx': nrm((D,), 1.0),
        'w_mod': nrm((L, D, N_MOD * D), 0.5 * D ** -0.5),
        'b_mod': nrm((L, N_MOD * D), 0.02),
        'norm1_g': 1.0 + nrm((L, D), 0.02),
        'norm2_g': 1.0 + nrm((L, D), 0.02),
        'w_in': nrm((L, D, IN_WIDTH), D ** -0.5),
        'w_out': nrm((L, MIX_WIDTH, D), MIX_WIDTH ** -0.5),
        'sgu_w': nrm((L, SGU_HEADS, SGU_CHUNK, SGU_CHUNK), SGU_CHUNK ** -0.5),
        'sgu_b': 1.0 + nrm((L, SGU_HEADS, SGU_CHUNK), 0.1),
        's5_lambda_re': -0.5 + nrm((L, 2, S5_GROUPS, S5_STATE), 0.01),
        's5_lambda_im': math.pi * n_idx + nrm((L, 2, S5_GROUPS, S5_STATE), 0.01),
        's5_b_re': nrm((L, 2, S5_GROUPS, S5_STATE, S5_GROUP), (2 * S5_GROUP) ** -0.5),
        's5_b_im': nrm((L, 2, S5_GROUPS, S5_STATE, S5_GROUP), (2 * S5_GROUP) ** -0.5),
        's5_c_re': nrm((L, 2, S5_GROUPS, S5_GROUP, S5_STATE), (2 * S5_STATE) ** -0.5),
        's5_c_im': nrm((L, 2, S5_GROUPS, S5_GROUP, S5_STATE), (2 * S5_STATE) ** -0.5),
        's5_log_step': jax.random.uniform(next(ks), (L, 2, S5_GROUPS), f32, math.log(1e-3), math.log(1e-1)),
        's5_d': nrm((L, S5_DIM), 1.0),
        's5_w_glu': nrm((L, S5_DIM, 2 * S5_DIM), S5_DIM ** -0.5),
        'gla_w_gate': nrm((L, 2, GLA_RANK, GLA_KEY_DIM), GLA_RANK ** -0.5),
        'gla_b_gate': nrm((L, 2, GLA_KEY_DIM), 0.1),
        'gla_norm_g': 1.0 + nrm((L, GLA_DIM), 0.02),
        'peer_w_query': nrm((L, D, PEER_HEADS * PEER_QDIM), D ** -0.5),
        'peer_sub_keys': nrm((L, 2, PEER_HEADS, PEER_NKEYS, PEER_HALF), PEER_HALF ** -0.5),
        'peer_expert_u': nrm((L, PEER_EXPERTS, D), D ** -0.5),
        'peer_expert_v': nrm((L, PEER_EXPERTS, D), PEER_HEADS ** -0.5),
        'final_norm_g': 1.0 + nrm((D,), 0.02),
    }


def reference(x, c, ctx, c_ctx, w_mod, b_mod, norm1_g, norm2_g, w_in, w_out, sgu_w, sgu_b,
              s5_lambda_re, s5_lambda_im, s5_b_re, s5_b_im, s5_c_re, s5_c_im, s5_log_step, s5_d, s5_w_glu,
              gla_w_gate, gla_b_gate, gla_norm_g, peer_w_query, peer_sub_keys, peer_expert_u, peer_expert_v,
              final_norm_g):
    rows = x.shape[1] // GRID_W
    xl, xc = x, ctx
    for l in range(DEPTH):
        ctx_out = l < DEPTH - 1
        mod_l = [m[:, None, :] for m in jnp.split(jax.nn.silu(c) @ w_mod[l] + b_mod[l], N_MOD, axis=-1)]
        mod_c = jnp.split(jax.nn.silu(c_ctx) @ w_mod[l] + b_mod[l], N_MOD, axis=-1)

        cols_l = split_cols(modulate(rmsnorm(xl, norm1_g[l]), mod_l[0], mod_l[1]) @ w_in[l])
        cols_c = split_cols(modulate(rmsnorm(xc, norm1_g[l]), mod_c[0], mod_c[1]) @ w_in[l])
        s5_l, s5_c = s5_mixer(cols_c[2], cols_l[2], s5_lambda_re[l], s5_lambda_im[l], s5_b_re[l], s5_b_im[l],
                              s5_c_re[l], s5_c_im[l], s5_log_step[l], s5_d[l], s5_w_glu[l], ctx_out)
        gla_l, gla_c = gla_mixer(cols_c[3:], cols_l[3:], gla_w_gate[l], gla_b_gate[l], gla_norm_g[l], rows, ctx_out)
        sgu_l = sgu_mixer(cols_l[0], cols_l[1], sgu_w[l], sgu_b[l])
        y_l = jnp.concatenate([sgu_l.astype(xl.dtype), s5_l.astype(xl.dtype), gla_l.astype(xl.dtype)], axis=-1) @ w_out[l]
        xl = xl + mod_l[2] * y_l

        h_l = modulate(rmsnorm(xl, norm2_g[l]), mod_l[3], mod_l[4])
        xl = xl + mod_l[5] * peer_ffn(h_l, peer_w_query[l], peer_sub_keys[l], peer_expert_u[l], peer_expert_v[l])

        if ctx_out:
            sgu_c = sgu_mixer(cols_c[0], cols_c[1], sgu_w[l], sgu_b[l])
            y_c = jnp.concatenate([sgu_c.astype(xc.dtype), s5_c.astype(xc.dtype), gla_c.astype(xc.dtype)], axis=-1) @ w_out[l]
            xc = xc + mod_c[2] * y_c
            h_c = modulate(rmsnorm(xc, norm2_g[l]), mod_c[3], mod_c[4])
            xc = xc + mod_c[5] * peer_ffn(h_c, peer_w_query[l], peer_sub_keys[l], peer_expert_u[l], peer_expert_v[l])
    return rmsnorm(xl, final_norm_g)
```

```python
from contextlib import ExitStack
import math
import numpy as np
import concourse.bass as bass
import concourse.mybir as mybir
from concourse.bass_utils import run_bass_kernel_spmd

F32 = mybir.dt.float32
I32 = mybir.dt.int32
U32 = mybir.dt.uint32
ALU = mybir.AluOpType
AF = mybir.ActivationFunctionType
AX = mybir.AxisListType


class Sched:
    NDS = 4

    def __init__(self, nc, stack, gate=None, gate_val=0, nds=None):
        self.nc = nc
        self.ndsq = dict(sp=self.NDS, act=self.NDS, pool=self.NDS)
        if nds:
            self.ndsq.update(nds)
        self.gate = gate
        self.gate_val = gate_val
        self.eng = {"pe": nc.tensor, "dve": nc.vector, "act": nc.scalar,
                    "pool": nc.gpsimd, "sp": nc.sync}
        self.sem = {}
        self.cnt = {}
        _UID[0] += 1
        u = "q%d" % _UID[0]
        for e in self.eng:
            self.sem[e] = stack.enter_context(nc.semaphore(u + "s_" + e))
            self.cnt[e] = 0
        self.dq = {}
        for q in ("sp", "act", "pool"):
            sems = [stack.enter_context(nc.semaphore(u + "d_%s%d" % (q, i))) for i in range(self.ndsq[q])]
            self.dq[q] = {"sems": sems, "n": 0}
            for i, s in enumerate(sems):
                self.sem["d_%s%d" % (q, i)] = s
        self.seen = {e: {} for e in self.eng}
        self.prog = {e: [] for e in self.eng}
        self.lastw = {}
        self.reads = {}
        self.ninst = 0
        if gate is not None:
            self.sem["gate"] = gate
        if gate is not None and gate_val > 0:
            for e in self.eng:
                self.prog[e].append(("w", "gate", gate_val))

    def _need(self, need, ev):
        if ev is None:
            return
        s, v = ev
        if need.get(s, 0) < v:
            need[s] = v

    def _waits(self, e, reads, writes):
        need = {}
        for k in reads:
            self._need(need, self.lastw.get(k))
        for k in writes:
            self._need(need, self.lastw.get(k))
            for ev in self.reads.get(k, ()):
                self._need(need, ev)
        eng = self.eng[e]
        seen = self.seen[e]
        for s, v in need.items():
            if seen.get(s, 0) >= v:
                continue
            self.prog[e].append(("w", s, v))
            seen[s] = v
            self.ninst += 1

    def _commit(self, ev, reads, writes):
        for k in writes:
            self.lastw[k] = ev
            self.reads[k] = []
        for k in reads:
            if k in writes:
                continue
            self.reads.setdefault(k, []).append(ev)
            if len(self.reads[k]) > 24:
                d = {}
                for s, v in self.reads[k]:
                    if d.get(s, 0) < v:
                        d[s] = v
                self.reads[k] = list(d.items())

    def op(self, e, fn, reads=(), writes=()):
        reads = tuple(reads)
        writes = tuple(writes)
        self._waits(e, reads, writes)
        self.cnt[e] += 1
        self.prog[e].append(("i", fn, e, 1))
        self.ninst += 1
        self._commit((e, self.cnt[e]), reads, writes)

    def dma(self, q, fn, reads=(), writes=()):
        reads = tuple(reads)
        writes = tuple(writes)
        st = self.dq[q]
        i = st["n"]
        slot = i % self.ndsq[q]
        sname = "d_%s%d" % (q, slot)
        rnd = i // self.ndsq[q]
        eng = self.eng[q]
        if rnd > 0 and self.seen[q].get(sname, 0) < 16 * rnd:
            self.prog[q].append(("w", sname, 16 * rnd))
            self.seen[q][sname] = 16 * rnd
            self.ninst += 1
        self._waits(q, reads, writes)
        self.prog[q].append(("i", fn, sname, 16))
        st["n"] = i + 1
        self.ninst += 1
        self._commit((sname, 16 * (rnd + 1)), reads, writes)

    def finish(self, keys, e="sp"):
        self._waits(e, tuple(keys), ())

    def drain_all(self, e="sp"):
        need = {}
        for en, c in self.cnt.items():
            if c:
                need[en] = c
        for q, stq in self.dq.items():
            n = stq["n"]
            nq = self.ndsq[q]
            for slot in range(nq):
                uses = (n - slot + nq - 1) // nq if n > slot else 0
                if uses:
                    need["d_%s%d" % (q, slot)] = 16 * uses
        eng = self.eng[e]
        for s, v in need.items():
            if self.seen[e].get(s, 0) >= v:
                continue
            self.prog[e].append(("w", s, v))
            self.seen[e][s] = v

    def emit(self):
        nc = self.nc
        with nc.Block() as block:
            def mk(e):
                def body(engine):
                    for it in self.prog[e]:
                        if it[0] == "w":
                            engine.wait_ge(self.sem[it[1]], it[2])
                        else:
                            it[1](engine).then_inc(self.sem[it[2]], it[3])
                return body
            block.sync(mk("sp"))
            block.scalar(mk("act"))
            block.vector(mk("dve"))
            block.gpsimd(mk("pool"))
            block.tensor(mk("pe"))
        self.prog = {e: [] for e in self.eng}

    def sync_all(self):
        for e in self.eng:
            self.drain_all(e)
        self.lastw = {}
        self.reads = {}

D = 1024
NMOD = 6
INW = 2336
INW_T = 19
EPS = 1e-6
NTOK = 2176


_UID = [0]


def _mk(nc, st):
    _UID[0] += 1
    u = "u%d_" % _UID[0]

    def sb(name, shape, dt=F32):
        return st.enter_context(nc.sbuf_tensor(u + name, shape, dt))

    def ps(name, shape, dt=F32):
        return st.enter_context(nc.psum_tensor(u + name, shape, dt))
    return sb, ps


def _groups(n, g=512):
    out = []
    t = 0
    while t < n:
        out.append((t, min(g, n - t)))
        t += g
    return out


def build_PA(ntok=NTOK, nctx=128):
    nc = bass.Bass("TRN2", target_bir_lowering=False)
    xT = nc.dram_tensor("xT", [D, ntok], F32, kind="ExternalInput").ap()
    cT = nc.dram_tensor("cT", [128, 16], F32, kind="ExternalInput").ap()
    w_mod = nc.dram_tensor("w_mod", [D, NMOD * D], F32, kind="ExternalInput").ap()
    b_mod = nc.dram_tensor("b_mod", [128, 48], F32, kind="ExternalInput").ap()
    g1 = nc.dram_tensor("g1", [128, 8], F32, kind="ExternalInput").ap()
    w_in = nc.dram_tensor("w_in", [D, INW], F32, kind="ExternalInput").ap()
    ones = nc.dram_tensor("ones", [128, 128], F32, kind="ExternalInput").ap()
    colsT = nc.dram_tensor("colsT", [INW_T * 128, ntok], F32, kind="ExternalOutput").ap()
    modT = nc.dram_tensor("modT", [128, 96], F32, kind="ExternalOutput").ap()
    with ExitStack() as st:
        S = Sched(nc, st)
        sb, ps = _mk(nc, st)
        CT = sb("CT", [128, 16]); SC = sb("SC", [128, 16]); BM = sb("BM", [128, 48]); G1 = sb("G1", [128, 8])
        ONES = sb("ONES", [128, 128]); MOD = sb("MOD", [128, 96])
        A1 = sb("A1", [128, 16]); TMPA = sb("TMPA", [128, 16])
        WM = [sb("WM%d" % i, [128, 8, 512]) for i in range(2)]
        WIN = sb("WIN", [128, 8, INW])
        XT = [sb("XT%d" % i, [128, 8, 512]) for i in range(2)]
        XSQ = sb("XSQ", [128, 512]); RSTD = sb("RSTD", [128, 512]); TMP = sb("TMP", [128, 512])
        HT = [sb("HT%d" % i, [128, 8, 512]) for i in range(2)]
        OUTB = [sb("OUTB%d" % i, [128, 512]) for i in range(4)]
        pmod = ps("pmod", [128, 96]); pss = ps("pss", [128, 512])
        pout = [ps("pout%d" % i, [128, 512]) for i in range(3)]

        S.dma("sp", lambda e: e.dma_start(out=CT[:], in_=cT), writes=["CT"])
        S.dma("sp", lambda e: e.dma_start(out=BM[:], in_=b_mod), writes=["BM"])
        S.dma("sp", lambda e: e.dma_start(out=G1[:], in_=g1), writes=["G1"])
        S.dma("sp", lambda e: e.dma_start(out=ONES[:], in_=ones), writes=["ONES"])
        S.op("act", lambda e: e.activation(SC[:], CT[:], AF.Silu), reads=["CT"], writes=["SC"])
        SC3 = SC[:].rearrange("p (k c) -> p k c", c=2)
        wm_v = w_mod.rearrange("(k p) f -> p k f", p=128)
        for jg in range(12):
            b = jg % 2
            S.dma("act" if jg % 2 else "sp",
                  lambda e, jg=jg, b=b: e.dma_start(out=WM[b][:], in_=wm_v[:, :, jg * 512:(jg + 1) * 512]),
                  writes=["WM%d" % b])
            for j8 in range(4):
                j = jg * 4 + j8
                for k in range(8):
                    S.op("pe", lambda e, j=j, j8=j8, k=k, b=b: e.matmul(
                        pmod[:, 2 * j:2 * j + 2], WM[b][:, k, j8 * 128:(j8 + 1) * 128], SC3[:, k, :],
                        start=(k == 0), stop=(k == 7)), reads=["WM%d" % b, "SC"], writes=["pmod"])
        S.op("dve", lambda e: e.tensor_tensor(MOD[:].rearrange("p (j c) -> p j c", c=2),
                                              pmod[:].rearrange("p (j c) -> p j c", c=2),
                                              BM[:].unsqueeze(2).to_broadcast([128, 48, 2]), ALU.add),
             reads=["pmod", "BM"], writes=["MOD"])
        S.dma("sp", lambda e: e.dma_start(out=modT, in_=MOD[:]), reads=["MOD"], writes=["modT"])
        MOD3 = MOD[:].rearrange("p (j c) -> p j c", c=2)
        S.op("dve", lambda e: e.tensor_scalar(TMPA[:].rearrange("p (j c) -> p j c", c=2), MOD3[:, 8:16, :], 1.0, None, ALU.add),
             reads=["MOD"], writes=["TMPA"])
        S.op("dve", lambda e: e.tensor_tensor(A1[:].rearrange("p (j c) -> p j c", c=2),
                                              TMPA[:].rearrange("p (j c) -> p j c", c=2),
                                              G1[:].unsqueeze(2).to_broadcast([128, 8, 2]), ALU.mult),
             reads=["TMPA", "G1"], writes=["A1"])
        A13 = A1[:].rearrange("p (j c) -> p j c", c=2)
        win_v = w_in.rearrange("(k p) f -> p k f", p=128)
        for k in range(8):
            S.dma("pool", lambda e, k=k: e.dma_start(out=WIN[:, k, :], in_=win_v[:, k, :]), writes=["WIN%d" % k])
        xT_v = xT.rearrange("(k p) t -> p k t", p=128)
        grp = [(0, nctx, 1)] if nctx else []
        grp += [(nctx + t0, tn, 0) for (t0, tn) in _groups(ntok - nctx)]
        oi = 0
        for gi, (t0, tn, col) in enumerate(grp):
            b = gi % 2
            xk, hk = "XT%d" % b, "HT%d" % b
            S.dma("sp", lambda e, b=b, t0=t0, tn=tn: e.dma_start(out=XT[b][:, :, :tn], in_=xT_v[:, :, t0:t0 + tn]), writes=[xk])
            for k in range(8):
                S.op("act", lambda e, b=b, k=k, tn=tn: e.activation(XSQ[:, :tn], XT[b][:, k, :tn], AF.Square), reads=[xk], writes=["XSQ"])
                S.op("pe", lambda e, k=k, tn=tn: e.matmul(pss[:, :tn], ONES[:], XSQ[:, :tn], start=(k == 0), stop=(k == 7)),
                     reads=["ONES", "XSQ"], writes=["pss"])
            S.op("dve", lambda e, tn=tn: e.tensor_scalar(RSTD[:, :tn], pss[:, :tn], 1.0 / D, EPS, ALU.mult, ALU.add), reads=["pss"], writes=["RSTD"])
            S.op("act", lambda e, tn=tn: e.sqrt(RSTD[:, :tn], RSTD[:, :tn]), reads=["RSTD"], writes=["RSTD"])
            S.op("dve", lambda e, tn=tn: e.reciprocal(RSTD[:, :tn], RSTD[:, :tn]), reads=["RSTD"], writes=["RSTD"])
            for k in range(8):
                S.op("dve", lambda e, b=b, k=k, tn=tn: e.tensor_tensor(TMP[:, :tn], XT[b][:, k, :tn], RSTD[:, :tn], ALU.mult),
                     reads=[xk, "RSTD"], writes=["TMP"])
                S.op("act", lambda e, b=b, k=k, tn=tn, col=col: e.activation(
                    HT[b][:, k, :tn], TMP[:, :tn], AF.Identity, bias=MOD3[:, k, col:col + 1], scale=A13[:, k, col:col + 1]),
                    reads=["TMP", "MOD", "A1"], writes=[hk])
            for ot in range(INW_T):
                m = 128 if ot < INW_T - 1 else INW - 128 * (INW_T - 1)
                pb = oi % 3
                ob = oi % 4
                oi += 1
                for k in range(8):
                    S.op("pe", lambda e, b=b, k=k, tn=tn, ot=ot, m=m, pb=pb: e.matmul(
                        pout[pb][:m, :tn], WIN[:, k, ot * 128:ot * 128 + m], HT[b][:, k, :tn], start=(k == 0), stop=(k == 7)),
                        reads=["WIN%d" % k, hk], writes=["pout%d" % pb])
                if m < 128:
                    S.op("dve", lambda e, ob=ob: e.memset(OUTB[ob][:], 0.0), writes=["OUTB%d" % ob])
                eng = "act" if oi % 2 else "dve"
                if eng == "act":
                    S.op("act", lambda e, ob=ob, pb=pb, m=m, tn=tn: e.copy(OUTB[ob][:m, :tn], pout[pb][:m, :tn]),
                         reads=["pout%d" % pb], writes=["OUTB%d" % ob])
                else:
                    S.op("dve", lambda e, ob=ob, pb=pb, m=m, tn=tn: e.tensor_copy(OUTB[ob][:m, :tn], pout[pb][:m, :tn]),
                         reads=["pout%d" % pb], writes=["OUTB%d" % ob])
                S.dma("sp" if oi % 2 else "act", lambda e, ob=ob, ot=ot, t0=t0, tn=tn: e.dma_start(
                    out=colsT[ot * 128:(ot + 1) * 128, t0:t0 + tn], in_=OUTB[ob][:, :tn]),
                    reads=["OUTB%d" % ob], writes=["colsT_%d" % oi])
        S.drain_all("sp")
        S.emit()
    return nc


SEQT = 4352
S5_CH = [(0, 256)] + [(256 + 512 * i, 512) for i in range(8)]


def build_PB():
    nc = bass.Bass("TRN2", target_bir_lowering=False)
    uin = [nc.dram_tensor(n, [128, SEQT], F32, kind="ExternalInput").ap() for n in ("uf", "ub")]
    prm = nc.dram_tensor("prm", [128, 24], F32, kind="ExternalInput").ap()
    bre = nc.dram_tensor("bre", [128, 128], F32, kind="ExternalInput").ap()
    bim = nc.dram_tensor("bim", [128, 128], F32, kind="ExternalInput").ap()
    cre = nc.dram_tensor("cre", [128, 128], F32, kind="ExternalInput").ap()
    cim = nc.dram_tensor("cim", [128, 128], F32, kind="ExternalInput").ap()
    tau = nc.dram_tensor("tau", [128, 512], F32, kind="ExternalInput").ap()
    ident = nc.dram_tensor("ident", [128, 128], F32, kind="ExternalInput").ap()
    yout = [nc.dram_tensor(n, [128, SEQT], F32, kind="ExternalOutput").ap() for n in ("yf", "yb")]
    TWO_PI = 2.0 * math.pi
    with ExitStack() as st:
        S = Sched(nc, st)
        sb, ps = _mk(nc, st)
        PRM = sb("PRM", [128, 24]); BRE = sb("BRE", [128, 128]); BIM = sb("BIM", [128, 128])
        CRE = sb("CRE", [128, 128]); CIM = sb("CIM", [128, 128]); TAU = sb("TAU", [128, 512]); ID = sb("ID", [128, 128])
        names = ["DT", "LR", "MAG", "TH", "R", "R2", "RF", "FR", "SIN", "COS", "ARE", "AIM", "DEN", "AM1", "FRE", "FIM", "T0", "T1"]
        P = {n: sb("p_" + n, [128, 8]) for n in names}
        RI = sb("p_RI", [128, 8], I32)
        BBR = sb("BBR", [128, 128]); BBI = sb("BBI", [128, 128]); TB = sb("TB", [128, 128])
        PAD = sb("PAD", [128, 128])
        WBR = sb("WBR", [128, 8, 128]); WBI = sb("WBI", [128, 8, 128]); CR = sb("CR", [128, 8, 128]); CIN = sb("CIN", [128, 8, 128])
        TC = sb("TC", [128, 8, 512]); TS = sb("TS", [128, 8, 512]); RHO = sb("RHO", [128, 8, 512])
        RR = sb("RR", [128, 512]); RRF = sb("RRF", [128, 512]); RRI = sb("RRI", [128, 512], I32)
        UC = [sb("UC%d" % i, [128, 512]) for i in range(2)]
        W = {}
        for n in ("BR", "BI", "T1", "T2", "T3", "T4", "XR", "XI", "QR", "QI", "HR", "HI"):
            for i in range(2):
                W[n, i] = sb("w_%s%d" % (n, i), [128, 512])
        HP = sb("HP", [128, 8])
        YO = [sb("YO%d" % i, [128, 512]) for i in range(2)]
        pbr = [ps("pbr%d" % i, [128, 512]) for i in range(2)]
        pbi = [ps("pbi%d" % i, [128, 512]) for i in range(2)]
        py = [ps("py%d" % i, [128, 512]) for i in range(2)]
        ptr = ps("ptr", [128, 128])

        for (t, src, k) in ((PRM, prm, "PRM"), (BRE, bre, "BRE"), (BIM, bim, "BIM"), (CRE, cre, "CRE"), (CIM, cim, "CIM"),
                            (TAU, tau, "TAU"), (ID, ident, "ID")):
            S.dma("sp", lambda e, t=t, src=src: e.dma_start(out=t[:], in_=src), writes=[k])
        PR3 = PRM[:].rearrange("p (a c) -> p a c", c=3)
        K = ["PP"]

        def V(fn, reads=(), writes=()):
            S.op("dve", fn, reads=list(reads) + K, writes=list(writes) + K)

        def A(fn, reads=(), writes=()):
            S.op("act", fn, reads=list(reads) + K, writes=list(writes) + K)

        A(lambda e: e.activation(P["DT"][:], PR3[:, :, 2], AF.Exp), reads=["PRM"])
        V(lambda e: e.tensor_scalar(P["LR"][:], PR3[:, :, 0], -1e-4, None, ALU.min), reads=["PRM"])
        V(lambda e: e.tensor_tensor(P["T0"][:], P["LR"][:], P["DT"][:], ALU.mult))
        A(lambda e: e.activation(P["MAG"][:], P["T0"][:], AF.Exp))
        V(lambda e: e.tensor_tensor(P["TH"][:], PR3[:, :, 1], P["DT"][:], ALU.mult), reads=["PRM"])
        V(lambda e: e.tensor_scalar(P["R"][:], P["TH"][:], 1.0 / TWO_PI, None, ALU.mult))
        V(lambda e: e.tensor_scalar(P["R2"][:], P["R"][:], 0.25, None, ALU.add))
        for (src, dst) in (("R", "SIN"), ("R2", "COS")):
            V(lambda e, src=src: e.tensor_copy(RI[:], P[src][:]))
            V(lambda e: e.tensor_copy(P["RF"][:], RI[:]))
            V(lambda e, src=src: e.tensor_tensor(P["FR"][:], P[src][:], P["RF"][:], ALU.subtract))
            A(lambda e, dst=dst: e.activation(P[dst][:], P["FR"][:], AF.Sin, scale=TWO_PI))
        V(lambda e: e.tensor_tensor(P["ARE"][:], P["MAG"][:], P["COS"][:], ALU.mult))
        V(lambda e: e.tensor_tensor(P["AIM"][:], P["MAG"][:], P["SIN"][:], ALU.mult))
        V(lambda e: e.tensor_tensor(P["T0"][:], P["LR"][:], P["LR"][:], ALU.mult))
        V(lambda e: e.tensor_tensor(P["T1"][:], PR3[:, :, 1], PR3[:, :, 1], ALU.mult), reads=["PRM"])
        V(lambda e: e.tensor_tensor(P["DEN"][:], P["T0"][:], P["T1"][:], ALU.add))
        V(lambda e: e.reciprocal(P["DEN"][:], P["DEN"][:]))
        V(lambda e: e.tensor_scalar(P["AM1"][:], P["ARE"][:], -1.0, None, ALU.add))
        V(lambda e: e.tensor_tensor(P["T0"][:], P["AM1"][:], P["LR"][:], ALU.mult))
        V(lambda e: e.tensor_tensor(P["T1"][:], P["AIM"][:], PR3[:, :, 1], ALU.mult), reads=["PRM"])
        V(lambda e: e.tensor_tensor(P["T0"][:], P["T0"][:], P["T1"][:], ALU.add))
        V(lambda e: e.tensor_tensor(P["FRE"][:], P["T0"][:], P["DEN"][:], ALU.mult))
        V(lambda e: e.tensor_tensor(P["T0"][:], P["AIM"][:], P["LR"][:], ALU.mult))
        V(lambda e: e.tensor_tensor(P["T1"][:], P["AM1"][:], PR3[:, :, 1], ALU.mult), reads=["PRM"])
        V(lambda e: e.tensor_tensor(P["T0"][:], P["T0"][:], P["T1"][:], ALU.subtract))
        V(lambda e: e.tensor_tensor(P["FIM"][:], P["T0"][:], P["DEN"][:], ALU.mult))

        def v3(t):
            return t[:].rearrange("p (a h) -> p a h", h=16)

        def bc(n):
            return P[n][:].unsqueeze(2).to_broadcast([128, 8, 16])
        V(lambda e: e.tensor_tensor(v3(BBR), v3(BRE), bc("FRE"), ALU.mult), reads=["BRE"])
        V(lambda e: e.tensor_tensor(v3(TB), v3(BIM), bc("FIM"), ALU.mult), reads=["BIM"])
        V(lambda e: e.tensor_tensor(BBR[:], BBR[:], TB[:], ALU.subtract))
        V(lambda e: e.tensor_tensor(v3(BBI), v3(BIM), bc("FRE"), ALU.mult), reads=["BIM"])
        V(lambda e: e.tensor_tensor(v3(TB), v3(BRE), bc("FIM"), ALU.mult), reads=["BRE"])
        V(lambda e: e.tensor_tensor(BBI[:], BBI[:], TB[:], ALU.add))
        V(lambda e: e.tensor_scalar(CIM[:], CIM[:], -1.0, None, ALU.mult), reads=["CIM"], writes=["CIM"])
        V(lambda e: e.memset(CR[:], 0.0)); V(lambda e: e.memset(CIN[:], 0.0))
        for dj in range(8):
            j = dj % 4
            for (src, dst) in ((BBR, WBR), (BBI, WBI)):
                V(lambda e: e.memset(PAD[:], 0.0), writes=["PAD"])
                V(lambda e, src=src, dj=dj, j=j: e.tensor_copy(PAD[0:64, 32 * j:32 * j + 16], src[0:64, dj * 16:dj * 16 + 16]), writes=["PAD"])
                V(lambda e, src=src, dj=dj, j=j: e.tensor_copy(PAD[64:128, 32 * j + 16:32 * j + 32], src[64:128, dj * 16:dj * 16 + 16]), writes=["PAD"])
                S.op("pe", lambda e: e.transpose(ptr[:], PAD[:], ID[:]), reads=["PAD", "ID"], writes=["ptr"])
                S.op("act", lambda e, dst=dst, dj=dj: e.copy(dst[:, dj, :], ptr[:]), reads=["ptr"], writes=["WB"])
            for (src, dst) in ((CRE, CR), (CIM, CIN)):
                V(lambda e, src=src, dst=dst, dj=dj, j=j: e.tensor_copy(dst[0:64, dj, 32 * j:32 * j + 16], src[0:64, dj * 16:dj * 16 + 16]), reads=["CRE", "CIM"], writes=["CC"])
                V(lambda e, src=src, dst=dst, dj=dj, j=j: e.tensor_copy(dst[64:128, dj, 32 * j + 16:32 * j + 32], src[64:128, dj * 16:dj * 16 + 16]), reads=["CRE", "CIM"], writes=["CC"])
            for (off, dst) in ((0.0, TS), (0.25, TC)):
                V(lambda e, dj=dj, off=off: e.tensor_scalar(RR[:], TAU[:], P["R"][:, dj:dj + 1], off, ALU.mult, ALU.add), reads=["TAU"], writes=["RR"])
                V(lambda e: e.tensor_copy(RRI[:], RR[:]), reads=["RR"], writes=["RRI"])
                V(lambda e: e.tensor_copy(RRF[:], RRI[:]), reads=["RRI"], writes=["RRF"])
                V(lambda e: e.tensor_tensor(RRF[:], RR[:], RRF[:], ALU.subtract), reads=["RR"], writes=["RRF"])
                S.op("act", lambda e, dst=dst, dj=dj: e.activation(dst[:, dj, :], RRF[:], AF.Sin, scale=TWO_PI), reads=["RRF"], writes=["TAB"])
            V(lambda e, dj=dj: e.tensor_copy(RHO[:, dj, :], P["MAG"][:, dj:dj + 1].to_broadcast([128, 512])), writes=["TAB"])

        G = "pool"
        oi = 0
        for d in range(2):
            V(lambda e: e.memset(HP[:], 0.0), writes=["HP"])
            for ci, (t0, T) in enumerate(S5_CH):
                ub_ = (d * 9 + ci) % 2
                uk = "UC%d" % ub_
                S.dma("sp", lambda e, d=d, t0=t0, T=T, ub_=ub_: e.dma_start(out=UC[ub_][:, :T], in_=uin[d][:, t0:t0 + T]), writes=[uk])
                yb_ = (d * 9 + ci) % 2
                for j in range(4):
                    dj = d * 4 + j
                    b = j % 2
                    w = lambda n, b=b, T=T: W[n, b][:, :T]
                    k = lambda n, b=b: "w_%s%d" % (n, b)
                    S.op("pe", lambda e, dj=dj, b=b, T=T, ub_=ub_: e.matmul(pbr[b][:, :T], WBR[:, dj, :], UC[ub_][:, :T], start=True, stop=True),
                         reads=["WB", uk], writes=["pbr%d" % b])
                    S.op("pe", lambda e, dj=dj, b=b, T=T, ub_=ub_: e.matmul(pbi[b][:, :T], WBI[:, dj, :], UC[ub_][:, :T], start=True, stop=True),
                         reads=["WB", uk], writes=["pbi%d" % b])
                    S.op("act", lambda e, w=w, b=b, T=T: e.copy(w("BR"), pbr[b][:, :T]), reads=["pbr%d" % b], writes=[k("BR")])
                    S.op("act", lambda e, w=w, b=b, T=T: e.copy(w("BI"), pbi[b][:, :T]), reads=["pbi%d" % b], writes=[k("BI")])
                    cs = lambda dj=dj, T=T: TC[:, dj, :T]
                    sn = lambda dj=dj, T=T: TS[:, dj, :T]
                    S.op("dve", lambda e, w=w, cs=cs: e.tensor_tensor(w("T1"), cs(), w("BR"), ALU.mult), reads=["TAB", k("BR")], writes=[k("T1")])
                    S.op("dve", lambda e, w=w, sn=sn: e.tensor_tensor(w("T2"), sn(), w("BI"), ALU.mult), reads=["TAB", k("BI")], writes=[k("T2")])
                    S.op("dve", lambda e, w=w: e.tensor_tensor(w("XR"), w("T1"), w("T2"), ALU.add), reads=[k("T1"), k("T2")], writes=[k("XR")])
                    S.op(G, lambda e, w=w, cs=cs: e.tensor_tensor(w("T3"), cs(), w("BI"), ALU.mult), reads=["TAB", k("BI")], writes=[k("T3")])
                    S.op(G, lambda e, w=w, sn=sn: e.tensor_tensor(w("T4"), sn(), w("BR"), ALU.mult), reads=["TAB", k("BR")], writes=[k("T4")])
                    S.op(G, lambda e, w=w: e.tensor_tensor(w("XI"), w("T3"), w("T4"), ALU.subtract), reads=[k("T3"), k("T4")], writes=[k("XI")])
                    S.op("dve", lambda e, w=w, dj=dj, j=j, T=T: e.tensor_tensor_scan(w("QR"), RHO[:, dj, :T], w("XR"), HP[:, 2 * j:2 * j + 1], ALU.mult, ALU.add),
                         reads=["TAB", k("XR"), "HP"], writes=[k("QR")])
                    S.op("dve", lambda e, w=w, dj=dj, j=j, T=T: e.tensor_tensor_scan(w("QI"), RHO[:, dj, :T], w("XI"), HP[:, 2 * j + 1:2 * j + 2], ALU.mult, ALU.add),
                         reads=["TAB", k("XI"), "HP"], writes=[k("QI")])
                    S.op("dve", lambda e, w=w, cs=cs: e.tensor_tensor(w("T1"), cs(), w("QR"), ALU.mult), reads=["TAB", k("QR")], writes=[k("T1")])
                    S.op("dve", lambda e, w=w, sn=sn: e.tensor_tensor(w("T2"), sn(), w("QI"), ALU.mult), reads=["TAB", k("QI")], writes=[k("T2")])
                    S.op("dve", lambda e, w=w: e.tensor_tensor(w("HR"), w("T1"), w("T2"), ALU.subtract), reads=[k("T1"), k("T2")], writes=[k("HR")])
                    S.op(G, lambda e, w=w, sn=sn: e.tensor_tensor(w("T3"), sn(), w("QR"), ALU.mult), reads=["TAB", k("QR")], writes=[k("T3")])
                    S.op(G, lambda e, w=w, cs=cs: e.tensor_tensor(w("T4"), cs(), w("QI"), ALU.mult), reads=["TAB", k("QI")], writes=[k("T4")])
                    S.op(G, lambda e, w=w: e.tensor_tensor(w("HI"), w("T3"), w("T4"), ALU.add), reads=[k("T3"), k("T4")], writes=[k("HI")])
                    S.op("act", lambda e, b=b, j=j, T=T: e.copy(HP[:, 2 * j:2 * j + 1], W["HR", b][:, T - 1:T]), reads=[k("HR")], writes=["HP"])
                    S.op("act", lambda e, b=b, j=j, T=T: e.copy(HP[:, 2 * j + 1:2 * j + 2], W["HI", b][:, T - 1:T]), reads=[k("HI")], writes=["HP"])
                    S.op("pe", lambda e, dj=dj, w=w, j=j, yb_=yb_, T=T: e.matmul(py[yb_][:, :T], CR[:, dj, :], w("HR"), start=(j == 0), stop=False),
                         reads=["CC", k("HR")], writes=["py%d" % yb_])
                    S.op("pe", lambda e, dj=dj, w=w, j=j, yb_=yb_, T=T: e.matmul(py[yb_][:, :T], CIN[:, dj, :], w("HI"), start=False, stop=(j == 3)),
                         reads=["CC", k("HI")], writes=["py%d" % yb_])
                S.op("act", lambda e, yb_=yb_, T=T: e.copy(YO[yb_][:, :T], py[yb_][:, :T]), reads=["py%d" % yb_], writes=["YO%d" % yb_])
                oi += 1
                S.dma("act", lambda e, d=d, yb_=yb_, t0=t0, T=T: e.dma_start(out=yout[d][:, t0:t0 + T], in_=YO[yb_][:, :T]),
                      reads=["YO%d" % yb_], writes=["yout_%d" % oi])
        S.drain_all("sp")
        S.emit()
    return nc


NCH = 68


def build_PC():
    nc = bass.Bass("TRN2", target_bir_lowering=False)
    I = {}
    for d in range(2):
        I["qT", d] = nc.dram_tensor("qT%d" % d, [128, SEQT], F32, kind="ExternalInput").ap()
        I["kT", d] = nc.dram_tensor("kT%d" % d, [128, SEQT], F32, kind="ExternalInput").ap()
        I["v", d] = nc.dram_tensor("v%d" % d, [SEQT, 256], F32, kind="ExternalInput").ap()
        I["zT", d] = nc.dram_tensor("zT%d" % d, [16, SEQT], F32, kind="ExternalInput").ap()
        I["wg", d] = nc.dram_tensor("wg%d" % d, [16, 128], F32, kind="ExternalInput").ap()
        I["bg", d] = nc.dram_tensor("bg%d" % d, [128, 1], F32, kind="ExternalInput").ap()
        I["o", d] = nc.dram_tensor("o%d" % d, [SEQT, 256], F32, kind="ExternalOutput").ap()
    rst = nc.dram_tensor("rst", [128, 512], F32, kind="ExternalInput").ap()
    tmask = nc.dram_tensor("tmask", [64, 256], F32, kind="ExternalInput").ap()
    blk = nc.dram_tensor("blk", [128, 256], F32, kind="ExternalInput").ap()
    hmask = nc.dram_tensor("hmask", [128, 4], F32, kind="ExternalInput").ap()
    ident = nc.dram_tensor("ident", [128, 128], F32, kind="ExternalInput").ap()
    QSC = 32 ** -0.5
    with ExitStack() as st:
        S = Sched(nc, st)
        sb, ps = _mk(nc, st)
        RST = sb("RST", [128, 512]); TM = sb("TM", [64, 256]); BLK = sb("BLK", [128, 256]); HM = sb("HM", [128, 4]); ID = sb("ID", [128, 128])
        WG = sb("WG", [16, 128]); BG = sb("BG", [128, 1]); NBG = sb("NBG", [128, 1])
        Wb = {}
        for n in ("Q", "K", "LA", "B", "E", "D", "QE", "QS", "KD", "KS0", "KS1", "KS2", "KS3"):
            for i in range(2):
                Wb[n, i] = sb("g_%s%d" % (n, i), [128, 512])
        Z = [sb("Z%d" % i, [16, 512]) for i in range(2)]
        VV = [sb("VV%d" % i, [64, 8, 256]) for i in range(2)]
        DEC = [sb("DEC%d" % i, [128, 8]) for i in range(2)]
        KDT = [sb("KDT%d" % i, [64, 128]) for i in range(2)]
        STt = [sb("ST%d" % i, [64, 256]) for i in range(2)]
        OB = [sb("OB%d" % i, [64, 256]) for i in range(3)]
        KVM = sb("KVM", [128, 256])
        SS = [sb("SS%d" % i, [128, 256]) for i in range(2)]
        pza = ps("pza", [128, 512])
        pt0 = ps("pt0", [64, 128])
        pt = [pt0, pt0]
        pkv = [ps("pkv%d" % i, [128, 256]) for i in range(2)]
        psc = [ps("psc%d" % i, [64, 256]) for i in range(2)]
        po = [ps("po%d" % i, [64, 256]) for i in range(2)]
        for (t, src, k) in ((RST, rst, "RST"), (TM, tmask, "TM"), (BLK, blk, "BLK"), (HM, hmask, "HM"), (ID, ident, "ID")):
            S.dma("sp", lambda e, t=t, src=src: e.dma_start(out=t[:], in_=src), writes=[k])
        gc = 0
        oi = 0
        for d in range(2):
            S.dma("sp", lambda e, d=d: e.dma_start(out=WG[:], in_=I["wg", d]), writes=["WG"])
            S.dma("sp", lambda e, d=d: e.dma_start(out=BG[:], in_=I["bg", d]), writes=["BG"])
            S.op("dve", lambda e: e.tensor_scalar(NBG[:], BG[:], -1.0, None, ALU.mult), reads=["BG"], writes=["NBG"])
            S.op("dve", lambda e: e.memset(SS[0][:], 0.0), writes=["SS0"])
            scur = 0
            for bi, (t0, T) in enumerate(S5_CH):
                nchk = T // 64
                b = (d * 9 + bi) % 2
                w = lambda n, b=b, T=T: Wb[n, b][:, :T]
                k = lambda n, b=b: "g_%s%d" % (n, b)
                w3 = lambda n, b=b, T=T: Wb[n, b][:, :T].rearrange("p (c s) -> p c s", s=64)
                S.dma("sp", lambda e, d=d, b=b, t0=t0, T=T: e.dma_start(out=Wb["Q", b][:, :T], in_=I["qT", d][:, t0:t0 + T]), writes=[k("Q")])
                S.dma("act", lambda e, d=d, b=b, t0=t0, T=T: e.dma_start(out=Wb["K", b][:, :T], in_=I["kT", d][:, t0:t0 + T]), writes=[k("K")])
                S.dma("sp", lambda e, d=d, b=b, t0=t0, T=T: e.dma_start(out=Z[b][:, :T], in_=I["zT", d][:, t0:t0 + T]), writes=["Z%d" % b])
                S.dma("act", lambda e, d=d, b=b, t0=t0, T=T, nchk=nchk: e.dma_start(
                    out=VV[b][:, :nchk, :], in_=I["v", d][t0:t0 + T, :].rearrange("(c s) f -> s c f", s=64)), writes=["VV%d" % b])
                S.op("pe", lambda e, b=b, T=T: e.matmul(pza[:, :T], WG[:], Z[b][:, :T], start=True, stop=True), reads=["WG", "Z%d" % b], writes=["pza"])
                S.op("act", lambda e, w=w, T=T: e.activation(w("E"), pza[:, :T], AF.Exp, bias=NBG[:], scale=-1.0), reads=["pza", "NBG"], writes=[k("E")])
                S.op("act", lambda e, w=w: e.activation(w("E"), w("E"), AF.Ln, bias=1.0), reads=[k("E")], writes=[k("E")])
                S.op("dve", lambda e, w=w: e.tensor_scalar(w("LA"), w("E"), -1.0 / 16.0, None, ALU.mult), reads=[k("E")], writes=[k("LA")])
                S.op("dve", lambda e, w=w, T=T: e.tensor_tensor_scan(w("B"), RST[:, :T], w("LA"), 0.0, ALU.mult, ALU.add), reads=["RST", k("LA")], writes=[k("B")])
                S.op("act", lambda e, b=b, w3=w3, nchk=nchk: e.activation(DEC[b][:, :nchk], w3("B")[:, :, 63], AF.Exp), reads=[k("B")], writes=["DEC%d" % b])
                S.op("act", lambda e, w=w: e.activation(w("E"), w("B"), AF.Exp), reads=[k("B")], writes=[k("E")])
                S.op("dve", lambda e, w=w: e.scalar_tensor_tensor(w("QE"), w("Q"), QSC, w("E"), ALU.mult, ALU.mult), reads=[k("Q"), k("E")], writes=[k("QE")])
                S.op("dve", lambda e, w3=w3, nchk=nchk: e.tensor_tensor(w3("D"), w3("B"), w3("B")[:, :, 32:33].to_broadcast([128, nchk, 64]), ALU.subtract),
                     reads=[k("B")], writes=[k("D")])
                S.op("act", lambda e, w=w: e.activation(w("E"), w("D"), AF.Exp), reads=[k("D"), k("QE")], writes=[k("E")])
                S.op("dve", lambda e, w=w: e.scalar_tensor_tensor(w("QS"), w("Q"), QSC, w("E"), ALU.mult, ALU.mult), reads=[k("Q"), k("E")], writes=[k("QS")])
                S.op("act", lambda e, w=w: e.activation(w("E"), w("D"), AF.Exp, scale=-1.0), reads=[k("D"), k("QS")], writes=[k("E")])
                S.op("dve", lambda e, w=w: e.tensor_tensor(w("LA"), w("K"), w("E"), ALU.mult), reads=[k("K"), k("E"), k("B")], writes=[k("LA")])
                for h in range(4):
                    S.op("pool", lambda e, w=w, h=h: e.tensor_scalar(w("KS%d" % h), w("LA"), HM[:, h:h + 1], None, ALU.mult),
                         reads=[k("LA"), "HM"], writes=[k("KS%d" % h)])
                S.op("dve", lambda e, w3=w3, nchk=nchk: e.tensor_tensor(w3("D"), w3("B")[:, :, 63:64].to_broadcast([128, nchk, 64]), w3("B"), ALU.subtract),
                     reads=[k("B"), k("E"), k("LA")], writes=[k("D")])
                S.op("act", lambda e, w=w: e.activation(w("D"), w("D"), AF.Exp), reads=[k("D")], writes=[k("D")])
                S.op("dve", lambda e, w=w: e.tensor_tensor(w("KD"), w("K"), w("D"), ALU.mult), reads=[k("K"), k("D")], writes=[k("KD")])
                for c in range(nchk):
                    p2 = gc % 2
                    gc += 1
                    cs = slice(c * 64, (c + 1) * 64)
                    S.op("pe", lambda e, b=b, cs=cs, p2=p2: e.transpose(pt[p2][:], Wb["KD", b][:, cs], ID[:]), reads=[k("KD"), "ID"], writes=["pt"])
                    S.op("act", lambda e, p2=p2: e.copy(KDT[p2][:], pt[p2][:]), reads=["pt"], writes=["KDT%d" % p2])
                    S.op("pe", lambda e, b=b, c=c, p2=p2: e.matmul(pkv[p2][:], KDT[p2][:], VV[b][:, c, :], start=True, stop=True),
                         reads=["KDT%d" % p2, "VV%d" % b], writes=["pkv%d" % p2])
                    for h in range(4):
                        S.op("pe", lambda e, b=b, cs=cs, p2=p2, h=h: e.matmul(psc[p2][:, h * 64:(h + 1) * 64], Wb["KS%d" % h, b][:, cs], Wb["QS", b][:, cs],
                                                                            start=True, stop=True),
                             reads=[k("KS%d" % h), k("QS")], writes=["psc%d" % p2])
                    S.op("dve", lambda e, p2=p2: e.tensor_tensor(STt[p2][:], psc[p2][:], TM[:], ALU.mult), reads=["psc%d" % p2, "TM"], writes=["ST%d" % p2])
                    for h in range(4):
                        hs = slice(h * 64, (h + 1) * 64)
                        S.op("pe", lambda e, b=b, c=c, p2=p2, hs=hs: e.matmul(po[p2][:, hs], STt[p2][:, hs], VV[b][:, c, hs], start=True, stop=False),
                             reads=["ST%d" % p2, "VV%d" % b], writes=["po%d" % p2])
                        S.op("pe", lambda e, b=b, cs=cs, p2=p2, hs=hs, scur=scur: e.matmul(po[p2][:, hs], Wb["QE", b][:, cs], SS[scur][:, hs], start=False, stop=True),
                             reads=[k("QE"), "SS%d" % scur], writes=["po%d" % p2])
                    ob = oi % 3
                    oi += 1
                    S.op("act", lambda e, ob=ob, p2=p2: e.copy(OB[ob][:], po[p2][:]), reads=["po%d" % p2], writes=["OB%d" % ob])
                    S.dma("sp" if oi % 2 else "act", lambda e, d=d, ob=ob, t0=t0, c=c: e.dma_start(out=I["o", d][t0 + c * 64:t0 + (c + 1) * 64, :], in_=OB[ob][:]),
                          reads=["OB%d" % ob], writes=["o_%d" % oi])
                    S.op("dve", lambda e, p2=p2: e.tensor_tensor(KVM[:], pkv[p2][:], BLK[:], ALU.mult), reads=["pkv%d" % p2, "BLK"], writes=["KVM"])
                    S.op("dve", lambda e, b=b, c=c, scur=scur: e.scalar_tensor_tensor(SS[1 - scur][:], SS[scur][:], DEC[b][:, c:c + 1], KVM[:], ALU.mult, ALU.add),
                         reads=["SS%d" % scur, "DEC%d" % b, "KVM"], writes=["SS%d" % (1 - scur)])
                    scur = 1 - scur
        S.drain_all("sp")
        S.emit()
    return nc


NEXP = 16384


def build_PD(ntok, nctx, last):
    nc = bass.Bass("TRN2", target_bir_lowering=False)
    def din(name, shape, dt=F32):
        return nc.dram_tensor(name, shape, dt, kind="ExternalInput").ap()
    xT = din("xT", [D, ntok]); modT = din("modT", [128, 96])
    su = din("su", [ntok, 256]); sv = din("sv", [ntok, 256]); gg = din("gg", [ntok, 512])
    s5u = din("s5u", [256, ntok]); yf = din("yf", [256, ntok]); yb = din("yb", [256, ntok])
    of_ = din("of", [ntok, 512]); ob_ = din("ob", [ntok, 512])
    w_out = din("w_out", [D, D]); wsT = din("wsT", [128, 512]); sgub = din("sgub", [128, 4]); s5d = din("s5d", [128, 2])
    wglu = din("wglu", [256, 512]); ng = din("ng", [128, 512]); g2 = din("g2", [128, 8]); wq = din("wq", [D, 2048])
    keysT = din("keysT", [128, 2048]); eu = din("eu", [NEXP, D]); ev = din("ev", [NEXP, D]); gfin = din("gfin", [128, 8])
    ones = din("ones", [128, 128]); ident = din("ident", [128, 128]); iota16 = din("iota16", [128, 16])
    xo = nc.dram_tensor("xo", [D, ntok], F32, kind="ExternalOutput").ap()
    nb = ntok // 128
    with ExitStack() as st:
        S = Sched(nc, st)
        sb, ps = _mk(nc, st)
        MOD = sb("MOD", [128, 96]); WOUT = sb("WOUT", [128, 8, D]); WS = sb("WS", [128, 512]); SGUB = sb("SGUB", [128, 4]); S5D = sb("S5D", [128, 2])
        WGLU = sb("WGLU", [128, 2, 512]); NG = sb("NG", [128, 512]); G2 = sb("G2", [128, 8]); WQ = sb("WQ", [128, 8, 2048]); KEYS = sb("KEYS", [128, 2048])
        GF = sb("GF", [128, 8]); ONES = sb("ONES", [128, 128]); ID = sb("ID", [128, 128]); IOTA = sb("IOTA", [128, 16])
        A2 = sb("A2", [128, 16]); TA = sb("TA", [128, 16])
        U_ = sb("U_", [128, 256]); V_ = sb("V_", [128, 256]); GU = sb("GU", [128, 256]); GV = sb("GV", [128, 256]); SQ = sb("SQ", [128, 512])
        SS = sb("SS", [128, 8]); VN = sb("VN", [128, 256]); MIX = sb("MIX", [128, D])
        S5U = sb("S5U", [128, 2, 128]); YF = sb("YF", [128, 2, 128]); YB = sb("YB", [128, 2, 128]); GE = sb("GE", [128, 2, 128]); SG = sb("SG", [128, 256])
        OF = sb("OF", [128, 512]); OB = sb("OB", [128, 512]); GG = sb("GG", [128, 512]); SL = sb("SL", [128, 512])
        XT = sb("XT", [128, 8, 128]); X1 = sb("X1", [128, 8, 128]); XO = XT; HT = sb("HT", [128, 8, 128]); MIXT = HT; HTOK = sb("HTOK", [128, D])
        RSTD = sb("RSTD", [128, 128]); TMPB = sb("TMPB", [128, 128])
        SC = sb("SC", [128, 2048])
        M16 = sb("M16", [128, 256]); I16 = sb("I16", [128, 256], U32); IF16 = sb("IF16", [128, 256]); I1S = sb("I1S", [128, 128])
        CS = sb("CS", [128, 2048]); SC2 = CS; QT = CS[:].rearrange("p (q t) -> p q t", t=128); CS2 = sb("CS2", [128, 256])
        T16 = sb("T16", [128, 128]); P16 = sb("P16", [128, 128], U32); PF = sb("PF", [128, 128]); AI = sb("AI", [128, 128], I32)
        AFL = sb("AFL", [128, 128]); BFL = sb("BFL", [128, 128]); E1 = sb("E1", [128, 128]); E2 = sb("E2", [128, 128])
        EG = sb("EG", [128, 128]); GATE = sb("GATE", [128, 128]); IDXTI = sb("IDXTI", [128, 128], I32); GATET = sb("GATET", [128, 128])
        ACTT = sb("ACTT", [128, 128]); WT = sb("WT", [128, 128])
        UG = [sb("UG%d" % i, [128, D]) for i in range(2)]; VG = UG
        HB = [sb("HB%d" % i, [128, D]) for i in range(2)]
        P0 = ps("P0", [128, 2048]); P1 = ps("P1", [128, 1024]); P2 = ps("P2", [128, 512]); P3 = ps("P3", [128, 512])

        def V(fn, r=(), w=()):
            S.op("dve", fn, reads=r, writes=w)

        def A(fn, r=(), w=()):
            S.op("act", fn, reads=r, writes=w)

        def PE(fn, r=(), w=()):
            S.op("pe", fn, reads=r, writes=w)

        def LD(q, t, src, key):
            S.dma(q, lambda e: e.dma_start(out=t, in_=src), writes=[key])

        LD("sp", MOD[:], modT, "MOD"); LD("sp", WS[:], wsT, "WS"); LD("sp", SGUB[:], sgub, "SGUB"); LD("sp", S5D[:], s5d, "S5D")
        LD("sp", WGLU[:], wglu.rearrange("(c p) f -> p c f", p=128), "WGLU"); LD("sp", NG[:], ng, "NG"); LD("sp", G2[:], g2, "G2")
        LD("sp", KEYS[:], keysT, "KEYS"); LD("sp", GF[:], gfin, "GF"); LD("sp", ONES[:], ones, "ONES"); LD("sp", ID[:], ident, "ID"); LD("sp", IOTA[:], iota16, "IOTA")
        wo_v = w_out.rearrange("(k p) f -> p k f", p=128)
        wq_v = wq.rearrange("(k p) f -> p k f", p=128)
        for k in range(8):
            LD("act", WOUT[:, k, :], wo_v[:, k, :], "WOUT")
            LD("act", WQ[:, k, :], wq_v[:, k, :], "WQ")
        MOD3 = MOD[:].rearrange("p (j c) -> p j c", c=2)
        V(lambda e: e.tensor_scalar(TA[:].rearrange("p (j c) -> p j c", c=2), MOD3[:, 32:40, :], 1.0, None, ALU.add), ["MOD"], ["TA"])
        V(lambda e: e.tensor_tensor(A2[:].rearrange("p (j c) -> p j c", c=2), TA[:].rearrange("p (j c) -> p j c", c=2),
                                    G2[:].unsqueeze(2).to_broadcast([128, 8, 2]), ALU.mult), ["TA", "G2"], ["A2"])
        A23 = A2[:].rearrange("p (j c) -> p j c", c=2)

        def rs_from_ss(ss, n, scale):
            V(lambda e: e.tensor_scalar(ss, ss, scale, EPS, ALU.mult, ALU.add), ["SS"], ["SS"])
            A(lambda e: e.sqrt(ss, ss), ["SS"], ["SS"])
            V(lambda e: e.reciprocal(ss, ss), ["SS"], ["SS"])

        def top16(src, scratch, mout, iout, n):
            V(lambda e: e.max(mout[:, 0:8], src), ["TK"], ["TK"])
            V(lambda e: e.max_index(iout[:, 0:8], mout[:, 0:8], src), ["TK"], ["TK"])
            V(lambda e: e.match_replace(scratch, mout[:, 0:8], src, -1e30), ["TK"], ["TK"])
            V(lambda e: e.max(mout[:, 8:16], scratch), ["TK"], ["TK"])
            V(lambda e: e.max_index(iout[:, 8:16], mout[:, 8:16], scratch), ["TK"], ["TK"])

        xT_v = xT.rearrange("(k p) t -> p k t", p=128)
        xo_v = xo.rearrange("(k p) t -> p k t", p=128)
        s5u_v = s5u.rearrange("(c p) t -> p c t", p=128)
        yf_v = yf.rearrange("(c p) t -> p c t", p=128)
        yb_v = yb.rearrange("(c p) t -> p c t", p=128)
        for bi in range(nb):
            tk = slice(bi * 128, (bi + 1) * 128)
            col = 1 if bi * 128 < nctx else 0
            LD("sp", U_[:], su[tk, :], "U_"); LD("sp", V_[:], sv[tk, :], "V_"); LD("sp", GG[:], gg[tk, :], "GG")
            LD("act", S5U[:], s5u_v[:, :, tk], "S5U"); LD("act", YF[:], yf_v[:, :, tk], "YF"); LD("act", YB[:], yb_v[:, :, tk], "YB")
            LD("sp", OF[:], of_[tk, :], "OF"); LD("sp", OB[:], ob_[tk, :], "OB"); LD("act", XT[:], xT_v[:, :, tk], "XT")
            A(lambda e: e.activation(GU[:], U_[:], AF.Gelu), ["U_"], ["GU"])
            A(lambda e: e.activation(GV[:], V_[:], AF.Gelu), ["V_"], ["GV"])
            V(lambda e: e.tensor_tensor(SQ[:, 0:256], GV[:], GV[:], ALU.mult), ["GV"], ["SQ"])
            V(lambda e: e.tensor_reduce(SS[:, 0:4], SQ[:, 0:256].rearrange("p (h d) -> p h d", d=64), AX.X, ALU.add), ["SQ"], ["SS"])
            rs_from_ss(SS[:, 0:4], 4, 1.0 / 64)
            V(lambda e: e.tensor_tensor(VN[:].rearrange("p (h d) -> p h d", d=64), GV[:].rearrange("p (h d) -> p h d", d=64),
                                        SS[:, 0:4].unsqueeze(2).to_broadcast([128, 4, 64]), ALU.mult), ["GV", "SS"], ["VN"])
            for h in range(4):
                PE(lambda e, h=h: e.matmul(P2[:, h * 64:(h + 1) * 64], WS[:, h * 128:(h + 1) * 128], VN[:, h * 64:(h + 1) * 64], start=True, stop=True),
                   ["WS", "VN"], ["P2"])
            V(lambda e: e.tensor_tensor(MIX[:, 0:256].rearrange("p (h d) -> p h d", d=64), P2[:, 0:256].rearrange("p (h d) -> p h d", d=64),
                                        SGUB[:].unsqueeze(2).to_broadcast([128, 4, 64]), ALU.add), ["P2", "SGUB"], ["MIXa"])
            V(lambda e: e.tensor_tensor(MIX[:, 0:256], MIX[:, 0:256], GU[:], ALU.mult), ["GU"], ["MIXa"])
            V(lambda e: e.tensor_tensor(YF[:], YF[:], YB[:], ALU.add), ["YB"], ["YF"])
            for ct in range(2):
                V(lambda e, ct=ct: e.scalar_tensor_tensor(YF[:, ct, :], S5U[:, ct, :], S5D[:, ct:ct + 1], YF[:, ct, :], ALU.mult, ALU.add),
                  ["S5U", "S5D"], ["YF"])
            A(lambda e: e.activation(GE[:], YF[:], AF.Gelu), ["YF"], ["GE"])
            for ct in range(2):
                PE(lambda e, ct=ct: e.matmul(P3[:, 0:512], GE[:, ct, :], WGLU[:, ct, :], start=(ct == 0), stop=(ct == 1)), ["GE", "WGLU"], ["P3"])
            A(lambda e: e.activation(SG[:], P3[:, 256:512], AF.Sigmoid), ["P3"], ["SG"])
            V(lambda e: e.tensor_tensor(MIX[:, 256:512], P3[:, 0:256], SG[:], ALU.mult), ["P3", "SG"], ["MIXb"])
            V(lambda e: e.tensor_tensor(OF[:], OF[:], OB[:], ALU.add), ["OB"], ["OF"])
            V(lambda e: e.tensor_tensor(SQ[:], OF[:], OF[:], ALU.mult), ["OF"], ["SQ"])
            V(lambda e: e.tensor_reduce(SS[:, 0:8], SQ[:].rearrange("p (h d) -> p h d", d=64), AX.X, ALU.add), ["SQ"], ["SS"])
            rs_from_ss(SS[:, 0:8], 8, 1.0 / 64)
            V(lambda e: e.tensor_tensor(OF[:].rearrange("p (h d) -> p h d", d=64), OF[:].rearrange("p (h d) -> p h d", d=64),
                                        SS[:, 0:8].unsqueeze(2).to_broadcast([128, 8, 64]), ALU.mult), ["SS"], ["OF"])
            V(lambda e: e.tensor_tensor(OF[:], OF[:], NG[:], ALU.mult), ["NG"], ["OF"])
            A(lambda e: e.activation(SL[:], GG[:], AF.Silu), ["GG"], ["SL"])
            V(lambda e: e.tensor_tensor(MIX[:, 512:1024], OF[:], SL[:], ALU.mult), ["OF", "SL"], ["MIXc"])
            for f in range(8):
                pp, pk = (P2, "P2") if f % 2 == 0 else (P3, "P3")
                PE(lambda e, f=f, pp=pp: e.transpose(pp[:, 0:128], MIX[:, f * 128:(f + 1) * 128], ID[:]), ["MIXa", "MIXb", "MIXc", "ID"], [pk])
                A(lambda e, f=f, pp=pp: e.copy(MIXT[:, f, :], pp[:, 0:128]), [pk], ["HT"])
            for ot in range(8):
                pp, pk = (P2, "P2") if ot % 2 == 0 else (P3, "P3")
                for k in range(8):
                    PE(lambda e, ot=ot, k=k, pp=pp: e.matmul(pp[:, 0:128], WOUT[:, k, ot * 128:(ot + 1) * 128], MIXT[:, k, :], start=(k == 0), stop=(k == 7)),
                       ["WOUT", "HT"], [pk])
                V(lambda e, ot=ot, pp=pp, col=col: e.scalar_tensor_tensor(X1[:, ot, :], pp[:, 0:128], MOD3[:, 16 + ot, col:col + 1], XT[:, ot, :], ALU.mult, ALU.add),
                  [pk, "MOD", "XT"], ["X1"])
            for k in range(8):
                A(lambda e, k=k: e.activation(TMPB[:], X1[:, k, :], AF.Square), ["X1"], ["TMPB"])
                PE(lambda e, k=k: e.matmul(P2[:, 0:128], ONES[:], TMPB[:], start=(k == 0), stop=(k == 7)), ["ONES", "TMPB"], ["P2"])
            V(lambda e: e.tensor_scalar(RSTD[:], P2[:, 0:128], 1.0 / D, EPS, ALU.mult, ALU.add), ["P2"], ["RSTD"])
            A(lambda e: e.sqrt(RSTD[:], RSTD[:]), ["RSTD"], ["RSTD"])
            V(lambda e: e.reciprocal(RSTD[:], RSTD[:]), ["RSTD"], ["RSTD"])
            for k in range(8):
                V(lambda e, k=k: e.tensor_tensor(TMPB[:], X1[:, k, :], RSTD[:], ALU.mult), ["X1", "RSTD"], ["TMPB"])
                A(lambda e, k=k, col=col: e.activation(HT[:, k, :], TMPB[:], AF.Identity, bias=MOD3[:, 24 + k, col:col + 1], scale=A23[:, k, col:col + 1]),
                  ["TMPB", "MOD", "A2"], ["HT"])
            for k in range(8):
                pp, pk = (P2, "P2") if k % 2 == 0 else (P3, "P3")
                PE(lambda e, k=k, pp=pp: e.transpose(pp[:, 0:128], HT[:, k, :], ID[:]), ["HT", "ID"], [pk])
                A(lambda e, k=k, pp=pp: e.copy(HTOK[:, k * 128:(k + 1) * 128], pp[:, 0:128]), [pk], ["HTOK"])
            for qt in range(16):
                pp, pk = (P2, "P2") if qt % 2 == 0 else (P3, "P3")
                for k in range(8):
                    PE(lambda e, qt=qt, k=k, pp=pp: e.matmul(pp[:, 0:128], WQ[:, k, qt * 128:(qt + 1) * 128], HT[:, k, :], start=(k == 0), stop=(k == 7)),
                       ["WQ", "HT"], [pk])
                if qt % 2 == 0:
                    A(lambda e, qt=qt, pp=pp: e.copy(QT[:, qt, :], pp[:, 0:128]), [pk], ["TK"])
                else:
                    V(lambda e, qt=qt, pp=pp: e.tensor_copy(QT[:, qt, :], pp[:, 0:128]), [pk], ["TK"])
            for qt in range(16):
                PE(lambda e, qt=qt: e.matmul(P0[:, qt * 128:(qt + 1) * 128], QT[:, qt, :], KEYS[:, qt * 128:(qt + 1) * 128], start=True, stop=True),
                   ["TK", "KEYS"], ["P0a", "P0b"])
            for q4 in range(4):
                A(lambda e, q4=q4: e.copy(SC[:, q4 * 512:(q4 + 1) * 512], P0[:, q4 * 512:(q4 + 1) * 512]), ["P0a", "P0b"], ["TK"])
            for qt in range(16):
                top16(SC[:, qt * 128:(qt + 1) * 128], SC2[:, qt * 128:(qt + 1) * 128], M16[:, qt * 16:(qt + 1) * 16], I16[:, qt * 16:(qt + 1) * 16], 128)
            V(lambda e: e.tensor_copy(IF16[:], I16[:]), ["TK"], ["TK"])
            M4 = M16[:].rearrange("p (h q k) -> p h q k", q=2, k=16)
            IF4 = IF16[:].rearrange("p (h q k) -> p h q k", q=2, k=16)
            I1S3 = I1S[:].rearrange("p (h k) -> p h k", k=16)
            V(lambda e: e.tensor_scalar(I1S3, IF4[:, :, 0, :], 128.0, None, ALU.mult), ["TK"], ["TK"])
            CS4 = CS[:].rearrange("p (h a b) -> p h a b", a=16, b=16)
            V(lambda e: e.tensor_tensor(CS4, M4[:, :, 0, :].unsqueeze(3).to_broadcast([128, 8, 16, 16]),
                                        M4[:, :, 1, :].unsqueeze(2).to_broadcast([128, 8, 16, 16]), ALU.add), ["TK"], ["TK"])
            for h in range(8):
                top16(CS[:, h * 256:(h + 1) * 256], CS2[:], T16[:, h * 16:(h + 1) * 16], P16[:, h * 16:(h + 1) * 16], 256)
            V(lambda e: e.tensor_copy(PF[:], P16[:]), ["TK"], ["TK"])
            V(lambda e: e.tensor_scalar(AFL[:], PF[:], -7.5, 1.0 / 16, ALU.add, ALU.mult), ["TK"], ["TK"])
            V(lambda e: e.tensor_copy(AI[:], AFL[:]), ["TK"], ["TK"])
            V(lambda e: e.tensor_copy(AFL[:], AI[:]), ["TK"], ["TK"])
            V(lambda e: e.scalar_tensor_tensor(BFL[:], AFL[:], -16.0, PF[:], ALU.mult, ALU.add), ["TK"], ["TK"])
            EQ4 = CS[:].rearrange("p (h k a) -> p h k a", k=16, a=16)
            io4 = IOTA[:].unsqueeze(1).unsqueeze(1).to_broadcast([128, 8, 16, 16])
            for (sel, src, dst) in ((AFL, I1S3, E1), (BFL, IF4[:, :, 1, :], E2)):
                V(lambda e, sel=sel: e.tensor_tensor(EQ4, io4, sel[:].rearrange("p (h k) -> p h k", k=16).unsqueeze(3).to_broadcast([128, 8, 16, 16]), ALU.is_equal),
                  ["TK", "IOTA"], ["TK"])
                V(lambda e, src=src: e.tensor_tensor(EQ4, EQ4, src.unsqueeze(2).to_broadcast([128, 8, 16, 16]), ALU.mult), ["TK"], ["TK"])
                V(lambda e, dst=dst: e.tensor_reduce(dst[:], CS[:].rearrange("p (m a) -> p m a", a=16), AX.X, ALU.add), ["TK"], ["TK"])
            V(lambda e: e.tensor_tensor(E1[:], E1[:], E2[:], ALU.add), ["TK"], ["TK"])
            T3 = T16[:].rearrange("p (h k) -> p h k", k=16)
            V(lambda e: e.tensor_tensor(EG[:].rearrange("p (h k) -> p h k", k=16), T3, T3[:, :, 0:1].to_broadcast([128, 8, 16]), ALU.subtract), ["TK"], ["TK"])
            A(lambda e: e.activation(EG[:], EG[:], AF.Exp), ["TK"], ["TK"])
            V(lambda e: e.tensor_reduce(SS[:, 0:8], EG[:].rearrange("p (h k) -> p h k", k=16), AX.X, ALU.add), ["TK"], ["SS"])
            V(lambda e: e.reciprocal(SS[:, 0:8], SS[:, 0:8]), ["SS"], ["SS"])
            V(lambda e: e.tensor_tensor(GATE[:].rearrange("p (h k) -> p h k", k=16), EG[:].rearrange("p (h k) -> p h k", k=16),
                                        SS[:, 0:8].unsqueeze(2).to_broadcast([128, 8, 16]), ALU.mult), ["TK", "SS"], ["GATE"])
            PE(lambda e: e.transpose(P2[:, 0:128], E1[:], ID[:]), ["TK", "ID"], ["P2"])
            V(lambda e: e.tensor_copy(IDXTI[:], P2[:, 0:128]), ["P2"], ["IDXTI"])
            PE(lambda e: e.transpose(P3[:, 0:128], GATE[:], ID[:]), ["GATE", "ID"], ["P3"])
            A(lambda e: e.copy(GATET[:], P3[:, 0:128]), ["P3"], ["GATET"])
            for t in range(128):
                b = t % 2
                S.dma("pool", lambda e, t=t, b=b: e.indirect_dma_start(out=UG[b][:], out_offset=None, in_=eu,
                                                                       in_offset=bass.IndirectOffsetOnAxis(ap=IDXTI[:, t:t + 1], axis=0)),
                      reads=["IDXTI"], writes=["UG%d" % b])
                pk = "P0a" if b == 0 else "P0b"
                for hf in range(2):
                    PE(lambda e, t=t, b=b, hf=hf: e.matmul(P0[:, b * 1024 + hf * 512:b * 1024 + (hf + 1) * 512], ID[:, t:t + 1].to_broadcast([128, 128]),
                                                           HTOK[:, hf * 512:(hf + 1) * 512], start=True, stop=True), ["ID", "HTOK"], [pk])
                    A(lambda e, b=b, hf=hf: e.copy(HB[b][:, hf * 512:(hf + 1) * 512], P0[:, b * 1024 + hf * 512:b * 1024 + (hf + 1) * 512]), [pk], ["HB%d" % b])
                V(lambda e, t=t, b=b: e.scalar_tensor_tensor(UG[b][:], UG[b][:], 1.0, HB[b][:], ALU.mult, ALU.mult, accum_out=ACTT[:, t:t + 1]),
                  ["HB%d" % b], ["UG%d" % b, "ACTT"])
            A(lambda e: e.activation(WT[:], ACTT[:], AF.Gelu), ["ACTT"], ["WT"])
            V(lambda e: e.tensor_tensor(WT[:], WT[:], GATET[:], ALU.mult), ["GATET"], ["WT"])
            for t in range(128):
                b = t % 2
                S.dma("pool", lambda e, t=t, b=b: e.indirect_dma_start(out=VG[b][:], out_offset=None, in_=ev,
                                                                       in_offset=bass.IndirectOffsetOnAxis(ap=IDXTI[:, t:t + 1], axis=0)),
                      reads=["IDXTI"], writes=["UG%d" % b])
                for ot in range(8):
                    PE(lambda e, t=t, b=b, ot=ot: e.matmul(P1[:, ot * 128 + t:ot * 128 + t + 1], VG[b][:, ot * 128:(ot + 1) * 128], WT[:, t:t + 1],
                                                           start=True, stop=True), ["UG%d" % b, "WT"], ["P1"])
            for ot in range(8):
                V(lambda e, ot=ot, col=col: e.scalar_tensor_tensor(XO[:, ot, :], P1[:, ot * 128:(ot + 1) * 128], MOD3[:, 40 + ot, col:col + 1], X1[:, ot, :],
                                                                   ALU.mult, ALU.add), ["P1", "MOD", "X1"], ["XT"])
            if last:
                for k in range(8):
                    A(lambda e, k=k: e.activation(TMPB[:], XO[:, k, :], AF.Square), ["XT"], ["TMPB"])
                    PE(lambda e, k=k: e.matmul(P2[:, 0:128], ONES[:], TMPB[:], start=(k == 0), stop=(k == 7)), ["ONES", "TMPB"], ["P2"])
                V(lambda e: e.tensor_scalar(RSTD[:], P2[:, 0:128], 1.0 / D, EPS, ALU.mult, ALU.add), ["P2"], ["RSTD"])
                A(lambda e: e.sqrt(RSTD[:], RSTD[:]), ["RSTD"], ["RSTD"])
                V(lambda e: e.reciprocal(RSTD[:], RSTD[:]), ["RSTD"], ["RSTD"])
                for k in range(8):
                    V(lambda e, k=k: e.scalar_tensor_tensor(XO[:, k, :], XO[:, k, :], GF[:, k:k + 1], RSTD[:], ALU.mult, ALU.mult), ["RSTD", "GF"], ["XT"])
            S.dma("sp", lambda e, tk=tk: e.dma_start(out=xo_v[:, :, tk], in_=XO[:]), reads=["XT"], writes=["xo_%d" % bi])
        S.drain_all("sp")
        S.emit()
    return nc


SEQC = 256
SEQL = 4096
OWN = 2176


def _mirror(t0, T):
    if t0 < SEQC:
        return 0, SEQC
    i = (t0 - SEQC) // 512
    return SEQC + SEQL - 512 * (i + 1), 512


def emit_A(nc, S, sb, ps, io, tl_all=False):
    xT, cT, w_mod, b_mod, g1, w_in, ones = io["xT"], io["cT"], io["w_mod"], io["b_mod"], io["g1"], io["w_in"], io["ones"]
    FMU, FMQ, FMK, FMZ, VT, TL, MODS = io["FMU"], io["FMQ"], io["FMK"], io["FMZ"], io["VT"], io["TL"], io["MODS"]
    CT = sb("CT", [128, 16]); SC = sb("SC", [128, 16]); BM = sb("BM", [128, 48]); G1 = sb("G1", [128, 8])
    ONES = sb("ONES", [128, 128]); MOD = sb("MOD", [128, 96])
    A1 = sb("A1", [128, 16]); TMPA = sb("TMPA", [128, 16])
    WM = [sb("WM%d" % i, [128, 8, 512]) for i in range(2)]
    WIN = sb("WIN", [128, 8, INW])
    XT = [sb("XT%d" % i, [128, 8, 512]) for i in range(2)]
    XSQ = sb("XSQ", [128, 512]); RSTD = sb("RSTD", [128, 512]); TMP = sb("TMP", [128, 512])
    HT = [sb("HT%d" % i, [128, 8, 512]) for i in range(2)]
    OUTB = [sb("OUTB%d" % i, [128, 512]) for i in range(4)]
    pmod = ps("pmod", [128, 96]); pss = ps("pss", [128, 512])
    pout = [ps("pout%d" % i, [128, 512]) for i in range(3)]
    S.dma("sp", lambda e: e.dma_start(out=CT[:], in_=cT), writes=["CT"])
    S.dma("sp", lambda e: e.dma_start(out=BM[:], in_=b_mod), writes=["BM"])
    S.dma("sp", lambda e: e.dma_start(out=G1[:], in_=g1), writes=["G1"])
    S.dma("sp", lambda e: e.dma_start(out=ONES[:], in_=ones), writes=["ONES"])
    S.op("act", lambda e: e.activation(SC[:], CT[:], AF.Silu), reads=["CT"], writes=["SC"])
    SC3 = SC[:].rearrange("p (k c) -> p k c", c=2)
    wm_v = w_mod.rearrange("(k p) f -> p k f", p=128)
    for jg in range(12):
        b = jg % 2
        S.dma("act" if jg % 2 else "sp",
              lambda e, jg=jg, b=b: e.dma_start(out=WM[b][:], in_=wm_v[:, :, jg * 512:(jg + 1) * 512]), writes=["WM%d" % b])
        for j8 in range(4):
            j = jg * 4 + j8
            for k in range(8):
                S.op("pe", lambda e, j=j, j8=j8, k=k, b=b: e.matmul(
                    pmod[:, 2 * j:2 * j + 2], WM[b][:, k, j8 * 128:(j8 + 1) * 128], SC3[:, k, :],
                    start=(k == 0), stop=(k == 7)), reads=["WM%d" % b, "SC"], writes=["pmod"])
    S.op("dve", lambda e: e.tensor_tensor(MOD[:].rearrange("p (j c) -> p j c", c=2), pmod[:].rearrange("p (j c) -> p j c", c=2),
                                          BM[:].unsqueeze(2).to_broadcast([128, 48, 2]), ALU.add), reads=["pmod", "BM"], writes=["MOD"])
    S.dma("sp", lambda e: e.dma_start(out=MODS, in_=MOD[:]), reads=["MOD"], writes=["MODS"])
    MOD3 = MOD[:].rearrange("p (j c) -> p j c", c=2)
    S.op("dve", lambda e: e.tensor_scalar(TMPA[:].rearrange("p (j c) -> p j c", c=2), MOD3[:, 8:16, :], 1.0, None, ALU.add), reads=["MOD"], writes=["TMPA"])
    S.op("dve", lambda e: e.tensor_tensor(A1[:].rearrange("p (j c) -> p j c", c=2), TMPA[:].rearrange("p (j c) -> p j c", c=2),
                                          G1[:].unsqueeze(2).to_broadcast([128, 8, 2]), ALU.mult), reads=["TMPA", "G1"], writes=["A1"])
    A13 = A1[:].rearrange("p (j c) -> p j c", c=2)
    win_v = w_in.rearrange("(k p) f -> p k f", p=128)
    for k in range(8):
        S.dma("pool", lambda e, k=k: e.dma_start(out=WIN[:, k, :], in_=win_v[:, k, :]), writes=["WIN%d" % k])
    WK = ["WIN%d" % k for k in range(8)]
    xT_v = xT.rearrange("(k p) t -> p k t", p=128)
    grp = [(0, SEQC, 1, -1)] + [(SEQC + 512 * g, 512, 0, g) for g in range(8)]
    cnt = {"o": 0}

    def evac(dst_ap_fn, pb, m, tn, key, cm=False):
        ob = cnt["o"] % 4
        cnt["o"] += 1
        if cm:
            o_ap = lambda: OUTB[ob][:m, :512].rearrange("p (w r) -> p w r", r=8)
            i_ap = lambda: pout[pb][:m, :512].rearrange("p (r w) -> p w r", w=64)
        else:
            o_ap = lambda: OUTB[ob][:m, :tn]
            i_ap = lambda: pout[pb][:m, :tn]
        if cnt["o"] % 2:
            S.op("act", lambda e: e.copy(o_ap(), i_ap()), reads=["pout%d" % pb], writes=["OUTB%d" % ob])
        else:
            S.op("dve", lambda e: e.tensor_copy(o_ap(), i_ap()), reads=["pout%d" % pb], writes=["OUTB%d" % ob])
        S.dma("sp" if cnt["o"] % 2 else "act", lambda e: dst_ap_fn(e, OUTB[ob]), reads=["OUTB%d" % ob], writes=["%s_%d" % (key, cnt["o"])])

    pi = 0
    for gi, (t0, tn, col, g) in enumerate(grp):
        b = gi % 2
        xk, hk = "XT%d" % b, "HT%d" % b
        S.dma("sp", lambda e, b=b, t0=t0, tn=tn: e.dma_start(out=XT[b][:, :, :tn], in_=xT_v[:, :, t0:t0 + tn]), writes=[xk])
        for k in range(8):
            S.op("act", lambda e, b=b, k=k, tn=tn: e.activation(XSQ[:, :tn], XT[b][:, k, :tn], AF.Square), reads=[xk], writes=["XSQ"])
            S.op("pe", lambda e, k=k, tn=tn: e.matmul(pss[:, :tn], ONES[:], XSQ[:, :tn], start=(k == 0), stop=(k == 7)), reads=["ONES", "XSQ"], writes=["pss"])
        S.op("dve", lambda e, tn=tn: e.tensor_scalar(RSTD[:, :tn], pss[:, :tn], 1.0 / D, EPS, ALU.mult, ALU.add), reads=["pss"], writes=["RSTD"])
        S.op("act", lambda e, tn=tn: e.sqrt(RSTD[:, :tn], RSTD[:, :tn]), reads=["RSTD"], writes=["RSTD"])
        S.op("dve", lambda e, tn=tn: e.reciprocal(RSTD[:, :tn], RSTD[:, :tn]), reads=["RSTD"], writes=["RSTD"])
        for k in range(8):
            S.op("dve", lambda e, b=b, k=k, tn=tn: e.tensor_tensor(TMP[:, :tn], XT[b][:, k, :tn], RSTD[:, :tn], ALU.mult), reads=[xk, "RSTD"], writes=["TMP"])
            S.op("act", lambda e, b=b, k=k, tn=tn, col=col: e.activation(HT[b][:, k, :tn], TMP[:, :tn], AF.Identity, bias=MOD3[:, k, col:col + 1],
                                                                         scale=A13[:, k, col:col + 1]), reads=["TMP", "MOD", "A1"], writes=[hk])
        fm = [(FMU, 0, 512, 128), (FMU, 128, 640, 128), (FMQ, 0, 768, 128), (FMQ, 128, 896, 128), (FMK, 0, 1024, 128), (FMK, 128, 1152, 128), (FMZ, 0, 2304, 32)]
        for (dst, r0, c0, m) in fm:
            pb = pi % 3
            pi += 1
            for k in range(8):
                S.op("pe", lambda e, b=b, k=k, tn=tn, c0=c0, m=m, pb=pb: e.matmul(pout[pb][:m, :tn], WIN[:, k, c0:c0 + m], HT[b][:, k, :tn], start=(k == 0), stop=(k == 7)),
                     reads=WK + [hk], writes=["pout%d" % pb])
            if dst is FMU or g < 0:
                evac(lambda e, ob, dst=dst, r0=r0, m=m, t0=t0, tn=tn: e.dma_start(out=dst[r0:r0 + m, t0:t0 + tn], in_=ob[:m, :tn]), pb, m, tn, "fm")
            else:
                evac(lambda e, ob, dst=dst, r0=r0, m=m, g=g: e.dma_start(
                    out=dst[r0:r0 + m, SEQC:].rearrange("p (w r) -> p w r", r=64)[:, :, 8 * g:8 * g + 8],
                    in_=ob[:m, :512].rearrange("p (w r) -> p w r", r=8)), pb, m, 512, "fm", cm=True)
        for ti in range(tn // 128):
            tl = [(VT, t0 + ti * 128, 0, 1280)]
            own_row = None
            if tl_all:
                own_row = t0 + ti * 128
            elif g < 0 and ti == 0:
                own_row = 0
            elif 0 <= g < 4:
                own_row = 128 + g * 512 + ti * 128
            if own_row is not None:
                tl += [(TL, own_row, 0, 0), (TL, own_row, 512, 1792)]
            for (dst, row, dc, c0) in tl:
                pb = pi % 3
                pi += 1
                for k in range(8):
                    S.op("pe", lambda e, b=b, k=k, ti=ti, c0=c0, pb=pb: e.matmul(pout[pb][:, :], HT[b][:, k, ti * 128:(ti + 1) * 128], WIN[:, k, c0:c0 + 512],
                                                                               start=(k == 0), stop=(k == 7)), reads=WK + [hk], writes=["pout%d" % pb])
                evac(lambda e, ob, dst=dst, row=row, dc=dc: e.dma_start(out=dst[row:row + 128, dc:dc + 512], in_=ob[:, :]), pb, 128, 512, "tm")


def emit_B(nc, S, sb, ps, io):
    FMU, YFs, YBs = io["FMU"], io["YF"], io["YB"]
    tau, ident = io["tau"], io["ident"]
    for ct in range(2):
        prm, bre, bim, cre, cim = io["prm"][ct], io["bre"][ct], io["bim"][ct], io["cre"][ct], io["cim"][ct]
        uin = [FMU, FMU]
        yout = [YFs, YBs]
        X = "c%d_" % ct
        sub = ExitStack()
        sb, ps = _mk(nc, sub)
        TWO_PI = 2.0 * math.pi
        PRM = sb(X + "PRM", [128, 24]); BRE = sb(X + "BRE", [128, 128]); BIM = sb(X + "BIM", [128, 128])
        CRE = sb(X + "CRE", [128, 128]); CIM = sb(X + "CIM", [128, 128]); TAU = sb(X + "TAU", [128, 512]); ID = sb(X + "ID", [128, 128])
        names = ["DT", "LR", "MAG", "TH", "R", "R2", "RF", "FR", "SIN", "COS", "ARE", "AIM", "DEN", "AM1", "FRE", "FIM", "T0", "T1"]
        P = {n: sb(X + "p_" + n, [128, 8]) for n in names}
        RI = sb(X + "p_RI", [128, 8], I32)
        BBR = sb(X + "BBR", [128, 128]); BBI = sb(X + "BBI", [128, 128]); TB = sb(X + "TB", [128, 128])
        PAD = sb(X + "PAD", [128, 128])
        WBR = sb(X + "WBR", [128, 8, 128]); WBI = sb(X + "WBI", [128, 8, 128]); CR = sb(X + "CR", [128, 8, 128]); CIN = sb(X + "CIN", [128, 8, 128])
        TC = sb(X + "TC", [128, 8, 512]); TS = sb(X + "TS", [128, 8, 512]); RHO = sb(X + "RHO", [128, 8, 512])
        RR = sb(X + "RR", [128, 512]); RRF = sb(X + "RRF", [128, 512]); RRI = sb(X + "RRI", [128, 512], I32)
        UC = [sb(X + "UC%d" % i, [128, 512]) for i in range(2)]
        W = {}
        for n in ("BR", "BI", "T1", "T2", "T3", "T4", "XR", "XI", "QR", "QI", "HR", "HI"):
            for i in range(2):
                W[n, i] = sb(X + "w_%s%d" % (n, i), [128, 512])
        HP = sb(X + "HP", [128, 8])
        YO = [sb(X + "YO%d" % i, [128, 512]) for i in range(2)]
        pbr = [ps(X + "pbr%d" % i, [128, 512]) for i in range(2)]
        pbi = [ps(X + "pbi%d" % i, [128, 512]) for i in range(2)]
        py = [ps(X + "py%d" % i, [128, 512]) for i in range(2)]
        ptr = ps(X + "ptr", [128, 128])

        for (t, src, k) in ((PRM, prm, "PRM"), (BRE, bre, "BRE"), (BIM, bim, "BIM"), (CRE, cre, "CRE"), (CIM, cim, "CIM"),
                            (TAU, tau, "TAU"), (ID, ident, "ID")):
            S.dma("sp", lambda e, t=t, src=src: e.dma_start(out=t[:], in_=src), writes=[k])
        PR3 = PRM[:].rearrange("p (a c) -> p a c", c=3)
        K = ["PP"]

        def V(fn, reads=(), writes=()):
            S.op("dve", fn, reads=list(reads) + K, writes=list(writes) + K)

        def A(fn, reads=(), writes=()):
            S.op("act", fn, reads=list(reads) + K, writes=list(writes) + K)

        A(lambda e: e.activation(P["DT"][:], PR3[:, :, 2], AF.Exp), reads=["PRM"])
        V(lambda e: e.tensor_scalar(P["LR"][:], PR3[:, :, 0], -1e-4, None, ALU.min), reads=["PRM"])
        V(lambda e: e.tensor_tensor(P["T0"][:], P["LR"][:], P["DT"][:], ALU.mult))
        A(lambda e: e.activation(P["MAG"][:], P["T0"][:], AF.Exp))
        V(lambda e: e.tensor_tensor(P["TH"][:], PR3[:, :, 1], P["DT"][:], ALU.mult), reads=["PRM"])
        V(lambda e: e.tensor_scalar(P["R"][:], P["TH"][:], 1.0 / TWO_PI, None, ALU.mult))
        V(lambda e: e.tensor_scalar(P["R2"][:], P["R"][:], 0.25, None, ALU.add))
        for (src, dst) in (("R", "SIN"), ("R2", "COS")):
            V(lambda e, src=src: e.tensor_copy(RI[:], P[src][:]))
            V(lambda e: e.tensor_copy(P["RF"][:], RI[:]))
            V(lambda e, src=src: e.tensor_tensor(P["FR"][:], P[src][:], P["RF"][:], ALU.subtract))
            A(lambda e, dst=dst: e.activation(P[dst][:], P["FR"][:], AF.Sin, scale=TWO_PI))
        V(lambda e: e.tensor_tensor(P["ARE"][:], P["MAG"][:], P["COS"][:], ALU.mult))
        V(lambda e: e.tensor_tensor(P["AIM"][:], P["MAG"][:], P["SIN"][:], ALU.mult))
        V(lambda e: e.tensor_tensor(P["T0"][:], P["LR"][:], P["LR"][:], ALU.mult))
        V(lambda e: e.tensor_tensor(P["T1"][:], PR3[:, :, 1], PR3[:, :, 1], ALU.mult), reads=["PRM"])
        V(lambda e: e.tensor_tensor(P["DEN"][:], P["T0"][:], P["T1"][:], ALU.add))
        V(lambda e: e.reciprocal(P["DEN"][:], P["DEN"][:]))
        V(lambda e: e.tensor_scalar(P["AM1"][:], P["ARE"][:], -1.0, None, ALU.add))
        V(lambda e: e.tensor_tensor(P["T0"][:], P["AM1"][:], P["LR"][:], ALU.mult))
        V(lambda e: e.tensor_tensor(P["T1"][:], P["AIM"][:], PR3[:, :, 1], ALU.mult), reads=["PRM"])
        V(lambda e: e.tensor_tensor(P["T0"][:], P["T0"][:], P["T1"][:], ALU.add))
        V(lambda e: e.tensor_tensor(P["FRE"][:], P["T0"][:], P["DEN"][:], ALU.mult))
        V(lambda e: e.tensor_tensor(P["T0"][:], P["AIM"][:], P["LR"][:], ALU.mult))
        V(lambda e: e.tensor_tensor(P["T1"][:], P["AM1"][:], PR3[:, :, 1], ALU.mult), reads=["PRM"])
        V(lambda e: e.tensor_tensor(P["T0"][:], P["T0"][:], P["T1"][:], ALU.subtract))
        V(lambda e: e.tensor_tensor(P["FIM"][:], P["T0"][:], P["DEN"][:], ALU.mult))

        def v3(t):
            return t[:].rearrange("p (a h) -> p a h", h=16)

        def bc(n):
            return P[n][:].unsqueeze(2).to_broadcast([128, 8, 16])
        V(lambda e: e.tensor_tensor(v3(BBR), v3(BRE), bc("FRE"), ALU.mult), reads=["BRE"])
        V(lambda e: e.tensor_tensor(v3(TB), v3(BIM), bc("FIM"), ALU.mult), reads=["BIM"])
        V(lambda e: e.tensor_tensor(BBR[:], BBR[:], TB[:], ALU.subtract))
        V(lambda e: e.tensor_tensor(v3(BBI), v3(BIM), bc("FRE"), ALU.mult), reads=["BIM"])
        V(lambda e: e.tensor_tensor(v3(TB), v3(BRE), bc("FIM"), ALU.mult), reads=["BRE"])
        V(lambda e: e.tensor_tensor(BBI[:], BBI[:], TB[:], ALU.add))
        V(lambda e: e.tensor_scalar(CIM[:], CIM[:], -1.0, None, ALU.mult), reads=["CIM"], writes=["CIM"])
        V(lambda e: e.memset(CR[:], 0.0)); V(lambda e: e.memset(CIN[:], 0.0))
        for dj in range(8):
            j = dj % 4
            for (src, dst) in ((BBR, WBR), (BBI, WBI)):
                V(lambda e: e.memset(PAD[:], 0.0), writes=["PAD"])
                V(lambda e, src=src, dj=dj, j=j: e.tensor_copy(PAD[0:64, 32 * j:32 * j + 16], src[0:64, dj * 16:dj * 16 + 16]), writes=["PAD"])
                V(lambda e, src=src, dj=dj, j=j: e.tensor_copy(PAD[64:128, 32 * j + 16:32 * j + 32], src[64:128, dj * 16:dj * 16 + 16]), writes=["PAD"])
                S.op("pe", lambda e: e.transpose(ptr[:], PAD[:], ID[:]), reads=["PAD", "ID"], writes=["ptr"])
                S.op("act", lambda e, dst=dst, dj=dj: e.copy(dst[:, dj, :], ptr[:]), reads=["ptr"], writes=["WB"])
            for (src, dst) in ((CRE, CR), (CIM, CIN)):
                V(lambda e, src=src, dst=dst, dj=dj, j=j: e.tensor_copy(dst[0:64, dj, 32 * j:32 * j + 16], src[0:64, dj * 16:dj * 16 + 16]), reads=["CRE", "CIM"], writes=["CC"])
                V(lambda e, src=src, dst=dst, dj=dj, j=j: e.tensor_copy(dst[64:128, dj, 32 * j + 16:32 * j + 32], src[64:128, dj * 16:dj * 16 + 16]), reads=["CRE", "CIM"], writes=["CC"])
            for (off, dst) in ((0.0, TS), (0.25, TC)):
                V(lambda e, dj=dj, off=off: e.tensor_scalar(RR[:], TAU[:], P["R"][:, dj:dj + 1], off, ALU.mult, ALU.add), reads=["TAU"], writes=["RR"])
                V(lambda e: e.tensor_copy(RRI[:], RR[:]), reads=["RR"], writes=["RRI"])
                V(lambda e: e.tensor_copy(RRF[:], RRI[:]), reads=["RRI"], writes=["RRF"])
                V(lambda e: e.tensor_tensor(RRF[:], RR[:], RRF[:], ALU.subtract), reads=["RR"], writes=["RRF"])
                S.op("act", lambda e, dst=dst, dj=dj: e.activation(dst[:, dj, :], RRF[:], AF.Sin, scale=TWO_PI), reads=["RRF"], writes=["TAB"])
            V(lambda e, dj=dj: e.tensor_copy(RHO[:, dj, :], P["MAG"][:, dj:dj + 1].to_broadcast([128, 512])), writes=["TAB"])

        G = "pool"
        oi = 0
        for d in range(2):
            V(lambda e: e.memset(HP[:], 0.0), writes=["HP"])
            for ci, (t0, T) in enumerate(S5_CH):
                ub_ = (d * 9 + ci) % 2
                uk = "UC%d" % ub_
                n0 = t0 if d == 0 else _mirror(t0, T)[0]
                S.dma("sp", lambda e, d=d, n0=n0, T=T, ub_=ub_, ct=ct: e.dma_start(out=UC[ub_][:, :T], in_=uin[d][ct * 128:(ct + 1) * 128, n0:n0 + T]), writes=[uk])
                ucv = (lambda ub_=ub_, T=T: UC[ub_][:, :T]) if d == 0 else (lambda ub_=ub_, T=T: UC[ub_][:, :T][:, ::-1])
                yb_ = (d * 9 + ci) % 2
                for j in range(4):
                    dj = d * 4 + j
                    b = j % 2
                    w = lambda n, b=b, T=T: W[n, b][:, :T]
                    k = lambda n, b=b: "w_%s%d" % (n, b)
                    S.op("pe", lambda e, dj=dj, b=b, T=T, ucv=ucv: e.matmul(pbr[b][:, :T], WBR[:, dj, :], ucv(), start=True, stop=True),
                         reads=["WB", uk], writes=["pbr%d" % b])
                    S.op("pe", lambda e, dj=dj, b=b, T=T, ucv=ucv: e.matmul(pbi[b][:, :T], WBI[:, dj, :], ucv(), start=True, stop=True),
                         reads=["WB", uk], writes=["pbi%d" % b])
                    S.op("act", lambda e, w=w, b=b, T=T: e.copy(w("BR"), pbr[b][:, :T]), reads=["pbr%d" % b], writes=[k("BR")])
                    S.op("act", lambda e, w=w, b=b, T=T: e.copy(w("BI"), pbi[b][:, :T]), reads=["pbi%d" % b], writes=[k("BI")])
                    cs = lambda dj=dj, T=T: TC[:, dj, :T]
                    sn = lambda dj=dj, T=T: TS[:, dj, :T]
                    S.op("dve", lambda e, w=w, cs=cs: e.tensor_tensor(w("T1"), cs(), w("BR"), ALU.mult), reads=["TAB", k("BR")], writes=[k("T1")])
                    S.op("dve", lambda e, w=w, sn=sn: e.tensor_tensor(w("T2"), sn(), w("BI"), ALU.mult), reads=["TAB", k("BI")], writes=[k("T2")])
                    S.op("dve", lambda e, w=w: e.tensor_tensor(w("XR"), w("T1"), w("T2"), ALU.add), reads=[k("T1"), k("T2")], writes=[k("XR")])
                    S.op(G, lambda e, w=w, cs=cs: e.tensor_tensor(w("T3"), cs(), w("BI"), ALU.mult), reads=["TAB", k("BI")], writes=[k("T3")])
                    S.op(G, lambda e, w=w, sn=sn: e.tensor_tensor(w("T4"), sn(), w("BR"), ALU.mult), reads=["TAB", k("BR")], writes=[k("T4")])
                    S.op(G, lambda e, w=w: e.tensor_tensor(w("XI"), w("T3"), w("T4"), ALU.subtract), reads=[k("T3"), k("T4")], writes=[k("XI")])
                    S.op("dve", lambda e, w=w, dj=dj, j=j, T=T: e.tensor_tensor_scan(w("QR"), RHO[:, dj, :T], w("XR"), HP[:, 2 * j:2 * j + 1], ALU.mult, ALU.add),
                         reads=["TAB", k("XR"), "HP"], writes=[k("QR")])
                    S.op("dve", lambda e, w=w, dj=dj, j=j, T=T: e.tensor_tensor_scan(w("QI"), RHO[:, dj, :T], w("XI"), HP[:, 2 * j + 1:2 * j + 2], ALU.mult, ALU.add),
                         reads=["TAB", k("XI"), "HP"], writes=[k("QI")])
                    S.op("dve", lambda e, w=w, cs=cs: e.tensor_tensor(w("T1"), cs(), w("QR"), ALU.mult), reads=["TAB", k("QR")], writes=[k("T1")])
                    S.op("dve", lambda e, w=w, sn=sn: e.tensor_tensor(w("T2"), sn(), w("QI"), ALU.mult), reads=["TAB", k("QI")], writes=[k("T2")])
                    S.op("dve", lambda e, w=w: e.tensor_tensor(w("HR"), w("T1"), w("T2"), ALU.subtract), reads=[k("T1"), k("T2")], writes=[k("HR")])
                    S.op(G, lambda e, w=w, sn=sn: e.tensor_tensor(w("T3"), sn(), w("QR"), ALU.mult), reads=["TAB", k("QR")], writes=[k("T3")])
                    S.op(G, lambda e, w=w, cs=cs: e.tensor_tensor(w("T4"), cs(), w("QI"), ALU.mult), reads=["TAB", k("QI")], writes=[k("T4")])
                    S.op(G, lambda e, w=w: e.tensor_tensor(w("HI"), w("T3"), w("T4"), ALU.add), reads=[k("T3"), k("T4")], writes=[k("HI")])
                    S.op("act", lambda e, b=b, j=j, T=T: e.copy(HP[:, 2 * j:2 * j + 1], W["HR", b][:, T - 1:T]), reads=[k("HR")], writes=["HP"])
                    S.op("act", lambda e, b=b, j=j, T=T: e.copy(HP[:, 2 * j + 1:2 * j + 2], W["HI", b][:, T - 1:T]), reads=[k("HI")], writes=["HP"])
                    S.op("pe", lambda e, dj=dj, w=w, j=j, yb_=yb_, T=T: e.matmul(py[yb_][:, :T], CR[:, dj, :], w("HR"), start=(j == 0), stop=False),
                         reads=["CC", k("HR")], writes=["py%d" % yb_])
                    S.op("pe", lambda e, dj=dj, w=w, j=j, yb_=yb_, T=T: e.matmul(py[yb_][:, :T], CIN[:, dj, :], w("HI"), start=False, stop=(j == 3)),
                         reads=["CC", k("HI")], writes=["py%d" % yb_])
                if d == 0:
                    S.op("act", lambda e, yb_=yb_, T=T: e.copy(YO[yb_][:, :T], py[yb_][:, :T]), reads=["py%d" % yb_], writes=["YO%d" % yb_])
                else:
                    S.op("act", lambda e, yb_=yb_, T=T: e.copy(YO[yb_][:, :T][:, ::-1], py[yb_][:, :T]), reads=["py%d" % yb_], writes=["YO%d" % yb_])
                oi += 1
                S.dma("act", lambda e, d=d, yb_=yb_, n0=n0, T=T, ct=ct: e.dma_start(out=yout[d][ct * 128:(ct + 1) * 128, n0:n0 + T], in_=YO[yb_][:, :T]),
                      reads=["YO%d" % yb_], writes=["yout_%d" % oi])
        S.sync_all()
        S.emit()
        sub.close()


def emit_C(nc, S, sb_unused, ps_unused, io):
    FMQ, FMK, FMZ, VT = io["FMQ"], io["FMK"], io["FMZ"], io["VT"]
    OS = [io["OF"], io["OB"]]
    rst, tmask, tmask2, blk, hmask, ident = io["rst"], io["tmask"], io["tmask2"], io["blk"], io["hmask"], io["ident"]
    QSC = 32 ** -0.5
    for hh in range(2):
        X = "h%d_" % hh
        sub = ExitStack()
        sb, ps = _mk(nc, sub)
        cols = slice(hh * 256, hh * 256 + 256)
        TM2 = sb(X + "TM2", [64, 256])
        S.dma("sp", lambda e: e.dma_start(out=TM2[:], in_=tmask2), writes=["TM2"])
        RST = sb(X + "RST", [128, 512]); TM = sb(X + "TM", [64, 256]); BLK = sb(X + "BLK", [128, 256]); HM = sb(X + "HM", [128, 4]); ID = sb(X + "ID", [128, 128])
        WG = sb(X + "WG", [16, 128]); BG = sb(X + "BG", [128, 1]); NBG = sb(X + "NBG", [128, 1])
        Wb = {}
        for n in ("Q", "K", "LA", "B", "E", "D", "QE", "QS", "KD", "KS0", "KS1", "KS2", "KS3"):
            for i in range(2):
                Wb[n, i] = sb(X + "g_%s%d" % (n, i), [128, 512])
        Z = [sb(X + "Z%d" % i, [16, 512]) for i in range(2)]
        VV = [sb(X + "VV%d" % i, [64, 8, 256]) for i in range(2)]
        DEC = [sb(X + "DEC%d" % i, [128, 8]) for i in range(2)]
        KDT = [sb(X + "KDT%d" % i, [64, 128]) for i in range(2)]
        STt = [sb(X + "ST%d" % i, [64, 256]) for i in range(2)]
        OB = [sb(X + "OB%d" % i, [64, 256]) for i in range(3)]
        KVM = sb(X + "KVM", [128, 256])
        SS = [sb(X + "SS%d" % i, [128, 256]) for i in range(2)]
        pza = ps(X + "pza", [128, 512])
        pt0 = ps(X + "pt0", [64, 128])
        pt = [pt0, pt0]
        pkv = [ps(X + "pkv%d" % i, [128, 256]) for i in range(2)]
        psc = [ps(X + "psc%d" % i, [64, 256]) for i in range(2)]
        po = [ps(X + "po%d" % i, [64, 256]) for i in range(2)]
        for (t, src, k) in ((RST, rst, "RST"), (TM, tmask, "TM"), (BLK, blk, "BLK"), (HM, hmask, "HM"), (ID, ident, "ID")):
            S.dma("sp", lambda e, t=t, src=src: e.dma_start(out=t[:], in_=src), writes=[k])
        gc = 0
        oi = 0
        for d in range(2):
            S.dma("sp", lambda e, d=d, hh=hh: e.dma_start(out=WG[:], in_=io["wg"][hh][d]), writes=["WG"])
            S.dma("sp", lambda e, d=d, hh=hh: e.dma_start(out=BG[:], in_=io["bg"][hh][d]), writes=["BG"])
            S.op("dve", lambda e: e.tensor_scalar(NBG[:], BG[:], -1.0, None, ALU.mult), reads=["BG"], writes=["NBG"])
            S.op("dve", lambda e: e.memset(SS[0][:], 0.0), writes=["SS0"])
            scur = 0
            for bi, (t0, T) in enumerate(S5_CH):
                nchk = T // 64
                n0 = t0 if d == 0 else _mirror(t0, T)[0]
                w0 = (n0 - SEQC) // 64
                b = (d * 9 + bi) % 2
                w = lambda n, b=b, T=T: Wb[n, b][:, :T]
                k = lambda n, b=b: "g_%s%d" % (n, b)
                w3 = lambda n, b=b, T=T: Wb[n, b][:, :T].rearrange("p (c s) -> p c s", s=64)
                S.dma("sp", lambda e, d=d, b=b, n0=n0, T=T, hh=hh: e.dma_start(out=Wb["Q", b][:, :T], in_=FMQ[hh * 128:(hh + 1) * 128, n0:n0 + T]), writes=[k("Q")])
                S.dma("act", lambda e, d=d, b=b, n0=n0, T=T, hh=hh: e.dma_start(out=Wb["K", b][:, :T], in_=FMK[hh * 128:(hh + 1) * 128, n0:n0 + T]), writes=[k("K")])
                S.dma("sp", lambda e, d=d, b=b, n0=n0, T=T: e.dma_start(out=Z[b][:, :T], in_=FMZ[d * 16:(d + 1) * 16, n0:n0 + T]), writes=["Z%d" % b])
                if t0 < SEQC:
                    S.dma("act", lambda e, b=b, nchk=nchk, cols=cols: e.dma_start(
                        out=VV[b][:, :nchk, :], in_=VT[0:SEQC, cols].rearrange("(c s) f -> s c f", s=64)), writes=["VV%d" % b])
                else:
                    S.dma("act", lambda e, b=b, w0=w0, cols=cols: e.dma_start(
                        out=VV[b][:, :, :], in_=VT[SEQC:, cols].rearrange("(r w) f -> r w f", w=64)[:, w0:w0 + 8, :]), writes=["VV%d" % b])
                S.op("pe", lambda e, b=b, T=T: e.matmul(pza[:, :T], WG[:], Z[b][:, :T], start=True, stop=True), reads=["WG", "Z%d" % b], writes=["pza"])
                S.op("act", lambda e, w=w, T=T: e.activation(w("E"), pza[:, :T], AF.Exp, bias=NBG[:], scale=-1.0), reads=["pza", "NBG"], writes=[k("E")])
                S.op("act", lambda e, w=w: e.activation(w("E"), w("E"), AF.Ln, bias=1.0), reads=[k("E")], writes=[k("E")])
                S.op("dve", lambda e, w=w: e.tensor_scalar(w("LA"), w("E"), -1.0 / 16.0, None, ALU.mult), reads=[k("E")], writes=[k("LA")])
                S.op("dve", lambda e, w=w, T=T: e.tensor_tensor_scan(w("B"), RST[:, :T], w("LA"), 0.0, ALU.mult, ALU.add), reads=["RST", k("LA")], writes=[k("B")])
                iref, ilast = (32, 63) if d == 0 else (31, 0)
                if d == 1:
                    S.op("dve", lambda e, w3=w3, nchk=nchk: e.tensor_tensor(w3("D"), w3("B")[:, :, 63:64].to_broadcast([128, nchk, 64]), w3("B"), ALU.subtract),
                         reads=[k("B")], writes=[k("D")])
                    S.op("dve", lambda e, w=w: e.tensor_tensor(w("B"), w("D"), w("LA"), ALU.add), reads=[k("D"), k("LA")], writes=[k("B")])
                S.op("act", lambda e, b=b, w3=w3, nchk=nchk, ilast=ilast: e.activation(DEC[b][:, :nchk], w3("B")[:, :, ilast], AF.Exp), reads=[k("B")], writes=["DEC%d" % b])
                S.op("act", lambda e, w=w: e.activation(w("E"), w("B"), AF.Exp), reads=[k("B")], writes=[k("E")])
                S.op("dve", lambda e, w=w: e.scalar_tensor_tensor(w("QE"), w("Q"), QSC, w("E"), ALU.mult, ALU.mult), reads=[k("Q"), k("E")], writes=[k("QE")])
                S.op("dve", lambda e, w3=w3, nchk=nchk, iref=iref: e.tensor_tensor(w3("D"), w3("B"), w3("B")[:, :, iref:iref + 1].to_broadcast([128, nchk, 64]), ALU.subtract),
                     reads=[k("B")], writes=[k("D")])
                S.op("act", lambda e, w=w: e.activation(w("E"), w("D"), AF.Exp), reads=[k("D"), k("QE")], writes=[k("E")])
                S.op("dve", lambda e, w=w: e.scalar_tensor_tensor(w("QS"), w("Q"), QSC, w("E"), ALU.mult, ALU.mult), reads=[k("Q"), k("E")], writes=[k("QS")])
                S.op("act", lambda e, w=w: e.activation(w("E"), w("D"), AF.Exp, scale=-1.0), reads=[k("D"), k("QS")], writes=[k("E")])
                S.op("dve", lambda e, w=w: e.tensor_tensor(w("LA"), w("K"), w("E"), ALU.mult), reads=[k("K"), k("E"), k("B")], writes=[k("LA")])
                for h in range(4):
                    S.op("pool", lambda e, w=w, h=h: e.tensor_scalar(w("KS%d" % h), w("LA"), HM[:, h:h + 1], None, ALU.mult),
                         reads=[k("LA"), "HM"], writes=[k("KS%d" % h)])
                S.op("dve", lambda e, w3=w3, nchk=nchk, ilast=ilast: e.tensor_tensor(w3("D"), w3("B")[:, :, ilast:ilast + 1].to_broadcast([128, nchk, 64]), w3("B"), ALU.subtract),
                     reads=[k("B"), k("E"), k("LA")], writes=[k("D")])
                S.op("act", lambda e, w=w: e.activation(w("D"), w("D"), AF.Exp), reads=[k("D")], writes=[k("D")])
                S.op("dve", lambda e, w=w: e.tensor_tensor(w("KD"), w("K"), w("D"), ALU.mult), reads=[k("K"), k("D")], writes=[k("KD")])
                for c in (range(nchk) if d == 0 else range(nchk - 1, -1, -1)):
                    p2 = gc % 2
                    gc += 1
                    cs = slice(c * 64, (c + 1) * 64)
                    S.op("pe", lambda e, b=b, cs=cs, p2=p2: e.transpose(pt[p2][:], Wb["KD", b][:, cs], ID[:]), reads=[k("KD"), "ID"], writes=["pt"])
                    S.op("act", lambda e, p2=p2: e.copy(KDT[p2][:], pt[p2][:]), reads=["pt"], writes=["KDT%d" % p2])
                    S.op("pe", lambda e, b=b, c=c, p2=p2: e.matmul(pkv[p2][:], KDT[p2][:], VV[b][:, c, :], start=True, stop=True),
                         reads=["KDT%d" % p2, "VV%d" % b], writes=["pkv%d" % p2])
                    for h in range(4):
                        S.op("pe", lambda e, b=b, cs=cs, p2=p2, h=h: e.matmul(psc[p2][:, h * 64:(h + 1) * 64], Wb["KS%d" % h, b][:, cs], Wb["QS", b][:, cs],
                                                                            start=True, stop=True),
                             reads=[k("KS%d" % h), k("QS")], writes=["psc%d" % p2])
                    S.op("dve", lambda e, p2=p2, d=d: e.tensor_tensor(STt[p2][:], psc[p2][:], (TM if d == 0 else TM2)[:], ALU.mult), reads=["psc%d" % p2, "TM", "TM2"], writes=["ST%d" % p2])
                    for h in range(4):
                        hs = slice(h * 64, (h + 1) * 64)
                        S.op("pe", lambda e, b=b, c=c, p2=p2, hs=hs: e.matmul(po[p2][:, hs], STt[p2][:, hs], VV[b][:, c, hs], start=True, stop=False),
                             reads=["ST%d" % p2, "VV%d" % b], writes=["po%d" % p2])
                        S.op("pe", lambda e, b=b, cs=cs, p2=p2, hs=hs, scur=scur: e.matmul(po[p2][:, hs], Wb["QE", b][:, cs], SS[scur][:, hs], start=False, stop=True),
                             reads=[k("QE"), "SS%d" % scur], writes=["po%d" % p2])
                    ob = oi % 3
                    oi += 1
                    S.op("act", lambda e, ob=ob, p2=p2: e.copy(OB[ob][:], po[p2][:]), reads=["po%d" % p2], writes=["OB%d" % ob])
                    if t0 < SEQC:
                        S.dma("sp" if oi % 2 else "act", lambda e, d=d, ob=ob, c=c, cols=cols: e.dma_start(out=OS[d][c * 64:(c + 1) * 64, cols], in_=OB[ob][:]),
                              reads=["OB%d" % ob], writes=["o_%d" % oi])
                    else:
                        S.dma("sp" if oi % 2 else "act", lambda e, d=d, ob=ob, c=c, w0=w0, cols=cols: e.dma_start(
                            out=OS[d][SEQC:, cols].rearrange("(r w) f -> r w f", w=64)[:, w0 + c, :], in_=OB[ob][:]),
                              reads=["OB%d" % ob], writes=["o_%d" % oi])
                    S.op("dve", lambda e, p2=p2: e.tensor_tensor(KVM[:], pkv[p2][:], BLK[:], ALU.mult), reads=["pkv%d" % p2, "BLK"], writes=["KVM"])
                    S.op("dve", lambda e, b=b, c=c, scur=scur: e.scalar_tensor_tensor(SS[1 - scur][:], SS[scur][:], DEC[b][:, c:c + 1], KVM[:], ALU.mult, ALU.add),
                         reads=["SS%d" % scur, "DEC%d" % b, "KVM"], writes=["SS%d" % (1 - scur)])
                    scur = 1 - scur
        S.sync_all()
        S.emit()
        sub.close()


def emit_D(nc, S, sb, ps, io, blocks, last):
    xT = io["xT"]; modT = io["MODS"]; TLs = io["TL"]
    su = TLs[:, 0:256]; sv = TLs[:, 256:512]; gg = TLs[:, 512:1024]
    s5u = io["FMU"]; yf = io["YF"]; yb = io["YB"]; of_ = io["OF"]; ob_ = io["OB"]
    w_out, wsT, sgub, s5d, wglu, ng, g2, wq = io["w_out"], io["wsT"], io["sgub"], io["s5d"], io["wglu"], io["ng"], io["g2"], io["wq"]
    keysT, eu, ev, gfin, ones, ident, iota16 = io["keysT"], io["eu"], io["ev"], io["gfin"], io["ones"], io["ident"], io["iota16"]
    xo = io["xo"]
    nb = len(blocks)
    MOD = sb("MOD", [128, 96]); WOUT = sb("WOUT", [128, 8, D]); WS = sb("WS", [128, 512]); SGUB = sb("SGUB", [128, 4]); S5D = sb("S5D", [128, 2])
    WGLU = sb("WGLU", [128, 2, 512]); NG = sb("NG", [128, 512]); G2 = sb("G2", [128, 8]); WQ = sb("WQ", [128, 8, 2048]); KEYS = sb("KEYS", [128, 2048])
    GF = sb("GF", [128, 8]); ONES = sb("ONES", [128, 128]); ID = sb("ID", [128, 128]); IOTA = sb("IOTA", [128, 16])
    A2 = sb("A2", [128, 16]); TA = sb("TA", [128, 16])
    U_ = sb("U_", [128, 256]); V_ = sb("V_", [128, 256]); GU = sb("GU", [128, 256]); GV = sb("GV", [128, 256]); SQ = sb("SQ", [128, 512])
    SS = sb("SS", [128, 8]); VN = sb("VN", [128, 256]); MIX = sb("MIX", [128, D])
    S5U = sb("S5U", [128, 2, 128]); YF = sb("YF", [128, 2, 128]); YB = sb("YB", [128, 2, 128]); GE = sb("GE", [128, 2, 128]); SG = sb("SG", [128, 256])
    OF = sb("OF", [128, 512]); OB = sb("OB", [128, 512]); GG = sb("GG", [128, 512]); SL = sb("SL", [128, 512])
    XT = sb("XT", [128, 8, 128]); X1 = sb("X1", [128, 8, 128]); XO = XT; HT = sb("HT", [128, 8, 128]); MIXT = HT; HTOK = sb("HTOK", [128, D])
    RSTD = sb("RSTD", [128, 128]); TMPB = sb("TMPB", [128, 128])
    SC = sb("SC", [128, 2048])
    M16 = sb("M16", [128, 256]); I16 = sb("I16", [128, 256], U32); IF16 = sb("IF16", [128, 256]); I1S = sb("I1S", [128, 128])
    CS = sb("CS", [128, 2048]); SC2 = CS; QT = CS[:].rearrange("p (q t) -> p q t", t=128); CS2 = sb("CS2", [128, 256])
    T16 = sb("T16", [128, 128]); P16 = sb("P16", [128, 128], U32); PF = sb("PF", [128, 128]); AI = sb("AI", [128, 128], I32)
    AFL = sb("AFL", [128, 128]); BFL = sb("BFL", [128, 128]); E1 = sb("E1", [128, 128]); E2 = sb("E2", [128, 128])
    EG = sb("EG", [128, 128]); GATE = sb("GATE", [128, 128]); IDXTI = sb("IDXTI", [128, 128], I32); GATET = sb("GATET", [128, 128])
    ACTT = sb("ACTT", [128, 128]); WT = sb("WT", [128, 128])
    UG = [sb("UG%d" % i, [128, D]) for i in range(2)]; VG = UG
    HB = [sb("HB%d" % i, [128, D]) for i in range(2)]
    P0 = ps("P0", [128, 2048]); P1 = ps("P1", [128, 1024]); P2 = ps("P2", [128, 512]); P3 = ps("P3", [128, 512])

    def V(fn, r=(), w=()):
        S.op("dve", fn, reads=r, writes=w)

    def A(fn, r=(), w=()):
        S.op("act", fn, reads=r, writes=w)

    def PE(fn, r=(), w=()):
        S.op("pe", fn, reads=r, writes=w)

    def LD(q, t, src, key):
        S.dma(q, lambda e: e.dma_start(out=t, in_=src), writes=[key])

    LD("sp", MOD[:], modT, "MOD"); LD("sp", WS[:], wsT, "WS"); LD("sp", SGUB[:], sgub, "SGUB"); LD("sp", S5D[:], s5d, "S5D")
    LD("sp", WGLU[:], wglu.rearrange("(c p) f -> p c f", p=128), "WGLU"); LD("sp", NG[:], ng, "NG"); LD("sp", G2[:], g2, "G2")
    LD("sp", KEYS[:], keysT, "KEYS"); LD("sp", GF[:], gfin, "GF"); LD("sp", ONES[:], ones, "ONES"); LD("sp", ID[:], ident, "ID"); LD("sp", IOTA[:], iota16, "IOTA")
    wo_v = w_out.rearrange("(k p) f -> p k f", p=128)
    wq_v = wq.rearrange("(k p) f -> p k f", p=128)
    for k in range(8):
        LD("act", WOUT[:, k, :], wo_v[:, k, :], "WOUT")
        LD("act", WQ[:, k, :], wq_v[:, k, :], "WQ")
    MOD3 = MOD[:].rearrange("p (j c) -> p j c", c=2)
    V(lambda e: e.tensor_scalar(TA[:].rearrange("p (j c) -> p j c", c=2), MOD3[:, 32:40, :], 1.0, None, ALU.add), ["MOD"], ["TA"])
    V(lambda e: e.tensor_tensor(A2[:].rearrange("p (j c) -> p j c", c=2), TA[:].rearrange("p (j c) -> p j c", c=2),
                                G2[:].unsqueeze(2).to_broadcast([128, 8, 2]), ALU.mult), ["TA", "G2"], ["A2"])
    A23 = A2[:].rearrange("p (j c) -> p j c", c=2)

    def rs_from_ss(ss, n, scale):
        V(lambda e: e.tensor_scalar(ss, ss, scale, EPS, ALU.mult, ALU.add), ["SS"], ["SS"])
        A(lambda e: e.sqrt(ss, ss), ["SS"], ["SS"])
        V(lambda e: e.reciprocal(ss, ss), ["SS"], ["SS"])

    def top16(src, scratch, mout, iout, n):
        V(lambda e: e.max(mout[:, 0:8], src), ["TK"], ["TK"])
        V(lambda e: e.max_index(iout[:, 0:8], mout[:, 0:8], src), ["TK"], ["TK"])
        V(lambda e: e.match_replace(scratch, mout[:, 0:8], src, -1e30), ["TK"], ["TK"])
        V(lambda e: e.max(mout[:, 8:16], scratch), ["TK"], ["TK"])
        V(lambda e: e.max_index(iout[:, 8:16], mout[:, 8:16], scratch), ["TK"], ["TK"])

    xT_v = xT.rearrange("(k p) t -> p k t", p=128)
    xo_v = xo.rearrange("(k p) t -> p k t", p=128)
    s5u_v = s5u.rearrange("(c p) t -> p c t", p=128)
    yf_v = yf.rearrange("(c p) t -> p c t", p=128)
    yb_v = yb.rearrange("(c p) t -> p c t", p=128)
    for bi in range(nb):
        sq0, r0_, oc0, isctx = blocks[bi]
        col = 1 if isctx else 0
        tk = slice(sq0, sq0 + 128)
        tr = slice(r0_, r0_ + 128)
        to = slice(oc0, oc0 + 128)
        LD("sp", U_[:], su[tr, :], "U_"); LD("sp", V_[:], sv[tr, :], "V_"); LD("sp", GG[:], gg[tr, :], "GG")
        LD("act", S5U[:], s5u_v[:, :, tk], "S5U"); LD("act", YF[:], yf_v[:, :, tk], "YF"); LD("act", YB[:], yb_v[:, :, tk], "YB")
        LD("sp", OF[:], of_[tk, :], "OF"); LD("sp", OB[:], ob_[tk, :], "OB"); LD("act", XT[:], xT_v[:, :, tk], "XT")
        A(lambda e: e.activation(GU[:], U_[:], AF.Gelu), ["U_"], ["GU"])
        A(lambda e: e.activation(GV[:], V_[:], AF.Gelu), ["V_"], ["GV"])
        V(lambda e: e.tensor_tensor(SQ[:, 0:256], GV[:], GV[:], ALU.mult), ["GV"], ["SQ"])
        V(lambda e: e.tensor_reduce(SS[:, 0:4], SQ[:, 0:256].rearrange("p (h d) -> p h d", d=64), AX.X, ALU.add), ["SQ"], ["SS"])
        rs_from_ss(SS[:, 0:4], 4, 1.0 / 64)
        V(lambda e: e.tensor_tensor(VN[:].rearrange("p (h d) -> p h d", d=64), GV[:].rearrange("p (h d) -> p h d", d=64),
                                    SS[:, 0:4].unsqueeze(2).to_broadcast([128, 4, 64]), ALU.mult), ["GV", "SS"], ["VN"])
        for h in range(4):
            PE(lambda e, h=h: e.matmul(P2[:, h * 64:(h + 1) * 64], WS[:, h * 128:(h + 1) * 128], VN[:, h * 64:(h + 1) * 64], start=True, stop=True),
               ["WS", "VN"], ["P2"])
        V(lambda e: e.tensor_tensor(MIX[:, 0:256].rearrange("p (h d) -> p h d", d=64), P2[:, 0:256].rearrange("p (h d) -> p h d", d=64),
                                    SGUB[:].unsqueeze(2).to_broadcast([128, 4, 64]), ALU.add), ["P2", "SGUB"], ["MIXa"])
        V(lambda e: e.tensor_tensor(MIX[:, 0:256], MIX[:, 0:256], GU[:], ALU.mult), ["GU"], ["MIXa"])
        V(lambda e: e.tensor_tensor(YF[:], YF[:], YB[:], ALU.add), ["YB"], ["YF"])
        for ct in range(2):
            V(lambda e, ct=ct: e.scalar_tensor_tensor(YF[:, ct, :], S5U[:, ct, :], S5D[:, ct:ct + 1], YF[:, ct, :], ALU.mult, ALU.add),
              ["S5U", "S5D"], ["YF"])
        A(lambda e: e.activation(GE[:], YF[:], AF.Gelu), ["YF"], ["GE"])
        for ct in range(2):
            PE(lambda e, ct=ct: e.matmul(P3[:, 0:512], GE[:, ct, :], WGLU[:, ct, :], start=(ct == 0), stop=(ct == 1)), ["GE", "WGLU"], ["P3"])
        A(lambda e: e.activation(SG[:], P3[:, 256:512], AF.Sigmoid), ["P3"], ["SG"])
        V(lambda e: e.tensor_tensor(MIX[:, 256:512], P3[:, 0:256], SG[:], ALU.mult), ["P3", "SG"], ["MIXb"])
        V(lambda e: e.tensor_tensor(OF[:], OF[:], OB[:], ALU.add), ["OB"], ["OF"])
        V(lambda e: e.tensor_tensor(SQ[:], OF[:], OF[:], ALU.mult), ["OF"], ["SQ"])
        V(lambda e: e.tensor_reduce(SS[:, 0:8], SQ[:].rearrange("p (h d) -> p h d", d=64), AX.X, ALU.add), ["SQ"], ["SS"])
        rs_from_ss(SS[:, 0:8], 8, 1.0 / 64)
        V(lambda e: e.tensor_tensor(OF[:].rearrange("p (h d) -> p h d", d=64), OF[:].rearrange("p (h d) -> p h d", d=64),
                                    SS[:, 0:8].unsqueeze(2).to_broadcast([128, 8, 64]), ALU.mult), ["SS"], ["OF"])
        V(lambda e: e.tensor_tensor(OF[:], OF[:], NG[:], ALU.mult), ["NG"], ["OF"])
        A(lambda e: e.activation(SL[:], GG[:], AF.Silu), ["GG"], ["SL"])
        V(lambda e: e.tensor_tensor(MIX[:, 512:1024], OF[:], SL[:], ALU.mult), ["OF", "SL"], ["MIXc"])
        for f in range(8):
            pp, pk = (P2, "P2") if f % 2 == 0 else (P3, "P3")
            PE(lambda e, f=f, pp=pp: e.transpose(pp[:, 0:128], MIX[:, f * 128:(f + 1) * 128], ID[:]), ["MIXa", "MIXb", "MIXc", "ID"], [pk])
            A(lambda e, f=f, pp=pp: e.copy(MIXT[:, f, :], pp[:, 0:128]), [pk], ["HT"])
        for ot in range(8):
            pp, pk = (P2, "P2") if ot % 2 == 0 else (P3, "P3")
            for k in range(8):
                PE(lambda e, ot=ot, k=k, pp=pp: e.matmul(pp[:, 0:128], WOUT[:, k, ot * 128:(ot + 1) * 128], MIXT[:, k, :], start=(k == 0), stop=(k == 7)),
                   ["WOUT", "HT"], [pk])
            V(lambda e, ot=ot, pp=pp, col=col: e.scalar_tensor_tensor(X1[:, ot, :], pp[:, 0:128], MOD3[:, 16 + ot, col:col + 1], XT[:, ot, :], ALU.mult, ALU.add),
              [pk, "MOD", "XT"], ["X1"])
        for k in range(8):
            A(lambda e, k=k: e.activation(TMPB[:], X1[:, k, :], AF.Square), ["X1"], ["TMPB"])
            PE(lambda e, k=k: e.matmul(P2[:, 0:128], ONES[:], TMPB[:], start=(k == 0), stop=(k == 7)), ["ONES", "TMPB"], ["P2"])
        V(lambda e: e.tensor_scalar(RSTD[:], P2[:, 0:128], 1.0 / D, EPS, ALU.mult, ALU.add), ["P2"], ["RSTD"])
        A(lambda e: e.sqrt(RSTD[:], RSTD[:]), ["RSTD"], ["RSTD"])
        V(lambda e: e.reciprocal(RSTD[:], RSTD[:]), ["RSTD"], ["RSTD"])
        for k in range(8):
            V(lambda e, k=k: e.tensor_tensor(TMPB[:], X1[:, k, :], RSTD[:], ALU.mult), ["X1", "RSTD"], ["TMPB"])
            A(lambda e, k=k, col=col: e.activation(HT[:, k, :], TMPB[:], AF.Identity, bias=MOD3[:, 24 + k, col:col + 1], scale=A23[:, k, col:col + 1]),
              ["TMPB", "MOD", "A2"], ["HT"])
        for k in range(8):
            pp, pk = (P2, "P2") if k % 2 == 0 else (P3, "P3")
            PE(lambda e, k=k, pp=pp: e.transpose(pp[:, 0:128], HT[:, k, :], ID[:]), ["HT", "ID"], [pk])
            A(lambda e, k=k, pp=pp: e.copy(HTOK[:, k * 128:(k + 1) * 128], pp[:, 0:128]), [pk], ["HTOK"])
        for qt in range(16):
            pp, pk = (P2, "P2") if qt % 2 == 0 else (P3, "P3")
            for k in range(8):
                PE(lambda e, qt=qt, k=k, pp=pp: e.matmul(pp[:, 0:128], WQ[:, k, qt * 128:(qt + 1) * 128], HT[:, k, :], start=(k == 0), stop=(k == 7)),
                   ["WQ", "HT"], [pk])
            if qt % 2 == 0:
                A(lambda e, qt=qt, pp=pp: e.copy(QT[:, qt, :], pp[:, 0:128]), [pk], ["TK"])
            else:
                V(lambda e, qt=qt, pp=pp: e.tensor_copy(QT[:, qt, :], pp[:, 0:128]), [pk], ["TK"])
        for qt in range(16):
            PE(lambda e, qt=qt: e.matmul(P0[:, qt * 128:(qt + 1) * 128], QT[:, qt, :], KEYS[:, qt * 128:(qt + 1) * 128], start=True, stop=True),
               ["TK", "KEYS"], ["P0a", "P0b"])
        for q4 in range(4):
            A(lambda e, q4=q4: e.copy(SC[:, q4 * 512:(q4 + 1) * 512], P0[:, q4 * 512:(q4 + 1) * 512]), ["P0a", "P0b"], ["TK"])
        for qt in range(16):
            top16(SC[:, qt * 128:(qt + 1) * 128], SC2[:, qt * 128:(qt + 1) * 128], M16[:, qt * 16:(qt + 1) * 16], I16[:, qt * 16:(qt + 1) * 16], 128)
        V(lambda e: e.tensor_copy(IF16[:], I16[:]), ["TK"], ["TK"])
        M4 = M16[:].rearrange("p (h q k) -> p h q k", q=2, k=16)
        IF4 = IF16[:].rearrange("p (h q k) -> p h q k", q=2, k=16)
        I1S3 = I1S[:].rearrange("p (h k) -> p h k", k=16)
        V(lambda e: e.tensor_scalar(I1S3, IF4[:, :, 0, :], 128.0, None, ALU.mult), ["TK"], ["TK"])
        CS4 = CS[:].rearrange("p (h a b) -> p h a b", a=16, b=16)
        V(lambda e: e.tensor_tensor(CS4, M4[:, :, 0, :].unsqueeze(3).to_broadcast([128, 8, 16, 16]),
                                    M4[:, :, 1, :].unsqueeze(2).to_broadcast([128, 8, 16, 16]), ALU.add), ["TK"], ["TK"])
        for h in range(8):
            top16(CS[:, h * 256:(h + 1) * 256], CS2[:], T16[:, h * 16:(h + 1) * 16], P16[:, h * 16:(h + 1) * 16], 256)
        V(lambda e: e.tensor_copy(PF[:], P16[:]), ["TK"], ["TK"])
        V(lambda e: e.tensor_scalar(AFL[:], PF[:], -7.5, 1.0 / 16, ALU.add, ALU.mult), ["TK"], ["TK"])
        V(lambda e: e.tensor_copy(AI[:], AFL[:]), ["TK"], ["TK"])
        V(lambda e: e.tensor_copy(AFL[:], AI[:]), ["TK"], ["TK"])
        V(lambda e: e.scalar_tensor_tensor(BFL[:], AFL[:], -16.0, PF[:], ALU.mult, ALU.add), ["TK"], ["TK"])
        EQ4 = CS[:].rearrange("p (h k a) -> p h k a", k=16, a=16)
        io4 = IOTA[:].unsqueeze(1).unsqueeze(1).to_broadcast([128, 8, 16, 16])
        for (sel, src, dst) in ((AFL, I1S3, E1), (BFL, IF4[:, :, 1, :], E2)):
            V(lambda e, sel=sel: e.tensor_tensor(EQ4, io4, sel[:].rearrange("p (h k) -> p h k", k=16).unsqueeze(3).to_broadcast([128, 8, 16, 16]), ALU.is_equal),
              ["TK", "IOTA"], ["TK"])
            V(lambda e, src=src: e.tensor_tensor(EQ4, EQ4, src.unsqueeze(2).to_broadcast([128, 8, 16, 16]), ALU.mult), ["TK"], ["TK"])
            V(lambda e, dst=dst: e.tensor_reduce(dst[:], CS[:].rearrange("p (m a) -> p m a", a=16), AX.X, ALU.add), ["TK"], ["TK"])
        V(lambda e: e.tensor_tensor(E1[:], E1[:], E2[:], ALU.add), ["TK"], ["TK"])
        T3 = T16[:].rearrange("p (h k) -> p h k", k=16)
        V(lambda e: e.tensor_tensor(EG[:].rearrange("p (h k) -> p h k", k=16), T3, T3[:, :, 0:1].to_broadcast([128, 8, 16]), ALU.subtract), ["TK"], ["TK"])
        A(lambda e: e.activation(EG[:], EG[:], AF.Exp), ["TK"], ["TK"])
        V(lambda e: e.tensor_reduce(SS[:, 0:8], EG[:].rearrange("p (h k) -> p h k", k=16), AX.X, ALU.add), ["TK"], ["SS"])
        V(lambda e: e.reciprocal(SS[:, 0:8], SS[:, 0:8]), ["SS"], ["SS"])
        V(lambda e: e.tensor_tensor(GATE[:].rearrange("p (h k) -> p h k", k=16), EG[:].rearrange("p (h k) -> p h k", k=16),
                                    SS[:, 0:8].unsqueeze(2).to_broadcast([128, 8, 16]), ALU.mult), ["TK", "SS"], ["GATE"])
        PE(lambda e: e.transpose(P2[:, 0:128], E1[:], ID[:]), ["TK", "ID"], ["P2"])
        V(lambda e: e.tensor_copy(IDXTI[:], P2[:, 0:128]), ["P2"], ["IDXTI"])
        PE(lambda e: e.transpose(P3[:, 0:128], GATE[:], ID[:]), ["GATE", "ID"], ["P3"])
        A(lambda e: e.copy(GATET[:], P3[:, 0:128]), ["P3"], ["GATET"])
        for t in range(128):
            b = t % 2
            S.dma("pool", lambda e, t=t, b=b: e.indirect_dma_start(out=UG[b][:], out_offset=None, in_=eu,
                                                                   in_offset=bass.IndirectOffsetOnAxis(ap=IDXTI[:, t:t + 1], axis=0)),
                  reads=["IDXTI"], writes=["UG%d" % b])
            pk = "P0a" if b == 0 else "P0b"
            for hf in range(2):
                PE(lambda e, t=t, b=b, hf=hf: e.matmul(P0[:, b * 1024 + hf * 512:b * 1024 + (hf + 1) * 512], ID[:, t:t + 1].to_broadcast([128, 128]),
                                                       HTOK[:, hf * 512:(hf + 1) * 512], start=True, stop=True), ["ID", "HTOK"], [pk])
                A(lambda e, b=b, hf=hf: e.copy(HB[b][:, hf * 512:(hf + 1) * 512], P0[:, b * 1024 + hf * 512:b * 1024 + (hf + 1) * 512]), [pk], ["HB%d" % b])
            V(lambda e, t=t, b=b: e.scalar_tensor_tensor(UG[b][:], UG[b][:], 1.0, HB[b][:], ALU.mult, ALU.mult, accum_out=ACTT[:, t:t + 1]),
              ["HB%d" % b], ["UG%d" % b, "ACTT"])
        A(lambda e: e.activation(WT[:], ACTT[:], AF.Gelu), ["ACTT"], ["WT"])
        V(lambda e: e.tensor_tensor(WT[:], WT[:], GATET[:], ALU.mult), ["GATET"], ["WT"])
        for t in range(128):
            b = t % 2
            S.dma("pool", lambda e, t=t, b=b: e.indirect_dma_start(out=VG[b][:], out_offset=None, in_=ev,
                                                                   in_offset=bass.IndirectOffsetOnAxis(ap=IDXTI[:, t:t + 1], axis=0)),
                  reads=["IDXTI"], writes=["UG%d" % b])
            for ot in range(8):
                PE(lambda e, t=t, b=b, ot=ot: e.matmul(P1[:, ot * 128 + t:ot * 128 + t + 1], VG[b][:, ot * 128:(ot + 1) * 128], WT[:, t:t + 1],
                                                       start=True, stop=True), ["UG%d" % b, "WT"], ["P1"])
        for ot in range(8):
            V(lambda e, ot=ot, col=col: e.scalar_tensor_tensor(XO[:, ot, :], P1[:, ot * 128:(ot + 1) * 128], MOD3[:, 40 + ot, col:col + 1], X1[:, ot, :],
                                                               ALU.mult, ALU.add), ["P1", "MOD", "X1"], ["XT"])
        if last:
            for k in range(8):
                A(lambda e, k=k: e.activation(TMPB[:], XO[:, k, :], AF.Square), ["XT"], ["TMPB"])
                PE(lambda e, k=k: e.matmul(P2[:, 0:128], ONES[:], TMPB[:], start=(k == 0), stop=(k == 7)), ["ONES", "TMPB"], ["P2"])
            V(lambda e: e.tensor_scalar(RSTD[:], P2[:, 0:128], 1.0 / D, EPS, ALU.mult, ALU.add), ["P2"], ["RSTD"])
            A(lambda e: e.sqrt(RSTD[:], RSTD[:]), ["RSTD"], ["RSTD"])
            V(lambda e: e.reciprocal(RSTD[:], RSTD[:]), ["RSTD"], ["RSTD"])
            for k in range(8):
                V(lambda e, k=k: e.scalar_tensor_tensor(XO[:, k, :], XO[:, k, :], GF[:, k:k + 1], RSTD[:], ALU.mult, ALU.mult), ["RSTD", "GF"], ["XT"])
        S.dma("sp", lambda e, to=to: e.dma_start(out=xo_v[:, :, to], in_=XO[:]), reads=["XT"], writes=["xo_%d" % bi])


def build_layer(last, dbg=False):
    nc = bass.Bass("TRN2", target_bir_lowering=False)
    io = {}

    def din(name, shape, dt=F32):
        io[name] = nc.dram_tensor(name, shape, dt, kind="ExternalInput").ap()

    def scr(name, shape):
        io[name] = nc.dram_tensor(name, shape, F32, kind=("ExternalOutput" if dbg else "Internal")).ap()
    nctx = 0 if last else 128
    ntok = 2048 + nctx
    din("xT", [D, SEQT]); din("cT", [128, 16]); din("w_mod", [D, NMOD * D]); din("b_mod", [128, 48]); din("g1", [128, 8]); din("w_in", [D, INW])
    din("ones", [128, 128]); din("ident", [128, 128]); din("tau", [128, 512]); din("iota16", [128, 16])
    din("prm", [2, 128, 24]); din("bre", [2, 128, 128]); din("bim", [2, 128, 128]); din("cre", [2, 128, 128]); din("cim", [2, 128, 128])
    din("wg", [2, 2, 16, 128]); din("bg", [2, 2, 128, 1])
    din("rst", [128, 512]); din("tmask", [64, 256]); din("tmask2", [64, 256]); din("blk", [128, 256]); din("hmask", [128, 4])
    din("w_out", [D, D]); din("wsT", [128, 512]); din("sgub", [128, 4]); din("s5d", [128, 2]); din("wglu", [256, 512]); din("ng", [128, 512])
    din("g2", [128, 8]); din("wq", [D, 2048]); din("keysT", [128, 2048]); din("eu", [NEXP, D]); din("ev", [NEXP, D]); din("gfin", [128, 8])
    scr("FMU", [256, SEQT]); scr("FMQ", [256, SEQT]); scr("FMK", [256, SEQT]); scr("FMZ", [32, SEQT]); scr("VT", [SEQT, 512]); scr("TL", [OWN, 1024])
    scr("MODS", [128, 96]); scr("YF", [256, SEQT]); scr("YB", [256, SEQT]); scr("OF", [SEQT, 512]); scr("OB", [SEQT, 512]); scr("GS", [2, 16])
    io["xo"] = nc.dram_tensor("xo", [D, ntok], F32, kind="ExternalOutput").ap()
    with ExitStack() as top:
        gate = top.enter_context(nc.semaphore("gate"))
        ph = [0]

        def phase(fn):
            with ExitStack() as st:
                S = Sched(nc, top, gate, 16 * ph[0])
                sb, ps = _mk(nc, st)
                fn(S, sb, ps)
                S.drain_all("sp")
                GS = io["GS"]
                S.prog["sp"].append(("i", lambda e: e.dma_start(out=GS[0:1, :], in_=GS[1:2, :]), "gate", 16))
                S.emit()
            ph[0] += 1
        phase(lambda S, sb, ps: emit_A(nc, S, sb, ps, io))
        phase(lambda S, sb, ps: emit_B(nc, S, sb, ps, io))
        phase(lambda S, sb, ps: emit_C(nc, S, sb, ps, io))
        if last:
            blocks = [(SEQC + i * 128, 128 + i * 128, i * 128, False) for i in range(16)]
        else:
            blocks = [(0, 0, 0, True)] + [(SEQC + i * 128, 128 + i * 128, 128 + i * 128, False) for i in range(16)]
        phase(lambda S, sb, ps: emit_D(nc, S, sb, ps, io, blocks, last))
    return nc


def _lay_vec(v, n):
    return np.ascontiguousarray(np.asarray(v, np.float32).reshape(n, 128).T)


_CONST = {}


def _consts():
    if not _CONST:
        rst = np.ones((128, 512), np.float32)
        rst[:, ::64] = 0
        tm = (np.arange(64)[None, :] >= np.arange(64)[:, None]).astype(np.float32)
        _CONST.update(
            ones=np.ones((128, 128), np.float32), ident=np.eye(128, dtype=np.float32),
            iota16=np.ascontiguousarray(np.broadcast_to(np.arange(16, dtype=np.float32), (128, 16))),
            tau=np.ascontiguousarray(np.broadcast_to(np.arange(1, 513, dtype=np.float32), (128, 512))),
            rst=rst, tmask=np.ascontiguousarray(np.tile(tm, (1, 4))), tmask2=np.ascontiguousarray(np.tile(tm.T, (1, 4))),
            blk=np.kron(np.eye(4, dtype=np.float32), np.ones((32, 64), np.float32)),
            hmask=np.kron(np.eye(4, dtype=np.float32), np.ones((32, 1), np.float32)))
    return _CONST


def _pb_params(P, l, gh, swap):
    def stt(a):
        if swap:
            a = a[::-1]
        g = a[:, gh * 8:gh * 8 + 8]
        g = g.reshape((2, 4, 2, 64) + a.shape[3:])
        g = np.moveaxis(g, (2, 3), (0, 1))
        return np.ascontiguousarray(g.reshape((128, 2, 4) + a.shape[3:]))
    lre = stt(P["s5_lambda_re"][l]); lim = stt(P["s5_lambda_im"][l])
    ls = stt(np.broadcast_to(P["s5_log_step"][l][:, :, None], (2, 16, 64)))
    prm = np.ascontiguousarray(np.stack([lre, lim, ls], -1).reshape(128, 24).astype(np.float32))
    return dict(prm=prm, bre=stt(P["s5_b_re"][l]).reshape(128, 128), bim=stt(P["s5_b_im"][l]).reshape(128, 128),
                cre=stt(np.swapaxes(P["s5_c_re"][l], 2, 3)).reshape(128, 128), cim=stt(np.swapaxes(P["s5_c_im"][l], 2, 3)).reshape(128, 128))


def _layer_weights(P, l, swap):
    C = _consts()
    sk = P["peer_sub_keys"][l]
    keysT = np.zeros((128, 16, 128), np.float32)
    for h in range(8):
        for p in range(2):
            keysT[:, h * 2 + p, :] = sk[p, h].T
    sw = P["sgu_w"][l]
    sbb = P["sgu_b"][l]
    wgate = P["gla_w_gate"][l]
    bgate = P["gla_b_gate"][l]
    if swap:
        sw = sw[:, ::-1, ::-1]
        sbb = sbb[:, ::-1]
        wgate = wgate[::-1]
        bgate = bgate[::-1]
    pb = [_pb_params(P, l, ct, swap) for ct in range(2)]
    w_in = P["w_in"][l]
    if swap:
        w_in = np.ascontiguousarray(np.concatenate([w_in[:, :2304], w_in[:, 2320:2336], w_in[:, 2304:2320]], 1))
    W = dict(w_mod=P["w_mod"][l], b_mod=_lay_vec(P["b_mod"][l], 48), g1=_lay_vec(P["norm1_g"][l], 8), w_in=w_in,
             w_out=P["w_out"][l], wsT=np.ascontiguousarray(np.transpose(sw, (2, 0, 1)).reshape(128, 512)),
             sgub=np.ascontiguousarray(sbb.T), s5d=_lay_vec(P["s5_d"][l], 2), wglu=P["s5_w_glu"][l],
             ng=np.ascontiguousarray(np.broadcast_to(P["gla_norm_g"][l], (128, 512))), g2=_lay_vec(P["norm2_g"][l], 8),
             wq=P["peer_w_query"][l], keysT=keysT.reshape(128, 2048), eu=P["peer_expert_u"][l], ev=P["peer_expert_v"][l],
             gfin=_lay_vec(P["final_norm_g"], 8),
             wg=np.ascontiguousarray(np.stack([[wgate[d][:, hh * 128:hh * 128 + 128] for d in range(2)] for hh in range(2)])),
             bg=np.ascontiguousarray(np.stack([[bgate[d][hh * 128:hh * 128 + 128][:, None] for d in range(2)] for hh in range(2)])))
    for k in ("prm", "bre", "bim", "cre", "cim"):
        W[k] = np.ascontiguousarray(np.stack([pb[0][k], pb[1][k]]))
    W.update(C)
    return W


LAYER_W = ["w_mod", "b_mod", "g1", "w_in", "prm", "bre", "bim", "cre", "cim", "wg", "bg", "w_out", "wsT", "sgub", "s5d", "wglu", "ng", "g2",
           "wq", "keysT", "eu", "ev"]
LAYER_W_SHAPES = dict(w_mod=[D, NMOD * D], b_mod=[128, 48], g1=[128, 8], w_in=[D, INW], prm=[2, 128, 24], bre=[2, 128, 128], bim=[2, 128, 128],
                      cre=[2, 128, 128], cim=[2, 128, 128], wg=[2, 2, 16, 128], bg=[2, 2, 128, 1], w_out=[D, D], wsT=[128, 512], sgub=[128, 4],
                      s5d=[128, 2], wglu=[256, 512], ng=[128, 512], g2=[128, 8], wq=[D, 2048], keysT=[128, 2048], eu=[NEXP, D], ev=[NEXP, D])


def build_full():
    nc = bass.Bass("TRN2", target_bir_lowering=False)
    io = {}

    def din(name, shape, dt=F32):
        io[name] = nc.dram_tensor(name, shape, dt, kind="ExternalInput").ap()

    def scr(name, shape):
        io[name] = nc.dram_tensor(name, shape, F32, kind="Internal").ap()
    din("xT", [D, SEQT]); din("cT", [128, 16]); din("gfin", [128, 8])
    din("ones", [128, 128]); din("ident", [128, 128]); din("tau", [128, 512]); din("iota16", [128, 16])
    din("rst", [128, 512]); din("tmask", [64, 256]); din("tmask2", [64, 256]); din("blk", [128, 256]); din("hmask", [128, 4])
    for l in range(2):
        for n in LAYER_W:
            din("%s_%d" % (n, l), LAYER_W_SHAPES[n])
    scr("FMU", [256, SEQT]); scr("FMQ", [256, SEQT]); scr("FMK", [256, SEQT]); scr("FMZ", [32, SEQT]); scr("VT", [SEQT, 512]); scr("TL", [SEQT, 1024])
    scr("MODS", [128, 96]); scr("YF", [256, SEQT]); scr("YB", [256, SEQT]); scr("OF", [SEQT, 512]); scr("OB", [SEQT, 512]); scr("GS", [2, 16])
    scr("X1S", [D, SEQT])
    io["xo"] = nc.dram_tensor("xo", [D, 2048], F32, kind="ExternalOutput").ap()
    with ExitStack() as top:
        gate = top.enter_context(nc.semaphore("gate"))
        ph = [0]

        def phase(fn, nds=None):
            with ExitStack() as st:
                S = Sched(nc, top, gate, 16 * ph[0], nds=nds)
                sb, ps = _mk(nc, st)
                fn(S, sb, ps)
                S.drain_all("sp")
                GS = io["GS"]
                S.prog["sp"].append(("i", lambda e: e.dma_start(out=GS[0:1, :], in_=GS[1:2, :]), "gate", 16))
                S.emit()
            ph[0] += 1
        for l in range(2):
            last = l == 1
            iol = dict(io)
            for n in LAYER_W:
                iol[n] = io["%s_%d" % (n, l)]
            if last:
                iol["xT"] = io["X1S"]
                blocks = [(SEQC + i * 128, 128 + i * 128, i * 128, False) for i in range(16)]
            else:
                iol["xo"] = io["X1S"]
                blocks = [(0, 0, 0, True), (128, 128, 128, True)] + [(SEQC + i * 128, SEQC + i * 128, SEQC + i * 128, False) for i in range(32)]
            n2 = dict(sp=2, act=2, pool=2)
            phase(lambda S, sb, ps: emit_A(nc, S, sb, ps, iol, tl_all=not last), nds=n2)
            phase(lambda S, sb, ps: emit_B(nc, S, sb, ps, iol), nds=n2)
            phase(lambda S, sb, ps: emit_C(nc, S, sb, ps, iol), nds=n2)
            phase(lambda S, sb, ps: emit_D(nc, S, sb, ps, iol, blocks, last), nds=dict(sp=2, act=2, pool=4))
    return nc


_PROG = {}


def _prog(name, fn):
    if name not in _PROG:
        _PROG[name] = fn()
    return _PROG[name]


def kernel(**inputs):
    P = {k: np.ascontiguousarray(np.asarray(v)) for k, v in inputs.items()}
    cores = list(range(8))
    C = _consts()
    ncF = _prog("F", build_full)
    WL = {(l, sw): _layer_weights(P, l, sw) for l in range(2) for sw in (False, True)}
    in_maps = []
    for core in cores:
        b, half = divmod(core, 2)
        xc, xl = P["ctx"][b], P["x"][b]
        seq = np.concatenate([xc, xl], 0) if half == 0 else np.concatenate([xc[::-1], xl[::-1]], 0)
        cT = np.stack([_lay_vec(P["c"][b], 8), _lay_vec(P["c_ctx"], 8)], -1).reshape(128, 16)
        m = dict(xT=np.ascontiguousarray(seq.T), cT=np.ascontiguousarray(cT), gfin=_lay_vec(P["final_norm_g"], 8))
        m.update(C)
        for l in range(2):
            W = WL[l, half == 1]
            for n in LAYER_W:
                m["%s_%d" % (n, l)] = W[n]
        in_maps.append(m)
    rF = run_bass_kernel_spmd(ncF, in_maps, core_ids=cores).results
    out = np.zeros_like(P["x"])
    for core in cores:
        b, half = divmod(core, 2)
        xo = rF[core]["xo"].T
        if half == 0:
            out[b, 0:2048] = xo
        else:
            out[b, 2048:4096] = xo[::-1]
    return out.astype(np.float32)
```

```python
from contextlib import ExitStack
import math
import numpy as np
import concourse.bass as bass
import concourse.mybir as mybir
from concourse.bass_utils import run_bass_kernel_spmd

F32 = mybir.dt.float32
I32 = mybir.dt.int32
U32 = mybir.dt.uint32
ALU = mybir.AluOpType
AF = mybir.ActivationFunctionType
AX = mybir.AxisListType


class Sched:
    NDS = 4

    def __init__(self, nc, stack, gate=None, gate_val=0, nds=None):
        self.nc = nc
        self.ndsq = dict(sp=self.NDS, act=self.NDS, pool=self.NDS)
        if nds:
            self.ndsq.update(nds)
        self.gate = gate
        self.gate_val = gate_val
        self.eng = {"pe": nc.tensor, "dve": nc.vector, "act": nc.scalar,
                    "pool": nc.gpsimd, "sp": nc.sync}
        self.sem = {}
        self.cnt = {}
        _UID[0] += 1
        u = "q%d" % _UID[0]
        for e in self.eng:
            self.sem[e] = stack.enter_context(nc.semaphore(u + "s_" + e))
            self.cnt[e] = 0
        self.dq = {}
        for q in ("sp", "act", "pool"):
            sems = [stack.enter_context(nc.semaphore(u + "d_%s%d" % (q, i))) for i in range(self.ndsq[q])]
            self.dq[q] = {"sems": sems, "n": 0}
            for i, s in enumerate(sems):
                self.sem["d_%s%d" % (q, i)] = s
        self.seen = {e: {} for e in self.eng}
        self.prog = {e: [] for e in self.eng}
        self.lastw = {}
        self.reads = {}
        self.ninst = 0
        if gate is not None:
            self.sem["gate"] = gate
        if gate is not None and gate_val > 0:
            for e in self.eng:
                self.prog[e].append(("w", "gate", gate_val))

    def _need(self, need, ev):
        if ev is None:
            return
        s, v = ev
        if need.get(s, 0) < v:
            need[s] = v

    def _waits(self, e, reads, writes):
        need = {}
        for k in reads:
            self._need(need, self.lastw.get(k))
        for k in writes:
            self._need(need, self.lastw.get(k))
            for ev in self.reads.get(k, ()):
                self._need(need, ev)
        eng = self.eng[e]
        seen = self.seen[e]
        for s, v in need.items():
            if seen.get(s, 0) >= v:
                continue
            self.prog[e].append(("w", s, v))
            seen[s] = v
            self.ninst += 1

    def _commit(self, ev, reads, writes):
        for k in writes:
            self.lastw[k] = ev
            self.reads[k] = []
        for k in reads:
            if k in writes:
                continue
            self.reads.setdefault(k, []).append(ev)
            if len(self.reads[k]) > 24:
                d = {}
                for s, v in self.reads[k]:
                    if d.get(s, 0) < v:
                        d[s] = v
                self.reads[k] = list(d.items())

    def op(self, e, fn, reads=(), writes=()):
        reads = tuple(reads)
        writes = tuple(writes)
        self._waits(e, reads, writes)
        self.cnt[e] += 1
        self.prog[e].append(("i", fn, e, 1))
        self.ninst += 1
        self._commit((e, self.cnt[e]), reads, writes)

    def dma(self, q, fn, reads=(), writes=()):
        reads = tuple(reads)
        writes = tuple(writes)
        st = self.dq[q]
        i = st["n"]
        slot = i % self.ndsq[q]
        sname = "d_%s%d" % (q, slot)
        rnd = i // self.ndsq[q]
        eng = self.eng[q]
        if rnd > 0 and self.seen[q].get(sname, 0) < 16 * rnd:
            self.prog[q].append(("w", sname, 16 * rnd))
            self.seen[q][sname] = 16 * rnd
            self.ninst += 1
        self._waits(q, reads, writes)
        self.prog[q].append(("i", fn, sname, 16))
        st["n"] = i + 1
        self.ninst += 1
        self._commit((sname, 16 * (rnd + 1)), reads, writes)

    def finish(self, keys, e="sp"):
        self._waits(e, tuple(keys), ())

    def drain_all(self, e="sp"):
        need = {}
        for en, c in self.cnt.items():
            if c:
                need[en] = c
        for q, stq in self.dq.items():
            n = stq["n"]
            nq = self.ndsq[q]
            for slot in range(nq):
                uses = (n - slot + nq - 1) // nq if n > slot else 0
                if uses:
                    need["d_%s%d" % (q, slot)] = 16 * uses
        eng = self.eng[e]
        for s, v in need.items():
            if self.seen[e].get(s, 0) >= v:
                continue
            self.prog[e].append(("w", s, v))
            self.seen[e][s] = v

    def emit(self):
        nc = self.nc
        with nc.Block() as block:
            def mk(e):
                def body(engine):
                    for it in self.prog[e]:
                        if it[0] == "w":
                            engine.wait_ge(self.sem[it[1]], it[2])
                        else:
                            it[1](engine).then_inc(self.sem[it[2]], it[3])
                return body
            block.sync(mk("sp"))
            block.scalar(mk("act"))
            block.vector(mk("dve"))
            block.gpsimd(mk("pool"))
            block.tensor(mk("pe"))
        self.prog = {e: [] for e in self.eng}

    def sync_all(self):
        for e in self.eng:
            self.drain_all(e)
        self.lastw = {}
        self.reads = {}

D = 1024
NMOD = 6
INW = 2336
INW_T = 19
EPS = 1e-6
NTOK = 2176


_UID = [0]


def _mk(nc, st):
    _UID[0] += 1
    u = "u%d_" % _UID[0]

    def sb(name, shape, dt=F32):
        return st.enter_context(nc.sbuf_tensor(u + name, shape, dt))

    def ps(name, shape, dt=F32):
        return st.enter_context(nc.psum_tensor(u + name, shape, dt))
    return sb, ps


def _groups(n, g=512):
    out = []
    t = 0
    while t < n:
        out.append((t, min(g, n - t)))
        t += g
    return out


def build_PA(ntok=NTOK, nctx=128):
    nc = bass.Bass("TRN2", target_bir_lowering=False)
    xT = nc.dram_tensor("xT", [D, ntok], F32, kind="ExternalInput").ap()
    cT = nc.dram_tensor("cT", [128, 16], F32, kind="ExternalInput").ap()
    w_mod = nc.dram_tensor("w_mod", [D, NMOD * D], F32, kind="ExternalInput").ap()
    b_mod = nc.dram_tensor("b_mod", [128, 48], F32, kind="ExternalInput").ap()
    g1 = nc.dram_tensor("g1", [128, 8], F32, kind="ExternalInput").ap()
    w_in = nc.dram_tensor("w_in", [D, INW], F32, kind="ExternalInput").ap()
    ones = nc.dram_tensor("ones", [128, 128], F32, kind="ExternalInput").ap()
    colsT = nc.dram_tensor("colsT", [INW_T * 128, ntok], F32, kind="ExternalOutput").ap()
    modT = nc.dram_tensor("modT", [128, 96], F32, kind="ExternalOutput").ap()
    with ExitStack() as st:
        S = Sched(nc, st)
        sb, ps = _mk(nc, st)
        CT = sb("CT", [128, 16]); SC = sb("SC", [128, 16]); BM = sb("BM", [128, 48]); G1 = sb("G1", [128, 8])
        ONES = sb("ONES", [128, 128]); MOD = sb("MOD", [128, 96])
        A1 = sb("A1", [128, 16]); TMPA = sb("TMPA", [128, 16])
        WM = [sb("WM%d" % i, [128, 8, 512]) for i in range(2)]
        WIN = sb("WIN", [128, 8, INW])
        XT = [sb("XT%d" % i, [128, 8, 512]) for i in range(2)]
        XSQ = sb("XSQ", [128, 512]); RSTD = sb("RSTD", [128, 512]); TMP = sb("TMP", [128, 512])
        HT = [sb("HT%d" % i, [128, 8, 512]) for i in range(2)]
        OUTB = [sb("OUTB%d" % i, [128, 512]) for i in range(4)]
        pmod = ps("pmod", [128, 96]); pss = ps("pss", [128, 512])
        pout = [ps("pout%d" % i, [128, 512]) for i in range(3)]

        S.dma("sp", lambda e: e.dma_start(out=CT[:], in_=cT), writes=["CT"])
        S.dma("sp", lambda e: e.dma_start(out=BM[:], in_=b_mod), writes=["BM"])
        S.dma("sp", lambda e: e.dma_start(out=G1[:], in_=g1), writes=["G1"])
        S.dma("sp", lambda e: e.dma_start(out=ONES[:], in_=ones), writes=["ONES"])
        S.op("act", lambda e: e.activation(SC[:], CT[:], AF.Silu), reads=["CT"], writes=["SC"])
        SC3 = SC[:].rearrange("p (k c) -> p k c", c=2)
        wm_v = w_mod.rearrange("(k p) f -> p k f", p=128)
        for jg in range(12):
            b = jg % 2
            S.dma("act" if jg % 2 else "sp",
                  lambda e, jg=jg, b=b: e.dma_start(out=WM[b][:], in_=wm_v[:, :, jg * 512:(jg + 1) * 512]),
                  writes=["WM%d" % b])
            for j8 in range(4):
                j = jg * 4 + j8
                for k in range(8):
                    S.op("pe", lambda e, j=j, j8=j8, k=k, b=b: e.matmul(
                        pmod[:, 2 * j:2 * j + 2], WM[b][:, k, j8 * 128:(j8 + 1) * 128], SC3[:, k, :],
                        start=(k == 0), stop=(k == 7)), reads=["WM%d" % b, "SC"], writes=["pmod"])
        S.op("dve", lambda e: e.tensor_tensor(MOD[:].rearrange("p (j c) -> p j c", c=2),
                                              pmod[:].rearrange("p (j c) -> p j c", c=2),
                                              BM[:].unsqueeze(2).to_broadcast([128, 48, 2]), ALU.add),
             reads=["pmod", "BM"], writes=["MOD"])
        S.dma("sp", lambda e: e.dma_start(out=modT, in_=MOD[:]), reads=["MOD"], writes=["modT"])
        MOD3 = MOD[:].rearrange("p (j c) -> p j c", c=2)
        S.op("dve", lambda e: e.tensor_scalar(TMPA[:].rearrange("p (j c) -> p j c", c=2), MOD3[:, 8:16, :], 1.0, None, ALU.add),
             reads=["MOD"], writes=["TMPA"])
        S.op("dve", lambda e: e.tensor_tensor(A1[:].rearrange("p (j c) -> p j c", c=2),
                                              TMPA[:].rearrange("p (j c) -> p j c", c=2),
                                              G1[:].unsqueeze(2).to_broadcast([128, 8, 2]), ALU.mult),
             reads=["TMPA", "G1"], writes=["A1"])
        A13 = A1[:].rearrange("p (j c) -> p j c", c=2)
        win_v = w_in.rearrange("(k p) f -> p k f", p=128)
        for k in range(8):
            S.dma("pool", lambda e, k=k: e.dma_start(out=WIN[:, k, :], in_=win_v[:, k, :]), writes=["WIN%d" % k])
        xT_v = xT.rearrange("(k p) t -> p k t", p=128)
        grp = [(0, nctx, 1)] if nctx else []
        grp += [(nctx + t0, tn, 0) for (t0, tn) in _groups(ntok - nctx)]
        oi = 0
        for gi, (t0, tn, col) in enumerate(grp):
            b = gi % 2
            xk, hk = "XT%d" % b, "HT%d" % b
            S.dma("sp", lambda e, b=b, t0=t0, tn=tn: e.dma_start(out=XT[b][:, :, :tn], in_=xT_v[:, :, t0:t0 + tn]), writes=[xk])
            for k in range(8):
                S.op("act", lambda e, b=b, k=k, tn=tn: e.activation(XSQ[:, :tn], XT[b][:, k, :tn], AF.Square), reads=[xk], writes=["XSQ"])
                S.op("pe", lambda e, k=k, tn=tn: e.matmul(pss[:, :tn], ONES[:], XSQ[:, :tn], start=(k == 0), stop=(k == 7)),
                     reads=["ONES", "XSQ"], writes=["pss"])
            S.op("dve", lambda e, tn=tn: e.tensor_scalar(RSTD[:, :tn], pss[:, :tn], 1.0 / D, EPS, ALU.mult, ALU.add), reads=["pss"], writes=["RSTD"])
            S.op("act", lambda e, tn=tn: e.sqrt(RSTD[:, :tn], RSTD[:, :tn]), reads=["RSTD"], writes=["RSTD"])
            S.op("dve", lambda e, tn=tn: e.reciprocal(RSTD[:, :tn], RSTD[:, :tn]), reads=["RSTD"], writes=["RSTD"])
            for k in range(8):
                S.op("dve", lambda e, b=b, k=k, tn=tn: e.tensor_tensor(TMP[:, :tn], XT[b][:, k, :tn], RSTD[:, :tn], ALU.mult),
                     reads=[xk, "RSTD"], writes=["TMP"])
                S.op("act", lambda e, b=b, k=k, tn=tn, col=col: e.activation(
                    HT[b][:, k, :tn], TMP[:, :tn], AF.Identity, bias=MOD3[:, k, col:col + 1], scale=A13[:, k, col:col + 1]),
                    reads=["TMP", "MOD", "A1"], writes=[hk])
            for ot in range(INW_T):
                m = 128 if ot < INW_T - 1 else INW - 128 * (INW_T - 1)
                pb = oi % 3
                ob = oi % 4
                oi += 1
                for k in range(8):
                    S.op("pe", lambda e, b=b, k=k, tn=tn, ot=ot, m=m, pb=pb: e.matmul(
                        pout[pb][:m, :tn], WIN[:, k, ot * 128:ot * 128 + m], HT[b][:, k, :tn], start=(k == 0), stop=(k == 7)),
                        reads=["WIN%d" % k, hk], writes=["pout%d" % pb])
                if m < 128:
                    S.op("dve", lambda e, ob=ob: e.memset(OUTB[ob][:], 0.0), writes=["OUTB%d" % ob])
                eng = "act" if oi % 2 else "dve"
                if eng == "act":
                    S.op("act", lambda e, ob=ob, pb=pb, m=m, tn=tn: e.copy(OUTB[ob][:m, :tn], pout[pb][:m, :tn]),
                         reads=["pout%d" % pb], writes=["OUTB%d" % ob])
                else:
                    S.op("dve", lambda e, ob=ob, pb=pb, m=m, tn=tn: e.tensor_copy(OUTB[ob][:m, :tn], pout[pb][:m, :tn]),
                         reads=["pout%d" % pb], writes=["OUTB%d" % ob])
                S.dma("sp" if oi % 2 else "act", lambda e, ob=ob, ot=ot, t0=t0, tn=tn: e.dma_start(
                    out=colsT[ot * 128:(ot + 1) * 128, t0:t0 + tn], in_=OUTB[ob][:, :tn]),
                    reads=["OUTB%d" % ob], writes=["colsT_%d" % oi])
        S.drain_all("sp")
        S.emit()
    return nc


SEQT = 4352
S5_CH = [(0, 256)] + [(256 + 512 * i, 512) for i in range(8)]


def build_PB():
    nc = bass.Bass("TRN2", target_bir_lowering=False)
    uin = [nc.dram_tensor(n, [128, SEQT], F32, kind="ExternalInput").ap() for n in ("uf", "ub")]
    prm = nc.dram_tensor("prm", [128, 24], F32, kind="ExternalInput").ap()
    bre = nc.dram_tensor("bre", [128, 128], F32, kind="ExternalInput").ap()
    bim = nc.dram_tensor("bim", [128, 128], F32, kind="ExternalInput").ap()
    cre = nc.dram_tensor("cre", [128, 128], F32, kind="ExternalInput").ap()
    cim = nc.dram_tensor("cim", [128, 128], F32, kind="ExternalInput").ap()
    tau = nc.dram_tensor("tau", [128, 512], F32, kind="ExternalInput").ap()
    ident = nc.dram_tensor("ident", [128, 128], F32, kind="ExternalInput").ap()
    yout = [nc.dram_tensor(n, [128, SEQT], F32, kind="ExternalOutput").ap() for n in ("yf", "yb")]
    TWO_PI = 2.0 * math.pi
    with ExitStack() as st:
        S = Sched(nc, st)
        sb, ps = _mk(nc, st)
        PRM = sb("PRM", [128, 24]); BRE = sb("BRE", [128, 128]); BIM = sb("BIM", [128, 128])
        CRE = sb("CRE", [128, 128]); CIM = sb("CIM", [128, 128]); TAU = sb("TAU", [128, 512]); ID = sb("ID", [128, 128])
        names = ["DT", "LR", "MAG", "TH", "R", "R2", "RF", "FR", "SIN", "COS", "ARE", "AIM", "DEN", "AM1", "FRE", "FIM", "T0", "T1"]
        P = {n: sb("p_" + n, [128, 8]) for n in names}
        RI = sb("p_RI", [128, 8], I32)
        BBR = sb("BBR", [128, 128]); BBI = sb("BBI", [128, 128]); TB = sb("TB", [128, 128])
        PAD = sb("PAD", [128, 128])
        WBR = sb("WBR", [128, 8, 128]); WBI = sb("WBI", [128, 8, 128]); CR = sb("CR", [128, 8, 128]); CIN = sb("CIN", [128, 8, 128])
        TC = sb("TC", [128, 8, 512]); TS = sb("TS", [128, 8, 512]); RHO = sb("RHO", [128, 8, 512])
        RR = sb("RR", [128, 512]); RRF = sb("RRF", [128, 512]); RRI = sb("RRI", [128, 512], I32)
        UC = [sb("UC%d" % i, [128, 512]) for i in range(2)]
        W = {}
        for n in ("BR", "BI", "T1", "T2", "T3", "T4", "XR", "XI", "QR", "QI", "HR", "HI"):
            for i in range(2):
                W[n, i] = sb("w_%s%d" % (n, i), [128, 512])
        HP = sb("HP", [128, 8])
        YO = [sb("YO%d" % i, [128, 512]) for i in range(2)]
        pbr = [ps("pbr%d" % i, [128, 512]) for i in range(2)]
        pbi = [ps("pbi%d" % i, [128, 512]) for i in range(2)]
        py = [ps("py%d" % i, [128, 512]) for i in range(2)]
        ptr = ps("ptr", [128, 128])

        for (t, src, k) in ((PRM, prm, "PRM"), (BRE, bre, "BRE"), (BIM, bim, "BIM"), (CRE, cre, "CRE"), (CIM, cim, "CIM"),
                            (TAU, tau, "TAU"), (ID, ident, "ID")):
            S.dma("sp", lambda e, t=t, src=src: e.dma_start(out=t[:], in_=src), writes=[k])
        PR3 = PRM[:].rearrange("p (a c) -> p a c", c=3)
        K = ["PP"]

        def V(fn, reads=(), writes=()):
            S.op("dve", fn, reads=list(reads) + K, writes=list(writes) + K)

        def A(fn, reads=(), writes=()):
            S.op("act", fn, reads=list(reads) + K, writes=list(writes) + K)

        A(lambda e: e.activation(P["DT"][:], PR3[:, :, 2], AF.Exp), reads=["PRM"])
        V(lambda e: e.tensor_scalar(P["LR"][:], PR3[:, :, 0], -1e-4, None, ALU.min), reads=["PRM"])
        V(lambda e: e.tensor_tensor(P["T0"][:], P["LR"][:], P["DT"][:], ALU.mult))
        A(lambda e: e.activation(P["MAG"][:], P["T0"][:], AF.Exp))
        V(lambda e: e.tensor_tensor(P["TH"][:], PR3[:, :, 1], P["DT"][:], ALU.mult), reads=["PRM"])
        V(lambda e: e.tensor_scalar(P["R"][:], P["TH"][:], 1.0 / TWO_PI, None, ALU.mult))
        V(lambda e: e.tensor_scalar(P["R2"][:], P["R"][:], 0.25, None, ALU.add))
        for (src, dst) in (("R", "SIN"), ("R2", "COS")):
            V(lambda e, src=src: e.tensor_copy(RI[:], P[src][:]))
            V(lambda e: e.tensor_copy(P["RF"][:], RI[:]))
            V(lambda e, src=src: e.tensor_tensor(P["FR"][:], P[src][:], P["RF"][:], ALU.subtract))
            A(lambda e, dst=dst: e.activation(P[dst][:], P["FR"][:], AF.Sin, scale=TWO_PI))
        V(lambda e: e.tensor_tensor(P["ARE"][:], P["MAG"][:], P["COS"][:], ALU.mult))
        V(lambda e: e.tensor_tensor(P["AIM"][:], P["MAG"][:], P["SIN"][:], ALU.mult))
        V(lambda e: e.tensor_tensor(P["T0"][:], P["LR"][:], P["LR"][:], ALU.mult))
        V(lambda e: e.tensor_tensor(P["T1"][:], PR3[:, :, 1], PR3[:, :, 1], ALU.mult), reads=["PRM"])
        V(lambda e: e.tensor_tensor(P["DEN"][:], P["T0"][:], P["T1"][:], ALU.add))
        V(lambda e: e.reciprocal(P["DEN"][:], P["DEN"][:]))
        V(lambda e: e.tensor_scalar(P["AM1"][:], P["ARE"][:], -1.0, None, ALU.add))
        V(lambda e: e.tensor_tensor(P["T0"][:], P["AM1"][:], P["LR"][:], ALU.mult))
        V(lambda e: e.tensor_tensor(P["T1"][:], P["AIM"][:], PR3[:, :, 1], ALU.mult), reads=["PRM"])
        V(lambda e: e.tensor_tensor(P["T0"][:], P["T0"][:], P["T1"][:], ALU.add))
        V(lambda e: e.tensor_tensor(P["FRE"][:], P["T0"][:], P["DEN"][:], ALU.mult))
        V(lambda e: e.tensor_tensor(P["T0"][:], P["AIM"][:], P["LR"][:], ALU.mult))
        V(lambda e: e.tensor_tensor(P["T1"][:], P["AM1"][:], PR3[:, :, 1], ALU.mult), reads=["PRM"])
        V(lambda e: e.tensor_tensor(P["T0"][:], P["T0"][:], P["T1"][:], ALU.subtract))
        V(lambda e: e.tensor_tensor(P["FIM"][:], P["T0"][:], P["DEN"][:], ALU.mult))

        def v3(t):
            return t[:].rearrange("p (a h) -> p a h", h=16)

        def bc(n):
            return P[n][:].unsqueeze(2).to_broadcast([128, 8, 16])
        V(lambda e: e.tensor_tensor(v3(BBR), v3(BRE), bc("FRE"), ALU.mult), reads=["BRE"])
        V(lambda e: e.tensor_tensor(v3(TB), v3(BIM), bc("FIM"), ALU.mult), reads=["BIM"])
        V(lambda e: e.tensor_tensor(BBR[:], BBR[:], TB[:], ALU.subtract))
        V(lambda e: e.tensor_tensor(v3(BBI), v3(BIM), bc("FRE"), ALU.mult), reads=["BIM"])
        V(lambda e: e.tensor_tensor(v3(TB), v3(BRE), bc("FIM"), ALU.mult), reads=["BRE"])
        V(lambda e: e.tensor_tensor(BBI[:], BBI[:], TB[:], ALU.add))
        V(lambda e: e.tensor_scalar(CIM[:], CIM[:], -1.0, None, ALU.mult), reads=["CIM"], writes=["CIM"])
        V(lambda e: e.memset(CR[:], 0.0)); V(lambda e: e.memset(CIN[:], 0.0))
        for dj in range(8):
            j = dj % 4
            for (src, dst) in ((BBR, WBR), (BBI, WBI)):
                V(lambda e: e.memset(PAD[:], 0.0), writes=["PAD"])
                V(lambda e, src=src, dj=dj, j=j: e.tensor_copy(PAD[0:64, 32 * j:32 * j + 16], src[0:64, dj * 16:dj * 16 + 16]), writes=["PAD"])
                V(lambda e, src=src, dj=dj, j=j: e.tensor_copy(PAD[64:128, 32 * j + 16:32 * j + 32], src[64:128, dj * 16:dj * 16 + 16]), writes=["PAD"])
                S.op("pe", lambda e: e.transpose(ptr[:], PAD[:], ID[:]), reads=["PAD", "ID"], writes=["ptr"])
                S.op("act", lambda e, dst=dst, dj=dj: e.copy(dst[:, dj, :], ptr[:]), reads=["ptr"], writes=["WB"])
            for (src, dst) in ((CRE, CR), (CIM, CIN)):
                V(lambda e, src=src, dst=dst, dj=dj, j=j: e.tensor_copy(dst[0:64, dj, 32 * j:32 * j + 16], src[0:64, dj * 16:dj * 16 + 16]), reads=["CRE", "CIM"], writes=["CC"])
                V(lambda e, src=src, dst=dst, dj=dj, j=j: e.tensor_copy(dst[64:128, dj, 32 * j + 16:32 * j + 32], src[64:128, dj * 16:dj * 16 + 16]), reads=["CRE", "CIM"], writes=["CC"])
            for (off, dst) in ((0.0, TS), (0.25, TC)):
                V(lambda e, dj=dj, off=off: e.tensor_scalar(RR[:], TAU[:], P["R"][:, dj:dj + 1], off, ALU.mult, ALU.add), reads=["TAU"], writes=["RR"])
                V(lambda e: e.tensor_copy(RRI[:], RR[:]), reads=["RR"], writes=["RRI"])
                V(lambda e: e.tensor_copy(RRF[:], RRI[:]), reads=["RRI"], writes=["RRF"])
                V(lambda e: e.tensor_tensor(RRF[:], RR[:], RRF[:], ALU.subtract), reads=["RR"], writes=["RRF"])
                S.op("act", lambda e, dst=dst, dj=dj: e.activation(dst[:, dj, :], RRF[:], AF.Sin, scale=TWO_PI), reads=["RRF"], writes=["TAB"])
            V(lambda e, dj=dj: e.tensor_copy(RHO[:, dj, :], P["MAG"][:, dj:dj + 1].to_broadcast([128, 512])), writes=["TAB"])

        G = "pool"
        oi = 0
        for d in range(2):
            V(lambda e: e.memset(HP[:], 0.0), writes=["HP"])
            for ci, (t0, T) in enumerate(S5_CH):
                ub_ = (d * 9 + ci) % 2
                uk = "UC%d" % ub_
                S.dma("sp", lambda e, d=d, t0=t0, T=T, ub_=ub_: e.dma_start(out=UC[ub_][:, :T], in_=uin[d][:, t0:t0 + T]), writes=[uk])
                yb_ = (d * 9 + ci) % 2
                for j in range(4):
                    dj = d * 4 + j
                    b = j % 2
                    w = lambda n, b=b, T=T: W[n, b][:, :T]
                    k = lambda n, b=b: "w_%s%d" % (n, b)
                    S.op("pe", lambda e, dj=dj, b=b, T=T, ub_=ub_: e.matmul(pbr[b][:, :T], WBR[:, dj, :], UC[ub_][:, :T], start=True, stop=True),
                         reads=["WB", uk], writes=["pbr%d" % b])
                    S.op("pe", lambda e, dj=dj, b=b, T=T, ub_=ub_: e.matmul(pbi[b][:, :T], WBI[:, dj, :], UC[ub_][:, :T], start=True, stop=True),
                         reads=["WB", uk], writes=["pbi%d" % b])
                    S.op("act", lambda e, w=w, b=b, T=T: e.copy(w("BR"), pbr[b][:, :T]), reads=["pbr%d" % b], writes=[k("BR")])
                    S.op("act", lambda e, w=w, b=b, T=T: e.copy(w("BI"), pbi[b][:, :T]), reads=["pbi%d" % b], writes=[k("BI")])
                    cs = lambda dj=dj, T=T: TC[:, dj, :T]
                    sn = lambda dj=dj, T=T: TS[:, dj, :T]
                    S.op("dve", lambda e, w=w, cs=cs: e.tensor_tensor(w("T1"), cs(), w("BR"), ALU.mult), reads=["TAB", k("BR")], writes=[k("T1")])
                    S.op("dve", lambda e, w=w, sn=sn: e.tensor_tensor(w("T2"), sn(), w("BI"), ALU.mult), reads=["TAB", k("BI")], writes=[k("T2")])
                    S.op("dve", lambda e, w=w: e.tensor_tensor(w("XR"), w("T1"), w("T2"), ALU.add), reads=[k("T1"), k("T2")], writes=[k("XR")])
                    S.op(G, lambda e, w=w, cs=cs: e.tensor_tensor(w("T3"), cs(), w("BI"), ALU.mult), reads=["TAB", k("BI")], writes=[k("T3")])
                    S.op(G, lambda e, w=w, sn=sn: e.tensor_tensor(w("T4"), sn(), w("BR"), ALU.mult), reads=["TAB", k("BR")], writes=[k("T4")])
                    S.op(G, lambda e, w=w: e.tensor_tensor(w("XI"), w("T3"), w("T4"), ALU.subtract), reads=[k("T3"), k("T4")], writes=[k("XI")])
                    S.op("dve", lambda e, w=w, dj=dj, j=j, T=T: e.tensor_tensor_scan(w("QR"), RHO[:, dj, :T], w("XR"), HP[:, 2 * j:2 * j + 1], ALU.mult, ALU.add),
                         reads=["TAB", k("XR"), "HP"], writes=[k("QR")])
                    S.op("dve", lambda e, w=w, dj=dj, j=j, T=T: e.tensor_tensor_scan(w("QI"), RHO[:, dj, :T], w("XI"), HP[:, 2 * j + 1:2 * j + 2], ALU.mult, ALU.add),
                         reads=["TAB", k("XI"), "HP"], writes=[k("QI")])
                    S.op("dve", lambda e, w=w, cs=cs: e.tensor_tensor(w("T1"), cs(), w("QR"), ALU.mult), reads=["TAB", k("QR")], writes=[k("T1")])
                    S.op("dve", lambda e, w=w, sn=sn: e.tensor_tensor(w("T2"), sn(), w("QI"), ALU.mult), reads=["TAB", k("QI")], writes=[k("T2")])
                    S.op("dve", lambda e, w=w: e.tensor_tensor(w("HR"), w("T1"), w("T2"), ALU.subtract), reads=[k("T1"), k("T2")], writes=[k("HR")])
                    S.op(G, lambda e, w=w, sn=sn: e.tensor_tensor(w("T3"), sn(), w("QR"), ALU.mult), reads=["TAB", k("QR")], writes=[k("T3")])
                    S.op(G, lambda e, w=w, cs=cs: e.tensor_tensor(w("T4"), cs(), w("QI"), ALU.mult), reads=["TAB", k("QI")], writes=[k("T4")])
                    S.op(G, lambda e, w=w: e.tensor_tensor(w("HI"), w("T3"), w("T4"), ALU.add), reads=[k("T3"), k("T4")], writes=[k("HI")])
                    S.op("act", lambda e, b=b, j=j, T=T: e.copy(HP[:, 2 * j:2 * j + 1], W["HR", b][:, T - 1:T]), reads=[k("HR")], writes=["HP"])
                    S.op("act", lambda e, b=b, j=j, T=T: e.copy(HP[:, 2 * j + 1:2 * j + 2], W["HI", b][:, T - 1:T]), reads=[k("HI")], writes=["HP"])
                    S.op("pe", lambda e, dj=dj, w=w, j=j, yb_=yb_, T=T: e.matmul(py[yb_][:, :T], CR[:, dj, :], w("HR"), start=(j == 0), stop=False),
                         reads=["CC", k("HR")], writes=["py%d" % yb_])
                    S.op("pe", lambda e, dj=dj, w=w, j=j, yb_=yb_, T=T: e.matmul(py[yb_][:, :T], CIN[:, dj, :], w("HI"), start=False, stop=(j == 3)),
                         reads=["CC", k("HI")], writes=["py%d" % yb_])
                S.op("act", lambda e, yb_=yb_, T=T: e.copy(YO[yb_][:, :T], py[yb_][:, :T]), reads=["py%d" % yb_], writes=["YO%d" % yb_])
                oi += 1
                S.dma("act", lambda e, d=d, yb_=yb_, t0=t0, T=T: e.dma_start(out=yout[d][:, t0:t0 + T], in_=YO[yb_][:, :T]),
                      reads=["YO%d" % yb_], writes=["yout_%d" % oi])
        S.drain_all("sp")
        S.emit()
    return nc


NCH = 68


def build_PC():
    nc = bass.Bass("TRN2", target_bir_lowering=False)
    I = {}
    for d in range(2):
        I["qT", d] = nc.dram_tensor("qT%d" % d, [128, SEQT], F32, kind="ExternalInput").ap()
        I["kT", d] = nc.dram_tensor("kT%d" % d, [128, SEQT], F32, kind="ExternalInput").ap()
        I["v", d] = nc.dram_tensor("v%d" % d, [SEQT, 256], F32, kind="ExternalInput").ap()
        I["zT", d] = nc.dram_tensor("zT%d" % d, [16, SEQT], F32, kind="ExternalInput").ap()
        I["wg", d] = nc.dram_tensor("wg%d" % d, [16, 128], F32, kind="ExternalInput").ap()
        I["bg", d] = nc.dram_tensor("bg%d" % d, [128, 1], F32, kind="ExternalInput").ap()
        I["o", d] = nc.dram_tensor("o%d" % d, [SEQT, 256], F32, kind="ExternalOutput").ap()
    rst = nc.dram_tensor("rst", [128, 512], F32, kind="ExternalInput").ap()
    tmask = nc.dram_tensor("tmask", [64, 256], F32, kind="ExternalInput").ap()
    blk = nc.dram_tensor("blk", [128, 256], F32, kind="ExternalInput").ap()
    hmask = nc.dram_tensor("hmask", [128, 4], F32, kind="ExternalInput").ap()
    ident = nc.dram_tensor("ident", [128, 128], F32, kind="ExternalInput").ap()
    QSC = 32 ** -0.5
    with ExitStack() as st:
        S = Sched(nc, st)
        sb, ps = _mk(nc, st)
        RST = sb("RST", [128, 512]); TM = sb("TM", [64, 256]); BLK = sb("BLK", [128, 256]); HM = sb("HM", [128, 4]); ID = sb("ID", [128, 128])
        WG = sb("WG", [16, 128]); BG = sb("BG", [128, 1]); NBG = sb("NBG", [128, 1])
        Wb = {}
        for n in ("Q", "K", "LA", "B", "E", "D", "QE", "QS", "KD", "KS0", "KS1", "KS2", "KS3"):
            for i in range(2):
                Wb[n, i] = sb("g_%s%d" % (n, i), [128, 512])
        Z = [sb("Z%d" % i, [16, 512]) for i in range(2)]
        VV = [sb("VV%d" % i, [64, 8, 256]) for i in range(2)]
        DEC = [sb("DEC%d" % i, [128, 8]) for i in range(2)]
        KDT = [sb("KDT%d" % i, [64, 128]) for i in range(2)]
        STt = [sb("ST%d" % i, [64, 256]) for i in range(2)]
        OB = [sb("OB%d" % i, [64, 256]) for i in range(3)]
        KVM = sb("KVM", [128, 256])
        SS = [sb("SS%d" % i, [128, 256]) for i in range(2)]
        pza = ps("pza", [128, 512])
        pt0 = ps("pt0", [64, 128])
        pt = [pt0, pt0]
        pkv = [ps("pkv%d" % i, [128, 256]) for i in range(2)]
        psc = [ps("psc%d" % i, [64, 256]) for i in range(2)]
        po = [ps("po%d" % i, [64, 256]) for i in range(2)]
        for (t, src, k) in ((RST, rst, "RST"), (TM, tmask, "TM"), (BLK, blk, "BLK"), (HM, hmask, "HM"), (ID, ident, "ID")):
            S.dma("sp", lambda e, t=t, src=src: e.dma_start(out=t[:], in_=src), writes=[k])
        gc = 0
        oi = 0
        for d in range(2):
            S.dma("sp", lambda e, d=d: e.dma_start(out=WG[:], in_=I["wg", d]), writes=["WG"])
            S.dma("sp", lambda e, d=d: e.dma_start(out=BG[:], in_=I["bg", d]), writes=["BG"])
            S.op("dve", lambda e: e.tensor_scalar(NBG[:], BG[:], -1.0, None, ALU.mult), reads=["BG"], writes=["NBG"])
            S.op("dve", lambda e: e.memset(SS[0][:], 0.0), writes=["SS0"])
            scur = 0
            for bi, (t0, T) in enumerate(S5_CH):
                nchk = T // 64
                b = (d * 9 + bi) % 2
                w = lambda n, b=b, T=T: Wb[n, b][:, :T]
                k = lambda n, b=b: "g_%s%d" % (n, b)
                w3 = lambda n, b=b, T=T: Wb[n, b][:, :T].rearrange("p (c s) -> p c s", s=64)
                S.dma("sp", lambda e, d=d, b=b, t0=t0, T=T: e.dma_start(out=Wb["Q", b][:, :T], in_=I["qT", d][:, t0:t0 + T]), writes=[k("Q")])
                S.dma("act", lambda e, d=d, b=b, t0=t0, T=T: e.dma_start(out=Wb["K", b][:, :T], in_=I["kT", d][:, t0:t0 + T]), writes=[k("K")])
                S.dma("sp", lambda e, d=d, b=b, t0=t0, T=T: e.dma_start(out=Z[b][:, :T], in_=I["zT", d][:, t0:t0 + T]), writes=["Z%d" % b])
                S.dma("act", lambda e, d=d, b=b, t0=t0, T=T, nchk=nchk: e.dma_start(
                    out=VV[b][:, :nchk, :], in_=I["v", d][t0:t0 + T, :].rearrange("(c s) f -> s c f", s=64)), writes=["VV%d" % b])
                S.op("pe", lambda e, b=b, T=T: e.matmul(pza[:, :T], WG[:], Z[b][:, :T], start=True, stop=True), reads=["WG", "Z%d" % b], writes=["pza"])
                S.op("act", lambda e, w=w, T=T: e.activation(w("E"), pza[:, :T], AF.Exp, bias=NBG[:], scale=-1.0), reads=["pza", "NBG"], writes=[k("E")])
                S.op("act", lambda e, w=w: e.activation(w("E"), w("E"), AF.Ln, bias=1.0), reads=[k("E")], writes=[k("E")])
                S.op("dve", lambda e, w=w: e.tensor_scalar(w("LA"), w("E"), -1.0 / 16.0, None, ALU.mult), reads=[k("E")], writes=[k("LA")])
                S.op("dve", lambda e, w=w, T=T: e.tensor_tensor_scan(w("B"), RST[:, :T], w("LA"), 0.0, ALU.mult, ALU.add), reads=["RST", k("LA")], writes=[k("B")])
                S.op("act", lambda e, b=b, w3=w3, nchk=nchk: e.activation(DEC[b][:, :nchk], w3("B")[:, :, 63], AF.Exp), reads=[k("B")], writes=["DEC%d" % b])
                S.op("act", lambda e, w=w: e.activation(w("E"), w("B"), AF.Exp), reads=[k("B")], writes=[k("E")])
                S.op("dve", lambda e, w=w: e.scalar_tensor_tensor(w("QE"), w("Q"), QSC, w("E"), ALU.mult, ALU.mult), reads=[k("Q"), k("E")], writes=[k("QE")])
                S.op("dve", lambda e, w3=w3, nchk=nchk: e.tensor_tensor(w3("D"), w3("B"), w3("B")[:, :, 32:33].to_broadcast([128, nchk, 64]), ALU.subtract),
                     reads=[k("B")], writes=[k("D")])
                S.op("act", lambda e, w=w: e.activation(w("E"), w("D"), AF.Exp), reads=[k("D"), k("QE")], writes=[k("E")])
                S.op("dve", lambda e, w=w: e.scalar_tensor_tensor(w("QS"), w("Q"), QSC, w("E"), ALU.mult, ALU.mult), reads=[k("Q"), k("E")], writes=[k("QS")])
                S.op("act", lambda e, w=w: e.activation(w("E"), w("D"), AF.Exp, scale=-1.0), reads=[k("D"), k("QS")], writes=[k("E")])
                S.op("dve", lambda e, w=w: e.tensor_tensor(w("LA"), w("K"), w("E"), ALU.mult), reads=[k("K"), k("E"), k("B")], writes=[k("LA")])
                for h in range(4):
                    S.op("pool", lambda e, w=w, h=h: e.tensor_scalar(w("KS%d" % h), w("LA"), HM[:, h:h + 1], None, ALU.mult),
                         reads=[k("LA"), "HM"], writes=[k("KS%d" % h)])
                S.op("dve", lambda e, w3=w3, nchk=nchk: e.tensor_tensor(w3("D"), w3("B")[:, :, 63:64].to_broadcast([128, nchk, 64]), w3("B"), ALU.subtract),
                     reads=[k("B"), k("E"), k("LA")], writes=[k("D")])
                S.op("act", lambda e, w=w: e.activation(w("D"), w("D"), AF.Exp), reads=[k("D")], writes=[k("D")])
                S.op("dve", lambda e, w=w: e.tensor_tensor(w("KD"), w("K"), w("D"), ALU.mult), reads=[k("K"), k("D")], writes=[k("KD")])
                for c in range(nchk):
                    p2 = gc % 2
                    gc += 1
                    cs = slice(c * 64, (c + 1) * 64)
                    S.op("pe", lambda e, b=b, cs=cs, p2=p2: e.transpose(pt[p2][:], Wb["KD", b][:, cs], ID[:]), reads=[k("KD"), "ID"], writes=["pt"])
                    S.op("act", lambda e, p2=p2: e.copy(KDT[p2][:], pt[p2][:]), reads=["pt"], writes=["KDT%d" % p2])
                    S.op("pe", lambda e, b=b, c=c, p2=p2: e.matmul(pkv[p2][:], KDT[p2][:], VV[b][:, c, :], start=True, stop=True),
                         reads=["KDT%d" % p2, "VV%d" % b], writes=["pkv%d" % p2])
                    for h in range(4):
                        S.op("pe", lambda e, b=b, cs=cs, p2=p2, h=h: e.matmul(psc[p2][:, h * 64:(h + 1) * 64], Wb["KS%d" % h, b][:, cs], Wb["QS", b][:, cs],
                                                                            start=True, stop=True),
                             reads=[k("KS%d" % h), k("QS")], writes=["psc%d" % p2])
                    S.op("dve", lambda e, p2=p2: e.tensor_tensor(STt[p2][:], psc[p2][:], TM[:], ALU.mult), reads=["psc%d" % p2, "TM"], writes=["ST%d" % p2])
                    for h in range(4):
                        hs = slice(h * 64, (h + 1) * 64)
                        S.op("pe", lambda e, b=b, c=c, p2=p2, hs=hs: e.matmul(po[p2][:, hs], STt[p2][:, hs], VV[b][:, c, hs], start=True, stop=False),
                             reads=["ST%d" % p2, "VV%d" % b], writes=["po%d" % p2])
                        S.op("pe", lambda e, b=b, cs=cs, p2=p2, hs=hs, scur=scur: e.matmul(po[p2][:, hs], Wb["QE", b][:, cs], SS[scur][:, hs], start=False, stop=True),
                             reads=[k("QE"), "SS%d" % scur], writes=["po%d" % p2])
                    ob = oi % 3
                    oi += 1
                    S.op("act", lambda e, ob=ob, p2=p2: e.copy(OB[ob][:], po[p2][:]), reads=["po%d" % p2], writes=["OB%d" % ob])
                    S.dma("sp" if oi % 2 else "act", lambda e, d=d, ob=ob, t0=t0, c=c: e.dma_start(out=I["o", d][t0 + c * 64:t0 + (c + 1) * 64, :], in_=OB[ob][:]),
                          reads=["OB%d" % ob], writes=["o_%d" % oi])
                    S.op("dve", lambda e, p2=p2: e.tensor_tensor(KVM[:], pkv[p2][:], BLK[:], ALU.mult), reads=["pkv%d" % p2, "BLK"], writes=["KVM"])
                    S.op("dve", lambda e, b=b, c=c, scur=scur: e.scalar_tensor_tensor(SS[1 - scur][:], SS[scur][:], DEC[b][:, c:c + 1], KVM[:], ALU.mult, ALU.add),
                         reads=["SS%d" % scur, "DEC%d" % b, "KVM"], writes=["SS%d" % (1 - scur)])
                    scur = 1 - scur
        S.drain_all("sp")
        S.emit()
    return nc


NEXP = 16384


def build_PD(ntok, nctx, last):
    nc = bass.Bass("TRN2", target_bir_lowering=False)
    def din(name, shape, dt=F32):
        return nc.dram_tensor(name, shape, dt, kind="ExternalInput").ap()
    xT = din("xT", [D, ntok]); modT = din("modT", [128, 96])
    su = din("su", [ntok, 256]); sv = din("sv", [ntok, 256]); gg = din("gg", [ntok, 512])
    s5u = din("s5u", [256, ntok]); yf = din("yf", [256, ntok]); yb = din("yb", [256, ntok])
    of_ = din("of", [ntok, 512]); ob_ = din("ob", [ntok, 512])
    w_out = din("w_out", [D, D]); wsT = din("wsT", [128, 512]); sgub = din("sgub", [128, 4]); s5d = din("s5d", [128, 2])
    wglu = din("wglu", [256, 512]); ng = din("ng", [128, 512]); g2 = din("g2", [128, 8]); wq = din("wq", [D, 2048])
    keysT = din("keysT", [128, 2048]); eu = din("eu", [NEXP, D]); ev = din("ev", [NEXP, D]); gfin = din("gfin", [128, 8])
    ones = din("ones", [128, 128]); ident = din("ident", [128, 128]); iota16 = din("iota16", [128, 16])
    xo = nc.dram_tensor("xo", [D, ntok], F32, kind="ExternalOutput").ap()
    nb = ntok // 128
    with ExitStack() as st:
        S = Sched(nc, st)
        sb, ps = _mk(nc, st)
        MOD = sb("MOD", [128, 96]); WOUT = sb("WOUT", [128, 8, D]); WS = sb("WS", [128, 512]); SGUB = sb("SGUB", [128, 4]); S5D = sb("S5D", [128, 2])
        WGLU = sb("WGLU", [128, 2, 512]); NG = sb("NG", [128, 512]); G2 = sb("G2", [128, 8]); WQ = sb("WQ", [128, 8, 2048]); KEYS = sb("KEYS", [128, 2048])
        GF = sb("GF", [128, 8]); ONES = sb("ONES", [128, 128]); ID = sb("ID", [128, 128]); IOTA = sb("IOTA", [128, 16])
        A2 = sb("A2", [128, 16]); TA = sb("TA", [128, 16])
        U_ = sb("U_", [128, 256]); V_ = sb("V_", [128, 256]); GU = sb("GU", [128, 256]); GV = sb("GV", [128, 256]); SQ = sb("SQ", [128, 512])
        SS = sb("SS", [128, 8]); VN = sb("VN", [128, 256]); MIX = sb("MIX", [128, D])
        S5U = sb("S5U", [128, 2, 128]); YF = sb("YF", [128, 2, 128]); YB = sb("YB", [128, 2, 128]); GE = sb("GE", [128, 2, 128]); SG = sb("SG", [128, 256])
        OF = sb("OF", [128, 512]); OB = sb("OB", [128, 512]); GG = sb("GG", [128, 512]); SL = sb("SL", [128, 512])
        XT = sb("XT", [128, 8, 128]); X1 = sb("X1", [128, 8, 128]); XO = XT; HT = sb("HT", [128, 8, 128]); MIXT = HT; HTOK = sb("HTOK", [128, D])
        RSTD = sb("RSTD", [128, 128]); TMPB = sb("TMPB", [128, 128])
        SC = sb("SC", [128, 2048])
        M16 = sb("M16", [128, 256]); I16 = sb("I16", [128, 256], U32); IF16 = sb("IF16", [128, 256]); I1S = sb("I1S", [128, 128])
        CS = sb("CS", [128, 2048]); SC2 = CS; QT = CS[:].rearrange("p (q t) -> p q t", t=128); CS2 = sb("CS2", [128, 256])
        T16 = sb("T16", [128, 128]); P16 = sb("P16", [128, 128], U32); PF = sb("PF", [128, 128]); AI = sb("AI", [128, 128], I32)
        AFL = sb("AFL", [128, 128]); BFL = sb("BFL", [128, 128]); E1 = sb("E1", [128, 128]); E2 = sb("E2", [128, 128])
        EG = sb("EG", [128, 128]); GATE = sb("GATE", [128, 128]); IDXTI = sb("IDXTI", [128, 128], I32); GATET = sb("GATET", [128, 128])
        ACTT = sb("ACTT", [128, 128]); WT = sb("WT", [128, 128])
        UG = [sb("UG%d" % i, [128, D]) for i in range(2)]; VG = UG
        HB = [sb("HB%d" % i, [128, D]) for i in range(2)]
        P0 = ps("P0", [128, 2048]); P1 = ps("P1", [128, 1024]); P2 = ps("P2", [128, 512]); P3 = ps("P3", [128, 512])

        def V(fn, r=(), w=()):
            S.op("dve", fn, reads=r, writes=w)

        def A(fn, r=(), w=()):
            S.op("act", fn, reads=r, writes=w)

        def PE(fn, r=(), w=()):
            S.op("pe", fn, reads=r, writes=w)

        def LD(q, t, src, key):
            S.dma(q, lambda e: e.dma_start(out=t, in_=src), writes=[key])

        LD("sp", MOD[:], modT, "MOD"); LD("sp", WS[:], wsT, "WS"); LD("sp", SGUB[:], sgub, "SGUB"); LD("sp", S5D[:], s5d, "S5D")
        LD("sp", WGLU[:], wglu.rearrange("(c p) f -> p c f", p=128), "WGLU"); LD("sp", NG[:], ng, "NG"); LD("sp", G2[:], g2, "G2")
        LD("sp", KEYS[:], keysT, "KEYS"); LD("sp", GF[:], gfin, "GF"); LD("sp", ONES[:], ones, "ONES"); LD("sp", ID[:], ident, "ID"); LD("sp", IOTA[:], iota16, "IOTA")
        wo_v = w_out.rearrange("(k p) f -> p k f", p=128)
        wq_v = wq.rearrange("(k p) f -> p k f", p=128)
        for k in range(8):
            LD("act", WOUT[:, k, :], wo_v[:, k, :], "WOUT")
            LD("act", WQ[:, k, :], wq_v[:, k, :], "WQ")
        MOD3 = MOD[:].rearrange("p (j c) -> p j c", c=2)
        V(lambda e: e.tensor_scalar(TA[:].rearrange("p (j c) -> p j c", c=2), MOD3[:, 32:40, :], 1.0, None, ALU.add), ["MOD"], ["TA"])
        V(lambda e: e.tensor_tensor(A2[:].rearrange("p (j c) -> p j c", c=2), TA[:].rearrange("p (j c) -> p j c", c=2),
                                    G2[:].unsqueeze(2).to_broadcast([128, 8, 2]), ALU.mult), ["TA", "G2"], ["A2"])
        A23 = A2[:].rearrange("p (j c) -> p j c", c=2)

        def rs_from_ss(ss, n, scale):
            V(lambda e: e.tensor_scalar(ss, ss, scale, EPS, ALU.mult, ALU.add), ["SS"], ["SS"])
            A(lambda e: e.sqrt(ss, ss), ["SS"], ["SS"])
            V(lambda e: e.reciprocal(ss, ss), ["SS"], ["SS"])

        def top16(src, scratch, mout, iout, n):
            V(lambda e: e.max(mout[:, 0:8], src), ["TK"], ["TK"])
            V(lambda e: e.max_index(iout[:, 0:8], mout[:, 0:8], src), ["TK"], ["TK"])
            V(lambda e: e.match_replace(scratch, mout[:, 0:8], src, -1e30), ["TK"], ["TK"])
            V(lambda e: e.max(mout[:, 8:16], scratch), ["TK"], ["TK"])
            V(lambda e: e.max_index(iout[:, 8:16], mout[:, 8:16], scratch), ["TK"], ["TK"])

        xT_v = xT.rearrange("(k p) t -> p k t", p=128)
        xo_v = xo.rearrange("(k p) t -> p k t", p=128)
        s5u_v = s5u.rearrange("(c p) t -> p c t", p=128)
        yf_v = yf.rearrange("(c p) t -> p c t", p=128)
        yb_v = yb.rearrange("(c p) t -> p c t", p=128)
        for bi in range(nb):
            tk = slice(bi * 128, (bi + 1) * 128)
            col = 1 if bi * 128 < nctx else 0
            LD("sp", U_[:], su[tk, :], "U_"); LD("sp", V_[:], sv[tk, :], "V_"); LD("sp", GG[:], gg[tk, :], "GG")
            LD("act", S5U[:], s5u_v[:, :, tk], "S5U"); LD("act", YF[:], yf_v[:, :, tk], "YF"); LD("act", YB[:], yb_v[:, :, tk], "YB")
            LD("sp", OF[:], of_[tk, :], "OF"); LD("sp", OB[:], ob_[tk, :], "OB"); LD("act", XT[:], xT_v[:, :, tk], "XT")
            A(lambda e: e.activation(GU[:], U_[:], AF.Gelu), ["U_"], ["GU"])
            A(lambda e: e.activation(GV[:], V_[:], AF.Gelu), ["V_"], ["GV"])
            V(lambda e: e.tensor_tensor(SQ[:, 0:256], GV[:], GV[:], ALU.mult), ["GV"], ["SQ"])
            V(lambda e: e.tensor_reduce(SS[:, 0:4], SQ[:, 0:256].rearrange("p (h d) -> p h d", d=64), AX.X, ALU.add), ["SQ"], ["SS"])
            rs_from_ss(SS[:, 0:4], 4, 1.0 / 64)
            V(lambda e: e.tensor_tensor(VN[:].rearrange("p (h d) -> p h d", d=64), GV[:].rearrange("p (h d) -> p h d", d=64),
                                        SS[:, 0:4].unsqueeze(2).to_broadcast([128, 4, 64]), ALU.mult), ["GV", "SS"], ["VN"])
            for h in range(4):
                PE(lambda e, h=h: e.matmul(P2[:, h * 64:(h + 1) * 64], WS[:, h * 128:(h + 1) * 128], VN[:, h * 64:(h + 1) * 64], start=True, stop=True),
                   ["WS", "VN"], ["P2"])
            V(lambda e: e.tensor_tensor(MIX[:, 0:256].rearrange("p (h d) -> p h d", d=64), P2[:, 0:256].rearrange("p (h d) -> p h d", d=64),
                                        SGUB[:].unsqueeze(2).to_broadcast([128, 4, 64]), ALU.add), ["P2", "SGUB"], ["MIXa"])
            V(lambda e: e.tensor_tensor(MIX[:, 0:256], MIX[:, 0:256], GU[:], ALU.mult), ["GU"], ["MIXa"])
            V(lambda e: e.tensor_tensor(YF[:], YF[:], YB[:], ALU.add), ["YB"], ["YF"])
            for ct in range(2):
                V(lambda e, ct=ct: e.scalar_tensor_tensor(YF[:, ct, :], S5U[:, ct, :], S5D[:, ct:ct + 1], YF[:, ct, :], ALU.mult, ALU.add),
                  ["S5U", "S5D"], ["YF"])
            A(lambda e: e.activation(GE[:], YF[:], AF.Gelu), ["YF"], ["GE"])
            for ct in range(2):
                PE(lambda e, ct=ct: e.matmul(P3[:, 0:512], GE[:, ct, :], WGLU[:, ct, :], start=(ct == 0), stop=(ct == 1)), ["GE", "WGLU"], ["P3"])
            A(lambda e: e.activation(SG[:], P3[:, 256:512], AF.Sigmoid), ["P3"], ["SG"])
            V(lambda e: e.tensor_tensor(MIX[:, 256:512], P3[:, 0:256], SG[:], ALU.mult), ["P3", "SG"], ["MIXb"])
            V(lambda e: e.tensor_tensor(OF[:], OF[:], OB[:], ALU.add), ["OB"], ["OF"])
            V(lambda e: e.tensor_tensor(SQ[:], OF[:], OF[:], ALU.mult), ["OF"], ["SQ"])
            V(lambda e: e.tensor_reduce(SS[:, 0:8], SQ[:].rearrange("p (h d) -> p h d", d=64), AX.X, ALU.add), ["SQ"], ["SS"])
            rs_from_ss(SS[:, 0:8], 8, 1.0 / 64)
            V(lambda e: e.tensor_tensor(OF[:].rearrange("p (h d) -> p h d", d=64), OF[:].rearrange("p (h d) -> p h d", d=64),
                                        SS[:, 0:8].unsqueeze(2).to_broadcast([128, 8, 64]), ALU.mult), ["SS"], ["OF"])
            V(lambda e: e.tensor_tensor(OF[:], OF[:], NG[:], ALU.mult), ["NG"], ["OF"])
            A(lambda e: e.activation(SL[:], GG[:], AF.Silu), ["GG"], ["SL"])
            V(lambda e: e.tensor_tensor(MIX[:, 512:1024], OF[:], SL[:], ALU.mult), ["OF", "SL"], ["MIXc"])
            for f in range(8):
                pp, pk = (P2, "P2") if f % 2 == 0 else (P3, "P3")
                PE(lambda e, f=f, pp=pp: e.transpose(pp[:, 0:128], MIX[:, f * 128:(f + 1) * 128], ID[:]), ["MIXa", "MIXb", "MIXc", "ID"], [pk])
                A(lambda e, f=f, pp=pp: e.copy(MIXT[:, f, :], pp[:, 0:128]), [pk], ["HT"])
            for ot in range(8):
                pp, pk = (P2, "P2") if ot % 2 == 0 else (P3, "P3")
                for k in range(8):
                    PE(lambda e, ot=ot, k=k, pp=pp: e.matmul(pp[:, 0:128], WOUT[:, k, ot * 128:(ot + 1) * 128], MIXT[:, k, :], start=(k == 0), stop=(k == 7)),
                       ["WOUT", "HT"], [pk])
                V(lambda e, ot=ot, pp=pp, col=col: e.scalar_tensor_tensor(X1[:, ot, :], pp[:, 0:128], MOD3[:, 16 + ot, col:col + 1], XT[:, ot, :], ALU.mult, ALU.add),
                  [pk, "MOD", "XT"], ["X1"])
            for k in range(8):
                A(lambda e, k=k: e.activation(TMPB[:], X1[:, k, :], AF.Square), ["X1"], ["TMPB"])
                PE(lambda e, k=k: e.matmul(P2[:, 0:128], ONES[:], TMPB[:], start=(k == 0), stop=(k == 7)), ["ONES", "TMPB"], ["P2"])
            V(lambda e: e.tensor_scalar(RSTD[:], P2[:, 0:128], 1.0 / D, EPS, ALU.mult, ALU.add), ["P2"], ["RSTD"])
            A(lambda e: e.sqrt(RSTD[:], RSTD[:]), ["RSTD"], ["RSTD"])
            V(lambda e: e.reciprocal(RSTD[:], RSTD[:]), ["RSTD"], ["RSTD"])
            for k in range(8):
                V(lambda e, k=k: e.tensor_tensor(TMPB[:], X1[:, k, :], RSTD[:], ALU.mult), ["X1", "RSTD"], ["TMPB"])
                A(lambda e, k=k, col=col: e.activation(HT[:, k, :], TMPB[:], AF.Identity, bias=MOD3[:, 24 + k, col:col + 1], scale=A23[:, k, col:col + 1]),
                  ["TMPB", "MOD", "A2"], ["HT"])
            for k in range(8):
                pp, pk = (P2, "P2") if k % 2 == 0 else (P3, "P3")
                PE(lambda e, k=k, pp=pp: e.transpose(pp[:, 0:128], HT[:, k, :], ID[:]), ["HT", "ID"], [pk])
                A(lambda e, k=k, pp=pp: e.copy(HTOK[:, k * 128:(k + 1) * 128], pp[:, 0:128]), [pk], ["HTOK"])
            for qt in range(16):
                pp, pk = (P2, "P2") if qt % 2 == 0 else (P3, "P3")
                for k in range(8):
                    PE(lambda e, qt=qt, k=k, pp=pp: e.matmul(pp[:, 0:128], WQ[:, k, qt * 128:(qt + 1) * 128], HT[:, k, :], start=(k == 0), stop=(k == 7)),
                       ["WQ", "HT"], [pk])
                if qt % 2 == 0:
                    A(lambda e, qt=qt, pp=pp: e.copy(QT[:, qt, :], pp[:, 0:128]), [pk], ["TK"])
                else:
                    V(lambda e, qt=qt, pp=pp: e.tensor_copy(QT[:, qt, :], pp[:, 0:128]), [pk], ["TK"])
            for qt in range(16):
                PE(lambda e, qt=qt: e.matmul(P0[:, qt * 128:(qt + 1) * 128], QT[:, qt, :], KEYS[:, qt * 128:(qt + 1) * 128], start=True, stop=True),
                   ["TK", "KEYS"], ["P0a", "P0b"])
            for q4 in range(4):
                A(lambda e, q4=q4: e.copy(SC[:, q4 * 512:(q4 + 1) * 512], P0[:, q4 * 512:(q4 + 1) * 512]), ["P0a", "P0b"], ["TK"])
            for qt in range(16):
                top16(SC[:, qt * 128:(qt + 1) * 128], SC2[:, qt * 128:(qt + 1) * 128], M16[:, qt * 16:(qt + 1) * 16], I16[:, qt * 16:(qt + 1) * 16], 128)
            V(lambda e: e.tensor_copy(IF16[:], I16[:]), ["TK"], ["TK"])
            M4 = M16[:].rearrange("p (h q k) -> p h q k", q=2, k=16)
            IF4 = IF16[:].rearrange("p (h q k) -> p h q k", q=2, k=16)
            I1S3 = I1S[:].rearrange("p (h k) -> p h k", k=16)
            V(lambda e: e.tensor_scalar(I1S3, IF4[:, :, 0, :], 128.0, None, ALU.mult), ["TK"], ["TK"])
            CS4 = CS[:].rearrange("p (h a b) -> p h a b", a=16, b=16)
            V(lambda e: e.tensor_tensor(CS4, M4[:, :, 0, :].unsqueeze(3).to_broadcast([128, 8, 16, 16]),
                                        M4[:, :, 1, :].unsqueeze(2).to_broadcast([128, 8, 16, 16]), ALU.add), ["TK"], ["TK"])
            for h in range(8):
                top16(CS[:, h * 256:(h + 1) * 256], CS2[:], T16[:, h * 16:(h + 1) * 16], P16[:, h * 16:(h + 1) * 16], 256)
            V(lambda e: e.tensor_copy(PF[:], P16[:]), ["TK"], ["TK"])
            V(lambda e: e.tensor_scalar(AFL[:], PF[:], -7.5, 1.0 / 16, ALU.add, ALU.mult), ["TK"], ["TK"])
            V(lambda e: e.tensor_copy(AI[:], AFL[:]), ["TK"], ["TK"])
            V(lambda e: e.tensor_copy(AFL[:], AI[:]), ["TK"], ["TK"])
            V(lambda e: e.scalar_tensor_tensor(BFL[:], AFL[:], -16.0, PF[:], ALU.mult, ALU.add), ["TK"], ["TK"])
            EQ4 = CS[:].rearrange("p (h k a) -> p h k a", k=16, a=16)
            io4 = IOTA[:].unsqueeze(1).unsqueeze(1).to_broadcast([128, 8, 16, 16])
            for (sel, src, dst) in ((AFL, I1S3, E1), (BFL, IF4[:, :, 1, :], E2)):
                V(lambda e, sel=sel: e.tensor_tensor(EQ4, io4, sel[:].rearrange("p (h k) -> p h k", k=16).unsqueeze(3).to_broadcast([128, 8, 16, 16]), ALU.is_equal),
                  ["TK", "IOTA"], ["TK"])
                V(lambda e, src=src: e.tensor_tensor(EQ4, EQ4, src.unsqueeze(2).to_broadcast([128, 8, 16, 16]), ALU.mult), ["TK"], ["TK"])
                V(lambda e, dst=dst: e.tensor_reduce(dst[:], CS[:].rearrange("p (m a) -> p m a", a=16), AX.X, ALU.add), ["TK"], ["TK"])
            V(lambda e: e.tensor_tensor(E1[:], E1[:], E2[:], ALU.add), ["TK"], ["TK"])
            T3 = T16[:].rearrange("p (h k) -> p h k", k=16)
            V(lambda e: e.tensor_tensor(EG[:].rearrange("p (h k) -> p h k", k=16), T3, T3[:, :, 0:1].to_broadcast([128, 8, 16]), ALU.subtract), ["TK"], ["TK"])
            A(lambda e: e.activation(EG[:], EG[:], AF.Exp), ["TK"], ["TK"])
            V(lambda e: e.tensor_reduce(SS[:, 0:8], EG[:].rearrange("p (h k) -> p h k", k=16), AX.X, ALU.add), ["TK"], ["SS"])
            V(lambda e: e.reciprocal(SS[:, 0:8], SS[:, 0:8]), ["SS"], ["SS"])
            V(lambda e: e.tensor_tensor(GATE[:].rearrange("p (h k) -> p h k", k=16), EG[:].rearrange("p (h k) -> p h k", k=16),
                                        SS[:, 0:8].unsqueeze(2).to_broadcast([128, 8, 16]), ALU.mult), ["TK", "SS"], ["GATE"])
            PE(lambda e: e.transpose(P2[:, 0:128], E1[:], ID[:]), ["TK", "ID"], ["P2"])
            V(lambda e: e.tensor_copy(IDXTI[:], P2[:, 0:128]), ["P2"], ["IDXTI"])
            PE(lambda e: e.transpose(P3[:, 0:128], GATE[:], ID[:]), ["GATE", "ID"], ["P3"])
            A(lambda e: e.copy(GATET[:], P3[:, 0:128]), ["P3"], ["GATET"])
            for t in range(128):
                b = t % 2
                S.dma("pool", lambda e, t=t, b=b: e.indirect_dma_start(out=UG[b][:], out_offset=None, in_=eu,
                                                                       in_offset=bass.IndirectOffsetOnAxis(ap=IDXTI[:, t:t + 1], axis=0)),
                      reads=["IDXTI"], writes=["UG%d" % b])
                pk = "P0a" if b == 0 else "P0b"
                for hf in range(2):
                    PE(lambda e, t=t, b=b, hf=hf: e.matmul(P0[:, b * 1024 + hf * 512:b * 1024 + (hf + 1) * 512], ID[:, t:t + 1].to_broadcast([128, 128]),
                                                           HTOK[:, hf * 512:(hf + 1) * 512], start=True, stop=True), ["ID", "HTOK"], [pk])
                    A(lambda e, b=b, hf=hf: e.copy(HB[b][:, hf * 512:(hf + 1) * 512], P0[:, b * 1024 + hf * 512:b * 1024 + (hf + 1) * 512]), [pk], ["HB%d" % b])
                V(lambda e, t=t, b=b: e.scalar_tensor_tensor(UG[b][:], UG[b][:], 1.0, HB[b][:], ALU.mult, ALU.mult, accum_out=ACTT[:, t:t + 1]),
                  ["HB%d" % b], ["UG%d" % b, "ACTT"])
            A(lambda e: e.activation(WT[:], ACTT[:], AF.Gelu), ["ACTT"], ["WT"])
            V(lambda e: e.tensor_tensor(WT[:], WT[:], GATET[:], ALU.mult), ["GATET"], ["WT"])
            for t in range(128):
                b = t % 2
                S.dma("pool", lambda e, t=t, b=b: e.indirect_dma_start(out=VG[b][:], out_offset=None, in_=ev,
                                                                       in_offset=bass.IndirectOffsetOnAxis(ap=IDXTI[:, t:t + 1], axis=0)),
                      reads=["IDXTI"], writes=["UG%d" % b])
                for ot in range(8):
                    PE(lambda e, t=t, b=b, ot=ot: e.matmul(P1[:, ot * 128 + t:ot * 128 + t + 1], VG[b][:, ot * 128:(ot + 1) * 128], WT[:, t:t + 1],
                                                           start=True, stop=True), ["UG%d" % b, "WT"], ["P1"])
            for ot in range(8):
                V(lambda e, ot=ot, col=col: e.scalar_tensor_tensor(XO[:, ot, :], P1[:, ot * 128:(ot + 1) * 128], MOD3[:, 40 + ot, col:col + 1], X1[:, ot, :],
                                                                   ALU.mult, ALU.add), ["P1", "MOD", "X1"], ["XT"])
            if last:
                for k in range(8):
                    A(lambda e, k=k: e.activation(TMPB[:], XO[:, k, :], AF.Square), ["XT"], ["TMPB"])
                    PE(lambda e, k=k: e.matmul(P2[:, 0:128], ONES[:], TMPB[:], start=(k == 0), stop=(k == 7)), ["ONES", "TMPB"], ["P2"])
                V(lambda e: e.tensor_scalar(RSTD[:], P2[:, 0:128], 1.0 / D, EPS, ALU.mult, ALU.add), ["P2"], ["RSTD"])
                A(lambda e: e.sqrt(RSTD[:], RSTD[:]), ["RSTD"], ["RSTD"])
                V(lambda e: e.reciprocal(RSTD[:], RSTD[:]), ["RSTD"], ["RSTD"])
                for k in range(8):
                    V(lambda e, k=k: e.scalar_tensor_tensor(XO[:, k, :], XO[:, k, :], GF[:, k:k + 1], RSTD[:], ALU.mult, ALU.mult), ["RSTD", "GF"], ["XT"])
            S.dma("sp", lambda e, tk=tk: e.dma_start(out=xo_v[:, :, tk], in_=XO[:]), reads=["XT"], writes=["xo_%d" % bi])
        S.drain_all("sp")
        S.emit()
    return nc


SEQC = 256
SEQL = 4096
OWN = 2176


def _mirror(t0, T):
    if t0 < SEQC:
        return 0, SEQC
    i = (t0 - SEQC) // 512
    return SEQC + SEQL - 512 * (i + 1), 512


def emit_A(nc, S, sb, ps, io, tl_all=False):
    xT, cT, w_mod, b_mod, g1, w_in, ones = io["xT"], io["cT"], io["w_mod"], io["b_mod"], io["g1"], io["w_in"], io["ones"]
    FMU, FMQ, FMK, FMZ, VT, TL, MODS = io["FMU"], io["FMQ"], io["FMK"], io["FMZ"], io["VT"], io["TL"], io["MODS"]
    CT = sb("CT", [128, 16]); SC = sb("SC", [128, 16]); BM = sb("BM", [128, 48]); G1 = sb("G1", [128, 8])
    ONES = sb("ONES", [128, 128]); MOD = sb("MOD", [128, 96])
    A1 = sb("A1", [128, 16]); TMPA = sb("TMPA", [128, 16])
    WM = [sb("WM%d" % i, [128, 8, 512]) for i in range(2)]
    WIN = sb("WIN", [128, 8, INW])
    XT = [sb("XT%d" % i, [128, 8, 512]) for i in range(2)]
    XSQ = sb("XSQ", [128, 512]); RSTD = sb("RSTD", [128, 512]); TMP = sb("TMP", [128, 512])
    HT = [sb("HT%d" % i, [128, 8, 512]) for i in range(2)]
    OUTB = [sb("OUTB%d" % i, [128, 512]) for i in range(4)]
    pmod = ps("pmod", [128, 96]); pss = ps("pss", [128, 512])
    pout = [ps("pout%d" % i, [128, 512]) for i in range(3)]
    S.dma("sp", lambda e: e.dma_start(out=CT[:], in_=cT), writes=["CT"])
    S.dma("sp", lambda e: e.dma_start(out=BM[:], in_=b_mod), writes=["BM"])
    S.dma("sp", lambda e: e.dma_start(out=G1[:], in_=g1), writes=["G1"])
    S.dma("sp", lambda e: e.dma_start(out=ONES[:], in_=ones), writes=["ONES"])
    S.op("act", lambda e: e.activation(SC[:], CT[:], AF.Silu), reads=["CT"], writes=["SC"])
    SC3 = SC[:].rearrange("p (k c) -> p k c", c=2)
    wm_v = w_mod.rearrange("(k p) f -> p k f", p=128)
    for jg in range(12):
        b = jg % 2
        S.dma("act" if jg % 2 else "sp",
              lambda e, jg=jg, b=b: e.dma_start(out=WM[b][:], in_=wm_v[:, :, jg * 512:(jg + 1) * 512]), writes=["WM%d" % b])
        for j8 in range(4):
            j = jg * 4 + j8
            for k in range(8):
                S.op("pe", lambda e, j=j, j8=j8, k=k, b=b: e.matmul(
                    pmod[:, 2 * j:2 * j + 2], WM[b][:, k, j8 * 128:(j8 + 1) * 128], SC3[:, k, :],
                    start=(k == 0), stop=(k == 7)), reads=["WM%d" % b, "SC"], writes=["pmod"])
    S.op("dve", lambda e: e.tensor_tensor(MOD[:].rearrange("p (j c) -> p j c", c=2), pmod[:].rearrange("p (j c) -> p j c", c=2),
                                          BM[:].unsqueeze(2).to_broadcast([128, 48, 2]), ALU.add), reads=["pmod", "BM"], writes=["MOD"])
    S.dma("sp", lambda e: e.dma_start(out=MODS, in_=MOD[:]), reads=["MOD"], writes=["MODS"])
    MOD3 = MOD[:].rearrange("p (j c) -> p j c", c=2)
    S.op("dve", lambda e: e.tensor_scalar(TMPA[:].rearrange("p (j c) -> p j c", c=2), MOD3[:, 8:16, :], 1.0, None, ALU.add), reads=["MOD"], writes=["TMPA"])
    S.op("dve", lambda e: e.tensor_tensor(A1[:].rearrange("p (j c) -> p j c", c=2), TMPA[:].rearrange("p (j c) -> p j c", c=2),
                                          G1[:].unsqueeze(2).to_broadcast([128, 8, 2]), ALU.mult), reads=["TMPA", "G1"], writes=["A1"])
    A13 = A1[:].rearrange("p (j c) -> p j c", c=2)
    win_v = w_in.rearrange("(k p) f -> p k f", p=128)
    for k in range(8):
        S.dma("pool", lambda e, k=k: e.dma_start(out=WIN[:, k, :], in_=win_v[:, k, :]), writes=["WIN%d" % k])
    WK = ["WIN%d" % k for k in range(8)]
    xT_v = xT.rearrange("(k p) t -> p k t", p=128)
    grp = [(0, SEQC, 1, -1)] + [(SEQC + 512 * g, 512, 0, g) for g in range(8)]
    cnt = {"o": 0}

    def evac(dst_ap_fn, pb, m, tn, key, cm=False):
        ob = cnt["o"] % 4
        cnt["o"] += 1
        if cm:
            o_ap = lambda: OUTB[ob][:m, :512].rearrange("p (w r) -> p w r", r=8)
            i_ap = lambda: pout[pb][:m, :512].rearrange("p (r w) -> p w r", w=64)
        else:
            o_ap = lambda: OUTB[ob][:m, :tn]
            i_ap = lambda: pout[pb][:m, :tn]
        if cnt["o"] % 2:
            S.op("act", lambda e: e.copy(o_ap(), i_ap()), reads=["pout%d" % pb], writes=["OUTB%d" % ob])
        else:
            S.op("dve", lambda e: e.tensor_copy(o_ap(), i_ap()), reads=["pout%d" % pb], writes=["OUTB%d" % ob])
        S.dma("sp" if cnt["o"] % 2 else "act", lambda e: dst_ap_fn(e, OUTB[ob]), reads=["OUTB%d" % ob], writes=["%s_%d" % (key, cnt["o"])])

    pi = 0
    for gi, (t0, tn, col, g) in enumerate(grp):
        b = gi % 2
        xk, hk = "XT%d" % b, "HT%d" % b
        S.dma("sp", lambda e, b=b, t0=t0, tn=tn: e.dma_start(out=XT[b][:, :, :tn], in_=xT_v[:, :, t0:t0 + tn]), writes=[xk])
        for k in range(8):
            S.op("act", lambda e, b=b, k=k, tn=tn: e.activation(XSQ[:, :tn], XT[b][:, k, :tn], AF.Square), reads=[xk], writes=["XSQ"])
            S.op("pe", lambda e, k=k, tn=tn: e.matmul(pss[:, :tn], ONES[:], XSQ[:, :tn], start=(k == 0), stop=(k == 7)), reads=["ONES", "XSQ"], writes=["pss"])
        S.op("dve", lambda e, tn=tn: e.tensor_scalar(RSTD[:, :tn], pss[:, :tn], 1.0 / D, EPS, ALU.mult, ALU.add), reads=["pss"], writes=["RSTD"])
        S.op("act", lambda e, tn=tn: e.sqrt(RSTD[:, :tn], RSTD[:, :tn]), reads=["RSTD"], writes=["RSTD"])
        S.op("dve", lambda e, tn=tn: e.reciprocal(RSTD[:, :tn], RSTD[:, :tn]), reads=["RSTD"], writes=["RSTD"])
        for k in range(8):
            S.op("dve", lambda e, b=b, k=k, tn=tn: e.tensor_tensor(TMP[:, :tn], XT[b][:, k, :tn], RSTD[:, :tn], ALU.mult), reads=[xk, "RSTD"], writes=["TMP"])
            S.op("act", lambda e, b=b, k=k, tn=tn, col=col: e.activation(HT[b][:, k, :tn], TMP[:, :tn], AF.Identity, bias=MOD3[:, k, col:col + 1],
                                                                         scale=A13[:, k, col:col + 1]), reads=["TMP", "MOD", "A1"], writes=[hk])
        fm = [(FMU, 0, 512, 128), (FMU, 128, 640, 128), (FMQ, 0, 768, 128), (FMQ, 128, 896, 128), (FMK, 0, 1024, 128), (FMK, 128, 1152, 128), (FMZ, 0, 2304, 32)]
        for (dst, r0, c0, m) in fm:
            pb = pi % 3
            pi += 1
            for k in range(8):
                S.op("pe", lambda e, b=b, k=k, tn=tn, c0=c0, m=m, pb=pb: e.matmul(pout[pb][:m, :tn], WIN[:, k, c0:c0 + m], HT[b][:, k, :tn], start=(k == 0), stop=(k == 7)),
                     reads=WK + [hk], writes=["pout%d" % pb])
            if dst is FMU or g < 0:
                evac(lambda e, ob, dst=dst, r0=r0, m=m, t0=t0, tn=tn: e.dma_start(out=dst[r0:r0 + m, t0:t0 + tn], in_=ob[:m, :tn]), pb, m, tn, "fm")
            else:
                evac(lambda e, ob, dst=dst, r0=r0, m=m, g=g: e.dma_start(
                    out=dst[r0:r0 + m, SEQC:].rearrange("p (w r) -> p w r", r=64)[:, :, 8 * g:8 * g + 8],
                    in_=ob[:m, :512].rearrange("p (w r) -> p w r", r=8)), pb, m, 512, "fm", cm=True)
        for ti in range(tn // 128):
            tl = [(VT, t0 + ti * 128, 0, 1280)]
            own_row = None
            if tl_all:
                own_row = t0 + ti * 128
            elif g < 0 and ti == 0:
                own_row = 0
            elif 0 <= g < 4:
                own_row = 128 + g * 512 + ti * 128
            if own_row is not None:
                tl += [(TL, own_row, 0, 0), (TL, own_row, 512, 1792)]
            for (dst, row, dc, c0) in tl:
                pb = pi % 3
                pi += 1
                for k in range(8):
                    S.op("pe", lambda e, b=b, k=k, ti=ti, c0=c0, pb=pb: e.matmul(pout[pb][:, :], HT[b][:, k, ti * 128:(ti + 1) * 128], WIN[:, k, c0:c0 + 512],
                                                                               start=(k == 0), stop=(k == 7)), reads=WK + [hk], writes=["pout%d" % pb])
                evac(lambda e, ob, dst=dst, row=row, dc=dc: e.dma_start(out=dst[row:row + 128, dc:dc + 512], in_=ob[:, :]), pb, 128, 512, "tm")


def emit_B(nc, S, sb, ps, io):
    FMU, YFs, YBs = io["FMU"], io["YF"], io["YB"]
    tau, ident = io["tau"], io["ident"]
    for ct in range(2):
        prm, bre, bim, cre, cim = io["prm"][ct], io["bre"][ct], io["bim"][ct], io["cre"][ct], io["cim"][ct]
        uin = [FMU, FMU]
        yout = [YFs, YBs]
        X = "c%d_" % ct
        sub = ExitStack()
        sb, ps = _mk(nc, sub)
        TWO_PI = 2.0 * math.pi
        PRM = sb(X + "PRM", [128, 24]); BRE = sb(X + "BRE", [128, 128]); BIM = sb(X + "BIM", [128, 128])
        CRE = sb(X + "CRE", [128, 128]); CIM = sb(X + "CIM", [128, 128]); TAU = sb(X + "TAU", [128, 512]); ID = sb(X + "ID", [128, 128])
        names = ["DT", "LR", "MAG", "TH", "R", "R2", "RF", "FR", "SIN", "COS", "ARE", "AIM", "DEN", "AM1", "FRE", "FIM", "T0", "T1"]
        P = {n: sb(X + "p_" + n, [128, 8]) for n in names}
        RI = sb(X + "p_RI", [128, 8], I32)
        BBR = sb(X + "BBR", [128, 128]); BBI = sb(X + "BBI", [128, 128]); TB = sb(X + "TB", [128, 128])
        PAD = sb(X + "PAD", [128, 128])
        WBR = sb(X + "WBR", [128, 8, 128]); WBI = sb(X + "WBI", [128, 8, 128]); CR = sb(X + "CR", [128, 8, 128]); CIN = sb(X + "CIN", [128, 8, 128])
        TC = sb(X + "TC", [128, 8, 512]); TS = sb(X + "TS", [128, 8, 512]); RHO = sb(X + "RHO", [128, 8, 512])
        RR = sb(X + "RR", [128, 512]); RRF = sb(X + "RRF", [128, 512]); RRI = sb(X + "RRI", [128, 512], I32)
        UC = [sb(X + "UC%d" % i, [128, 512]) for i in range(2)]
        W = {}
        for n in ("BR", "BI", "T1", "T2", "T3", "T4", "XR", "XI", "QR", "QI", "HR", "HI"):
            for i in range(2):
                W[n, i] = sb(X + "w_%s%d" % (n, i), [128, 512])
        HP = sb(X + "HP", [128, 8])
        YO = [sb(X + "YO%d" % i, [128, 512]) for i in range(2)]
        pbr = [ps(X + "pbr%d" % i, [128, 512]) for i in range(2)]
        pbi = [ps(X + "pbi%d" % i, [128, 512]) for i in range(2)]
        py = [ps(X + "py%d" % i, [128, 512]) for i in range(2)]
        ptr = ps(X + "ptr", [128, 128])

        for (t, src, k) in ((PRM, prm, "PRM"), (BRE, bre, "BRE"), (BIM, bim, "BIM"), (CRE, cre, "CRE"), (CIM, cim, "CIM"),
                            (TAU, tau, "TAU"), (ID, ident, "ID")):
            S.dma("sp", lambda e, t=t, src=src: e.dma_start(out=t[:], in_=src), writes=[k])
        PR3 = PRM[:].rearrange("p (a c) -> p a c", c=3)
        K = ["PP"]

        def V(fn, reads=(), writes=()):
            S.op("dve", fn, reads=list(reads) + K, writes=list(writes) + K)

        def A(fn, reads=(), writes=()):
            S.op("act", fn, reads=list(reads) + K, writes=list(writes) + K)

        A(lambda e: e.activation(P["DT"][:], PR3[:, :, 2], AF.Exp), reads=["PRM"])
        V(lambda e: e.tensor_scalar(P["LR"][:], PR3[:, :, 0], -1e-4, None, ALU.min), reads=["PRM"])
        V(lambda e: e.tensor_tensor(P["T0"][:], P["LR"][:], P["DT"][:], ALU.mult))
        A(lambda e: e.activation(P["MAG"][:], P["T0"][:], AF.Exp))
        V(lambda e: e.tensor_tensor(P["TH"][:], PR3[:, :, 1], P["DT"][:], ALU.mult), reads=["PRM"])
        V(lambda e: e.tensor_scalar(P["R"][:], P["TH"][:], 1.0 / TWO_PI, None, ALU.mult))
        V(lambda e: e.tensor_scalar(P["R2"][:], P["R"][:], 0.25, None, ALU.add))
        for (src, dst) in (("R", "SIN"), ("R2", "COS")):
            V(lambda e, src=src: e.tensor_copy(RI[:], P[src][:]))
            V(lambda e: e.tensor_copy(P["RF"][:], RI[:]))
            V(lambda e, src=src: e.tensor_tensor(P["FR"][:], P[src][:], P["RF"][:], ALU.subtract))
            A(lambda e, dst=dst: e.activation(P[dst][:], P["FR"][:], AF.Sin, scale=TWO_PI))
        V(lambda e: e.tensor_tensor(P["ARE"][:], P["MAG"][:], P["COS"][:], ALU.mult))
        V(lambda e: e.tensor_tensor(P["AIM"][:], P["MAG"][:], P["SIN"][:], ALU.mult))
        V(lambda e: e.tensor_tensor(P["T0"][:], P["LR"][:], P["LR"][:], ALU.mult))
        V(lambda e: e.tensor_tensor(P["T1"][:], PR3[:, :, 1], PR3[:, :, 1], ALU.mult), reads=["PRM"])
        V(lambda e: e.tensor_tensor(P["DEN"][:], P["T0"][:], P["T1"][:], ALU.add))
        V(lambda e: e.reciprocal(P["DEN"][:], P["DEN"][:]))
        V(lambda e: e.tensor_scalar(P["AM1"][:], P["ARE"][:], -1.0, None, ALU.add))
        V(lambda e: e.tensor_tensor(P["T0"][:], P["AM1"][:], P["LR"][:], ALU.mult))
        V(lambda e: e.tensor_tensor(P["T1"][:], P["AIM"][:], PR3[:, :, 1], ALU.mult), reads=["PRM"])
        V(lambda e: e.tensor_tensor(P["T0"][:], P["T0"][:], P["T1"][:], ALU.add))
        V(lambda e: e.tensor_tensor(P["FRE"][:], P["T0"][:], P["DEN"][:], ALU.mult))
        V(lambda e: e.tensor_tensor(P["T0"][:], P["AIM"][:], P["LR"][:], ALU.mult))
        V(lambda e: e.tensor_tensor(P["T1"][:], P["AM1"][:], PR3[:, :, 1], ALU.mult), reads=["PRM"])
        V(lambda e: e.tensor_tensor(P["T0"][:], P["T0"][:], P["T1"][:], ALU.subtract))
        V(lambda e: e.tensor_tensor(P["FIM"][:], P["T0"][:], P["DEN"][:], ALU.mult))

        def v3(t):
            return t[:].rearrange("p (a h) -> p a h", h=16)

        def bc(n):
            return P[n][:].unsqueeze(2).to_broadcast([128, 8, 16])
        V(lambda e: e.tensor_tensor(v3(BBR), v3(BRE), bc("FRE"), ALU.mult), reads=["BRE"])
        V(lambda e: e.tensor_tensor(v3(TB), v3(BIM), bc("FIM"), ALU.mult), reads=["BIM"])
        V(lambda e: e.tensor_tensor(BBR[:], BBR[:], TB[:], ALU.subtract))
        V(lambda e: e.tensor_tensor(v3(BBI), v3(BIM), bc("FRE"), ALU.mult), reads=["BIM"])
        V(lambda e: e.tensor_tensor(v3(TB), v3(BRE), bc("FIM"), ALU.mult), reads=["BRE"])
        V(lambda e: e.tensor_tensor(BBI[:], BBI[:], TB[:], ALU.add))
        V(lambda e: e.tensor_scalar(CIM[:], CIM[:], -1.0, None, ALU.mult), reads=["CIM"], writes=["CIM"])
        V(lambda e: e.memset(CR[:], 0.0)); V(lambda e: e.memset(CIN[:], 0.0))
        for dj in range(8):
            j = dj % 4
            for (src, dst) in ((BBR, WBR), (BBI, WBI)):
                V(lambda e: e.memset(PAD[:], 0.0), writes=["PAD"])
                V(lambda e, src=src, dj=dj, j=j: e.tensor_copy(PAD[0:64, 32 * j:32 * j + 16], src[0:64, dj * 16:dj * 16 + 16]), writes=["PAD"])
                V(lambda e, src=src, dj=dj, j=j: e.tensor_copy(PAD[64:128, 32 * j + 16:32 * j + 32], src[64:128, dj * 16:dj * 16 + 16]), writes=["PAD"])
                S.op("pe", lambda e: e.transpose(ptr[:], PAD[:], ID[:]), reads=["PAD", "ID"], writes=["ptr"])
                S.op("act", lambda e, dst=dst, dj=dj: e.copy(dst[:, dj, :], ptr[:]), reads=["ptr"], writes=["WB"])
            for (src, dst) in ((CRE, CR), (CIM, CIN)):
                V(lambda e, src=src, dst=dst, dj=dj, j=j: e.tensor_copy(dst[0:64, dj, 32 * j:32 * j + 16], src[0:64, dj * 16:dj * 16 + 16]), reads=["CRE", "CIM"], writes=["CC"])
                V(lambda e, src=src, dst=dst, dj=dj, j=j: e.tensor_copy(dst[64:128, dj, 32 * j + 16:32 * j + 32], src[64:128, dj * 16:dj * 16 + 16]), reads=["CRE", "CIM"], writes=["CC"])
            for (off, dst) in ((0.0, TS), (0.25, TC)):
                V(lambda e, dj=dj, off=off: e.tensor_scalar(RR[:], TAU[:], P["R"][:, dj:dj + 1], off, ALU.mult, ALU.add), reads=["TAU"], writes=["RR"])
                V(lambda e: e.tensor_copy(RRI[:], RR[:]), reads=["RR"], writes=["RRI"])
                V(lambda e: e.tensor_copy(RRF[:], RRI[:]), reads=["RRI"], writes=["RRF"])
                V(lambda e: e.tensor_tensor(RRF[:], RR[:], RRF[:], ALU.subtract), reads=["RR"], writes=["RRF"])
                S.op("act", lambda e, dst=dst, dj=dj: e.activation(dst[:, dj, :], RRF[:], AF.Sin, scale=TWO_PI), reads=["RRF"], writes=["TAB"])
            V(lambda e, dj=dj: e.tensor_copy(RHO[:, dj, :], P["MAG"][:, dj:dj + 1].to_broadcast([128, 512])), writes=["TAB"])

        G = "pool"
        oi = 0
        for d in range(2):
            V(lambda e: e.memset(HP[:], 0.0), writes=["HP"])
            for ci, (t0, T) in enumerate(S5_CH):
                ub_ = (d * 9 + ci) % 2
                uk = "UC%d" % ub_
                n0 = t0 if d == 0 else _mirror(t0, T)[0]
                S.dma("sp", lambda e, d=d, n0=n0, T=T, ub_=ub_, ct=ct: e.dma_start(out=UC[ub_][:, :T], in_=uin[d][ct * 128:(ct + 1) * 128, n0:n0 + T]), writes=[uk])
                ucv = (lambda ub_=ub_, T=T: UC[ub_][:, :T]) if d == 0 else (lambda ub_=ub_, T=T: UC[ub_][:, :T][:, ::-1])
                yb_ = (d * 9 + ci) % 2
                for j in range(4):
                    dj = d * 4 + j
                    b = j % 2
                    w = lambda n, b=b, T=T: W[n, b][:, :T]
                    k = lambda n, b=b: "w_%s%d" % (n, b)
                    S.op("pe", lambda e, dj=dj, b=b, T=T, ucv=ucv: e.matmul(pbr[b][:, :T], WBR[:, dj, :], ucv(), start=True, stop=True),
                         reads=["WB", uk], writes=["pbr%d" % b])
                    S.op("pe", lambda e, dj=dj, b=b, T=T, ucv=ucv: e.matmul(pbi[b][:, :T], WBI[:, dj, :], ucv(), start=True, stop=True),
                         reads=["WB", uk], writes=["pbi%d" % b])
                    S.op("act", lambda e, w=w, b=b, T=T: e.copy(w("BR"), pbr[b][:, :T]), reads=["pbr%d" % b], writes=[k("BR")])
                    S.op("act", lambda e, w=w, b=b, T=T: e.copy(w("BI"), pbi[b][:, :T]), reads=["pbi%d" % b], writes=[k("BI")])
                    cs = lambda dj=dj, T=T: TC[:, dj, :T]
                    sn = lambda dj=dj, T=T: TS[:, dj, :T]
                    S.op("dve", lambda e, w=w, cs=cs: e.tensor_tensor(w("T1"), cs(), w("BR"), ALU.mult), reads=["TAB", k("BR")], writes=[k("T1")])
                    S.op("dve", lambda e, w=w, sn=sn: e.tensor_tensor(w("T2"), sn(), w("BI"), ALU.mult), reads=["TAB", k("BI")], writes=[k("T2")])
                    S.op("dve", lambda e, w=w: e.tensor_tensor(w("XR"), w("T1"), w("T2"), ALU.add), reads=[k("T1"), k("T2")], writes=[k("XR")])
                    S.op(G, lambda e, w=w, cs=cs: e.tensor_tensor(w("T3"), cs(), w("BI"), ALU.mult), reads=["TAB", k("BI")], writes=[k("T3")])
                    S.op(G, lambda e, w=w, sn=sn: e.tensor_tensor(w("T4"), sn(), w("BR"), ALU.mult), reads=["TAB", k("BR")], writes=[k("T4")])
                    S.op(G, lambda e, w=w: e.tensor_tensor(w("XI"), w("T3"), w("T4"), ALU.subtract), reads=[k("T3"), k("T4")], writes=[k("XI")])
                    S.op("dve", lambda e, w=w, dj=dj, j=j, T=T: e.tensor_tensor_scan(w("QR"), RHO[:, dj, :T], w("XR"), HP[:, 2 * j:2 * j + 1], ALU.mult, ALU.add),
                         reads=["TAB", k("XR"), "HP"], writes=[k("QR")])
                    S.op("dve", lambda e, w=w, dj=dj, j=j, T=T: e.tensor_tensor_scan(w("QI"), RHO[:, dj, :T], w("XI"), HP[:, 2 * j + 1:2 * j + 2], ALU.mult, ALU.add),
                         reads=["TAB", k("XI"), "HP"], writes=[k("QI")])
                    S.op("dve", lambda e, w=w, cs=cs: e.tensor_tensor(w("T1"), cs(), w("QR"), ALU.mult), reads=["TAB", k("QR")], writes=[k("T1")])
                    S.op("dve", lambda e, w=w, sn=sn: e.tensor_tensor(w("T2"), sn(), w("QI"), ALU.mult), reads=["TAB", k("QI")], writes=[k("T2")])
                    S.op("dve", lambda e, w=w: e.tensor_tensor(w("HR"), w("T1"), w("T2"), ALU.subtract), reads=[k("T1"), k("T2")], writes=[k("HR")])
                    S.op(G, lambda e, w=w, sn=sn: e.tensor_tensor(w("T3"), sn(), w("QR"), ALU.mult), reads=["TAB", k("QR")], writes=[k("T3")])
                    S.op(G, lambda e, w=w, cs=cs: e.tensor_tensor(w("T4"), cs(), w("QI"), ALU.mult), reads=["TAB", k("QI")], writes=[k("T4")])
                    S.op(G, lambda e, w=w: e.tensor_tensor(w("HI"), w("T3"), w("T4"), ALU.add), reads=[k("T3"), k("T4")], writes=[k("HI")])
                    S.op("act", lambda e, b=b, j=j, T=T: e.copy(HP[:, 2 * j:2 * j + 1], W["HR", b][:, T - 1:T]), reads=[k("HR")], writes=["HP"])
                    S.op("act", lambda e, b=b, j=j, T=T: e.copy(HP[:, 2 * j + 1:2 * j + 2], W["HI", b][:, T - 1:T]), reads=[k("HI")], writes=["HP"])
                    S.op("pe", lambda e, dj=dj, w=w, j=j, yb_=yb_, T=T: e.matmul(py[yb_][:, :T], CR[:, dj, :], w("HR"), start=(j == 0), stop=False),
                         reads=["CC", k("HR")], writes=["py%d" % yb_])
                    S.op("pe", lambda e, dj=dj, w=w, j=j, yb_=yb_, T=T: e.matmul(py[yb_][:, :T], CIN[:, dj, :], w("HI"), start=False, stop=(j == 3)),
                         reads=["CC", k("HI")], writes=["py%d" % yb_])
                if d == 0:
                    S.op("act", lambda e, yb_=yb_, T=T: e.copy(YO[yb_][:, :T], py[yb_][:, :T]), reads=["py%d" % yb_], writes=["YO%d" % yb_])
                else:
                    S.op("act", lambda e, yb_=yb_, T=T: e.copy(YO[yb_][:, :T][:, ::-1], py[yb_][:, :T]), reads=["py%d" % yb_], writes=["YO%d" % yb_])
                oi += 1
                S.dma("act", lambda e, d=d, yb_=yb_, n0=n0, T=T, ct=ct: e.dma_start(out=yout[d][ct * 128:(ct + 1) * 128, n0:n0 + T], in_=YO[yb_][:, :T]),
                      reads=["YO%d" % yb_], writes=["yout_%d" % oi])
        S.sync_all()
        S.emit()
        sub.close()


def emit_C(nc, S, sb_unused, ps_unused, io):
    FMQ, FMK, FMZ, VT = io["FMQ"], io["FMK"], io["FMZ"], io["VT"]
    OS = [io["OF"], io["OB"]]
    rst, tmask, tmask2, blk, hmask, ident = io["rst"], io["tmask"], io["tmask2"], io["blk"], io["hmask"], io["ident"]
    QSC = 32 ** -0.5
    for hh in range(2):
        X = "h%d_" % hh
        sub = ExitStack()
        sb, ps = _mk(nc, sub)
        cols = slice(hh * 256, hh * 256 + 256)
        TM2 = sb(X + "TM2", [64, 256])
        S.dma("sp", lambda e: e.dma_start(out=TM2[:], in_=tmask2), writes=["TM2"])
        RST = sb(X + "RST", [128, 512]); TM = sb(X + "TM", [64, 256]); BLK = sb(X + "BLK", [128, 256]); HM = sb(X + "HM", [128, 4]); ID = sb(X + "ID", [128, 128])
        WG = sb(X + "WG", [16, 128]); BG = sb(X + "BG", [128, 1]); NBG = sb(X + "NBG", [128, 1])
        Wb = {}
        for n in ("Q", "K", "LA", "B", "E", "D", "QE", "QS", "KD", "KS0", "KS1", "KS2", "KS3"):
            for i in range(2):
                Wb[n, i] = sb(X + "g_%s%d" % (n, i), [128, 512])
        Z = [sb(X + "Z%d" % i, [16, 512]) for i in range(2)]
        VV = [sb(X + "VV%d" % i, [64, 8, 256]) for i in range(2)]
        DEC = [sb(X + "DEC%d" % i, [128, 8]) for i in range(2)]
        KDT = [sb(X + "KDT%d" % i, [64, 128]) for i in range(2)]
        STt = [sb(X + "ST%d" % i, [64, 256]) for i in range(2)]
        OB = [sb(X + "OB%d" % i, [64, 256]) for i in range(3)]
        KVM = sb(X + "KVM", [128, 256])
        SS = [sb(X + "SS%d" % i, [128, 256]) for i in range(2)]
        pza = ps(X + "pza", [128, 512])
        pt0 = ps(X + "pt0", [64, 128])
        pt = [pt0, pt0]
        pkv = [ps(X + "pkv%d" % i, [128, 256]) for i in range(2)]
        psc = [ps(X + "psc%d" % i, [64, 256]) for i in range(2)]
        po = [ps(X + "po%d" % i, [64, 256]) for i in range(2)]
        for (t, src, k) in ((RST, rst, "RST"), (TM, tmask, "TM"), (BLK, blk, "BLK"), (HM, hmask, "HM"), (ID, ident, "ID")):
            S.dma("sp", lambda e, t=t, src=src: e.dma_start(out=t[:], in_=src), writes=[k])
        gc = 0
        oi = 0
        for d in range(2):
            S.dma("sp", lambda e, d=d, hh=hh: e.dma_start(out=WG[:], in_=io["wg"][hh][d]), writes=["WG"])
            S.dma("sp", lambda e, d=d, hh=hh: e.dma_start(out=BG[:], in_=io["bg"][hh][d]), writes=["BG"])
            S.op("dve", lambda e: e.tensor_scalar(NBG[:], BG[:], -1.0, None, ALU.mult), reads=["BG"], writes=["NBG"])
            S.op("dve", lambda e: e.memset(SS[0][:], 0.0), writes=["SS0"])
            scur = 0
            for bi, (t0, T) in enumerate(S5_CH):
                nchk = T // 64
                n0 = t0 if d == 0 else _mirror(t0, T)[0]
                w0 = (n0 - SEQC) // 64
                b = (d * 9 + bi) % 2
                w = lambda n, b=b, T=T: Wb[n, b][:, :T]
                k = lambda n, b=b: "g_%s%d" % (n, b)
                w3 = lambda n, b=b, T=T: Wb[n, b][:, :T].rearrange("p (c s) -> p c s", s=64)
                S.dma("sp", lambda e, d=d, b=b, n0=n0, T=T, hh=hh: e.dma_start(out=Wb["Q", b][:, :T], in_=FMQ[hh * 128:(hh + 1) * 128, n0:n0 + T]), writes=[k("Q")])
                S.dma("act", lambda e, d=d, b=b, n0=n0, T=T, hh=hh: e.dma_start(out=Wb["K", b][:, :T], in_=FMK[hh * 128:(hh + 1) * 128, n0:n0 + T]), writes=[k("K")])
                S.dma("sp", lambda e, d=d, b=b, n0=n0, T=T: e.dma_start(out=Z[b][:, :T], in_=FMZ[d * 16:(d + 1) * 16, n0:n0 + T]), writes=["Z%d" % b])
                if t0 < SEQC:
                    S.dma("act", lambda e, b=b, nchk=nchk, cols=cols: e.dma_start(
                        out=VV[b][:, :nchk, :], in_=VT[0:SEQC, cols].rearrange("(c s) f -> s c f", s=64)), writes=["VV%d" % b])
                else:
                    S.dma("act", lambda e, b=b, w0=w0, cols=cols: e.dma_start(
                        out=VV[b][:, :, :], in_=VT[SEQC:, cols].rearrange("(r w) f -> r w f", w=64)[:, w0:w0 + 8, :]), writes=["VV%d" % b])
                S.op("pe", lambda e, b=b, T=T: e.matmul(pza[:, :T], WG[:], Z[b][:, :T], start=True, stop=True), reads=["WG", "Z%d" % b], writes=["pza"])
                S.op("act", lambda e, w=w, T=T: e.activation(w("E"), pza[:, :T], AF.Exp, bias=NBG[:], scale=-1.0), reads=["pza", "NBG"], writes=[k("E")])
                S.op("act", lambda e, w=w: e.activation(w("E"), w("E"), AF.Ln, bias=1.0), reads=[k("E")], writes=[k("E")])
                S.op("dve", lambda e, w=w: e.tensor_scalar(w("LA"), w("E"), -1.0 / 16.0, None, ALU.mult), reads=[k("E")], writes=[k("LA")])
                S.op("dve", lambda e, w=w, T=T: e.tensor_tensor_scan(w("B"), RST[:, :T], w("LA"), 0.0, ALU.mult, ALU.add), reads=["RST", k("LA")], writes=[k("B")])
                iref, ilast = (32, 63) if d == 0 else (31, 0)
                if d == 1:
                    S.op("dve", lambda e, w3=w3, nchk=nchk: e.tensor_tensor(w3("D"), w3("B")[:, :, 63:64].to_broadcast([128, nchk, 64]), w3("B"), ALU.subtract),
                         reads=[k("B")], writes=[k("D")])
                    S.op("dve", lambda e, w=w: e.tensor_tensor(w("B"), w("D"), w("LA"), ALU.add), reads=[k("D"), k("LA")], writes=[k("B")])
                S.op("act", lambda e, b=b, w3=w3, nchk=nchk, ilast=ilast: e.activation(DEC[b][:, :nchk], w3("B")[:, :, ilast], AF.Exp), reads=[k("B")], writes=["DEC%d" % b])
                S.op("act", lambda e, w=w: e.activation(w("E"), w("B"), AF.Exp), reads=[k("B")], writes=[k("E")])
                S.op("dve", lambda e, w=w: e.scalar_tensor_tensor(w("QE"), w("Q"), QSC, w("E"), ALU.mult, ALU.mult), reads=[k("Q"), k("E")], writes=[k("QE")])
                S.op("dve", lambda e, w3=w3, nchk=nchk, iref=iref: e.tensor_tensor(w3("D"), w3("B"), w3("B")[:, :, iref:iref + 1].to_broadcast([128, nchk, 64]), ALU.subtract),
                     reads=[k("B")], writes=[k("D")])
                S.op("act", lambda e, w=w: e.activation(w("E"), w("D"), AF.Exp), reads=[k("D"), k("QE")], writes=[k("E")])
                S.op("dve", lambda e, w=w: e.scalar_tensor_tensor(w("QS"), w("Q"), QSC, w("E"), ALU.mult, ALU.mult), reads=[k("Q"), k("E")], writes=[k("QS")])
                S.op("act", lambda e, w=w: e.activation(w("E"), w("D"), AF.Exp, scale=-1.0), reads=[k("D"), k("QS")], writes=[k("E")])
                S.op("dve", lambda e, w=w: e.tensor_tensor(w("LA"), w("K"), w("E"), ALU.mult), reads=[k("K"), k("E"), k("B")], writes=[k("LA")])
                for h in range(4):
                    S.op("pool", lambda e, w=w, h=h: e.tensor_scalar(w("KS%d" % h), w("LA"), HM[:, h:h + 1], None, ALU.mult),
                         reads=[k("LA"), "HM"], writes=[k("KS%d" % h)])
                S.op("dve", lambda e, w3=w3, nchk=nchk, ilast=ilast: e.tensor_tensor(w3("D"), w3("B")[:, :, ilast:ilast + 1].to_broadcast([128, nchk, 64]), w3("B"), ALU.subtract),
                     reads=[k("B"), k("E"), k("LA")], writes=[k("D")])
                S.op("act", lambda e, w=w: e.activation(w("D"), w("D"), AF.Exp), reads=[k("D")], writes=[k("D")])
                S.op("dve", lambda e, w=w: e.tensor_tensor(w("KD"), w("K"), w("D"), ALU.mult), reads=[k("K"), k("D")], writes=[k("KD")])
                for c in (range(nchk) if d == 0 else range(nchk - 1, -1, -1)):
                    p2 = gc % 2
                    gc += 1
                    cs = slice(c * 64, (c + 1) * 64)
                    S.op("pe", lambda e, b=b, cs=cs, p2=p2: e.transpose(pt[p2][:], Wb["KD", b][:, cs], ID[:]), reads=[k("KD"), "ID"], writes=["pt"])
                    S.op("act", lambda e, p2=p2: e.copy(KDT[p2][:], pt[p2][:]), reads=["pt"], writes=["KDT%d" % p2])
                    S.op("pe", lambda e, b=b, c=c, p2=p2: e.matmul(pkv[p2][:], KDT[p2][:], VV[b][:, c, :], start=True, stop=True),
                         reads=["KDT%d" % p2, "VV%d" % b], writes=["pkv%d" % p2])
                    for h in range(4):
                        S.op("pe", lambda e, b=b, cs=cs, p2=p2, h=h: e.matmul(psc[p2][:, h * 64:(h + 1) * 64], Wb["KS%d" % h, b][:, cs], Wb["QS", b][:, cs],
                                                                            start=True, stop=True),
                             reads=[k("KS%d" % h), k("QS")], writes=["psc%d" % p2])
                    S.op("dve", lambda e, p2=p2, d=d: e.tensor_tensor(STt[p2][:], psc[p2][:], (TM if d == 0 else TM2)[:], ALU.mult), reads=["psc%d" % p2, "TM", "TM2"], writes=["ST%d" % p2])
                    for h in range(4):
                        hs = slice(h * 64, (h + 1) * 64)
                        S.op("pe", lambda e, b=b, c=c, p2=p2, hs=hs: e.matmul(po[p2][:, hs], STt[p2][:, hs], VV[b][:, c, hs], start=True, stop=False),
                             reads=["ST%d" % p2, "VV%d" % b], writes=["po%d" % p2])
                        S.op("pe", lambda e, b=b, cs=cs, p2=p2, hs=hs, scur=scur: e.matmul(po[p2][:, hs], Wb["QE", b][:, cs], SS[scur][:, hs], start=False, stop=True),
                             reads=[k("QE"), "SS%d" % scur], writes=["po%d" % p2])
                    ob = oi % 3
                    oi += 1
                    S.op("act", lambda e, ob=ob, p2=p2: e.copy(OB[ob][:], po[p2][:]), reads=["po%d" % p2], writes=["OB%d" % ob])
                    if t0 < SEQC:
                        S.dma("sp" if oi % 2 else "act", lambda e, d=d, ob=ob, c=c, cols=cols: e.dma_start(out=OS[d][c * 64:(c + 1) * 64, cols], in_=OB[ob][:]),
                              reads=["OB%d" % ob], writes=["o_%d" % oi])
                    else:
                        S.dma("sp" if oi % 2 else "act", lambda e, d=d, ob=ob, c=c, w0=w0, cols=cols: e.dma_start(
                            out=OS[d][SEQC:, cols].rearrange("(r w) f -> r w f", w=64)[:, w0 + c, :], in_=OB[ob][:]),
                              reads=["OB%d" % ob], writes=["o_%d" % oi])
                    S.op("dve", lambda e, p2=p2: e.tensor_tensor(KVM[:], pkv[p2][:], BLK[:], ALU.mult), reads=["pkv%d" % p2, "BLK"], writes=["KVM"])
                    S.op("dve", lambda e, b=b, c=c, scur=scur: e.scalar_tensor_tensor(SS[1 - scur][:], SS[scur][:], DEC[b][:, c:c + 1], KVM[:], ALU.mult, ALU.add),
                         reads=["SS%d" % scur, "DEC%d" % b, "KVM"], writes=["SS%d" % (1 - scur)])
                    scur = 1 - scur
        S.sync_all()
        S.emit()
        sub.close()


def emit_D(nc, S, sb, ps, io, blocks, last):
    xT = io["xT"]; modT = io["MODS"]; TLs = io["TL"]
    su = TLs[:, 0:256]; sv = TLs[:, 256:512]; gg = TLs[:, 512:1024]
    s5u = io["FMU"]; yf = io["YF"]; yb = io["YB"]; of_ = io["OF"]; ob_ = io["OB"]
    w_out, wsT, sgub, s5d, wglu, ng, g2, wq = io["w_out"], io["wsT"], io["sgub"], io["s5d"], io["wglu"], io["ng"], io["g2"], io["wq"]
    keysT, eu, ev, gfin, ones, ident, iota16 = io["keysT"], io["eu"], io["ev"], io["gfin"], io["ones"], io["ident"], io["iota16"]
    xo = io["xo"]
    nb = len(blocks)
    MOD = sb("MOD", [128, 96]); WOUT = sb("WOUT", [128, 8, D]); WS = sb("WS", [128, 512]); SGUB = sb("SGUB", [128, 4]); S5D = sb("S5D", [128, 2])
    WGLU = sb("WGLU", [128, 2, 512]); NG = sb("NG", [128, 512]); G2 = sb("G2", [128, 8]); RR_ = sb("RR_", [128, 16384]); KEYS = sb("KEYS", [128, 2048])
    GF = sb("GF", [128, 8]); ONES = sb("ONES", [128, 128]); ID = sb("ID", [128, 128]); IOTA = sb("IOTA", [128, 16])
    A2 = sb("A2", [128, 16]); TA = sb("TA", [128, 16])
    U_ = sb("U_", [128, 256]); V_ = sb("V_", [128, 256]); GU = sb("GU", [128, 256]); GV = sb("GV", [128, 256]); SQ = sb("SQ", [128, 512])
    SS = sb("SS", [128, 8]); VN = sb("VN", [128, 256]); MIX = sb("MIX", [128, D])
    S5U = sb("S5U", [128, 2, 128]); YF = sb("YF", [128, 2, 128]); YB = sb("YB", [128, 2, 128]); GE = sb("GE", [128, 2, 128]); SG = sb("SG", [128, 256])
    OF = sb("OF", [128, 512]); OB = sb("OB", [128, 512]); GG = sb("GG", [128, 512]); SL = sb("SL", [128, 512])
    XT = sb("XT", [128, 8, 128]); X1 = sb("X1", [128, 8, 128]); XO = XT; HT = sb("HT", [128, 8, 128]); MIXT = HT; HTOK = sb("HTOK", [128, D])
    RSTD = sb("RSTD", [128, 128]); TMPB = sb("TMPB", [128, 128])
    SC = sb("SC", [128, 2048])
    M16 = sb("M16", [128, 256]); I16 = sb("I16", [128, 256], U32); IF16 = sb("IF16", [128, 256]); I1S = sb("I1S", [128, 128])
    CS = sb("CS", [128, 2048]); SC2 = CS; QT = CS[:].rearrange("p (q t) -> p q t", t=128); CS2 = sb("CS2", [128, 256])
    T16 = sb("T16", [128, 128]); P16 = sb("P16", [128, 128], U32); PF = sb("PF", [128, 128]); AI = sb("AI", [128, 128], I32)
    AFL = sb("AFL", [128, 128]); BFL = sb("BFL", [128, 128]); E1 = sb("E1", [128, 128]); E2 = sb("E2", [128, 128])
    EG = sb("EG", [128, 128]); GATE = sb("GATE", [128, 128]); IDXTI = sb("IDXTI", [128, 128], I32); GATET = sb("GATET", [128, 128])
    ACTT = sb("ACTT", [128, 128]); WT = sb("WT", [128, 128])
    NBUF = 8
    WQ = RR_[:].rearrange("p (k f) -> p k f", f=2048)
    UG = [RR_[:, i * 1024:(i + 1) * 1024] for i in range(NBUF)]; VG = UG
    HB = [sb("HB%d" % i, [128, D]) for i in range(2)]
    P0 = ps("P0", [128, 2048]); P1 = ps("P1", [128, 1024]); P2 = ps("P2", [128, 512]); P3 = ps("P3", [128, 512])

    def V(fn, r=(), w=()):
        S.op("dve", fn, reads=r, writes=w)

    def A(fn, r=(), w=()):
        S.op("act", fn, reads=r, writes=w)

    def PE(fn, r=(), w=()):
        S.op("pe", fn, reads=r, writes=w)

    def LD(q, t, src, key):
        S.dma(q, lambda e: e.dma_start(out=t, in_=src), writes=(key if isinstance(key, list) else [key]))

    LD("sp", MOD[:], modT, "MOD"); LD("sp", WS[:], wsT, "WS"); LD("sp", SGUB[:], sgub, "SGUB"); LD("sp", S5D[:], s5d, "S5D")
    LD("sp", WGLU[:], wglu.rearrange("(c p) f -> p c f", p=128), "WGLU"); LD("sp", NG[:], ng, "NG"); LD("sp", G2[:], g2, "G2")
    LD("sp", KEYS[:], keysT, "KEYS"); LD("sp", GF[:], gfin, "GF"); LD("sp", ONES[:], ones, "ONES"); LD("sp", ID[:], ident, "ID"); LD("sp", IOTA[:], iota16, "IOTA")
    wo_v = w_out.rearrange("(k p) f -> p k f", p=128)
    wq_v = wq.rearrange("(k p) f -> p k f", p=128)
    for k in range(8):
        LD("act", WOUT[:, k, :], wo_v[:, k, :], "WOUT")
    MOD3 = MOD[:].rearrange("p (j c) -> p j c", c=2)
    V(lambda e: e.tensor_scalar(TA[:].rearrange("p (j c) -> p j c", c=2), MOD3[:, 32:40, :], 1.0, None, ALU.add), ["MOD"], ["TA"])
    V(lambda e: e.tensor_tensor(A2[:].rearrange("p (j c) -> p j c", c=2), TA[:].rearrange("p (j c) -> p j c", c=2),
                                G2[:].unsqueeze(2).to_broadcast([128, 8, 2]), ALU.mult), ["TA", "G2"], ["A2"])
    A23 = A2[:].rearrange("p (j c) -> p j c", c=2)

    def rs_from_ss(ss, n, scale):
        V(lambda e: e.tensor_scalar(ss, ss, scale, EPS, ALU.mult, ALU.add), ["SS"], ["SS"])
        A(lambda e: e.sqrt(ss, ss), ["SS"], ["SS"])
        V(lambda e: e.reciprocal(ss, ss), ["SS"], ["SS"])

    def top16(src, scratch, mout, iout, n):
        V(lambda e: e.max(mout[:, 0:8], src), ["TK"], ["TK"])
        V(lambda e: e.max_index(iout[:, 0:8], mout[:, 0:8], src), ["TK"], ["TK"])
        V(lambda e: e.match_replace(scratch, mout[:, 0:8], src, -1e30), ["TK"], ["TK"])
        V(lambda e: e.max(mout[:, 8:16], scratch), ["TK"], ["TK"])
        V(lambda e: e.max_index(iout[:, 8:16], mout[:, 8:16], scratch), ["TK"], ["TK"])

    xT_v = xT.rearrange("(k p) t -> p k t", p=128)
    xo_v = xo.rearrange("(k p) t -> p k t", p=128)
    s5u_v = s5u.rearrange("(c p) t -> p c t", p=128)
    yf_v = yf.rearrange("(c p) t -> p c t", p=128)
    yb_v = yb.rearrange("(c p) t -> p c t", p=128)
    for bi in range(nb):
        sq0, r0_, oc0, isctx = blocks[bi]
        col = 1 if isctx else 0
        tk = slice(sq0, sq0 + 128)
        tr = slice(r0_, r0_ + 128)
        to = slice(oc0, oc0 + 128)
        LD("sp", U_[:], su[tr, :], "U_"); LD("sp", V_[:], sv[tr, :], "V_"); LD("sp", GG[:], gg[tr, :], "GG")
        LD("act", S5U[:], s5u_v[:, :, tk], "S5U"); LD("act", YF[:], yf_v[:, :, tk], "YF"); LD("act", YB[:], yb_v[:, :, tk], "YB")
        LD("sp", OF[:], of_[tk, :], "OF"); LD("sp", OB[:], ob_[tk, :], "OB"); LD("act", XT[:], xT_v[:, :, tk], "XT")
        for k in range(8):
            LD("act" if k % 2 else "sp", WQ[:, k, :], wq_v[:, k, :], ["R%d" % (2 * k), "R%d" % (2 * k + 1)])
        A(lambda e: e.activation(GU[:], U_[:], AF.Gelu), ["U_"], ["GU"])
        A(lambda e: e.activation(GV[:], V_[:], AF.Gelu), ["V_"], ["GV"])
        V(lambda e: e.tensor_tensor(SQ[:, 0:256], GV[:], GV[:], ALU.mult), ["GV"], ["SQ"])
        V(lambda e: e.tensor_reduce(SS[:, 0:4], SQ[:, 0:256].rearrange("p (h d) -> p h d", d=64), AX.X, ALU.add), ["SQ"], ["SS"])
        rs_from_ss(SS[:, 0:4], 4, 1.0 / 64)
        V(lambda e: e.tensor_tensor(VN[:].rearrange("p (h d) -> p h d", d=64), GV[:].rearrange("p (h d) -> p h d", d=64),
                                    SS[:, 0:4].unsqueeze(2).to_broadcast([128, 4, 64]), ALU.mult), ["GV", "SS"], ["VN"])
        for h in range(4):
            PE(lambda e, h=h: e.matmul(P2[:, h * 64:(h + 1) * 64], WS[:, h * 128:(h + 1) * 128], VN[:, h * 64:(h + 1) * 64], start=True, stop=True),
               ["WS", "VN"], ["P2"])
        V(lambda e: e.tensor_tensor(MIX[:, 0:256].rearrange("p (h d) -> p h d", d=64), P2[:, 0:256].rearrange("p (h d) -> p h d", d=64),
                                    SGUB[:].unsqueeze(2).to_broadcast([128, 4, 64]), ALU.add), ["P2", "SGUB"], ["MIXa"])
        V(lambda e: e.tensor_tensor(MIX[:, 0:256], MIX[:, 0:256], GU[:], ALU.mult), ["GU"], ["MIXa"])
        V(lambda e: e.tensor_tensor(YF[:], YF[:], YB[:], ALU.add), ["YB"], ["YF"])
        for ct in range(2):
            V(lambda e, ct=ct: e.scalar_tensor_tensor(YF[:, ct, :], S5U[:, ct, :], S5D[:, ct:ct + 1], YF[:, ct, :], ALU.mult, ALU.add),
              ["S5U", "S5D"], ["YF"])
        A(lambda e: e.activation(GE[:], YF[:], AF.Gelu), ["YF"], ["GE"])
        for ct in range(2):
            PE(lambda e, ct=ct: e.matmul(P3[:, 0:512], GE[:, ct, :], WGLU[:, ct, :], start=(ct == 0), stop=(ct == 1)), ["GE", "WGLU"], ["P3"])
        A(lambda e: e.activation(SG[:], P3[:, 256:512], AF.Sigmoid), ["P3"], ["SG"])
        V(lambda e: e.tensor_tensor(MIX[:, 256:512], P3[:, 0:256], SG[:], ALU.mult), ["P3", "SG"], ["MIXb"])
        V(lambda e: e.tensor_tensor(OF[:], OF[:], OB[:], ALU.add), ["OB"], ["OF"])
        V(lambda e: e.tensor_tensor(SQ[:], OF[:], OF[:], ALU.mult), ["OF"], ["SQ"])
        V(lambda e: e.tensor_reduce(SS[:, 0:8], SQ[:].rearrange("p (h d) -> p h d", d=64), AX.X, ALU.add), ["SQ"], ["SS"])
        rs_from_ss(SS[:, 0:8], 8, 1.0 / 64)
        V(lambda e: e.tensor_tensor(OF[:].rearrange("p (h d) -> p h d", d=64), OF[:].rearrange("p (h d) -> p h d", d=64),
                                    SS[:, 0:8].unsqueeze(2).to_broadcast([128, 8, 64]), ALU.mult), ["SS"], ["OF"])
        V(lambda e: e.tensor_tensor(OF[:], OF[:], NG[:], ALU.mult), ["NG"], ["OF"])
        A(lambda e: e.activation(SL[:], GG[:], AF.Silu), ["GG"], ["SL"])
        V(lambda e: e.tensor_tensor(MIX[:, 512:1024], OF[:], SL[:], ALU.mult), ["OF", "SL"], ["MIXc"])
        for f in range(8):
            pp, pk = (P2, "P2") if f % 2 == 0 else (P3, "P3")
            PE(lambda e, f=f, pp=pp: e.transpose(pp[:, 0:128], MIX[:, f * 128:(f + 1) * 128], ID[:]), ["MIXa", "MIXb", "MIXc", "ID"], [pk])
            A(lambda e, f=f, pp=pp: e.copy(MIXT[:, f, :], pp[:, 0:128]), [pk], ["HT"])
        for ot in range(8):
            pp, pk = (P2, "P2") if ot % 2 == 0 else (P3, "P3")
            for k in range(8):
                PE(lambda e, ot=ot, k=k, pp=pp: e.matmul(pp[:, 0:128], WOUT[:, k, ot * 128:(ot + 1) * 128], MIXT[:, k, :], start=(k == 0), stop=(k == 7)),
                   ["WOUT", "HT"], [pk])
            V(lambda e, ot=ot, pp=pp, col=col: e.scalar_tensor_tensor(X1[:, ot, :], pp[:, 0:128], MOD3[:, 16 + ot, col:col + 1], XT[:, ot, :], ALU.mult, ALU.add),
              [pk, "MOD", "XT"], ["X1"])
        for k in range(8):
            A(lambda e, k=k: e.activation(TMPB[:], X1[:, k, :], AF.Square), ["X1"], ["TMPB"])
            PE(lambda e, k=k: e.matmul(P2[:, 0:128], ONES[:], TMPB[:], start=(k == 0), stop=(k == 7)), ["ONES", "TMPB"], ["P2"])
        V(lambda e: e.tensor_scalar(RSTD[:], P2[:, 0:128], 1.0 / D, EPS, ALU.mult, ALU.add), ["P2"], ["RSTD"])
        A(lambda e: e.sqrt(RSTD[:], RSTD[:]), ["RSTD"], ["RSTD"])
        V(lambda e: e.reciprocal(RSTD[:], RSTD[:]), ["RSTD"], ["RSTD"])
        for k in range(8):
            V(lambda e, k=k: e.tensor_tensor(TMPB[:], X1[:, k, :], RSTD[:], ALU.mult), ["X1", "RSTD"], ["TMPB"])
            A(lambda e, k=k, col=col: e.activation(HT[:, k, :], TMPB[:], AF.Identity, bias=MOD3[:, 24 + k, col:col + 1], scale=A23[:, k, col:col + 1]),
              ["TMPB", "MOD", "A2"], ["HT"])
        for k in range(8):
            pp, pk = (P2, "P2") if k % 2 == 0 else (P3, "P3")
            PE(lambda e, k=k, pp=pp: e.transpose(pp[:, 0:128], HT[:, k, :], ID[:]), ["HT", "ID"], [pk])
            A(lambda e, k=k, pp=pp: e.copy(HTOK[:, k * 128:(k + 1) * 128], pp[:, 0:128]), [pk], ["HTOK"])
        for qt in range(16):
            pp, pk = (P2, "P2") if qt % 2 == 0 else (P3, "P3")
            for k in range(8):
                PE(lambda e, qt=qt, k=k, pp=pp: e.matmul(pp[:, 0:128], WQ[:, k, qt * 128:(qt + 1) * 128], HT[:, k, :], start=(k == 0), stop=(k == 7)),
                   ["R%d" % (2 * k), "R%d" % (2 * k + 1), "HT"], [pk])
            if qt % 2 == 0:
                A(lambda e, qt=qt, pp=pp: e.copy(QT[:, qt, :], pp[:, 0:128]), [pk], ["TK"])
            else:
                V(lambda e, qt=qt, pp=pp: e.tensor_copy(QT[:, qt, :], pp[:, 0:128]), [pk], ["TK"])
        for qt in range(16):
            PE(lambda e, qt=qt: e.matmul(P0[:, qt * 128:(qt + 1) * 128], QT[:, qt, :], KEYS[:, qt * 128:(qt + 1) * 128], start=True, stop=True),
               ["TK", "KEYS"], ["P0a", "P0b"])
        for q4 in range(4):
            A(lambda e, q4=q4: e.copy(SC[:, q4 * 512:(q4 + 1) * 512], P0[:, q4 * 512:(q4 + 1) * 512]), ["P0a", "P0b"], ["TK"])
        for qt in range(16):
            top16(SC[:, qt * 128:(qt + 1) * 128], SC2[:, qt * 128:(qt + 1) * 128], M16[:, qt * 16:(qt + 1) * 16], I16[:, qt * 16:(qt + 1) * 16], 128)
        V(lambda e: e.tensor_copy(IF16[:], I16[:]), ["TK"], ["TK"])
        M4 = M16[:].rearrange("p (h q k) -> p h q k", q=2, k=16)
        IF4 = IF16[:].rearrange("p (h q k) -> p h q k", q=2, k=16)
        I1S3 = I1S[:].rearrange("p (h k) -> p h k", k=16)
        V(lambda e: e.tensor_scalar(I1S3, IF4[:, :, 0, :], 128.0, None, ALU.mult), ["TK"], ["TK"])
        CS4 = CS[:].rearrange("p (h a b) -> p h a b", a=16, b=16)
        V(lambda e: e.tensor_tensor(CS4, M4[:, :, 0, :].unsqueeze(3).to_broadcast([128, 8, 16, 16]),
                                    M4[:, :, 1, :].unsqueeze(2).to_broadcast([128, 8, 16, 16]), ALU.add), ["TK"], ["TK"])
        for h in range(8):
            top16(CS[:, h * 256:(h + 1) * 256], CS2[:], T16[:, h * 16:(h + 1) * 16], P16[:, h * 16:(h + 1) * 16], 256)
        V(lambda e: e.tensor_copy(PF[:], P16[:]), ["TK"], ["TK"])
        V(lambda e: e.tensor_scalar(AFL[:], PF[:], -7.5, 1.0 / 16, ALU.add, ALU.mult), ["TK"], ["TK"])
        V(lambda e: e.tensor_copy(AI[:], AFL[:]), ["TK"], ["TK"])
        V(lambda e: e.tensor_copy(AFL[:], AI[:]), ["TK"], ["TK"])
        V(lambda e: e.scalar_tensor_tensor(BFL[:], AFL[:], -16.0, PF[:], ALU.mult, ALU.add), ["TK"], ["TK"])
        EQ4 = CS[:].rearrange("p (h k a) -> p h k a", k=16, a=16)
        io4 = IOTA[:].unsqueeze(1).unsqueeze(1).to_broadcast([128, 8, 16, 16])
        for (sel, src, dst) in ((AFL, I1S3, E1), (BFL, IF4[:, :, 1, :], E2)):
            V(lambda e, sel=sel: e.tensor_tensor(EQ4, io4, sel[:].rearrange("p (h k) -> p h k", k=16).unsqueeze(3).to_broadcast([128, 8, 16, 16]), ALU.is_equal),
              ["TK", "IOTA"], ["TK"])
            V(lambda e, src=src: e.tensor_tensor(EQ4, EQ4, src.unsqueeze(2).to_broadcast([128, 8, 16, 16]), ALU.mult), ["TK"], ["TK"])
            V(lambda e, dst=dst: e.tensor_reduce(dst[:], CS[:].rearrange("p (m a) -> p m a", a=16), AX.X, ALU.add), ["TK"], ["TK"])
        V(lambda e: e.tensor_tensor(E1[:], E1[:], E2[:], ALU.add), ["TK"], ["TK"])
        T3 = T16[:].rearrange("p (h k) -> p h k", k=16)
        V(lambda e: e.tensor_tensor(EG[:].rearrange("p (h k) -> p h k", k=16), T3, T3[:, :, 0:1].to_broadcast([128, 8, 16]), ALU.subtract), ["TK"], ["TK"])
        A(lambda e: e.activation(EG[:], EG[:], AF.Exp), ["TK"], ["TK"])
        V(lambda e: e.tensor_reduce(SS[:, 0:8], EG[:].rearrange("p (h k) -> p h k", k=16), AX.X, ALU.add), ["TK"], ["SS"])
        V(lambda e: e.reciprocal(SS[:, 0:8], SS[:, 0:8]), ["SS"], ["SS"])
        V(lambda e: e.tensor_tensor(GATE[:].rearrange("p (h k) -> p h k", k=16), EG[:].rearrange("p (h k) -> p h k", k=16),
                                    SS[:, 0:8].unsqueeze(2).to_broadcast([128, 8, 16]), ALU.mult), ["TK", "SS"], ["GATE"])
        PE(lambda e: e.transpose(P2[:, 0:128], E1[:], ID[:]), ["TK", "ID"], ["P2"])
        V(lambda e: e.tensor_copy(IDXTI[:], P2[:, 0:128]), ["P2"], ["IDXTI"])
        PE(lambda e: e.transpose(P3[:, 0:128], GATE[:], ID[:]), ["GATE", "ID"], ["P3"])
        A(lambda e: e.copy(GATET[:], P3[:, 0:128]), ["P3"], ["GATET"])
        for t in range(128):
            g = t % NBUF
            b = t % 2
            S.dma("pool", lambda e, t=t, g=g: e.indirect_dma_start(out=UG[g], out_offset=None, in_=eu,
                                                                   in_offset=bass.IndirectOffsetOnAxis(ap=IDXTI[:, t:t + 1], axis=0)),
                  reads=["IDXTI"], writes=["R%d" % g])
            pk = "P0a" if b == 0 else "P0b"
            for hf in range(2):
                PE(lambda e, t=t, b=b, hf=hf: e.matmul(P0[:, b * 1024 + hf * 512:b * 1024 + (hf + 1) * 512], ID[:, t:t + 1].to_broadcast([128, 128]),
                                                       HTOK[:, hf * 512:(hf + 1) * 512], start=True, stop=True), ["ID", "HTOK"], [pk])
                A(lambda e, b=b, hf=hf: e.copy(HB[b][:, hf * 512:(hf + 1) * 512], P0[:, b * 1024 + hf * 512:b * 1024 + (hf + 1) * 512]), [pk], ["HB%d" % b])
            V(lambda e, t=t, b=b, g=g: e.scalar_tensor_tensor(UG[g], UG[g], 1.0, HB[b][:], ALU.mult, ALU.mult, accum_out=ACTT[:, t:t + 1]),
              ["HB%d" % b], ["R%d" % g, "ACTT"])
        A(lambda e: e.activation(WT[:], ACTT[:], AF.Gelu), ["ACTT"], ["WT"])
        V(lambda e: e.tensor_tensor(WT[:], WT[:], GATET[:], ALU.mult), ["GATET"], ["WT"])
        for t in range(128):
            g = t % NBUF
            S.dma("pool", lambda e, t=t, g=g: e.indirect_dma_start(out=VG[g], out_offset=None, in_=ev,
                                                                   in_offset=bass.IndirectOffsetOnAxis(ap=IDXTI[:, t:t + 1], axis=0)),
                  reads=["IDXTI"], writes=["R%d" % g])
            for ot in range(8):
                PE(lambda e, t=t, g=g, ot=ot: e.matmul(P1[:, ot * 128 + t:ot * 128 + t + 1], VG[g][:, ot * 128:(ot + 1) * 128], WT[:, t:t + 1],
                                                       start=True, stop=True), ["R%d" % g, "WT"], ["P1"])
        for ot in range(8):
            V(lambda e, ot=ot, col=col: e.scalar_tensor_tensor(XO[:, ot, :], P1[:, ot * 128:(ot + 1) * 128], MOD3[:, 40 + ot, col:col + 1], X1[:, ot, :],
                                                               ALU.mult, ALU.add), ["P1", "MOD", "X1"], ["XT"])
        if last:
            for k in range(8):
                A(lambda e, k=k: e.activation(TMPB[:], XO[:, k, :], AF.Square), ["XT"], ["TMPB"])
                PE(lambda e, k=k: e.matmul(P2[:, 0:128], ONES[:], TMPB[:], start=(k == 0), stop=(k == 7)), ["ONES", "TMPB"], ["P2"])
            V(lambda e: e.tensor_scalar(RSTD[:], P2[:, 0:128], 1.0 / D, EPS, ALU.mult, ALU.add), ["P2"], ["RSTD"])
            A(lambda e: e.sqrt(RSTD[:], RSTD[:]), ["RSTD"], ["RSTD"])
            V(lambda e: e.reciprocal(RSTD[:], RSTD[:]), ["RSTD"], ["RSTD"])
            for k in range(8):
                V(lambda e, k=k: e.scalar_tensor_tensor(XO[:, k, :], XO[:, k, :], GF[:, k:k + 1], RSTD[:], ALU.mult, ALU.mult), ["RSTD", "GF"], ["XT"])
        S.dma("sp", lambda e, to=to: e.dma_start(out=xo_v[:, :, to], in_=XO[:]), reads=["XT"], writes=["xo_%d" % bi])


def build_layer(last, dbg=False):
    nc = bass.Bass("TRN2", target_bir_lowering=False)
    io = {}

    def din(name, shape, dt=F32):
        io[name] = nc.dram_tensor(name, shape, dt, kind="ExternalInput").ap()

    def scr(name, shape):
        io[name] = nc.dram_tensor(name, shape, F32, kind=("ExternalOutput" if dbg else "Internal")).ap()
    nctx = 0 if last else 128
    ntok = 2048 + nctx
    din("xT", [D, SEQT]); din("cT", [128, 16]); din("w_mod", [D, NMOD * D]); din("b_mod", [128, 48]); din("g1", [128, 8]); din("w_in", [D, INW])
    din("ones", [128, 128]); din("ident", [128, 128]); din("tau", [128, 512]); din("iota16", [128, 16])
    din("prm", [2, 128, 24]); din("bre", [2, 128, 128]); din("bim", [2, 128, 128]); din("cre", [2, 128, 128]); din("cim", [2, 128, 128])
    din("wg", [2, 2, 16, 128]); din("bg", [2, 2, 128, 1])
    din("rst", [128, 512]); din("tmask", [64, 256]); din("tmask2", [64, 256]); din("blk", [128, 256]); din("hmask", [128, 4])
    din("w_out", [D, D]); din("wsT", [128, 512]); din("sgub", [128, 4]); din("s5d", [128, 2]); din("wglu", [256, 512]); din("ng", [128, 512])
    din("g2", [128, 8]); din("wq", [D, 2048]); din("keysT", [128, 2048]); din("eu", [NEXP, D]); din("ev", [NEXP, D]); din("gfin", [128, 8])
    scr("FMU", [256, SEQT]); scr("FMQ", [256, SEQT]); scr("FMK", [256, SEQT]); scr("FMZ", [32, SEQT]); scr("VT", [SEQT, 512]); scr("TL", [OWN, 1024])
    scr("MODS", [128, 96]); scr("YF", [256, SEQT]); scr("YB", [256, SEQT]); scr("OF", [SEQT, 512]); scr("OB", [SEQT, 512]); scr("GS", [2, 16])
    io["xo"] = nc.dram_tensor("xo", [D, ntok], F32, kind="ExternalOutput").ap()
    with ExitStack() as top:
        gate = top.enter_context(nc.semaphore("gate"))
        ph = [0]

        def phase(fn):
            with ExitStack() as st:
                S = Sched(nc, top, gate, 16 * ph[0])
                sb, ps = _mk(nc, st)
                fn(S, sb, ps)
                S.drain_all("sp")
                GS = io["GS"]
                S.prog["sp"].append(("i", lambda e: e.dma_start(out=GS[0:1, :], in_=GS[1:2, :]), "gate", 16))
                S.emit()
            ph[0] += 1
        phase(lambda S, sb, ps: emit_A(nc, S, sb, ps, io))
        phase(lambda S, sb, ps: emit_B(nc, S, sb, ps, io))
        phase(lambda S, sb, ps: emit_C(nc, S, sb, ps, io))
        if last:
            blocks = [(SEQC + i * 128, 128 + i * 128, i * 128, False) for i in range(16)]
        else:
            blocks = [(0, 0, 0, True)] + [(SEQC + i * 128, 128 + i * 128, 128 + i * 128, False) for i in range(16)]
        phase(lambda S, sb, ps: emit_D(nc, S, sb, ps, io, blocks, last))
    return nc


def _lay_vec(v, n):
    return np.ascontiguousarray(np.asarray(v, np.float32).reshape(n, 128).T)


_CONST = {}


def _consts():
    if not _CONST:
        rst = np.ones((128, 512), np.float32)
        rst[:, ::64] = 0
        tm = (np.arange(64)[None, :] >= np.arange(64)[:, None]).astype(np.float32)
        _CONST.update(
            ones=np.ones((128, 128), np.float32), ident=np.eye(128, dtype=np.float32),
            iota16=np.ascontiguousarray(np.broadcast_to(np.arange(16, dtype=np.float32), (128, 16))),
            tau=np.ascontiguousarray(np.broadcast_to(np.arange(1, 513, dtype=np.float32), (128, 512))),
            rst=rst, tmask=np.ascontiguousarray(np.tile(tm, (1, 4))), tmask2=np.ascontiguousarray(np.tile(tm.T, (1, 4))),
            blk=np.kron(np.eye(4, dtype=np.float32), np.ones((32, 64), np.float32)),
            hmask=np.kron(np.eye(4, dtype=np.float32), np.ones((32, 1), np.float32)))
    return _CONST


def _pb_params(P, l, gh, swap):
    def stt(a):
        if swap:
            a = a[::-1]
        g = a[:, gh * 8:gh * 8 + 8]
        g = g.reshape((2, 4, 2, 64) + a.shape[3:])
        g = np.moveaxis(g, (2, 3), (0, 1))
        return np.ascontiguousarray(g.reshape((128, 2, 4) + a.shape[3:]))
    lre = stt(P["s5_lambda_re"][l]); lim = stt(P["s5_lambda_im"][l])
    ls = stt(np.broadcast_to(P["s5_log_step"][l][:, :, None], (2, 16, 64)))
    prm = np.ascontiguousarray(np.stack([lre, lim, ls], -1).reshape(128, 24).astype(np.float32))
    return dict(prm=prm, bre=stt(P["s5_b_re"][l]).reshape(128, 128), bim=stt(P["s5_b_im"][l]).reshape(128, 128),
                cre=stt(np.swapaxes(P["s5_c_re"][l], 2, 3)).reshape(128, 128), cim=stt(np.swapaxes(P["s5_c_im"][l], 2, 3)).reshape(128, 128))


def _layer_weights(P, l, swap):
    C = _consts()
    sk = P["peer_sub_keys"][l]
    keysT = np.zeros((128, 16, 128), np.float32)
    for h in range(8):
        for p in range(2):
            keysT[:, h * 2 + p, :] = sk[p, h].T
    sw = P["sgu_w"][l]
    sbb = P["sgu_b"][l]
    wgate = P["gla_w_gate"][l]
    bgate = P["gla_b_gate"][l]
    if swap:
        sw = sw[:, ::-1, ::-1]
        sbb = sbb[:, ::-1]
        wgate = wgate[::-1]
        bgate = bgate[::-1]
    pb = [_pb_params(P, l, ct, swap) for ct in range(2)]
    w_in = P["w_in"][l]
    if swap:
        w_in = np.ascontiguousarray(np.concatenate([w_in[:, :2304], w_in[:, 2320:2336], w_in[:, 2304:2320]], 1))
    W = dict(w_mod=P["w_mod"][l], b_mod=_lay_vec(P["b_mod"][l], 48), g1=_lay_vec(P["norm1_g"][l], 8), w_in=w_in,
             w_out=P["w_out"][l], wsT=np.ascontiguousarray(np.transpose(sw, (2, 0, 1)).reshape(128, 512)),
             sgub=np.ascontiguousarray(sbb.T), s5d=_lay_vec(P["s5_d"][l], 2), wglu=P["s5_w_glu"][l],
             ng=np.ascontiguousarray(np.broadcast_to(P["gla_norm_g"][l], (128, 512))), g2=_lay_vec(P["norm2_g"][l], 8),
             wq=P["peer_w_query"][l], keysT=keysT.reshape(128, 2048), eu=P["peer_expert_u"][l], ev=P["peer_expert_v"][l],
             gfin=_lay_vec(P["final_norm_g"], 8),
             wg=np.ascontiguousarray(np.stack([[wgate[d][:, hh * 128:hh * 128 + 128] for d in range(2)] for hh in range(2)])),
             bg=np.ascontiguousarray(np.stack([[bgate[d][hh * 128:hh * 128 + 128][:, None] for d in range(2)] for hh in range(2)])))
    for k in ("prm", "bre", "bim", "cre", "cim"):
        W[k] = np.ascontiguousarray(np.stack([pb[0][k], pb[1][k]]))
    W.update(C)
    return W


LAYER_W = ["w_mod", "b_mod", "g1", "w_in", "prm", "bre", "bim", "cre", "cim", "wg", "bg", "w_out", "wsT", "sgub", "s5d", "wglu", "ng", "g2",
           "wq", "keysT", "eu", "ev"]
LAYER_W_SHAPES = dict(w_mod=[D, NMOD * D], b_mod=[128, 48], g1=[128, 8], w_in=[D, INW], prm=[2, 128, 24], bre=[2, 128, 128], bim=[2, 128, 128],
                      cre=[2, 128, 128], cim=[2, 128, 128], wg=[2, 2, 16, 128], bg=[2, 2, 128, 1], w_out=[D, D], wsT=[128, 512], sgub=[128, 4],
                      s5d=[128, 2], wglu=[256, 512], ng=[128, 512], g2=[128, 8], wq=[D, 2048], keysT=[128, 2048], eu=[NEXP, D], ev=[NEXP, D])


def build_full():
    nc = bass.Bass("TRN2", target_bir_lowering=False)
    io = {}

    def din(name, shape, dt=F32):
        io[name] = nc.dram_tensor(name, shape, dt, kind="ExternalInput").ap()

    def scr(name, shape):
        io[name] = nc.dram_tensor(name, shape, F32, kind="Internal").ap()
    din("xT", [D, SEQT]); din("cT", [128, 16]); din("gfin", [128, 8])
    din("ones", [128, 128]); din("ident", [128, 128]); din("tau", [128, 512]); din("iota16", [128, 16])
    din("rst", [128, 512]); din("tmask", [64, 256]); din("tmask2", [64, 256]); din("blk", [128, 256]); din("hmask", [128, 4])
    for l in range(2):
        for n in LAYER_W:
            din("%s_%d" % (n, l), LAYER_W_SHAPES[n])
    scr("FMU", [256, SEQT]); scr("FMQ", [256, SEQT]); scr("FMK", [256, SEQT]); scr("FMZ", [32, SEQT]); scr("VT", [SEQT, 512]); scr("TL", [SEQT, 1024])
    scr("MODS", [128, 96]); scr("YF", [256, SEQT]); scr("YB", [256, SEQT]); scr("OF", [SEQT, 512]); scr("OB", [SEQT, 512]); scr("GS", [2, 16])
    scr("X1S", [D, SEQT])
    io["xo"] = nc.dram_tensor("xo", [D, 2048], F32, kind="ExternalOutput").ap()
    with ExitStack() as top:
        gate = top.enter_context(nc.semaphore("gate"))
        ph = [0]

        def phase(fn, nds=None):
            with ExitStack() as st:
                S = Sched(nc, top, gate, 16 * ph[0], nds=nds)
                sb, ps = _mk(nc, st)
                fn(S, sb, ps)
                S.drain_all("sp")
                GS = io["GS"]
                S.prog["sp"].append(("i", lambda e: e.dma_start(out=GS[0:1, :], in_=GS[1:2, :]), "gate", 16))
                S.emit()
            ph[0] += 1
        for l in range(2):
            last = l == 1
            iol = dict(io)
            for n in LAYER_W:
                iol[n] = io["%s_%d" % (n, l)]
            if last:
                iol["xT"] = io["X1S"]
                blocks = [(SEQC + i * 128, 128 + i * 128, i * 128, False) for i in range(16)]
            else:
                iol["xo"] = io["X1S"]
                blocks = [(0, 0, 0, True), (128, 128, 128, True)] + [(SEQC + i * 128, SEQC + i * 128, SEQC + i * 128, False) for i in range(32)]
            n2 = dict(sp=2, act=2, pool=2)
            phase(lambda S, sb, ps: emit_A(nc, S, sb, ps, iol, tl_all=not last), nds=n2)
            phase(lambda S, sb, ps: emit_B(nc, S, sb, ps, iol), nds=n2)
            phase(lambda S, sb, ps: emit_C(nc, S, sb, ps, iol), nds=n2)
            phase(lambda S, sb, ps: emit_D(nc, S, sb, ps, iol, blocks, last), nds=dict(sp=2, act=2, pool=7))
    return nc


_PROG = {}


def _prog(name, fn):
    if name not in _PROG:
        _PROG[name] = fn()
    return _PROG[name]


def kernel(**inputs):
    P = {k: np.ascontiguousarray(np.asarray(v)) for k, v in inputs.items()}
    cores = list(range(8))
    C = _consts()
    ncF = _prog("F", build_full)
    WL = {(l, sw): _layer_weights(P, l, sw) for l in range(2) for sw in (False, True)}
    in_maps = []
    for core in cores:
        b, half = divmod(core, 2)
        xc, xl = P["ctx"][b], P["x"][b]
        seq = np.concatenate([xc, xl], 0) if half == 0 else np.concatenate([xc[::-1], xl[::-1]], 0)
        cT = np.stack([_lay_vec(P["c"][b], 8), _lay_vec(P["c_ctx"], 8)], -1).reshape(128, 16)
        m = dict(xT=np.ascontiguousarray(seq.T), cT=np.ascontiguousarray(cT), gfin=_lay_vec(P["final_norm_g"], 8))
        m.update(C)
        for l in range(2):
            W = WL[l, half == 1]
            for n in LAYER_W:
                m["%s_%d" % (n, l)] = W[n]
        in_maps.append(m)
    rF = run_bass_kernel_spmd(ncF, in_maps, core_ids=cores).results
    out = np.zeros_like(P["x"])
    for core in cores:
        b, half = divmod(core, 2)
        xo = rF[core]["xo"].T
        if half == 0:
            out[b, 0:2048] = xo
        else:
            out[b, 2048:4096] = xo[::-1]
    return out.astype(np.float32)
```

```python
from contextlib import ExitStack
import math
import numpy as np
import concourse.bass as bass
import concourse.mybir as mybir
from concourse.bass_utils import run_bass_kernel_spmd

F32 = mybir.dt.float32
I32 = mybir.dt.int32
U32 = mybir.dt.uint32
ALU = mybir.AluOpType
AF = mybir.ActivationFunctionType
AX = mybir.AxisListType


class Sched:
    NDS = 4

    def __init__(self, nc, stack, gate=None, gate_val=0, nds=None):
        self.nc = nc
        self.ndsq = dict(sp=self.NDS, act=self.NDS, pool=self.NDS)
        if nds:
            self.ndsq.update(nds)
        self.gate = gate
        self.gate_val = gate_val
        self.eng = {"pe": nc.tensor, "dve": nc.vector, "act": nc.scalar,
                    "pool": nc.gpsimd, "sp": nc.sync}
        self.sem = {}
        self.cnt = {}
        _UID[0] += 1
        u = "q%d" % _UID[0]
        for e in self.eng:
            self.sem[e] = stack.enter_context(nc.semaphore(u + "s_" + e))
            self.cnt[e] = 0
        self.dq = {}
        for q in ("sp", "act", "pool"):
            sems = [stack.enter_context(nc.semaphore(u + "d_%s%d" % (q, i))) for i in range(self.ndsq[q])]
            self.dq[q] = {"sems": sems, "n": 0}
            for i, s in enumerate(sems):
                self.sem["d_%s%d" % (q, i)] = s
        self.seen = {e: {} for e in self.eng}
        self.prog = {e: [] for e in self.eng}
        self.lastw = {}
        self.reads = {}
        self.ninst = 0
        if gate is not None:
            self.sem["gate"] = gate
        if gate is not None and gate_val > 0:
            for e in self.eng:
                self.prog[e].append(("w", "gate", gate_val))

    def _need(self, need, ev):
        if ev is None:
            return
        s, v = ev
        if need.get(s, 0) < v:
            need[s] = v

    def _waits(self, e, reads, writes):
        need = {}
        for k in reads:
            self._need(need, self.lastw.get(k))
        for k in writes:
            self._need(need, self.lastw.get(k))
            for ev in self.reads.get(k, ()):
                self._need(need, ev)
        eng = self.eng[e]
        seen = self.seen[e]
        for s, v in need.items():
            if seen.get(s, 0) >= v:
                continue
            self.prog[e].append(("w", s, v))
            seen[s] = v
            self.ninst += 1

    def _commit(self, ev, reads, writes):
        for k in writes:
            self.lastw[k] = ev
            self.reads[k] = []
        for k in reads:
            if k in writes:
                continue
            self.reads.setdefault(k, []).append(ev)
            if len(self.reads[k]) > 24:
                d = {}
                for s, v in self.reads[k]:
                    if d.get(s, 0) < v:
                        d[s] = v
                self.reads[k] = list(d.items())

    def op(self, e, fn, reads=(), writes=()):
        reads = tuple(reads)
        writes = tuple(writes)
        self._waits(e, reads, writes)
        self.cnt[e] += 1
        self.prog[e].append(("i", fn, e, 1))
        self.ninst += 1
        self._commit((e, self.cnt[e]), reads, writes)

    def dma(self, q, fn, reads=(), writes=()):
        reads = tuple(reads)
        writes = tuple(writes)
        st = self.dq[q]
        i = st["n"]
        slot = i % self.ndsq[q]
        sname = "d_%s%d" % (q, slot)
        rnd = i // self.ndsq[q]
        eng = self.eng[q]
        if rnd > 0 and self.seen[q].get(sname, 0) < 16 * rnd:
            self.prog[q].append(("w", sname, 16 * rnd))
            self.seen[q][sname] = 16 * rnd
            self.ninst += 1
        self._waits(q, reads, writes)
        self.prog[q].append(("i", fn, sname, 16))
        st["n"] = i + 1
        self.ninst += 1
        self._commit((sname, 16 * (rnd + 1)), reads, writes)

    def finish(self, keys, e="sp"):
        self._waits(e, tuple(keys), ())

    def drain_all(self, e="sp"):
        need = {}
        for en, c in self.cnt.items():
            if c:
                need[en] = c
        for q, stq in self.dq.items():
            n = stq["n"]
            nq = self.ndsq[q]
            for slot in range(nq):
                uses = (n - slot + nq - 1) // nq if n > slot else 0
                if uses:
                    need["d_%s%d" % (q, slot)] = 16 * uses
        eng = self.eng[e]
        for s, v in need.items():
            if self.seen[e].get(s, 0) >= v:
                continue
            self.prog[e].append(("w", s, v))
            self.seen[e][s] = v

    def emit(self):
        nc = self.nc
        with nc.Block() as block:
            def mk(e):
                def body(engine):
                    for it in self.prog[e]:
                        if it[0] == "w":
                            engine.wait_ge(self.sem[it[1]], it[2])
                        else:
                            it[1](engine).then_inc(self.sem[it[2]], it[3])
                return body
            block.sync(mk("sp"))
            block.scalar(mk("act"))
            block.vector(mk("dve"))
            block.gpsimd(mk("pool"))
            block.tensor(mk("pe"))
        self.prog = {e: [] for e in self.eng}

    def sync_all(self):
        for e in self.eng:
            self.drain_all(e)
        self.lastw = {}
        self.reads = {}

D = 1024
NMOD = 6
INW = 2336
INW_T = 19
EPS = 1e-6
NTOK = 2176


_UID = [0]


def _mk(nc, st):
    _UID[0] += 1
    u = "u%d_" % _UID[0]

    def sb(name, shape, dt=F32):
        return st.enter_context(nc.sbuf_tensor(u + name, shape, dt))

    def ps(name, shape, dt=F32):
        return st.enter_context(nc.psum_tensor(u + name, shape, dt))
    return sb, ps


def _groups(n, g=512):
    out = []
    t = 0
    while t < n:
        out.append((t, min(g, n - t)))
        t += g
    return out


def build_PA(ntok=NTOK, nctx=128):
    nc = bass.Bass("TRN2", target_bir_lowering=False)
    xT = nc.dram_tensor("xT", [D, ntok], F32, kind="ExternalInput").ap()
    cT = nc.dram_tensor("cT", [128, 16], F32, kind="ExternalInput").ap()
    w_mod = nc.dram_tensor("w_mod", [D, NMOD * D], F32, kind="ExternalInput").ap()
    b_mod = nc.dram_tensor("b_mod", [128, 48], F32, kind="ExternalInput").ap()
    g1 = nc.dram_tensor("g1", [128, 8], F32, kind="ExternalInput").ap()
    w_in = nc.dram_tensor("w_in", [D, INW], F32, kind="ExternalInput").ap()
    ones = nc.dram_tensor("ones", [128, 128], F32, kind="ExternalInput").ap()
    colsT = nc.dram_tensor("colsT", [INW_T * 128, ntok], F32, kind="ExternalOutput").ap()
    modT = nc.dram_tensor("modT", [128, 96], F32, kind="ExternalOutput").ap()
    with ExitStack() as st:
        S = Sched(nc, st)
        sb, ps = _mk(nc, st)
        CT = sb("CT", [128, 16]); SC = sb("SC", [128, 16]); BM = sb("BM", [128, 48]); G1 = sb("G1", [128, 8])
        ONES = sb("ONES", [128, 128]); MOD = sb("MOD", [128, 96])
        A1 = sb("A1", [128, 16]); TMPA = sb("TMPA", [128, 16])
        WM = [sb("WM%d" % i, [128, 8, 512]) for i in range(2)]
        WIN = sb("WIN", [128, 8, INW])
        XT = [sb("XT%d" % i, [128, 8, 512]) for i in range(2)]
        XSQ = sb("XSQ", [128, 512]); RSTD = sb("RSTD", [128, 512]); TMP = sb("TMP", [128, 512])
        HT = [sb("HT%d" % i, [128, 8, 512]) for i in range(2)]
        OUTB = [sb("OUTB%d" % i, [128, 512]) for i in range(4)]
        pmod = ps("pmod", [128, 96]); pss = ps("pss", [128, 512])
        pout = [ps("pout%d" % i, [128, 512]) for i in range(3)]

        S.dma("sp", lambda e: e.dma_start(out=CT[:], in_=cT), writes=["CT"])
        S.dma("sp", lambda e: e.dma_start(out=BM[:], in_=b_mod), writes=["BM"])
        S.dma("sp", lambda e: e.dma_start(out=G1[:], in_=g1), writes=["G1"])
        S.dma("sp", lambda e: e.dma_start(out=ONES[:], in_=ones), writes=["ONES"])
        S.op("act", lambda e: e.activation(SC[:], CT[:], AF.Silu), reads=["CT"], writes=["SC"])
        SC3 = SC[:].rearrange("p (k c) -> p k c", c=2)
        wm_v = w_mod.rearrange("(k p) f -> p k f", p=128)
        for jg in range(12):
            b = jg % 2
            S.dma("act" if jg % 2 else "sp",
                  lambda e, jg=jg, b=b: e.dma_start(out=WM[b][:], in_=wm_v[:, :, jg * 512:(jg + 1) * 512]),
                  writes=["WM%d" % b])
            for j8 in range(4):
                j = jg * 4 + j8
                for k in range(8):
                    S.op("pe", lambda e, j=j, j8=j8, k=k, b=b: e.matmul(
                        pmod[:, 2 * j:2 * j + 2], WM[b][:, k, j8 * 128:(j8 + 1) * 128], SC3[:, k, :],
                        start=(k == 0), stop=(k == 7)), reads=["WM%d" % b, "SC"], writes=["pmod"])
        S.op("dve", lambda e: e.tensor_tensor(MOD[:].rearrange("p (j c) -> p j c", c=2),
                                              pmod[:].rearrange("p (j c) -> p j c", c=2),
                                              BM[:].unsqueeze(2).to_broadcast([128, 48, 2]), ALU.add),
             reads=["pmod", "BM"], writes=["MOD"])
        S.dma("sp", lambda e: e.dma_start(out=modT, in_=MOD[:]), reads=["MOD"], writes=["modT"])
        MOD3 = MOD[:].rearrange("p (j c) -> p j c", c=2)
        S.op("dve", lambda e: e.tensor_scalar(TMPA[:].rearrange("p (j c) -> p j c", c=2), MOD3[:, 8:16, :], 1.0, None, ALU.add),
             reads=["MOD"], writes=["TMPA"])
        S.op("dve", lambda e: e.tensor_tensor(A1[:].rearrange("p (j c) -> p j c", c=2),
                                              TMPA[:].rearrange("p (j c) -> p j c", c=2),
                                              G1[:].unsqueeze(2).to_broadcast([128, 8, 2]), ALU.mult),
             reads=["TMPA", "G1"], writes=["A1"])
        A13 = A1[:].rearrange("p (j c) -> p j c", c=2)
        win_v = w_in.rearrange("(k p) f -> p k f", p=128)
        for k in range(8):
            S.dma("pool", lambda e, k=k: e.dma_start(out=WIN[:, k, :], in_=win_v[:, k, :]), writes=["WIN%d" % k])
        xT_v = xT.rearrange("(k p) t -> p k t", p=128)
        grp = [(0, nctx, 1)] if nctx else []
        grp += [(nctx + t0, tn, 0) for (t0, tn) in _groups(ntok - nctx)]
        oi = 0
        for gi, (t0, tn, col) in enumerate(grp):
            b = gi % 2
            xk, hk = "XT%d" % b, "HT%d" % b
            S.dma("sp", lambda e, b=b, t0=t0, tn=tn: e.dma_start(out=XT[b][:, :, :tn], in_=xT_v[:, :, t0:t0 + tn]), writes=[xk])
            for k in range(8):
                S.op("act", lambda e, b=b, k=k, tn=tn: e.activation(XSQ[:, :tn], XT[b][:, k, :tn], AF.Square), reads=[xk], writes=["XSQ"])
                S.op("pe", lambda e, k=k, tn=tn: e.matmul(pss[:, :tn], ONES[:], XSQ[:, :tn], start=(k == 0), stop=(k == 7)),
                     reads=["ONES", "XSQ"], writes=["pss"])
            S.op("dve", lambda e, tn=tn: e.tensor_scalar(RSTD[:, :tn], pss[:, :tn], 1.0 / D, EPS, ALU.mult, ALU.add), reads=["pss"], writes=["RSTD"])
            S.op("act", lambda e, tn=tn: e.sqrt(RSTD[:, :tn], RSTD[:, :tn]), reads=["RSTD"], writes=["RSTD"])
            S.op("dve", lambda e, tn=tn: e.reciprocal(RSTD[:, :tn], RSTD[:, :tn]), reads=["RSTD"], writes=["RSTD"])
            for k in range(8):
                S.op("dve", lambda e, b=b, k=k, tn=tn: e.tensor_tensor(TMP[:, :tn], XT[b][:, k, :tn], RSTD[:, :tn], ALU.mult),
                     reads=[xk, "RSTD"], writes=["TMP"])
                S.op("act", lambda e, b=b, k=k, tn=tn, col=col: e.activation(
                    HT[b][:, k, :tn], TMP[:, :tn], AF.Identity, bias=MOD3[:, k, col:col + 1], scale=A13[:, k, col:col + 1]),
                    reads=["TMP", "MOD", "A1"], writes=[hk])
            for ot in range(INW_T):
                m = 128 if ot < INW_T - 1 else INW - 128 * (INW_T - 1)
                pb = oi % 3
                ob = oi % 4
                oi += 1
                for k in range(8):
                    S.op("pe", lambda e, b=b, k=k, tn=tn, ot=ot, m=m, pb=pb: e.matmul(
                        pout[pb][:m, :tn], WIN[:, k, ot * 128:ot * 128 + m], HT[b][:, k, :tn], start=(k == 0), stop=(k == 7)),
                        reads=["WIN%d" % k, hk], writes=["pout%d" % pb])
                if m < 128:
                    S.op("dve", lambda e, ob=ob: e.memset(OUTB[ob][:], 0.0), writes=["OUTB%d" % ob])
                eng = "act" if oi % 2 else "dve"
                if eng == "act":
                    S.op("act", lambda e, ob=ob, pb=pb, m=m, tn=tn: e.copy(OUTB[ob][:m, :tn], pout[pb][:m, :tn]),
                         reads=["pout%d" % pb], writes=["OUTB%d" % ob])
                else:
                    S.op("dve", lambda e, ob=ob, pb=pb, m=m, tn=tn: e.tensor_copy(OUTB[ob][:m, :tn], pout[pb][:m, :tn]),
                         reads=["pout%d" % pb], writes=["OUTB%d" % ob])
                S.dma("sp" if oi % 2 else "act", lambda e, ob=ob, ot=ot, t0=t0, tn=tn: e.dma_start(
                    out=colsT[ot * 128:(ot + 1) * 128, t0:t0 + tn], in_=OUTB[ob][:, :tn]),
                    reads=["OUTB%d" % ob], writes=["colsT_%d" % oi])
        S.drain_all("sp")
        S.emit()
    return nc


SEQT = 4352
S5_CH = [(0, 256)] + [(256 + 512 * i, 512) for i in range(8)]


def build_PB():
    nc = bass.Bass("TRN2", target_bir_lowering=False)
    uin = [nc.dram_tensor(n, [128, SEQT], F32, kind="ExternalInput").ap() for n in ("uf", "ub")]
    prm = nc.dram_tensor("prm", [128, 24], F32, kind="ExternalInput").ap()
    bre = nc.dram_tensor("bre", [128, 128], F32, kind="ExternalInput").ap()
    bim = nc.dram_tensor("bim", [128, 128], F32, kind="ExternalInput").ap()
    cre = nc.dram_tensor("cre", [128, 128], F32, kind="ExternalInput").ap()
    cim = nc.dram_tensor("cim", [128, 128], F32, kind="ExternalInput").ap()
    tau = nc.dram_tensor("tau", [128, 512], F32, kind="ExternalInput").ap()
    ident = nc.dram_tensor("ident", [128, 128], F32, kind="ExternalInput").ap()
    yout = [nc.dram_tensor(n, [128, SEQT], F32, kind="ExternalOutput").ap() for n in ("yf", "yb")]
    TWO_PI = 2.0 * math.pi
    with ExitStack() as st:
        S = Sched(nc, st)
        sb, ps = _mk(nc, st)
        PRM = sb("PRM", [128, 24]); BRE = sb("BRE", [128, 128]); BIM = sb("BIM", [128, 128])
        CRE = sb("CRE", [128, 128]); CIM = sb("CIM", [128, 128]); TAU = sb("TAU", [128, 512]); ID = sb("ID", [128, 128])
        names = ["DT", "LR", "MAG", "TH", "R", "R2", "RF", "FR", "SIN", "COS", "ARE", "AIM", "DEN", "AM1", "FRE", "FIM", "T0", "T1"]
        P = {n: sb("p_" + n, [128, 8]) for n in names}
        RI = sb("p_RI", [128, 8], I32)
        BBR = sb("BBR", [128, 128]); BBI = sb("BBI", [128, 128]); TB = sb("TB", [128, 128])
        PAD = sb("PAD", [128, 128])
        WBR = sb("WBR", [128, 8, 128]); WBI = sb("WBI", [128, 8, 128]); CR = sb("CR", [128, 8, 128]); CIN = sb("CIN", [128, 8, 128])
        TC = sb("TC", [128, 8, 512]); TS = sb("TS", [128, 8, 512]); RHO = sb("RHO", [128, 8, 512])
        RR = sb("RR", [128, 512]); RRF = sb("RRF", [128, 512]); RRI = sb("RRI", [128, 512], I32)
        UC = [sb("UC%d" % i, [128, 512]) for i in range(2)]
        W = {}
        for n in ("BR", "BI", "T1", "T2", "T3", "T4", "XR", "XI", "QR", "QI", "HR", "HI"):
            for i in range(2):
                W[n, i] = sb("w_%s%d" % (n, i), [128, 512])
        HP = sb("HP", [128, 8])
        YO = [sb("YO%d" % i, [128, 512]) for i in range(2)]
        pbr = [ps("pbr%d" % i, [128, 512]) for i in range(2)]
        pbi = [ps("pbi%d" % i, [128, 512]) for i in range(2)]
        py = [ps("py%d" % i, [128, 512]) for i in range(2)]
        ptr = ps("ptr", [128, 128])

        for (t, src, k) in ((PRM, prm, "PRM"), (BRE, bre, "BRE"), (BIM, bim, "BIM"), (CRE, cre, "CRE"), (CIM, cim, "CIM"),
                            (TAU, tau, "TAU"), (ID, ident, "ID")):
            S.dma("sp", lambda e, t=t, src=src: e.dma_start(out=t[:], in_=src), writes=[k])
        PR3 = PRM[:].rearrange("p (a c) -> p a c", c=3)
        K = ["PP"]

        def V(fn, reads=(), writes=()):
            S.op("dve", fn, reads=list(reads) + K, writes=list(writes) + K)

        def A(fn, reads=(), writes=()):
            S.op("act", fn, reads=list(reads) + K, writes=list(writes) + K)

        A(lambda e: e.activation(P["DT"][:], PR3[:, :, 2], AF.Exp), reads=["PRM"])
        V(lambda e: e.tensor_scalar(P["LR"][:], PR3[:, :, 0], -1e-4, None, ALU.min), reads=["PRM"])
        V(lambda e: e.tensor_tensor(P["T0"][:], P["LR"][:], P["DT"][:], ALU.mult))
        A(lambda e: e.activation(P["MAG"][:], P["T0"][:], AF.Exp))
        V(lambda e: e.tensor_tensor(P["TH"][:], PR3[:, :, 1], P["DT"][:], ALU.mult), reads=["PRM"])
        V(lambda e: e.tensor_scalar(P["R"][:], P["TH"][:], 1.0 / TWO_PI, None, ALU.mult))
        V(lambda e: e.tensor_scalar(P["R2"][:], P["R"][:], 0.25, None, ALU.add))
        for (src, dst) in (("R", "SIN"), ("R2", "COS")):
            V(lambda e, src=src: e.tensor_copy(RI[:], P[src][:]))
            V(lambda e: e.tensor_copy(P["RF"][:], RI[:]))
            V(lambda e, src=src: e.tensor_tensor(P["FR"][:], P[src][:], P["RF"][:], ALU.subtract))
            A(lambda e, dst=dst: e.activation(P[dst][:], P["FR"][:], AF.Sin, scale=TWO_PI))
        V(lambda e: e.tensor_tensor(P["ARE"][:], P["MAG"][:], P["COS"][:], ALU.mult))
        V(lambda e: e.tensor_tensor(P["AIM"][:], P["MAG"][:], P["SIN"][:], ALU.mult))
        V(lambda e: e.tensor_tensor(P["T0"][:], P["LR"][:], P["LR"][:], ALU.mult))
        V(lambda e: e.tensor_tensor(P["T1"][:], PR3[:, :, 1], PR3[:, :, 1], ALU.mult), reads=["PRM"])
        V(lambda e: e.tensor_tensor(P["DEN"][:], P["T0"][:], P["T1"][:], ALU.add))
        V(lambda e: e.reciprocal(P["DEN"][:], P["DEN"][:]))
        V(lambda e: e.tensor_scalar(P["AM1"][:], P["ARE"][:], -1.0, None, ALU.add))
        V(lambda e: e.tensor_tensor(P["T0"][:], P["AM1"][:], P["LR"][:], ALU.mult))
        V(lambda e: e.tensor_tensor(P["T1"][:], P["AIM"][:], PR3[:, :, 1], ALU.mult), reads=["PRM"])
        V(lambda e: e.tensor_tensor(P["T0"][:], P["T0"][:], P["T1"][:], ALU.add))
        V(lambda e: e.tensor_tensor(P["FRE"][:], P["T0"][:], P["DEN"][:], ALU.mult))
        V(lambda e: e.tensor_tensor(P["T0"][:], P["AIM"][:], P["LR"][:], ALU.mult))
        V(lambda e: e.tensor_tensor(P["T1"][:], P["AM1"][:], PR3[:, :, 1], ALU.mult), reads=["PRM"])
        V(lambda e: e.tensor_tensor(P["T0"][:], P["T0"][:], P["T1"][:], ALU.subtract))
        V(lambda e: e.tensor_tensor(P["FIM"][:], P["T0"][:], P["DEN"][:], ALU.mult))

        def v3(t):
            return t[:].rearrange("p (a h) -> p a h", h=16)

        def bc(n):
            return P[n][:].unsqueeze(2).to_broadcast([128, 8, 16])
        V(lambda e: e.tensor_tensor(v3(BBR), v3(BRE), bc("FRE"), ALU.mult), reads=["BRE"])
        V(lambda e: e.tensor_tensor(v3(TB), v3(BIM), bc("FIM"), ALU.mult), reads=["BIM"])
        V(lambda e: e.tensor_tensor(BBR[:], BBR[:], TB[:], ALU.subtract))
        V(lambda e: e.tensor_tensor(v3(BBI), v3(BIM), bc("FRE"), ALU.mult), reads=["BIM"])
        V(lambda e: e.tensor_tensor(v3(TB), v3(BRE), bc("FIM"), ALU.mult), reads=["BRE"])
        V(lambda e: e.tensor_tensor(BBI[:], BBI[:], TB[:], ALU.add))
        V(lambda e: e.tensor_scalar(CIM[:], CIM[:], -1.0, None, ALU.mult), reads=["CIM"], writes=["CIM"])
        V(lambda e: e.memset(CR[:], 0.0)); V(lambda e: e.memset(CIN[:], 0.0))
        for dj in range(8):
            j = dj % 4
            for (src, dst) in ((BBR, WBR), (BBI, WBI)):
                V(lambda e: e.memset(PAD[:], 0.0), writes=["PAD"])
                V(lambda e, src=src, dj=dj, j=j: e.tensor_copy(PAD[0:64, 32 * j:32 * j + 16], src[0:64, dj * 16:dj * 16 + 16]), writes=["PAD"])
                V(lambda e, src=src, dj=dj, j=j: e.tensor_copy(PAD[64:128, 32 * j + 16:32 * j + 32], src[64:128, dj * 16:dj * 16 + 16]), writes=["PAD"])
                S.op("pe", lambda e: e.transpose(ptr[:], PAD[:], ID[:]), reads=["PAD", "ID"], writes=["ptr"])
                S.op("act", lambda e, dst=dst, dj=dj: e.copy(dst[:, dj, :], ptr[:]), reads=["ptr"], writes=["WB"])
            for (src, dst) in ((CRE, CR), (CIM, CIN)):
                V(lambda e, src=src, dst=dst, dj=dj, j=j: e.tensor_copy(dst[0:64, dj, 32 * j:32 * j + 16], src[0:64, dj * 16:dj * 16 + 16]), reads=["CRE", "CIM"], writes=["CC"])
                V(lambda e, src=src, dst=dst, dj=dj, j=j: e.tensor_copy(dst[64:128, dj, 32 * j + 16:32 * j + 32], src[64:128, dj * 16:dj * 16 + 16]), reads=["CRE", "CIM"], writes=["CC"])
            for (off, dst) in ((0.0, TS), (0.25, TC)):
                V(lambda e, dj=dj, off=off: e.tensor_scalar(RR[:], TAU[:], P["R"][:, dj:dj + 1], off, ALU.mult, ALU.add), reads=["TAU"], writes=["RR"])
                V(lambda e: e.tensor_copy(RRI[:], RR[:]), reads=["RR"], writes=["RRI"])
                V(lambda e: e.tensor_copy(RRF[:], RRI[:]), reads=["RRI"], writes=["RRF"])
                V(lambda e: e.tensor_tensor(RRF[:], RR[:], RRF[:], ALU.subtract), reads=["RR"], writes=["RRF"])
                S.op("act", lambda e, dst=dst, dj=dj: e.activation(dst[:, dj, :], RRF[:], AF.Sin, scale=TWO_PI), reads=["RRF"], writes=["TAB"])
            V(lambda e, dj=dj: e.tensor_copy(RHO[:, dj, :], P["MAG"][:, dj:dj + 1].to_broadcast([128, 512])), writes=["TAB"])

        G = "pool"
        oi = 0
        for d in range(2):
            V(lambda e: e.memset(HP[:], 0.0), writes=["HP"])
            for ci, (t0, T) in enumerate(S5_CH):
                ub_ = (d * 9 + ci) % 2
                uk = "UC%d" % ub_
                S.dma("sp", lambda e, d=d, t0=t0, T=T, ub_=ub_: e.dma_start(out=UC[ub_][:, :T], in_=uin[d][:, t0:t0 + T]), writes=[uk])
                yb_ = (d * 9 + ci) % 2
                for j in range(4):
                    dj = d * 4 + j
                    b = j % 2
                    w = lambda n, b=b, T=T: W[n, b][:, :T]
                    k = lambda n, b=b: "w_%s%d" % (n, b)
                    S.op("pe", lambda e, dj=dj, b=b, T=T, ub_=ub_: e.matmul(pbr[b][:, :T], WBR[:, dj, :], UC[ub_][:, :T], start=True, stop=True),
                         reads=["WB", uk], writes=["pbr%d" % b])
                    S.op("pe", lambda e, dj=dj, b=b, T=T, ub_=ub_: e.matmul(pbi[b][:, :T], WBI[:, dj, :], UC[ub_][:, :T], start=True, stop=True),
                         reads=["WB", uk], writes=["pbi%d" % b])
                    S.op("act", lambda e, w=w, b=b, T=T: e.copy(w("BR"), pbr[b][:, :T]), reads=["pbr%d" % b], writes=[k("BR")])
                    S.op("act", lambda e, w=w, b=b, T=T: e.copy(w("BI"), pbi[b][:, :T]), reads=["pbi%d" % b], writes=[k("BI")])
                    cs = lambda dj=dj, T=T: TC[:, dj, :T]
                    sn = lambda dj=dj, T=T: TS[:, dj, :T]
                    S.op("dve", lambda e, w=w, cs=cs: e.tensor_tensor(w("T1"), cs(), w("BR"), ALU.mult), reads=["TAB", k("BR")], writes=[k("T1")])
                    S.op("dve", lambda e, w=w, sn=sn: e.tensor_tensor(w("T2"), sn(), w("BI"), ALU.mult), reads=["TAB", k("BI")], writes=[k("T2")])
                    S.op("dve", lambda e, w=w: e.tensor_tensor(w("XR"), w("T1"), w("T2"), ALU.add), reads=[k("T1"), k("T2")], writes=[k("XR")])
                    S.op(G, lambda e, w=w, cs=cs: e.tensor_tensor(w("T3"), cs(), w("BI"), ALU.mult), reads=["TAB", k("BI")], writes=[k("T3")])
                    S.op(G, lambda e, w=w, sn=sn: e.tensor_tensor(w("T4"), sn(), w("BR"), ALU.mult), reads=["TAB", k("BR")], writes=[k("T4")])
                    S.op(G, lambda e, w=w: e.tensor_tensor(w("XI"), w("T3"), w("T4"), ALU.subtract), reads=[k("T3"), k("T4")], writes=[k("XI")])
                    S.op("dve", lambda e, w=w, dj=dj, j=j, T=T: e.tensor_tensor_scan(w("QR"), RHO[:, dj, :T], w("XR"), HP[:, 2 * j:2 * j + 1], ALU.mult, ALU.add),
                         reads=["TAB", k("XR"), "HP"], writes=[k("QR")])
                    S.op("dve", lambda e, w=w, dj=dj, j=j, T=T: e.tensor_tensor_scan(w("QI"), RHO[:, dj, :T], w("XI"), HP[:, 2 * j + 1:2 * j + 2], ALU.mult, ALU.add),
                         reads=["TAB", k("XI"), "HP"], writes=[k("QI")])
                    S.op("dve", lambda e, w=w, cs=cs: e.tensor_tensor(w("T1"), cs(), w("QR"), ALU.mult), reads=["TAB", k("QR")], writes=[k("T1")])
                    S.op("dve", lambda e, w=w, sn=sn: e.tensor_tensor(w("T2"), sn(), w("QI"), ALU.mult), reads=["TAB", k("QI")], writes=[k("T2")])
                    S.op("dve", lambda e, w=w: e.tensor_tensor(w("HR"), w("T1"), w("T2"), ALU.subtract), reads=[k("T1"), k("T2")], writes=[k("HR")])
                    S.op(G, lambda e, w=w, sn=sn: e.tensor_tensor(w("T3"), sn(), w("QR"), ALU.mult), reads=["TAB", k("QR")], writes=[k("T3")])
                    S.op(G, lambda e, w=w, cs=cs: e.tensor_tensor(w("T4"), cs(), w("QI"), ALU.mult), reads=["TAB", k("QI")], writes=[k("T4")])
                    S.op(G, lambda e, w=w: e.tensor_tensor(w("HI"), w("T3"), w("T4"), ALU.add), reads=[k("T3"), k("T4")], writes=[k("HI")])
                    S.op("act", lambda e, b=b, j=j, T=T: e.copy(HP[:, 2 * j:2 * j + 1], W["HR", b][:, T - 1:T]), reads=[k("HR")], writes=["HP"])
                    S.op("act", lambda e, b=b, j=j, T=T: e.copy(HP[:, 2 * j + 1:2 * j + 2], W["HI", b][:, T - 1:T]), reads=[k("HI")], writes=["HP"])
                    S.op("pe", lambda e, dj=dj, w=w, j=j, yb_=yb_, T=T: e.matmul(py[yb_][:, :T], CR[:, dj, :], w("HR"), start=(j == 0), stop=False),
                         reads=["CC", k("HR")], writes=["py%d" % yb_])
                    S.op("pe", lambda e, dj=dj, w=w, j=j, yb_=yb_, T=T: e.matmul(py[yb_][:, :T], CIN[:, dj, :], w("HI"), start=False, stop=(j == 3)),
                         reads=["CC", k("HI")], writes=["py%d" % yb_])
                S.op("act", lambda e, yb_=yb_, T=T: e.copy(YO[yb_][:, :T], py[yb_][:, :T]), reads=["py%d" % yb_], writes=["YO%d" % yb_])
                oi += 1
                S.dma("act", lambda e, d=d, yb_=yb_, t0=t0, T=T: e.dma_start(out=yout[d][:, t0:t0 + T], in_=YO[yb_][:, :T]),
                      reads=["YO%d" % yb_], writes=["yout_%d" % oi])
        S.drain_all("sp")
        S.emit()
    return nc


NCH = 68


def build_PC():
    nc = bass.Bass("TRN2", target_bir_lowering=False)
    I = {}
    for d in range(2):
        I["qT", d] = nc.dram_tensor("qT%d" % d, [128, SEQT], F32, kind="ExternalInput").ap()
        I["kT", d] = nc.dram_tensor("kT%d" % d, [128, SEQT], F32, kind="ExternalInput").ap()
        I["v", d] = nc.dram_tensor("v%d" % d, [SEQT, 256], F32, kind="ExternalInput").ap()
        I["zT", d] = nc.dram_tensor("zT%d" % d, [16, SEQT], F32, kind="ExternalInput").ap()
        I["wg", d] = nc.dram_tensor("wg%d" % d, [16, 128], F32, kind="ExternalInput").ap()
        I["bg", d] = nc.dram_tensor("bg%d" % d, [128, 1], F32, kind="ExternalInput").ap()
        I["o", d] = nc.dram_tensor("o%d" % d, [SEQT, 256], F32, kind="ExternalOutput").ap()
    rst = nc.dram_tensor("rst", [128, 512], F32, kind="ExternalInput").ap()
    tmask = nc.dram_tensor("tmask", [64, 256], F32, kind="ExternalInput").ap()
    blk = nc.dram_tensor("blk", [128, 256], F32, kind="ExternalInput").ap()
    hmask = nc.dram_tensor("hmask", [128, 4], F32, kind="ExternalInput").ap()
    ident = nc.dram_tensor("ident", [128, 128], F32, kind="ExternalInput").ap()
    QSC = 32 ** -0.5
    with ExitStack() as st:
        S = Sched(nc, st)
        sb, ps = _mk(nc, st)
        RST = sb("RST", [128, 512]); TM = sb("TM", [64, 256]); BLK = sb("BLK", [128, 256]); HM = sb("HM", [128, 4]); ID = sb("ID", [128, 128])
        WG = sb("WG", [16, 128]); BG = sb("BG", [128, 1]); NBG = sb("NBG", [128, 1])
        Wb = {}
        for n in ("Q", "K", "LA", "B", "E", "D", "QE", "QS", "KD", "KS0", "KS1", "KS2", "KS3"):
            for i in range(2):
                Wb[n, i] = sb("g_%s%d" % (n, i), [128, 512])
        Z = [sb("Z%d" % i, [16, 512]) for i in range(2)]
        VV = [sb("VV%d" % i, [64, 8, 256]) for i in range(2)]
        DEC = [sb("DEC%d" % i, [128, 8]) for i in range(2)]
        KDT = [sb("KDT%d" % i, [64, 128]) for i in range(2)]
        STt = [sb("ST%d" % i, [64, 256]) for i in range(2)]
        OB = [sb("OB%d" % i, [64, 256]) for i in range(3)]
        KVM = sb("KVM", [128, 256])
        SS = [sb("SS%d" % i, [128, 256]) for i in range(2)]
        pza = ps("pza", [128, 512])
        pt0 = ps("pt0", [64, 128])
        pt = [pt0, pt0]
        pkv = [ps("pkv%d" % i, [128, 256]) for i in range(2)]
        psc = [ps("psc%d" % i, [64, 256]) for i in range(2)]
        po = [ps("po%d" % i, [64, 256]) for i in range(2)]
        for (t, src, k) in ((RST, rst, "RST"), (TM, tmask, "TM"), (BLK, blk, "BLK"), (HM, hmask, "HM"), (ID, ident, "ID")):
            S.dma("sp", lambda e, t=t, src=src: e.dma_start(out=t[:], in_=src), writes=[k])
        gc = 0
        oi = 0
        for d in range(2):
            S.dma("sp", lambda e, d=d: e.dma_start(out=WG[:], in_=I["wg", d]), writes=["WG"])
            S.dma("sp", lambda e, d=d: e.dma_start(out=BG[:], in_=I["bg", d]), writes=["BG"])
            S.op("dve", lambda e: e.tensor_scalar(NBG[:], BG[:], -1.0, None, ALU.mult), reads=["BG"], writes=["NBG"])
            S.op("dve", lambda e: e.memset(SS[0][:], 0.0), writes=["SS0"])
            scur = 0
            for bi, (t0, T) in enumerate(S5_CH):
                nchk = T // 64
                b = (d * 9 + bi) % 2
                w = lambda n, b=b, T=T: Wb[n, b][:, :T]
                k = lambda n, b=b: "g_%s%d" % (n, b)
                w3 = lambda n, b=b, T=T: Wb[n, b][:, :T].rearrange("p (c s) -> p c s", s=64)
                S.dma("sp", lambda e, d=d, b=b, t0=t0, T=T: e.dma_start(out=Wb["Q", b][:, :T], in_=I["qT", d][:, t0:t0 + T]), writes=[k("Q")])
                S.dma("act", lambda e, d=d, b=b, t0=t0, T=T: e.dma_start(out=Wb["K", b][:, :T], in_=I["kT", d][:, t0:t0 + T]), writes=[k("K")])
                S.dma("sp", lambda e, d=d, b=b, t0=t0, T=T: e.dma_start(out=Z[b][:, :T], in_=I["zT", d][:, t0:t0 + T]), writes=["Z%d" % b])
                S.dma("act", lambda e, d=d, b=b, t0=t0, T=T, nchk=nchk: e.dma_start(
                    out=VV[b][:, :nchk, :], in_=I["v", d][t0:t0 + T, :].rearrange("(c s) f -> s c f", s=64)), writes=["VV%d" % b])
                S.op("pe", lambda e, b=b, T=T: e.matmul(pza[:, :T], WG[:], Z[b][:, :T], start=True, stop=True), reads=["WG", "Z%d" % b], writes=["pza"])
                S.op("act", lambda e, w=w, T=T: e.activation(w("E"), pza[:, :T], AF.Exp, bias=NBG[:], scale=-1.0), reads=["pza", "NBG"], writes=[k("E")])
                S.op("act", lambda e, w=w: e.activation(w("E"), w("E"), AF.Ln, bias=1.0), reads=[k("E")], writes=[k("E")])
                S.op("dve", lambda e, w=w: e.tensor_scalar(w("LA"), w("E"), -1.0 / 16.0, None, ALU.mult), reads=[k("E")], writes=[k("LA")])
                S.op("dve", lambda e, w=w, T=T: e.tensor_tensor_scan(w("B"), RST[:, :T], w("LA"), 0.0, ALU.mult, ALU.add), reads=["RST", k("LA")], writes=[k("B")])
                S.op("act", lambda e, b=b, w3=w3, nchk=nchk: e.activation(DEC[b][:, :nchk], w3("B")[:, :, 63], AF.Exp), reads=[k("B")], writes=["DEC%d" % b])
                S.op("act", lambda e, w=w: e.activation(w("E"), w("B"), AF.Exp), reads=[k("B")], writes=[k("E")])
                S.op("dve", lambda e, w=w: e.scalar_tensor_tensor(w("QE"), w("Q"), QSC, w("E"), ALU.mult, ALU.mult), reads=[k("Q"), k("E")], writes=[k("QE")])
                S.op("dve", lambda e, w3=w3, nchk=nchk: e.tensor_tensor(w3("D"), w3("B"), w3("B")[:, :, 32:33].to_broadcast([128, nchk, 64]), ALU.subtract),
                     reads=[k("B")], writes=[k("D")])
                S.op("act", lambda e, w=w: e.activation(w("E"), w("D"), AF.Exp), reads=[k("D"), k("QE")], writes=[k("E")])
                S.op("dve", lambda e, w=w: e.scalar_tensor_tensor(w("QS"), w("Q"), QSC, w("E"), ALU.mult, ALU.mult), reads=[k("Q"), k("E")], writes=[k("QS")])
                S.op("act", lambda e, w=w: e.activation(w("E"), w("D"), AF.Exp, scale=-1.0), reads=[k("D"), k("QS")], writes=[k("E")])
                S.op("dve", lambda e, w=w: e.tensor_tensor(w("LA"), w("K"), w("E"), ALU.mult), reads=[k("K"), k("E"), k("B")], writes=[k("LA")])
                for h in range(4):
                    S.op("pool", lambda e, w=w, h=h: e.tensor_scalar(w("KS%d" % h), w("LA"), HM[:, h:h + 1], None, ALU.mult),
                         reads=[k("LA"), "HM"], writes=[k("KS%d" % h)])
                S.op("dve", lambda e, w3=w3, nchk=nchk: e.tensor_tensor(w3("D"), w3("B")[:, :, 63:64].to_broadcast([128, nchk, 64]), w3("B"), ALU.subtract),
                     reads=[k("B"), k("E"), k("LA")], writes=[k("D")])
                S.op("act", lambda e, w=w: e.activation(w("D"), w("D"), AF.Exp), reads=[k("D")], writes=[k("D")])
                S.op("dve", lambda e, w=w: e.tensor_tensor(w("KD"), w("K"), w("D"), ALU.mult), reads=[k("K"), k("D")], writes=[k("KD")])
                for c in range(nchk):
                    p2 = gc % 2
                    gc += 1
                    cs = slice(c * 64, (c + 1) * 64)
                    S.op("pe", lambda e, b=b, cs=cs, p2=p2: e.transpose(pt[p2][:], Wb["KD", b][:, cs], ID[:]), reads=[k("KD"), "ID"], writes=["pt"])
                    S.op("act", lambda e, p2=p2: e.copy(KDT[p2][:], pt[p2][:]), reads=["pt"], writes=["KDT%d" % p2])
                    S.op("pe", lambda e, b=b, c=c, p2=p2: e.matmul(pkv[p2][:], KDT[p2][:], VV[b][:, c, :], start=True, stop=True),
                         reads=["KDT%d" % p2, "VV%d" % b], writes=["pkv%d" % p2])
                    for h in range(4):
                        S.op("pe", lambda e, b=b, cs=cs, p2=p2, h=h: e.matmul(psc[p2][:, h * 64:(h + 1) * 64], Wb["KS%d" % h, b][:, cs], Wb["QS", b][:, cs],
                                                                            start=True, stop=True),
                             reads=[k("KS%d" % h), k("QS")], writes=["psc%d" % p2])
                    S.op("dve", lambda e, p2=p2: e.tensor_tensor(STt[p2][:], psc[p2][:], TM[:], ALU.mult), reads=["psc%d" % p2, "TM"], writes=["ST%d" % p2])
                    for h in range(4):
                        hs = slice(h * 64, (h + 1) * 64)
                        S.op("pe", lambda e, b=b, c=c, p2=p2, hs=hs: e.matmul(po[p2][:, hs], STt[p2][:, hs], VV[b][:, c, hs], start=True, stop=False),
                             reads=["ST%d" % p2, "VV%d" % b], writes=["po%d" % p2])
                        S.op("pe", lambda e, b=b, cs=cs, p2=p2, hs=hs, scur=scur: e.matmul(po[p2][:, hs], Wb["QE", b][:, cs], SS[scur][:, hs], start=False, stop=True),
                             reads=[k("QE"), "SS%d" % scur], writes=["po%d" % p2])
                    ob = oi % 3
                    oi += 1
                    S.op("act", lambda e, ob=ob, p2=p2: e.copy(OB[ob][:], po[p2][:]), reads=["po%d" % p2], writes=["OB%d" % ob])
                    S.dma("sp" if oi % 2 else "act", lambda e, d=d, ob=ob, t0=t0, c=c: e.dma_start(out=I["o", d][t0 + c * 64:t0 + (c + 1) * 64, :], in_=OB[ob][:]),
                          reads=["OB%d" % ob], writes=["o_%d" % oi])
                    S.op("dve", lambda e, p2=p2: e.tensor_tensor(KVM[:], pkv[p2][:], BLK[:], ALU.mult), reads=["pkv%d" % p2, "BLK"], writes=["KVM"])
                    S.op("dve", lambda e, b=b, c=c, scur=scur: e.scalar_tensor_tensor(SS[1 - scur][:], SS[scur][:], DEC[b][:, c:c + 1], KVM[:], ALU.mult, ALU.add),
                         reads=["SS%d" % scur, "DEC%d" % b, "KVM"], writes=["SS%d" % (1 - scur)])
                    scur = 1 - scur
        S.drain_all("sp")
        S.emit()
    return nc


NEXP = 16384


def build_PD(ntok, nctx, last):
    nc = bass.Bass("TRN2", target_bir_lowering=False)
    def din(name, shape, dt=F32):
        return nc.dram_tensor(name, shape, dt, kind="ExternalInput").ap()
    xT = din("xT", [D, ntok]); modT = din("modT", [128, 96])
    su = din("su", [ntok, 256]); sv = din("sv", [ntok, 256]); gg = din("gg", [ntok, 512])
    s5u = din("s5u", [256, ntok]); yf = din("yf", [256, ntok]); yb = din("yb", [256, ntok])
    of_ = din("of", [ntok, 512]); ob_ = din("ob", [ntok, 512])
    w_out = din("w_out", [D, D]); wsT = din("wsT", [128, 512]); sgub = din("sgub", [128, 4]); s5d = din("s5d", [128, 2])
    wglu = din("wglu", [256, 512]); ng = din("ng", [128, 512]); g2 = din("g2", [128, 8]); wq = din("wq", [D, 2048])
    keysT = din("keysT", [128, 2048]); eu = din("eu", [NEXP, D]); ev = din("ev", [NEXP, D]); gfin = din("gfin", [128, 8])
    ones = din("ones", [128, 128]); ident = din("ident", [128, 128]); iota16 = din("iota16", [128, 16])
    xo = nc.dram_tensor("xo", [D, ntok], F32, kind="ExternalOutput").ap()
    nb = ntok // 128
    with ExitStack() as st:
        S = Sched(nc, st)
        sb, ps = _mk(nc, st)
        MOD = sb("MOD", [128, 96]); WOUT = sb("WOUT", [128, 8, D]); WS = sb("WS", [128, 512]); SGUB = sb("SGUB", [128, 4]); S5D = sb("S5D", [128, 2])
        WGLU = sb("WGLU", [128, 2, 512]); NG = sb("NG", [128, 512]); G2 = sb("G2", [128, 8]); WQ = sb("WQ", [128, 8, 2048]); KEYS = sb("KEYS", [128, 2048])
        GF = sb("GF", [128, 8]); ONES = sb("ONES", [128, 128]); ID = sb("ID", [128, 128]); IOTA = sb("IOTA", [128, 16])
        A2 = sb("A2", [128, 16]); TA = sb("TA", [128, 16])
        U_ = sb("U_", [128, 256]); V_ = sb("V_", [128, 256]); GU = sb("GU", [128, 256]); GV = sb("GV", [128, 256]); SQ = sb("SQ", [128, 512])
        SS = sb("SS", [128, 8]); VN = sb("VN", [128, 256]); MIX = sb("MIX", [128, D])
        S5U = sb("S5U", [128, 2, 128]); YF = sb("YF", [128, 2, 128]); YB = sb("YB", [128, 2, 128]); GE = sb("GE", [128, 2, 128]); SG = sb("SG", [128, 256])
        OF = sb("OF", [128, 512]); OB = sb("OB", [128, 512]); GG = sb("GG", [128, 512]); SL = sb("SL", [128, 512])
        XT = sb("XT", [128, 8, 128]); X1 = sb("X1", [128, 8, 128]); XO = XT; HT = sb("HT", [128, 8, 128]); MIXT = HT; HTOK = sb("HTOK", [128, D])
        RSTD = sb("RSTD", [128, 128]); TMPB = sb("TMPB", [128, 128])
        SC = sb("SC", [128, 2048])
        M16 = sb("M16", [128, 256]); I16 = sb("I16", [128, 256], U32); IF16 = sb("IF16", [128, 256]); I1S = sb("I1S", [128, 128])
        CS = sb("CS", [128, 2048]); SC2 = CS; QT = CS[:].rearrange("p (q t) -> p q t", t=128); CS2 = sb("CS2", [128, 256])
        T16 = sb("T16", [128, 128]); P16 = sb("P16", [128, 128], U32); PF = sb("PF", [128, 128]); AI = sb("AI", [128, 128], I32)
        AFL = sb("AFL", [128, 128]); BFL = sb("BFL", [128, 128]); E1 = sb("E1", [128, 128]); E2 = sb("E2", [128, 128])
        EG = sb("EG", [128, 128]); GATE = sb("GATE", [128, 128]); IDXTI = sb("IDXTI", [128, 128], I32); GATET = sb("GATET", [128, 128])
        ACTT = sb("ACTT", [128, 128]); WT = sb("WT", [128, 128])
        UG = [sb("UG%d" % i, [128, D]) for i in range(2)]; VG = UG
        HB = [sb("HB%d" % i, [128, D]) for i in range(2)]
        P0 = ps("P0", [128, 2048]); P1 = ps("P1", [128, 1024]); P2 = ps("P2", [128, 512]); P3 = ps("P3", [128, 512])

        def V(fn, r=(), w=()):
            S.op("dve", fn, reads=r, writes=w)

        def A(fn, r=(), w=()):
            S.op("act", fn, reads=r, writes=w)

        def PE(fn, r=(), w=()):
            S.op("pe", fn, reads=r, writes=w)

        def LD(q, t, src, key):
            S.dma(q, lambda e: e.dma_start(out=t, in_=src), writes=[key])

        LD("sp", MOD[:], modT, "MOD"); LD("sp", WS[:], wsT, "WS"); LD("sp", SGUB[:], sgub, "SGUB"); LD("sp", S5D[:], s5d, "S5D")
        LD("sp", WGLU[:], wglu.rearrange("(c p) f -> p c f", p=128), "WGLU"); LD("sp", NG[:], ng, "NG"); LD("sp", G2[:], g2, "G2")
        LD("sp", KEYS[:], keysT, "KEYS"); LD("sp", GF[:], gfin, "GF"); LD("sp", ONES[:], ones, "ONES"); LD("sp", ID[:], ident, "ID"); LD("sp", IOTA[:], iota16, "IOTA")
        wo_v = w_out.rearrange("(k p) f -> p k f", p=128)
        wq_v = wq.rearrange("(k p) f -> p k f", p=128)
        for k in range(8):
            LD("act", WOUT[:, k, :], wo_v[:, k, :], "WOUT")
            LD("act", WQ[:, k, :], wq_v[:, k, :], "WQ")
        MOD3 = MOD[:].rearrange("p (j c) -> p j c", c=2)
        V(lambda e: e.tensor_scalar(TA[:].rearrange("p (j c) -> p j c", c=2), MOD3[:, 32:40, :], 1.0, None, ALU.add), ["MOD"], ["TA"])
        V(lambda e: e.tensor_tensor(A2[:].rearrange("p (j c) -> p j c", c=2), TA[:].rearrange("p (j c) -> p j c", c=2),
                                    G2[:].unsqueeze(2).to_broadcast([128, 8, 2]), ALU.mult), ["TA", "G2"], ["A2"])
        A23 = A2[:].rearrange("p (j c) -> p j c", c=2)

        def rs_from_ss(ss, n, scale):
            V(lambda e: e.tensor_scalar(ss, ss, scale, EPS, ALU.mult, ALU.add), ["SS"], ["SS"])
            A(lambda e: e.sqrt(ss, ss), ["SS"], ["SS"])
            V(lambda e: e.reciprocal(ss, ss), ["SS"], ["SS"])

        def top16(src, scratch, mout, iout, n):
            V(lambda e: e.max(mout[:, 0:8], src), ["TK"], ["TK"])
            V(lambda e: e.max_index(iout[:, 0:8], mout[:, 0:8], src), ["TK"], ["TK"])
            V(lambda e: e.match_replace(scratch, mout[:, 0:8], src, -1e30), ["TK"], ["TK"])
            V(lambda e: e.max(mout[:, 8:16], scratch), ["TK"], ["TK"])
            V(lambda e: e.max_index(iout[:, 8:16], mout[:, 8:16], scratch), ["TK"], ["TK"])

        xT_v = xT.rearrange("(k p) t -> p k t", p=128)
        xo_v = xo.rearrange("(k p) t -> p k t", p=128)
        s5u_v = s5u.rearrange("(c p) t -> p c t", p=128)
        yf_v = yf.rearrange("(c p) t -> p c t", p=128)
        yb_v = yb.rearrange("(c p) t -> p c t", p=128)
        for bi in range(nb):
            tk = slice(bi * 128, (bi + 1) * 128)
            col = 1 if bi * 128 < nctx else 0
            LD("sp", U_[:], su[tk, :], "U_"); LD("sp", V_[:], sv[tk, :], "V_"); LD("sp", GG[:], gg[tk, :], "GG")
            LD("act", S5U[:], s5u_v[:, :, tk], "S5U"); LD("act", YF[:], yf_v[:, :, tk], "YF"); LD("act", YB[:], yb_v[:, :, tk], "YB")
            LD("sp", OF[:], of_[tk, :], "OF"); LD("sp", OB[:], ob_[tk, :], "OB"); LD("act", XT[:], xT_v[:, :, tk], "XT")
            A(lambda e: e.activation(GU[:], U_[:], AF.Gelu), ["U_"], ["GU"])
            A(lambda e: e.activation(GV[:], V_[:], AF.Gelu), ["V_"], ["GV"])
            V(lambda e: e.tensor_tensor(SQ[:, 0:256], GV[:], GV[:], ALU.mult), ["GV"], ["SQ"])
            V(lambda e: e.tensor_reduce(SS[:, 0:4], SQ[:, 0:256].rearrange("p (h d) -> p h d", d=64), AX.X, ALU.add), ["SQ"], ["SS"])
            rs_from_ss(SS[:, 0:4], 4, 1.0 / 64)
            V(lambda e: e.tensor_tensor(VN[:].rearrange("p (h d) -> p h d", d=64), GV[:].rearrange("p (h d) -> p h d", d=64),
                                        SS[:, 0:4].unsqueeze(2).to_broadcast([128, 4, 64]), ALU.mult), ["GV", "SS"], ["VN"])
            for h in range(4):
                PE(lambda e, h=h: e.matmul(P2[:, h * 64:(h + 1) * 64], WS[:, h * 128:(h + 1) * 128], VN[:, h * 64:(h + 1) * 64], start=True, stop=True),
                   ["WS", "VN"], ["P2"])
            V(lambda e: e.tensor_tensor(MIX[:, 0:256].rearrange("p (h d) -> p h d", d=64), P2[:, 0:256].rearrange("p (h d) -> p h d", d=64),
                                        SGUB[:].unsqueeze(2).to_broadcast([128, 4, 64]), ALU.add), ["P2", "SGUB"], ["MIXa"])
            V(lambda e: e.tensor_tensor(MIX[:, 0:256], MIX[:, 0:256], GU[:], ALU.mult), ["GU"], ["MIXa"])
            V(lambda e: e.tensor_tensor(YF[:], YF[:], YB[:], ALU.add), ["YB"], ["YF"])
            for ct in range(2):
                V(lambda e, ct=ct: e.scalar_tensor_tensor(YF[:, ct, :], S5U[:, ct, :], S5D[:, ct:ct + 1], YF[:, ct, :], ALU.mult, ALU.add),
                  ["S5U", "S5D"], ["YF"])
            A(lambda e: e.activation(GE[:], YF[:], AF.Gelu), ["YF"], ["GE"])
            for ct in range(2):
                PE(lambda e, ct=ct: e.matmul(P3[:, 0:512], GE[:, ct, :], WGLU[:, ct, :], start=(ct == 0), stop=(ct == 1)), ["GE", "WGLU"], ["P3"])
            A(lambda e: e.activation(SG[:], P3[:, 256:512], AF.Sigmoid), ["P3"], ["SG"])
            V(lambda e: e.tensor_tensor(MIX[:, 256:512], P3[:, 0:256], SG[:], ALU.mult), ["P3", "SG"], ["MIXb"])
            V(lambda e: e.tensor_tensor(OF[:], OF[:], OB[:], ALU.add), ["OB"], ["OF"])
            V(lambda e: e.tensor_tensor(SQ[:], OF[:], OF[:], ALU.mult), ["OF"], ["SQ"])
            V(lambda e: e.tensor_reduce(SS[:, 0:8], SQ[:].rearrange("p (h d) -> p h d", d=64), AX.X, ALU.add), ["SQ"], ["SS"])
            rs_from_ss(SS[:, 0:8], 8, 1.0 / 64)
            V(lambda e: e.tensor_tensor(OF[:].rearrange("p (h d) -> p h d", d=64), OF[:].rearrange("p (h d) -> p h d", d=64),
                                        SS[:, 0:8].unsqueeze(2).to_broadcast([128, 8, 64]), ALU.mult), ["SS"], ["OF"])
            V(lambda e: e.tensor_tensor(OF[:], OF[:], NG[:], ALU.mult), ["NG"], ["OF"])
            A(lambda e: e.activation(SL[:], GG[:], AF.Silu), ["GG"], ["SL"])
            V(lambda e: e.tensor_tensor(MIX[:, 512:1024], OF[:], SL[:], ALU.mult), ["OF", "SL"], ["MIXc"])
            for f in range(8):
                pp, pk = (P2, "P2") if f % 2 == 0 else (P3, "P3")
                PE(lambda e, f=f, pp=pp: e.transpose(pp[:, 0:128], MIX[:, f * 128:(f + 1) * 128], ID[:]), ["MIXa", "MIXb", "MIXc", "ID"], [pk])
                A(lambda e, f=f, pp=pp: e.copy(MIXT[:, f, :], pp[:, 0:128]), [pk], ["HT"])
            for ot in range(8):
                pp, pk = (P2, "P2") if ot % 2 == 0 else (P3, "P3")
                for k in range(8):
                    PE(lambda e, ot=ot, k=k, pp=pp: e.matmul(pp[:, 0:128], WOUT[:, k, ot * 128:(ot + 1) * 128], MIXT[:, k, :], start=(k == 0), stop=(k == 7)),
                       ["WOUT", "HT"], [pk])
                V(lambda e, ot=ot, pp=pp, col=col: e.scalar_tensor_tensor(X1[:, ot, :], pp[:, 0:128], MOD3[:, 16 + ot, col:col + 1], XT[:, ot, :], ALU.mult, ALU.add),
                  [pk, "MOD", "XT"], ["X1"])
            for k in range(8):
                A(lambda e, k=k: e.activation(TMPB[:], X1[:, k, :], AF.Square), ["X1"], ["TMPB"])
                PE(lambda e, k=k: e.matmul(P2[:, 0:128], ONES[:], TMPB[:], start=(k == 0), stop=(k == 7)), ["ONES", "TMPB"], ["P2"])
            V(lambda e: e.tensor_scalar(RSTD[:], P2[:, 0:128], 1.0 / D, EPS, ALU.mult, ALU.add), ["P2"], ["RSTD"])
            A(lambda e: e.sqrt(RSTD[:], RSTD[:]), ["RSTD"], ["RSTD"])
            V(lambda e: e.reciprocal(RSTD[:], RSTD[:]), ["RSTD"], ["RSTD"])
            for k in range(8):
                V(lambda e, k=k: e.tensor_tensor(TMPB[:], X1[:, k, :], RSTD[:], ALU.mult), ["X1", "RSTD"], ["TMPB"])
                A(lambda e, k=k, col=col: e.activation(HT[:, k, :], TMPB[:], AF.Identity, bias=MOD3[:, 24 + k, col:col + 1], scale=A23[:, k, col:col + 1]),
                  ["TMPB", "MOD", "A2"], ["HT"])
            for k in range(8):
                pp, pk = (P2, "P2") if k % 2 == 0 else (P3, "P3")
                PE(lambda e, k=k, pp=pp: e.transpose(pp[:, 0:128], HT[:, k, :], ID[:]), ["HT", "ID"], [pk])
                A(lambda e, k=k, pp=pp: e.copy(HTOK[:, k * 128:(k + 1) * 128], pp[:, 0:128]), [pk], ["HTOK"])
            for qt in range(16):
                pp, pk = (P2, "P2") if qt % 2 == 0 else (P3, "P3")
                for k in range(8):
                    PE(lambda e, qt=qt, k=k, pp=pp: e.matmul(pp[:, 0:128], WQ[:, k, qt * 128:(qt + 1) * 128], HT[:, k, :], start=(k == 0), stop=(k == 7)),
                       ["WQ", "HT"], [pk])
                if qt % 2 == 0:
                    A(lambda e, qt=qt, pp=pp: e.copy(QT[:, qt, :], pp[:, 0:128]), [pk], ["TK"])
                else:
                    V(lambda e, qt=qt, pp=pp: e.tensor_copy(QT[:, qt, :], pp[:, 0:128]), [pk], ["TK"])
            for qt in range(16):
                PE(lambda e, qt=qt: e.matmul(P0[:, qt * 128:(qt + 1) * 128], QT[:, qt, :], KEYS[:, qt * 128:(qt + 1) * 128], start=True, stop=True),
                   ["TK", "KEYS"], ["P0a", "P0b"])
            for q4 in range(4):
                A(lambda e, q4=q4: e.copy(SC[:, q4 * 512:(q4 + 1) * 512], P0[:, q4 * 512:(q4 + 1) * 512]), ["P0a", "P0b"], ["TK"])
            for qt in range(16):
                top16(SC[:, qt * 128:(qt + 1) * 128], SC2[:, qt * 128:(qt + 1) * 128], M16[:, qt * 16:(qt + 1) * 16], I16[:, qt * 16:(qt + 1) * 16], 128)
            V(lambda e: e.tensor_copy(IF16[:], I16[:]), ["TK"], ["TK"])
            M4 = M16[:].rearrange("p (h q k) -> p h q k", q=2, k=16)
            IF4 = IF16[:].rearrange("p (h q k) -> p h q k", q=2, k=16)
            I1S3 = I1S[:].rearrange("p (h k) -> p h k", k=16)
            V(lambda e: e.tensor_scalar(I1S3, IF4[:, :, 0, :], 128.0, None, ALU.mult), ["TK"], ["TK"])
            CS4 = CS[:].rearrange("p (h a b) -> p h a b", a=16, b=16)
            V(lambda e: e.tensor_tensor(CS4, M4[:, :, 0, :].unsqueeze(3).to_broadcast([128, 8, 16, 16]),
                                        M4[:, :, 1, :].unsqueeze(2).to_broadcast([128, 8, 16, 16]), ALU.add), ["TK"], ["TK"])
            for h in range(8):
                top16(CS[:, h * 256:(h + 1) * 256], CS2[:], T16[:, h * 16:(h + 1) * 16], P16[:, h * 16:(h + 1) * 16], 256)
            V(lambda e: e.tensor_copy(PF[:], P16[:]), ["TK"], ["TK"])
            V(lambda e: e.tensor_scalar(AFL[:], PF[:], -7.5, 1.0 / 16, ALU.add, ALU.mult), ["TK"], ["TK"])
            V(lambda e: e.tensor_copy(AI[:], AFL[:]), ["TK"], ["TK"])
            V(lambda e: e.tensor_copy(AFL[:], AI[:]), ["TK"], ["TK"])
            V(lambda e: e.scalar_tensor_tensor(BFL[:], AFL[:], -16.0, PF[:], ALU.mult, ALU.add), ["TK"], ["TK"])
            EQ4 = CS[:].rearrange("p (h k a) -> p h k a", k=16, a=16)
            io4 = IOTA[:].unsqueeze(1).unsqueeze(1).to_broadcast([128, 8, 16, 16])
            for (sel, src, dst) in ((AFL, I1S3, E1), (BFL, IF4[:, :, 1, :], E2)):
                V(lambda e, sel=sel: e.tensor_tensor(EQ4, io4, sel[:].rearrange("p (h k) -> p h k", k=16).unsqueeze(3).to_broadcast([128, 8, 16, 16]), ALU.is_equal),
                  ["TK", "IOTA"], ["TK"])
                V(lambda e, src=src: e.tensor_tensor(EQ4, EQ4, src.unsqueeze(2).to_broadcast([128, 8, 16, 16]), ALU.mult), ["TK"], ["TK"])
                V(lambda e, dst=dst: e.tensor_reduce(dst[:], CS[:].rearrange("p (m a) -> p m a", a=16), AX.X, ALU.add), ["TK"], ["TK"])
            V(lambda e: e.tensor_tensor(E1[:], E1[:], E2[:], ALU.add), ["TK"], ["TK"])
            T3 = T16[:].rearrange("p (h k) -> p h k", k=16)
            V(lambda e: e.tensor_tensor(EG[:].rearrange("p (h k) -> p h k", k=16), T3, T3[:, :, 0:1].to_broadcast([128, 8, 16]), ALU.subtract), ["TK"], ["TK"])
            A(lambda e: e.activation(EG[:], EG[:], AF.Exp), ["TK"], ["TK"])
            V(lambda e: e.tensor_reduce(SS[:, 0:8], EG[:].rearrange("p (h k) -> p h k", k=16), AX.X, ALU.add), ["TK"], ["SS"])
            V(lambda e: e.reciprocal(SS[:, 0:8], SS[:, 0:8]), ["SS"], ["SS"])
            V(lambda e: e.tensor_tensor(GATE[:].rearrange("p (h k) -> p h k", k=16), EG[:].rearrange("p (h k) -> p h k", k=16),
                                        SS[:, 0:8].unsqueeze(2).to_broadcast([128, 8, 16]), ALU.mult), ["TK", "SS"], ["GATE"])
            PE(lambda e: e.transpose(P2[:, 0:128], E1[:], ID[:]), ["TK", "ID"], ["P2"])
            V(lambda e: e.tensor_copy(IDXTI[:], P2[:, 0:128]), ["P2"], ["IDXTI"])
            PE(lambda e: e.transpose(P3[:, 0:128], GATE[:], ID[:]), ["GATE", "ID"], ["P3"])
            A(lambda e: e.copy(GATET[:], P3[:, 0:128]), ["P3"], ["GATET"])
            for t in range(128):
                b = t % 2
                S.dma("pool", lambda e, t=t, b=b: e.indirect_dma_start(out=UG[b][:], out_offset=None, in_=eu,
                                                                       in_offset=bass.IndirectOffsetOnAxis(ap=IDXTI[:, t:t + 1], axis=0)),
                      reads=["IDXTI"], writes=["UG%d" % b])
                pk = "P0a" if b == 0 else "P0b"
                for hf in range(2):
                    PE(lambda e, t=t, b=b, hf=hf: e.matmul(P0[:, b * 1024 + hf * 512:b * 1024 + (hf + 1) * 512], ID[:, t:t + 1].to_broadcast([128, 128]),
                                                           HTOK[:, hf * 512:(hf + 1) * 512], start=True, stop=True), ["ID", "HTOK"], [pk])
                    A(lambda e, b=b, hf=hf: e.copy(HB[b][:, hf * 512:(hf + 1) * 512], P0[:, b * 1024 + hf * 512:b * 1024 + (hf + 1) * 512]), [pk], ["HB%d" % b])
                V(lambda e, t=t, b=b: e.scalar_tensor_tensor(UG[b][:], UG[b][:], 1.0, HB[b][:], ALU.mult, ALU.mult, accum_out=ACTT[:, t:t + 1]),
                  ["HB%d" % b], ["UG%d" % b, "ACTT"])
            A(lambda e: e.activation(WT[:], ACTT[:], AF.Gelu), ["ACTT"], ["WT"])
            V(lambda e: e.tensor_tensor(WT[:], WT[:], GATET[:], ALU.mult), ["GATET"], ["WT"])
            for t in range(128):
                b = t % 2
                S.dma("pool", lambda e, t=t, b=b: e.indirect_dma_start(out=VG[b][:], out_offset=None, in_=ev,
                                                                       in_offset=bass.IndirectOffsetOnAxis(ap=IDXTI[:, t:t + 1], axis=0)),
                      reads=["IDXTI"], writes=["UG%d" % b])
                for ot in range(8):
                    PE(lambda e, t=t, b=b, ot=ot: e.matmul(P1[:, ot * 128 + t:ot * 128 + t + 1], VG[b][:, ot * 128:(ot + 1) * 128], WT[:, t:t + 1],
                                                           start=True, stop=True), ["UG%d" % b, "WT"], ["P1"])
            for ot in range(8):
                V(lambda e, ot=ot, col=col: e.scalar_tensor_tensor(XO[:, ot, :], P1[:, ot * 128:(ot + 1) * 128], MOD3[:, 40 + ot, col:col + 1], X1[:, ot, :],
                                                                   ALU.mult, ALU.add), ["P1", "MOD", "X1"], ["XT"])
            if last:
                for k in range(8):
                    A(lambda e, k=k: e.activation(TMPB[:], XO[:, k, :], AF.Square), ["XT"], ["TMPB"])
                    PE(lambda e, k=k: e.matmul(P2[:, 0:128], ONES[:], TMPB[:], start=(k == 0), stop=(k == 7)), ["ONES", "TMPB"], ["P2"])
                V(lambda e: e.tensor_scalar(RSTD[:], P2[:, 0:128], 1.0 / D, EPS, ALU.mult, ALU.add), ["P2"], ["RSTD"])
                A(lambda e: e.sqrt(RSTD[:], RSTD[:]), ["RSTD"], ["RSTD"])
                V(lambda e: e.reciprocal(RSTD[:], RSTD[:]), ["RSTD"], ["RSTD"])
                for k in range(8):
                    V(lambda e, k=k: e.scalar_tensor_tensor(XO[:, k, :], XO[:, k, :], GF[:, k:k + 1], RSTD[:], ALU.mult, ALU.mult), ["RSTD", "GF"], ["XT"])
            S.dma("sp", lambda e, tk=tk: e.dma_start(out=xo_v[:, :, tk], in_=XO[:]), reads=["XT"], writes=["xo_%d" % bi])
        S.drain_all("sp")
        S.emit()
    return nc


SEQC = 256
SEQL = 4096
OWN = 2176


def _mirror(t0, T):
    if t0 < SEQC:
        return 0, SEQC
    i = (t0 - SEQC) // 512
    return SEQC + SEQL - 512 * (i + 1), 512


def emit_A(nc, S, sb, ps, io, tl_all=False):
    xT, cT, w_mod, b_mod, g1, w_in, ones = io["xT"], io["cT"], io["w_mod"], io["b_mod"], io["g1"], io["w_in"], io["ones"]
    FMU, FMQ, FMK, FMZ, VT, TL, MODS = io["FMU"], io["FMQ"], io["FMK"], io["FMZ"], io["VT"], io["TL"], io["MODS"]
    CT = sb("CT", [128, 16]); SC = sb("SC", [128, 16]); BM = sb("BM", [128, 48]); G1 = sb("G1", [128, 8])
    ONES = sb("ONES", [128, 128]); MOD = sb("MOD", [128, 96])
    A1 = sb("A1", [128, 16]); TMPA = sb("TMPA", [128, 16])
    WM = [sb("WM%d" % i, [128, 8, 512]) for i in range(2)]
    WIN = sb("WIN", [128, 8, INW])
    XT = [sb("XT%d" % i, [128, 8, 512]) for i in range(2)]
    XSQ = sb("XSQ", [128, 512]); RSTD = sb("RSTD", [128, 512]); TMP = sb("TMP", [128, 512])
    HT = [sb("HT%d" % i, [128, 8, 512]) for i in range(2)]
    OUTB = [sb("OUTB%d" % i, [128, 512]) for i in range(4)]
    pmod = ps("pmod", [128, 96]); pss = ps("pss", [128, 512])
    pout = [ps("pout%d" % i, [128, 512]) for i in range(3)]
    S.dma("sp", lambda e: e.dma_start(out=CT[:], in_=cT), writes=["CT"])
    S.dma("sp", lambda e: e.dma_start(out=BM[:], in_=b_mod), writes=["BM"])
    S.dma("sp", lambda e: e.dma_start(out=G1[:], in_=g1), writes=["G1"])
    S.dma("sp", lambda e: e.dma_start(out=ONES[:], in_=ones), writes=["ONES"])
    S.op("act", lambda e: e.activation(SC[:], CT[:], AF.Silu), reads=["CT"], writes=["SC"])
    SC3 = SC[:].rearrange("p (k c) -> p k c", c=2)
    wm_v = w_mod.rearrange("(k p) f -> p k f", p=128)
    for jg in range(12):
        b = jg % 2
        S.dma("act" if jg % 2 else "sp",
              lambda e, jg=jg, b=b: e.dma_start(out=WM[b][:], in_=wm_v[:, :, jg * 512:(jg + 1) * 512]), writes=["WM%d" % b])
        for j8 in range(4):
            j = jg * 4 + j8
            for k in range(8):
                S.op("pe", lambda e, j=j, j8=j8, k=k, b=b: e.matmul(
                    pmod[:, 2 * j:2 * j + 2], WM[b][:, k, j8 * 128:(j8 + 1) * 128], SC3[:, k, :],
                    start=(k == 0), stop=(k == 7)), reads=["WM%d" % b, "SC"], writes=["pmod"])
    S.op("dve", lambda e: e.tensor_tensor(MOD[:].rearrange("p (j c) -> p j c", c=2), pmod[:].rearrange("p (j c) -> p j c", c=2),
                                          BM[:].unsqueeze(2).to_broadcast([128, 48, 2]), ALU.add), reads=["pmod", "BM"], writes=["MOD"])
    S.dma("sp", lambda e: e.dma_start(out=MODS, in_=MOD[:]), reads=["MOD"], writes=["MODS"])
    MOD3 = MOD[:].rearrange("p (j c) -> p j c", c=2)
    S.op("dve", lambda e: e.tensor_scalar(TMPA[:].rearrange("p (j c) -> p j c", c=2), MOD3[:, 8:16, :], 1.0, None, ALU.add), reads=["MOD"], writes=["TMPA"])
    S.op("dve", lambda e: e.tensor_tensor(A1[:].rearrange("p (j c) -> p j c", c=2), TMPA[:].rearrange("p (j c) -> p j c", c=2),
                                          G1[:].unsqueeze(2).to_broadcast([128, 8, 2]), ALU.mult), reads=["TMPA", "G1"], writes=["A1"])
    A13 = A1[:].rearrange("p (j c) -> p j c", c=2)
    win_v = w_in.rearrange("(k p) f -> p k f", p=128)
    for k in range(8):
        S.dma("pool", lambda e, k=k: e.dma_start(out=WIN[:, k, :], in_=win_v[:, k, :]), writes=["WIN%d" % k])
    WK = ["WIN%d" % k for k in range(8)]
    xT_v = xT.rearrange("(k p) t -> p k t", p=128)
    grp = [(0, SEQC, 1, -1)] + [(SEQC + 512 * g, 512, 0, g) for g in range(8)]
    cnt = {"o": 0}

    def evac(dst_ap_fn, pb, m, tn, key, cm=False):
        ob = cnt["o"] % 4
        cnt["o"] += 1
        if cm:
            o_ap = lambda: OUTB[ob][:m, :512].rearrange("p (w r) -> p w r", r=8)
            i_ap = lambda: pout[pb][:m, :512].rearrange("p (r w) -> p w r", w=64)
        else:
            o_ap = lambda: OUTB[ob][:m, :tn]
            i_ap = lambda: pout[pb][:m, :tn]
        if cnt["o"] % 2:
            S.op("act", lambda e: e.copy(o_ap(), i_ap()), reads=["pout%d" % pb], writes=["OUTB%d" % ob])
        else:
            S.op("dve", lambda e: e.tensor_copy(o_ap(), i_ap()), reads=["pout%d" % pb], writes=["OUTB%d" % ob])
        S.dma("sp" if cnt["o"] % 2 else "act", lambda e: dst_ap_fn(e, OUTB[ob]), reads=["OUTB%d" % ob], writes=["%s_%d" % (key, cnt["o"])])

    pi = 0
    for gi, (t0, tn, col, g) in enumerate(grp):
        b = gi % 2
        xk, hk = "XT%d" % b, "HT%d" % b
        S.dma("sp", lambda e, b=b, t0=t0, tn=tn: e.dma_start(out=XT[b][:, :, :tn], in_=xT_v[:, :, t0:t0 + tn]), writes=[xk])
        for k in range(8):
            S.op("act", lambda e, b=b, k=k, tn=tn: e.activation(XSQ[:, :tn], XT[b][:, k, :tn], AF.Square), reads=[xk], writes=["XSQ"])
            S.op("pe", lambda e, k=k, tn=tn: e.matmul(pss[:, :tn], ONES[:], XSQ[:, :tn], start=(k == 0), stop=(k == 7)), reads=["ONES", "XSQ"], writes=["pss"])
        S.op("dve", lambda e, tn=tn: e.tensor_scalar(RSTD[:, :tn], pss[:, :tn], 1.0 / D, EPS, ALU.mult, ALU.add), reads=["pss"], writes=["RSTD"])
        S.op("act", lambda e, tn=tn: e.sqrt(RSTD[:, :tn], RSTD[:, :tn]), reads=["RSTD"], writes=["RSTD"])
        S.op("dve", lambda e, tn=tn: e.reciprocal(RSTD[:, :tn], RSTD[:, :tn]), reads=["RSTD"], writes=["RSTD"])
        for k in range(8):
            S.op("dve", lambda e, b=b, k=k, tn=tn: e.tensor_tensor(TMP[:, :tn], XT[b][:, k, :tn], RSTD[:, :tn], ALU.mult), reads=[xk, "RSTD"], writes=["TMP"])
            S.op("act", lambda e, b=b, k=k, tn=tn, col=col: e.activation(HT[b][:, k, :tn], TMP[:, :tn], AF.Identity, bias=MOD3[:, k, col:col + 1],
                                                                         scale=A13[:, k, col:col + 1]), reads=["TMP", "MOD", "A1"], writes=[hk])
        fm = [(FMU, 0, 512, 128), (FMU, 128, 640, 128), (FMQ, 0, 768, 128), (FMQ, 128, 896, 128), (FMK, 0, 1024, 128), (FMK, 128, 1152, 128), (FMZ, 0, 2304, 32)]
        for (dst, r0, c0, m) in fm:
            pb = pi % 3
            pi += 1
            for k in range(8):
                S.op("pe", lambda e, b=b, k=k, tn=tn, c0=c0, m=m, pb=pb: e.matmul(pout[pb][:m, :tn], WIN[:, k, c0:c0 + m], HT[b][:, k, :tn], start=(k == 0), stop=(k == 7)),
                     reads=WK + [hk], writes=["pout%d" % pb])
            if dst is FMU or g < 0:
                evac(lambda e, ob, dst=dst, r0=r0, m=m, t0=t0, tn=tn: e.dma_start(out=dst[r0:r0 + m, t0:t0 + tn], in_=ob[:m, :tn]), pb, m, tn, "fm")
            else:
                evac(lambda e, ob, dst=dst, r0=r0, m=m, g=g: e.dma_start(
                    out=dst[r0:r0 + m, SEQC:].rearrange("p (w r) -> p w r", r=64)[:, :, 8 * g:8 * g + 8],
                    in_=ob[:m, :512].rearrange("p (w r) -> p w r", r=8)), pb, m, 512, "fm", cm=True)
        for ti in range(tn // 128):
            tl = [(VT, t0 + ti * 128, 0, 1280)]
            own_row = None
            if tl_all:
                own_row = t0 + ti * 128
            elif g < 0 and ti == 0:
                own_row = 0
            elif 0 <= g < 4:
                own_row = 128 + g * 512 + ti * 128
            if own_row is not None:
                tl += [(TL, own_row, 0, 0), (TL, own_row, 512, 1792)]
            for (dst, row, dc, c0) in tl:
                pb = pi % 3
                pi += 1
                for k in range(8):
                    S.op("pe", lambda e, b=b, k=k, ti=ti, c0=c0, pb=pb: e.matmul(pout[pb][:, :], HT[b][:, k, ti * 128:(ti + 1) * 128], WIN[:, k, c0:c0 + 512],
                                                                               start=(k == 0), stop=(k == 7)), reads=WK + [hk], writes=["pout%d" % pb])
                evac(lambda e, ob, dst=dst, row=row, dc=dc: e.dma_start(out=dst[row:row + 128, dc:dc + 512], in_=ob[:, :]), pb, 128, 512, "tm")


def emit_B(nc, S, sb, ps, io):
    FMU, YFs, YBs = io["FMU"], io["YF"], io["YB"]
    tau, ident = io["tau"], io["ident"]
    for ct in range(2):
        prm, bre, bim, cre, cim = io["prm"][ct], io["bre"][ct], io["bim"][ct], io["cre"][ct], io["cim"][ct]
        uin = [FMU, FMU]
        yout = [YFs, YBs]
        X = "c%d_" % ct
        sub = ExitStack()
        sb, ps = _mk(nc, sub)
        TWO_PI = 2.0 * math.pi
        PRM = sb(X + "PRM", [128, 24]); BRE = sb(X + "BRE", [128, 128]); BIM = sb(X + "BIM", [128, 128])
        CRE = sb(X + "CRE", [128, 128]); CIM = sb(X + "CIM", [128, 128]); TAU = sb(X + "TAU", [128, 512]); ID = sb(X + "ID", [128, 128])
        names = ["DT", "LR", "MAG", "TH", "R", "R2", "RF", "FR", "SIN", "COS", "ARE", "AIM", "DEN", "AM1", "FRE", "FIM", "T0", "T1"]
        P = {n: sb(X + "p_" + n, [128, 8]) for n in names}
        RI = sb(X + "p_RI", [128, 8], I32)
        BBR = sb(X + "BBR", [128, 128]); BBI = sb(X + "BBI", [128, 128]); TB = sb(X + "TB", [128, 128])
        PAD = sb(X + "PAD", [128, 128])
        WBR = sb(X + "WBR", [128, 8, 128]); WBI = sb(X + "WBI", [128, 8, 128]); CR = sb(X + "CR", [128, 8, 128]); CIN = sb(X + "CIN", [128, 8, 128])
        TC = sb(X + "TC", [128, 8, 512]); TS = sb(X + "TS", [128, 8, 512]); RHO = sb(X + "RHO", [128, 8, 512])
        RR = sb(X + "RR", [128, 512]); RRF = sb(X + "RRF", [128, 512]); RRI = sb(X + "RRI", [128, 512], I32)
        UC = [sb(X + "UC%d" % i, [128, 512]) for i in range(2)]
        W = {}
        for n in ("BR", "BI", "T1", "T2", "T3", "T4", "XR", "XI", "QR", "QI", "HR", "HI"):
            for i in range(2):
                W[n, i] = sb(X + "w_%s%d" % (n, i), [128, 512])
        HP = sb(X + "HP", [128, 8])
        YO = [sb(X + "YO%d" % i, [128, 512]) for i in range(2)]
        pbr = [ps(X + "pbr%d" % i, [128, 512]) for i in range(2)]
        pbi = [ps(X + "pbi%d" % i, [128, 512]) for i in range(2)]
        py = [ps(X + "py%d" % i, [128, 512]) for i in range(2)]
        ptr = ps(X + "ptr", [128, 128])

        for (t, src, k) in ((PRM, prm, "PRM"), (BRE, bre, "BRE"), (BIM, bim, "BIM"), (CRE, cre, "CRE"), (CIM, cim, "CIM"),
                            (TAU, tau, "TAU"), (ID, ident, "ID")):
            S.dma("sp", lambda e, t=t, src=src: e.dma_start(out=t[:], in_=src), writes=[k])
        PR3 = PRM[:].rearrange("p (a c) -> p a c", c=3)
        K = ["PP"]

        def V(fn, reads=(), writes=()):
            S.op("dve", fn, reads=list(reads) + K, writes=list(writes) + K)

        def A(fn, reads=(), writes=()):
            S.op("act", fn, reads=list(reads) + K, writes=list(writes) + K)

        A(lambda e: e.activation(P["DT"][:], PR3[:, :, 2], AF.Exp), reads=["PRM"])
        V(lambda e: e.tensor_scalar(P["LR"][:], PR3[:, :, 0], -1e-4, None, ALU.min), reads=["PRM"])
        V(lambda e: e.tensor_tensor(P["T0"][:], P["LR"][:], P["DT"][:], ALU.mult))
        A(lambda e: e.activation(P["MAG"][:], P["T0"][:], AF.Exp))
        V(lambda e: e.tensor_tensor(P["TH"][:], PR3[:, :, 1], P["DT"][:], ALU.mult), reads=["PRM"])
        V(lambda e: e.tensor_scalar(P["R"][:], P["TH"][:], 1.0 / TWO_PI, None, ALU.mult))
        V(lambda e: e.tensor_scalar(P["R2"][:], P["R"][:], 0.25, None, ALU.add))
        for (src, dst) in (("R", "SIN"), ("R2", "COS")):
            V(lambda e, src=src: e.tensor_copy(RI[:], P[src][:]))
            V(lambda e: e.tensor_copy(P["RF"][:], RI[:]))
            V(lambda e, src=src: e.tensor_tensor(P["FR"][:], P[src][:], P["RF"][:], ALU.subtract))
            A(lambda e, dst=dst: e.activation(P[dst][:], P["FR"][:], AF.Sin, scale=TWO_PI))
        V(lambda e: e.tensor_tensor(P["ARE"][:], P["MAG"][:], P["COS"][:], ALU.mult))
        V(lambda e: e.tensor_tensor(P["AIM"][:], P["MAG"][:], P["SIN"][:], ALU.mult))
        V(lambda e: e.tensor_tensor(P["T0"][:], P["LR"][:], P["LR"][:], ALU.mult))
        V(lambda e: e.tensor_tensor(P["T1"][:], PR3[:, :, 1], PR3[:, :, 1], ALU.mult), reads=["PRM"])
        V(lambda e: e.tensor_tensor(P["DEN"][:], P["T0"][:], P["T1"][:], ALU.add))
        V(lambda e: e.reciprocal(P["DEN"][:], P["DEN"][:]))
        V(lambda e: e.tensor_scalar(P["AM1"][:], P["ARE"][:], -1.0, None, ALU.add))
        V(lambda e: e.tensor_tensor(P["T0"][:], P["AM1"][:], P["LR"][:], ALU.mult))
        V(lambda e: e.tensor_tensor(P["T1"][:], P["AIM"][:], PR3[:, :, 1], ALU.mult), reads=["PRM"])
        V(lambda e: e.tensor_tensor(P["T0"][:], P["T0"][:], P["T1"][:], ALU.add))
        V(lambda e: e.tensor_tensor(P["FRE"][:], P["T0"][:], P["DEN"][:], ALU.mult))
        V(lambda e: e.tensor_tensor(P["T0"][:], P["AIM"][:], P["LR"][:], ALU.mult))
        V(lambda e: e.tensor_tensor(P["T1"][:], P["AM1"][:], PR3[:, :, 1], ALU.mult), reads=["PRM"])
        V(lambda e: e.tensor_tensor(P["T0"][:], P["T0"][:], P["T1"][:], ALU.subtract))
        V(lambda e: e.tensor_tensor(P["FIM"][:], P["T0"][:], P["DEN"][:], ALU.mult))

        def v3(t):
            return t[:].rearrange("p (a h) -> p a h", h=16)

        def bc(n):
            return P[n][:].unsqueeze(2).to_broadcast([128, 8, 16])
        V(lambda e: e.tensor_tensor(v3(BBR), v3(BRE), bc("FRE"), ALU.mult), reads=["BRE"])
        V(lambda e: e.tensor_tensor(v3(TB), v3(BIM), bc("FIM"), ALU.mult), reads=["BIM"])
        V(lambda e: e.tensor_tensor(BBR[:], BBR[:], TB[:], ALU.subtract))
        V(lambda e: e.tensor_tensor(v3(BBI), v3(BIM), bc("FRE"), ALU.mult), reads=["BIM"])
        V(lambda e: e.tensor_tensor(v3(TB), v3(BRE), bc("FIM"), ALU.mult), reads=["BRE"])
        V(lambda e: e.tensor_tensor(BBI[:], BBI[:], TB[:], ALU.add))
        V(lambda e: e.tensor_scalar(CIM[:], CIM[:], -1.0, None, ALU.mult), reads=["CIM"], writes=["CIM"])
        V(lambda e: e.memset(CR[:], 0.0)); V(lambda e: e.memset(CIN[:], 0.0))
        for dj in range(8):
            j = dj % 4
            for (src, dst) in ((BBR, WBR), (BBI, WBI)):
                V(lambda e: e.memset(PAD[:], 0.0), writes=["PAD"])
                V(lambda e, src=src, dj=dj, j=j: e.tensor_copy(PAD[0:64, 32 * j:32 * j + 16], src[0:64, dj * 16:dj * 16 + 16]), writes=["PAD"])
                V(lambda e, src=src, dj=dj, j=j: e.tensor_copy(PAD[64:128, 32 * j + 16:32 * j + 32], src[64:128, dj * 16:dj * 16 + 16]), writes=["PAD"])
                S.op("pe", lambda e: e.transpose(ptr[:], PAD[:], ID[:]), reads=["PAD", "ID"], writes=["ptr"])
                S.op("act", lambda e, dst=dst, dj=dj: e.copy(dst[:, dj, :], ptr[:]), reads=["ptr"], writes=["WB"])
            for (src, dst) in ((CRE, CR), (CIM, CIN)):
                V(lambda e, src=src, dst=dst, dj=dj, j=j: e.tensor_copy(dst[0:64, dj, 32 * j:32 * j + 16], src[0:64, dj * 16:dj * 16 + 16]), reads=["CRE", "CIM"], writes=["CC"])
                V(lambda e, src=src, dst=dst, dj=dj, j=j: e.tensor_copy(dst[64:128, dj, 32 * j + 16:32 * j + 32], src[64:128, dj * 16:dj * 16 + 16]), reads=["CRE", "CIM"], writes=["CC"])
            for (off, dst) in ((0.0, TS), (0.25, TC)):
                V(lambda e, dj=dj, off=off: e.tensor_scalar(RR[:], TAU[:], P["R"][:, dj:dj + 1], off, ALU.mult, ALU.add), reads=["TAU"], writes=["RR"])
                V(lambda e: e.tensor_copy(RRI[:], RR[:]), reads=["RR"], writes=["RRI"])
                V(lambda e: e.tensor_copy(RRF[:], RRI[:]), reads=["RRI"], writes=["RRF"])
                V(lambda e: e.tensor_tensor(RRF[:], RR[:], RRF[:], ALU.subtract), reads=["RR"], writes=["RRF"])
                S.op("act", lambda e, dst=dst, dj=dj: e.activation(dst[:, dj, :], RRF[:], AF.Sin, scale=TWO_PI), reads=["RRF"], writes=["TAB"])
            V(lambda e, dj=dj: e.tensor_copy(RHO[:, dj, :], P["MAG"][:, dj:dj + 1].to_broadcast([128, 512])), writes=["TAB"])

        G = "pool"
        oi = 0
        for d in range(2):
            V(lambda e: e.memset(HP[:], 0.0), writes=["HP"])
            for ci, (t0, T) in enumerate(S5_CH):
                ub_ = (d * 9 + ci) % 2
                uk = "UC%d" % ub_
                n0 = t0 if d == 0 else _mirror(t0, T)[0]
                S.dma("sp", lambda e, d=d, n0=n0, T=T, ub_=ub_, ct=ct: e.dma_start(out=UC[ub_][:, :T], in_=uin[d][ct * 128:(ct + 1) * 128, n0:n0 + T]), writes=[uk])
                ucv = (lambda ub_=ub_, T=T: UC[ub_][:, :T]) if d == 0 else (lambda ub_=ub_, T=T: UC[ub_][:, :T][:, ::-1])
                yb_ = (d * 9 + ci) % 2
                for j in range(4):
                    dj = d * 4 + j
                    b = j % 2
                    w = lambda n, b=b, T=T: W[n, b][:, :T]
                    k = lambda n, b=b: "w_%s%d" % (n, b)
                    S.op("pe", lambda e, dj=dj, b=b, T=T, ucv=ucv: e.matmul(pbr[b][:, :T], WBR[:, dj, :], ucv(), start=True, stop=True),
                         reads=["WB", uk], writes=["pbr%d" % b])
                    S.op("pe", lambda e, dj=dj, b=b, T=T, ucv=ucv: e.matmul(pbi[b][:, :T], WBI[:, dj, :], ucv(), start=True, stop=True),
                         reads=["WB", uk], writes=["pbi%d" % b])
                    S.op("act", lambda e, w=w, b=b, T=T: e.copy(w("BR"), pbr[b][:, :T]), reads=["pbr%d" % b], writes=[k("BR")])
                    S.op("act", lambda e, w=w, b=b, T=T: e.copy(w("BI"), pbi[b][:, :T]), reads=["pbi%d" % b], writes=[k("BI")])
                    cs = lambda dj=dj, T=T: TC[:, dj, :T]
                    sn = lambda dj=dj, T=T: TS[:, dj, :T]
                    S.op("dve", lambda e, w=w, cs=cs: e.tensor_tensor(w("T1"), cs(), w("BR"), ALU.mult), reads=["TAB", k("BR")], writes=[k("T1")])
                    S.op("dve", lambda e, w=w, sn=sn: e.tensor_tensor(w("T2"), sn(), w("BI"), ALU.mult), reads=["TAB", k("BI")], writes=[k("T2")])
                    S.op("dve", lambda e, w=w: e.tensor_tensor(w("XR"), w("T1"), w("T2"), ALU.add), reads=[k("T1"), k("T2")], writes=[k("XR")])
                    S.op(G, lambda e, w=w, cs=cs: e.tensor_tensor(w("T3"), cs(), w("BI"), ALU.mult), reads=["TAB", k("BI")], writes=[k("T3")])
                    S.op(G, lambda e, w=w, sn=sn: e.tensor_tensor(w("T4"), sn(), w("BR"), ALU.mult), reads=["TAB", k("BR")], writes=[k("T4")])
                    S.op(G, lambda e, w=w: e.tensor_tensor(w("XI"), w("T3"), w("T4"), ALU.subtract), reads=[k("T3"), k("T4")], writes=[k("XI")])
                    S.op("dve", lambda e, w=w, dj=dj, j=j, T=T: e.tensor_tensor_scan(w("QR"), RHO[:, dj, :T], w("XR"), HP[:, 2 * j:2 * j + 1], ALU.mult, ALU.add),
                         reads=["TAB", k("XR"), "HP"], writes=[k("QR")])
                    S.op("dve", lambda e, w=w, dj=dj, j=j, T=T: e.tensor_tensor_scan(w("QI"), RHO[:, dj, :T], w("XI"), HP[:, 2 * j + 1:2 * j + 2], ALU.mult, ALU.add),
                         reads=["TAB", k("XI"), "HP"], writes=[k("QI")])
                    S.op("dve", lambda e, w=w, cs=cs: e.tensor_tensor(w("T1"), cs(), w("QR"), ALU.mult), reads=["TAB", k("QR")], writes=[k("T1")])
                    S.op("dve", lambda e, w=w, sn=sn: e.tensor_tensor(w("T2"), sn(), w("QI"), ALU.mult), reads=["TAB", k("QI")], writes=[k("T2")])
                    S.op("dve", lambda e, w=w: e.tensor_tensor(w("HR"), w("T1"), w("T2"), ALU.subtract), reads=[k("T1"), k("T2")], writes=[k("HR")])
                    S.op(G, lambda e, w=w, sn=sn: e.tensor_tensor(w("T3"), sn(), w("QR"), ALU.mult), reads=["TAB", k("QR")], writes=[k("T3")])
                    S.op(G, lambda e, w=w, cs=cs: e.tensor_tensor(w("T4"), cs(), w("QI"), ALU.mult), reads=["TAB", k("QI")], writes=[k("T4")])
                    S.op(G, lambda e, w=w: e.tensor_tensor(w("HI"), w("T3"), w("T4"), ALU.add), reads=[k("T3"), k("T4")], writes=[k("HI")])
                    S.op("act", lambda e, b=b, j=j, T=T: e.copy(HP[:, 2 * j:2 * j + 1], W["HR", b][:, T - 1:T]), reads=[k("HR")], writes=["HP"])
                    S.op("act", lambda e, b=b, j=j, T=T: e.copy(HP[:, 2 * j + 1:2 * j + 2], W["HI", b][:, T - 1:T]), reads=[k("HI")], writes=["HP"])
                    S.op("pe", lambda e, dj=dj, w=w, j=j, yb_=yb_, T=T: e.matmul(py[yb_][:, :T], CR[:, dj, :], w("HR"), start=(j == 0), stop=False),
                         reads=["CC", k("HR")], writes=["py%d" % yb_])
                    S.op("pe", lambda e, dj=dj, w=w, j=j, yb_=yb_, T=T: e.matmul(py[yb_][:, :T], CIN[:, dj, :], w("HI"), start=False, stop=(j == 3)),
                         reads=["CC", k("HI")], writes=["py%d" % yb_])
                if d == 0:
                    S.op("act", lambda e, yb_=yb_, T=T: e.copy(YO[yb_][:, :T], py[yb_][:, :T]), reads=["py%d" % yb_], writes=["YO%d" % yb_])
                else:
                    S.op("act", lambda e, yb_=yb_, T=T: e.copy(YO[yb_][:, :T][:, ::-1], py[yb_][:, :T]), reads=["py%d" % yb_], writes=["YO%d" % yb_])
                oi += 1
                S.dma("act", lambda e, d=d, yb_=yb_, n0=n0, T=T, ct=ct: e.dma_start(out=yout[d][ct * 128:(ct + 1) * 128, n0:n0 + T], in_=YO[yb_][:, :T]),
                      reads=["YO%d" % yb_], writes=["yout_%d" % oi])
        S.sync_all()
        S.emit()
        sub.close()


def emit_C(nc, S, sb_unused, ps_unused, io):
    FMQ, FMK, FMZ, VT = io["FMQ"], io["FMK"], io["FMZ"], io["VT"]
    OS = [io["OF"], io["OB"]]
    rst, tmask, tmask2, blk, hmask, ident = io["rst"], io["tmask"], io["tmask2"], io["blk"], io["hmask"], io["ident"]
    QSC = 32 ** -0.5
    for hh in range(2):
        X = "h%d_" % hh
        sub = ExitStack()
        sb, ps = _mk(nc, sub)
        cols = slice(hh * 256, hh * 256 + 256)
        TM2 = sb(X + "TM2", [64, 256])
        S.dma("sp", lambda e: e.dma_start(out=TM2[:], in_=tmask2), writes=["TM2"])
        RST = sb(X + "RST", [128, 512]); TM = sb(X + "TM", [64, 256]); BLK = sb(X + "BLK", [128, 256]); HM = sb(X + "HM", [128, 4]); ID = sb(X + "ID", [128, 128])
        WG = sb(X + "WG", [16, 128]); BG = sb(X + "BG", [128, 1]); NBG = sb(X + "NBG", [128, 1])
        Wb = {}
        for n in ("Q", "K", "LA", "B", "E", "D", "QE", "QS", "KD", "KS0", "KS1", "KS2", "KS3"):
            for i in range(2):
                Wb[n, i] = sb(X + "g_%s%d" % (n, i), [128, 512])
        Z = [sb(X + "Z%d" % i, [16, 512]) for i in range(2)]
        VV = [sb(X + "VV%d" % i, [64, 8, 256]) for i in range(2)]
        DEC = [sb(X + "DEC%d" % i, [128, 8]) for i in range(2)]
        KDT = [sb(X + "KDT%d" % i, [64, 128]) for i in range(2)]
        STt = [sb(X + "ST%d" % i, [64, 256]) for i in range(2)]
        OB = [sb(X + "OB%d" % i, [64, 256]) for i in range(3)]
        KVM = sb(X + "KVM", [128, 256])
        SS = [sb(X + "SS%d" % i, [128, 256]) for i in range(2)]
        pza = ps(X + "pza", [128, 512])
        pt0 = ps(X + "pt0", [64, 128])
        pt = [pt0, pt0]
        pkv = [ps(X + "pkv%d" % i, [128, 256]) for i in range(2)]
        psc = [ps(X + "psc%d" % i, [64, 256]) for i in range(2)]
        po = [ps(X + "po%d" % i, [64, 256]) for i in range(2)]
        for (t, src, k) in ((RST, rst, "RST"), (TM, tmask, "TM"), (BLK, blk, "BLK"), (HM, hmask, "HM"), (ID, ident, "ID")):
            S.dma("sp", lambda e, t=t, src=src: e.dma_start(out=t[:], in_=src), writes=[k])
        gc = 0
        oi = 0
        for d in range(2):
            S.dma("sp", lambda e, d=d, hh=hh: e.dma_start(out=WG[:], in_=io["wg"][hh][d]), writes=["WG"])
            S.dma("sp", lambda e, d=d, hh=hh: e.dma_start(out=BG[:], in_=io["bg"][hh][d]), writes=["BG"])
            S.op("dve", lambda e: e.tensor_scalar(NBG[:], BG[:], -1.0, None, ALU.mult), reads=["BG"], writes=["NBG"])
            S.op("dve", lambda e: e.memset(SS[0][:], 0.0), writes=["SS0"])
            scur = 0
            for bi, (t0, T) in enumerate(S5_CH):
                nchk = T // 64
                n0 = t0 if d == 0 else _mirror(t0, T)[0]
                w0 = (n0 - SEQC) // 64
                b = (d * 9 + bi) % 2
                w = lambda n, b=b, T=T: Wb[n, b][:, :T]
                k = lambda n, b=b: "g_%s%d" % (n, b)
                w3 = lambda n, b=b, T=T: Wb[n, b][:, :T].rearrange("p (c s) -> p c s", s=64)
                S.dma("sp", lambda e, d=d, b=b, n0=n0, T=T, hh=hh: e.dma_start(out=Wb["Q", b][:, :T], in_=FMQ[hh * 128:(hh + 1) * 128, n0:n0 + T]), writes=[k("Q")])
                S.dma("act", lambda e, d=d, b=b, n0=n0, T=T, hh=hh: e.dma_start(out=Wb["K", b][:, :T], in_=FMK[hh * 128:(hh + 1) * 128, n0:n0 + T]), writes=[k("K")])
                S.dma("sp", lambda e, d=d, b=b, n0=n0, T=T: e.dma_start(out=Z[b][:, :T], in_=FMZ[d * 16:(d + 1) * 16, n0:n0 + T]), writes=["Z%d" % b])
                if t0 < SEQC:
                    S.dma("act", lambda e, b=b, nchk=nchk, cols=cols: e.dma_start(
                        out=VV[b][:, :nchk, :], in_=VT[0:SEQC, cols].rearrange("(c s) f -> s c f", s=64)), writes=["VV%d" % b])
                else:
                    S.dma("act", lambda e, b=b, w0=w0, cols=cols: e.dma_start(
                        out=VV[b][:, :, :], in_=VT[SEQC:, cols].rearrange("(r w) f -> r w f", w=64)[:, w0:w0 + 8, :]), writes=["VV%d" % b])
                S.op("pe", lambda e, b=b, T=T: e.matmul(pza[:, :T], WG[:], Z[b][:, :T], start=True, stop=True), reads=["WG", "Z%d" % b], writes=["pza"])
                S.op("act", lambda e, w=w, T=T: e.activation(w("E"), pza[:, :T], AF.Exp, bias=NBG[:], scale=-1.0), reads=["pza", "NBG"], writes=[k("E")])
                S.op("act", lambda e, w=w: e.activation(w("E"), w("E"), AF.Ln, bias=1.0), reads=[k("E")], writes=[k("E")])
                S.op("dve", lambda e, w=w: e.tensor_scalar(w("LA"), w("E"), -1.0 / 16.0, None, ALU.mult), reads=[k("E")], writes=[k("LA")])
                S.op("dve", lambda e, w=w, T=T: e.tensor_tensor_scan(w("B"), RST[:, :T], w("LA"), 0.0, ALU.mult, ALU.add), reads=["RST", k("LA")], writes=[k("B")])
                iref, ilast = (32, 63) if d == 0 else (31, 0)
                if d == 1:
                    S.op("dve", lambda e, w3=w3, nchk=nchk: e.tensor_tensor(w3("D"), w3("B")[:, :, 63:64].to_broadcast([128, nchk, 64]), w3("B"), ALU.subtract),
                         reads=[k("B")], writes=[k("D")])
                    S.op("dve", lambda e, w=w: e.tensor_tensor(w("B"), w("D"), w("LA"), ALU.add), reads=[k("D"), k("LA")], writes=[k("B")])
                S.op("act", lambda e, b=b, w3=w3, nchk=nchk, ilast=ilast: e.activation(DEC[b][:, :nchk], w3("B")[:, :, ilast], AF.Exp), reads=[k("B")], writes=["DEC%d" % b])
                S.op("act", lambda e, w=w: e.activation(w("E"), w("B"), AF.Exp), reads=[k("B")], writes=[k("E")])
                S.op("dve", lambda e, w=w: e.scalar_tensor_tensor(w("QE"), w("Q"), QSC, w("E"), ALU.mult, ALU.mult), reads=[k("Q"), k("E")], writes=[k("QE")])
                S.op("dve", lambda e, w3=w3, nchk=nchk, iref=iref: e.tensor_tensor(w3("D"), w3("B"), w3("B")[:, :, iref:iref + 1].to_broadcast([128, nchk, 64]), ALU.subtract),
                     reads=[k("B")], writes=[k("D")])
                S.op("act", lambda e, w=w: e.activation(w("E"), w("D"), AF.Exp), reads=[k("D"), k("QE")], writes=[k("E")])
                S.op("dve", lambda e, w=w: e.scalar_tensor_tensor(w("QS"), w("Q"), QSC, w("E"), ALU.mult, ALU.mult), reads=[k("Q"), k("E")], writes=[k("QS")])
                S.op("act", lambda e, w=w: e.activation(w("E"), w("D"), AF.Exp, scale=-1.0), reads=[k("D"), k("QS")], writes=[k("E")])
                S.op("dve", lambda e, w=w: e.tensor_tensor(w("LA"), w("K"), w("E"), ALU.mult), reads=[k("K"), k("E"), k("B")], writes=[k("LA")])
                for h in range(4):
                    S.op("pool", lambda e, w=w, h=h: e.tensor_scalar(w("KS%d" % h), w("LA"), HM[:, h:h + 1], None, ALU.mult),
                         reads=[k("LA"), "HM"], writes=[k("KS%d" % h)])
                S.op("dve", lambda e, w3=w3, nchk=nchk, ilast=ilast: e.tensor_tensor(w3("D"), w3("B")[:, :, ilast:ilast + 1].to_broadcast([128, nchk, 64]), w3("B"), ALU.subtract),
                     reads=[k("B"), k("E"), k("LA")], writes=[k("D")])
                S.op("act", lambda e, w=w: e.activation(w("D"), w("D"), AF.Exp), reads=[k("D")], writes=[k("D")])
                S.op("dve", lambda e, w=w: e.tensor_tensor(w("KD"), w("K"), w("D"), ALU.mult), reads=[k("K"), k("D")], writes=[k("KD")])
                for c in (range(nchk) if d == 0 else range(nchk - 1, -1, -1)):
                    p2 = gc % 2
                    gc += 1
                    cs = slice(c * 64, (c + 1) * 64)
                    S.op("pe", lambda e, b=b, cs=cs, p2=p2: e.transpose(pt[p2][:], Wb["KD", b][:, cs], ID[:]), reads=[k("KD"), "ID"], writes=["pt"])
                    S.op("act", lambda e, p2=p2: e.copy(KDT[p2][:], pt[p2][:]), reads=["pt"], writes=["KDT%d" % p2])
                    S.op("pe", lambda e, b=b, c=c, p2=p2: e.matmul(pkv[p2][:], KDT[p2][:], VV[b][:, c, :], start=True, stop=True),
                         reads=["KDT%d" % p2, "VV%d" % b], writes=["pkv%d" % p2])
                    for h in range(4):
                        S.op("pe", lambda e, b=b, cs=cs, p2=p2, h=h: e.matmul(psc[p2][:, h * 64:(h + 1) * 64], Wb["KS%d" % h, b][:, cs], Wb["QS", b][:, cs],
                                                                            start=True, stop=True),
                             reads=[k("KS%d" % h), k("QS")], writes=["psc%d" % p2])
                    S.op("dve", lambda e, p2=p2, d=d: e.tensor_tensor(STt[p2][:], psc[p2][:], (TM if d == 0 else TM2)[:], ALU.mult), reads=["psc%d" % p2, "TM", "TM2"], writes=["ST%d" % p2])
                    for h in range(4):
                        hs = slice(h * 64, (h + 1) * 64)
                        S.op("pe", lambda e, b=b, c=c, p2=p2, hs=hs: e.matmul(po[p2][:, hs], STt[p2][:, hs], VV[b][:, c, hs], start=True, stop=False),
                             reads=["ST%d" % p2, "VV%d" % b], writes=["po%d" % p2])
                        S.op("pe", lambda e, b=b, cs=cs, p2=p2, hs=hs, scur=scur: e.matmul(po[p2][:, hs], Wb["QE", b][:, cs], SS[scur][:, hs], start=False, stop=True),
                             reads=[k("QE"), "SS%d" % scur], writes=["po%d" % p2])
                    ob = oi % 3
                    oi += 1
                    S.op("act", lambda e, ob=ob, p2=p2: e.copy(OB[ob][:], po[p2][:]), reads=["po%d" % p2], writes=["OB%d" % ob])
                    if t0 < SEQC:
                        S.dma("sp" if oi % 2 else "act", lambda e, d=d, ob=ob, c=c, cols=cols: e.dma_start(out=OS[d][c * 64:(c + 1) * 64, cols], in_=OB[ob][:]),
                              reads=["OB%d" % ob], writes=["o_%d" % oi])
                    else:
                        S.dma("sp" if oi % 2 else "act", lambda e, d=d, ob=ob, c=c, w0=w0, cols=cols: e.dma_start(
                            out=OS[d][SEQC:, cols].rearrange("(r w) f -> r w f", w=64)[:, w0 + c, :], in_=OB[ob][:]),
                              reads=["OB%d" % ob], writes=["o_%d" % oi])
                    S.op("dve", lambda e, p2=p2: e.tensor_tensor(KVM[:], pkv[p2][:], BLK[:], ALU.mult), reads=["pkv%d" % p2, "BLK"], writes=["KVM"])
                    S.op("dve", lambda e, b=b, c=c, scur=scur: e.scalar_tensor_tensor(SS[1 - scur][:], SS[scur][:], DEC[b][:, c:c + 1], KVM[:], ALU.mult, ALU.add),
                         reads=["SS%d" % scur, "DEC%d" % b, "KVM"], writes=["SS%d" % (1 - scur)])
                    scur = 1 - scur
        S.sync_all()
        S.emit()
        sub.close()


def emit_D(nc, S, sb, ps, io, blocks, last):
    xT = io["xT"]; modT = io["MODS"]; TLs = io["TL"]
    su = TLs[:, 0:256]; sv = TLs[:, 256:512]; gg = TLs[:, 512:1024]
    s5u = io["FMU"]; yf = io["YF"]; yb = io["YB"]; of_ = io["OF"]; ob_ = io["OB"]
    w_out, wsT, sgub, s5d, wglu, ng, g2, wq = io["w_out"], io["wsT"], io["sgub"], io["s5d"], io["wglu"], io["ng"], io["g2"], io["wq"]
    keysT, eu, ev, gfin, ones, ident, iota16 = io["keysT"], io["eu"], io["ev"], io["gfin"], io["ones"], io["ident"], io["iota16"]
    xo = io["xo"]
    nb = len(blocks)
    MOD = sb("MOD", [128, 96]); WOUT = sb("WOUT", [128, 8, D]); WS = sb("WS", [128, 512]); SGUB = sb("SGUB", [128, 4]); S5D = sb("S5D", [128, 2])
    WGLU = sb("WGLU", [128, 2, 512]); NG = sb("NG", [128, 512]); G2 = sb("G2", [128, 8]); RR_ = sb("RR_", [128, 16384]); KEYS = sb("KEYS", [128, 2048])
    GF = sb("GF", [128, 8]); ONES = sb("ONES", [128, 128]); ID = sb("ID", [128, 128]); IOTA = sb("IOTA", [128, 16]); IO128 = sb("IO128", [128, 128])
    A2 = sb("A2", [128, 16]); TA = sb("TA", [128, 16])
    U_ = sb("U_", [128, 256]); V_ = sb("V_", [128, 256]); GU = sb("GU", [128, 256]); GV = sb("GV", [128, 256]); SQ = sb("SQ", [128, 512])
    SS = sb("SS", [128, 8]); VN = sb("VN", [128, 256]); MIX = sb("MIX", [128, D])
    S5U = sb("S5U", [128, 2, 128]); YF = sb("YF", [128, 2, 128]); YB = sb("YB", [128, 2, 128]); GE = sb("GE", [128, 2, 128]); SG = sb("SG", [128, 256])
    OF = sb("OF", [128, 512]); OB = sb("OB", [128, 512]); GG = sb("GG", [128, 512]); SL = sb("SL", [128, 512])
    XT = sb("XT", [128, 8, 128]); X1 = sb("X1", [128, 8, 128]); XO = XT; HT = sb("HT", [128, 8, 128]); MIXT = HT; HTOK = sb("HTOK", [128, D])
    RSTD = sb("RSTD", [128, 128]); TMPB = sb("TMPB", [128, 128])
    SC = sb("SC", [128, 2048])
    M16 = sb("M16", [128, 256]); I16 = sb("I16", [128, 256], U32); IF16 = sb("IF16", [128, 256]); I1S = sb("I1S", [128, 128])
    CS = sb("CS", [128, 2048]); SC2 = CS; QT = CS[:].rearrange("p (q t) -> p q t", t=128); CS2 = sb("CS2", [128, 256])
    T16 = sb("T16", [128, 128]); P16 = sb("P16", [128, 128], U32); PF = sb("PF", [128, 128]); AI = sb("AI", [128, 128], I32)
    AFL = sb("AFL", [128, 128]); BFL = sb("BFL", [128, 128]); E1 = sb("E1", [128, 128]); E2 = sb("E2", [128, 128])
    EG = sb("EG", [128, 128]); GATE = sb("GATE", [128, 128]); IDXTI = sb("IDXTI", [128, 128], I32); GATET = sb("GATET", [128, 128])
    ACTT = sb("ACTT", [128, 128]); WT = sb("WT", [128, 128])
    NBUF = 8
    WQ = RR_[:].rearrange("p (k f) -> p k f", f=2048)
    UG = [RR_[:, i * 1024:(i + 1) * 1024] for i in range(NBUF)]; VG = UG
    HB = [sb("HB%d" % i, [128, D]) for i in range(2)]
    P0 = ps("P0", [128, 2048]); P1 = ps("P1", [128, 1024]); P2 = ps("P2", [128, 512]); P3 = ps("P3", [128, 512])

    def V(fn, r=(), w=()):
        S.op("dve", fn, reads=r, writes=w)

    def A(fn, r=(), w=()):
        S.op("act", fn, reads=r, writes=w)

    def PE(fn, r=(), w=()):
        S.op("pe", fn, reads=r, writes=w)

    def LD(q, t, src, key):
        S.dma(q, lambda e: e.dma_start(out=t, in_=src), writes=(key if isinstance(key, list) else [key]))

    LD("sp", MOD[:], modT, "MOD"); LD("sp", WS[:], wsT, "WS"); LD("sp", SGUB[:], sgub, "SGUB"); LD("sp", S5D[:], s5d, "S5D")
    LD("sp", WGLU[:], wglu.rearrange("(c p) f -> p c f", p=128), "WGLU"); LD("sp", NG[:], ng, "NG"); LD("sp", G2[:], g2, "G2")
    LD("sp", KEYS[:], keysT, "KEYS"); LD("sp", GF[:], gfin, "GF"); LD("sp", ONES[:], ones, "ONES"); LD("sp", ID[:], ident, "ID"); LD("sp", IOTA[:], iota16, "IOTA"); LD("sp", IO128[:], io["iota128"], "IO128")
    wo_v = w_out.rearrange("(k p) f -> p k f", p=128)
    wq_v = wq.rearrange("(k p) f -> p k f", p=128)
    for k in range(8):
        LD("act", WOUT[:, k, :], wo_v[:, k, :], "WOUT")
    MOD3 = MOD[:].rearrange("p (j c) -> p j c", c=2)
    V(lambda e: e.tensor_scalar(TA[:].rearrange("p (j c) -> p j c", c=2), MOD3[:, 32:40, :], 1.0, None, ALU.add), ["MOD"], ["TA"])
    V(lambda e: e.tensor_tensor(A2[:].rearrange("p (j c) -> p j c", c=2), TA[:].rearrange("p (j c) -> p j c", c=2),
                                G2[:].unsqueeze(2).to_broadcast([128, 8, 2]), ALU.mult), ["TA", "G2"], ["A2"])
    A23 = A2[:].rearrange("p (j c) -> p j c", c=2)

    def rs_from_ss(ss, n, scale):
        V(lambda e: e.tensor_scalar(ss, ss, scale, EPS, ALU.mult, ALU.add), ["SS"], ["SS"])
        A(lambda e: e.sqrt(ss, ss), ["SS"], ["SS"])
        V(lambda e: e.reciprocal(ss, ss), ["SS"], ["SS"])

    def top16(src, scratch, mout, iout, n):
        V(lambda e: e.max(mout[:, 0:8], src), ["TK"], ["TK"])
        V(lambda e: e.max_index(iout[:, 0:8], mout[:, 0:8], src), ["TK"], ["TK"])
        V(lambda e: e.match_replace(scratch, mout[:, 0:8], src, -1e30), ["TK"], ["TK"])
        V(lambda e: e.max(mout[:, 8:16], scratch), ["TK"], ["TK"])
        V(lambda e: e.max_index(iout[:, 8:16], mout[:, 8:16], scratch), ["TK"], ["TK"])

    xT_v = xT.rearrange("(k p) t -> p k t", p=128)
    xo_v = xo.rearrange("(k p) t -> p k t", p=128)
    s5u_v = s5u.rearrange("(c p) t -> p c t", p=128)
    yf_v = yf.rearrange("(c p) t -> p c t", p=128)
    yb_v = yb.rearrange("(c p) t -> p c t", p=128)
    for bi in range(nb):
        sq0, r0_, oc0, isctx = blocks[bi]
        col = 1 if isctx else 0
        tk = slice(sq0, sq0 + 128)
        tr = slice(r0_, r0_ + 128)
        to = slice(oc0, oc0 + 128)
        LD("sp", U_[:], su[tr, :], "U_"); LD("sp", V_[:], sv[tr, :], "V_"); LD("sp", GG[:], gg[tr, :], "GG")
        LD("act", S5U[:], s5u_v[:, :, tk], "S5U"); LD("act", YF[:], yf_v[:, :, tk], "YF"); LD("act", YB[:], yb_v[:, :, tk], "YB")
        LD("sp", OF[:], of_[tk, :], "OF"); LD("sp", OB[:], ob_[tk, :], "OB"); LD("act", XT[:], xT_v[:, :, tk], "XT")
        for k in range(8):
            LD("act" if k % 2 else "sp", WQ[:, k, :], wq_v[:, k, :], ["R%d" % (2 * k), "R%d" % (2 * k + 1)])
        A(lambda e: e.activation(GU[:], U_[:], AF.Gelu), ["U_"], ["GU"])
        A(lambda e: e.activation(GV[:], V_[:], AF.Gelu), ["V_"], ["GV"])
        V(lambda e: e.tensor_tensor(SQ[:, 0:256], GV[:], GV[:], ALU.mult), ["GV"], ["SQ"])
        V(lambda e: e.tensor_reduce(SS[:, 0:4], SQ[:, 0:256].rearrange("p (h d) -> p h d", d=64), AX.X, ALU.add), ["SQ"], ["SS"])
        rs_from_ss(SS[:, 0:4], 4, 1.0 / 64)
        V(lambda e: e.tensor_tensor(VN[:].rearrange("p (h d) -> p h d", d=64), GV[:].rearrange("p (h d) -> p h d", d=64),
                                    SS[:, 0:4].unsqueeze(2).to_broadcast([128, 4, 64]), ALU.mult), ["GV", "SS"], ["VN"])
        for h in range(4):
            PE(lambda e, h=h: e.matmul(P2[:, h * 64:(h + 1) * 64], WS[:, h * 128:(h + 1) * 128], VN[:, h * 64:(h + 1) * 64], start=True, stop=True),
               ["WS", "VN"], ["P2"])
        V(lambda e: e.tensor_tensor(MIX[:, 0:256].rearrange("p (h d) -> p h d", d=64), P2[:, 0:256].rearrange("p (h d) -> p h d", d=64),
                                    SGUB[:].unsqueeze(2).to_broadcast([128, 4, 64]), ALU.add), ["P2", "SGUB"], ["MIXa"])
        V(lambda e: e.tensor_tensor(MIX[:, 0:256], MIX[:, 0:256], GU[:], ALU.mult), ["GU"], ["MIXa"])
        V(lambda e: e.tensor_tensor(YF[:], YF[:], YB[:], ALU.add), ["YB"], ["YF"])
        for ct in range(2):
            V(lambda e, ct=ct: e.scalar_tensor_tensor(YF[:, ct, :], S5U[:, ct, :], S5D[:, ct:ct + 1], YF[:, ct, :], ALU.mult, ALU.add),
              ["S5U", "S5D"], ["YF"])
        A(lambda e: e.activation(GE[:], YF[:], AF.Gelu), ["YF"], ["GE"])
        for ct in range(2):
            PE(lambda e, ct=ct: e.matmul(P3[:, 0:512], GE[:, ct, :], WGLU[:, ct, :], start=(ct == 0), stop=(ct == 1)), ["GE", "WGLU"], ["P3"])
        A(lambda e: e.activation(SG[:], P3[:, 256:512], AF.Sigmoid), ["P3"], ["SG"])
        V(lambda e: e.tensor_tensor(MIX[:, 256:512], P3[:, 0:256], SG[:], ALU.mult), ["P3", "SG"], ["MIXb"])
        V(lambda e: e.tensor_tensor(OF[:], OF[:], OB[:], ALU.add), ["OB"], ["OF"])
        V(lambda e: e.tensor_tensor(SQ[:], OF[:], OF[:], ALU.mult), ["OF"], ["SQ"])
        V(lambda e: e.tensor_reduce(SS[:, 0:8], SQ[:].rearrange("p (h d) -> p h d", d=64), AX.X, ALU.add), ["SQ"], ["SS"])
        rs_from_ss(SS[:, 0:8], 8, 1.0 / 64)
        V(lambda e: e.tensor_tensor(OF[:].rearrange("p (h d) -> p h d", d=64), OF[:].rearrange("p (h d) -> p h d", d=64),
                                    SS[:, 0:8].unsqueeze(2).to_broadcast([128, 8, 64]), ALU.mult), ["SS"], ["OF"])
        V(lambda e: e.tensor_tensor(OF[:], OF[:], NG[:], ALU.mult), ["NG"], ["OF"])
        A(lambda e: e.activation(SL[:], GG[:], AF.Silu), ["GG"], ["SL"])
        V(lambda e: e.tensor_tensor(MIX[:, 512:1024], OF[:], SL[:], ALU.mult), ["OF", "SL"], ["MIXc"])
        for f in range(8):
            pp, pk = (P2, "P2") if f % 2 == 0 else (P3, "P3")
            PE(lambda e, f=f, pp=pp: e.transpose(pp[:, 0:128], MIX[:, f * 128:(f + 1) * 128], ID[:]), ["MIXa", "MIXb", "MIXc", "ID"], [pk])
            A(lambda e, f=f, pp=pp: e.copy(MIXT[:, f, :], pp[:, 0:128]), [pk], ["HT"])
        for ot in range(8):
            pp, pk = (P2, "P2") if ot % 2 == 0 else (P3, "P3")
            for k in range(8):
                PE(lambda e, ot=ot, k=k, pp=pp: e.matmul(pp[:, 0:128], WOUT[:, k, ot * 128:(ot + 1) * 128], MIXT[:, k, :], start=(k == 0), stop=(k == 7)),
                   ["WOUT", "HT"], [pk])
            V(lambda e, ot=ot, pp=pp, col=col: e.scalar_tensor_tensor(X1[:, ot, :], pp[:, 0:128], MOD3[:, 16 + ot, col:col + 1], XT[:, ot, :], ALU.mult, ALU.add),
              [pk, "MOD", "XT"], ["X1"])
        for k in range(8):
            A(lambda e, k=k: e.activation(TMPB[:], X1[:, k, :], AF.Square), ["X1"], ["TMPB"])
            PE(lambda e, k=k: e.matmul(P2[:, 0:128], ONES[:], TMPB[:], start=(k == 0), stop=(k == 7)), ["ONES", "TMPB"], ["P2"])
        V(lambda e: e.tensor_scalar(RSTD[:], P2[:, 0:128], 1.0 / D, EPS, ALU.mult, ALU.add), ["P2"], ["RSTD"])
        A(lambda e: e.sqrt(RSTD[:], RSTD[:]), ["RSTD"], ["RSTD"])
        V(lambda e: e.reciprocal(RSTD[:], RSTD[:]), ["RSTD"], ["RSTD"])
        for k in range(8):
            V(lambda e, k=k: e.tensor_tensor(TMPB[:], X1[:, k, :], RSTD[:], ALU.mult), ["X1", "RSTD"], ["TMPB"])
            A(lambda e, k=k, col=col: e.activation(HT[:, k, :], TMPB[:], AF.Identity, bias=MOD3[:, 24 + k, col:col + 1], scale=A23[:, k, col:col + 1]),
              ["TMPB", "MOD", "A2"], ["HT"])
        for k in range(8):
            pp, pk = (P2, "P2") if k % 2 == 0 else (P3, "P3")
            PE(lambda e, k=k, pp=pp: e.transpose(pp[:, 0:128], HT[:, k, :], ID[:]), ["HT", "ID"], [pk])
            A(lambda e, k=k, pp=pp: e.copy(HTOK[:, k * 128:(k + 1) * 128], pp[:, 0:128]), [pk], ["HTOK"])
        for qt in range(16):
            pp, pk = (P2, "P2") if qt % 2 == 0 else (P3, "P3")
            for k in range(8):
                PE(lambda e, qt=qt, k=k, pp=pp: e.matmul(pp[:, 0:128], WQ[:, k, qt * 128:(qt + 1) * 128], HT[:, k, :], start=(k == 0), stop=(k == 7)),
                   ["R%d" % (2 * k), "R%d" % (2 * k + 1), "HT"], [pk])
            if qt % 2 == 0:
                A(lambda e, qt=qt, pp=pp: e.copy(QT[:, qt, :], pp[:, 0:128]), [pk], ["TK"])
            else:
                V(lambda e, qt=qt, pp=pp: e.tensor_copy(QT[:, qt, :], pp[:, 0:128]), [pk], ["TK"])
        for qt in range(16):
            PE(lambda e, qt=qt: e.matmul(P0[:, qt * 128:(qt + 1) * 128], QT[:, qt, :], KEYS[:, qt * 128:(qt + 1) * 128], start=True, stop=True),
               ["TK", "KEYS"], ["P0a", "P0b"])
        for q4 in range(4):
            A(lambda e, q4=q4: e.copy(SC[:, q4 * 512:(q4 + 1) * 512], P0[:, q4 * 512:(q4 + 1) * 512]), ["P0a", "P0b"], ["TK"])
        for qt in range(16):
            top16(SC[:, qt * 128:(qt + 1) * 128], SC2[:, qt * 128:(qt + 1) * 128], M16[:, qt * 16:(qt + 1) * 16], I16[:, qt * 16:(qt + 1) * 16], 128)
        V(lambda e: e.tensor_copy(IF16[:], I16[:]), ["TK"], ["TK"])
        M4 = M16[:].rearrange("p (h q k) -> p h q k", q=2, k=16)
        IF4 = IF16[:].rearrange("p (h q k) -> p h q k", q=2, k=16)
        I1S3 = I1S[:].rearrange("p (h k) -> p h k", k=16)
        V(lambda e: e.tensor_scalar(I1S3, IF4[:, :, 0, :], 128.0, None, ALU.mult), ["TK"], ["TK"])
        CS4 = CS[:].rearrange("p (h a b) -> p h a b", a=16, b=16)
        V(lambda e: e.tensor_tensor(CS4, M4[:, :, 0, :].unsqueeze(3).to_broadcast([128, 8, 16, 16]),
                                    M4[:, :, 1, :].unsqueeze(2).to_broadcast([128, 8, 16, 16]), ALU.add), ["TK"], ["TK"])
        for h in range(8):
            top16(CS[:, h * 256:(h + 1) * 256], CS2[:], T16[:, h * 16:(h + 1) * 16], P16[:, h * 16:(h + 1) * 16], 256)
        V(lambda e: e.tensor_copy(PF[:], P16[:]), ["TK"], ["TK"])
        V(lambda e: e.tensor_scalar(AFL[:], PF[:], -7.5, 1.0 / 16, ALU.add, ALU.mult), ["TK"], ["TK"])
        V(lambda e: e.tensor_copy(AI[:], AFL[:]), ["TK"], ["TK"])
        V(lambda e: e.tensor_copy(AFL[:], AI[:]), ["TK"], ["TK"])
        V(lambda e: e.scalar_tensor_tensor(BFL[:], AFL[:], -16.0, PF[:], ALU.mult, ALU.add), ["TK"], ["TK"])
        EQ4 = CS[:].rearrange("p (h k a) -> p h k a", k=16, a=16)
        io4 = IOTA[:].unsqueeze(1).unsqueeze(1).to_broadcast([128, 8, 16, 16])
        for (sel, src, dst) in ((AFL, I1S3, E1), (BFL, IF4[:, :, 1, :], E2)):
            V(lambda e, sel=sel: e.tensor_tensor(EQ4, io4, sel[:].rearrange("p (h k) -> p h k", k=16).unsqueeze(3).to_broadcast([128, 8, 16, 16]), ALU.is_equal),
              ["TK", "IOTA"], ["TK"])
            V(lambda e, src=src: e.tensor_tensor(EQ4, EQ4, src.unsqueeze(2).to_broadcast([128, 8, 16, 16]), ALU.mult), ["TK"], ["TK"])
            V(lambda e, dst=dst: e.tensor_reduce(dst[:], CS[:].rearrange("p (m a) -> p m a", a=16), AX.X, ALU.add), ["TK"], ["TK"])
        V(lambda e: e.tensor_tensor(E1[:], E1[:], E2[:], ALU.add), ["TK"], ["TK"])
        T3 = T16[:].rearrange("p (h k) -> p h k", k=16)
        V(lambda e: e.tensor_tensor(EG[:].rearrange("p (h k) -> p h k", k=16), T3, T3[:, :, 0:1].to_broadcast([128, 8, 16]), ALU.subtract), ["TK"], ["TK"])
        A(lambda e: e.activation(EG[:], EG[:], AF.Exp), ["TK"], ["TK"])
        V(lambda e: e.tensor_reduce(SS[:, 0:8], EG[:].rearrange("p (h k) -> p h k", k=16), AX.X, ALU.add), ["TK"], ["SS"])
        V(lambda e: e.reciprocal(SS[:, 0:8], SS[:, 0:8]), ["SS"], ["SS"])
        V(lambda e: e.tensor_tensor(GATE[:].rearrange("p (h k) -> p h k", k=16), EG[:].rearrange("p (h k) -> p h k", k=16),
                                    SS[:, 0:8].unsqueeze(2).to_broadcast([128, 8, 16]), ALU.mult), ["TK", "SS"], ["GATE"])
        PE(lambda e: e.transpose(P2[:, 0:128], E1[:], ID[:]), ["TK", "ID"], ["P2"])
        V(lambda e: e.tensor_copy(IDXTI[:], P2[:, 0:128]), ["P2"], ["IDXTI"])
        PE(lambda e: e.transpose(P3[:, 0:128], GATE[:], ID[:]), ["GATE", "ID"], ["P3"])
        A(lambda e: e.copy(GATET[:], P3[:, 0:128]), ["P3"], ["GATET"])
        for t in range(128):
            g = t % NBUF
            b = t % 2
            S.dma("pool", lambda e, t=t, g=g: e.indirect_dma_start(out=UG[g], out_offset=None, in_=eu,
                                                                   in_offset=bass.IndirectOffsetOnAxis(ap=IDXTI[:, t:t + 1], axis=0)),
                  reads=["IDXTI"], writes=["R%d" % g])
            pk = "P0a" if b == 0 else "P0b"
            for hf in range(2):
                PE(lambda e, t=t, b=b, hf=hf: e.matmul(P0[:, b * 1024 + hf * 512:b * 1024 + (hf + 1) * 512], ID[:, t:t + 1].to_broadcast([128, 128]),
                                                       HTOK[:, hf * 512:(hf + 1) * 512], start=True, stop=True), ["ID", "HTOK"], [pk])
                A(lambda e, b=b, hf=hf: e.copy(HB[b][:, hf * 512:(hf + 1) * 512], P0[:, b * 1024 + hf * 512:b * 1024 + (hf + 1) * 512]), [pk], ["HB%d" % b])
            V(lambda e, t=t, b=b, g=g: e.scalar_tensor_tensor(UG[g], UG[g], 1.0, HB[b][:], ALU.mult, ALU.mult, accum_out=ACTT[:, t:t + 1]),
              ["HB%d" % b], ["R%d" % g, "ACTT"])
        A(lambda e: e.activation(WT[:], ACTT[:], AF.Gelu), ["ACTT"], ["WT"])
        V(lambda e: e.tensor_tensor(WT[:], WT[:], GATET[:], ALU.mult), ["GATET"], ["WT"])
        BIG = RR_[:, 8192:16384].rearrange("p (t m) -> p t m", m=128)
        for t in range(128):
            g = t % NBUF
            if t % 64 == 0:
                V(lambda e, t=t: e.tensor_tensor(BIG, IO128[:].unsqueeze(1).to_broadcast([128, 64, 128]),
                                                 IO128[:, t:t + 64].unsqueeze(2).to_broadcast([128, 64, 128]), ALU.is_equal), ["IO128"], ["BIG"])
                V(lambda e, t=t: e.tensor_tensor(BIG, BIG, WT[:, t:t + 64].unsqueeze(2).to_broadcast([128, 64, 128]), ALU.mult), ["WT"], ["BIG"])
            S.dma("pool", lambda e, t=t, g=g: e.indirect_dma_start(out=VG[g], out_offset=None, in_=ev,
                                                                   in_offset=bass.IndirectOffsetOnAxis(ap=IDXTI[:, t:t + 1], axis=0)),
                  reads=["IDXTI"], writes=["R%d" % g])
            for hf in range(2):
                PE(lambda e, t=t, g=g, hf=hf: e.matmul(P1[:, hf * 512:(hf + 1) * 512], BIG[:, t % 64, :], VG[g][:, hf * 512:(hf + 1) * 512],
                                                       start=(t == 0), stop=(t == 127)), ["R%d" % g, "BIG"], ["P1"])
        for hf in range(2):
            A(lambda e, hf=hf: e.copy(HB[0][:, hf * 512:(hf + 1) * 512], P1[:, hf * 512:(hf + 1) * 512]), ["P1"], ["HB0"])
        for ot in range(8):
            pp, pk = (P2, "P2") if ot % 2 == 0 else (P3, "P3")
            PE(lambda e, ot=ot, pp=pp: e.transpose(pp[:, 0:128], HB[0][:, ot * 128:(ot + 1) * 128], ID[:]), ["HB0", "ID"], [pk])
            V(lambda e, ot=ot, col=col, pp=pp: e.scalar_tensor_tensor(XO[:, ot, :], pp[:, 0:128], MOD3[:, 40 + ot, col:col + 1], X1[:, ot, :],
                                                                      ALU.mult, ALU.add), [pk, "MOD", "X1"], ["XT"])
        if last:
            for k in range(8):
                A(lambda e, k=k: e.activation(TMPB[:], XO[:, k, :], AF.Square), ["XT"], ["TMPB"])
                PE(lambda e, k=k: e.matmul(P2[:, 0:128], ONES[:], TMPB[:], start=(k == 0), stop=(k == 7)), ["ONES", "TMPB"], ["P2"])
            V(lambda e: e.tensor_scalar(RSTD[:], P2[:, 0:128], 1.0 / D, EPS, ALU.mult, ALU.add), ["P2"], ["RSTD"])
            A(lambda e: e.sqrt(RSTD[:], RSTD[:]), ["RSTD"], ["RSTD"])
            V(lambda e: e.reciprocal(RSTD[:], RSTD[:]), ["RSTD"], ["RSTD"])
            for k in range(8):
                V(lambda e, k=k: e.scalar_tensor_tensor(XO[:, k, :], XO[:, k, :], GF[:, k:k + 1], RSTD[:], ALU.mult, ALU.mult), ["RSTD", "GF"], ["XT"])
        S.dma("sp", lambda e, to=to: e.dma_start(out=xo_v[:, :, to], in_=XO[:]), reads=["XT"], writes=["xo_%d" % bi])


def build_layer(last, dbg=False):
    nc = bass.Bass("TRN2", target_bir_lowering=False)
    io = {}

    def din(name, shape, dt=F32):
        io[name] = nc.dram_tensor(name, shape, dt, kind="ExternalInput").ap()

    def scr(name, shape):
        io[name] = nc.dram_tensor(name, shape, F32, kind=("ExternalOutput" if dbg else "Internal")).ap()
    nctx = 0 if last else 128
    ntok = 2048 + nctx
    din("xT", [D, SEQT]); din("cT", [128, 16]); din("w_mod", [D, NMOD * D]); din("b_mod", [128, 48]); din("g1", [128, 8]); din("w_in", [D, INW])
    din("ones", [128, 128]); din("ident", [128, 128]); din("tau", [128, 512]); din("iota16", [128, 16]); din("iota128", [128, 128])
    din("prm", [2, 128, 24]); din("bre", [2, 128, 128]); din("bim", [2, 128, 128]); din("cre", [2, 128, 128]); din("cim", [2, 128, 128])
    din("wg", [2, 2, 16, 128]); din("bg", [2, 2, 128, 1])
    din("rst", [128, 512]); din("tmask", [64, 256]); din("tmask2", [64, 256]); din("blk", [128, 256]); din("hmask", [128, 4])
    din("w_out", [D, D]); din("wsT", [128, 512]); din("sgub", [128, 4]); din("s5d", [128, 2]); din("wglu", [256, 512]); din("ng", [128, 512])
    din("g2", [128, 8]); din("wq", [D, 2048]); din("keysT", [128, 2048]); din("eu", [NEXP, D]); din("ev", [NEXP, D]); din("gfin", [128, 8])
    scr("FMU", [256, SEQT]); scr("FMQ", [256, SEQT]); scr("FMK", [256, SEQT]); scr("FMZ", [32, SEQT]); scr("VT", [SEQT, 512]); scr("TL", [OWN, 1024])
    scr("MODS", [128, 96]); scr("YF", [256, SEQT]); scr("YB", [256, SEQT]); scr("OF", [SEQT, 512]); scr("OB", [SEQT, 512]); scr("GS", [2, 16])
    io["xo"] = nc.dram_tensor("xo", [D, ntok], F32, kind="ExternalOutput").ap()
    with ExitStack() as top:
        gate = top.enter_context(nc.semaphore("gate"))
        ph = [0]

        def phase(fn):
            with ExitStack() as st:
                S = Sched(nc, top, gate, 16 * ph[0])
                sb, ps = _mk(nc, st)
                fn(S, sb, ps)
                S.drain_all("sp")
                GS = io["GS"]
                S.prog["sp"].append(("i", lambda e: e.dma_start(out=GS[0:1, :], in_=GS[1:2, :]), "gate", 16))
                S.emit()
            ph[0] += 1
        phase(lambda S, sb, ps: emit_A(nc, S, sb, ps, io))
        phase(lambda S, sb, ps: emit_B(nc, S, sb, ps, io))
        phase(lambda S, sb, ps: emit_C(nc, S, sb, ps, io))
        if last:
            blocks = [(SEQC + i * 128, 128 + i * 128, i * 128, False) for i in range(16)]
        else:
            blocks = [(0, 0, 0, True)] + [(SEQC + i * 128, 128 + i * 128, 128 + i * 128, False) for i in range(16)]
        phase(lambda S, sb, ps: emit_D(nc, S, sb, ps, io, blocks, last))
    return nc


def _lay_vec(v, n):
    return np.ascontiguousarray(np.asarray(v, np.float32).reshape(n, 128).T)


_CONST = {}


def _consts():
    if not _CONST:
        rst = np.ones((128, 512), np.float32)
        rst[:, ::64] = 0
        tm = (np.arange(64)[None, :] >= np.arange(64)[:, None]).astype(np.float32)
        _CONST.update(
            ones=np.ones((128, 128), np.float32), ident=np.eye(128, dtype=np.float32),
            iota16=np.ascontiguousarray(np.broadcast_to(np.arange(16, dtype=np.float32), (128, 16))),
            iota128=np.ascontiguousarray(np.broadcast_to(np.arange(128, dtype=np.float32), (128, 128))),
            tau=np.ascontiguousarray(np.broadcast_to(np.arange(1, 513, dtype=np.float32), (128, 512))),
            rst=rst, tmask=np.ascontiguousarray(np.tile(tm, (1, 4))), tmask2=np.ascontiguousarray(np.tile(tm.T, (1, 4))),
            blk=np.kron(np.eye(4, dtype=np.float32), np.ones((32, 64), np.float32)),
            hmask=np.kron(np.eye(4, dtype=np.float32), np.ones((32, 1), np.float32)))
    return _CONST


def _pb_params(P, l, gh, swap):
    def stt(a):
        if swap:
            a = a[::-1]
        g = a[:, gh * 8:gh * 8 + 8]
        g = g.reshape((2, 4, 2, 64) + a.shape[3:])
        g = np.moveaxis(g, (2, 3), (0, 1))
        return np.ascontiguousarray(g.reshape((128, 2, 4) + a.shape[3:]))
    lre = stt(P["s5_lambda_re"][l]); lim = stt(P["s5_lambda_im"][l])
    ls = stt(np.broadcast_to(P["s5_log_step"][l][:, :, None], (2, 16, 64)))
    prm = np.ascontiguousarray(np.stack([lre, lim, ls], -1).reshape(128, 24).astype(np.float32))
    return dict(prm=prm, bre=stt(P["s5_b_re"][l]).reshape(128, 128), bim=stt(P["s5_b_im"][l]).reshape(128, 128),
                cre=stt(np.swapaxes(P["s5_c_re"][l], 2, 3)).reshape(128, 128), cim=stt(np.swapaxes(P["s5_c_im"][l], 2, 3)).reshape(128, 128))


def _layer_weights(P, l, swap):
    C = _consts()
    sk = P["peer_sub_keys"][l]
    keysT = np.zeros((128, 16, 128), np.float32)
    for h in range(8):
        for p in range(2):
            keysT[:, h * 2 + p, :] = sk[p, h].T
    sw = P["sgu_w"][l]
    sbb = P["sgu_b"][l]
    wgate = P["gla_w_gate"][l]
    bgate = P["gla_b_gate"][l]
    if swap:
        sw = sw[:, ::-1, ::-1]
        sbb = sbb[:, ::-1]
        wgate = wgate[::-1]
        bgate = bgate[::-1]
    pb = [_pb_params(P, l, ct, swap) for ct in range(2)]
    w_in = P["w_in"][l]
    if swap:
        w_in = np.ascontiguousarray(np.concatenate([w_in[:, :2304], w_in[:, 2320:2336], w_in[:, 2304:2320]], 1))
    W = dict(w_mod=P["w_mod"][l], b_mod=_lay_vec(P["b_mod"][l], 48), g1=_lay_vec(P["norm1_g"][l], 8), w_in=w_in,
             w_out=P["w_out"][l], wsT=np.ascontiguousarray(np.transpose(sw, (2, 0, 1)).reshape(128, 512)),
             sgub=np.ascontiguousarray(sbb.T), s5d=_lay_vec(P["s5_d"][l], 2), wglu=P["s5_w_glu"][l],
             ng=np.ascontiguousarray(np.broadcast_to(P["gla_norm_g"][l], (128, 512))), g2=_lay_vec(P["norm2_g"][l], 8),
             wq=P["peer_w_query"][l], keysT=keysT.reshape(128, 2048), eu=P["peer_expert_u"][l], ev=P["peer_expert_v"][l],
             gfin=_lay_vec(P["final_norm_g"], 8),
             wg=np.ascontiguousarray(np.stack([[wgate[d][:, hh * 128:hh * 128 + 128] for d in range(2)] for hh in range(2)])),
             bg=np.ascontiguousarray(np.stack([[bgate[d][hh * 128:hh * 128 + 128][:, None] for d in range(2)] for hh in range(2)])))
    for k in ("prm", "bre", "bim", "cre", "cim"):
        W[k] = np.ascontiguousarray(np.stack([pb[0][k], pb[1][k]]))
    W.update(C)
    return W


LAYER_W = ["w_mod", "b_mod", "g1", "w_in", "prm", "bre", "bim", "cre", "cim", "wg", "bg", "w_out", "wsT", "sgub", "s5d", "wglu", "ng", "g2",
           "wq", "keysT", "eu", "ev"]
LAYER_W_SHAPES = dict(w_mod=[D, NMOD * D], b_mod=[128, 48], g1=[128, 8], w_in=[D, INW], prm=[2, 128, 24], bre=[2, 128, 128], bim=[2, 128, 128],
                      cre=[2, 128, 128], cim=[2, 128, 128], wg=[2, 2, 16, 128], bg=[2, 2, 128, 1], w_out=[D, D], wsT=[128, 512], sgub=[128, 4],
                      s5d=[128, 2], wglu=[256, 512], ng=[128, 512], g2=[128, 8], wq=[D, 2048], keysT=[128, 2048], eu=[NEXP, D], ev=[NEXP, D])


def build_full():
    nc = bass.Bass("TRN2", target_bir_lowering=False)
    io = {}

    def din(name, shape, dt=F32):
        io[name] = nc.dram_tensor(name, shape, dt, kind="ExternalInput").ap()

    def scr(name, shape):
        io[name] = nc.dram_tensor(name, shape, F32, kind="Internal").ap()
    din("xT", [D, SEQT]); din("cT", [128, 16]); din("gfin", [128, 8])
    din("ones", [128, 128]); din("ident", [128, 128]); din("tau", [128, 512]); din("iota16", [128, 16]); din("iota128", [128, 128])
    din("rst", [128, 512]); din("tmask", [64, 256]); din("tmask2", [64, 256]); din("blk", [128, 256]); din("hmask", [128, 4])
    for l in range(2):
        for n in LAYER_W:
            din("%s_%d" % (n, l), LAYER_W_SHAPES[n])
    scr("FMU", [256, SEQT]); scr("FMQ", [256, SEQT]); scr("FMK", [256, SEQT]); scr("FMZ", [32, SEQT]); scr("VT", [SEQT, 512]); scr("TL", [SEQT, 1024])
    scr("MODS", [128, 96]); scr("YF", [256, SEQT]); scr("YB", [256, SEQT]); scr("OF", [SEQT, 512]); scr("OB", [SEQT, 512]); scr("GS", [2, 16])
    scr("X1S", [D, SEQT])
    io["xo"] = nc.dram_tensor("xo", [D, 2048], F32, kind="ExternalOutput").ap()
    with ExitStack() as top:
        gate = top.enter_context(nc.semaphore("gate"))
        ph = [0]

        def phase(fn, nds=None):
            with ExitStack() as st:
                S = Sched(nc, top, gate, 16 * ph[0], nds=nds)
                sb, ps = _mk(nc, st)
                fn(S, sb, ps)
                S.drain_all("sp")
                GS = io["GS"]
                S.prog["sp"].append(("i", lambda e: e.dma_start(out=GS[0:1, :], in_=GS[1:2, :]), "gate", 16))
                S.emit()
            ph[0] += 1
        for l in range(2):
            last = l == 1
            iol = dict(io)
            for n in LAYER_W:
                iol[n] = io["%s_%d" % (n, l)]
            if last:
                iol["xT"] = io["X1S"]
                blocks = [(SEQC + i * 128, 128 + i * 128, i * 128, False) for i in range(16)]
            else:
                iol["xo"] = io["X1S"]
                blocks = [(0, 0, 0, True), (128, 128, 128, True)] + [(SEQC + i * 128, SEQC + i * 128, SEQC + i * 128, False) for i in range(32)]
            n2 = dict(sp=2, act=2, pool=2)
            phase(lambda S, sb, ps: emit_A(nc, S, sb, ps, iol, tl_all=not last), nds=n2)
            phase(lambda S, sb, ps: emit_B(nc, S, sb, ps, iol), nds=n2)
            phase(lambda S, sb, ps: emit_C(nc, S, sb, ps, iol), nds=n2)
            phase(lambda S, sb, ps: emit_D(nc, S, sb, ps, iol, blocks, last), nds=dict(sp=2, act=2, pool=7))
    return nc


_PROG = {}


def _prog(name, fn):
    if name not in _PROG:
        _PROG[name] = fn()
    return _PROG[name]


def kernel(**inputs):
    P = {k: np.ascontiguousarray(np.asarray(v)) for k, v in inputs.items()}
    cores = list(range(8))
    C = _consts()
    ncF = _prog("F", build_full)
    WL = {(l, sw): _layer_weights(P, l, sw) for l in range(2) for sw in (False, True)}
    in_maps = []
    for core in cores:
        b, half = divmod(core, 2)
        xc, xl = P["ctx"][b], P["x"][b]
        seq = np.concatenate([xc, xl], 0) if half == 0 else np.concatenate([xc[::-1], xl[::-1]], 0)
        cT = np.stack([_lay_vec(P["c"][b], 8), _lay_vec(P["c_ctx"], 8)], -1).reshape(128, 16)
        m = dict(xT=np.ascontiguousarray(seq.T), cT=np.ascontiguousarray(cT), gfin=_lay_vec(P["final_norm_g"], 8))
        m.update(C)
        for l in range(2):
            W = WL[l, half == 1]
            for n in LAYER_W:
                m["%s_%d" % (n, l)] = W[n]
        in_maps.append(m)
    rF = run_bass_kernel_spmd(ncF, in_maps, core_ids=cores).results
    out = np.zeros_like(P["x"])
    for core in cores:
        b, half = divmod(core, 2)
        xo = rF[core]["xo"].T
        if half == 0:
            out[b, 0:2048] = xo
        else:
            out[b, 2048:4096] = xo[::-1]
    return out.astype(np.float32)
```

```python
from contextlib import ExitStack
import math
import numpy as np
import concourse.bass as bass
import concourse.mybir as mybir
from concourse.bass_utils import run_bass_kernel_spmd

F32 = mybir.dt.float32
I32 = mybir.dt.int32
U32 = mybir.dt.uint32
ALU = mybir.AluOpType
AF = mybir.ActivationFunctionType
AX = mybir.AxisListType


class Sched:
    NDS = 4

    def __init__(self, nc, stack, gate=None, gate_val=0, nds=None):
        self.nc = nc
        self.ndsq = dict(sp=self.NDS, act=self.NDS, pool=self.NDS)
        if nds:
            self.ndsq.update(nds)
        self.gate = gate
        self.gate_val = gate_val
        self.eng = {"pe": nc.tensor, "dve": nc.vector, "act": nc.scalar,
                    "pool": nc.gpsimd, "sp": nc.sync}
        self.sem = {}
        self.cnt = {}
        _UID[0] += 1
        u = "q%d" % _UID[0]
        for e in self.eng:
            self.sem[e] = stack.enter_context(nc.semaphore(u + "s_" + e))
            self.cnt[e] = 0
        self.dq = {}
        for q in ("sp", "act", "pool"):
            sems = [stack.enter_context(nc.semaphore(u + "d_%s%d" % (q, i))) for i in range(self.ndsq[q])]
            self.dq[q] = {"sems": sems, "n": 0}
            for i, s in enumerate(sems):
                self.sem["d_%s%d" % (q, i)] = s
        self.seen = {e: {} for e in self.eng}
        self.prog = {e: [] for e in self.eng}
        self.lastw = {}
        self.reads = {}
        self.ninst = 0
        if gate is not None:
            self.sem["gate"] = gate
        if gate is not None and gate_val > 0:
            for e in self.eng:
                self.prog[e].append(("w", "gate", gate_val))

    def _need(self, need, ev):
        if ev is None:
            return
        s, v = ev
        if need.get(s, 0) < v:
            need[s] = v

    def _waits(self, e, reads, writes):
        need = {}
        for k in reads:
            self._need(need, self.lastw.get(k))
        for k in writes:
            self._need(need, self.lastw.get(k))
            for ev in self.reads.get(k, ()):
                self._need(need, ev)
        eng = self.eng[e]
        seen = self.seen[e]
        for s, v in need.items():
            if seen.get(s, 0) >= v:
                continue
            self.prog[e].append(("w", s, v))
            seen[s] = v
            self.ninst += 1

    def _commit(self, ev, reads, writes):
        for k in writes:
            self.lastw[k] = ev
            self.reads[k] = []
        for k in reads:
            if k in writes:
                continue
            self.reads.setdefault(k, []).append(ev)
            if len(self.reads[k]) > 24:
                d = {}
                for s, v in self.reads[k]:
                    if d.get(s, 0) < v:
                        d[s] = v
                self.reads[k] = list(d.items())

    def op(self, e, fn, reads=(), writes=()):
        reads = tuple(reads)
        writes = tuple(writes)
        self._waits(e, reads, writes)
        self.cnt[e] += 1
        self.prog[e].append(("i", fn, e, 1))
        self.ninst += 1
        self._commit((e, self.cnt[e]), reads, writes)

    def dma(self, q, fn, reads=(), writes=()):
        reads = tuple(reads)
        writes = tuple(writes)
        st = self.dq[q]
        i = st["n"]
        slot = i % self.ndsq[q]
        sname = "d_%s%d" % (q, slot)
        rnd = i // self.ndsq[q]
        eng = self.eng[q]
        if rnd > 0 and self.seen[q].get(sname, 0) < 16 * rnd:
            self.prog[q].append(("w", sname, 16 * rnd))
            self.seen[q][sname] = 16 * rnd
            self.ninst += 1
        self._waits(q, reads, writes)
        self.prog[q].append(("i", fn, sname, 16))
        st["n"] = i + 1
        self.ninst += 1
        self._commit((sname, 16 * (rnd + 1)), reads, writes)

    def finish(self, keys, e="sp"):
        self._waits(e, tuple(keys), ())

    def drain_all(self, e="sp"):
        need = {}
        for en, c in self.cnt.items():
            if c:
                need[en] = c
        for q, stq in self.dq.items():
            n = stq["n"]
            nq = self.ndsq[q]
            for slot in range(nq):
                uses = (n - slot + nq - 1) // nq if n > slot else 0
                if uses:
                    need["d_%s%d" % (q, slot)] = 16 * uses
        eng = self.eng[e]
        for s, v in need.items():
            if self.seen[e].get(s, 0) >= v:
                continue
            self.prog[e].append(("w", s, v))
            self.seen[e][s] = v

    def emit(self):
        nc = self.nc
        with nc.Block() as block:
            def mk(e):
                def body(engine):
                    for it in self.prog[e]:
                        if it[0] == "w":
                            engine.wait_ge(self.sem[it[1]], it[2])
                        else:
                            it[1](engine).then_inc(self.sem[it[2]], it[3])
                return body
            block.sync(mk("sp"))
            block.scalar(mk("act"))
            block.vector(mk("dve"))
            block.gpsimd(mk("pool"))
            block.tensor(mk("pe"))
        self.prog = {e: [] for e in self.eng}

    def sync_all(self):
        for e in self.eng:
            self.drain_all(e)
        self.lastw = {}
        self.reads = {}

D = 1024
NMOD = 6
INW = 2336
INW_T = 19
EPS = 1e-6
NTOK = 2176


_UID = [0]


def _mk(nc, st):
    _UID[0] += 1
    u = "u%d_" % _UID[0]

    def sb(name, shape, dt=F32):
        return st.enter_context(nc.sbuf_tensor(u + name, shape, dt))

    def ps(name, shape, dt=F32):
        return st.enter_context(nc.psum_tensor(u + name, shape, dt))
    return sb, ps


def _groups(n, g=512):
    out = []
    t = 0
    while t < n:
        out.append((t, min(g, n - t)))
        t += g
    return out


def build_PA(ntok=NTOK, nctx=128):
    nc = bass.Bass("TRN2", target_bir_lowering=False)
    xT = nc.dram_tensor("xT", [D, ntok], F32, kind="ExternalInput").ap()
    cT = nc.dram_tensor("cT", [128, 16], F32, kind="ExternalInput").ap()
    w_mod = nc.dram_tensor("w_mod", [D, NMOD * D], F32, kind="ExternalInput").ap()
    b_mod = nc.dram_tensor("b_mod", [128, 48], F32, kind="ExternalInput").ap()
    g1 = nc.dram_tensor("g1", [128, 8], F32, kind="ExternalInput").ap()
    w_in = nc.dram_tensor("w_in", [D, INW], F32, kind="ExternalInput").ap()
    ones = nc.dram_tensor("ones", [128, 128], F32, kind="ExternalInput").ap()
    colsT = nc.dram_tensor("colsT", [INW_T * 128, ntok], F32, kind="ExternalOutput").ap()
    modT = nc.dram_tensor("modT", [128, 96], F32, kind="ExternalOutput").ap()
    with ExitStack() as st:
        S = Sched(nc, st)
        sb, ps = _mk(nc, st)
        CT = sb("CT", [128, 16]); SC = sb("SC", [128, 16]); BM = sb("BM", [128, 48]); G1 = sb("G1", [128, 8])
        ONES = sb("ONES", [128, 128]); MOD = sb("MOD", [128, 96])
        A1 = sb("A1", [128, 16]); TMPA = sb("TMPA", [128, 16])
        WM = [sb("WM%d" % i, [128, 8, 512]) for i in range(2)]
        WIN = sb("WIN", [128, 8, INW])
        XT = [sb("XT%d" % i, [128, 8, 512]) for i in range(2)]
        XSQ = sb("XSQ", [128, 512]); RSTD = sb("RSTD", [128, 512]); TMP = sb("TMP", [128, 512])
        HT = [sb("HT%d" % i, [128, 8, 512]) for i in range(2)]
        OUTB = [sb("OUTB%d" % i, [128, 512]) for i in range(4)]
        pmod = ps("pmod", [128, 96]); pss = ps("pss", [128, 512])
        pout = [ps("pout%d" % i, [128, 512]) for i in range(3)]

        S.dma("sp", lambda e: e.dma_start(out=CT[:], in_=cT), writes=["CT"])
        S.dma("sp", lambda e: e.dma_start(out=BM[:], in_=b_mod), writes=["BM"])
        S.dma("sp", lambda e: e.dma_start(out=G1[:], in_=g1), writes=["G1"])
        S.dma("sp", lambda e: e.dma_start(out=ONES[:], in_=ones), writes=["ONES"])
        S.op("act", lambda e: e.activation(SC[:], CT[:], AF.Silu), reads=["CT"], writes=["SC"])
        SC3 = SC[:].rearrange("p (k c) -> p k c", c=2)
        wm_v = w_mod.rearrange("(k p) f -> p k f", p=128)
        for jg in range(12):
            b = jg % 2
            S.dma("act" if jg % 2 else "sp",
                  lambda e, jg=jg, b=b: e.dma_start(out=WM[b][:], in_=wm_v[:, :, jg * 512:(jg + 1) * 512]),
                  writes=["WM%d" % b])
            for j8 in range(4):
                j = jg * 4 + j8
                for k in range(8):
                    S.op("pe", lambda e, j=j, j8=j8, k=k, b=b: e.matmul(
                        pmod[:, 2 * j:2 * j + 2], WM[b][:, k, j8 * 128:(j8 + 1) * 128], SC3[:, k, :],
                        start=(k == 0), stop=(k == 7)), reads=["WM%d" % b, "SC"], writes=["pmod"])
        S.op("dve", lambda e: e.tensor_tensor(MOD[:].rearrange("p (j c) -> p j c", c=2),
                                              pmod[:].rearrange("p (j c) -> p j c", c=2),
                                              BM[:].unsqueeze(2).to_broadcast([128, 48, 2]), ALU.add),
             reads=["pmod", "BM"], writes=["MOD"])
        S.dma("sp", lambda e: e.dma_start(out=modT, in_=MOD[:]), reads=["MOD"], writes=["modT"])
        MOD3 = MOD[:].rearrange("p (j c) -> p j c", c=2)
        S.op("dve", lambda e: e.tensor_scalar(TMPA[:].rearrange("p (j c) -> p j c", c=2), MOD3[:, 8:16, :], 1.0, None, ALU.add),
             reads=["MOD"], writes=["TMPA"])
        S.op("dve", lambda e: e.tensor_tensor(A1[:].rearrange("p (j c) -> p j c", c=2),
                                              TMPA[:].rearrange("p (j c) -> p j c", c=2),
                                              G1[:].unsqueeze(2).to_broadcast([128, 8, 2]), ALU.mult),
             reads=["TMPA", "G1"], writes=["A1"])
        A13 = A1[:].rearrange("p (j c) -> p j c", c=2)
        win_v = w_in.rearrange("(k p) f -> p k f", p=128)
        for k in range(8):
            S.dma("pool", lambda e, k=k: e.dma_start(out=WIN[:, k, :], in_=win_v[:, k, :]), writes=["WIN%d" % k])
        xT_v = xT.rearrange("(k p) t -> p k t", p=128)
        grp = [(0, nctx, 1)] if nctx else []
        grp += [(nctx + t0, tn, 0) for (t0, tn) in _groups(ntok - nctx)]
        oi = 0
        for gi, (t0, tn, col) in enumerate(grp):
            b = gi % 2
            xk, hk = "XT%d" % b, "HT%d" % b
            S.dma("sp", lambda e, b=b, t0=t0, tn=tn: e.dma_start(out=XT[b][:, :, :tn], in_=xT_v[:, :, t0:t0 + tn]), writes=[xk])
            for k in range(8):
                S.op("act", lambda e, b=b, k=k, tn=tn: e.activation(XSQ[:, :tn], XT[b][:, k, :tn], AF.Square), reads=[xk], writes=["XSQ"])
                S.op("pe", lambda e, k=k, tn=tn: e.matmul(pss[:, :tn], ONES[:], XSQ[:, :tn], start=(k == 0), stop=(k == 7)),
                     reads=["ONES", "XSQ"], writes=["pss"])
            S.op("dve", lambda e, tn=tn: e.tensor_scalar(RSTD[:, :tn], pss[:, :tn], 1.0 / D, EPS, ALU.mult, ALU.add), reads=["pss"], writes=["RSTD"])
            S.op("act", lambda e, tn=tn: e.sqrt(RSTD[:, :tn], RSTD[:, :tn]), reads=["RSTD"], writes=["RSTD"])
            S.op("dve", lambda e, tn=tn: e.reciprocal(RSTD[:, :tn], RSTD[:, :tn]), reads=["RSTD"], writes=["RSTD"])
            for k in range(8):
                S.op("dve", lambda e, b=b, k=k, tn=tn: e.tensor_tensor(TMP[:, :tn], XT[b][:, k, :tn], RSTD[:, :tn], ALU.mult),
                     reads=[xk, "RSTD"], writes=["TMP"])
                S.op("act", lambda e, b=b, k=k, tn=tn, col=col: e.activation(
                    HT[b][:, k, :tn], TMP[:, :tn], AF.Identity, bias=MOD3[:, k, col:col + 1], scale=A13[:, k, col:col + 1]),
                    reads=["TMP", "MOD", "A1"], writes=[hk])
            for ot in range(INW_T):
                m = 128 if ot < INW_T - 1 else INW - 128 * (INW_T - 1)
                pb = oi % 3
                ob = oi % 4
                oi += 1
                for k in range(8):
                    S.op("pe", lambda e, b=b, k=k, tn=tn, ot=ot, m=m, pb=pb: e.matmul(
                        pout[pb][:m, :tn], WIN[:, k, ot * 128:ot * 128 + m], HT[b][:, k, :tn], start=(k == 0), stop=(k == 7)),
                        reads=["WIN%d" % k, hk], writes=["pout%d" % pb])
                if m < 128:
                    S.op("dve", lambda e, ob=ob: e.memset(OUTB[ob][:], 0.0), writes=["OUTB%d" % ob])
                eng = "act" if oi % 2 else "dve"
                if eng == "act":
                    S.op("act", lambda e, ob=ob, pb=pb, m=m, tn=tn: e.copy(OUTB[ob][:m, :tn], pout[pb][:m, :tn]),
                         reads=["pout%d" % pb], writes=["OUTB%d" % ob])
                else:
                    S.op("dve", lambda e, ob=ob, pb=pb, m=m, tn=tn: e.tensor_copy(OUTB[ob][:m, :tn], pout[pb][:m, :tn]),
                         reads=["pout%d" % pb], writes=["OUTB%d" % ob])
                S.dma("sp" if oi % 2 else "act", lambda e, ob=ob, ot=ot, t0=t0, tn=tn: e.dma_start(
                    out=colsT[ot * 128:(ot + 1) * 128, t0:t0 + tn], in_=OUTB[ob][:, :tn]),
                    reads=["OUTB%d" % ob], writes=["colsT_%d" % oi])
        S.drain_all("sp")
        S.emit()
    return nc


SEQT = 4352
S5_CH = [(0, 256)] + [(256 + 512 * i, 512) for i in range(8)]


def build_PB():
    nc = bass.Bass("TRN2", target_bir_lowering=False)
    uin = [nc.dram_tensor(n, [128, SEQT], F32, kind="ExternalInput").ap() for n in ("uf", "ub")]
    prm = nc.dram_tensor("prm", [128, 24], F32, kind="ExternalInput").ap()
    bre = nc.dram_tensor("bre", [128, 128], F32, kind="ExternalInput").ap()
    bim = nc.dram_tensor("bim", [128, 128], F32, kind="ExternalInput").ap()
    cre = nc.dram_tensor("cre", [128, 128], F32, kind="ExternalInput").ap()
    cim = nc.dram_tensor("cim", [128, 128], F32, kind="ExternalInput").ap()
    tau = nc.dram_tensor("tau", [128, 512], F32, kind="ExternalInput").ap()
    ident = nc.dram_tensor("ident", [128, 128], F32, kind="ExternalInput").ap()
    yout = [nc.dram_tensor(n, [128, SEQT], F32, kind="ExternalOutput").ap() for n in ("yf", "yb")]
    TWO_PI = 2.0 * math.pi
    with ExitStack() as st:
        S = Sched(nc, st)
        sb, ps = _mk(nc, st)
        PRM = sb("PRM", [128, 24]); BRE = sb("BRE", [128, 128]); BIM = sb("BIM", [128, 128])
        CRE = sb("CRE", [128, 128]); CIM = sb("CIM", [128, 128]); TAU = sb("TAU", [128, 512]); ID = sb("ID", [128, 128])
        names = ["DT", "LR", "MAG", "TH", "R", "R2", "RF", "FR", "SIN", "COS", "ARE", "AIM", "DEN", "AM1", "FRE", "FIM", "T0", "T1"]
        P = {n: sb("p_" + n, [128, 8]) for n in names}
        RI = sb("p_RI", [128, 8], I32)
        BBR = sb("BBR", [128, 128]); BBI = sb("BBI", [128, 128]); TB = sb("TB", [128, 128])
        PAD = sb("PAD", [128, 128])
        WBR = sb("WBR", [128, 8, 128]); WBI = sb("WBI", [128, 8, 128]); CR = sb("CR", [128, 8, 128]); CIN = sb("CIN", [128, 8, 128])
        TC = sb("TC", [128, 8, 512]); TS = sb("TS", [128, 8, 512]); RHO = sb("RHO", [128, 8, 512])
        RR = sb("RR", [128, 512]); RRF = sb("RRF", [128, 512]); RRI = sb("RRI", [128, 512], I32)
        UC = [sb("UC%d" % i, [128, 512]) for i in range(2)]
        W = {}
        for n in ("BR", "BI", "T1", "T2", "T3", "T4", "XR", "XI", "QR", "QI", "HR", "HI"):
            for i in range(2):
                W[n, i] = sb("w_%s%d" % (n, i), [128, 512])
        HP = sb("HP", [128, 8])
        YO = [sb("YO%d" % i, [128, 512]) for i in range(2)]
        pbr = [ps("pbr%d" % i, [128, 512]) for i in range(2)]
        pbi = [ps("pbi%d" % i, [128, 512]) for i in range(2)]
        py = [ps("py%d" % i, [128, 512]) for i in range(2)]
        ptr = ps("ptr", [128, 128])

        for (t, src, k) in ((PRM, prm, "PRM"), (BRE, bre, "BRE"), (BIM, bim, "BIM"), (CRE, cre, "CRE"), (CIM, cim, "CIM"),
                            (TAU, tau, "TAU"), (ID, ident, "ID")):
            S.dma("sp", lambda e, t=t, src=src: e.dma_start(out=t[:], in_=src), writes=[k])
        PR3 = PRM[:].rearrange("p (a c) -> p a c", c=3)
        K = ["PP"]

        def V(fn, reads=(), writes=()):
            S.op("dve", fn, reads=list(reads) + K, writes=list(writes) + K)

        def A(fn, reads=(), writes=()):
            S.op("act", fn, reads=list(reads) + K, writes=list(writes) + K)

        A(lambda e: e.activation(P["DT"][:], PR3[:, :, 2], AF.Exp), reads=["PRM"])
        V(lambda e: e.tensor_scalar(P["LR"][:], PR3[:, :, 0], -1e-4, None, ALU.min), reads=["PRM"])
        V(lambda e: e.tensor_tensor(P["T0"][:], P["LR"][:], P["DT"][:], ALU.mult))
        A(lambda e: e.activation(P["MAG"][:], P["T0"][:], AF.Exp))
        V(lambda e: e.tensor_tensor(P["TH"][:], PR3[:, :, 1], P["DT"][:], ALU.mult), reads=["PRM"])
        V(lambda e: e.tensor_scalar(P["R"][:], P["TH"][:], 1.0 / TWO_PI, None, ALU.mult))
        V(lambda e: e.tensor_scalar(P["R2"][:], P["R"][:], 0.25, None, ALU.add))
        for (src, dst) in (("R", "SIN"), ("R2", "COS")):
            V(lambda e, src=src: e.tensor_copy(RI[:], P[src][:]))
            V(lambda e: e.tensor_copy(P["RF"][:], RI[:]))
            V(lambda e, src=src: e.tensor_tensor(P["FR"][:], P[src][:], P["RF"][:], ALU.subtract))
            A(lambda e, dst=dst: e.activation(P[dst][:], P["FR"][:], AF.Sin, scale=TWO_PI))
        V(lambda e: e.tensor_tensor(P["ARE"][:], P["MAG"][:], P["COS"][:], ALU.mult))
        V(lambda e: e.tensor_tensor(P["AIM"][:], P["MAG"][:], P["SIN"][:], ALU.mult))
        V(lambda e: e.tensor_tensor(P["T0"][:], P["LR"][:], P["LR"][:], ALU.mult))
        V(lambda e: e.tensor_tensor(P["T1"][:], PR3[:, :, 1], PR3[:, :, 1], ALU.mult), reads=["PRM"])
        V(lambda e: e.tensor_tensor(P["DEN"][:], P["T0"][:], P["T1"][:], ALU.add))
        V(lambda e: e.reciprocal(P["DEN"][:], P["DEN"][:]))
        V(lambda e: e.tensor_scalar(P["AM1"][:], P["ARE"][:], -1.0, None, ALU.add))
        V(lambda e: e.tensor_tensor(P["T0"][:], P["AM1"][:], P["LR"][:], ALU.mult))
        V(lambda e: e.tensor_tensor(P["T1"][:], P["AIM"][:], PR3[:, :, 1], ALU.mult), reads=["PRM"])
        V(lambda e: e.tensor_tensor(P["T0"][:], P["T0"][:], P["T1"][:], ALU.add))
        V(lambda e: e.tensor_tensor(P["FRE"][:], P["T0"][:], P["DEN"][:], ALU.mult))
        V(lambda e: e.tensor_tensor(P["T0"][:], P["AIM"][:], P["LR"][:], ALU.mult))
        V(lambda e: e.tensor_tensor(P["T1"][:], P["AM1"][:], PR3[:, :, 1], ALU.mult), reads=["PRM"])
        V(lambda e: e.tensor_tensor(P["T0"][:], P["T0"][:], P["T1"][:], ALU.subtract))
        V(lambda e: e.tensor_tensor(P["FIM"][:], P["T0"][:], P["DEN"][:], ALU.mult))

        def v3(t):
            return t[:].rearrange("p (a h) -> p a h", h=16)

        def bc(n):
            return P[n][:].unsqueeze(2).to_broadcast([128, 8, 16])
        V(lambda e: e.tensor_tensor(v3(BBR), v3(BRE), bc("FRE"), ALU.mult), reads=["BRE"])
        V(lambda e: e.tensor_tensor(v3(TB), v3(BIM), bc("FIM"), ALU.mult), reads=["BIM"])
        V(lambda e: e.tensor_tensor(BBR[:], BBR[:], TB[:], ALU.subtract))
        V(lambda e: e.tensor_tensor(v3(BBI), v3(BIM), bc("FRE"), ALU.mult), reads=["BIM"])
        V(lambda e: e.tensor_tensor(v3(TB), v3(BRE), bc("FIM"), ALU.mult), reads=["BRE"])
        V(lambda e: e.tensor_tensor(BBI[:], BBI[:], TB[:], ALU.add))
        V(lambda e: e.tensor_scalar(CIM[:], CIM[:], -1.0, None, ALU.mult), reads=["CIM"], writes=["CIM"])
        V(lambda e: e.memset(CR[:], 0.0)); V(lambda e: e.memset(CIN[:], 0.0))
        for dj in range(8):
            j = dj % 4
            for (src, dst) in ((BBR, WBR), (BBI, WBI)):
                V(lambda e: e.memset(PAD[:], 0.0), writes=["PAD"])
                V(lambda e, src=src, dj=dj, j=j: e.tensor_copy(PAD[0:64, 32 * j:32 * j + 16], src[0:64, dj * 16:dj * 16 + 16]), writes=["PAD"])
                V(lambda e, src=src, dj=dj, j=j: e.tensor_copy(PAD[64:128, 32 * j + 16:32 * j + 32], src[64:128, dj * 16:dj * 16 + 16]), writes=["PAD"])
                S.op("pe", lambda e: e.transpose(ptr[:], PAD[:], ID[:]), reads=["PAD", "ID"], writes=["ptr"])
                S.op("act", lambda e, dst=dst, dj=dj: e.copy(dst[:, dj, :], ptr[:]), reads=["ptr"], writes=["WB"])
            for (src, dst) in ((CRE, CR), (CIM, CIN)):
                V(lambda e, src=src, dst=dst, dj=dj, j=j: e.tensor_copy(dst[0:64, dj, 32 * j:32 * j + 16], src[0:64, dj * 16:dj * 16 + 16]), reads=["CRE", "CIM"], writes=["CC"])
                V(lambda e, src=src, dst=dst, dj=dj, j=j: e.tensor_copy(dst[64:128, dj, 32 * j + 16:32 * j + 32], src[64:128, dj * 16:dj * 16 + 16]), reads=["CRE", "CIM"], writes=["CC"])
            for (off, dst) in ((0.0, TS), (0.25, TC)):
                V(lambda e, dj=dj, off=off: e.tensor_scalar(RR[:], TAU[:], P["R"][:, dj:dj + 1], off, ALU.mult, ALU.add), reads=["TAU"], writes=["RR"])
                V(lambda e: e.tensor_copy(RRI[:], RR[:]), reads=["RR"], writes=["RRI"])
                V(lambda e: e.tensor_copy(RRF[:], RRI[:]), reads=["RRI"], writes=["RRF"])
                V(lambda e: e.tensor_tensor(RRF[:], RR[:], RRF[:], ALU.subtract), reads=["RR"], writes=["RRF"])
                S.op("act", lambda e, dst=dst, dj=dj: e.activation(dst[:, dj, :], RRF[:], AF.Sin, scale=TWO_PI), reads=["RRF"], writes=["TAB"])
            V(lambda e, dj=dj: e.tensor_copy(RHO[:, dj, :], P["MAG"][:, dj:dj + 1].to_broadcast([128, 512])), writes=["TAB"])

        G = "pool"
        oi = 0
        for d in range(2):
            V(lambda e: e.memset(HP[:], 0.0), writes=["HP"])
            for ci, (t0, T) in enumerate(S5_CH):
                ub_ = (d * 9 + ci) % 2
                uk = "UC%d" % ub_
                S.dma("sp", lambda e, d=d, t0=t0, T=T, ub_=ub_: e.dma_start(out=UC[ub_][:, :T], in_=uin[d][:, t0:t0 + T]), writes=[uk])
                yb_ = (d * 9 + ci) % 2
                for j in range(4):
                    dj = d * 4 + j
                    b = j % 2
                    w = lambda n, b=b, T=T: W[n, b][:, :T]
                    k = lambda n, b=b: "w_%s%d" % (n, b)
                    S.op("pe", lambda e, dj=dj, b=b, T=T, ub_=ub_: e.matmul(pbr[b][:, :T], WBR[:, dj, :], UC[ub_][:, :T], start=True, stop=True),
                         reads=["WB", uk], writes=["pbr%d" % b])
                    S.op("pe", lambda e, dj=dj, b=b, T=T, ub_=ub_: e.matmul(pbi[b][:, :T], WBI[:, dj, :], UC[ub_][:, :T], start=True, stop=True),
                         reads=["WB", uk], writes=["pbi%d" % b])
                    S.op("act", lambda e, w=w, b=b, T=T: e.copy(w("BR"), pbr[b][:, :T]), reads=["pbr%d" % b], writes=[k("BR")])
                    S.op("act", lambda e, w=w, b=b, T=T: e.copy(w("BI"), pbi[b][:, :T]), reads=["pbi%d" % b], writes=[k("BI")])
                    cs = lambda dj=dj, T=T: TC[:, dj, :T]
                    sn = lambda dj=dj, T=T: TS[:, dj, :T]
                    S.op("dve", lambda e, w=w, cs=cs: e.tensor_tensor(w("T1"), cs(), w("BR"), ALU.mult), reads=["TAB", k("BR")], writes=[k("T1")])
                    S.op("dve", lambda e, w=w, sn=sn: e.tensor_tensor(w("T2"), sn(), w("BI"), ALU.mult), reads=["TAB", k("BI")], writes=[k("T2")])
                    S.op("dve", lambda e, w=w: e.tensor_tensor(w("XR"), w("T1"), w("T2"), ALU.add), reads=[k("T1"), k("T2")], writes=[k("XR")])
                    S.op(G, lambda e, w=w, cs=cs: e.tensor_tensor(w("T3"), cs(), w("BI"), ALU.mult), reads=["TAB", k("BI")], writes=[k("T3")])
                    S.op(G, lambda e, w=w, sn=sn: e.tensor_tensor(w("T4"), sn(), w("BR"), ALU.mult), reads=["TAB", k("BR")], writes=[k("T4")])
                    S.op(G, lambda e, w=w: e.tensor_tensor(w("XI"), w("T3"), w("T4"), ALU.subtract), reads=[k("T3"), k("T4")], writes=[k("XI")])
                    S.op("dve", lambda e, w=w, dj=dj, j=j, T=T: e.tensor_tensor_scan(w("QR"), RHO[:, dj, :T], w("XR"), HP[:, 2 * j:2 * j + 1], ALU.mult, ALU.add),
                         reads=["TAB", k("XR"), "HP"], writes=[k("QR")])
                    S.op("dve", lambda e, w=w, dj=dj, j=j, T=T: e.tensor_tensor_scan(w("QI"), RHO[:, dj, :T], w("XI"), HP[:, 2 * j + 1:2 * j + 2], ALU.mult, ALU.add),
                         reads=["TAB", k("XI"), "HP"], writes=[k("QI")])
                    S.op("dve", lambda e, w=w, cs=cs: e.tensor_tensor(w("T1"), cs(), w("QR"), ALU.mult), reads=["TAB", k("QR")], writes=[k("T1")])
                    S.op("dve", lambda e, w=w, sn=sn: e.tensor_tensor(w("T2"), sn(), w("QI"), ALU.mult), reads=["TAB", k("QI")], writes=[k("T2")])
                    S.op("dve", lambda e, w=w: e.tensor_tensor(w("HR"), w("T1"), w("T2"), ALU.subtract), reads=[k("T1"), k("T2")], writes=[k("HR")])
                    S.op(G, lambda e, w=w, sn=sn: e.tensor_tensor(w("T3"), sn(), w("QR"), ALU.mult), reads=["TAB", k("QR")], writes=[k("T3")])
                    S.op(G, lambda e, w=w, cs=cs: e.tensor_tensor(w("T4"), cs(), w("QI"), ALU.mult), reads=["TAB", k("QI")], writes=[k("T4")])
                    S.op(G, lambda e, w=w: e.tensor_tensor(w("HI"), w("T3"), w("T4"), ALU.add), reads=[k("T3"), k("T4")], writes=[k("HI")])
                    S.op("act", lambda e, b=b, j=j, T=T: e.copy(HP[:, 2 * j:2 * j + 1], W["HR", b][:, T - 1:T]), reads=[k("HR")], writes=["HP"])
                    S.op("act", lambda e, b=b, j=j, T=T: e.copy(HP[:, 2 * j + 1:2 * j + 2], W["HI", b][:, T - 1:T]), reads=[k("HI")], writes=["HP"])
                    S.op("pe", lambda e, dj=dj, w=w, j=j, yb_=yb_, T=T: e.matmul(py[yb_][:, :T], CR[:, dj, :], w("HR"), start=(j == 0), stop=False),
                         reads=["CC", k("HR")], writes=["py%d" % yb_])
                    S.op("pe", lambda e, dj=dj, w=w, j=j, yb_=yb_, T=T: e.matmul(py[yb_][:, :T], CIN[:, dj, :], w("HI"), start=False, stop=(j == 3)),
                         reads=["CC", k("HI")], writes=["py%d" % yb_])
                S.op("act", lambda e, yb_=yb_, T=T: e.copy(YO[yb_][:, :T], py[yb_][:, :T]), reads=["py%d" % yb_], writes=["YO%d" % yb_])
                oi += 1
                S.dma("act", lambda e, d=d, yb_=yb_, t0=t0, T=T: e.dma_start(out=yout[d][:, t0:t0 + T], in_=YO[yb_][:, :T]),
                      reads=["YO%d" % yb_], writes=["yout_%d" % oi])
        S.drain_all("sp")
        S.emit()
    return nc


NCH = 68


def build_PC():
    nc = bass.Bass("TRN2", target_bir_lowering=False)
    I = {}
    for d in range(2):
        I["qT", d] = nc.dram_tensor("qT%d" % d, [128, SEQT], F32, kind="ExternalInput").ap()
        I["kT", d] = nc.dram_tensor("kT%d" % d, [128, SEQT], F32, kind="ExternalInput").ap()
        I["v", d] = nc.dram_tensor("v%d" % d, [SEQT, 256], F32, kind="ExternalInput").ap()
        I["zT", d] = nc.dram_tensor("zT%d" % d, [16, SEQT], F32, kind="ExternalInput").ap()
        I["wg", d] = nc.dram_tensor("wg%d" % d, [16, 128], F32, kind="ExternalInput").ap()
        I["bg", d] = nc.dram_tensor("bg%d" % d, [128, 1], F32, kind="ExternalInput").ap()
        I["o", d] = nc.dram_tensor("o%d" % d, [SEQT, 256], F32, kind="ExternalOutput").ap()
    rst = nc.dram_tensor("rst", [128, 512], F32, kind="ExternalInput").ap()
    tmask = nc.dram_tensor("tmask", [64, 256], F32, kind="ExternalInput").ap()
    blk = nc.dram_tensor("blk", [128, 256], F32, kind="ExternalInput").ap()
    hmask = nc.dram_tensor("hmask", [128, 4], F32, kind="ExternalInput").ap()
    ident = nc.dram_tensor("ident", [128, 128], F32, kind="ExternalInput").ap()
    QSC = 32 ** -0.5
    with ExitStack() as st:
        S = Sched(nc, st)
        sb, ps = _mk(nc, st)
        RST = sb("RST", [128, 512]); TM = sb("TM", [64, 256]); BLK = sb("BLK", [128, 256]); HM = sb("HM", [128, 4]); ID = sb("ID", [128, 128])
        WG = sb("WG", [16, 128]); BG = sb("BG", [128, 1]); NBG = sb("NBG", [128, 1])
        Wb = {}
        for n in ("Q", "K", "LA", "B", "E", "D", "QE", "QS", "KD", "KS0", "KS1", "KS2", "KS3"):
            for i in range(2):
                Wb[n, i] = sb("g_%s%d" % (n, i), [128, 512])
        Z = [sb("Z%d" % i, [16, 512]) for i in range(2)]
        VV = [sb("VV%d" % i, [64, 8, 256]) for i in range(2)]
        DEC = [sb("DEC%d" % i, [128, 8]) for i in range(2)]
        KDT = [sb("KDT%d" % i, [64, 128]) for i in range(2)]
        STt = [sb("ST%d" % i, [64, 256]) for i in range(2)]
        OB = [sb("OB%d" % i, [64, 256]) for i in range(3)]
        KVM = sb("KVM", [128, 256])
        SS = [sb("SS%d" % i, [128, 256]) for i in range(2)]
        pza = ps("pza", [128, 512])
        pt0 = ps("pt0", [64, 128])
        pt = [pt0, pt0]
        pkv = [ps("pkv%d" % i, [128, 256]) for i in range(2)]
        psc = [ps("psc%d" % i, [64, 256]) for i in range(2)]
        po = [ps("po%d" % i, [64, 256]) for i in range(2)]
        for (t, src, k) in ((RST, rst, "RST"), (TM, tmask, "TM"), (BLK, blk, "BLK"), (HM, hmask, "HM"), (ID, ident, "ID")):
            S.dma("sp", lambda e, t=t, src=src: e.dma_start(out=t[:], in_=src), writes=[k])
        gc = 0
        oi = 0
        for d in range(2):
            S.dma("sp", lambda e, d=d: e.dma_start(out=WG[:], in_=I["wg", d]), writes=["WG"])
            S.dma("sp", lambda e, d=d: e.dma_start(out=BG[:], in_=I["bg", d]), writes=["BG"])
            S.op("dve", lambda e: e.tensor_scalar(NBG[:], BG[:], -1.0, None, ALU.mult), reads=["BG"], writes=["NBG"])
            S.op("dve", lambda e: e.memset(SS[0][:], 0.0), writes=["SS0"])
            scur = 0
            for bi, (t0, T) in enumerate(S5_CH):
                nchk = T // 64
                b = (d * 9 + bi) % 2
                w = lambda n, b=b, T=T: Wb[n, b][:, :T]
                k = lambda n, b=b: "g_%s%d" % (n, b)
                w3 = lambda n, b=b, T=T: Wb[n, b][:, :T].rearrange("p (c s) -> p c s", s=64)
                S.dma("sp", lambda e, d=d, b=b, t0=t0, T=T: e.dma_start(out=Wb["Q", b][:, :T], in_=I["qT", d][:, t0:t0 + T]), writes=[k("Q")])
                S.dma("act", lambda e, d=d, b=b, t0=t0, T=T: e.dma_start(out=Wb["K", b][:, :T], in_=I["kT", d][:, t0:t0 + T]), writes=[k("K")])
                S.dma("sp", lambda e, d=d, b=b, t0=t0, T=T: e.dma_start(out=Z[b][:, :T], in_=I["zT", d][:, t0:t0 + T]), writes=["Z%d" % b])
                S.dma("act", lambda e, d=d, b=b, t0=t0, T=T, nchk=nchk: e.dma_start(
                    out=VV[b][:, :nchk, :], in_=I["v", d][t0:t0 + T, :].rearrange("(c s) f -> s c f", s=64)), writes=["VV%d" % b])
                S.op("pe", lambda e, b=b, T=T: e.matmul(pza[:, :T], WG[:], Z[b][:, :T], start=True, stop=True), reads=["WG", "Z%d" % b], writes=["pza"])
                S.op("act", lambda e, w=w, T=T: e.activation(w("E"), pza[:, :T], AF.Exp, bias=NBG[:], scale=-1.0), reads=["pza", "NBG"], writes=[k("E")])
                S.op("act", lambda e, w=w: e.activation(w("E"), w("E"), AF.Ln, bias=1.0), reads=[k("E")], writes=[k("E")])
                S.op("dve", lambda e, w=w: e.tensor_scalar(w("LA"), w("E"), -1.0 / 16.0, None, ALU.mult), reads=[k("E")], writes=[k("LA")])
                S.op("dve", lambda e, w=w, T=T: e.tensor_tensor_scan(w("B"), RST[:, :T], w("LA"), 0.0, ALU.mult, ALU.add), reads=["RST", k("LA")], writes=[k("B")])
                S.op("act", lambda e, b=b, w3=w3, nchk=nchk: e.activation(DEC[b][:, :nchk], w3("B")[:, :, 63], AF.Exp), reads=[k("B")], writes=["DEC%d" % b])
                S.op("act", lambda e, w=w: e.activation(w("E"), w("B"), AF.Exp), reads=[k("B")], writes=[k("E")])
                S.op("dve", lambda e, w=w: e.scalar_tensor_tensor(w("QE"), w("Q"), QSC, w("E"), ALU.mult, ALU.mult), reads=[k("Q"), k("E")], writes=[k("QE")])
                S.op("dve", lambda e, w3=w3, nchk=nchk: e.tensor_tensor(w3("D"), w3("B"), w3("B")[:, :, 32:33].to_broadcast([128, nchk, 64]), ALU.subtract),
                     reads=[k("B")], writes=[k("D")])
                S.op("act", lambda e, w=w: e.activation(w("E"), w("D"), AF.Exp), reads=[k("D"), k("QE")], writes=[k("E")])
                S.op("dve", lambda e, w=w: e.scalar_tensor_tensor(w("QS"), w("Q"), QSC, w("E"), ALU.mult, ALU.mult), reads=[k("Q"), k("E")], writes=[k("QS")])
                S.op("act", lambda e, w=w: e.activation(w("E"), w("D"), AF.Exp, scale=-1.0), reads=[k("D"), k("QS")], writes=[k("E")])
                S.op("dve", lambda e, w=w: e.tensor_tensor(w("LA"), w("K"), w("E"), ALU.mult), reads=[k("K"), k("E"), k("B")], writes=[k("LA")])
                for h in range(4):
                    S.op("pool", lambda e, w=w, h=h: e.tensor_scalar(w("KS%d" % h), w("LA"), HM[:, h:h + 1], None, ALU.mult),
                         reads=[k("LA"), "HM"], writes=[k("KS%d" % h)])
                S.op("dve", lambda e, w3=w3, nchk=nchk: e.tensor_tensor(w3("D"), w3("B")[:, :, 63:64].to_broadcast([128, nchk, 64]), w3("B"), ALU.subtract),
                     reads=[k("B"), k("E"), k("LA")], writes=[k("D")])
                S.op("act", lambda e, w=w: e.activation(w("D"), w("D"), AF.Exp), reads=[k("D")], writes=[k("D")])
                S.op("dve", lambda e, w=w: e.tensor_tensor(w("KD"), w("K"), w("D"), ALU.mult), reads=[k("K"), k("D")], writes=[k("KD")])
                for c in range(nchk):
                    p2 = gc % 2
                    gc += 1
                    cs = slice(c * 64, (c + 1) * 64)
                    S.op("pe", lambda e, b=b, cs=cs, p2=p2: e.transpose(pt[p2][:], Wb["KD", b][:, cs], ID[:]), reads=[k("KD"), "ID"], writes=["pt"])
                    S.op("act", lambda e, p2=p2: e.copy(KDT[p2][:], pt[p2][:]), reads=["pt"], writes=["KDT%d" % p2])
                    S.op("pe", lambda e, b=b, c=c, p2=p2: e.matmul(pkv[p2][:], KDT[p2][:], VV[b][:, c, :], start=True, stop=True),
                         reads=["KDT%d" % p2, "VV%d" % b], writes=["pkv%d" % p2])
                    for h in range(4):
                        S.op("pe", lambda e, b=b, cs=cs, p2=p2, h=h: e.matmul(psc[p2][:, h * 64:(h + 1) * 64], Wb["KS%d" % h, b][:, cs], Wb["QS", b][:, cs],
                                                                            start=True, stop=True),
                             reads=[k("KS%d" % h), k("QS")], writes=["psc%d" % p2])
                    S.op("dve", lambda e, p2=p2: e.tensor_tensor(STt[p2][:], psc[p2][:], TM[:], ALU.mult), reads=["psc%d" % p2, "TM"], writes=["ST%d" % p2])
                    for h in range(4):
                        hs = slice(h * 64, (h + 1) * 64)
                        S.op("pe", lambda e, b=b, c=c, p2=p2, hs=hs: e.matmul(po[p2][:, hs], STt[p2][:, hs], VV[b][:, c, hs], start=True, stop=False),
                             reads=["ST%d" % p2, "VV%d" % b], writes=["po%d" % p2])
                        S.op("pe", lambda e, b=b, cs=cs, p2=p2, hs=hs, scur=scur: e.matmul(po[p2][:, hs], Wb["QE", b][:, cs], SS[scur][:, hs], start=False, stop=True),
                             reads=[k("QE"), "SS%d" % scur], writes=["po%d" % p2])
                    ob = oi % 3
                    oi += 1
                    S.op("act", lambda e, ob=ob, p2=p2: e.copy(OB[ob][:], po[p2][:]), reads=["po%d" % p2], writes=["OB%d" % ob])
                    S.dma("sp" if oi % 2 else "act", lambda e, d=d, ob=ob, t0=t0, c=c: e.dma_start(out=I["o", d][t0 + c * 64:t0 + (c + 1) * 64, :], in_=OB[ob][:]),
                          reads=["OB%d" % ob], writes=["o_%d" % oi])
                    S.op("dve", lambda e, p2=p2: e.tensor_tensor(KVM[:], pkv[p2][:], BLK[:], ALU.mult), reads=["pkv%d" % p2, "BLK"], writes=["KVM"])
                    S.op("dve", lambda e, b=b, c=c, scur=scur: e.scalar_tensor_tensor(SS[1 - scur][:], SS[scur][:], DEC[b][:, c:c + 1], KVM[:], ALU.mult, ALU.add),
                         reads=["SS%d" % scur, "DEC%d" % b, "KVM"], writes=["SS%d" % (1 - scur)])
                    scur = 1 - scur
        S.drain_all("sp")
        S.emit()
    return nc


NEXP = 16384


def build_PD(ntok, nctx, last):
    nc = bass.Bass("TRN2", target_bir_lowering=False)
    def din(name, shape, dt=F32):
        return nc.dram_tensor(name, shape, dt, kind="ExternalInput").ap()
    xT = din("xT", [D, ntok]); modT = din("modT", [128, 96])
    su = din("su", [ntok, 256]); sv = din("sv", [ntok, 256]); gg = din("gg", [ntok, 512])
    s5u = din("s5u", [256, ntok]); yf = din("yf", [256, ntok]); yb = din("yb", [256, ntok])
    of_ = din("of", [ntok, 512]); ob_ = din("ob", [ntok, 512])
    w_out = din("w_out", [D, D]); wsT = din("wsT", [128, 512]); sgub = din("sgub", [128, 4]); s5d = din("s5d", [128, 2])
    wglu = din("wglu", [256, 512]); ng = din("ng", [128, 512]); g2 = din("g2", [128, 8]); wq = din("wq", [D, 2048])
    keysT = din("keysT", [128, 2048]); eu = din("eu", [NEXP, D]); ev = din("ev", [NEXP, D]); gfin = din("gfin", [128, 8])
    ones = din("ones", [128, 128]); ident = din("ident", [128, 128]); iota16 = din("iota16", [128, 16])
    xo = nc.dram_tensor("xo", [D, ntok], F32, kind="ExternalOutput").ap()
    nb = ntok // 128
    with ExitStack() as st:
        S = Sched(nc, st)
        sb, ps = _mk(nc, st)
        MOD = sb("MOD", [128, 96]); WOUT = sb("WOUT", [128, 8, D]); WS = sb("WS", [128, 512]); SGUB = sb("SGUB", [128, 4]); S5D = sb("S5D", [128, 2])
        WGLU = sb("WGLU", [128, 2, 512]); NG = sb("NG", [128, 512]); G2 = sb("G2", [128, 8]); WQ = sb("WQ", [128, 8, 2048]); KEYS = sb("KEYS", [128, 2048])
        GF = sb("GF", [128, 8]); ONES = sb("ONES", [128, 128]); ID = sb("ID", [128, 128]); IOTA = sb("IOTA", [128, 16])
        A2 = sb("A2", [128, 16]); TA = sb("TA", [128, 16])
        U_ = sb("U_", [128, 256]); V_ = sb("V_", [128, 256]); GU = sb("GU", [128, 256]); GV = sb("GV", [128, 256]); SQ = sb("SQ", [128, 512])
        SS = sb("SS", [128, 8]); VN = sb("VN", [128, 256]); MIX = sb("MIX", [128, D])
        S5U = sb("S5U", [128, 2, 128]); YF = sb("YF", [128, 2, 128]); YB = sb("YB", [128, 2, 128]); GE = sb("GE", [128, 2, 128]); SG = sb("SG", [128, 256])
        OF = sb("OF", [128, 512]); OB = sb("OB", [128, 512]); GG = sb("GG", [128, 512]); SL = sb("SL", [128, 512])
        XT = sb("XT", [128, 8, 128]); X1 = sb("X1", [128, 8, 128]); XO = XT; HT = sb("HT", [128, 8, 128]); MIXT = HT; HTOK = sb("HTOK", [128, D])
        RSTD = sb("RSTD", [128, 128]); TMPB = sb("TMPB", [128, 128])
        SC = sb("SC", [128, 2048])
        M16 = sb("M16", [128, 256]); I16 = sb("I16", [128, 256], U32); IF16 = sb("IF16", [128, 256]); I1S = sb("I1S", [128, 128])
        CS = sb("CS", [128, 2048]); SC2 = CS; QT = CS[:].rearrange("p (q t) -> p q t", t=128); CS2 = sb("CS2", [128, 256])
        T16 = sb("T16", [128, 128]); P16 = sb("P16", [128, 128], U32); PF = sb("PF", [128, 128]); AI = sb("AI", [128, 128], I32)
        AFL = sb("AFL", [128, 128]); BFL = sb("BFL", [128, 128]); E1 = sb("E1", [128, 128]); E2 = sb("E2", [128, 128])
        EG = sb("EG", [128, 128]); GATE = sb("GATE", [128, 128]); IDXTI = sb("IDXTI", [128, 128], I32); GATET = sb("GATET", [128, 128])
        ACTT = sb("ACTT", [128, 128]); WT = sb("WT", [128, 128])
        UG = [sb("UG%d" % i, [128, D]) for i in range(2)]; VG = UG
        HB = [sb("HB%d" % i, [128, D]) for i in range(2)]
        P0 = ps("P0", [128, 2048]); P1 = ps("P1", [128, 1024]); P2 = ps("P2", [128, 512]); P3 = ps("P3", [128, 512])

        def V(fn, r=(), w=()):
            S.op("dve", fn, reads=r, writes=w)

        def A(fn, r=(), w=()):
            S.op("act", fn, reads=r, writes=w)

        def PE(fn, r=(), w=()):
            S.op("pe", fn, reads=r, writes=w)

        def LD(q, t, src, key):
            S.dma(q, lambda e: e.dma_start(out=t, in_=src), writes=[key])

        LD("sp", MOD[:], modT, "MOD"); LD("sp", WS[:], wsT, "WS"); LD("sp", SGUB[:], sgub, "SGUB"); LD("sp", S5D[:], s5d, "S5D")
        LD("sp", WGLU[:], wglu.rearrange("(c p) f -> p c f", p=128), "WGLU"); LD("sp", NG[:], ng, "NG"); LD("sp", G2[:], g2, "G2")
        LD("sp", KEYS[:], keysT, "KEYS"); LD("sp", GF[:], gfin, "GF"); LD("sp", ONES[:], ones, "ONES"); LD("sp", ID[:], ident, "ID"); LD("sp", IOTA[:], iota16, "IOTA")
        wo_v = w_out.rearrange("(k p) f -> p k f", p=128)
        wq_v = wq.rearrange("(k p) f -> p k f", p=128)
        for k in range(8):
            LD("act", WOUT[:, k, :], wo_v[:, k, :], "WOUT")
            LD("act", WQ[:, k, :], wq_v[:, k, :], "WQ")
        MOD3 = MOD[:].rearrange("p (j c) -> p j c", c=2)
        V(lambda e: e.tensor_scalar(TA[:].rearrange("p (j c) -> p j c", c=2), MOD3[:, 32:40, :], 1.0, None, ALU.add), ["MOD"], ["TA"])
        V(lambda e: e.tensor_tensor(A2[:].rearrange("p (j c) -> p j c", c=2), TA[:].rearrange("p (j c) -> p j c", c=2),
                                    G2[:].unsqueeze(2).to_broadcast([128, 8, 2]), ALU.mult), ["TA", "G2"], ["A2"])
        A23 = A2[:].rearrange("p (j c) -> p j c", c=2)

        def rs_from_ss(ss, n, scale):
            V(lambda e: e.tensor_scalar(ss, ss, scale, EPS, ALU.mult, ALU.add), ["SS"], ["SS"])
            A(lambda e: e.sqrt(ss, ss), ["SS"], ["SS"])
            V(lambda e: e.reciprocal(ss, ss), ["SS"], ["SS"])

        def top16(src, scratch, mout, iout, n):
            V(lambda e: e.max(mout[:, 0:8], src), ["TK"], ["TK"])
            V(lambda e: e.max_index(iout[:, 0:8], mout[:, 0:8], src), ["TK"], ["TK"])
            V(lambda e: e.match_replace(scratch, mout[:, 0:8], src, -1e30), ["TK"], ["TK"])
            V(lambda e: e.max(mout[:, 8:16], scratch), ["TK"], ["TK"])
            V(lambda e: e.max_index(iout[:, 8:16], mout[:, 8:16], scratch), ["TK"], ["TK"])

        xT_v = xT.rearrange("(k p) t -> p k t", p=128)
        xo_v = xo.rearrange("(k p) t -> p k t", p=128)
        s5u_v = s5u.rearrange("(c p) t -> p c t", p=128)
        yf_v = yf.rearrange("(c p) t -> p c t", p=128)
        yb_v = yb.rearrange("(c p) t -> p c t", p=128)
        for bi in range(nb):
            tk = slice(bi * 128, (bi + 1) * 128)
            col = 1 if bi * 128 < nctx else 0
            LD("sp", U_[:], su[tk, :], "U_"); LD("sp", V_[:], sv[tk, :], "V_"); LD("sp", GG[:], gg[tk, :], "GG")
            LD("act", S5U[:], s5u_v[:, :, tk], "S5U"); LD("act", YF[:], yf_v[:, :, tk], "YF"); LD("act", YB[:], yb_v[:, :, tk], "YB")
            LD("sp", OF[:], of_[tk, :], "OF"); LD("sp", OB[:], ob_[tk, :], "OB"); LD("act", XT[:], xT_v[:, :, tk], "XT")
            A(lambda e: e.activation(GU[:], U_[:], AF.Gelu), ["U_"], ["GU"])
            A(lambda e: e.activation(GV[:], V_[:], AF.Gelu), ["V_"], ["GV"])
            V(lambda e: e.tensor_tensor(SQ[:, 0:256], GV[:], GV[:], ALU.mult), ["GV"], ["SQ"])
            V(lambda e: e.tensor_reduce(SS[:, 0:4], SQ[:, 0:256].rearrange("p (h d) -> p h d", d=64), AX.X, ALU.add), ["SQ"], ["SS"])
            rs_from_ss(SS[:, 0:4], 4, 1.0 / 64)
            V(lambda e: e.tensor_tensor(VN[:].rearrange("p (h d) -> p h d", d=64), GV[:].rearrange("p (h d) -> p h d", d=64),
                                        SS[:, 0:4].unsqueeze(2).to_broadcast([128, 4, 64]), ALU.mult), ["GV", "SS"], ["VN"])
            for h in range(4):
                PE(lambda e, h=h: e.matmul(P2[:, h * 64:(h + 1) * 64], WS[:, h * 128:(h + 1) * 128], VN[:, h * 64:(h + 1) * 64], start=True, stop=True),
                   ["WS", "VN"], ["P2"])
            V(lambda e: e.tensor_tensor(MIX[:, 0:256].rearrange("p (h d) -> p h d", d=64), P2[:, 0:256].rearrange("p (h d) -> p h d", d=64),
                                        SGUB[:].unsqueeze(2).to_broadcast([128, 4, 64]), ALU.add), ["P2", "SGUB"], ["MIXa"])
            V(lambda e: e.tensor_tensor(MIX[:, 0:256], MIX[:, 0:256], GU[:], ALU.mult), ["GU"], ["MIXa"])
            V(lambda e: e.tensor_tensor(YF[:], YF[:], YB[:], ALU.add), ["YB"], ["YF"])
            for ct in range(2):
                V(lambda e, ct=ct: e.scalar_tensor_tensor(YF[:, ct, :], S5U[:, ct, :], S5D[:, ct:ct + 1], YF[:, ct, :], ALU.mult, ALU.add),
                  ["S5U", "S5D"], ["YF"])
            A(lambda e: e.activation(GE[:], YF[:], AF.Gelu), ["YF"], ["GE"])
            for ct in range(2):
                PE(lambda e, ct=ct: e.matmul(P3[:, 0:512], GE[:, ct, :], WGLU[:, ct, :], start=(ct == 0), stop=(ct == 1)), ["GE", "WGLU"], ["P3"])
            A(lambda e: e.activation(SG[:], P3[:, 256:512], AF.Sigmoid), ["P3"], ["SG"])
            V(lambda e: e.tensor_tensor(MIX[:, 256:512], P3[:, 0:256], SG[:], ALU.mult), ["P3", "SG"], ["MIXb"])
            V(lambda e: e.tensor_tensor(OF[:], OF[:], OB[:], ALU.add), ["OB"], ["OF"])
            V(lambda e: e.tensor_tensor(SQ[:], OF[:], OF[:], ALU.mult), ["OF"], ["SQ"])
            V(lambda e: e.tensor_reduce(SS[:, 0:8], SQ[:].rearrange("p (h d) -> p h d", d=64), AX.X, ALU.add), ["SQ"], ["SS"])
            rs_from_ss(SS[:, 0:8], 8, 1.0 / 64)
            V(lambda e: e.tensor_tensor(OF[:].rearrange("p (h d) -> p h d", d=64), OF[:].rearrange("p (h d) -> p h d", d=64),
                                        SS[:, 0:8].unsqueeze(2).to_broadcast([128, 8, 64]), ALU.mult), ["SS"], ["OF"])
            V(lambda e: e.tensor_tensor(OF[:], OF[:], NG[:], ALU.mult), ["NG"], ["OF"])
            A(lambda e: e.activation(SL[:], GG[:], AF.Silu), ["GG"], ["SL"])
            V(lambda e: e.tensor_tensor(MIX[:, 512:1024], OF[:], SL[:], ALU.mult), ["OF", "SL"], ["MIXc"])
            for f in range(8):
                pp, pk = (P2, "P2") if f % 2 == 0 else (P3, "P3")
                PE(lambda e, f=f, pp=pp: e.transpose(pp[:, 0:128], MIX[:, f * 128:(f + 1) * 128], ID[:]), ["MIXa", "MIXb", "MIXc", "ID"], [pk])
                A(lambda e, f=f, pp=pp: e.copy(MIXT[:, f, :], pp[:, 0:128]), [pk], ["HT"])
            for ot in range(8):
                pp, pk = (P2, "P2") if ot % 2 == 0 else (P3, "P3")
                for k in range(8):
                    PE(lambda e, ot=ot, k=k, pp=pp: e.matmul(pp[:, 0:128], WOUT[:, k, ot * 128:(ot + 1) * 128], MIXT[:, k, :], start=(k == 0), stop=(k == 7)),
                       ["WOUT", "HT"], [pk])
                V(lambda e, ot=ot, pp=pp, col=col: e.scalar_tensor_tensor(X1[:, ot, :], pp[:, 0:128], MOD3[:, 16 + ot, col:col + 1], XT[:, ot, :], ALU.mult, ALU.add),
                  [pk, "MOD", "XT"], ["X1"])
            for k in range(8):
                A(lambda e, k=k: e.activation(TMPB[:], X1[:, k, :], AF.Square), ["X1"], ["TMPB"])
                PE(lambda e, k=k: e.matmul(P2[:, 0:128], ONES[:], TMPB[:], start=(k == 0), stop=(k == 7)), ["ONES", "TMPB"], ["P2"])
            V(lambda e: e.tensor_scalar(RSTD[:], P2[:, 0:128], 1.0 / D, EPS, ALU.mult, ALU.add), ["P2"], ["RSTD"])
            A(lambda e: e.sqrt(RSTD[:], RSTD[:]), ["RSTD"], ["RSTD"])
            V(lambda e: e.reciprocal(RSTD[:], RSTD[:]), ["RSTD"], ["RSTD"])
            for k in range(8):
                V(lambda e, k=k: e.tensor_tensor(TMPB[:], X1[:, k, :], RSTD[:], ALU.mult), ["X1", "RSTD"], ["TMPB"])
                A(lambda e, k=k, col=col: e.activation(HT[:, k, :], TMPB[:], AF.Identity, bias=MOD3[:, 24 + k, col:col + 1], scale=A23[:, k, col:col + 1]),
                  ["TMPB", "MOD", "A2"], ["HT"])
            for k in range(8):
                pp, pk = (P2, "P2") if k % 2 == 0 else (P3, "P3")
                PE(lambda e, k=k, pp=pp: e.transpose(pp[:, 0:128], HT[:, k, :], ID[:]), ["HT", "ID"], [pk])
                A(lambda e, k=k, pp=pp: e.copy(HTOK[:, k * 128:(k + 1) * 128], pp[:, 0:128]), [pk], ["HTOK"])
            for qt in range(16):
                pp, pk = (P2, "P2") if qt % 2 == 0 else (P3, "P3")
                for k in range(8):
                    PE(lambda e, qt=qt, k=k, pp=pp: e.matmul(pp[:, 0:128], WQ[:, k, qt * 128:(qt + 1) * 128], HT[:, k, :], start=(k == 0), stop=(k == 7)),
                       ["WQ", "HT"], [pk])
                if qt % 2 == 0:
                    A(lambda e, qt=qt, pp=pp: e.copy(QT[:, qt, :], pp[:, 0:128]), [pk], ["TK"])
                else:
                    V(lambda e, qt=qt, pp=pp: e.tensor_copy(QT[:, qt, :], pp[:, 0:128]), [pk], ["TK"])
            for qt in range(16):
                PE(lambda e, qt=qt: e.matmul(P0[:, qt * 128:(qt + 1) * 128], QT[:, qt, :], KEYS[:, qt * 128:(qt + 1) * 128], start=True, stop=True),
                   ["TK", "KEYS"], ["P0a", "P0b"])
            for q4 in range(4):
                A(lambda e, q4=q4: e.copy(SC[:, q4 * 512:(q4 + 1) * 512], P0[:, q4 * 512:(q4 + 1) * 512]), ["P0a", "P0b"], ["TK"])
            for qt in range(16):
                top16(SC[:, qt * 128:(qt + 1) * 128], SC2[:, qt * 128:(qt + 1) * 128], M16[:, qt * 16:(qt + 1) * 16], I16[:, qt * 16:(qt + 1) * 16], 128)
            V(lambda e: e.tensor_copy(IF16[:], I16[:]), ["TK"], ["TK"])
            M4 = M16[:].rearrange("p (h q k) -> p h q k", q=2, k=16)
            IF4 = IF16[:].rearrange("p (h q k) -> p h q k", q=2, k=16)
            I1S3 = I1S[:].rearrange("p (h k) -> p h k", k=16)
            V(lambda e: e.tensor_scalar(I1S3, IF4[:, :, 0, :], 128.0, None, ALU.mult), ["TK"], ["TK"])
            CS4 = CS[:].rearrange("p (h a b) -> p h a b", a=16, b=16)
            V(lambda e: e.tensor_tensor(CS4, M4[:, :, 0, :].unsqueeze(3).to_broadcast([128, 8, 16, 16]),
                                        M4[:, :, 1, :].unsqueeze(2).to_broadcast([128, 8, 16, 16]), ALU.add), ["TK"], ["TK"])
            for h in range(8):
                top16(CS[:, h * 256:(h + 1) * 256], CS2[:], T16[:, h * 16:(h + 1) * 16], P16[:, h * 16:(h + 1) * 16], 256)
            V(lambda e: e.tensor_copy(PF[:], P16[:]), ["TK"], ["TK"])
            V(lambda e: e.tensor_scalar(AFL[:], PF[:], -7.5, 1.0 / 16, ALU.add, ALU.mult), ["TK"], ["TK"])
            V(lambda e: e.tensor_copy(AI[:], AFL[:]), ["TK"], ["TK"])
            V(lambda e: e.tensor_copy(AFL[:], AI[:]), ["TK"], ["TK"])
            V(lambda e: e.scalar_tensor_tensor(BFL[:], AFL[:], -16.0, PF[:], ALU.mult, ALU.add), ["TK"], ["TK"])
            EQ4 = CS[:].rearrange("p (h k a) -> p h k a", k=16, a=16)
            io4 = IOTA[:].unsqueeze(1).unsqueeze(1).to_broadcast([128, 8, 16, 16])
            for (sel, src, dst) in ((AFL, I1S3, E1), (BFL, IF4[:, :, 1, :], E2)):
                V(lambda e, sel=sel: e.tensor_tensor(EQ4, io4, sel[:].rearrange("p (h k) -> p h k", k=16).unsqueeze(3).to_broadcast([128, 8, 16, 16]), ALU.is_equal),
                  ["TK", "IOTA"], ["TK"])
                V(lambda e, src=src: e.tensor_tensor(EQ4, EQ4, src.unsqueeze(2).to_broadcast([128, 8, 16, 16]), ALU.mult), ["TK"], ["TK"])
                V(lambda e, dst=dst: e.tensor_reduce(dst[:], CS[:].rearrange("p (m a) -> p m a", a=16), AX.X, ALU.add), ["TK"], ["TK"])
            V(lambda e: e.tensor_tensor(E1[:], E1[:], E2[:], ALU.add), ["TK"], ["TK"])
            T3 = T16[:].rearrange("p (h k) -> p h k", k=16)
            V(lambda e: e.tensor_tensor(EG[:].rearrange("p (h k) -> p h k", k=16), T3, T3[:, :, 0:1].to_broadcast([128, 8, 16]), ALU.subtract), ["TK"], ["TK"])
            A(lambda e: e.activation(EG[:], EG[:], AF.Exp), ["TK"], ["TK"])
            V(lambda e: e.tensor_reduce(SS[:, 0:8], EG[:].rearrange("p (h k) -> p h k", k=16), AX.X, ALU.add), ["TK"], ["SS"])
            V(lambda e: e.reciprocal(SS[:, 0:8], SS[:, 0:8]), ["SS"], ["SS"])
            V(lambda e: e.tensor_tensor(GATE[:].rearrange("p (h k) -> p h k", k=16), EG[:].rearrange("p (h k) -> p h k", k=16),
                                        SS[:, 0:8].unsqueeze(2).to_broadcast([128, 8, 16]), ALU.mult), ["TK", "SS"], ["GATE"])
            PE(lambda e: e.transpose(P2[:, 0:128], E1[:], ID[:]), ["TK", "ID"], ["P2"])
            V(lambda e: e.tensor_copy(IDXTI[:], P2[:, 0:128]), ["P2"], ["IDXTI"])
            PE(lambda e: e.transpose(P3[:, 0:128], GATE[:], ID[:]), ["GATE", "ID"], ["P3"])
            A(lambda e: e.copy(GATET[:], P3[:, 0:128]), ["P3"], ["GATET"])
            for t in range(128):
                b = t % 2
                S.dma("pool", lambda e, t=t, b=b: e.indirect_dma_start(out=UG[b][:], out_offset=None, in_=eu,
                                                                       in_offset=bass.IndirectOffsetOnAxis(ap=IDXTI[:, t:t + 1], axis=0)),
                      reads=["IDXTI"], writes=["UG%d" % b])
                pk = "P0a" if b == 0 else "P0b"
                for hf in range(2):
                    PE(lambda e, t=t, b=b, hf=hf: e.matmul(P0[:, b * 1024 + hf * 512:b * 1024 + (hf + 1) * 512], ID[:, t:t + 1].to_broadcast([128, 128]),
                                                           HTOK[:, hf * 512:(hf + 1) * 512], start=True, stop=True), ["ID", "HTOK"], [pk])
                    A(lambda e, b=b, hf=hf: e.copy(HB[b][:, hf * 512:(hf + 1) * 512], P0[:, b * 1024 + hf * 512:b * 1024 + (hf + 1) * 512]), [pk], ["HB%d" % b])
                V(lambda e, t=t, b=b: e.scalar_tensor_tensor(UG[b][:], UG[b][:], 1.0, HB[b][:], ALU.mult, ALU.mult, accum_out=ACTT[:, t:t + 1]),
                  ["HB%d" % b], ["UG%d" % b, "ACTT"])
            A(lambda e: e.activation(WT[:], ACTT[:], AF.Gelu), ["ACTT"], ["WT"])
            V(lambda e: e.tensor_tensor(WT[:], WT[:], GATET[:], ALU.mult), ["GATET"], ["WT"])
            for t in range(128):
                b = t % 2
                S.dma("pool", lambda e, t=t, b=b: e.indirect_dma_start(out=VG[b][:], out_offset=None, in_=ev,
                                                                       in_offset=bass.IndirectOffsetOnAxis(ap=IDXTI[:, t:t + 1], axis=0)),
                      reads=["IDXTI"], writes=["UG%d" % b])
                for ot in range(8):
                    PE(lambda e, t=t, b=b, ot=ot: e.matmul(P1[:, ot * 128 + t:ot * 128 + t + 1], VG[b][:, ot * 128:(ot + 1) * 128], WT[:, t:t + 1],
                                                           start=True, stop=True), ["UG%d" % b, "WT"], ["P1"])
            for ot in range(8):
                V(lambda e, ot=ot, col=col: e.scalar_tensor_tensor(XO[:, ot, :], P1[:, ot * 128:(ot + 1) * 128], MOD3[:, 40 + ot, col:col + 1], X1[:, ot, :],
                                                                   ALU.mult, ALU.add), ["P1", "MOD", "X1"], ["XT"])
            if last:
                for k in range(8):
                    A(lambda e, k=k: e.activation(TMPB[:], XO[:, k, :], AF.Square), ["XT"], ["TMPB"])
                    PE(lambda e, k=k: e.matmul(P2[:, 0:128], ONES[:], TMPB[:], start=(k == 0), stop=(k == 7)), ["ONES", "TMPB"], ["P2"])
                V(lambda e: e.tensor_scalar(RSTD[:], P2[:, 0:128], 1.0 / D, EPS, ALU.mult, ALU.add), ["P2"], ["RSTD"])
                A(lambda e: e.sqrt(RSTD[:], RSTD[:]), ["RSTD"], ["RSTD"])
                V(lambda e: e.reciprocal(RSTD[:], RSTD[:]), ["RSTD"], ["RSTD"])
                for k in range(8):
                    V(lambda e, k=k: e.scalar_tensor_tensor(XO[:, k, :], XO[:, k, :], GF[:, k:k + 1], RSTD[:], ALU.mult, ALU.mult), ["RSTD", "GF"], ["XT"])
            S.dma("sp", lambda e, tk=tk: e.dma_start(out=xo_v[:, :, tk], in_=XO[:]), reads=["XT"], writes=["xo_%d" % bi])
        S.drain_all("sp")
        S.emit()
    return nc


SEQC = 256
SEQL = 4096
OWN = 2176


def _mirror(t0, T):
    if t0 < SEQC:
        return 0, SEQC
    i = (t0 - SEQC) // 512
    return SEQC + SEQL - 512 * (i + 1), 512


def emit_A(nc, S, sb, ps, io, tl_all=False):
    xT, cT, w_mod, b_mod, g1, w_in, ones = io["xT"], io["cT"], io["w_mod"], io["b_mod"], io["g1"], io["w_in"], io["ones"]
    FMU, FMQ, FMK, FMZ, VT, TL, MODS = io["FMU"], io["FMQ"], io["FMK"], io["FMZ"], io["VT"], io["TL"], io["MODS"]
    CT = sb("CT", [128, 16]); SC = sb("SC", [128, 16]); BM = sb("BM", [128, 48]); G1 = sb("G1", [128, 8])
    ONES = sb("ONES", [128, 128]); MOD = sb("MOD", [128, 96])
    A1 = sb("A1", [128, 16]); TMPA = sb("TMPA", [128, 16])
    WM = [sb("WM%d" % i, [128, 8, 512]) for i in range(2)]
    WIN = sb("WIN", [128, 8, INW])
    XT = [sb("XT%d" % i, [128, 8, 512]) for i in range(2)]
    XSQ = sb("XSQ", [128, 512]); RSTD = sb("RSTD", [128, 512]); TMP = sb("TMP", [128, 512])
    HT = [sb("HT%d" % i, [128, 8, 512]) for i in range(2)]
    OUTB = [sb("OUTB%d" % i, [128, 512]) for i in range(4)]
    pmod = ps("pmod", [128, 96]); pss = ps("pss", [128, 512])
    pout = [ps("pout%d" % i, [128, 512]) for i in range(3)]
    S.dma("sp", lambda e: e.dma_start(out=CT[:], in_=cT), writes=["CT"])
    S.dma("sp", lambda e: e.dma_start(out=BM[:], in_=b_mod), writes=["BM"])
    S.dma("sp", lambda e: e.dma_start(out=G1[:], in_=g1), writes=["G1"])
    S.dma("sp", lambda e: e.dma_start(out=ONES[:], in_=ones), writes=["ONES"])
    S.op("act", lambda e: e.activation(SC[:], CT[:], AF.Silu), reads=["CT"], writes=["SC"])
    SC3 = SC[:].rearrange("p (k c) -> p k c", c=2)
    wm_v = w_mod.rearrange("(k p) f -> p k f", p=128)
    for jg in range(12):
        b = jg % 2
        S.dma("act" if jg % 2 else "sp",
              lambda e, jg=jg, b=b: e.dma_start(out=WM[b][:], in_=wm_v[:, :, jg * 512:(jg + 1) * 512]), writes=["WM%d" % b])
        for j8 in range(4):
            j = jg * 4 + j8
            for k in range(8):
                S.op("pe", lambda e, j=j, j8=j8, k=k, b=b: e.matmul(
                    pmod[:, 2 * j:2 * j + 2], WM[b][:, k, j8 * 128:(j8 + 1) * 128], SC3[:, k, :],
                    start=(k == 0), stop=(k == 7)), reads=["WM%d" % b, "SC"], writes=["pmod"])
    S.op("dve", lambda e: e.tensor_tensor(MOD[:].rearrange("p (j c) -> p j c", c=2), pmod[:].rearrange("p (j c) -> p j c", c=2),
                                          BM[:].unsqueeze(2).to_broadcast([128, 48, 2]), ALU.add), reads=["pmod", "BM"], writes=["MOD"])
    S.dma("sp", lambda e: e.dma_start(out=MODS, in_=MOD[:]), reads=["MOD"], writes=["MODS"])
    MOD3 = MOD[:].rearrange("p (j c) -> p j c", c=2)
    S.op("dve", lambda e: e.tensor_scalar(TMPA[:].rearrange("p (j c) -> p j c", c=2), MOD3[:, 8:16, :], 1.0, None, ALU.add), reads=["MOD"], writes=["TMPA"])
    S.op("dve", lambda e: e.tensor_tensor(A1[:].rearrange("p (j c) -> p j c", c=2), TMPA[:].rearrange("p (j c) -> p j c", c=2),
                                          G1[:].unsqueeze(2).to_broadcast([128, 8, 2]), ALU.mult), reads=["TMPA", "G1"], writes=["A1"])
    A13 = A1[:].rearrange("p (j c) -> p j c", c=2)
    win_v = w_in.rearrange("(k p) f -> p k f", p=128)
    for k in range(8):
        S.dma("pool", lambda e, k=k: e.dma_start(out=WIN[:, k, :], in_=win_v[:, k, :]), writes=["WIN%d" % k])
    WK = ["WIN%d" % k for k in range(8)]
    xT_v = xT.rearrange("(k p) t -> p k t", p=128)
    grp = [(0, SEQC, 1, -1)] + [(SEQC + 512 * g, 512, 0, g) for g in range(8)]
    cnt = {"o": 0}

    def evac(dst_ap_fn, pb, m, tn, key, cm=False):
        ob = cnt["o"] % 4
        cnt["o"] += 1
        if cm:
            o_ap = lambda: OUTB[ob][:m, :512].rearrange("p (w r) -> p w r", r=8)
            i_ap = lambda: pout[pb][:m, :512].rearrange("p (r w) -> p w r", w=64)
        else:
            o_ap = lambda: OUTB[ob][:m, :tn]
            i_ap = lambda: pout[pb][:m, :tn]
        if cnt["o"] % 2:
            S.op("act", lambda e: e.copy(o_ap(), i_ap()), reads=["pout%d" % pb], writes=["OUTB%d" % ob])
        else:
            S.op("dve", lambda e: e.tensor_copy(o_ap(), i_ap()), reads=["pout%d" % pb], writes=["OUTB%d" % ob])
        S.dma("sp" if cnt["o"] % 2 else "act", lambda e: dst_ap_fn(e, OUTB[ob]), reads=["OUTB%d" % ob], writes=["%s_%d" % (key, cnt["o"])])

    pi = 0
    for gi, (t0, tn, col, g) in enumerate(grp):
        b = gi % 2
        xk, hk = "XT%d" % b, "HT%d" % b
        S.dma("sp", lambda e, b=b, t0=t0, tn=tn: e.dma_start(out=XT[b][:, :, :tn], in_=xT_v[:, :, t0:t0 + tn]), writes=[xk])
        for k in range(8):
            S.op("act", lambda e, b=b, k=k, tn=tn: e.activation(XSQ[:, :tn], XT[b][:, k, :tn], AF.Square), reads=[xk], writes=["XSQ"])
            S.op("pe", lambda e, k=k, tn=tn: e.matmul(pss[:, :tn], ONES[:], XSQ[:, :tn], start=(k == 0), stop=(k == 7)), reads=["ONES", "XSQ"], writes=["pss"])
        S.op("dve", lambda e, tn=tn: e.tensor_scalar(RSTD[:, :tn], pss[:, :tn], 1.0 / D, EPS, ALU.mult, ALU.add), reads=["pss"], writes=["RSTD"])
        S.op("act", lambda e, tn=tn: e.sqrt(RSTD[:, :tn], RSTD[:, :tn]), reads=["RSTD"], writes=["RSTD"])
        S.op("dve", lambda e, tn=tn: e.reciprocal(RSTD[:, :tn], RSTD[:, :tn]), reads=["RSTD"], writes=["RSTD"])
        for k in range(8):
            S.op("dve", lambda e, b=b, k=k, tn=tn: e.tensor_tensor(TMP[:, :tn], XT[b][:, k, :tn], RSTD[:, :tn], ALU.mult), reads=[xk, "RSTD"], writes=["TMP"])
            S.op("act", lambda e, b=b, k=k, tn=tn, col=col: e.activation(HT[b][:, k, :tn], TMP[:, :tn], AF.Identity, bias=MOD3[:, k, col:col + 1],
                                                                         scale=A13[:, k, col:col + 1]), reads=["TMP", "MOD", "A1"], writes=[hk])
        fm = [(FMU, 0, 512, 128), (FMU, 128, 640, 128), (FMQ, 0, 768, 128), (FMQ, 128, 896, 128), (FMK, 0, 1024, 128), (FMK, 128, 1152, 128), (FMZ, 0, 2304, 32)]
        for (dst, r0, c0, m) in fm:
            pb = pi % 3
            pi += 1
            for k in range(8):
                S.op("pe", lambda e, b=b, k=k, tn=tn, c0=c0, m=m, pb=pb: e.matmul(pout[pb][:m, :tn], WIN[:, k, c0:c0 + m], HT[b][:, k, :tn], start=(k == 0), stop=(k == 7)),
                     reads=WK + [hk], writes=["pout%d" % pb])
            if dst is FMU or g < 0:
                evac(lambda e, ob, dst=dst, r0=r0, m=m, t0=t0, tn=tn: e.dma_start(out=dst[r0:r0 + m, t0:t0 + tn], in_=ob[:m, :tn]), pb, m, tn, "fm")
            else:
                evac(lambda e, ob, dst=dst, r0=r0, m=m, g=g: e.dma_start(
                    out=dst[r0:r0 + m, SEQC:].rearrange("p (w r) -> p w r", r=64)[:, :, 8 * g:8 * g + 8],
                    in_=ob[:m, :512].rearrange("p (w r) -> p w r", r=8)), pb, m, 512, "fm", cm=True)
        for ti in range(tn // 128):
            tl = [(VT, t0 + ti * 128, 0, 1280)]
            own_row = None
            if tl_all:
                own_row = t0 + ti * 128
            elif g < 0 and ti == 0:
                own_row = 0
            elif 0 <= g < 4:
                own_row = 128 + g * 512 + ti * 128
            if own_row is not None:
                tl += [(TL, own_row, 0, 0), (TL, own_row, 512, 1792)]
            for (dst, row, dc, c0) in tl:
                pb = pi % 3
                pi += 1
                for k in range(8):
                    S.op("pe", lambda e, b=b, k=k, ti=ti, c0=c0, pb=pb: e.matmul(pout[pb][:, :], HT[b][:, k, ti * 128:(ti + 1) * 128], WIN[:, k, c0:c0 + 512],
                                                                               start=(k == 0), stop=(k == 7)), reads=WK + [hk], writes=["pout%d" % pb])
                evac(lambda e, ob, dst=dst, row=row, dc=dc: e.dma_start(out=dst[row:row + 128, dc:dc + 512], in_=ob[:, :]), pb, 128, 512, "tm")


def emit_B(nc, S, sb, ps, io):
    FMU, YFs, YBs = io["FMU"], io["YF"], io["YB"]
    tau, ident = io["tau"], io["ident"]
    for ct in range(2):
        prm, bre, bim, cre, cim = io["prm"][ct], io["bre"][ct], io["bim"][ct], io["cre"][ct], io["cim"][ct]
        uin = [FMU, FMU]
        yout = [YFs, YBs]
        X = "c%d_" % ct
        sub = ExitStack()
        sb, ps = _mk(nc, sub)
        TWO_PI = 2.0 * math.pi
        PRM = sb(X + "PRM", [128, 24]); BRE = sb(X + "BRE", [128, 128]); BIM = sb(X + "BIM", [128, 128])
        CRE = sb(X + "CRE", [128, 128]); CIM = sb(X + "CIM", [128, 128]); TAU = sb(X + "TAU", [128, 512]); ID = sb(X + "ID", [128, 128])
        names = ["DT", "LR", "MAG", "TH", "R", "R2", "RF", "FR", "SIN", "COS", "ARE", "AIM", "DEN", "AM1", "FRE", "FIM", "T0", "T1"]
        P = {n: sb(X + "p_" + n, [128, 8]) for n in names}
        RI = sb(X + "p_RI", [128, 8], I32)
        BBR = sb(X + "BBR", [128, 128]); BBI = sb(X + "BBI", [128, 128]); TB = sb(X + "TB", [128, 128])
        PAD = sb(X + "PAD", [128, 128])
        WBR = sb(X + "WBR", [128, 8, 128]); WBI = sb(X + "WBI", [128, 8, 128]); CR = sb(X + "CR", [128, 8, 128]); CIN = sb(X + "CIN", [128, 8, 128])
        TC = sb(X + "TC", [128, 8, 512]); TS = sb(X + "TS", [128, 8, 512]); RHO = sb(X + "RHO", [128, 8, 512])
        RR = sb(X + "RR", [128, 512]); RRF = sb(X + "RRF", [128, 512]); RRI = sb(X + "RRI", [128, 512], I32)
        UC = [sb(X + "UC%d" % i, [128, 512]) for i in range(2)]
        W = {}
        for n in ("BR", "BI", "T1", "T2", "T3", "T4", "XR", "XI", "QR", "QI", "HR", "HI"):
            for i in range(2):
                W[n, i] = sb(X + "w_%s%d" % (n, i), [128, 512])
        HP = sb(X + "HP", [128, 8])
        YO = [sb(X + "YO%d" % i, [128, 512]) for i in range(2)]
        pbr = [ps(X + "pbr%d" % i, [128, 512]) for i in range(2)]
        pbi = [ps(X + "pbi%d" % i, [128, 512]) for i in range(2)]
        py = [ps(X + "py%d" % i, [128, 512]) for i in range(2)]
        ptr = ps(X + "ptr", [128, 128])

        for (t, src, k) in ((PRM, prm, "PRM"), (BRE, bre, "BRE"), (BIM, bim, "BIM"), (CRE, cre, "CRE"), (CIM, cim, "CIM"),
                            (TAU, tau, "TAU"), (ID, ident, "ID")):
            S.dma("sp", lambda e, t=t, src=src: e.dma_start(out=t[:], in_=src), writes=[k])
        PR3 = PRM[:].rearrange("p (a c) -> p a c", c=3)
        K = ["PP"]

        def V(fn, reads=(), writes=()):
            S.op("dve", fn, reads=list(reads) + K, writes=list(writes) + K)

        def A(fn, reads=(), writes=()):
            S.op("act", fn, reads=list(reads) + K, writes=list(writes) + K)

        A(lambda e: e.activation(P["DT"][:], PR3[:, :, 2], AF.Exp), reads=["PRM"])
        V(lambda e: e.tensor_scalar(P["LR"][:], PR3[:, :, 0], -1e-4, None, ALU.min), reads=["PRM"])
        V(lambda e: e.tensor_tensor(P["T0"][:], P["LR"][:], P["DT"][:], ALU.mult))
        A(lambda e: e.activation(P["MAG"][:], P["T0"][:], AF.Exp))
        V(lambda e: e.tensor_tensor(P["TH"][:], PR3[:, :, 1], P["DT"][:], ALU.mult), reads=["PRM"])
        V(lambda e: e.tensor_scalar(P["R"][:], P["TH"][:], 1.0 / TWO_PI, None, ALU.mult))
        V(lambda e: e.tensor_scalar(P["R2"][:], P["R"][:], 0.25, None, ALU.add))
        for (src, dst) in (("R", "SIN"), ("R2", "COS")):
            V(lambda e, src=src: e.tensor_copy(RI[:], P[src][:]))
            V(lambda e: e.tensor_copy(P["RF"][:], RI[:]))
            V(lambda e, src=src: e.tensor_tensor(P["FR"][:], P[src][:], P["RF"][:], ALU.subtract))
            A(lambda e, dst=dst: e.activation(P[dst][:], P["FR"][:], AF.Sin, scale=TWO_PI))
        V(lambda e: e.tensor_tensor(P["ARE"][:], P["MAG"][:], P["COS"][:], ALU.mult))
        V(lambda e: e.tensor_tensor(P["AIM"][:], P["MAG"][:], P["SIN"][:], ALU.mult))
        V(lambda e: e.tensor_tensor(P["T0"][:], P["LR"][:], P["LR"][:], ALU.mult))
        V(lambda e: e.tensor_tensor(P["T1"][:], PR3[:, :, 1], PR3[:, :, 1], ALU.mult), reads=["PRM"])
        V(lambda e: e.tensor_tensor(P["DEN"][:], P["T0"][:], P["T1"][:], ALU.add))
        V(lambda e: e.reciprocal(P["DEN"][:], P["DEN"][:]))
        V(lambda e: e.tensor_scalar(P["AM1"][:], P["ARE"][:], -1.0, None, ALU.add))
        V(lambda e: e.tensor_tensor(P["T0"][:], P["AM1"][:], P["LR"][:], ALU.mult))
        V(lambda e: e.tensor_tensor(P["T1"][:], P["AIM"][:], PR3[:, :, 1], ALU.mult), reads=["PRM"])
        V(lambda e: e.tensor_tensor(P["T0"][:], P["T0"][:], P["T1"][:], ALU.add))
        V(lambda e: e.tensor_tensor(P["FRE"][:], P["T0"][:], P["DEN"][:], ALU.mult))
        V(lambda e: e.tensor_tensor(P["T0"][:], P["AIM"][:], P["LR"][:], ALU.mult))
        V(lambda e: e.tensor_tensor(P["T1"][:], P["AM1"][:], PR3[:, :, 1], ALU.mult), reads=["PRM"])
        V(lambda e: e.tensor_tensor(P["T0"][:], P["T0"][:], P["T1"][:], ALU.subtract))
        V(lambda e: e.tensor_tensor(P["FIM"][:], P["T0"][:], P["DEN"][:], ALU.mult))

        def v3(t):
            return t[:].rearrange("p (a h) -> p a h", h=16)

        def bc(n):
            return P[n][:].unsqueeze(2).to_broadcast([128, 8, 16])
        V(lambda e: e.tensor_tensor(v3(BBR), v3(BRE), bc("FRE"), ALU.mult), reads=["BRE"])
        V(lambda e: e.tensor_tensor(v3(TB), v3(BIM), bc("FIM"), ALU.mult), reads=["BIM"])
        V(lambda e: e.tensor_tensor(BBR[:], BBR[:], TB[:], ALU.subtract))
        V(lambda e: e.tensor_tensor(v3(BBI), v3(BIM), bc("FRE"), ALU.mult), reads=["BIM"])
        V(lambda e: e.tensor_tensor(v3(TB), v3(BRE), bc("FIM"), ALU.mult), reads=["BRE"])
        V(lambda e: e.tensor_tensor(BBI[:], BBI[:], TB[:], ALU.add))
        V(lambda e: e.tensor_scalar(CIM[:], CIM[:], -1.0, None, ALU.mult), reads=["CIM"], writes=["CIM"])
        V(lambda e: e.memset(CR[:], 0.0)); V(lambda e: e.memset(CIN[:], 0.0))
        for dj in range(8):
            j = dj % 4
            for (src, dst) in ((BBR, WBR), (BBI, WBI)):
                V(lambda e: e.memset(PAD[:], 0.0), writes=["PAD"])
                V(lambda e, src=src, dj=dj, j=j: e.tensor_copy(PAD[0:64, 32 * j:32 * j + 16], src[0:64, dj * 16:dj * 16 + 16]), writes=["PAD"])
                V(lambda e, src=src, dj=dj, j=j: e.tensor_copy(PAD[64:128, 32 * j + 16:32 * j + 32], src[64:128, dj * 16:dj * 16 + 16]), writes=["PAD"])
                S.op("pe", lambda e: e.transpose(ptr[:], PAD[:], ID[:]), reads=["PAD", "ID"], writes=["ptr"])
                S.op("act", lambda e, dst=dst, dj=dj: e.copy(dst[:, dj, :], ptr[:]), reads=["ptr"], writes=["WB"])
            for (src, dst) in ((CRE, CR), (CIM, CIN)):
                V(lambda e, src=src, dst=dst, dj=dj, j=j: e.tensor_copy(dst[0:64, dj, 32 * j:32 * j + 16], src[0:64, dj * 16:dj * 16 + 16]), reads=["CRE", "CIM"], writes=["CC"])
                V(lambda e, src=src, dst=dst, dj=dj, j=j: e.tensor_copy(dst[64:128, dj, 32 * j + 16:32 * j + 32], src[64:128, dj * 16:dj * 16 + 16]), reads=["CRE", "CIM"], writes=["CC"])
            for (off, dst) in ((0.0, TS), (0.25, TC)):
                V(lambda e, dj=dj, off=off: e.tensor_scalar(RR[:], TAU[:], P["R"][:, dj:dj + 1], off, ALU.mult, ALU.add), reads=["TAU"], writes=["RR"])
                V(lambda e: e.tensor_copy(RRI[:], RR[:]), reads=["RR"], writes=["RRI"])
                V(lambda e: e.tensor_copy(RRF[:], RRI[:]), reads=["RRI"], writes=["RRF"])
                V(lambda e: e.tensor_tensor(RRF[:], RR[:], RRF[:], ALU.subtract), reads=["RR"], writes=["RRF"])
                S.op("act", lambda e, dst=dst, dj=dj: e.activation(dst[:, dj, :], RRF[:], AF.Sin, scale=TWO_PI), reads=["RRF"], writes=["TAB"])
            V(lambda e, dj=dj: e.tensor_copy(RHO[:, dj, :], P["MAG"][:, dj:dj + 1].to_broadcast([128, 512])), writes=["TAB"])

        G = "pool"
        oi = 0
        for d in range(2):
            V(lambda e: e.memset(HP[:], 0.0), writes=["HP"])
            for ci, (t0, T) in enumerate(S5_CH):
                ub_ = (d * 9 + ci) % 2
                uk = "UC%d" % ub_
                n0 = t0 if d == 0 else _mirror(t0, T)[0]
                S.dma("sp", lambda e, d=d, n0=n0, T=T, ub_=ub_, ct=ct: e.dma_start(out=UC[ub_][:, :T], in_=uin[d][ct * 128:(ct + 1) * 128, n0:n0 + T]), writes=[uk])
                ucv = (lambda ub_=ub_, T=T: UC[ub_][:, :T]) if d == 0 else (lambda ub_=ub_, T=T: UC[ub_][:, :T][:, ::-1])
                yb_ = (d * 9 + ci) % 2
                for j in range(4):
                    dj = d * 4 + j
                    b = j % 2
                    w = lambda n, b=b, T=T: W[n, b][:, :T]
                    k = lambda n, b=b: "w_%s%d" % (n, b)
                    S.op("pe", lambda e, dj=dj, b=b, T=T, ucv=ucv: e.matmul(pbr[b][:, :T], WBR[:, dj, :], ucv(), start=True, stop=True),
                         reads=["WB", uk], writes=["pbr%d" % b])
                    S.op("pe", lambda e, dj=dj, b=b, T=T, ucv=ucv: e.matmul(pbi[b][:, :T], WBI[:, dj, :], ucv(), start=True, stop=True),
                         reads=["WB", uk], writes=["pbi%d" % b])
                    S.op("act", lambda e, w=w, b=b, T=T: e.copy(w("BR"), pbr[b][:, :T]), reads=["pbr%d" % b], writes=[k("BR")])
                    S.op("act", lambda e, w=w, b=b, T=T: e.copy(w("BI"), pbi[b][:, :T]), reads=["pbi%d" % b], writes=[k("BI")])
                    cs = lambda dj=dj, T=T: TC[:, dj, :T]
                    sn = lambda dj=dj, T=T: TS[:, dj, :T]
                    S.op("dve", lambda e, w=w, cs=cs: e.tensor_tensor(w("T1"), cs(), w("BR"), ALU.mult), reads=["TAB", k("BR")], writes=[k("T1")])
                    S.op("dve", lambda e, w=w, sn=sn: e.tensor_tensor(w("T2"), sn(), w("BI"), ALU.mult), reads=["TAB", k("BI")], writes=[k("T2")])
                    S.op("dve", lambda e, w=w: e.tensor_tensor(w("XR"), w("T1"), w("T2"), ALU.add), reads=[k("T1"), k("T2")], writes=[k("XR")])
                    S.op(G, lambda e, w=w, cs=cs: e.tensor_tensor(w("T3"), cs(), w("BI"), ALU.mult), reads=["TAB", k("BI")], writes=[k("T3")])
                    S.op(G, lambda e, w=w, sn=sn: e.tensor_tensor(w("T4"), sn(), w("BR"), ALU.mult), reads=["TAB", k("BR")], writes=[k("T4")])
                    S.op(G, lambda e, w=w: e.tensor_tensor(w("XI"), w("T3"), w("T4"), ALU.subtract), reads=[k("T3"), k("T4")], writes=[k("XI")])
                    S.op("dve", lambda e, w=w, dj=dj, j=j, T=T: e.tensor_tensor_scan(w("QR"), RHO[:, dj, :T], w("XR"), HP[:, 2 * j:2 * j + 1], ALU.mult, ALU.add),
                         reads=["TAB", k("XR"), "HP"], writes=[k("QR")])
                    S.op("dve", lambda e, w=w, dj=dj, j=j, T=T: e.tensor_tensor_scan(w("QI"), RHO[:, dj, :T], w("XI"), HP[:, 2 * j + 1:2 * j + 2], ALU.mult, ALU.add),
                         reads=["TAB", k("XI"), "HP"], writes=[k("QI")])
                    S.op("dve", lambda e, w=w, cs=cs: e.tensor_tensor(w("T1"), cs(), w("QR"), ALU.mult), reads=["TAB", k("QR")], writes=[k("T1")])
                    S.op("dve", lambda e, w=w, sn=sn: e.tensor_tensor(w("T2"), sn(), w("QI"), ALU.mult), reads=["TAB", k("QI")], writes=[k("T2")])
                    S.op("dve", lambda e, w=w: e.tensor_tensor(w("HR"), w("T1"), w("T2"), ALU.subtract), reads=[k("T1"), k("T2")], writes=[k("HR")])
                    S.op(G, lambda e, w=w, sn=sn: e.tensor_tensor(w("T3"), sn(), w("QR"), ALU.mult), reads=["TAB", k("QR")], writes=[k("T3")])
                    S.op(G, lambda e, w=w, cs=cs: e.tensor_tensor(w("T4"), cs(), w("QI"), ALU.mult), reads=["TAB", k("QI")], writes=[k("T4")])
                    S.op(G, lambda e, w=w: e.tensor_tensor(w("HI"), w("T3"), w("T4"), ALU.add), reads=[k("T3"), k("T4")], writes=[k("HI")])
                    S.op("act", lambda e, b=b, j=j, T=T: e.copy(HP[:, 2 * j:2 * j + 1], W["HR", b][:, T - 1:T]), reads=[k("HR")], writes=["HP"])
                    S.op("act", lambda e, b=b, j=j, T=T: e.copy(HP[:, 2 * j + 1:2 * j + 2], W["HI", b][:, T - 1:T]), reads=[k("HI")], writes=["HP"])
                    S.op("pe", lambda e, dj=dj, w=w, j=j, yb_=yb_, T=T: e.matmul(py[yb_][:, :T], CR[:, dj, :], w("HR"), start=(j == 0), stop=False),
                         reads=["CC", k("HR")], writes=["py%d" % yb_])
                    S.op("pe", lambda e, dj=dj, w=w, j=j, yb_=yb_, T=T: e.matmul(py[yb_][:, :T], CIN[:, dj, :], w("HI"), start=False, stop=(j == 3)),
                         reads=["CC", k("HI")], writes=["py%d" % yb_])
                if d == 0:
                    S.op("act", lambda e, yb_=yb_, T=T: e.copy(YO[yb_][:, :T], py[yb_][:, :T]), reads=["py%d" % yb_], writes=["YO%d" % yb_])
                else:
                    S.op("act", lambda e, yb_=yb_, T=T: e.copy(YO[yb_][:, :T][:, ::-1], py[yb_][:, :T]), reads=["py%d" % yb_], writes=["YO%d" % yb_])
                oi += 1
                S.dma("act", lambda e, d=d, yb_=yb_, n0=n0, T=T, ct=ct: e.dma_start(out=yout[d][ct * 128:(ct + 1) * 128, n0:n0 + T], in_=YO[yb_][:, :T]),
                      reads=["YO%d" % yb_], writes=["yout_%d" % oi])
        S.sync_all()
        S.emit()
        sub.close()


def emit_C(nc, S, sb_unused, ps_unused, io):
    FMQ, FMK, FMZ, VT = io["FMQ"], io["FMK"], io["FMZ"], io["VT"]
    OS = [io["OF"], io["OB"]]
    rst, tmask, tmask2, blk, hmask, ident = io["rst"], io["tmask"], io["tmask2"], io["blk"], io["hmask"], io["ident"]
    QSC = 32 ** -0.5
    for hh in range(2):
        X = "h%d_" % hh
        sub = ExitStack()
        sb, ps = _mk(nc, sub)
        cols = slice(hh * 256, hh * 256 + 256)
        TM2 = sb(X + "TM2", [64, 256])
        S.dma("sp", lambda e: e.dma_start(out=TM2[:], in_=tmask2), writes=["TM2"])
        RST = sb(X + "RST", [128, 512]); TM = sb(X + "TM", [64, 256]); BLK = sb(X + "BLK", [128, 256]); HM = sb(X + "HM", [128, 4]); ID = sb(X + "ID", [128, 128])
        WG = sb(X + "WG", [16, 128]); BG = sb(X + "BG", [128, 1]); NBG = sb(X + "NBG", [128, 1])
        Wb = {}
        for n in ("Q", "K", "LA", "B", "E", "D", "QE", "QS", "KD", "KS0", "KS1", "KS2", "KS3"):
            for i in range(2):
                Wb[n, i] = sb(X + "g_%s%d" % (n, i), [128, 512])
        Z = [sb(X + "Z%d" % i, [16, 512]) for i in range(2)]
        VV = [sb(X + "VV%d" % i, [64, 8, 256]) for i in range(2)]
        DEC = [sb(X + "DEC%d" % i, [128, 8]) for i in range(2)]
        KDT = [sb(X + "KDT%d" % i, [64, 128]) for i in range(2)]
        STt = [sb(X + "ST%d" % i, [64, 256]) for i in range(2)]
        OB = [sb(X + "OB%d" % i, [64, 256]) for i in range(3)]
        KVM = sb(X + "KVM", [128, 256])
        SS = [sb(X + "SS%d" % i, [128, 256]) for i in range(2)]
        pza = ps(X + "pza", [128, 512])
        pt0 = ps(X + "pt0", [64, 128])
        pt = [pt0, pt0]
        pkv = [ps(X + "pkv%d" % i, [128, 256]) for i in range(2)]
        psc = [ps(X + "psc%d" % i, [64, 256]) for i in range(2)]
        po = [ps(X + "po%d" % i, [64, 256]) for i in range(2)]
        for (t, src, k) in ((RST, rst, "RST"), (TM, tmask, "TM"), (BLK, blk, "BLK"), (HM, hmask, "HM"), (ID, ident, "ID")):
            S.dma("sp", lambda e, t=t, src=src: e.dma_start(out=t[:], in_=src), writes=[k])
        gc = 0
        oi = 0
        for d in range(2):
            S.dma("sp", lambda e, d=d, hh=hh: e.dma_start(out=WG[:], in_=io["wg"][hh][d]), writes=["WG"])
            S.dma("sp", lambda e, d=d, hh=hh: e.dma_start(out=BG[:], in_=io["bg"][hh][d]), writes=["BG"])
            S.op("dve", lambda e: e.tensor_scalar(NBG[:], BG[:], -1.0, None, ALU.mult), reads=["BG"], writes=["NBG"])
            S.op("dve", lambda e: e.memset(SS[0][:], 0.0), writes=["SS0"])
            scur = 0
            for bi, (t0, T) in enumerate(S5_CH):
                nchk = T // 64
                n0 = t0 if d == 0 else _mirror(t0, T)[0]
                w0 = (n0 - SEQC) // 64
                b = (d * 9 + bi) % 2
                w = lambda n, b=b, T=T: Wb[n, b][:, :T]
                k = lambda n, b=b: "g_%s%d" % (n, b)
                w3 = lambda n, b=b, T=T: Wb[n, b][:, :T].rearrange("p (c s) -> p c s", s=64)
                S.dma("sp", lambda e, d=d, b=b, n0=n0, T=T, hh=hh: e.dma_start(out=Wb["Q", b][:, :T], in_=FMQ[hh * 128:(hh + 1) * 128, n0:n0 + T]), writes=[k("Q")])
                S.dma("act", lambda e, d=d, b=b, n0=n0, T=T, hh=hh: e.dma_start(out=Wb["K", b][:, :T], in_=FMK[hh * 128:(hh + 1) * 128, n0:n0 + T]), writes=[k("K")])
                S.dma("sp", lambda e, d=d, b=b, n0=n0, T=T: e.dma_start(out=Z[b][:, :T], in_=FMZ[d * 16:(d + 1) * 16, n0:n0 + T]), writes=["Z%d" % b])
                if t0 < SEQC:
                    S.dma("act", lambda e, b=b, nchk=nchk, cols=cols: e.dma_start(
                        out=VV[b][:, :nchk, :], in_=VT[0:SEQC, cols].rearrange("(c s) f -> s c f", s=64)), writes=["VV%d" % b])
                else:
                    S.dma("act", lambda e, b=b, w0=w0, cols=cols: e.dma_start(
                        out=VV[b][:, :, :], in_=VT[SEQC:, cols].rearrange("(r w) f -> r w f", w=64)[:, w0:w0 + 8, :]), writes=["VV%d" % b])
                S.op("pe", lambda e, b=b, T=T: e.matmul(pza[:, :T], WG[:], Z[b][:, :T], start=True, stop=True), reads=["WG", "Z%d" % b], writes=["pza"])
                S.op("act", lambda e, w=w, T=T: e.activation(w("E"), pza[:, :T], AF.Exp, bias=NBG[:], scale=-1.0), reads=["pza", "NBG"], writes=[k("E")])
                S.op("act", lambda e, w=w: e.activation(w("E"), w("E"), AF.Ln, bias=1.0), reads=[k("E")], writes=[k("E")])
                S.op("dve", lambda e, w=w: e.tensor_scalar(w("LA"), w("E"), -1.0 / 16.0, None, ALU.mult), reads=[k("E")], writes=[k("LA")])
                S.op("dve", lambda e, w=w, T=T: e.tensor_tensor_scan(w("B"), RST[:, :T], w("LA"), 0.0, ALU.mult, ALU.add), reads=["RST", k("LA")], writes=[k("B")])
                iref, ilast = (32, 63) if d == 0 else (31, 0)
                if d == 1:
                    S.op("dve", lambda e, w3=w3, nchk=nchk: e.tensor_tensor(w3("D"), w3("B")[:, :, 63:64].to_broadcast([128, nchk, 64]), w3("B"), ALU.subtract),
                         reads=[k("B")], writes=[k("D")])
                    S.op("dve", lambda e, w=w: e.tensor_tensor(w("B"), w("D"), w("LA"), ALU.add), reads=[k("D"), k("LA")], writes=[k("B")])
                S.op("act", lambda e, b=b, w3=w3, nchk=nchk, ilast=ilast: e.activation(DEC[b][:, :nchk], w3("B")[:, :, ilast], AF.Exp), reads=[k("B")], writes=["DEC%d" % b])
                S.op("act", lambda e, w=w: e.activation(w("E"), w("B"), AF.Exp), reads=[k("B")], writes=[k("E")])
                S.op("dve", lambda e, w=w: e.scalar_tensor_tensor(w("QE"), w("Q"), QSC, w("E"), ALU.mult, ALU.mult), reads=[k("Q"), k("E")], writes=[k("QE")])
                S.op("dve", lambda e, w3=w3, nchk=nchk, iref=iref: e.tensor_tensor(w3("D"), w3("B"), w3("B")[:, :, iref:iref + 1].to_broadcast([128, nchk, 64]), ALU.subtract),
                     reads=[k("B")], writes=[k("D")])
                S.op("act", lambda e, w=w: e.activation(w("E"), w("D"), AF.Exp), reads=[k("D"), k("QE")], writes=[k("E")])
                S.op("dve", lambda e, w=w: e.scalar_tensor_tensor(w("QS"), w("Q"), QSC, w("E"), ALU.mult, ALU.mult), reads=[k("Q"), k("E")], writes=[k("QS")])
                S.op("act", lambda e, w=w: e.activation(w("E"), w("D"), AF.Exp, scale=-1.0), reads=[k("D"), k("QS")], writes=[k("E")])
                S.op("dve", lambda e, w=w: e.tensor_tensor(w("LA"), w("K"), w("E"), ALU.mult), reads=[k("K"), k("E"), k("B")], writes=[k("LA")])
                for h in range(4):
                    S.op("pool", lambda e, w=w, h=h: e.tensor_scalar(w("KS%d" % h), w("LA"), HM[:, h:h + 1], None, ALU.mult),
                         reads=[k("LA"), "HM"], writes=[k("KS%d" % h)])
                S.op("dve", lambda e, w3=w3, nchk=nchk, ilast=ilast: e.tensor_tensor(w3("D"), w3("B")[:, :, ilast:ilast + 1].to_broadcast([128, nchk, 64]), w3("B"), ALU.subtract),
                     reads=[k("B"), k("E"), k("LA")], writes=[k("D")])
                S.op("act", lambda e, w=w: e.activation(w("D"), w("D"), AF.Exp), reads=[k("D")], writes=[k("D")])
                S.op("dve", lambda e, w=w: e.tensor_tensor(w("KD"), w("K"), w("D"), ALU.mult), reads=[k("K"), k("D")], writes=[k("KD")])
                for c in (range(nchk) if d == 0 else range(nchk - 1, -1, -1)):
                    p2 = gc % 2
                    gc += 1
                    cs = slice(c * 64, (c + 1) * 64)
                    S.op("pe", lambda e, b=b, cs=cs, p2=p2: e.transpose(pt[p2][:], Wb["KD", b][:, cs], ID[:]), reads=[k("KD"), "ID"], writes=["pt"])
                    S.op("act", lambda e, p2=p2: e.copy(KDT[p2][:], pt[p2][:]), reads=["pt"], writes=["KDT%d" % p2])
                    S.op("pe", lambda e, b=b, c=c, p2=p2: e.matmul(pkv[p2][:], KDT[p2][:], VV[b][:, c, :], start=True, stop=True),
                         reads=["KDT%d" % p2, "VV%d" % b], writes=["pkv%d" % p2])
                    for h in range(4):
                        S.op("pe", lambda e, b=b, cs=cs, p2=p2, h=h: e.matmul(psc[p2][:, h * 64:(h + 1) * 64], Wb["KS%d" % h, b][:, cs], Wb["QS", b][:, cs],
                                                                            start=True, stop=True),
                             reads=[k("KS%d" % h), k("QS")], writes=["psc%d" % p2])
                    S.op("dve", lambda e, p2=p2, d=d: e.tensor_tensor(STt[p2][:], psc[p2][:], (TM if d == 0 else TM2)[:], ALU.mult), reads=["psc%d" % p2, "TM", "TM2"], writes=["ST%d" % p2])
                    for h in range(4):
                        hs = slice(h * 64, (h + 1) * 64)
                        S.op("pe", lambda e, b=b, c=c, p2=p2, hs=hs: e.matmul(po[p2][:, hs], STt[p2][:, hs], VV[b][:, c, hs], start=True, stop=False),
                             reads=["ST%d" % p2, "VV%d" % b], writes=["po%d" % p2])
                        S.op("pe", lambda e, b=b, cs=cs, p2=p2, hs=hs, scur=scur: e.matmul(po[p2][:, hs], Wb["QE", b][:, cs], SS[scur][:, hs], start=False, stop=True),
                             reads=[k("QE"), "SS%d" % scur], writes=["po%d" % p2])
                    ob = oi % 3
                    oi += 1
                    S.op("act", lambda e, ob=ob, p2=p2: e.copy(OB[ob][:], po[p2][:]), reads=["po%d" % p2], writes=["OB%d" % ob])
                    if t0 < SEQC:
                        S.dma("sp" if oi % 2 else "act", lambda e, d=d, ob=ob, c=c, cols=cols: e.dma_start(out=OS[d][c * 64:(c + 1) * 64, cols], in_=OB[ob][:]),
                              reads=["OB%d" % ob], writes=["o_%d" % oi])
                    else:
                        S.dma("sp" if oi % 2 else "act", lambda e, d=d, ob=ob, c=c, w0=w0, cols=cols: e.dma_start(
                            out=OS[d][SEQC:, cols].rearrange("(r w) f -> r w f", w=64)[:, w0 + c, :], in_=OB[ob][:]),
                              reads=["OB%d" % ob], writes=["o_%d" % oi])
                    S.op("dve", lambda e, p2=p2: e.tensor_tensor(KVM[:], pkv[p2][:], BLK[:], ALU.mult), reads=["pkv%d" % p2, "BLK"], writes=["KVM"])
                    S.op("dve", lambda e, b=b, c=c, scur=scur: e.scalar_tensor_tensor(SS[1 - scur][:], SS[scur][:], DEC[b][:, c:c + 1], KVM[:], ALU.mult, ALU.add),
                         reads=["SS%d" % scur, "DEC%d" % b, "KVM"], writes=["SS%d" % (1 - scur)])
                    scur = 1 - scur
        S.sync_all()
        S.emit()
        sub.close()


def emit_D(nc, S, sb, ps, io, blocks, last):
    xT = io["xT"]; modT = io["MODS"]; TLs = io["TL"]
    su = TLs[:, 0:256]; sv = TLs[:, 256:512]; gg = TLs[:, 512:1024]
    s5u = io["FMU"]; yf = io["YF"]; yb = io["YB"]; of_ = io["OF"]; ob_ = io["OB"]
    w_out, wsT, sgub, s5d, wglu, ng, g2, wq = io["w_out"], io["wsT"], io["sgub"], io["s5d"], io["wglu"], io["ng"], io["g2"], io["wq"]
    keysT, eu, ev, gfin, ones, ident, iota16 = io["keysT"], io["eu"], io["ev"], io["gfin"], io["ones"], io["ident"], io["iota16"]
    xo = io["xo"]
    nb = len(blocks)
    MOD = sb("MOD", [128, 96]); WOUT = sb("WOUT", [128, 8, D]); WS = sb("WS", [128, 512]); SGUB = sb("SGUB", [128, 4]); S5D = sb("S5D", [128, 2])
    WGLU = sb("WGLU", [128, 2, 512]); NG = sb("NG", [128, 512]); G2 = sb("G2", [128, 8]); RR_ = sb("RR_", [128, 16384]); KEYS = sb("KEYS", [128, 2048])
    GF = sb("GF", [128, 8]); ONES = sb("ONES", [128, 128]); ID = sb("ID", [128, 128]); IOTA = sb("IOTA", [128, 16]); IO128 = sb("IO128", [128, 128])
    A2 = sb("A2", [128, 16]); TA = sb("TA", [128, 16])
    U_ = sb("U_", [128, 256]); V_ = sb("V_", [128, 256]); GU = sb("GU", [128, 256]); GV = sb("GV", [128, 256]); SQ = sb("SQ", [128, 512])
    SS = sb("SS", [128, 8]); VN = sb("VN", [128, 256]); MIX = sb("MIX", [128, D])
    S5U = sb("S5U", [128, 2, 128]); YF = sb("YF", [128, 2, 128]); YB = sb("YB", [128, 2, 128]); GE = sb("GE", [128, 2, 128]); SG = sb("SG", [128, 256])
    OF = sb("OF", [128, 512]); OB = sb("OB", [128, 512]); GG = sb("GG", [128, 512]); SL = sb("SL", [128, 512])
    XT = sb("XT", [128, 8, 128]); X1 = sb("X1", [128, 8, 128]); XO = XT; HT = sb("HT", [128, 8, 128]); MIXT = HT; HTOK = sb("HTOK", [128, D])
    RSTD = sb("RSTD", [128, 128]); TMPB = sb("TMPB", [128, 128])
    SC = sb("SC", [128, 2048])
    M16 = sb("M16", [128, 256]); I16 = sb("I16", [128, 256], U32); IF16 = sb("IF16", [128, 256]); I1S = sb("I1S", [128, 128])
    CS = sb("CS", [128, 2048]); SC2 = CS; QT = CS[:].rearrange("p (q t) -> p q t", t=128); CS2 = sb("CS2", [128, 256])
    T16 = sb("T16", [128, 128]); P16 = sb("P16", [128, 128], U32); PF = sb("PF", [128, 128]); AI = sb("AI", [128, 128], I32)
    AFL = sb("AFL", [128, 128]); BFL = sb("BFL", [128, 128]); E1 = sb("E1", [128, 128]); E2 = sb("E2", [128, 128])
    EG = sb("EG", [128, 128]); GATE = sb("GATE", [128, 128]); IDXTI = sb("IDXTI", [128, 128], I32); GATET = sb("GATET", [128, 128])
    ACTT = sb("ACTT", [128, 128]); WT = sb("WT", [128, 128])
    NBUF = 8
    WQ = RR_[:].rearrange("p (k f) -> p k f", f=2048)
    UG = [RR_[:, i * 1024:(i + 1) * 1024] for i in range(NBUF)]; VG = UG
    HB = [sb("HB%d" % i, [128, D]) for i in range(2)]
    P0 = ps("P0", [128, 2048]); P1 = ps("P1", [128, 1024]); P2 = ps("P2", [128, 512]); P3 = ps("P3", [128, 512])

    def V(fn, r=(), w=()):
        S.op("dve", fn, reads=r, writes=w)

    def A(fn, r=(), w=()):
        S.op("act", fn, reads=r, writes=w)

    def PE(fn, r=(), w=()):
        S.op("pe", fn, reads=r, writes=w)

    def LD(q, t, src, key):
        S.dma(q, lambda e: e.dma_start(out=t, in_=src), writes=(key if isinstance(key, list) else [key]))

    LD("sp", MOD[:], modT, "MOD"); LD("sp", WS[:], wsT, "WS"); LD("sp", SGUB[:], sgub, "SGUB"); LD("sp", S5D[:], s5d, "S5D")
    LD("sp", WGLU[:], wglu.rearrange("(c p) f -> p c f", p=128), "WGLU"); LD("sp", NG[:], ng, "NG"); LD("sp", G2[:], g2, "G2")
    LD("sp", KEYS[:], keysT, "KEYS"); LD("sp", GF[:], gfin, "GF"); LD("sp", ONES[:], ones, "ONES"); LD("sp", ID[:], ident, "ID"); LD("sp", IOTA[:], iota16, "IOTA"); LD("sp", IO128[:], io["iota128"], "IO128")
    wo_v = w_out.rearrange("(k p) f -> p k f", p=128)
    wq_v = wq.rearrange("(k p) f -> p k f", p=128)
    for k in range(8):
        LD("act", WOUT[:, k, :], wo_v[:, k, :], "WOUT")
    MOD3 = MOD[:].rearrange("p (j c) -> p j c", c=2)
    V(lambda e: e.tensor_scalar(TA[:].rearrange("p (j c) -> p j c", c=2), MOD3[:, 32:40, :], 1.0, None, ALU.add), ["MOD"], ["TA"])
    V(lambda e: e.tensor_tensor(A2[:].rearrange("p (j c) -> p j c", c=2), TA[:].rearrange("p (j c) -> p j c", c=2),
                                G2[:].unsqueeze(2).to_broadcast([128, 8, 2]), ALU.mult), ["TA", "G2"], ["A2"])
    A23 = A2[:].rearrange("p (j c) -> p j c", c=2)

    def rs_from_ss(ss, n, scale):
        V(lambda e: e.tensor_scalar(ss, ss, scale, EPS, ALU.mult, ALU.add), ["SS"], ["SS"])
        A(lambda e: e.sqrt(ss, ss), ["SS"], ["SS"])
        V(lambda e: e.reciprocal(ss, ss), ["SS"], ["SS"])

    def top16(src, scratch, mout, iout, n):
        V(lambda e: e.max(mout[:, 0:8], src), ["TK"], ["TK"])
        V(lambda e: e.max_index(iout[:, 0:8], mout[:, 0:8], src), ["TK"], ["TK"])
        V(lambda e: e.match_replace(scratch, mout[:, 0:8], src, -1e30), ["TK"], ["TK"])
        V(lambda e: e.max(mout[:, 8:16], scratch), ["TK"], ["TK"])
        V(lambda e: e.max_index(iout[:, 8:16], mout[:, 8:16], scratch), ["TK"], ["TK"])

    xT_v = xT.rearrange("(k p) t -> p k t", p=128)
    xo_v = xo.rearrange("(k p) t -> p k t", p=128)
    s5u_v = s5u.rearrange("(c p) t -> p c t", p=128)
    yf_v = yf.rearrange("(c p) t -> p c t", p=128)
    yb_v = yb.rearrange("(c p) t -> p c t", p=128)
    for bi in range(nb):
        sq0, r0_, oc0, isctx = blocks[bi]
        col = 1 if isctx else 0
        tk = slice(sq0, sq0 + 128)
        tr = slice(r0_, r0_ + 128)
        to = slice(oc0, oc0 + 128)
        LD("sp", U_[:], su[tr, :], "U_"); LD("sp", V_[:], sv[tr, :], "V_"); LD("sp", GG[:], gg[tr, :], "GG")
        LD("act", S5U[:], s5u_v[:, :, tk], "S5U"); LD("act", YF[:], yf_v[:, :, tk], "YF"); LD("act", YB[:], yb_v[:, :, tk], "YB")
        LD("sp", OF[:], of_[tk, :], "OF"); LD("sp", OB[:], ob_[tk, :], "OB"); LD("act", XT[:], xT_v[:, :, tk], "XT")
        for k in range(8):
            LD("act" if k % 2 else "sp", WQ[:, k, :], wq_v[:, k, :], ["R%d" % (2 * k), "R%d" % (2 * k + 1)])
        A(lambda e: e.activation(GU[:], U_[:], AF.Gelu), ["U_"], ["GU"])
        A(lambda e: e.activation(GV[:], V_[:], AF.Gelu), ["V_"], ["GV"])
        V(lambda e: e.tensor_tensor(SQ[:, 0:256], GV[:], GV[:], ALU.mult), ["GV"], ["SQ"])
        V(lambda e: e.tensor_reduce(SS[:, 0:4], SQ[:, 0:256].rearrange("p (h d) -> p h d", d=64), AX.X, ALU.add), ["SQ"], ["SS"])
        rs_from_ss(SS[:, 0:4], 4, 1.0 / 64)
        V(lambda e: e.tensor_tensor(VN[:].rearrange("p (h d) -> p h d", d=64), GV[:].rearrange("p (h d) -> p h d", d=64),
                                    SS[:, 0:4].unsqueeze(2).to_broadcast([128, 4, 64]), ALU.mult), ["GV", "SS"], ["VN"])
        for h in range(4):
            PE(lambda e, h=h: e.matmul(P2[:, h * 64:(h + 1) * 64], WS[:, h * 128:(h + 1) * 128], VN[:, h * 64:(h + 1) * 64], start=True, stop=True),
               ["WS", "VN"], ["P2"])
        V(lambda e: e.tensor_tensor(MIX[:, 0:256].rearrange("p (h d) -> p h d", d=64), P2[:, 0:256].rearrange("p (h d) -> p h d", d=64),
                                    SGUB[:].unsqueeze(2).to_broadcast([128, 4, 64]), ALU.add), ["P2", "SGUB"], ["MIXa"])
        V(lambda e: e.tensor_tensor(MIX[:, 0:256], MIX[:, 0:256], GU[:], ALU.mult), ["GU"], ["MIXa"])
        V(lambda e: e.tensor_tensor(YF[:], YF[:], YB[:], ALU.add), ["YB"], ["YF"])
        for ct in range(2):
            V(lambda e, ct=ct: e.scalar_tensor_tensor(YF[:, ct, :], S5U[:, ct, :], S5D[:, ct:ct + 1], YF[:, ct, :], ALU.mult, ALU.add),
              ["S5U", "S5D"], ["YF"])
        A(lambda e: e.activation(GE[:], YF[:], AF.Gelu), ["YF"], ["GE"])
        for ct in range(2):
            PE(lambda e, ct=ct: e.matmul(P3[:, 0:512], GE[:, ct, :], WGLU[:, ct, :], start=(ct == 0), stop=(ct == 1)), ["GE", "WGLU"], ["P3"])
        A(lambda e: e.activation(SG[:], P3[:, 256:512], AF.Sigmoid), ["P3"], ["SG"])
        V(lambda e: e.tensor_tensor(MIX[:, 256:512], P3[:, 0:256], SG[:], ALU.mult), ["P3", "SG"], ["MIXb"])
        V(lambda e: e.tensor_tensor(OF[:], OF[:], OB[:], ALU.add), ["OB"], ["OF"])
        V(lambda e: e.tensor_tensor(SQ[:], OF[:], OF[:], ALU.mult), ["OF"], ["SQ"])
        V(lambda e: e.tensor_reduce(SS[:, 0:8], SQ[:].rearrange("p (h d) -> p h d", d=64), AX.X, ALU.add), ["SQ"], ["SS"])
        rs_from_ss(SS[:, 0:8], 8, 1.0 / 64)
        V(lambda e: e.tensor_tensor(OF[:].rearrange("p (h d) -> p h d", d=64), OF[:].rearrange("p (h d) -> p h d", d=64),
                                    SS[:, 0:8].unsqueeze(2).to_broadcast([128, 8, 64]), ALU.mult), ["SS"], ["OF"])
        V(lambda e: e.tensor_tensor(OF[:], OF[:], NG[:], ALU.mult), ["NG"], ["OF"])
        A(lambda e: e.activation(SL[:], GG[:], AF.Silu), ["GG"], ["SL"])
        V(lambda e: e.tensor_tensor(MIX[:, 512:1024], OF[:], SL[:], ALU.mult), ["OF", "SL"], ["MIXc"])
        for f in range(8):
            pp, pk = (P2, "P2") if f % 2 == 0 else (P3, "P3")
            PE(lambda e, f=f, pp=pp: e.transpose(pp[:, 0:128], MIX[:, f * 128:(f + 1) * 128], ID[:]), ["MIXa", "MIXb", "MIXc", "ID"], [pk])
            A(lambda e, f=f, pp=pp: e.copy(MIXT[:, f, :], pp[:, 0:128]), [pk], ["HT"])
        for ot in range(8):
            pp, pk = (P2, "P2") if ot % 2 == 0 else (P3, "P3")
            for k in range(8):
                PE(lambda e, ot=ot, k=k, pp=pp: e.matmul(pp[:, 0:128], WOUT[:, k, ot * 128:(ot + 1) * 128], MIXT[:, k, :], start=(k == 0), stop=(k == 7)),
                   ["WOUT", "HT"], [pk])
            V(lambda e, ot=ot, pp=pp, col=col: e.scalar_tensor_tensor(X1[:, ot, :], pp[:, 0:128], MOD3[:, 16 + ot, col:col + 1], XT[:, ot, :], ALU.mult, ALU.add),
              [pk, "MOD", "XT"], ["X1"])
        for k in range(8):
            A(lambda e, k=k: e.activation(TMPB[:], X1[:, k, :], AF.Square), ["X1"], ["TMPB"])
            PE(lambda e, k=k: e.matmul(P2[:, 0:128], ONES[:], TMPB[:], start=(k == 0), stop=(k == 7)), ["ONES", "TMPB"], ["P2"])
        V(lambda e: e.tensor_scalar(RSTD[:], P2[:, 0:128], 1.0 / D, EPS, ALU.mult, ALU.add), ["P2"], ["RSTD"])
        A(lambda e: e.sqrt(RSTD[:], RSTD[:]), ["RSTD"], ["RSTD"])
        V(lambda e: e.reciprocal(RSTD[:], RSTD[:]), ["RSTD"], ["RSTD"])
        for k in range(8):
            V(lambda e, k=k: e.tensor_tensor(TMPB[:], X1[:, k, :], RSTD[:], ALU.mult), ["X1", "RSTD"], ["TMPB"])
            A(lambda e, k=k, col=col: e.activation(HT[:, k, :], TMPB[:], AF.Identity, bias=MOD3[:, 24 + k, col:col + 1], scale=A23[:, k, col:col + 1]),
              ["TMPB", "MOD", "A2"], ["HT"])
        for k in range(8):
            pp, pk = (P2, "P2") if k % 2 == 0 else (P3, "P3")
            PE(lambda e, k=k, pp=pp: e.transpose(pp[:, 0:128], HT[:, k, :], ID[:]), ["HT", "ID"], [pk])
            A(lambda e, k=k, pp=pp: e.copy(HTOK[:, k * 128:(k + 1) * 128], pp[:, 0:128]), [pk], ["HTOK"])
        for qt in range(16):
            pp, pk = (P2, "P2") if qt % 2 == 0 else (P3, "P3")
            for k in range(8):
                PE(lambda e, qt=qt, k=k, pp=pp: e.matmul(pp[:, 0:128], WQ[:, k, qt * 128:(qt + 1) * 128], HT[:, k, :], start=(k == 0), stop=(k == 7)),
                   ["R%d" % (2 * k), "R%d" % (2 * k + 1), "HT"], [pk])
            if qt % 2 == 0:
                A(lambda e, qt=qt, pp=pp: e.copy(QT[:, qt, :], pp[:, 0:128]), [pk], ["TK"])
            else:
                V(lambda e, qt=qt, pp=pp: e.tensor_copy(QT[:, qt, :], pp[:, 0:128]), [pk], ["TK"])
        for qt in range(16):
            PE(lambda e, qt=qt: e.matmul(P0[:, qt * 128:(qt + 1) * 128], QT[:, qt, :], KEYS[:, qt * 128:(qt + 1) * 128], start=True, stop=True),
               ["TK", "KEYS"], ["P0a", "P0b"])
        for q4 in range(4):
            A(lambda e, q4=q4: e.copy(SC[:, q4 * 512:(q4 + 1) * 512], P0[:, q4 * 512:(q4 + 1) * 512]), ["P0a", "P0b"], ["TK"])
        for qt in range(16):
            top16(SC[:, qt * 128:(qt + 1) * 128], SC2[:, qt * 128:(qt + 1) * 128], M16[:, qt * 16:(qt + 1) * 16], I16[:, qt * 16:(qt + 1) * 16], 128)
        V(lambda e: e.tensor_copy(IF16[:], I16[:]), ["TK"], ["TK"])
        M4 = M16[:].rearrange("p (h q k) -> p h q k", q=2, k=16)
        IF4 = IF16[:].rearrange("p (h q k) -> p h q k", q=2, k=16)
        I1S3 = I1S[:].rearrange("p (h k) -> p h k", k=16)
        V(lambda e: e.tensor_scalar(I1S3, IF4[:, :, 0, :], 128.0, None, ALU.mult), ["TK"], ["TK"])
        CS4 = CS[:].rearrange("p (h a b) -> p h a b", a=16, b=16)
        V(lambda e: e.tensor_tensor(CS4, M4[:, :, 0, :].unsqueeze(3).to_broadcast([128, 8, 16, 16]),
                                    M4[:, :, 1, :].unsqueeze(2).to_broadcast([128, 8, 16, 16]), ALU.add), ["TK"], ["TK"])
        for h in range(8):
            top16(CS[:, h * 256:(h + 1) * 256], CS2[:], T16[:, h * 16:(h + 1) * 16], P16[:, h * 16:(h + 1) * 16], 256)
        V(lambda e: e.tensor_copy(PF[:], P16[:]), ["TK"], ["TK"])
        V(lambda e: e.tensor_scalar(AFL[:], PF[:], -7.5, 1.0 / 16, ALU.add, ALU.mult), ["TK"], ["TK"])
        V(lambda e: e.tensor_copy(AI[:], AFL[:]), ["TK"], ["TK"])
        V(lambda e: e.tensor_copy(AFL[:], AI[:]), ["TK"], ["TK"])
        V(lambda e: e.scalar_tensor_tensor(BFL[:], AFL[:], -16.0, PF[:], ALU.mult, ALU.add), ["TK"], ["TK"])
        EQ4 = CS[:].rearrange("p (h k a) -> p h k a", k=16, a=16)
        io4 = IOTA[:].unsqueeze(1).unsqueeze(1).to_broadcast([128, 8, 16, 16])
        for (sel, src, dst) in ((AFL, I1S3, E1), (BFL, IF4[:, :, 1, :], E2)):
            V(lambda e, sel=sel: e.tensor_tensor(EQ4, io4, sel[:].rearrange("p (h k) -> p h k", k=16).unsqueeze(3).to_broadcast([128, 8, 16, 16]), ALU.is_equal),
              ["TK", "IOTA"], ["TK"])
            V(lambda e, src=src: e.tensor_tensor(EQ4, EQ4, src.unsqueeze(2).to_broadcast([128, 8, 16, 16]), ALU.mult), ["TK"], ["TK"])
            V(lambda e, dst=dst: e.tensor_reduce(dst[:], CS[:].rearrange("p (m a) -> p m a", a=16), AX.X, ALU.add), ["TK"], ["TK"])
        V(lambda e: e.tensor_tensor(E1[:], E1[:], E2[:], ALU.add), ["TK"], ["TK"])
        T3 = T16[:].rearrange("p (h k) -> p h k", k=16)
        V(lambda e: e.tensor_tensor(EG[:].rearrange("p (h k) -> p h k", k=16), T3, T3[:, :, 0:1].to_broadcast([128, 8, 16]), ALU.subtract), ["TK"], ["TK"])
        A(lambda e: e.activation(EG[:], EG[:], AF.Exp), ["TK"], ["TK"])
        V(lambda e: e.tensor_reduce(SS[:, 0:8], EG[:].rearrange("p (h k) -> p h k", k=16), AX.X, ALU.add), ["TK"], ["SS"])
        V(lambda e: e.reciprocal(SS[:, 0:8], SS[:, 0:8]), ["SS"], ["SS"])
        V(lambda e: e.tensor_tensor(GATE[:].rearrange("p (h k) -> p h k", k=16), EG[:].rearrange("p (h k) -> p h k", k=16),
                                    SS[:, 0:8].unsqueeze(2).to_broadcast([128, 8, 16]), ALU.mult), ["TK", "SS"], ["GATE"])
        V(lambda e: e.tensor_copy(IDXTI[:], E1[:]), ["TK"], ["IDXTI"])
        for j in range(128):
            g = j % NBUF
            S.dma("pool", lambda e, j=j, g=g: e.indirect_dma_start(out=UG[g], out_offset=None, in_=eu,
                                                                   in_offset=bass.IndirectOffsetOnAxis(ap=IDXTI[:, j:j + 1], axis=0)),
                  reads=["IDXTI"], writes=["R%d" % g])
            V(lambda e, j=j, g=g: e.scalar_tensor_tensor(UG[g], UG[g], 1.0, HTOK[:], ALU.mult, ALU.mult, accum_out=ACTT[:, j:j + 1]),
              ["HTOK"], ["R%d" % g, "ACTT"])
        A(lambda e: e.activation(WT[:], ACTT[:], AF.Gelu), ["ACTT"], ["WT"])
        V(lambda e: e.tensor_tensor(WT[:], WT[:], GATE[:], ALU.mult), ["GATE"], ["WT"])
        ACC = HB[0]
        for j in range(128):
            g = j % NBUF
            S.dma("pool", lambda e, j=j, g=g: e.indirect_dma_start(out=VG[g], out_offset=None, in_=ev,
                                                                   in_offset=bass.IndirectOffsetOnAxis(ap=IDXTI[:, j:j + 1], axis=0)),
                  reads=["IDXTI"], writes=["R%d" % g])
            if j == 0:
                V(lambda e, g=g: e.tensor_scalar(ACC[:], VG[g], WT[:, 0:1], None, ALU.mult), ["R%d" % g, "WT"], ["HB0"])
            else:
                V(lambda e, j=j, g=g: e.scalar_tensor_tensor(ACC[:], VG[g], WT[:, j:j + 1], ACC[:], ALU.mult, ALU.add), ["R%d" % g, "WT"], ["HB0"])
        for ot in range(8):
            pp, pk = (P2, "P2") if ot % 2 == 0 else (P3, "P3")
            PE(lambda e, ot=ot, pp=pp: e.transpose(pp[:, 0:128], ACC[:, ot * 128:(ot + 1) * 128], ID[:]), ["HB0", "ID"], [pk])
            V(lambda e, ot=ot, col=col, pp=pp: e.scalar_tensor_tensor(XO[:, ot, :], pp[:, 0:128], MOD3[:, 40 + ot, col:col + 1], X1[:, ot, :],
                                                                      ALU.mult, ALU.add), [pk, "MOD", "X1"], ["XT"])
        if last:
            for k in range(8):
                A(lambda e, k=k: e.activation(TMPB[:], XO[:, k, :], AF.Square), ["XT"], ["TMPB"])
                PE(lambda e, k=k: e.matmul(P2[:, 0:128], ONES[:], TMPB[:], start=(k == 0), stop=(k == 7)), ["ONES", "TMPB"], ["P2"])
            V(lambda e: e.tensor_scalar(RSTD[:], P2[:, 0:128], 1.0 / D, EPS, ALU.mult, ALU.add), ["P2"], ["RSTD"])
            A(lambda e: e.sqrt(RSTD[:], RSTD[:]), ["RSTD"], ["RSTD"])
            V(lambda e: e.reciprocal(RSTD[:], RSTD[:]), ["RSTD"], ["RSTD"])
            for k in range(8):
                V(lambda e, k=k: e.scalar_tensor_tensor(XO[:, k, :], XO[:, k, :], GF[:, k:k + 1], RSTD[:], ALU.mult, ALU.mult), ["RSTD", "GF"], ["XT"])
        S.dma("sp", lambda e, to=to: e.dma_start(out=xo_v[:, :, to], in_=XO[:]), reads=["XT"], writes=["xo_%d" % bi])


def build_layer(last, dbg=False):
    nc = bass.Bass("TRN2", target_bir_lowering=False)
    io = {}

    def din(name, shape, dt=F32):
        io[name] = nc.dram_tensor(name, shape, dt, kind="ExternalInput").ap()

    def scr(name, shape):
        io[name] = nc.dram_tensor(name, shape, F32, kind=("ExternalOutput" if dbg else "Internal")).ap()
    nctx = 0 if last else 128
    ntok = 2048 + nctx
    din("xT", [D, SEQT]); din("cT", [128, 16]); din("w_mod", [D, NMOD * D]); din("b_mod", [128, 48]); din("g1", [128, 8]); din("w_in", [D, INW])
    din("ones", [128, 128]); din("ident", [128, 128]); din("tau", [128, 512]); din("iota16", [128, 16]); din("iota128", [128, 128])
    din("prm", [2, 128, 24]); din("bre", [2, 128, 128]); din("bim", [2, 128, 128]); din("cre", [2, 128, 128]); din("cim", [2, 128, 128])
    din("wg", [2, 2, 16, 128]); din("bg", [2, 2, 128, 1])
    din("rst", [128, 512]); din("tmask", [64, 256]); din("tmask2", [64, 256]); din("blk", [128, 256]); din("hmask", [128, 4])
    din("w_out", [D, D]); din("wsT", [128, 512]); din("sgub", [128, 4]); din("s5d", [128, 2]); din("wglu", [256, 512]); din("ng", [128, 512])
    din("g2", [128, 8]); din("wq", [D, 2048]); din("keysT", [128, 2048]); din("eu", [NEXP, D]); din("ev", [NEXP, D]); din("gfin", [128, 8])
    scr("FMU", [256, SEQT]); scr("FMQ", [256, SEQT]); scr("FMK", [256, SEQT]); scr("FMZ", [32, SEQT]); scr("VT", [SEQT, 512]); scr("TL", [OWN, 1024])
    scr("MODS", [128, 96]); scr("YF", [256, SEQT]); scr("YB", [256, SEQT]); scr("OF", [SEQT, 512]); scr("OB", [SEQT, 512]); scr("GS", [2, 16])
    io["xo"] = nc.dram_tensor("xo", [D, ntok], F32, kind="ExternalOutput").ap()
    with ExitStack() as top:
        gate = top.enter_context(nc.semaphore("gate"))
        ph = [0]

        def phase(fn):
            with ExitStack() as st:
                S = Sched(nc, top, gate, 16 * ph[0])
                sb, ps = _mk(nc, st)
                fn(S, sb, ps)
                S.drain_all("sp")
                GS = io["GS"]
                S.prog["sp"].append(("i", lambda e: e.dma_start(out=GS[0:1, :], in_=GS[1:2, :]), "gate", 16))
                S.emit()
            ph[0] += 1
        phase(lambda S, sb, ps: emit_A(nc, S, sb, ps, io))
        phase(lambda S, sb, ps: emit_B(nc, S, sb, ps, io))
        phase(lambda S, sb, ps: emit_C(nc, S, sb, ps, io))
        if last:
            blocks = [(SEQC + i * 128, 128 + i * 128, i * 128, False) for i in range(16)]
        else:
            blocks = [(0, 0, 0, True)] + [(SEQC + i * 128, 128 + i * 128, 128 + i * 128, False) for i in range(16)]
        phase(lambda S, sb, ps: emit_D(nc, S, sb, ps, io, blocks, last))
    return nc


def _lay_vec(v, n):
    return np.ascontiguousarray(np.asarray(v, np.float32).reshape(n, 128).T)


_CONST = {}


def _consts():
    if not _CONST:
        rst = np.ones((128, 512), np.float32)
        rst[:, ::64] = 0
        tm = (np.arange(64)[None, :] >= np.arange(64)[:, None]).astype(np.float32)
        _CONST.update(
            ones=np.ones((128, 128), np.float32), ident=np.eye(128, dtype=np.float32),
            iota16=np.ascontiguousarray(np.broadcast_to(np.arange(16, dtype=np.float32), (128, 16))),
            iota128=np.ascontiguousarray(np.broadcast_to(np.arange(128, dtype=np.float32), (128, 128))),
            tau=np.ascontiguousarray(np.broadcast_to(np.arange(1, 513, dtype=np.float32), (128, 512))),
            rst=rst, tmask=np.ascontiguousarray(np.tile(tm, (1, 4))), tmask2=np.ascontiguousarray(np.tile(tm.T, (1, 4))),
            blk=np.kron(np.eye(4, dtype=np.float32), np.ones((32, 64), np.float32)),
            hmask=np.kron(np.eye(4, dtype=np.float32), np.ones((32, 1), np.float32)))
    return _CONST


def _pb_params(P, l, gh, swap):
    def stt(a):
        if swap:
            a = a[::-1]
        g = a[:, gh * 8:gh * 8 + 8]
        g = g.reshape((2, 4, 2, 64) + a.shape[3:])
        g = np.moveaxis(g, (2, 3), (0, 1))
        return np.ascontiguousarray(g.reshape((128, 2, 4) + a.shape[3:]))
    lre = stt(P["s5_lambda_re"][l]); lim = stt(P["s5_lambda_im"][l])
    ls = stt(np.broadcast_to(P["s5_log_step"][l][:, :, None], (2, 16, 64)))
    prm = np.ascontiguousarray(np.stack([lre, lim, ls], -1).reshape(128, 24).astype(np.float32))
    return dict(prm=prm, bre=stt(P["s5_b_re"][l]).reshape(128, 128), bim=stt(P["s5_b_im"][l]).reshape(128, 128),
                cre=stt(np.swapaxes(P["s5_c_re"][l], 2, 3)).reshape(128, 128), cim=stt(np.swapaxes(P["s5_c_im"][l], 2, 3)).reshape(128, 128))


def _layer_weights(P, l, swap):
    C = _consts()
    sk = P["peer_sub_keys"][l]
    keysT = np.zeros((128, 16, 128), np.float32)
    for h in range(8):
        for p in range(2):
            keysT[:, h * 2 + p, :] = sk[p, h].T
    sw = P["sgu_w"][l]
    sbb = P["sgu_b"][l]
    wgate = P["gla_w_gate"][l]
    bgate = P["gla_b_gate"][l]
    if swap:
        sw = sw[:, ::-1, ::-1]
        sbb = sbb[:, ::-1]
        wgate = wgate[::-1]
        bgate = bgate[::-1]
    pb = [_pb_params(P, l, ct, swap) for ct in range(2)]
    w_in = P["w_in"][l]
    if swap:
        w_in = np.ascontiguousarray(np.concatenate([w_in[:, :2304], w_in[:, 2320:2336], w_in[:, 2304:2320]], 1))
    W = dict(w_mod=P["w_mod"][l], b_mod=_lay_vec(P["b_mod"][l], 48), g1=_lay_vec(P["norm1_g"][l], 8), w_in=w_in,
             w_out=P["w_out"][l], wsT=np.ascontiguousarray(np.transpose(sw, (2, 0, 1)).reshape(128, 512)),
             sgub=np.ascontiguousarray(sbb.T), s5d=_lay_vec(P["s5_d"][l], 2), wglu=P["s5_w_glu"][l],
             ng=np.ascontiguousarray(np.broadcast_to(P["gla_norm_g"][l], (128, 512))), g2=_lay_vec(P["norm2_g"][l], 8),
             wq=P["peer_w_query"][l], keysT=keysT.reshape(128, 2048), eu=P["peer_expert_u"][l], ev=P["peer_expert_v"][l],
             gfin=_lay_vec(P["final_norm_g"], 8),
             wg=np.ascontiguousarray(np.stack([[wgate[d][:, hh * 128:hh * 128 + 128] for d in range(2)] for hh in range(2)])),
             bg=np.ascontiguousarray(np.stack([[bgate[d][hh * 128:hh * 128 + 128][:, None] for d in range(2)] for hh in range(2)])))
    for k in ("prm", "bre", "bim", "cre", "cim"):
        W[k] = np.ascontiguousarray(np.stack([pb[0][k], pb[1][k]]))
    W.update(C)
    return W


LAYER_W = ["w_mod", "b_mod", "g1", "w_in", "prm", "bre", "bim", "cre", "cim", "wg", "bg", "w_out", "wsT", "sgub", "s5d", "wglu", "ng", "g2",
           "wq", "keysT", "eu", "ev"]
LAYER_W_SHAPES = dict(w_mod=[D, NMOD * D], b_mod=[128, 48], g1=[128, 8], w_in=[D, INW], prm=[2, 128, 24], bre=[2, 128, 128], bim=[2, 128, 128],
                      cre=[2, 128, 128], cim=[2, 128, 128], wg=[2, 2, 16, 128], bg=[2, 2, 128, 1], w_out=[D, D], wsT=[128, 512], sgub=[128, 4],
                      s5d=[128, 2], wglu=[256, 512], ng=[128, 512], g2=[128, 8], wq=[D, 2048], keysT=[128, 2048], eu=[NEXP, D], ev=[NEXP, D])


def build_full():
    nc = bass.Bass("TRN2", target_bir_lowering=False)
    io = {}

    def din(name, shape, dt=F32):
        io[name] = nc.dram_tensor(name, shape, dt, kind="ExternalInput").ap()

    def scr(name, shape):
        io[name] = nc.dram_tensor(name, shape, F32, kind="Internal").ap()
    din("xT", [D, SEQT]); din("cT", [128, 16]); din("gfin", [128, 8])
    din("ones", [128, 128]); din("ident", [128, 128]); din("tau", [128, 512]); din("iota16", [128, 16]); din("iota128", [128, 128])
    din("rst", [128, 512]); din("tmask", [64, 256]); din("tmask2", [64, 256]); din("blk", [128, 256]); din("hmask", [128, 4])
    for l in range(2):
        for n in LAYER_W:
            din("%s_%d" % (n, l), LAYER_W_SHAPES[n])
    scr("FMU", [256, SEQT]); scr("FMQ", [256, SEQT]); scr("FMK", [256, SEQT]); scr("FMZ", [32, SEQT]); scr("VT", [SEQT, 512]); scr("TL", [SEQT, 1024])
    scr("MODS", [128, 96]); scr("YF", [256, SEQT]); scr("YB", [256, SEQT]); scr("OF", [SEQT, 512]); scr("OB", [SEQT, 512]); scr("GS", [2, 16])
    scr("X1S", [D, SEQT])
    io["xo"] = nc.dram_tensor("xo", [D, 2048], F32, kind="ExternalOutput").ap()
    with ExitStack() as top:
        gate = top.enter_context(nc.semaphore("gate"))
        ph = [0]

        def phase(fn, nds=None):
            with ExitStack() as st:
                S = Sched(nc, top, gate, 16 * ph[0], nds=nds)
                sb, ps = _mk(nc, st)
                fn(S, sb, ps)
                S.drain_all("sp")
                GS = io["GS"]
                S.prog["sp"].append(("i", lambda e: e.dma_start(out=GS[0:1, :], in_=GS[1:2, :]), "gate", 16))
                S.emit()
            ph[0] += 1
        for l in range(2):
            last = l == 1
            iol = dict(io)
            for n in LAYER_W:
                iol[n] = io["%s_%d" % (n, l)]
            if last:
                iol["xT"] = io["X1S"]
                blocks = [(SEQC + i * 128, 128 + i * 128, i * 128, False) for i in range(16)]
            else:
                iol["xo"] = io["X1S"]
                blocks = [(0, 0, 0, True), (128, 128, 128, True)] + [(SEQC + i * 128, SEQC + i * 128, SEQC + i * 128, False) for i in range(32)]
            n2 = dict(sp=2, act=2, pool=2)
            phase(lambda S, sb, ps: emit_A(nc, S, sb, ps, iol, tl_all=not last), nds=n2)
            phase(lambda S, sb, ps: emit_B(nc, S, sb, ps, iol), nds=n2)
            phase(lambda S, sb, ps: emit_C(nc, S, sb, ps, iol), nds=n2)
            phase(lambda S, sb, ps: emit_D(nc, S, sb, ps, iol, blocks, last), nds=dict(sp=2, act=2, pool=7))
    return nc


_PROG = {}


def _prog(name, fn):
    if name not in _PROG:
        _PROG[name] = fn()
    return _PROG[name]


def kernel(**inputs):
    P = {k: np.ascontiguousarray(np.asarray(v)) for k, v in inputs.items()}
    cores = list(range(8))
    C = _consts()
    ncF = _prog("F", build_full)
    WL = {(l, sw): _layer_weights(P, l, sw) for l in range(2) for sw in (False, True)}
    in_maps = []
    for core in cores:
        b, half = divmod(core, 2)
        xc, xl = P["ctx"][b], P["x"][b]
        seq = np.concatenate([xc, xl], 0) if half == 0 else np.concatenate([xc[::-1], xl[::-1]], 0)
        cT = np.stack([_lay_vec(P["c"][b], 8), _lay_vec(P["c_ctx"], 8)], -1).reshape(128, 16)
        m = dict(xT=np.ascontiguousarray(seq.T), cT=np.ascontiguousarray(cT), gfin=_lay_vec(P["final_norm_g"], 8))
        m.update(C)
        for l in range(2):
            W = WL[l, half == 1]
            for n in LAYER_W:
                m["%s_%d" % (n, l)] = W[n]
        in_maps.append(m)
    rF = run_bass_kernel_spmd(ncF, in_maps, core_ids=cores).results
    out = np.zeros_like(P["x"])
    for core in cores:
        b, half = divmod(core, 2)
        xo = rF[core]["xo"].T
        if half == 0:
            out[b, 0:2048] = xo
        else:
            out[b, 2048:4096] = xo[::-1]
    return out.astype(np.float32)
```

```python
from contextlib import ExitStack
import math
import numpy as np
import concourse.bass as bass
import concourse.mybir as mybir
from concourse.bass_utils import run_bass_kernel_spmd

F32 = mybir.dt.float32
I32 = mybir.dt.int32
U32 = mybir.dt.uint32
ALU = mybir.AluOpType
AF = mybir.ActivationFunctionType
AX = mybir.AxisListType


class Sched:
    NDS = 4

    def __init__(self, nc, stack, gate=None, gate_val=0, nds=None):
        self.nc = nc
        self.ndsq = dict(sp=self.NDS, act=self.NDS, pool=self.NDS)
        if nds:
            self.ndsq.update(nds)
        self.gate = gate
        self.gate_val = gate_val
        self.eng = {"pe": nc.tensor, "dve": nc.vector, "act": nc.scalar,
                    "pool": nc.gpsimd, "sp": nc.sync}
        self.sem = {}
        self.cnt = {}
        _UID[0] += 1
        u = "q%d" % _UID[0]
        for e in self.eng:
            self.sem[e] = stack.enter_context(nc.semaphore(u + "s_" + e))
            self.cnt[e] = 0
        self.dq = {}
        for q in ("sp", "act", "pool"):
            sems = [stack.enter_context(nc.semaphore(u + "d_%s%d" % (q, i))) for i in range(self.ndsq[q])]
            self.dq[q] = {"sems": sems, "n": 0}
            for i, s in enumerate(sems):
                self.sem["d_%s%d" % (q, i)] = s
        self.seen = {e: {} for e in self.eng}
        self.prog = {e: [] for e in self.eng}
        self.lastw = {}
        self.reads = {}
        self.ninst = 0
        if gate is not None:
            self.sem["gate"] = gate
        if gate is not None and gate_val > 0:
            for e in self.eng:
                self.prog[e].append(("w", "gate", gate_val))

    def _need(self, need, ev):
        if ev is None:
            return
        s, v = ev
        if need.get(s, 0) < v:
            need[s] = v

    def _waits(self, e, reads, writes):
        need = {}
        for k in reads:
            self._need(need, self.lastw.get(k))
        for k in writes:
            self._need(need, self.lastw.get(k))
            for ev in self.reads.get(k, ()):
                self._need(need, ev)
        eng = self.eng[e]
        seen = self.seen[e]
        for s, v in need.items():
            if seen.get(s, 0) >= v:
                continue
            self.prog[e].append(("w", s, v))
            seen[s] = v
            self.ninst += 1

    def _commit(self, ev, reads, writes):
        for k in writes:
            self.lastw[k] = ev
            self.reads[k] = []
        for k in reads:
            if k in writes:
                continue
            self.reads.setdefault(k, []).append(ev)
            if len(self.reads[k]) > 24:
                d = {}
                for s, v in self.reads[k]:
                    if d.get(s, 0) < v:
                        d[s] = v
                self.reads[k] = list(d.items())

    def op(self, e, fn, reads=(), writes=()):
        reads = tuple(reads)
        writes = tuple(writes)
        self._waits(e, reads, writes)
        self.cnt[e] += 1
        self.prog[e].append(("i", fn, e, 1))
        self.ninst += 1
        self._commit((e, self.cnt[e]), reads, writes)

    def dma(self, q, fn, reads=(), writes=()):
        reads = tuple(reads)
        writes = tuple(writes)
        st = self.dq[q]
        i = st["n"]
        slot = i % self.ndsq[q]
        sname = "d_%s%d" % (q, slot)
        rnd = i // self.ndsq[q]
        eng = self.eng[q]
        if rnd > 0 and self.seen[q].get(sname, 0) < 16 * rnd:
            self.prog[q].append(("w", sname, 16 * rnd))
            self.seen[q][sname] = 16 * rnd
            self.ninst += 1
        self._waits(q, reads, writes)
        self.prog[q].append(("i", fn, sname, 16))
        st["n"] = i + 1
        self.ninst += 1
        self._commit((sname, 16 * (rnd + 1)), reads, writes)

    def finish(self, keys, e="sp"):
        self._waits(e, tuple(keys), ())

    def drain_all(self, e="sp"):
        need = {}
        for en, c in self.cnt.items():
            if c:
                need[en] = c
        for q, stq in self.dq.items():
            n = stq["n"]
            nq = self.ndsq[q]
            for slot in range(nq):
                uses = (n - slot + nq - 1) // nq if n > slot else 0
                if uses:
                    need["d_%s%d" % (q, slot)] = 16 * uses
        eng = self.eng[e]
        for s, v in need.items():
            if self.seen[e].get(s, 0) >= v:
                continue
            self.prog[e].append(("w", s, v))
            self.seen[e][s] = v

    def emit(self):
        nc = self.nc
        with nc.Block() as block:
            def mk(e):
                def body(engine):
                    for it in self.prog[e]:
                        if it[0] == "w":
                            engine.wait_ge(self.sem[it[1]], it[2])
                        else:
                            it[1](engine).then_inc(self.sem[it[2]], it[3])
                return body
            block.sync(mk("sp"))
            block.scalar(mk("act"))
            block.vector(mk("dve"))
            block.gpsimd(mk("pool"))
            block.tensor(mk("pe"))
        self.prog = {e: [] for e in self.eng}

    def sync_all(self):
        for e in self.eng:
            self.drain_all(e)
        self.lastw = {}
        self.reads = {}

D = 1024
NMOD = 6
INW = 2336
INW_T = 19
EPS = 1e-6
NTOK = 2176


_UID = [0]


def _mk(nc, st):
    _UID[0] += 1
    u = "u%d_" % _UID[0]

    def sb(name, shape, dt=F32):
        return st.enter_context(nc.sbuf_tensor(u + name, shape, dt))

    def ps(name, shape, dt=F32):
        return st.enter_context(nc.psum_tensor(u + name, shape, dt))
    return sb, ps


def _groups(n, g=512):
    out = []
    t = 0
    while t < n:
        out.append((t, min(g, n - t)))
        t += g
    return out


def build_PA(ntok=NTOK, nctx=128):
    nc = bass.Bass("TRN2", target_bir_lowering=False)
    xT = nc.dram_tensor("xT", [D, ntok], F32, kind="ExternalInput").ap()
    cT = nc.dram_tensor("cT", [128, 16], F32, kind="ExternalInput").ap()
    w_mod = nc.dram_tensor("w_mod", [D, NMOD * D], F32, kind="ExternalInput").ap()
    b_mod = nc.dram_tensor("b_mod", [128, 48], F32, kind="ExternalInput").ap()
    g1 = nc.dram_tensor("g1", [128, 8], F32, kind="ExternalInput").ap()
    w_in = nc.dram_tensor("w_in", [D, INW], F32, kind="ExternalInput").ap()
    ones = nc.dram_tensor("ones", [128, 128], F32, kind="ExternalInput").ap()
    colsT = nc.dram_tensor("colsT", [INW_T * 128, ntok], F32, kind="ExternalOutput").ap()
    modT = nc.dram_tensor("modT", [128, 96], F32, kind="ExternalOutput").ap()
    with ExitStack() as st:
        S = Sched(nc, st)
        sb, ps = _mk(nc, st)
        CT = sb("CT", [128, 16]); SC = sb("SC", [128, 16]); BM = sb("BM", [128, 48]); G1 = sb("G1", [128, 8])
        ONES = sb("ONES", [128, 128]); MOD = sb("MOD", [128, 96])
        A1 = sb("A1", [128, 16]); TMPA = sb("TMPA", [128, 16])
        WM = [sb("WM%d" % i, [128, 8, 512]) for i in range(2)]
        WIN = sb("WIN", [128, 8, INW])
        XT = [sb("XT%d" % i, [128, 8, 512]) for i in range(2)]
        XSQ = sb("XSQ", [128, 512]); RSTD = sb("RSTD", [128, 512]); TMP = sb("TMP", [128, 512])
        HT = [sb("HT%d" % i, [128, 8, 512]) for i in range(2)]
        OUTB = [sb("OUTB%d" % i, [128, 512]) for i in range(4)]
        pmod = ps("pmod", [128, 96]); pss = ps("pss", [128, 512])
        pout = [ps("pout%d" % i, [128, 512]) for i in range(3)]

        S.dma("sp", lambda e: e.dma_start(out=CT[:], in_=cT), writes=["CT"])
        S.dma("sp", lambda e: e.dma_start(out=BM[:], in_=b_mod), writes=["BM"])
        S.dma("sp", lambda e: e.dma_start(out=G1[:], in_=g1), writes=["G1"])
        S.dma("sp", lambda e: e.dma_start(out=ONES[:], in_=ones), writes=["ONES"])
        S.op("act", lambda e: e.activation(SC[:], CT[:], AF.Silu), reads=["CT"], writes=["SC"])
        SC3 = SC[:].rearrange("p (k c) -> p k c", c=2)
        wm_v = w_mod.rearrange("(k p) f -> p k f", p=128)
        for jg in range(12):
            b = jg % 2
            S.dma("act" if jg % 2 else "sp",
                  lambda e, jg=jg, b=b: e.dma_start(out=WM[b][:], in_=wm_v[:, :, jg * 512:(jg + 1) * 512]),
                  writes=["WM%d" % b])
            for j8 in range(4):
                j = jg * 4 + j8
                for k in range(8):
                    S.op("pe", lambda e, j=j, j8=j8, k=k, b=b: e.matmul(
                        pmod[:, 2 * j:2 * j + 2], WM[b][:, k, j8 * 128:(j8 + 1) * 128], SC3[:, k, :],
                        start=(k == 0), stop=(k == 7)), reads=["WM%d" % b, "SC"], writes=["pmod"])
        S.op("dve", lambda e: e.tensor_tensor(MOD[:].rearrange("p (j c) -> p j c", c=2),
                                              pmod[:].rearrange("p (j c) -> p j c", c=2),
                                              BM[:].unsqueeze(2).to_broadcast([128, 48, 2]), ALU.add),
             reads=["pmod", "BM"], writes=["MOD"])
        S.dma("sp", lambda e: e.dma_start(out=modT, in_=MOD[:]), reads=["MOD"], writes=["modT"])
        MOD3 = MOD[:].rearrange("p (j c) -> p j c", c=2)
        S.op("dve", lambda e: e.tensor_scalar(TMPA[:].rearrange("p (j c) -> p j c", c=2), MOD3[:, 8:16, :], 1.0, None, ALU.add),
             reads=["MOD"], writes=["TMPA"])
        S.op("dve", lambda e: e.tensor_tensor(A1[:].rearrange("p (j c) -> p j c", c=2),
                                              TMPA[:].rearrange("p (j c) -> p j c", c=2),
                                              G1[:].unsqueeze(2).to_broadcast([128, 8, 2]), ALU.mult),
             reads=["TMPA", "G1"], writes=["A1"])
        A13 = A1[:].rearrange("p (j c) -> p j c", c=2)
        win_v = w_in.rearrange("(k p) f -> p k f", p=128)
        for k in range(8):
            S.dma("pool", lambda e, k=k: e.dma_start(out=WIN[:, k, :], in_=win_v[:, k, :]), writes=["WIN%d" % k])
        xT_v = xT.rearrange("(k p) t -> p k t", p=128)
        grp = [(0, nctx, 1)] if nctx else []
        grp += [(nctx + t0, tn, 0) for (t0, tn) in _groups(ntok - nctx)]
        oi = 0
        for gi, (t0, tn, col) in enumerate(grp):
            b = gi % 2
            xk, hk = "XT%d" % b, "HT%d" % b
            S.dma("sp", lambda e, b=b, t0=t0, tn=tn: e.dma_start(out=XT[b][:, :, :tn], in_=xT_v[:, :, t0:t0 + tn]), writes=[xk])
            for k in range(8):
                S.op("act", lambda e, b=b, k=k, tn=tn: e.activation(XSQ[:, :tn], XT[b][:, k, :tn], AF.Square), reads=[xk], writes=["XSQ"])
                S.op("pe", lambda e, k=k, tn=tn: e.matmul(pss[:, :tn], ONES[:], XSQ[:, :tn], start=(k == 0), stop=(k == 7)),
                     reads=["ONES", "XSQ"], writes=["pss"])
            S.op("dve", lambda e, tn=tn: e.tensor_scalar(RSTD[:, :tn], pss[:, :tn], 1.0 / D, EPS, ALU.mult, ALU.add), reads=["pss"], writes=["RSTD"])
            S.op("act", lambda e, tn=tn: e.sqrt(RSTD[:, :tn], RSTD[:, :tn]), reads=["RSTD"], writes=["RSTD"])
            S.op("dve", lambda e, tn=tn: e.reciprocal(RSTD[:, :tn], RSTD[:, :tn]), reads=["RSTD"], writes=["RSTD"])
            for k in range(8):
                S.op("dve", lambda e, b=b, k=k, tn=tn: e.tensor_tensor(TMP[:, :tn], XT[b][:, k, :tn], RSTD[:, :tn], ALU.mult),
                     reads=[xk, "RSTD"], writes=["TMP"])
                S.op("act", lambda e, b=b, k=k, tn=tn, col=col: e.activation(
                    HT[b][:, k, :tn], TMP[:, :tn], AF.Identity, bias=MOD3[:, k, col:col + 1], scale=A13[:, k, col:col + 1]),
                    reads=["TMP", "MOD", "A1"], writes=[hk])
            for ot in range(INW_T):
                m = 128 if ot < INW_T - 1 else INW - 128 * (INW_T - 1)
                pb = oi % 3
                ob = oi % 4
                oi += 1
                for k in range(8):
                    S.op("pe", lambda e, b=b, k=k, tn=tn, ot=ot, m=m, pb=pb: e.matmul(
                        pout[pb][:m, :tn], WIN[:, k, ot * 128:ot * 128 + m], HT[b][:, k, :tn], start=(k == 0), stop=(k == 7)),
                        reads=["WIN%d" % k, hk], writes=["pout%d" % pb])
                if m < 128:
                    S.op("dve", lambda e, ob=ob: e.memset(OUTB[ob][:], 0.0), writes=["OUTB%d" % ob])
                eng = "act" if oi % 2 else "dve"
                if eng == "act":
                    S.op("act", lambda e, ob=ob, pb=pb, m=m, tn=tn: e.copy(OUTB[ob][:m, :tn], pout[pb][:m, :tn]),
                         reads=["pout%d" % pb], writes=["OUTB%d" % ob])
                else:
                    S.op("dve", lambda e, ob=ob, pb=pb, m=m, tn=tn: e.tensor_copy(OUTB[ob][:m, :tn], pout[pb][:m, :tn]),
                         reads=["pout%d" % pb], writes=["OUTB%d" % ob])
                S.dma("sp" if oi % 2 else "act", lambda e, ob=ob, ot=ot, t0=t0, tn=tn: e.dma_start(
                    out=colsT[ot * 128:(ot + 1) * 128, t0:t0 + tn], in_=OUTB[ob][:, :tn]),
                    reads=["OUTB%d" % ob], writes=["colsT_%d" % oi])
        S.drain_all("sp")
        S.emit()
    return nc


SEQT = 4352
S5_CH = [(0, 256)] + [(256 + 512 * i, 512) for i in range(8)]


def build_PB():
    nc = bass.Bass("TRN2", target_bir_lowering=False)
    uin = [nc.dram_tensor(n, [128, SEQT], F32, kind="ExternalInput").ap() for n in ("uf", "ub")]
    prm = nc.dram_tensor("prm", [128, 24], F32, kind="ExternalInput").ap()
    bre = nc.dram_tensor("bre", [128, 128], F32, kind="ExternalInput").ap()
    bim = nc.dram_tensor("bim", [128, 128], F32, kind="ExternalInput").ap()
    cre = nc.dram_tensor("cre", [128, 128], F32, kind="ExternalInput").ap()
    cim = nc.dram_tensor("cim", [128, 128], F32, kind="ExternalInput").ap()
    tau = nc.dram_tensor("tau", [128, 512], F32, kind="ExternalInput").ap()
    ident = nc.dram_tensor("ident", [128, 128], F32, kind="ExternalInput").ap()
    yout = [nc.dram_tensor(n, [128, SEQT], F32, kind="ExternalOutput").ap() for n in ("yf", "yb")]
    TWO_PI = 2.0 * math.pi
    with ExitStack() as st:
        S = Sched(nc, st)
        sb, ps = _mk(nc, st)
        PRM = sb("PRM", [128, 24]); BRE = sb("BRE", [128, 128]); BIM = sb("BIM", [128, 128])
        CRE = sb("CRE", [128, 128]); CIM = sb("CIM", [128, 128]); TAU = sb("TAU", [128, 512]); ID = sb("ID", [128, 128])
        names = ["DT", "LR", "MAG", "TH", "R", "R2", "RF", "FR", "SIN", "COS", "ARE", "AIM", "DEN", "AM1", "FRE", "FIM", "T0", "T1"]
        P = {n: sb("p_" + n, [128, 8]) for n in names}
        RI = sb("p_RI", [128, 8], I32)
        BBR = sb("BBR", [128, 128]); BBI = sb("BBI", [128, 128]); TB = sb("TB", [128, 128])
        PAD = sb("PAD", [128, 128])
        WBR = sb("WBR", [128, 8, 128]); WBI = sb("WBI", [128, 8, 128]); CR = sb("CR", [128, 8, 128]); CIN = sb("CIN", [128, 8, 128])
        TC = sb("TC", [128, 8, 512]); TS = sb("TS", [128, 8, 512]); RHO = sb("RHO", [128, 8, 512])
        RR = sb("RR", [128, 512]); RRF = sb("RRF", [128, 512]); RRI = sb("RRI", [128, 512], I32)
        UC = [sb("UC%d" % i, [128, 512]) for i in range(2)]
        W = {}
        for n in ("BR", "BI", "T1", "T2", "T3", "T4", "XR", "XI", "QR", "QI", "HR", "HI"):
            for i in range(2):
                W[n, i] = sb("w_%s%d" % (n, i), [128, 512])
        HP = sb("HP", [128, 8])
        YO = [sb("YO%d" % i, [128, 512]) for i in range(2)]
        pbr = [ps("pbr%d" % i, [128, 512]) for i in range(2)]
        pbi = [ps("pbi%d" % i, [128, 512]) for i in range(2)]
        py = [ps("py%d" % i, [128, 512]) for i in range(2)]
        ptr = ps("ptr", [128, 128])

        for (t, src, k) in ((PRM, prm, "PRM"), (BRE, bre, "BRE"), (BIM, bim, "BIM"), (CRE, cre, "CRE"), (CIM, cim, "CIM"),
                            (TAU, tau, "TAU"), (ID, ident, "ID")):
            S.dma("sp", lambda e, t=t, src=src: e.dma_start(out=t[:], in_=src), writes=[k])
        PR3 = PRM[:].rearrange("p (a c) -> p a c", c=3)
        K = ["PP"]

        def V(fn, reads=(), writes=()):
            S.op("dve", fn, reads=list(reads) + K, writes=list(writes) + K)

        def A(fn, reads=(), writes=()):
            S.op("act", fn, reads=list(reads) + K, writes=list(writes) + K)

        A(lambda e: e.activation(P["DT"][:], PR3[:, :, 2], AF.Exp), reads=["PRM"])
        V(lambda e: e.tensor_scalar(P["LR"][:], PR3[:, :, 0], -1e-4, None, ALU.min), reads=["PRM"])
        V(lambda e: e.tensor_tensor(P["T0"][:], P["LR"][:], P["DT"][:], ALU.mult))
        A(lambda e: e.activation(P["MAG"][:], P["T0"][:], AF.Exp))
        V(lambda e: e.tensor_tensor(P["TH"][:], PR3[:, :, 1], P["DT"][:], ALU.mult), reads=["PRM"])
        V(lambda e: e.tensor_scalar(P["R"][:], P["TH"][:], 1.0 / TWO_PI, None, ALU.mult))
        V(lambda e: e.tensor_scalar(P["R2"][:], P["R"][:], 0.25, None, ALU.add))
        for (src, dst) in (("R", "SIN"), ("R2", "COS")):
            V(lambda e, src=src: e.tensor_copy(RI[:], P[src][:]))
            V(lambda e: e.tensor_copy(P["RF"][:], RI[:]))
            V(lambda e, src=src: e.tensor_tensor(P["FR"][:], P[src][:], P["RF"][:], ALU.subtract))
            A(lambda e, dst=dst: e.activation(P[dst][:], P["FR"][:], AF.Sin, scale=TWO_PI))
        V(lambda e: e.tensor_tensor(P["ARE"][:], P["MAG"][:], P["COS"][:], ALU.mult))
        V(lambda e: e.tensor_tensor(P["AIM"][:], P["MAG"][:], P["SIN"][:], ALU.mult))
        V(lambda e: e.tensor_tensor(P["T0"][:], P["LR"][:], P["LR"][:], ALU.mult))
        V(lambda e: e.tensor_tensor(P["T1"][:], PR3[:, :, 1], PR3[:, :, 1], ALU.mult), reads=["PRM"])
        V(lambda e: e.tensor_tensor(P["DEN"][:], P["T0"][:], P["T1"][:], ALU.add))
        V(lambda e: e.reciprocal(P["DEN"][:], P["DEN"][:]))
        V(lambda e: e.tensor_scalar(P["AM1"][:], P["ARE"][:], -1.0, None, ALU.add))
        V(lambda e: e.tensor_tensor(P["T0"][:], P["AM1"][:], P["LR"][:], ALU.mult))
        V(lambda e: e.tensor_tensor(P["T1"][:], P["AIM"][:], PR3[:, :, 1], ALU.mult), reads=["PRM"])
        V(lambda e: e.tensor_tensor(P["T0"][:], P["T0"][:], P["T1"][:], ALU.add))
        V(lambda e: e.tensor_tensor(P["FRE"][:], P["T0"][:], P["DEN"][:], ALU.mult))
        V(lambda e: e.tensor_tensor(P["T0"][:], P["AIM"][:], P["LR"][:], ALU.mult))
        V(lambda e: e.tensor_tensor(P["T1"][:], P["AM1"][:], PR3[:, :, 1], ALU.mult), reads=["PRM"])
        V(lambda e: e.tensor_tensor(P["T0"][:], P["T0"][:], P["T1"][:], ALU.subtract))
        V(lambda e: e.tensor_tensor(P["FIM"][:], P["T0"][:], P["DEN"][:], ALU.mult))

        def v3(t):
            return t[:].rearrange("p (a h) -> p a h", h=16)

        def bc(n):
            return P[n][:].unsqueeze(2).to_broadcast([128, 8, 16])
        V(lambda e: e.tensor_tensor(v3(BBR), v3(BRE), bc("FRE"), ALU.mult), reads=["BRE"])
        V(lambda e: e.tensor_tensor(v3(TB), v3(BIM), bc("FIM"), ALU.mult), reads=["BIM"])
        V(lambda e: e.tensor_tensor(BBR[:], BBR[:], TB[:], ALU.subtract))
        V(lambda e: e.tensor_tensor(v3(BBI), v3(BIM), bc("FRE"), ALU.mult), reads=["BIM"])
        V(lambda e: e.tensor_tensor(v3(TB), v3(BRE), bc("FIM"), ALU.mult), reads=["BRE"])
        V(lambda e: e.tensor_tensor(BBI[:], BBI[:], TB[:], ALU.add))
        V(lambda e: e.tensor_scalar(CIM[:], CIM[:], -1.0, None, ALU.mult), reads=["CIM"], writes=["CIM"])
        V(lambda e: e.memset(CR[:], 0.0)); V(lambda e: e.memset(CIN[:], 0.0))
        for dj in range(8):
            j = dj % 4
            for (src, dst) in ((BBR, WBR), (BBI, WBI)):
                V(lambda e: e.memset(PAD[:], 0.0), writes=["PAD"])
                V(lambda e, src=src, dj=dj, j=j: e.tensor_copy(PAD[0:64, 32 * j:32 * j + 16], src[0:64, dj * 16:dj * 16 + 16]), writes=["PAD"])
                V(lambda e, src=src, dj=dj, j=j: e.tensor_copy(PAD[64:128, 32 * j + 16:32 * j + 32], src[64:128, dj * 16:dj * 16 + 16]), writes=["PAD"])
                S.op("pe", lambda e: e.transpose(ptr[:], PAD[:], ID[:]), reads=["PAD", "ID"], writes=["ptr"])
                S.op("act", lambda e, dst=dst, dj=dj: e.copy(dst[:, dj, :], ptr[:]), reads=["ptr"], writes=["WB"])
            for (src, dst) in ((CRE, CR), (CIM, CIN)):
                V(lambda e, src=src, dst=dst, dj=dj, j=j: e.tensor_copy(dst[0:64, dj, 32 * j:32 * j + 16], src[0:64, dj * 16:dj * 16 + 16]), reads=["CRE", "CIM"], writes=["CC"])
                V(lambda e, src=src, dst=dst, dj=dj, j=j: e.tensor_copy(dst[64:128, dj, 32 * j + 16:32 * j + 32], src[64:128, dj * 16:dj * 16 + 16]), reads=["CRE", "CIM"], writes=["CC"])
            for (off, dst) in ((0.0, TS), (0.25, TC)):
                V(lambda e, dj=dj, off=off: e.tensor_scalar(RR[:], TAU[:], P["R"][:, dj:dj + 1], off, ALU.mult, ALU.add), reads=["TAU"], writes=["RR"])
                V(lambda e: e.tensor_copy(RRI[:], RR[:]), reads=["RR"], writes=["RRI"])
                V(lambda e: e.tensor_copy(RRF[:], RRI[:]), reads=["RRI"], writes=["RRF"])
                V(lambda e: e.tensor_tensor(RRF[:], RR[:], RRF[:], ALU.subtract), reads=["RR"], writes=["RRF"])
                S.op("act", lambda e, dst=dst, dj=dj: e.activation(dst[:, dj, :], RRF[:], AF.Sin, scale=TWO_PI), reads=["RRF"], writes=["TAB"])
            V(lambda e, dj=dj: e.tensor_copy(RHO[:, dj, :], P["MAG"][:, dj:dj + 1].to_broadcast([128, 512])), writes=["TAB"])

        G = "pool"
        oi = 0
        for d in range(2):
            V(lambda e: e.memset(HP[:], 0.0), writes=["HP"])
            for ci, (t0, T) in enumerate(S5_CH):
                ub_ = (d * 9 + ci) % 2
                uk = "UC%d" % ub_
                S.dma("sp", lambda e, d=d, t0=t0, T=T, ub_=ub_: e.dma_start(out=UC[ub_][:, :T], in_=uin[d][:, t0:t0 + T]), writes=[uk])
                yb_ = (d * 9 + ci) % 2
                for j in range(4):
                    dj = d * 4 + j
                    b = j % 2
                    w = lambda n, b=b, T=T: W[n, b][:, :T]
                    k = lambda n, b=b: "w_%s%d" % (n, b)
                    S.op("pe", lambda e, dj=dj, b=b, T=T, ub_=ub_: e.matmul(pbr[b][:, :T], WBR[:, dj, :], UC[ub_][:, :T], start=True, stop=True),
                         reads=["WB", uk], writes=["pbr%d" % b])
                    S.op("pe", lambda e, dj=dj, b=b, T=T, ub_=ub_: e.matmul(pbi[b][:, :T], WBI[:, dj, :], UC[ub_][:, :T], start=True, stop=True),
                         reads=["WB", uk], writes=["pbi%d" % b])
                    S.op("act", lambda e, w=w, b=b, T=T: e.copy(w("BR"), pbr[b][:, :T]), reads=["pbr%d" % b], writes=[k("BR")])
                    S.op("act", lambda e, w=w, b=b, T=T: e.copy(w("BI"), pbi[b][:, :T]), reads=["pbi%d" % b], writes=[k("BI")])
                    cs = lambda dj=dj, T=T: TC[:, dj, :T]
                    sn = lambda dj=dj, T=T: TS[:, dj, :T]
                    S.op("dve", lambda e, w=w, cs=cs: e.tensor_tensor(w("T1"), cs(), w("BR"), ALU.mult), reads=["TAB", k("BR")], writes=[k("T1")])
                    S.op("dve", lambda e, w=w, sn=sn: e.tensor_tensor(w("T2"), sn(), w("BI"), ALU.mult), reads=["TAB", k("BI")], writes=[k("T2")])
                    S.op("dve", lambda e, w=w: e.tensor_tensor(w("XR"), w("T1"), w("T2"), ALU.add), reads=[k("T1"), k("T2")], writes=[k("XR")])
                    S.op(G, lambda e, w=w, cs=cs: e.tensor_tensor(w("T3"), cs(), w("BI"), ALU.mult), reads=["TAB", k("BI")], writes=[k("T3")])
                    S.op(G, lambda e, w=w, sn=sn: e.tensor_tensor(w("T4"), sn(), w("BR"), ALU.mult), reads=["TAB", k("BR")], writes=[k("T4")])
                    S.op(G, lambda e, w=w: e.tensor_tensor(w("XI"), w("T3"), w("T4"), ALU.subtract), reads=[k("T3"), k("T4")], writes=[k("XI")])
                    S.op("dve", lambda e, w=w, dj=dj, j=j, T=T: e.tensor_tensor_scan(w("QR"), RHO[:, dj, :T], w("XR"), HP[:, 2 * j:2 * j + 1], ALU.mult, ALU.add),
                         reads=["TAB", k("XR"), "HP"], writes=[k("QR")])
                    S.op("dve", lambda e, w=w, dj=dj, j=j, T=T: e.tensor_tensor_scan(w("QI"), RHO[:, dj, :T], w("XI"), HP[:, 2 * j + 1:2 * j + 2], ALU.mult, ALU.add),
                         reads=["TAB", k("XI"), "HP"], writes=[k("QI")])
                    S.op("dve", lambda e, w=w, cs=cs: e.tensor_tensor(w("T1"), cs(), w("QR"), ALU.mult), reads=["TAB", k("QR")], writes=[k("T1")])
                    S.op("dve", lambda e, w=w, sn=sn: e.tensor_tensor(w("T2"), sn(), w("QI"), ALU.mult), reads=["TAB", k("QI")], writes=[k("T2")])
                    S.op("dve", lambda e, w=w: e.tensor_tensor(w("HR"), w("T1"), w("T2"), ALU.subtract), reads=[k("T1"), k("T2")], writes=[k("HR")])
                    S.op(G, lambda e, w=w, sn=sn: e.tensor_tensor(w("T3"), sn(), w("QR"), ALU.mult), reads=["TAB", k("QR")], writes=[k("T3")])
                    S.op(G, lambda e, w=w, cs=cs: e.tensor_tensor(w("T4"), cs(), w("QI"), ALU.mult), reads=["TAB", k("QI")], writes=[k("T4")])
                    S.op(G, lambda e, w=w: e.tensor_tensor(w("HI"), w("T3"), w("T4"), ALU.add), reads=[k("T3"), k("T4")], writes=[k("HI")])
                    S.op("act", lambda e, b=b, j=j, T=T: e.copy(HP[:, 2 * j:2 * j + 1], W["HR", b][:, T - 1:T]), reads=[k("HR")], writes=["HP"])
                    S.op("act", lambda e, b=b, j=j, T=T: e.copy(HP[:, 2 * j + 1:2 * j + 2], W["HI", b][:, T - 1:T]), reads=[k("HI")], writes=["HP"])
                    S.op("pe", lambda e, dj=dj, w=w, j=j, yb_=yb_, T=T: e.matmul(py[yb_][:, :T], CR[:, dj, :], w("HR"), start=(j == 0), stop=False),
                         reads=["CC", k("HR")], writes=["py%d" % yb_])
                    S.op("pe", lambda e, dj=dj, w=w, j=j, yb_=yb_, T=T: e.matmul(py[yb_][:, :T], CIN[:, dj, :], w("HI"), start=False, stop=(j == 3)),
                         reads=["CC", k("HI")], writes=["py%d" % yb_])
                S.op("act", lambda e, yb_=yb_, T=T: e.copy(YO[yb_][:, :T], py[yb_][:, :T]), reads=["py%d" % yb_], writes=["YO%d" % yb_])
                oi += 1
                S.dma("act", lambda e, d=d, yb_=yb_, t0=t0, T=T: e.dma_start(out=yout[d][:, t0:t0 + T], in_=YO[yb_][:, :T]),
                      reads=["YO%d" % yb_], writes=["yout_%d" % oi])
        S.drain_all("sp")
        S.emit()
    return nc


NCH = 68


def build_PC():
    nc = bass.Bass("TRN2", target_bir_lowering=False)
    I = {}
    for d in range(2):
        I["qT", d] = nc.dram_tensor("qT%d" % d, [128, SEQT], F32, kind="ExternalInput").ap()
        I["kT", d] = nc.dram_tensor("kT%d" % d, [128, SEQT], F32, kind="ExternalInput").ap()
        I["v", d] = nc.dram_tensor("v%d" % d, [SEQT, 256], F32, kind="ExternalInput").ap()
        I["zT", d] = nc.dram_tensor("zT%d" % d, [16, SEQT], F32, kind="ExternalInput").ap()
        I["wg", d] = nc.dram_tensor("wg%d" % d, [16, 128], F32, kind="ExternalInput").ap()
        I["bg", d] = nc.dram_tensor("bg%d" % d, [128, 1], F32, kind="ExternalInput").ap()
        I["o", d] = nc.dram_tensor("o%d" % d, [SEQT, 256], F32, kind="ExternalOutput").ap()
    rst = nc.dram_tensor("rst", [128, 512], F32, kind="ExternalInput").ap()
    tmask = nc.dram_tensor("tmask", [64, 256], F32, kind="ExternalInput").ap()
    blk = nc.dram_tensor("blk", [128, 256], F32, kind="ExternalInput").ap()
    hmask = nc.dram_tensor("hmask", [128, 4], F32, kind="ExternalInput").ap()
    ident = nc.dram_tensor("ident", [128, 128], F32, kind="ExternalInput").ap()
    QSC = 32 ** -0.5
    with ExitStack() as st:
        S = Sched(nc, st)
        sb, ps = _mk(nc, st)
        RST = sb("RST", [128, 512]); TM = sb("TM", [64, 256]); BLK = sb("BLK", [128, 256]); HM = sb("HM", [128, 4]); ID = sb("ID", [128, 128])
        WG = sb("WG", [16, 128]); BG = sb("BG", [128, 1]); NBG = sb("NBG", [128, 1])
        Wb = {}
        for n in ("Q", "K", "LA", "B", "E", "D", "QE", "QS", "KD", "KS0", "KS1", "KS2", "KS3"):
            for i in range(2):
                Wb[n, i] = sb("g_%s%d" % (n, i), [128, 512])
        Z = [sb("Z%d" % i, [16, 512]) for i in range(2)]
        VV = [sb("VV%d" % i, [64, 8, 256]) for i in range(2)]
        DEC = [sb("DEC%d" % i, [128, 8]) for i in range(2)]
        KDT = [sb("KDT%d" % i, [64, 128]) for i in range(2)]
        STt = [sb("ST%d" % i, [64, 256]) for i in range(2)]
        OB = [sb("OB%d" % i, [64, 256]) for i in range(3)]
        KVM = sb("KVM", [128, 256])
        SS = [sb("SS%d" % i, [128, 256]) for i in range(2)]
        pza = ps("pza", [128, 512])
        pt0 = ps("pt0", [64, 128])
        pt = [pt0, pt0]
        pkv = [ps("pkv%d" % i, [128, 256]) for i in range(2)]
        psc = [ps("psc%d" % i, [64, 256]) for i in range(2)]
        po = [ps("po%d" % i, [64, 256]) for i in range(2)]
        for (t, src, k) in ((RST, rst, "RST"), (TM, tmask, "TM"), (BLK, blk, "BLK"), (HM, hmask, "HM"), (ID, ident, "ID")):
            S.dma("sp", lambda e, t=t, src=src: e.dma_start(out=t[:], in_=src), writes=[k])
        gc = 0
        oi = 0
        for d in range(2):
            S.dma("sp", lambda e, d=d: e.dma_start(out=WG[:], in_=I["wg", d]), writes=["WG"])
            S.dma("sp", lambda e, d=d: e.dma_start(out=BG[:], in_=I["bg", d]), writes=["BG"])
            S.op("dve", lambda e: e.tensor_scalar(NBG[:], BG[:], -1.0, None, ALU.mult), reads=["BG"], writes=["NBG"])
            S.op("dve", lambda e: e.memset(SS[0][:], 0.0), writes=["SS0"])
            scur = 0
            for bi, (t0, T) in enumerate(S5_CH):
                nchk = T // 64
                b = (d * 9 + bi) % 2
                w = lambda n, b=b, T=T: Wb[n, b][:, :T]
                k = lambda n, b=b: "g_%s%d" % (n, b)
                w3 = lambda n, b=b, T=T: Wb[n, b][:, :T].rearrange("p (c s) -> p c s", s=64)
                S.dma("sp", lambda e, d=d, b=b, t0=t0, T=T: e.dma_start(out=Wb["Q", b][:, :T], in_=I["qT", d][:, t0:t0 + T]), writes=[k("Q")])
                S.dma("act", lambda e, d=d, b=b, t0=t0, T=T: e.dma_start(out=Wb["K", b][:, :T], in_=I["kT", d][:, t0:t0 + T]), writes=[k("K")])
                S.dma("sp", lambda e, d=d, b=b, t0=t0, T=T: e.dma_start(out=Z[b][:, :T], in_=I["zT", d][:, t0:t0 + T]), writes=["Z%d" % b])
                S.dma("act", lambda e, d=d, b=b, t0=t0, T=T, nchk=nchk: e.dma_start(
                    out=VV[b][:, :nchk, :], in_=I["v", d][t0:t0 + T, :].rearrange("(c s) f -> s c f", s=64)), writes=["VV%d" % b])
                S.op("pe", lambda e, b=b, T=T: e.matmul(pza[:, :T], WG[:], Z[b][:, :T], start=True, stop=True), reads=["WG", "Z%d" % b], writes=["pza"])
                S.op("act", lambda e, w=w, T=T: e.activation(w("E"), pza[:, :T], AF.Exp, bias=NBG[:], scale=-1.0), reads=["pza", "NBG"], writes=[k("E")])
                S.op("act", lambda e, w=w: e.activation(w("E"), w("E"), AF.Ln, bias=1.0), reads=[k("E")], writes=[k("E")])
                S.op("dve", lambda e, w=w: e.tensor_scalar(w("LA"), w("E"), -1.0 / 16.0, None, ALU.mult), reads=[k("E")], writes=[k("LA")])
                S.op("dve", lambda e, w=w, T=T: e.tensor_tensor_scan(w("B"), RST[:, :T], w("LA"), 0.0, ALU.mult, ALU.add), reads=["RST", k("LA")], writes=[k("B")])
                S.op("act", lambda e, b=b, w3=w3, nchk=nchk: e.activation(DEC[b][:, :nchk], w3("B")[:, :, 63], AF.Exp), reads=[k("B")], writes=["DEC%d" % b])
                S.op("act", lambda e, w=w: e.activation(w("E"), w("B"), AF.Exp), reads=[k("B")], writes=[k("E")])
                S.op("dve", lambda e, w=w: e.scalar_tensor_tensor(w("QE"), w("Q"), QSC, w("E"), ALU.mult, ALU.mult), reads=[k("Q"), k("E")], writes=[k("QE")])
                S.op("dve", lambda e, w3=w3, nchk=nchk: e.tensor_tensor(w3("D"), w3("B"), w3("B")[:, :, 32:33].to_broadcast([128, nchk, 64]), ALU.subtract),
                     reads=[k("B")], writes=[k("D")])
                S.op("act", lambda e, w=w: e.activation(w("E"), w("D"), AF.Exp), reads=[k("D"), k("QE")], writes=[k("E")])
                S.op("dve", lambda e, w=w: e.scalar_tensor_tensor(w("QS"), w("Q"), QSC, w("E"), ALU.mult, ALU.mult), reads=[k("Q"), k("E")], writes=[k("QS")])
                S.op("act", lambda e, w=w: e.activation(w("E"), w("D"), AF.Exp, scale=-1.0), reads=[k("D"), k("QS")], writes=[k("E")])
                S.op("dve", lambda e, w=w: e.tensor_tensor(w("LA"), w("K"), w("E"), ALU.mult), reads=[k("K"), k("E"), k("B")], writes=[k("LA")])
                for h in range(4):
                    S.op("pool", lambda e, w=w, h=h: e.tensor_scalar(w("KS%d" % h), w("LA"), HM[:, h:h + 1], None, ALU.mult),
                         reads=[k("LA"), "HM"], writes=[k("KS%d" % h)])
                S.op("dve", lambda e, w3=w3, nchk=nchk: e.tensor_tensor(w3("D"), w3("B")[:, :, 63:64].to_broadcast([128, nchk, 64]), w3("B"), ALU.subtract),
                     reads=[k("B"), k("E"), k("LA")], writes=[k("D")])
                S.op("act", lambda e, w=w: e.activation(w("D"), w("D"), AF.Exp), reads=[k("D")], writes=[k("D")])
                S.op("dve", lambda e, w=w: e.tensor_tensor(w("KD"), w("K"), w("D"), ALU.mult), reads=[k("K"), k("D")], writes=[k("KD")])
                for c in range(nchk):
                    p2 = gc % 2
                    gc += 1
                    cs = slice(c * 64, (c + 1) * 64)
                    S.op("pe", lambda e, b=b, cs=cs, p2=p2: e.transpose(pt[p2][:], Wb["KD", b][:, cs], ID[:]), reads=[k("KD"), "ID"], writes=["pt"])
                    S.op("act", lambda e, p2=p2: e.copy(KDT[p2][:], pt[p2][:]), reads=["pt"], writes=["KDT%d" % p2])
                    S.op("pe", lambda e, b=b, c=c, p2=p2: e.matmul(pkv[p2][:], KDT[p2][:], VV[b][:, c, :], start=True, stop=True),
                         reads=["KDT%d" % p2, "VV%d" % b], writes=["pkv%d" % p2])
                    for h in range(4):
                        S.op("pe", lambda e, b=b, cs=cs, p2=p2, h=h: e.matmul(psc[p2][:, h * 64:(h + 1) * 64], Wb["KS%d" % h, b][:, cs], Wb["QS", b][:, cs],
                                                                            start=True, stop=True),
                             reads=[k("KS%d" % h), k("QS")], writes=["psc%d" % p2])
                    S.op("dve", lambda e, p2=p2: e.tensor_tensor(STt[p2][:], psc[p2][:], TM[:], ALU.mult), reads=["psc%d" % p2, "TM"], writes=["ST%d" % p2])
                    for h in range(4):
                        hs = slice(h * 64, (h + 1) * 64)
                        S.op("pe", lambda e, b=b, c=c, p2=p2, hs=hs: e.matmul(po[p2][:, hs], STt[p2][:, hs], VV[b][:, c, hs], start=True, stop=False),
                             reads=["ST%d" % p2, "VV%d" % b], writes=["po%d" % p2])
                        S.op("pe", lambda e, b=b, cs=cs, p2=p2, hs=hs, scur=scur: e.matmul(po[p2][:, hs], Wb["QE", b][:, cs], SS[scur][:, hs], start=False, stop=True),
                             reads=[k("QE"), "SS%d" % scur], writes=["po%d" % p2])
                    ob = oi % 3
                    oi += 1
                    S.op("act", lambda e, ob=ob, p2=p2: e.copy(OB[ob][:], po[p2][:]), reads=["po%d" % p2], writes=["OB%d" % ob])
                    S.dma("sp" if oi % 2 else "act", lambda e, d=d, ob=ob, t0=t0, c=c: e.dma_start(out=I["o", d][t0 + c * 64:t0 + (c + 1) * 64, :], in_=OB[ob][:]),
                          reads=["OB%d" % ob], writes=["o_%d" % oi])
                    S.op("dve", lambda e, p2=p2: e.tensor_tensor(KVM[:], pkv[p2][:], BLK[:], ALU.mult), reads=["pkv%d" % p2, "BLK"], writes=["KVM"])
                    S.op("dve", lambda e, b=b, c=c, scur=scur: e.scalar_tensor_tensor(SS[1 - scur][:], SS[scur][:], DEC[b][:, c:c + 1], KVM[:], ALU.mult, ALU.add),
                         reads=["SS%d" % scur, "DEC%d" % b, "KVM"], writes=["SS%d" % (1 - scur)])
                    scur = 1 - scur
        S.drain_all("sp")
        S.emit()
    return nc


NEXP = 16384


def build_PD(ntok, nctx, last):
    nc = bass.Bass("TRN2", target_bir_lowering=False)
    def din(name, shape, dt=F32):
        return nc.dram_tensor(name, shape, dt, kind="ExternalInput").ap()
    xT = din("xT", [D, ntok]); modT = din("modT", [128, 96])
    su = din("su", [ntok, 256]); sv = din("sv", [ntok, 256]); gg = din("gg", [ntok, 512])
    s5u = din("s5u", [256, ntok]); yf = din("yf", [256, ntok]); yb = din("yb", [256, ntok])
    of_ = din("of", [ntok, 512]); ob_ = din("ob", [ntok, 512])
    w_out = din("w_out", [D, D]); wsT = din("wsT", [128, 512]); sgub = din("sgub", [128, 4]); s5d = din("s5d", [128, 2])
    wglu = din("wglu", [256, 512]); ng = din("ng", [128, 512]); g2 = din("g2", [128, 8]); wq = din("wq", [D, 2048])
    keysT = din("keysT", [128, 2048]); eu = din("eu", [NEXP, D]); ev = din("ev", [NEXP, D]); gfin = din("gfin", [128, 8])
    ones = din("ones", [128, 128]); ident = din("ident", [128, 128]); iota16 = din("iota16", [128, 16])
    xo = nc.dram_tensor("xo", [D, ntok], F32, kind="ExternalOutput").ap()
    nb = ntok // 128
    with ExitStack() as st:
        S = Sched(nc, st)
        sb, ps = _mk(nc, st)
        MOD = sb("MOD", [128, 96]); WOUT = sb("WOUT", [128, 8, D]); WS = sb("WS", [128, 512]); SGUB = sb("SGUB", [128, 4]); S5D = sb("S5D", [128, 2])
        WGLU = sb("WGLU", [128, 2, 512]); NG = sb("NG", [128, 512]); G2 = sb("G2", [128, 8]); WQ = sb("WQ", [128, 8, 2048]); KEYS = sb("KEYS", [128, 2048])
        GF = sb("GF", [128, 8]); ONES = sb("ONES", [128, 128]); ID = sb("ID", [128, 128]); IOTA = sb("IOTA", [128, 16])
        A2 = sb("A2", [128, 16]); TA = sb("TA", [128, 16])
        U_ = sb("U_", [128, 256]); V_ = sb("V_", [128, 256]); GU = sb("GU", [128, 256]); GV = sb("GV", [128, 256]); SQ = sb("SQ", [128, 512])
        SS = sb("SS", [128, 8]); VN = sb("VN", [128, 256]); MIX = sb("MIX", [128, D])
        S5U = sb("S5U", [128, 2, 128]); YF = sb("YF", [128, 2, 128]); YB = sb("YB", [128, 2, 128]); GE = sb("GE", [128, 2, 128]); SG = sb("SG", [128, 256])
        OF = sb("OF", [128, 512]); OB = sb("OB", [128, 512]); GG = sb("GG", [128, 512]); SL = sb("SL", [128, 512])
        XT = sb("XT", [128, 8, 128]); X1 = sb("X1", [128, 8, 128]); XO = XT; HT = sb("HT", [128, 8, 128]); MIXT = HT; HTOK = sb("HTOK", [128, D])
        RSTD = sb("RSTD", [128, 128]); TMPB = sb("TMPB", [128, 128])
        SC = sb("SC", [128, 2048])
        M16 = sb("M16", [128, 256]); I16 = sb("I16", [128, 256], U32); IF16 = sb("IF16", [128, 256]); I1S = sb("I1S", [128, 128])
        CS = sb("CS", [128, 2048]); SC2 = CS; QT = CS[:].rearrange("p (q t) -> p q t", t=128); CS2 = sb("CS2", [128, 256])
        T16 = sb("T16", [128, 128]); P16 = sb("P16", [128, 128], U32); PF = sb("PF", [128, 128]); AI = sb("AI", [128, 128], I32)
        AFL = sb("AFL", [128, 128]); BFL = sb("BFL", [128, 128]); E1 = sb("E1", [128, 128]); E2 = sb("E2", [128, 128])
        EG = sb("EG", [128, 128]); GATE = sb("GATE", [128, 128]); IDXTI = sb("IDXTI", [128, 128], I32); GATET = sb("GATET", [128, 128])
        ACTT = sb("ACTT", [128, 128]); WT = sb("WT", [128, 128])
        UG = [sb("UG%d" % i, [128, D]) for i in range(2)]; VG = UG
        HB = [sb("HB%d" % i, [128, D]) for i in range(2)]
        P0 = ps("P0", [128, 2048]); P1 = ps("P1", [128, 1024]); P2 = ps("P2", [128, 512]); P3 = ps("P3", [128, 512])

        def V(fn, r=(), w=()):
            S.op("dve", fn, reads=r, writes=w)

        def A(fn, r=(), w=()):
            S.op("act", fn, reads=r, writes=w)

        def PE(fn, r=(), w=()):
            S.op("pe", fn, reads=r, writes=w)

        def LD(q, t, src, key):
            S.dma(q, lambda e: e.dma_start(out=t, in_=src), writes=[key])

        LD("sp", MOD[:], modT, "MOD"); LD("sp", WS[:], wsT, "WS"); LD("sp", SGUB[:], sgub, "SGUB"); LD("sp", S5D[:], s5d, "S5D")
        LD("sp", WGLU[:], wglu.rearrange("(c p) f -> p c f", p=128), "WGLU"); LD("sp", NG[:], ng, "NG"); LD("sp", G2[:], g2, "G2")
        LD("sp", KEYS[:], keysT, "KEYS"); LD("sp", GF[:], gfin, "GF"); LD("sp", ONES[:], ones, "ONES"); LD("sp", ID[:], ident, "ID"); LD("sp", IOTA[:], iota16, "IOTA")
        wo_v = w_out.rearrange("(k p) f -> p k f", p=128)
        wq_v = wq.rearrange("(k p) f -> p k f", p=128)
        for k in range(8):
            LD("act", WOUT[:, k, :], wo_v[:, k, :], "WOUT")
            LD("act", WQ[:, k, :], wq_v[:, k, :], "WQ")
        MOD3 = MOD[:].rearrange("p (j c) -> p j c", c=2)
        V(lambda e: e.tensor_scalar(TA[:].rearrange("p (j c) -> p j c", c=2), MOD3[:, 32:40, :], 1.0, None, ALU.add), ["MOD"], ["TA"])
        V(lambda e: e.tensor_tensor(A2[:].rearrange("p (j c) -> p j c", c=2), TA[:].rearrange("p (j c) -> p j c", c=2),
                                    G2[:].unsqueeze(2).to_broadcast([128, 8, 2]), ALU.mult), ["TA", "G2"], ["A2"])
        A23 = A2[:].rearrange("p (j c) -> p j c", c=2)

        def rs_from_ss(ss, n, scale):
            V(lambda e: e.tensor_scalar(ss, ss, scale, EPS, ALU.mult, ALU.add), ["SS"], ["SS"])
            A(lambda e: e.sqrt(ss, ss), ["SS"], ["SS"])
            V(lambda e: e.reciprocal(ss, ss), ["SS"], ["SS"])

        def top16(src, scratch, mout, iout, n):
            V(lambda e: e.max(mout[:, 0:8], src), ["TK"], ["TK"])
            V(lambda e: e.max_index(iout[:, 0:8], mout[:, 0:8], src), ["TK"], ["TK"])
            V(lambda e: e.match_replace(scratch, mout[:, 0:8], src, -1e30), ["TK"], ["TK"])
            V(lambda e: e.max(mout[:, 8:16], scratch), ["TK"], ["TK"])
            V(lambda e: e.max_index(iout[:, 8:16], mout[:, 8:16], scratch), ["TK"], ["TK"])

        xT_v = xT.rearrange("(k p) t -> p k t", p=128)
        xo_v = xo.rearrange("(k p) t -> p k t", p=128)
        s5u_v = s5u.rearrange("(c p) t -> p c t", p=128)
        yf_v = yf.rearrange("(c p) t -> p c t", p=128)
        yb_v = yb.rearrange("(c p) t -> p c t", p=128)
        for bi in range(nb):
            tk = slice(bi * 128, (bi + 1) * 128)
            col = 1 if bi * 128 < nctx else 0
            LD("sp", U_[:], su[tk, :], "U_"); LD("sp", V_[:], sv[tk, :], "V_"); LD("sp", GG[:], gg[tk, :], "GG")
            LD("act", S5U[:], s5u_v[:, :, tk], "S5U"); LD("act", YF[:], yf_v[:, :, tk], "YF"); LD("act", YB[:], yb_v[:, :, tk], "YB")
            LD("sp", OF[:], of_[tk, :], "OF"); LD("sp", OB[:], ob_[tk, :], "OB"); LD("act", XT[:], xT_v[:, :, tk], "XT")
            A(lambda e: e.activation(GU[:], U_[:], AF.Gelu), ["U_"], ["GU"])
            A(lambda e: e.activation(GV[:], V_[:], AF.Gelu), ["V_"], ["GV"])
            V(lambda e: e.tensor_tensor(SQ[:, 0:256], GV[:], GV[:], ALU.mult), ["GV"], ["SQ"])
            V(lambda e: e.tensor_reduce(SS[:, 0:4], SQ[:, 0:256].rearrange("p (h d) -> p h d", d=64), AX.X, ALU.add), ["SQ"], ["SS"])
            rs_from_ss(SS[:, 0:4], 4, 1.0 / 64)
            V(lambda e: e.tensor_tensor(VN[:].rearrange("p (h d) -> p h d", d=64), GV[:].rearrange("p (h d) -> p h d", d=64),
                                        SS[:, 0:4].unsqueeze(2).to_broadcast([128, 4, 64]), ALU.mult), ["GV", "SS"], ["VN"])
            for h in range(4):
                PE(lambda e, h=h: e.matmul(P2[:, h * 64:(h + 1) * 64], WS[:, h * 128:(h + 1) * 128], VN[:, h * 64:(h + 1) * 64], start=True, stop=True),
                   ["WS", "VN"], ["P2"])
            V(lambda e: e.tensor_tensor(MIX[:, 0:256].rearrange("p (h d) -> p h d", d=64), P2[:, 0:256].rearrange("p (h d) -> p h d", d=64),
                                        SGUB[:].unsqueeze(2).to_broadcast([128, 4, 64]), ALU.add), ["P2", "SGUB"], ["MIXa"])
            V(lambda e: e.tensor_tensor(MIX[:, 0:256], MIX[:, 0:256], GU[:], ALU.mult), ["GU"], ["MIXa"])
            V(lambda e: e.tensor_tensor(YF[:], YF[:], YB[:], ALU.add), ["YB"], ["YF"])
            for ct in range(2):
                V(lambda e, ct=ct: e.scalar_tensor_tensor(YF[:, ct, :], S5U[:, ct, :], S5D[:, ct:ct + 1], YF[:, ct, :], ALU.mult, ALU.add),
                  ["S5U", "S5D"], ["YF"])
            A(lambda e: e.activation(GE[:], YF[:], AF.Gelu), ["YF"], ["GE"])
            for ct in range(2):
                PE(lambda e, ct=ct: e.matmul(P3[:, 0:512], GE[:, ct, :], WGLU[:, ct, :], start=(ct == 0), stop=(ct == 1)), ["GE", "WGLU"], ["P3"])
            A(lambda e: e.activation(SG[:], P3[:, 256:512], AF.Sigmoid), ["P3"], ["SG"])
            V(lambda e: e.tensor_tensor(MIX[:, 256:512], P3[:, 0:256], SG[:], ALU.mult), ["P3", "SG"], ["MIXb"])
            V(lambda e: e.tensor_tensor(OF[:], OF[:], OB[:], ALU.add), ["OB"], ["OF"])
            V(lambda e: e.tensor_tensor(SQ[:], OF[:], OF[:], ALU.mult), ["OF"], ["SQ"])
            V(lambda e: e.tensor_reduce(SS[:, 0:8], SQ[:].rearrange("p (h d) -> p h d", d=64), AX.X, ALU.add), ["SQ"], ["SS"])
            rs_from_ss(SS[:, 0:8], 8, 1.0 / 64)
            V(lambda e: e.tensor_tensor(OF[:].rearrange("p (h d) -> p h d", d=64), OF[:].rearrange("p (h d) -> p h d", d=64),
                                        SS[:, 0:8].unsqueeze(2).to_broadcast([128, 8, 64]), ALU.mult), ["SS"], ["OF"])
            V(lambda e: e.tensor_tensor(OF[:], OF[:], NG[:], ALU.mult), ["NG"], ["OF"])
            A(lambda e: e.activation(SL[:], GG[:], AF.Silu), ["GG"], ["SL"])
            V(lambda e: e.tensor_tensor(MIX[:, 512:1024], OF[:], SL[:], ALU.mult), ["OF", "SL"], ["MIXc"])
            for f in range(8):
                pp, pk = (P2, "P2") if f % 2 == 0 else (P3, "P3")
                PE(lambda e, f=f, pp=pp: e.transpose(pp[:, 0:128], MIX[:, f * 128:(f + 1) * 128], ID[:]), ["MIXa", "MIXb", "MIXc", "ID"], [pk])
                A(lambda e, f=f, pp=pp: e.copy(MIXT[:, f, :], pp[:, 0:128]), [pk], ["HT"])
            for ot in range(8):
                pp, pk = (P2, "P2") if ot % 2 == 0 else (P3, "P3")
                for k in range(8):
                    PE(lambda e, ot=ot, k=k, pp=pp: e.matmul(pp[:, 0:128], WOUT[:, k, ot * 128:(ot + 1) * 128], MIXT[:, k, :], start=(k == 0), stop=(k == 7)),
                       ["WOUT", "HT"], [pk])
                V(lambda e, ot=ot, pp=pp, col=col: e.scalar_tensor_tensor(X1[:, ot, :], pp[:, 0:128], MOD3[:, 16 + ot, col:col + 1], XT[:, ot, :], ALU.mult, ALU.add),
                  [pk, "MOD", "XT"], ["X1"])
            for k in range(8):
                A(lambda e, k=k: e.activation(TMPB[:], X1[:, k, :], AF.Square), ["X1"], ["TMPB"])
                PE(lambda e, k=k: e.matmul(P2[:, 0:128], ONES[:], TMPB[:], start=(k == 0), stop=(k == 7)), ["ONES", "TMPB"], ["P2"])
            V(lambda e: e.tensor_scalar(RSTD[:], P2[:, 0:128], 1.0 / D, EPS, ALU.mult, ALU.add), ["P2"], ["RSTD"])
            A(lambda e: e.sqrt(RSTD[:], RSTD[:]), ["RSTD"], ["RSTD"])
            V(lambda e: e.reciprocal(RSTD[:], RSTD[:]), ["RSTD"], ["RSTD"])
            for k in range(8):
                V(lambda e, k=k: e.tensor_tensor(TMPB[:], X1[:, k, :], RSTD[:], ALU.mult), ["X1", "RSTD"], ["TMPB"])
                A(lambda e, k=k, col=col: e.activation(HT[:, k, :], TMPB[:], AF.Identity, bias=MOD3[:, 24 + k, col:col + 1], scale=A23[:, k, col:col + 1]),
                  ["TMPB", "MOD", "A2"], ["HT"])
            for k in range(8):
                pp, pk = (P2, "P2") if k % 2 == 0 else (P3, "P3")
                PE(lambda e, k=k, pp=pp: e.transpose(pp[:, 0:128], HT[:, k, :], ID[:]), ["HT", "ID"], [pk])
                A(lambda e, k=k, pp=pp: e.copy(HTOK[:, k * 128:(k + 1) * 128], pp[:, 0:128]), [pk], ["HTOK"])
            for qt in range(16):
                pp, pk = (P2, "P2") if qt % 2 == 0 else (P3, "P3")
                for k in range(8):
                    PE(lambda e, qt=qt, k=k, pp=pp: e.matmul(pp[:, 0:128], WQ[:, k, qt * 128:(qt + 1) * 128], HT[:, k, :], start=(k == 0), stop=(k == 7)),
                       ["WQ", "HT"], [pk])
                if qt % 2 == 0:
                    A(lambda e, qt=qt, pp=pp: e.copy(QT[:, qt, :], pp[:, 0:128]), [pk], ["TK"])
                else:
                    V(lambda e, qt=qt, pp=pp: e.tensor_copy(QT[:, qt, :], pp[:, 0:128]), [pk], ["TK"])
            for qt in range(16):
                PE(lambda e, qt=qt: e.matmul(P0[:, qt * 128:(qt + 1) * 128], QT[:, qt, :], KEYS[:, qt * 128:(qt + 1) * 128], start=True, stop=True),
                   ["TK", "KEYS"], ["P0a", "P0b"])
            for q4 in range(4):
                A(lambda e, q4=q4: e.copy(SC[:, q4 * 512:(q4 + 1) * 512], P0[:, q4 * 512:(q4 + 1) * 512]), ["P0a", "P0b"], ["TK"])
            for qt in range(16):
                top16(SC[:, qt * 128:(qt + 1) * 128], SC2[:, qt * 128:(qt + 1) * 128], M16[:, qt * 16:(qt + 1) * 16], I16[:, qt * 16:(qt + 1) * 16], 128)
            V(lambda e: e.tensor_copy(IF16[:], I16[:]), ["TK"], ["TK"])
            M4 = M16[:].rearrange("p (h q k) -> p h q k", q=2, k=16)
            IF4 = IF16[:].rearrange("p (h q k) -> p h q k", q=2, k=16)
            I1S3 = I1S[:].rearrange("p (h k) -> p h k", k=16)
            V(lambda e: e.tensor_scalar(I1S3, IF4[:, :, 0, :], 128.0, None, ALU.mult), ["TK"], ["TK"])
            CS4 = CS[:].rearrange("p (h a b) -> p h a b", a=16, b=16)
            V(lambda e: e.tensor_tensor(CS4, M4[:, :, 0, :].unsqueeze(3).to_broadcast([128, 8, 16, 16]),
                                        M4[:, :, 1, :].unsqueeze(2).to_broadcast([128, 8, 16, 16]), ALU.add), ["TK"], ["TK"])
            for h in range(8):
                top16(CS[:, h * 256:(h + 1) * 256], CS2[:], T16[:, h * 16:(h + 1) * 16], P16[:, h * 16:(h + 1) * 16], 256)
            V(lambda e: e.tensor_copy(PF[:], P16[:]), ["TK"], ["TK"])
            V(lambda e: e.tensor_scalar(AFL[:], PF[:], -7.5, 1.0 / 16, ALU.add, ALU.mult), ["TK"], ["TK"])
            V(lambda e: e.tensor_copy(AI[:], AFL[:]), ["TK"], ["TK"])
            V(lambda e: e.tensor_copy(AFL[:], AI[:]), ["TK"], ["TK"])
            V(lambda e: e.scalar_tensor_tensor(BFL[:], AFL[:], -16.0, PF[:], ALU.mult, ALU.add), ["TK"], ["TK"])
            EQ4 = CS[:].rearrange("p (h k a) -> p h k a", k=16, a=16)
            io4 = IOTA[:].unsqueeze(1).unsqueeze(1).to_broadcast([128, 8, 16, 16])
            for (sel, src, dst) in ((AFL, I1S3, E1), (BFL, IF4[:, :, 1, :], E2)):
                V(lambda e, sel=sel: e.tensor_tensor(EQ4, io4, sel[:].rearrange("p (h k) -> p h k", k=16).unsqueeze(3).to_broadcast([128, 8, 16, 16]), ALU.is_equal),
                  ["TK", "IOTA"], ["TK"])
                V(lambda e, src=src: e.tensor_tensor(EQ4, EQ4, src.unsqueeze(2).to_broadcast([128, 8, 16, 16]), ALU.mult), ["TK"], ["TK"])
                V(lambda e, dst=dst: e.tensor_reduce(dst[:], CS[:].rearrange("p (m a) -> p m a", a=16), AX.X, ALU.add), ["TK"], ["TK"])
            V(lambda e: e.tensor_tensor(E1[:], E1[:], E2[:], ALU.add), ["TK"], ["TK"])
            T3 = T16[:].rearrange("p (h k) -> p h k", k=16)
            V(lambda e: e.tensor_tensor(EG[:].rearrange("p (h k) -> p h k", k=16), T3, T3[:, :, 0:1].to_broadcast([128, 8, 16]), ALU.subtract), ["TK"], ["TK"])
            A(lambda e: e.activation(EG[:], EG[:], AF.Exp), ["TK"], ["TK"])
            V(lambda e: e.tensor_reduce(SS[:, 0:8], EG[:].rearrange("p (h k) -> p h k", k=16), AX.X, ALU.add), ["TK"], ["SS"])
            V(lambda e: e.reciprocal(SS[:, 0:8], SS[:, 0:8]), ["SS"], ["SS"])
            V(lambda e: e.tensor_tensor(GATE[:].rearrange("p (h k) -> p h k", k=16), EG[:].rearrange("p (h k) -> p h k", k=16),
                                        SS[:, 0:8].unsqueeze(2).to_broadcast([128, 8, 16]), ALU.mult), ["TK", "SS"], ["GATE"])
            PE(lambda e: e.transpose(P2[:, 0:128], E1[:], ID[:]), ["TK", "ID"], ["P2"])
            V(lambda e: e.tensor_copy(IDXTI[:], P2[:, 0:128]), ["P2"], ["IDXTI"])
            PE(lambda e: e.transpose(P3[:, 0:128], GATE[:], ID[:]), ["GATE", "ID"], ["P3"])
            A(lambda e: e.copy(GATET[:], P3[:, 0:128]), ["P3"], ["GATET"])
            for t in range(128):
                b = t % 2
                S.dma("pool", lambda e, t=t, b=b: e.indirect_dma_start(out=UG[b][:], out_offset=None, in_=eu,
                                                                       in_offset=bass.IndirectOffsetOnAxis(ap=IDXTI[:, t:t + 1], axis=0)),
                      reads=["IDXTI"], writes=["UG%d" % b])
                pk = "P0a" if b == 0 else "P0b"
                for hf in range(2):
                    PE(lambda e, t=t, b=b, hf=hf: e.matmul(P0[:, b * 1024 + hf * 512:b * 1024 + (hf + 1) * 512], ID[:, t:t + 1].to_broadcast([128, 128]),
                                                           HTOK[:, hf * 512:(hf + 1) * 512], start=True, stop=True), ["ID", "HTOK"], [pk])
                    A(lambda e, b=b, hf=hf: e.copy(HB[b][:, hf * 512:(hf + 1) * 512], P0[:, b * 1024 + hf * 512:b * 1024 + (hf + 1) * 512]), [pk], ["HB%d" % b])
                V(lambda e, t=t, b=b: e.scalar_tensor_tensor(UG[b][:], UG[b][:], 1.0, HB[b][:], ALU.mult, ALU.mult, accum_out=ACTT[:, t:t + 1]),
                  ["HB%d" % b], ["UG%d" % b, "ACTT"])
            A(lambda e: e.activation(WT[:], ACTT[:], AF.Gelu), ["ACTT"], ["WT"])
            V(lambda e: e.tensor_tensor(WT[:], WT[:], GATET[:], ALU.mult), ["GATET"], ["WT"])
            for t in range(128):
                b = t % 2
                S.dma("pool", lambda e, t=t, b=b: e.indirect_dma_start(out=VG[b][:], out_offset=None, in_=ev,
                                                                       in_offset=bass.IndirectOffsetOnAxis(ap=IDXTI[:, t:t + 1], axis=0)),
                      reads=["IDXTI"], writes=["UG%d" % b])
                for ot in range(8):
                    PE(lambda e, t=t, b=b, ot=ot: e.matmul(P1[:, ot * 128 + t:ot * 128 + t + 1], VG[b][:, ot * 128:(ot + 1) * 128], WT[:, t:t + 1],
                                                           start=True, stop=True), ["UG%d" % b, "WT"], ["P1"])
            for ot in range(8):
                V(lambda e, ot=ot, col=col: e.scalar_tensor_tensor(XO[:, ot, :], P1[:, ot * 128:(ot + 1) * 128], MOD3[:, 40 + ot, col:col + 1], X1[:, ot, :],
                                                                   ALU.mult, ALU.add), ["P1", "MOD", "X1"], ["XT"])
            if last:
                for k in range(8):
                    A(lambda e, k=k: e.activation(TMPB[:], XO[:, k, :], AF.Square), ["XT"], ["TMPB"])
                    PE(lambda e, k=k: e.matmul(P2[:, 0:128], ONES[:], TMPB[:], start=(k == 0), stop=(k == 7)), ["ONES", "TMPB"], ["P2"])
                V(lambda e: e.tensor_scalar(RSTD[:], P2[:, 0:128], 1.0 / D, EPS, ALU.mult, ALU.add), ["P2"], ["RSTD"])
                A(lambda e: e.sqrt(RSTD[:], RSTD[:]), ["RSTD"], ["RSTD"])
                V(lambda e: e.reciprocal(RSTD[:], RSTD[:]), ["RSTD"], ["RSTD"])
                for k in range(8):
                    V(lambda e, k=k: e.scalar_tensor_tensor(XO[:, k, :], XO[:, k, :], GF[:, k:k + 1], RSTD[:], ALU.mult, ALU.mult), ["RSTD", "GF"], ["XT"])
            S.dma("sp", lambda e, tk=tk: e.dma_start(out=xo_v[:, :, tk], in_=XO[:]), reads=["XT"], writes=["xo_%d" % bi])
        S.drain_all("sp")
        S.emit()
    return nc


SEQC = 256
SEQL = 4096
OWN = 2176


def _mirror(t0, T):
    if t0 < SEQC:
        return 0, SEQC
    i = (t0 - SEQC) // 512
    return SEQC + SEQL - 512 * (i + 1), 512


def emit_A(nc, S, sb, ps, io, tl_all=False):
    xT, cT, w_mod, b_mod, g1, w_in, ones = io["xT"], io["cT"], io["w_mod"], io["b_mod"], io["g1"], io["w_in"], io["ones"]
    FMU, FMQ, FMK, FMZ, VT, TL, MODS = io["FMU"], io["FMQ"], io["FMK"], io["FMZ"], io["VT"], io["TL"], io["MODS"]
    CT = sb("CT", [128, 16]); SC = sb("SC", [128, 16]); BM = sb("BM", [128, 48]); G1 = sb("G1", [128, 8])
    ONES = sb("ONES", [128, 128]); MOD = sb("MOD", [128, 96])
    A1 = sb("A1", [128, 16]); TMPA = sb("TMPA", [128, 16])
    WM = [sb("WM%d" % i, [128, 8, 512]) for i in range(2)]
    WIN = sb("WIN", [128, 8, INW])
    XT = [sb("XT%d" % i, [128, 8, 512]) for i in range(2)]
    XSQ = sb("XSQ", [128, 512]); RSTD = sb("RSTD", [128, 512]); TMP = sb("TMP", [128, 512])
    HT = [sb("HT%d" % i, [128, 8, 512]) for i in range(2)]
    OUTB = [sb("OUTB%d" % i, [128, 512]) for i in range(4)]
    pmod = ps("pmod", [128, 96]); pss = ps("pss", [128, 512])
    pout = [ps("pout%d" % i, [128, 512]) for i in range(3)]
    S.dma("sp", lambda e: e.dma_start(out=CT[:], in_=cT), writes=["CT"])
    S.dma("sp", lambda e: e.dma_start(out=BM[:], in_=b_mod), writes=["BM"])
    S.dma("sp", lambda e: e.dma_start(out=G1[:], in_=g1), writes=["G1"])
    S.dma("sp", lambda e: e.dma_start(out=ONES[:], in_=ones), writes=["ONES"])
    S.op("act", lambda e: e.activation(SC[:], CT[:], AF.Silu), reads=["CT"], writes=["SC"])
    SC3 = SC[:].rearrange("p (k c) -> p k c", c=2)
    wm_v = w_mod.rearrange("(k p) f -> p k f", p=128)
    for jg in range(12):
        b = jg % 2
        S.dma("act" if jg % 2 else "sp",
              lambda e, jg=jg, b=b: e.dma_start(out=WM[b][:], in_=wm_v[:, :, jg * 512:(jg + 1) * 512]), writes=["WM%d" % b])
        for j8 in range(4):
            j = jg * 4 + j8
            for k in range(8):
                S.op("pe", lambda e, j=j, j8=j8, k=k, b=b: e.matmul(
                    pmod[:, 2 * j:2 * j + 2], WM[b][:, k, j8 * 128:(j8 + 1) * 128], SC3[:, k, :],
                    start=(k == 0), stop=(k == 7)), reads=["WM%d" % b, "SC"], writes=["pmod"])
    S.op("dve", lambda e: e.tensor_tensor(MOD[:].rearrange("p (j c) -> p j c", c=2), pmod[:].rearrange("p (j c) -> p j c", c=2),
                                          BM[:].unsqueeze(2).to_broadcast([128, 48, 2]), ALU.add), reads=["pmod", "BM"], writes=["MOD"])
    S.dma("sp", lambda e: e.dma_start(out=MODS, in_=MOD[:]), reads=["MOD"], writes=["MODS"])
    MOD3 = MOD[:].rearrange("p (j c) -> p j c", c=2)
    S.op("dve", lambda e: e.tensor_scalar(TMPA[:].rearrange("p (j c) -> p j c", c=2), MOD3[:, 8:16, :], 1.0, None, ALU.add), reads=["MOD"], writes=["TMPA"])
    S.op("dve", lambda e: e.tensor_tensor(A1[:].rearrange("p (j c) -> p j c", c=2), TMPA[:].rearrange("p (j c) -> p j c", c=2),
                                          G1[:].unsqueeze(2).to_broadcast([128, 8, 2]), ALU.mult), reads=["TMPA", "G1"], writes=["A1"])
    A13 = A1[:].rearrange("p (j c) -> p j c", c=2)
    win_v = w_in.rearrange("(k p) f -> p k f", p=128)
    for k in range(8):
        S.dma("pool", lambda e, k=k: e.dma_start(out=WIN[:, k, :], in_=win_v[:, k, :]), writes=["WIN%d" % k])
    WK = ["WIN%d" % k for k in range(8)]
    xT_v = xT.rearrange("(k p) t -> p k t", p=128)
    grp = [(0, SEQC, 1, -1)] + [(SEQC + 512 * g, 512, 0, g) for g in range(8)]
    cnt = {"o": 0}

    def evac(dst_ap_fn, pb, m, tn, key, cm=False):
        ob = cnt["o"] % 4
        cnt["o"] += 1
        if cm:
            o_ap = lambda: OUTB[ob][:m, :512].rearrange("p (w r) -> p w r", r=8)
            i_ap = lambda: pout[pb][:m, :512].rearrange("p (r w) -> p w r", w=64)
        else:
            o_ap = lambda: OUTB[ob][:m, :tn]
            i_ap = lambda: pout[pb][:m, :tn]
        if cnt["o"] % 2:
            S.op("act", lambda e: e.copy(o_ap(), i_ap()), reads=["pout%d" % pb], writes=["OUTB%d" % ob])
        else:
            S.op("dve", lambda e: e.tensor_copy(o_ap(), i_ap()), reads=["pout%d" % pb], writes=["OUTB%d" % ob])
        S.dma("sp" if cnt["o"] % 2 else "act", lambda e: dst_ap_fn(e, OUTB[ob]), reads=["OUTB%d" % ob], writes=["%s_%d" % (key, cnt["o"])])

    pi = 0
    for gi, (t0, tn, col, g) in enumerate(grp):
        b = gi % 2
        xk, hk = "XT%d" % b, "HT%d" % b
        S.dma("sp", lambda e, b=b, t0=t0, tn=tn: e.dma_start(out=XT[b][:, :, :tn], in_=xT_v[:, :, t0:t0 + tn]), writes=[xk])
        for k in range(8):
            S.op("act", lambda e, b=b, k=k, tn=tn: e.activation(XSQ[:, :tn], XT[b][:, k, :tn], AF.Square), reads=[xk], writes=["XSQ"])
            S.op("pe", lambda e, k=k, tn=tn: e.matmul(pss[:, :tn], ONES[:], XSQ[:, :tn], start=(k == 0), stop=(k == 7)), reads=["ONES", "XSQ"], writes=["pss"])
        S.op("dve", lambda e, tn=tn: e.tensor_scalar(RSTD[:, :tn], pss[:, :tn], 1.0 / D, EPS, ALU.mult, ALU.add), reads=["pss"], writes=["RSTD"])
        S.op("act", lambda e, tn=tn: e.sqrt(RSTD[:, :tn], RSTD[:, :tn]), reads=["RSTD"], writes=["RSTD"])
        S.op("dve", lambda e, tn=tn: e.reciprocal(RSTD[:, :tn], RSTD[:, :tn]), reads=["RSTD"], writes=["RSTD"])
        for k in range(8):
            S.op("dve", lambda e, b=b, k=k, tn=tn: e.tensor_tensor(TMP[:, :tn], XT[b][:, k, :tn], RSTD[:, :tn], ALU.mult), reads=[xk, "RSTD"], writes=["TMP"])
            S.op("act", lambda e, b=b, k=k, tn=tn, col=col: e.activation(HT[b][:, k, :tn], TMP[:, :tn], AF.Identity, bias=MOD3[:, k, col:col + 1],
                                                                         scale=A13[:, k, col:col + 1]), reads=["TMP", "MOD", "A1"], writes=[hk])
        fm = [(FMU, 0, 512, 128), (FMU, 128, 640, 128), (FMQ, 0, 768, 128), (FMQ, 128, 896, 128), (FMK, 0, 1024, 128), (FMK, 128, 1152, 128), (FMZ, 0, 2304, 32)]
        for (dst, r0, c0, m) in fm:
            pb = pi % 3
            pi += 1
            for k in range(8):
                S.op("pe", lambda e, b=b, k=k, tn=tn, c0=c0, m=m, pb=pb: e.matmul(pout[pb][:m, :tn], WIN[:, k, c0:c0 + m], HT[b][:, k, :tn], start=(k == 0), stop=(k == 7)),
                     reads=WK + [hk], writes=["pout%d" % pb])
            if dst is FMU or g < 0:
                evac(lambda e, ob, dst=dst, r0=r0, m=m, t0=t0, tn=tn: e.dma_start(out=dst[r0:r0 + m, t0:t0 + tn], in_=ob[:m, :tn]), pb, m, tn, "fm")
            else:
                evac(lambda e, ob, dst=dst, r0=r0, m=m, g=g: e.dma_start(
                    out=dst[r0:r0 + m, SEQC:].rearrange("p (w r) -> p w r", r=64)[:, :, 8 * g:8 * g + 8],
                    in_=ob[:m, :512].rearrange("p (w r) -> p w r", r=8)), pb, m, 512, "fm", cm=True)
        for ti in range(tn // 128):
            tl = [(VT, t0 + ti * 128, 0, 1280)]
            own_row = None
            if tl_all:
                own_row = t0 + ti * 128
            elif g < 0 and ti == 0:
                own_row = 0
            elif 0 <= g < 4:
                own_row = 128 + g * 512 + ti * 128
            if own_row is not None:
                tl += [(TL, own_row, 0, 0), (TL, own_row, 512, 1792)]
            for (dst, row, dc, c0) in tl:
                pb = pi % 3
                pi += 1
                for k in range(8):
                    S.op("pe", lambda e, b=b, k=k, ti=ti, c0=c0, pb=pb: e.matmul(pout[pb][:, :], HT[b][:, k, ti * 128:(ti + 1) * 128], WIN[:, k, c0:c0 + 512],
                                                                               start=(k == 0), stop=(k == 7)), reads=WK + [hk], writes=["pout%d" % pb])
                evac(lambda e, ob, dst=dst, row=row, dc=dc: e.dma_start(out=dst[row:row + 128, dc:dc + 512], in_=ob[:, :]), pb, 128, 512, "tm")


def emit_B(nc, S, sb, ps, io):
    FMU, YFs, YBs = io["FMU"], io["YF"], io["YB"]
    tau, ident = io["tau"], io["ident"]
    for ct in range(2):
        prm, bre, bim, cre, cim = io["prm"][ct], io["bre"][ct], io["bim"][ct], io["cre"][ct], io["cim"][ct]
        uin = [FMU, FMU]
        yout = [YFs, YBs]
        X = "c%d_" % ct
        sub = ExitStack()
        sb, ps = _mk(nc, sub)
        TWO_PI = 2.0 * math.pi
        PRM = sb(X + "PRM", [128, 24]); BRE = sb(X + "BRE", [128, 128]); BIM = sb(X + "BIM", [128, 128])
        CRE = sb(X + "CRE", [128, 128]); CIM = sb(X + "CIM", [128, 128]); TAU = sb(X + "TAU", [128, 512]); ID = sb(X + "ID", [128, 128])
        names = ["DT", "LR", "MAG", "TH", "R", "R2", "RF", "FR", "SIN", "COS", "ARE", "AIM", "DEN", "AM1", "FRE", "FIM", "T0", "T1"]
        P = {n: sb(X + "p_" + n, [128, 8]) for n in names}
        RI = sb(X + "p_RI", [128, 8], I32)
        BBR = sb(X + "BBR", [128, 128]); BBI = sb(X + "BBI", [128, 128]); TB = sb(X + "TB", [128, 128])
        PAD = sb(X + "PAD", [128, 128])
        WBR = sb(X + "WBR", [128, 8, 128]); WBI = sb(X + "WBI", [128, 8, 128]); CR = sb(X + "CR", [128, 8, 128]); CIN = sb(X + "CIN", [128, 8, 128])
        TC = sb(X + "TC", [128, 8, 512]); TS = sb(X + "TS", [128, 8, 512]); RHO = sb(X + "RHO", [128, 8, 512])
        RR = sb(X + "RR", [128, 512]); RRF = sb(X + "RRF", [128, 512]); RRI = sb(X + "RRI", [128, 512], I32)
        UC = [sb(X + "UC%d" % i, [128, 512]) for i in range(2)]
        W = {}
        for n in ("BR", "BI", "T1", "T2", "T3", "T4", "XR", "XI", "QR", "QI", "HR", "HI"):
            for i in range(2):
                W[n, i] = sb(X + "w_%s%d" % (n, i), [128, 512])
        HP = sb(X + "HP", [128, 8])
        YO = [sb(X + "YO%d" % i, [128, 512]) for i in range(2)]
        pbr = [ps(X + "pbr%d" % i, [128, 512]) for i in range(2)]
        pbi = [ps(X + "pbi%d" % i, [128, 512]) for i in range(2)]
        py = [ps(X + "py%d" % i, [128, 512]) for i in range(2)]
        ptr = ps(X + "ptr", [128, 128])

        for (t, src, k) in ((PRM, prm, "PRM"), (BRE, bre, "BRE"), (BIM, bim, "BIM"), (CRE, cre, "CRE"), (CIM, cim, "CIM"),
                            (TAU, tau, "TAU"), (ID, ident, "ID")):
            S.dma("sp", lambda e, t=t, src=src: e.dma_start(out=t[:], in_=src), writes=[k])
        PR3 = PRM[:].rearrange("p (a c) -> p a c", c=3)
        K = ["PP"]

        def V(fn, reads=(), writes=()):
            S.op("dve", fn, reads=list(reads) + K, writes=list(writes) + K)

        def A(fn, reads=(), writes=()):
            S.op("act", fn, reads=list(reads) + K, writes=list(writes) + K)

        A(lambda e: e.activation(P["DT"][:], PR3[:, :, 2], AF.Exp), reads=["PRM"])
        V(lambda e: e.tensor_scalar(P["LR"][:], PR3[:, :, 0], -1e-4, None, ALU.min), reads=["PRM"])
        V(lambda e: e.tensor_tensor(P["T0"][:], P["LR"][:], P["DT"][:], ALU.mult))
        A(lambda e: e.activation(P["MAG"][:], P["T0"][:], AF.Exp))
        V(lambda e: e.tensor_tensor(P["TH"][:], PR3[:, :, 1], P["DT"][:], ALU.mult), reads=["PRM"])
        V(lambda e: e.tensor_scalar(P["R"][:], P["TH"][:], 1.0 / TWO_PI, None, ALU.mult))
        V(lambda e: e.tensor_scalar(P["R2"][:], P["R"][:], 0.25, None, ALU.add))
        for (src, dst) in (("R", "SIN"), ("R2", "COS")):
            V(lambda e, src=src: e.tensor_copy(RI[:], P[src][:]))
            V(lambda e: e.tensor_copy(P["RF"][:], RI[:]))
            V(lambda e, src=src: e.tensor_tensor(P["FR"][:], P[src][:], P["RF"][:], ALU.subtract))
            A(lambda e, dst=dst: e.activation(P[dst][:], P["FR"][:], AF.Sin, scale=TWO_PI))
        V(lambda e: e.tensor_tensor(P["ARE"][:], P["MAG"][:], P["COS"][:], ALU.mult))
        V(lambda e: e.tensor_tensor(P["AIM"][:], P["MAG"][:], P["SIN"][:], ALU.mult))
        V(lambda e: e.tensor_tensor(P["T0"][:], P["LR"][:], P["LR"][:], ALU.mult))
        V(lambda e: e.tensor_tensor(P["T1"][:], PR3[:, :, 1], PR3[:, :, 1], ALU.mult), reads=["PRM"])
        V(lambda e: e.tensor_tensor(P["DEN"][:], P["T0"][:], P["T1"][:], ALU.add))
        V(lambda e: e.reciprocal(P["DEN"][:], P["DEN"][:]))
        V(lambda e: e.tensor_scalar(P["AM1"][:], P["ARE"][:], -1.0, None, ALU.add))
        V(lambda e: e.tensor_tensor(P["T0"][:], P["AM1"][:], P["LR"][:], ALU.mult))
        V(lambda e: e.tensor_tensor(P["T1"][:], P["AIM"][:], PR3[:, :, 1], ALU.mult), reads=["PRM"])
        V(lambda e: e.tensor_tensor(P["T0"][:], P["T0"][:], P["T1"][:], ALU.add))
        V(lambda e: e.tensor_tensor(P["FRE"][:], P["T0"][:], P["DEN"][:], ALU.mult))
        V(lambda e: e.tensor_tensor(P["T0"][:], P["AIM"][:], P["LR"][:], ALU.mult))
        V(lambda e: e.tensor_tensor(P["T1"][:], P["AM1"][:], PR3[:, :, 1], ALU.mult), reads=["PRM"])
        V(lambda e: e.tensor_tensor(P["T0"][:], P["T0"][:], P["T1"][:], ALU.subtract))
        V(lambda e: e.tensor_tensor(P["FIM"][:], P["T0"][:], P["DEN"][:], ALU.mult))

        def v3(t):
            return t[:].rearrange("p (a h) -> p a h", h=16)

        def bc(n):
            return P[n][:].unsqueeze(2).to_broadcast([128, 8, 16])
        V(lambda e: e.tensor_tensor(v3(BBR), v3(BRE), bc("FRE"), ALU.mult), reads=["BRE"])
        V(lambda e: e.tensor_tensor(v3(TB), v3(BIM), bc("FIM"), ALU.mult), reads=["BIM"])
        V(lambda e: e.tensor_tensor(BBR[:], BBR[:], TB[:], ALU.subtract))
        V(lambda e: e.tensor_tensor(v3(BBI), v3(BIM), bc("FRE"), ALU.mult), reads=["BIM"])
        V(lambda e: e.tensor_tensor(v3(TB), v3(BRE), bc("FIM"), ALU.mult), reads=["BRE"])
        V(lambda e: e.tensor_tensor(BBI[:], BBI[:], TB[:], ALU.add))
        V(lambda e: e.tensor_scalar(CIM[:], CIM[:], -1.0, None, ALU.mult), reads=["CIM"], writes=["CIM"])
        V(lambda e: e.memset(CR[:], 0.0)); V(lambda e: e.memset(CIN[:], 0.0))
        for dj in range(8):
            j = dj % 4
            for (src, dst) in ((BBR, WBR), (BBI, WBI)):
                V(lambda e: e.memset(PAD[:], 0.0), writes=["PAD"])
                V(lambda e, src=src, dj=dj, j=j: e.tensor_copy(PAD[0:64, 32 * j:32 * j + 16], src[0:64, dj * 16:dj * 16 + 16]), writes=["PAD"])
                V(lambda e, src=src, dj=dj, j=j: e.tensor_copy(PAD[64:128, 32 * j + 16:32 * j + 32], src[64:128, dj * 16:dj * 16 + 16]), writes=["PAD"])
                S.op("pe", lambda e: e.transpose(ptr[:], PAD[:], ID[:]), reads=["PAD", "ID"], writes=["ptr"])
                S.op("act", lambda e, dst=dst, dj=dj: e.copy(dst[:, dj, :], ptr[:]), reads=["ptr"], writes=["WB"])
            for (src, dst) in ((CRE, CR), (CIM, CIN)):
                V(lambda e, src=src, dst=dst, dj=dj, j=j: e.tensor_copy(dst[0:64, dj, 32 * j:32 * j + 16], src[0:64, dj * 16:dj * 16 + 16]), reads=["CRE", "CIM"], writes=["CC"])
                V(lambda e, src=src, dst=dst, dj=dj, j=j: e.tensor_copy(dst[64:128, dj, 32 * j + 16:32 * j + 32], src[64:128, dj * 16:dj * 16 + 16]), reads=["CRE", "CIM"], writes=["CC"])
            for (off, dst) in ((0.0, TS), (0.25, TC)):
                V(lambda e, dj=dj, off=off: e.tensor_scalar(RR[:], TAU[:], P["R"][:, dj:dj + 1], off, ALU.mult, ALU.add), reads=["TAU"], writes=["RR"])
                V(lambda e: e.tensor_copy(RRI[:], RR[:]), reads=["RR"], writes=["RRI"])
                V(lambda e: e.tensor_copy(RRF[:], RRI[:]), reads=["RRI"], writes=["RRF"])
                V(lambda e: e.tensor_tensor(RRF[:], RR[:], RRF[:], ALU.subtract), reads=["RR"], writes=["RRF"])
                S.op("act", lambda e, dst=dst, dj=dj: e.activation(dst[:, dj, :], RRF[:], AF.Sin, scale=TWO_PI), reads=["RRF"], writes=["TAB"])
            V(lambda e, dj=dj: e.tensor_copy(RHO[:, dj, :], P["MAG"][:, dj:dj + 1].to_broadcast([128, 512])), writes=["TAB"])

        G = "pool"
        oi = 0
        for d in range(2):
            V(lambda e: e.memset(HP[:], 0.0), writes=["HP"])
            for ci, (t0, T) in enumerate(S5_CH):
                ub_ = (d * 9 + ci) % 2
                uk = "UC%d" % ub_
                n0 = t0 if d == 0 else _mirror(t0, T)[0]
                S.dma("sp", lambda e, d=d, n0=n0, T=T, ub_=ub_, ct=ct: e.dma_start(out=UC[ub_][:, :T], in_=uin[d][ct * 128:(ct + 1) * 128, n0:n0 + T]), writes=[uk])
                ucv = (lambda ub_=ub_, T=T: UC[ub_][:, :T]) if d == 0 else (lambda ub_=ub_, T=T: UC[ub_][:, :T][:, ::-1])
                yb_ = (d * 9 + ci) % 2
                for j in range(4):
                    dj = d * 4 + j
                    b = j % 2
                    w = lambda n, b=b, T=T: W[n, b][:, :T]
                    k = lambda n, b=b: "w_%s%d" % (n, b)
                    S.op("pe", lambda e, dj=dj, b=b, T=T, ucv=ucv: e.matmul(pbr[b][:, :T], WBR[:, dj, :], ucv(), start=True, stop=True),
                         reads=["WB", uk], writes=["pbr%d" % b])
                    S.op("pe", lambda e, dj=dj, b=b, T=T, ucv=ucv: e.matmul(pbi[b][:, :T], WBI[:, dj, :], ucv(), start=True, stop=True),
                         reads=["WB", uk], writes=["pbi%d" % b])
                    S.op("act", lambda e, w=w, b=b, T=T: e.copy(w("BR"), pbr[b][:, :T]), reads=["pbr%d" % b], writes=[k("BR")])
                    S.op("act", lambda e, w=w, b=b, T=T: e.copy(w("BI"), pbi[b][:, :T]), reads=["pbi%d" % b], writes=[k("BI")])
                    cs = lambda dj=dj, T=T: TC[:, dj, :T]
                    sn = lambda dj=dj, T=T: TS[:, dj, :T]
                    S.op("dve", lambda e, w=w, cs=cs: e.tensor_tensor(w("T1"), cs(), w("BR"), ALU.mult), reads=["TAB", k("BR")], writes=[k("T1")])
                    S.op("dve", lambda e, w=w, sn=sn: e.tensor_tensor(w("T2"), sn(), w("BI"), ALU.mult), reads=["TAB", k("BI")], writes=[k("T2")])
                    S.op("dve", lambda e, w=w: e.tensor_tensor(w("XR"), w("T1"), w("T2"), ALU.add), reads=[k("T1"), k("T2")], writes=[k("XR")])
                    S.op(G, lambda e, w=w, cs=cs: e.tensor_tensor(w("T3"), cs(), w("BI"), ALU.mult), reads=["TAB", k("BI")], writes=[k("T3")])
                    S.op(G, lambda e, w=w, sn=sn: e.tensor_tensor(w("T4"), sn(), w("BR"), ALU.mult), reads=["TAB", k("BR")], writes=[k("T4")])
                    S.op(G, lambda e, w=w: e.tensor_tensor(w("XI"), w("T3"), w("T4"), ALU.subtract), reads=[k("T3"), k("T4")], writes=[k("XI")])
                    S.op("dve", lambda e, w=w, dj=dj, j=j, T=T: e.tensor_tensor_scan(w("QR"), RHO[:, dj, :T], w("XR"), HP[:, 2 * j:2 * j + 1], ALU.mult, ALU.add),
                         reads=["TAB", k("XR"), "HP"], writes=[k("QR")])
                    S.op("dve", lambda e, w=w, dj=dj, j=j, T=T: e.tensor_tensor_scan(w("QI"), RHO[:, dj, :T], w("XI"), HP[:, 2 * j + 1:2 * j + 2], ALU.mult, ALU.add),
                         reads=["TAB", k("XI"), "HP"], writes=[k("QI")])
                    S.op("dve", lambda e, w=w, cs=cs: e.tensor_tensor(w("T1"), cs(), w("QR"), ALU.mult), reads=["TAB", k("QR")], writes=[k("T1")])
                    S.op("dve", lambda e, w=w, sn=sn: e.tensor_tensor(w("T2"), sn(), w("QI"), ALU.mult), reads=["TAB", k("QI")], writes=[k("T2")])
                    S.op("dve", lambda e, w=w: e.tensor_tensor(w("HR"), w("T1"), w("T2"), ALU.subtract), reads=[k("T1"), k("T2")], writes=[k("HR")])
                    S.op(G, lambda e, w=w, sn=sn: e.tensor_tensor(w("T3"), sn(), w("QR"), ALU.mult), reads=["TAB", k("QR")], writes=[k("T3")])
                    S.op(G, lambda e, w=w, cs=cs: e.tensor_tensor(w("T4"), cs(), w("QI"), ALU.mult), reads=["TAB", k("QI")], writes=[k("T4")])
                    S.op(G, lambda e, w=w: e.tensor_tensor(w("HI"), w("T3"), w("T4"), ALU.add), reads=[k("T3"), k("T4")], writes=[k("HI")])
                    S.op("act", lambda e, b=b, j=j, T=T: e.copy(HP[:, 2 * j:2 * j + 1], W["HR", b][:, T - 1:T]), reads=[k("HR")], writes=["HP"])
                    S.op("act", lambda e, b=b, j=j, T=T: e.copy(HP[:, 2 * j + 1:2 * j + 2], W["HI", b][:, T - 1:T]), reads=[k("HI")], writes=["HP"])
                    S.op("pe", lambda e, dj=dj, w=w, j=j, yb_=yb_, T=T: e.matmul(py[yb_][:, :T], CR[:, dj, :], w("HR"), start=(j == 0), stop=False),
                         reads=["CC", k("HR")], writes=["py%d" % yb_])
                    S.op("pe", lambda e, dj=dj, w=w, j=j, yb_=yb_, T=T: e.matmul(py[yb_][:, :T], CIN[:, dj, :], w("HI"), start=False, stop=(j == 3)),
                         reads=["CC", k("HI")], writes=["py%d" % yb_])
                if d == 0:
                    S.op("act", lambda e, yb_=yb_, T=T: e.copy(YO[yb_][:, :T], py[yb_][:, :T]), reads=["py%d" % yb_], writes=["YO%d" % yb_])
                else:
                    S.op("act", lambda e, yb_=yb_, T=T: e.copy(YO[yb_][:, :T][:, ::-1], py[yb_][:, :T]), reads=["py%d" % yb_], writes=["YO%d" % yb_])
                oi += 1
                S.dma("act", lambda e, d=d, yb_=yb_, n0=n0, T=T, ct=ct: e.dma_start(out=yout[d][ct * 128:(ct + 1) * 128, n0:n0 + T], in_=YO[yb_][:, :T]),
                      reads=["YO%d" % yb_], writes=["yout_%d" % oi])
        S.sync_all()
        S.emit()
        sub.close()


def emit_C(nc, S, sb_unused, ps_unused, io):
    FMQ, FMK, FMZ, VT = io["FMQ"], io["FMK"], io["FMZ"], io["VT"]
    OS = [io["OF"], io["OB"]]
    rst, tmask, tmask2, blk, hmask, ident = io["rst"], io["tmask"], io["tmask2"], io["blk"], io["hmask"], io["ident"]
    QSC = 32 ** -0.5
    for hh in range(2):
        X = "h%d_" % hh
        sub = ExitStack()
        sb, ps = _mk(nc, sub)
        cols = slice(hh * 256, hh * 256 + 256)
        TM2 = sb(X + "TM2", [64, 256])
        S.dma("sp", lambda e: e.dma_start(out=TM2[:], in_=tmask2), writes=["TM2"])
        RST = sb(X + "RST", [128, 512]); TM = sb(X + "TM", [64, 256]); BLK = sb(X + "BLK", [128, 256]); HM = sb(X + "HM", [128, 4]); ID = sb(X + "ID", [128, 128])
        WG = sb(X + "WG", [16, 128]); BG = sb(X + "BG", [128, 1]); NBG = sb(X + "NBG", [128, 1])
        Wb = {}
        for n in ("Q", "K", "LA", "B", "E", "D", "QE", "QS", "KD", "KS0", "KS1", "KS2", "KS3"):
            for i in range(2):
                Wb[n, i] = sb(X + "g_%s%d" % (n, i), [128, 512])
        Z = [sb(X + "Z%d" % i, [16, 512]) for i in range(2)]
        VV = [sb(X + "VV%d" % i, [64, 8, 256]) for i in range(2)]
        DEC = [sb(X + "DEC%d" % i, [128, 8]) for i in range(2)]
        KDT = [sb(X + "KDT%d" % i, [64, 128]) for i in range(2)]
        STt = [sb(X + "ST%d" % i, [64, 256]) for i in range(2)]
        OB = [sb(X + "OB%d" % i, [64, 256]) for i in range(3)]
        KVM = sb(X + "KVM", [128, 256])
        SS = [sb(X + "SS%d" % i, [128, 256]) for i in range(2)]
        pza = ps(X + "pza", [128, 512])
        pt0 = ps(X + "pt0", [64, 128])
        pt = [pt0, pt0]
        pkv = [ps(X + "pkv%d" % i, [128, 256]) for i in range(2)]
        psc = [ps(X + "psc%d" % i, [64, 256]) for i in range(2)]
        po = [ps(X + "po%d" % i, [64, 256]) for i in range(2)]
        for (t, src, k) in ((RST, rst, "RST"), (TM, tmask, "TM"), (BLK, blk, "BLK"), (HM, hmask, "HM"), (ID, ident, "ID")):
            S.dma("sp", lambda e, t=t, src=src: e.dma_start(out=t[:], in_=src), writes=[k])
        gc = 0
        oi = 0
        for d in range(2):
            S.dma("sp", lambda e, d=d, hh=hh: e.dma_start(out=WG[:], in_=io["wg"][hh][d]), writes=["WG"])
            S.dma("sp", lambda e, d=d, hh=hh: e.dma_start(out=BG[:], in_=io["bg"][hh][d]), writes=["BG"])
            S.op("dve", lambda e: e.tensor_scalar(NBG[:], BG[:], -1.0, None, ALU.mult), reads=["BG"], writes=["NBG"])
            S.op("dve", lambda e: e.memset(SS[0][:], 0.0), writes=["SS0"])
            scur = 0
            for bi, (t0, T) in enumerate(S5_CH):
                nchk = T // 64
                n0 = t0 if d == 0 else _mirror(t0, T)[0]
                w0 = (n0 - SEQC) // 64
                b = (d * 9 + bi) % 2
                w = lambda n, b=b, T=T: Wb[n, b][:, :T]
                k = lambda n, b=b: "g_%s%d" % (n, b)
                w3 = lambda n, b=b, T=T: Wb[n, b][:, :T].rearrange("p (c s) -> p c s", s=64)
                S.dma("sp", lambda e, d=d, b=b, n0=n0, T=T, hh=hh: e.dma_start(out=Wb["Q", b][:, :T], in_=FMQ[hh * 128:(hh + 1) * 128, n0:n0 + T]), writes=[k("Q")])
                S.dma("act", lambda e, d=d, b=b, n0=n0, T=T, hh=hh: e.dma_start(out=Wb["K", b][:, :T], in_=FMK[hh * 128:(hh + 1) * 128, n0:n0 + T]), writes=[k("K")])
                S.dma("sp", lambda e, d=d, b=b, n0=n0, T=T: e.dma_start(out=Z[b][:, :T], in_=FMZ[d * 16:(d + 1) * 16, n0:n0 + T]), writes=["Z%d" % b])
                if t0 < SEQC:
                    S.dma("act", lambda e, b=b, nchk=nchk, cols=cols: e.dma_start(
                        out=VV[b][:, :nchk, :], in_=VT[0:SEQC, cols].rearrange("(c s) f -> s c f", s=64)), writes=["VV%d" % b])
                else:
                    S.dma("act", lambda e, b=b, w0=w0, cols=cols: e.dma_start(
                        out=VV[b][:, :, :], in_=VT[SEQC:, cols].rearrange("(r w) f -> r w f", w=64)[:, w0:w0 + 8, :]), writes=["VV%d" % b])
                S.op("pe", lambda e, b=b, T=T: e.matmul(pza[:, :T], WG[:], Z[b][:, :T], start=True, stop=True), reads=["WG", "Z%d" % b], writes=["pza"])
                S.op("act", lambda e, w=w, T=T: e.activation(w("E"), pza[:, :T], AF.Exp, bias=NBG[:], scale=-1.0), reads=["pza", "NBG"], writes=[k("E")])
                S.op("act", lambda e, w=w: e.activation(w("E"), w("E"), AF.Ln, bias=1.0), reads=[k("E")], writes=[k("E")])
                S.op("dve", lambda e, w=w: e.tensor_scalar(w("LA"), w("E"), -1.0 / 16.0, None, ALU.mult), reads=[k("E")], writes=[k("LA")])
                S.op("dve", lambda e, w=w, T=T: e.tensor_tensor_scan(w("B"), RST[:, :T], w("LA"), 0.0, ALU.mult, ALU.add), reads=["RST", k("LA")], writes=[k("B")])
                iref, ilast = (32, 63) if d == 0 else (31, 0)
                if d == 1:
                    S.op("dve", lambda e, w3=w3, nchk=nchk: e.tensor_tensor(w3("D"), w3("B")[:, :, 63:64].to_broadcast([128, nchk, 64]), w3("B"), ALU.subtract),
                         reads=[k("B")], writes=[k("D")])
                    S.op("dve", lambda e, w=w: e.tensor_tensor(w("B"), w("D"), w("LA"), ALU.add), reads=[k("D"), k("LA")], writes=[k("B")])
                S.op("act", lambda e, b=b, w3=w3, nchk=nchk, ilast=ilast: e.activation(DEC[b][:, :nchk], w3("B")[:, :, ilast], AF.Exp), reads=[k("B")], writes=["DEC%d" % b])
                S.op("act", lambda e, w=w: e.activation(w("E"), w("B"), AF.Exp), reads=[k("B")], writes=[k("E")])
                S.op("dve", lambda e, w=w: e.scalar_tensor_tensor(w("QE"), w("Q"), QSC, w("E"), ALU.mult, ALU.mult), reads=[k("Q"), k("E")], writes=[k("QE")])
                S.op("dve", lambda e, w3=w3, nchk=nchk, iref=iref: e.tensor_tensor(w3("D"), w3("B"), w3("B")[:, :, iref:iref + 1].to_broadcast([128, nchk, 64]), ALU.subtract),
                     reads=[k("B")], writes=[k("D")])
                S.op("act", lambda e, w=w: e.activation(w("E"), w("D"), AF.Exp), reads=[k("D"), k("QE")], writes=[k("E")])
                S.op("dve", lambda e, w=w: e.scalar_tensor_tensor(w("QS"), w("Q"), QSC, w("E"), ALU.mult, ALU.mult), reads=[k("Q"), k("E")], writes=[k("QS")])
                S.op("act", lambda e, w=w: e.activation(w("E"), w("D"), AF.Exp, scale=-1.0), reads=[k("D"), k("QS")], writes=[k("E")])
                S.op("dve", lambda e, w=w: e.tensor_tensor(w("LA"), w("K"), w("E"), ALU.mult), reads=[k("K"), k("E"), k("B")], writes=[k("LA")])
                for h in range(4):
                    S.op("pool", lambda e, w=w, h=h: e.tensor_scalar(w("KS%d" % h), w("LA"), HM[:, h:h + 1], None, ALU.mult),
                         reads=[k("LA"), "HM"], writes=[k("KS%d" % h)])
                S.op("dve", lambda e, w3=w3, nchk=nchk, ilast=ilast: e.tensor_tensor(w3("D"), w3("B")[:, :, ilast:ilast + 1].to_broadcast([128, nchk, 64]), w3("B"), ALU.subtract),
                     reads=[k("B"), k("E"), k("LA")], writes=[k("D")])
                S.op("act", lambda e, w=w: e.activation(w("D"), w("D"), AF.Exp), reads=[k("D")], writes=[k("D")])
                S.op("dve", lambda e, w=w: e.tensor_tensor(w("KD"), w("K"), w("D"), ALU.mult), reads=[k("K"), k("D")], writes=[k("KD")])
                for c in (range(nchk) if d == 0 else range(nchk - 1, -1, -1)):
                    p2 = gc % 2
                    gc += 1
                    cs = slice(c * 64, (c + 1) * 64)
                    S.op("pe", lambda e, b=b, cs=cs, p2=p2: e.transpose(pt[p2][:], Wb["KD", b][:, cs], ID[:]), reads=[k("KD"), "ID"], writes=["pt"])
                    S.op("act", lambda e, p2=p2: e.copy(KDT[p2][:], pt[p2][:]), reads=["pt"], writes=["KDT%d" % p2])
                    S.op("pe", lambda e, b=b, c=c, p2=p2: e.matmul(pkv[p2][:], KDT[p2][:], VV[b][:, c, :], start=True, stop=True),
                         reads=["KDT%d" % p2, "VV%d" % b], writes=["pkv%d" % p2])
                    for h in range(4):
                        S.op("pe", lambda e, b=b, cs=cs, p2=p2, h=h: e.matmul(psc[p2][:, h * 64:(h + 1) * 64], Wb["KS%d" % h, b][:, cs], Wb["QS", b][:, cs],
                                                                            start=True, stop=True),
                             reads=[k("KS%d" % h), k("QS")], writes=["psc%d" % p2])
                    S.op("dve", lambda e, p2=p2, d=d: e.tensor_tensor(STt[p2][:], psc[p2][:], (TM if d == 0 else TM2)[:], ALU.mult), reads=["psc%d" % p2, "TM", "TM2"], writes=["ST%d" % p2])
                    for h in range(4):
                        hs = slice(h * 64, (h + 1) * 64)
                        S.op("pe", lambda e, b=b, c=c, p2=p2, hs=hs: e.matmul(po[p2][:, hs], STt[p2][:, hs], VV[b][:, c, hs], start=True, stop=False),
                             reads=["ST%d" % p2, "VV%d" % b], writes=["po%d" % p2])
                        S.op("pe", lambda e, b=b, cs=cs, p2=p2, hs=hs, scur=scur: e.matmul(po[p2][:, hs], Wb["QE", b][:, cs], SS[scur][:, hs], start=False, stop=True),
                             reads=[k("QE"), "SS%d" % scur], writes=["po%d" % p2])
                    ob = oi % 3
                    oi += 1
                    S.op("act", lambda e, ob=ob, p2=p2: e.copy(OB[ob][:], po[p2][:]), reads=["po%d" % p2], writes=["OB%d" % ob])
                    if t0 < SEQC:
                        S.dma("sp" if oi % 2 else "act", lambda e, d=d, ob=ob, c=c, cols=cols: e.dma_start(out=OS[d][c * 64:(c + 1) * 64, cols], in_=OB[ob][:]),
                              reads=["OB%d" % ob], writes=["o_%d" % oi])
                    else:
                        S.dma("sp" if oi % 2 else "act", lambda e, d=d, ob=ob, c=c, w0=w0, cols=cols: e.dma_start(
                            out=OS[d][SEQC:, cols].rearrange("(r w) f -> r w f", w=64)[:, w0 + c, :], in_=OB[ob][:]),
                              reads=["OB%d" % ob], writes=["o_%d" % oi])
                    S.op("dve", lambda e, p2=p2: e.tensor_tensor(KVM[:], pkv[p2][:], BLK[:], ALU.mult), reads=["pkv%d" % p2, "BLK"], writes=["KVM"])
                    S.op("dve", lambda e, b=b, c=c, scur=scur: e.scalar_tensor_tensor(SS[1 - scur][:], SS[scur][:], DEC[b][:, c:c + 1], KVM[:], ALU.mult, ALU.add),
                         reads=["SS%d" % scur, "DEC%d" % b, "KVM"], writes=["SS%d" % (1 - scur)])
                    scur = 1 - scur
        S.sync_all()
        S.emit()
        sub.close()


def emit_D(nc, S, sb, ps, io, blocks, last):
    xT = io["xT"]; modT = io["MODS"]; TLs = io["TL"]
    su = TLs[:, 0:256]; sv = TLs[:, 256:512]; gg = TLs[:, 512:1024]
    s5u = io["FMU"]; yf = io["YF"]; yb = io["YB"]; of_ = io["OF"]; ob_ = io["OB"]
    w_out, wsT, sgub, s5d, wglu, ng, g2, wq = io["w_out"], io["wsT"], io["sgub"], io["s5d"], io["wglu"], io["ng"], io["g2"], io["wq"]
    keysT, eu, ev, gfin, ones, ident, iota16 = io["keysT"], io["eu"], io["ev"], io["gfin"], io["ones"], io["ident"], io["iota16"]
    xo = io["xo"]
    nb = len(blocks)
    MOD = sb("MOD", [128, 96]); WOUT = sb("WOUT", [128, 8, D]); WS = sb("WS", [128, 512]); SGUB = sb("SGUB", [128, 4]); S5D = sb("S5D", [128, 2])
    WGLU = sb("WGLU", [128, 2, 512]); NG = sb("NG", [128, 512]); G2 = sb("G2", [128, 8]); WQt = sb("WQt", [128, 8, 2048]); GB = sb("GB", [128, 4 * 1024]); KEYS = sb("KEYS", [128, 2048])
    GF = sb("GF", [128, 8]); ONES = sb("ONES", [128, 128]); ID = sb("ID", [128, 128]); IOTA = sb("IOTA", [128, 16]); IO128 = sb("IO128", [128, 128])
    A2 = sb("A2", [128, 16]); TA = sb("TA", [128, 16])
    U_ = sb("U_", [128, 256]); V_ = sb("V_", [128, 256]); GU = U_; GV = V_; SQ = sb("SQ", [128, 512])
    SS = sb("SS", [128, 8]); VN = sb("VN", [128, 256]); MIX = None
    S5U = sb("S5U", [128, 2, 128]); YF = sb("YF", [128, 2, 128]); YB = sb("YB", [128, 2, 128]); GE = S5U; SG = sb("SG", [128, 256])
    OF = sb("OF", [128, 512]); OB = sb("OB", [128, 512]); GG = sb("GG", [128, 512]); SL = GG
    XT = sb("XT", [128, 8, 128]); X1S_ = [sb("X1_%d" % i, [128, 8, 128]) for i in range(2)]; HT = sb("HT", [128, 8, 128]); MIXT = HT; HTOKS = [sb("HTOK%d" % i, [128, D]) for i in range(2)]
    RSTD = sb("RSTD", [128, 128]); TMPB = sb("TMPB", [128, 128]); RSTD2 = sb("RSTD2", [128, 128]); TMPB2 = sb("TMPB2", [128, 128])
    SC = sb("SC", [128, 2048]); MIX = SC[:, 0:1024]
    M16 = sb("M16", [128, 256]); I16 = sb("I16", [128, 256], U32); IF16 = sb("IF16", [128, 256]); I1S = sb("I1S", [128, 128])
    CS = sb("CS", [128, 2048]); SC2 = CS; QT = CS[:].rearrange("p (q t) -> p q t", t=128); CS2 = sb("CS2", [128, 256])
    T16 = sb("T16", [128, 128]); P16 = sb("P16", [128, 128], U32); PF = sb("PF", [128, 128]); AI = sb("AI", [128, 128], I32)
    AFL = sb("AFL", [128, 128]); BFL = sb("BFL", [128, 128]); E1 = sb("E1", [128, 128]); E2 = sb("E2", [128, 128])
    EG = sb("EG", [128, 128]); GATES = [sb("GATE%d" % i, [128, 128]) for i in range(2)]; IDXS = [sb("IDXTI%d" % i, [128, 128], I32) for i in range(2)]
    ACTT = sb("ACTT", [128, 128]); WT = sb("WT", [128, 128])
    NBUF = 4
    WQ = WQt
    UG = [GB[:, i * 1024:(i + 1) * 1024] for i in range(NBUF)]; VG = UG
    HB = [sb("HB%d" % i, [128, D]) for i in range(2)]
    P0 = ps("P0", [128, 2048]); P1 = ps("P1", [128, 1024]); P2 = ps("P2", [128, 512]); P3 = ps("P3", [128, 512])

    def V(fn, r=(), w=()):
        S.op("dve", fn, reads=r, writes=w)

    def A(fn, r=(), w=()):
        S.op("act", fn, reads=r, writes=w)

    def PE(fn, r=(), w=()):
        S.op("pe", fn, reads=r, writes=w)

    def LD(q, t, src, key):
        S.dma(q, lambda e: e.dma_start(out=t, in_=src), writes=(key if isinstance(key, list) else [key]))

    LD("sp", MOD[:], modT, "MOD"); LD("sp", WS[:], wsT, "WS"); LD("sp", SGUB[:], sgub, "SGUB"); LD("sp", S5D[:], s5d, "S5D")
    LD("sp", WGLU[:], wglu.rearrange("(c p) f -> p c f", p=128), "WGLU"); LD("sp", NG[:], ng, "NG"); LD("sp", G2[:], g2, "G2")
    LD("sp", KEYS[:], keysT, "KEYS"); LD("sp", GF[:], gfin, "GF"); LD("sp", ONES[:], ones, "ONES"); LD("sp", ID[:], ident, "ID"); LD("sp", IOTA[:], iota16, "IOTA"); LD("sp", IO128[:], io["iota128"], "IO128")
    wo_v = w_out.rearrange("(k p) f -> p k f", p=128)
    wq_v = wq.rearrange("(k p) f -> p k f", p=128)
    for k in range(8):
        LD("act", WOUT[:, k, :], wo_v[:, k, :], "WOUT")
        LD("sp", WQ[:, k, :], wq_v[:, k, :], "WQ")
    MOD3 = MOD[:].rearrange("p (j c) -> p j c", c=2)
    V(lambda e: e.tensor_scalar(TA[:].rearrange("p (j c) -> p j c", c=2), MOD3[:, 32:40, :], 1.0, None, ALU.add), ["MOD"], ["TA"])
    V(lambda e: e.tensor_tensor(A2[:].rearrange("p (j c) -> p j c", c=2), TA[:].rearrange("p (j c) -> p j c", c=2),
                                G2[:].unsqueeze(2).to_broadcast([128, 8, 2]), ALU.mult), ["TA", "G2"], ["A2"])
    A23 = A2[:].rearrange("p (j c) -> p j c", c=2)

    def rs_from_ss(ss, n, scale):
        V(lambda e: e.tensor_scalar(ss, ss, scale, EPS, ALU.mult, ALU.add), ["SS"], ["SS"])
        A(lambda e: e.sqrt(ss, ss), ["SS"], ["SS"])
        V(lambda e: e.reciprocal(ss, ss), ["SS"], ["SS"])

    def top16(src, scratch, mout, iout, n):
        V(lambda e: e.max(mout[:, 0:8], src), ["TK"], ["TK"])
        V(lambda e: e.max_index(iout[:, 0:8], mout[:, 0:8], src), ["TK"], ["TK"])
        V(lambda e: e.match_replace(scratch, mout[:, 0:8], src, -1e30), ["TK"], ["TK"])
        V(lambda e: e.max(mout[:, 8:16], scratch), ["TK"], ["TK"])
        V(lambda e: e.max_index(iout[:, 8:16], mout[:, 8:16], scratch), ["TK"], ["TK"])

    xT_v = xT.rearrange("(k p) t -> p k t", p=128)
    xo_v = xo.rearrange("(k p) t -> p k t", p=128)
    s5u_v = s5u.rearrange("(c p) t -> p c t", p=128)
    yf_v = yf.rearrange("(c p) t -> p c t", p=128)
    yb_v = yb.rearrange("(c p) t -> p c t", p=128)
    def front(bi):
        pb_ = bi % 2
        HTOK = HTOKS[pb_]; X1 = X1S_[pb_]; IDXTI = IDXS[pb_]; GATE = GATES[pb_]
        KH = "HTOK%d" % pb_; KX = "X1_%d" % pb_; KI = "IDXTI%d" % pb_; KG = "GATE%d" % pb_
        sq0, r0_, oc0, isctx = blocks[bi]
        col = 1 if isctx else 0
        tk = slice(sq0, sq0 + 128)
        tr = slice(r0_, r0_ + 128)
        to = slice(oc0, oc0 + 128)
        XO = HB[1][:].rearrange("p (k t) -> p k t", t=128)
        yield
        LD("sp", U_[:], su[tr, :], "U_"); LD("sp", V_[:], sv[tr, :], "V_"); LD("sp", GG[:], gg[tr, :], "GG")
        yield
        LD("act", S5U[:], s5u_v[:, :, tk], "S5U"); LD("act", YF[:], yf_v[:, :, tk], "YF"); LD("act", YB[:], yb_v[:, :, tk], "YB")
        yield
        LD("sp", OF[:], of_[tk, :], "OF"); LD("sp", OB[:], ob_[tk, :], "OB"); LD("act", XT[:], xT_v[:, :, tk], "XT")
        yield
        A(lambda e: e.activation(GU[:], U_[:], AF.Gelu), ["U_"], ["U_"])
        yield
        A(lambda e: e.activation(GV[:], V_[:], AF.Gelu), ["V_"], ["V_"])
        yield
        V(lambda e: e.tensor_tensor(SQ[:, 0:256], GV[:], GV[:], ALU.mult), ["V_"], ["SQ"])
        yield
        V(lambda e: e.tensor_reduce(SS[:, 0:4], SQ[:, 0:256].rearrange("p (h d) -> p h d", d=64), AX.X, ALU.add), ["SQ"], ["SS"])
        yield
        rs_from_ss(SS[:, 0:4], 4, 1.0 / 64)
        yield
        V(lambda e: e.tensor_tensor(VN[:].rearrange("p (h d) -> p h d", d=64), GV[:].rearrange("p (h d) -> p h d", d=64),
                                    SS[:, 0:4].unsqueeze(2).to_broadcast([128, 4, 64]), ALU.mult), ["V_", "SS"], ["VN"])
        yield
        for h in range(4):
            PE(lambda e, h=h: e.matmul(P2[:, h * 64:(h + 1) * 64], WS[:, h * 128:(h + 1) * 128], VN[:, h * 64:(h + 1) * 64], start=True, stop=True),
               ["WS", "VN"], ["P2"])
            yield
        yield
        V(lambda e: e.tensor_tensor(MIX[:, 0:256].rearrange("p (h d) -> p h d", d=64), P2[:, 0:256].rearrange("p (h d) -> p h d", d=64),
                                    SGUB[:].unsqueeze(2).to_broadcast([128, 4, 64]), ALU.add), ["P2", "SGUB"], ["MIXa"])
        yield
        V(lambda e: e.tensor_tensor(MIX[:, 0:256], MIX[:, 0:256], GU[:], ALU.mult), ["U_"], ["MIXa"])
        yield
        V(lambda e: e.tensor_tensor(YF[:], YF[:], YB[:], ALU.add), ["YB"], ["YF"])
        yield
        for ct in range(2):
            V(lambda e, ct=ct: e.scalar_tensor_tensor(YF[:, ct, :], S5U[:, ct, :], S5D[:, ct:ct + 1], YF[:, ct, :], ALU.mult, ALU.add),
              ["S5U", "S5D"], ["YF"])
            yield
        yield
        A(lambda e: e.activation(GE[:], YF[:], AF.Gelu), ["YF"], ["S5U"])
        yield
        for ct in range(2):
            PE(lambda e, ct=ct: e.matmul(P3[:, 0:512], GE[:, ct, :], WGLU[:, ct, :], start=(ct == 0), stop=(ct == 1)), ["S5U", "WGLU"], ["P3"])
            yield
        yield
        A(lambda e: e.activation(SG[:], P3[:, 256:512], AF.Sigmoid), ["P3"], ["SG"])
        yield
        V(lambda e: e.tensor_tensor(MIX[:, 256:512], P3[:, 0:256], SG[:], ALU.mult), ["P3", "SG"], ["MIXb"])
        yield
        V(lambda e: e.tensor_tensor(OF[:], OF[:], OB[:], ALU.add), ["OB"], ["OF"])
        yield
        V(lambda e: e.tensor_tensor(SQ[:], OF[:], OF[:], ALU.mult), ["OF"], ["SQ"])
        yield
        V(lambda e: e.tensor_reduce(SS[:, 0:8], SQ[:].rearrange("p (h d) -> p h d", d=64), AX.X, ALU.add), ["SQ"], ["SS"])
        yield
        rs_from_ss(SS[:, 0:8], 8, 1.0 / 64)
        yield
        V(lambda e: e.tensor_tensor(OF[:].rearrange("p (h d) -> p h d", d=64), OF[:].rearrange("p (h d) -> p h d", d=64),
                                    SS[:, 0:8].unsqueeze(2).to_broadcast([128, 8, 64]), ALU.mult), ["SS"], ["OF"])
        yield
        V(lambda e: e.tensor_tensor(OF[:], OF[:], NG[:], ALU.mult), ["NG"], ["OF"])
        yield
        A(lambda e: e.activation(SL[:], GG[:], AF.Silu), ["GG"], ["GG"])
        yield
        V(lambda e: e.tensor_tensor(MIX[:, 512:1024], OF[:], SL[:], ALU.mult), ["OF", "GG"], ["MIXc"])
        yield
        for f in range(8):
            pp, pk = (P2, "P2") if f % 2 == 0 else (P3, "P3")
            PE(lambda e, f=f, pp=pp: e.transpose(pp[:, 0:128], MIX[:, f * 128:(f + 1) * 128], ID[:]), ["MIXa", "MIXb", "MIXc", "ID"], [pk])
            A(lambda e, f=f, pp=pp: e.copy(MIXT[:, f, :], pp[:, 0:128]), [pk], ["HT"])
            yield
        yield
        for ot in range(8):
            pp, pk = (P2, "P2") if ot % 2 == 0 else (P3, "P3")
            for k in range(8):
                PE(lambda e, ot=ot, k=k, pp=pp: e.matmul(pp[:, 0:128], WOUT[:, k, ot * 128:(ot + 1) * 128], MIXT[:, k, :], start=(k == 0), stop=(k == 7)),
                   ["WOUT", "HT"], [pk])
            V(lambda e, ot=ot, pp=pp, col=col: e.scalar_tensor_tensor(X1[:, ot, :], pp[:, 0:128], MOD3[:, 16 + ot, col:col + 1], XT[:, ot, :], ALU.mult, ALU.add),
              [pk, "MOD", "XT"], [KX])
            yield
        yield
        for k in range(8):
            A(lambda e, k=k: e.activation(TMPB[:], X1[:, k, :], AF.Square), [KX], ["TMPB"])
            PE(lambda e, k=k: e.matmul(P2[:, 0:128], ONES[:], TMPB[:], start=(k == 0), stop=(k == 7)), ["ONES", "TMPB"], ["P2"])
            yield
        yield
        V(lambda e: e.tensor_scalar(RSTD[:], P2[:, 0:128], 1.0 / D, EPS, ALU.mult, ALU.add), ["P2"], ["RSTD"])
        yield
        A(lambda e: e.sqrt(RSTD[:], RSTD[:]), ["RSTD"], ["RSTD"])
        yield
        V(lambda e: e.reciprocal(RSTD[:], RSTD[:]), ["RSTD"], ["RSTD"])
        yield
        for k in range(8):
            V(lambda e, k=k: e.tensor_tensor(TMPB[:], X1[:, k, :], RSTD[:], ALU.mult), [KX, "RSTD"], ["TMPB"])
            A(lambda e, k=k, col=col: e.activation(HT[:, k, :], TMPB[:], AF.Identity, bias=MOD3[:, 24 + k, col:col + 1], scale=A23[:, k, col:col + 1]),
              ["TMPB", "MOD", "A2"], ["HT"])
            yield
        yield
        for k in range(8):
            pp, pk = (P2, "P2") if k % 2 == 0 else (P3, "P3")
            PE(lambda e, k=k, pp=pp: e.transpose(pp[:, 0:128], HT[:, k, :], ID[:]), ["HT", "ID"], [pk])
            A(lambda e, k=k, pp=pp: e.copy(HTOK[:, k * 128:(k + 1) * 128], pp[:, 0:128]), [pk], [KH])
            yield
        yield
        for qt in range(16):
            pp, pk = (P2, "P2") if qt % 2 == 0 else (P3, "P3")
            for k in range(8):
                PE(lambda e, qt=qt, k=k, pp=pp: e.matmul(pp[:, 0:128], WQ[:, k, qt * 128:(qt + 1) * 128], HT[:, k, :], start=(k == 0), stop=(k == 7)),
                   ["WQ", "HT"], [pk])
            if qt % 2 == 0:
                A(lambda e, qt=qt, pp=pp: e.copy(QT[:, qt, :], pp[:, 0:128]), [pk], ["TK"])
            else:
                V(lambda e, qt=qt, pp=pp: e.tensor_copy(QT[:, qt, :], pp[:, 0:128]), [pk], ["TK"])
            yield
        yield
        for qt in range(16):
            PE(lambda e, qt=qt: e.matmul(P0[:, qt * 128:(qt + 1) * 128], QT[:, qt, :], KEYS[:, qt * 128:(qt + 1) * 128], start=True, stop=True),
               ["TK", "KEYS"], ["P0a", "P0b"])
            yield
        yield
        for q4 in range(4):
            A(lambda e, q4=q4: e.copy(SC[:, q4 * 512:(q4 + 1) * 512], P0[:, q4 * 512:(q4 + 1) * 512]), ["P0a", "P0b"], ["TK"])
            yield
        yield
        for qt in range(16):
            top16(SC[:, qt * 128:(qt + 1) * 128], SC2[:, qt * 128:(qt + 1) * 128], M16[:, qt * 16:(qt + 1) * 16], I16[:, qt * 16:(qt + 1) * 16], 128)
            yield
        yield
        V(lambda e: e.tensor_copy(IF16[:], I16[:]), ["TK"], ["TK"])
        yield
        M4 = M16[:].rearrange("p (h q k) -> p h q k", q=2, k=16)
        yield
        IF4 = IF16[:].rearrange("p (h q k) -> p h q k", q=2, k=16)
        yield
        I1S3 = I1S[:].rearrange("p (h k) -> p h k", k=16)
        yield
        V(lambda e: e.tensor_scalar(I1S3, IF4[:, :, 0, :], 128.0, None, ALU.mult), ["TK"], ["TK"])
        yield
        CS4 = CS[:].rearrange("p (h a b) -> p h a b", a=16, b=16)
        yield
        V(lambda e: e.tensor_tensor(CS4, M4[:, :, 0, :].unsqueeze(3).to_broadcast([128, 8, 16, 16]),
                                    M4[:, :, 1, :].unsqueeze(2).to_broadcast([128, 8, 16, 16]), ALU.add), ["TK"], ["TK"])
        yield
        for h in range(8):
            top16(CS[:, h * 256:(h + 1) * 256], CS2[:], T16[:, h * 16:(h + 1) * 16], P16[:, h * 16:(h + 1) * 16], 256)
            yield
        yield
        V(lambda e: e.tensor_copy(PF[:], P16[:]), ["TK"], ["TK"])
        yield
        V(lambda e: e.tensor_scalar(AFL[:], PF[:], -7.5, 1.0 / 16, ALU.add, ALU.mult), ["TK"], ["TK"])
        yield
        V(lambda e: e.tensor_copy(AI[:], AFL[:]), ["TK"], ["TK"])
        yield
        V(lambda e: e.tensor_copy(AFL[:], AI[:]), ["TK"], ["TK"])
        yield
        V(lambda e: e.scalar_tensor_tensor(BFL[:], AFL[:], -16.0, PF[:], ALU.mult, ALU.add), ["TK"], ["TK"])
        yield
        EQ4 = CS[:].rearrange("p (h k a) -> p h k a", k=16, a=16)
        yield
        io4 = IOTA[:].unsqueeze(1).unsqueeze(1).to_broadcast([128, 8, 16, 16])
        yield
        for (sel, src, dst) in ((AFL, I1S3, E1), (BFL, IF4[:, :, 1, :], E2)):
            V(lambda e, sel=sel: e.tensor_tensor(EQ4, io4, sel[:].rearrange("p (h k) -> p h k", k=16).unsqueeze(3).to_broadcast([128, 8, 16, 16]), ALU.is_equal),
              ["TK", "IOTA"], ["TK"])
            V(lambda e, src=src: e.tensor_tensor(EQ4, EQ4, src.unsqueeze(2).to_broadcast([128, 8, 16, 16]), ALU.mult), ["TK"], ["TK"])
            V(lambda e, dst=dst: e.tensor_reduce(dst[:], CS[:].rearrange("p (m a) -> p m a", a=16), AX.X, ALU.add), ["TK"], ["TK"])
            yield
        yield
        V(lambda e: e.tensor_tensor(E1[:], E1[:], E2[:], ALU.add), ["TK"], ["TK"])
        yield
        T3 = T16[:].rearrange("p (h k) -> p h k", k=16)
        yield
        V(lambda e: e.tensor_tensor(EG[:].rearrange("p (h k) -> p h k", k=16), T3, T3[:, :, 0:1].to_broadcast([128, 8, 16]), ALU.subtract), ["TK"], ["TK"])
        yield
        A(lambda e: e.activation(EG[:], EG[:], AF.Exp), ["TK"], ["TK"])
        yield
        V(lambda e: e.tensor_reduce(SS[:, 0:8], EG[:].rearrange("p (h k) -> p h k", k=16), AX.X, ALU.add), ["TK"], ["SS"])
        yield
        V(lambda e: e.reciprocal(SS[:, 0:8], SS[:, 0:8]), ["SS"], ["SS"])
        yield
        V(lambda e: e.tensor_tensor(GATE[:].rearrange("p (h k) -> p h k", k=16), EG[:].rearrange("p (h k) -> p h k", k=16),
                                    SS[:, 0:8].unsqueeze(2).to_broadcast([128, 8, 16]), ALU.mult), ["TK", "SS"], [KG])
        yield
        V(lambda e: e.tensor_copy(IDXTI[:], E1[:]), ["TK"], [KI])


    def loops(bi):
        pb_ = bi % 2
        HTOK = HTOKS[pb_]; X1 = X1S_[pb_]; IDXTI = IDXS[pb_]; GATE = GATES[pb_]
        KH = "HTOK%d" % pb_; KX = "X1_%d" % pb_; KI = "IDXTI%d" % pb_; KG = "GATE%d" % pb_
        sq0, r0_, oc0, isctx = blocks[bi]
        col = 1 if isctx else 0
        tk = slice(sq0, sq0 + 128)
        tr = slice(r0_, r0_ + 128)
        to = slice(oc0, oc0 + 128)
        XO = HB[1][:].rearrange("p (k t) -> p k t", t=128)
        yield
        for j in range(128):
            g = j % NBUF
            S.dma("pool", lambda e, j=j, g=g: e.indirect_dma_start(out=UG[g], out_offset=None, in_=eu,
                                                                   in_offset=bass.IndirectOffsetOnAxis(ap=IDXTI[:, j:j + 1], axis=0)),
                  reads=[KI], writes=["R%d" % g])
            V(lambda e, j=j, g=g: e.scalar_tensor_tensor(UG[g], UG[g], 1.0, HTOK[:], ALU.mult, ALU.mult, accum_out=ACTT[:, j:j + 1]),
              [KH], ["R%d" % g, "ACTT"])
            yield
        yield
        A(lambda e: e.activation(WT[:], ACTT[:], AF.Gelu), ["ACTT"], ["WT"])
        yield
        V(lambda e: e.tensor_tensor(WT[:], WT[:], GATE[:], ALU.mult), [KG], ["WT"])
        yield
        ACC = HB[0]
        yield
        for j in range(128):
            g = j % NBUF
            S.dma("pool", lambda e, j=j, g=g: e.indirect_dma_start(out=VG[g], out_offset=None, in_=ev,
                                                                   in_offset=bass.IndirectOffsetOnAxis(ap=IDXTI[:, j:j + 1], axis=0)),
                  reads=[KI], writes=["R%d" % g])
            if j == 0:
                V(lambda e, g=g: e.tensor_scalar(ACC[:], VG[g], WT[:, 0:1], None, ALU.mult), ["R%d" % g, "WT"], ["HB0"])
            else:
                V(lambda e, j=j, g=g: e.scalar_tensor_tensor(ACC[:], VG[g], WT[:, j:j + 1], ACC[:], ALU.mult, ALU.add), ["R%d" % g, "WT"], ["HB0"])
            yield
        yield
        for ot in range(8):
            pp, pk = (P1[:, 0:512], "P1a") if ot % 2 == 0 else (P1[:, 512:1024], "P1b")
            PE(lambda e, ot=ot, pp=pp: e.transpose(pp[:, 0:128], ACC[:, ot * 128:(ot + 1) * 128], ID[:]), ["HB0", "ID"], [pk])
            V(lambda e, ot=ot, col=col, pp=pp: e.scalar_tensor_tensor(XO[:, ot, :], pp[:, 0:128], MOD3[:, 40 + ot, col:col + 1], X1[:, ot, :],
                                                                      ALU.mult, ALU.add), [pk, "MOD", KX], ["HB1"])
            yield
        yield
        if last:
            for k in range(8):
                A(lambda e, k=k: e.activation(TMPB2[:], XO[:, k, :], AF.Square), ["HB1"], ["TMPB2"])
                PE(lambda e, k=k: e.matmul(P1[:, 0:128], ONES[:], TMPB2[:], start=(k == 0), stop=(k == 7)), ["ONES", "TMPB2"], ["P1a"])
            V(lambda e: e.tensor_scalar(RSTD2[:], P1[:, 0:128], 1.0 / D, EPS, ALU.mult, ALU.add), ["P1a"], ["RSTD2"])
            A(lambda e: e.sqrt(RSTD2[:], RSTD2[:]), ["RSTD2"], ["RSTD2"])
            V(lambda e: e.reciprocal(RSTD2[:], RSTD2[:]), ["RSTD2"], ["RSTD2"])
            for k in range(8):
                V(lambda e, k=k: e.scalar_tensor_tensor(XO[:, k, :], XO[:, k, :], GF[:, k:k + 1], RSTD2[:], ALU.mult, ALU.mult), ["RSTD2", "GF"], ["HB1"])
        yield
        S.dma("sp", lambda e, to=to: e.dma_start(out=xo_v[:, :, to], in_=XO[:]), reads=["HB1"], writes=["xo_%d" % bi])


    def run_all(g):
        for _ in g:
            pass
    run_all(front(0))
    for bi in range(nb):
        nxt = front(bi + 1) if bi + 1 < nb else None
        for step, _ in enumerate(loops(bi)):
            if nxt is not None and step % 2 == 1:
                if next(nxt, "END") == "END":
                    nxt = None
        if nxt is not None:
            run_all(nxt)


def build_layer(last, dbg=False):
    nc = bass.Bass("TRN2", target_bir_lowering=False)
    io = {}

    def din(name, shape, dt=F32):
        io[name] = nc.dram_tensor(name, shape, dt, kind="ExternalInput").ap()

    def scr(name, shape):
        io[name] = nc.dram_tensor(name, shape, F32, kind=("ExternalOutput" if dbg else "Internal")).ap()
    nctx = 0 if last else 128
    ntok = 2048 + nctx
    din("xT", [D, SEQT]); din("cT", [128, 16]); din("w_mod", [D, NMOD * D]); din("b_mod", [128, 48]); din("g1", [128, 8]); din("w_in", [D, INW])
    din("ones", [128, 128]); din("ident", [128, 128]); din("tau", [128, 512]); din("iota16", [128, 16]); din("iota128", [128, 128])
    din("prm", [2, 128, 24]); din("bre", [2, 128, 128]); din("bim", [2, 128, 128]); din("cre", [2, 128, 128]); din("cim", [2, 128, 128])
    din("wg", [2, 2, 16, 128]); din("bg", [2, 2, 128, 1])
    din("rst", [128, 512]); din("tmask", [64, 256]); din("tmask2", [64, 256]); din("blk", [128, 256]); din("hmask", [128, 4])
    din("w_out", [D, D]); din("wsT", [128, 512]); din("sgub", [128, 4]); din("s5d", [128, 2]); din("wglu", [256, 512]); din("ng", [128, 512])
    din("g2", [128, 8]); din("wq", [D, 2048]); din("keysT", [128, 2048]); din("eu", [NEXP, D]); din("ev", [NEXP, D]); din("gfin", [128, 8])
    scr("FMU", [256, SEQT]); scr("FMQ", [256, SEQT]); scr("FMK", [256, SEQT]); scr("FMZ", [32, SEQT]); scr("VT", [SEQT, 512]); scr("TL", [OWN, 1024])
    scr("MODS", [128, 96]); scr("YF", [256, SEQT]); scr("YB", [256, SEQT]); scr("OF", [SEQT, 512]); scr("OB", [SEQT, 512]); scr("GS", [2, 16])
    io["xo"] = nc.dram_tensor("xo", [D, ntok], F32, kind="ExternalOutput").ap()
    with ExitStack() as top:
        gate = top.enter_context(nc.semaphore("gate"))
        ph = [0]

        def phase(fn):
            with ExitStack() as st:
                S = Sched(nc, top, gate, 16 * ph[0])
                sb, ps = _mk(nc, st)
                fn(S, sb, ps)
                S.drain_all("sp")
                GS = io["GS"]
                S.prog["sp"].append(("i", lambda e: e.dma_start(out=GS[0:1, :], in_=GS[1:2, :]), "gate", 16))
                S.emit()
            ph[0] += 1
        phase(lambda S, sb, ps: emit_A(nc, S, sb, ps, io))
        phase(lambda S, sb, ps: emit_B(nc, S, sb, ps, io))
        phase(lambda S, sb, ps: emit_C(nc, S, sb, ps, io))
        if last:
            blocks = [(SEQC + i * 128, 128 + i * 128, i * 128, False) for i in range(16)]
        else:
            blocks = [(0, 0, 0, True)] + [(SEQC + i * 128, 128 + i * 128, 128 + i * 128, False) for i in range(16)]
        phase(lambda S, sb, ps: emit_D(nc, S, sb, ps, io, blocks, last))
    return nc


def _lay_vec(v, n):
    return np.ascontiguousarray(np.asarray(v, np.float32).reshape(n, 128).T)


_CONST = {}


def _consts():
    if not _CONST:
        rst = np.ones((128, 512), np.float32)
        rst[:, ::64] = 0
        tm = (np.arange(64)[None, :] >= np.arange(64)[:, None]).astype(np.float32)
        _CONST.update(
            ones=np.ones((128, 128), np.float32), ident=np.eye(128, dtype=np.float32),
            iota16=np.ascontiguousarray(np.broadcast_to(np.arange(16, dtype=np.float32), (128, 16))),
            iota128=np.ascontiguousarray(np.broadcast_to(np.arange(128, dtype=np.float32), (128, 128))),
            tau=np.ascontiguousarray(np.broadcast_to(np.arange(1, 513, dtype=np.float32), (128, 512))),
            rst=rst, tmask=np.ascontiguousarray(np.tile(tm, (1, 4))), tmask2=np.ascontiguousarray(np.tile(tm.T, (1, 4))),
            blk=np.kron(np.eye(4, dtype=np.float32), np.ones((32, 64), np.float32)),
            hmask=np.kron(np.eye(4, dtype=np.float32), np.ones((32, 1), np.float32)))
    return _CONST


def _pb_params(P, l, gh, swap):
    def stt(a):
        if swap:
            a = a[::-1]
        g = a[:, gh * 8:gh * 8 + 8]
        g = g.reshape((2, 4, 2, 64) + a.shape[3:])
        g = np.moveaxis(g, (2, 3), (0, 1))
        return np.ascontiguousarray(g.reshape((128, 2, 4) + a.shape[3:]))
    lre = stt(P["s5_lambda_re"][l]); lim = stt(P["s5_lambda_im"][l])
    ls = stt(np.broadcast_to(P["s5_log_step"][l][:, :, None], (2, 16, 64)))
    prm = np.ascontiguousarray(np.stack([lre, lim, ls], -1).reshape(128, 24).astype(np.float32))
    return dict(prm=prm, bre=stt(P["s5_b_re"][l]).reshape(128, 128), bim=stt(P["s5_b_im"][l]).reshape(128, 128),
                cre=stt(np.swapaxes(P["s5_c_re"][l], 2, 3)).reshape(128, 128), cim=stt(np.swapaxes(P["s5_c_im"][l], 2, 3)).reshape(128, 128))


def _layer_weights(P, l, swap):
    C = _consts()
    sk = P["peer_sub_keys"][l]
    keysT = np.zeros((128, 16, 128), np.float32)
    for h in range(8):
        for p in range(2):
            keysT[:, h * 2 + p, :] = sk[p, h].T
    sw = P["sgu_w"][l]
    sbb = P["sgu_b"][l]
    wgate = P["gla_w_gate"][l]
    bgate = P["gla_b_gate"][l]
    if swap:
        sw = sw[:, ::-1, ::-1]
        sbb = sbb[:, ::-1]
        wgate = wgate[::-1]
        bgate = bgate[::-1]
    pb = [_pb_params(P, l, ct, swap) for ct in range(2)]
    w_in = P["w_in"][l]
    if swap:
        w_in = np.ascontiguousarray(np.concatenate([w_in[:, :2304], w_in[:, 2320:2336], w_in[:, 2304:2320]], 1))
    W = dict(w_mod=P["w_mod"][l], b_mod=_lay_vec(P["b_mod"][l], 48), g1=_lay_vec(P["norm1_g"][l], 8), w_in=w_in,
             w_out=P["w_out"][l], wsT=np.ascontiguousarray(np.transpose(sw, (2, 0, 1)).reshape(128, 512)),
             sgub=np.ascontiguousarray(sbb.T), s5d=_lay_vec(P["s5_d"][l], 2), wglu=P["s5_w_glu"][l],
             ng=np.ascontiguousarray(np.broadcast_to(P["gla_norm_g"][l], (128, 512))), g2=_lay_vec(P["norm2_g"][l], 8),
             wq=P["peer_w_query"][l], keysT=keysT.reshape(128, 2048), eu=P["peer_expert_u"][l], ev=P["peer_expert_v"][l],
             gfin=_lay_vec(P["final_norm_g"], 8),
             wg=np.ascontiguousarray(np.stack([[wgate[d][:, hh * 128:hh * 128 + 128] for d in range(2)] for hh in range(2)])),
             bg=np.ascontiguousarray(np.stack([[bgate[d][hh * 128:hh * 128 + 128][:, None] for d in range(2)] for hh in range(2)])))
    for k in ("prm", "bre", "bim", "cre", "cim"):
        W[k] = np.ascontiguousarray(np.stack([pb[0][k], pb[1][k]]))
    W.update(C)
    return W


LAYER_W = ["w_mod", "b_mod", "g1", "w_in", "prm", "bre", "bim", "cre", "cim", "wg", "bg", "w_out", "wsT", "sgub", "s5d", "wglu", "ng", "g2",
           "wq", "keysT", "eu", "ev"]
LAYER_W_SHAPES = dict(w_mod=[D, NMOD * D], b_mod=[128, 48], g1=[128, 8], w_in=[D, INW], prm=[2, 128, 24], bre=[2, 128, 128], bim=[2, 128, 128],
                      cre=[2, 128, 128], cim=[2, 128, 128], wg=[2, 2, 16, 128], bg=[2, 2, 128, 1], w_out=[D, D], wsT=[128, 512], sgub=[128, 4],
                      s5d=[128, 2], wglu=[256, 512], ng=[128, 512], g2=[128, 8], wq=[D, 2048], keysT=[128, 2048], eu=[NEXP, D], ev=[NEXP, D])


def build_full():
    nc = bass.Bass("TRN2", target_bir_lowering=False)
    io = {}

    def din(name, shape, dt=F32):
        io[name] = nc.dram_tensor(name, shape, dt, kind="ExternalInput").ap()

    def scr(name, shape):
        io[name] = nc.dram_tensor(name, shape, F32, kind="Internal").ap()
    din("xT", [D, SEQT]); din("cT", [128, 16]); din("gfin", [128, 8])
    din("ones", [128, 128]); din("ident", [128, 128]); din("tau", [128, 512]); din("iota16", [128, 16]); din("iota128", [128, 128])
    din("rst", [128, 512]); din("tmask", [64, 256]); din("tmask2", [64, 256]); din("blk", [128, 256]); din("hmask", [128, 4])
    for l in range(2):
        for n in LAYER_W:
            din("%s_%d" % (n, l), LAYER_W_SHAPES[n])
    scr("FMU", [256, SEQT]); scr("FMQ", [256, SEQT]); scr("FMK", [256, SEQT]); scr("FMZ", [32, SEQT]); scr("VT", [SEQT, 512]); scr("TL", [SEQT, 1024])
    scr("MODS", [128, 96]); scr("YF", [256, SEQT]); scr("YB", [256, SEQT]); scr("OF", [SEQT, 512]); scr("OB", [SEQT, 512]); scr("GS", [2, 16])
    scr("X1S", [D, SEQT])
    io["xo"] = nc.dram_tensor("xo", [D, 2048], F32, kind="ExternalOutput").ap()
    with ExitStack() as top:
        gate = top.enter_context(nc.semaphore("gate"))
        ph = [0]

        def phase(fn, nds=None):
            with ExitStack() as st:
                S = Sched(nc, top, gate, 16 * ph[0], nds=nds)
                sb, ps = _mk(nc, st)
                fn(S, sb, ps)
                S.drain_all("sp")
                GS = io["GS"]
                S.prog["sp"].append(("i", lambda e: e.dma_start(out=GS[0:1, :], in_=GS[1:2, :]), "gate", 16))
                S.emit()
            ph[0] += 1
        for l in range(2):
            last = l == 1
            iol = dict(io)
            for n in LAYER_W:
                iol[n] = io["%s_%d" % (n, l)]
            if last:
                iol["xT"] = io["X1S"]
                blocks = [(SEQC + i * 128, 128 + i * 128, i * 128, False) for i in range(16)]
            else:
                iol["xo"] = io["X1S"]
                blocks = [(0, 0, 0, True), (128, 128, 128, True)] + [(SEQC + i * 128, SEQC + i * 128, SEQC + i * 128, False) for i in range(32)]
            n2 = dict(sp=2, act=2, pool=2)
            phase(lambda S, sb, ps: emit_A(nc, S, sb, ps, iol, tl_all=not last), nds=n2)
            phase(lambda S, sb, ps: emit_B(nc, S, sb, ps, iol), nds=n2)
            phase(lambda S, sb, ps: emit_C(nc, S, sb, ps, iol), nds=n2)
            phase(lambda S, sb, ps: emit_D(nc, S, sb, ps, iol, blocks, last), nds=dict(sp=2, act=2, pool=7))
    return nc


_PROG = {}


def _prog(name, fn):
    if name not in _PROG:
        _PROG[name] = fn()
    return _PROG[name]


def kernel(**inputs):
    P = {k: np.ascontiguousarray(np.asarray(v)) for k, v in inputs.items()}
    cores = list(range(8))
    C = _consts()
    ncF = _prog("F", build_full)
    WL = {(l, sw): _layer_weights(P, l, sw) for l in range(2) for sw in (False, True)}
    in_maps = []
    for core in cores:
        b, half = divmod(core, 2)
        xc, xl = P["ctx"][b], P["x"][b]
        seq = np.concatenate([xc, xl], 0) if half == 0 else np.concatenate([xc[::-1], xl[::-1]], 0)
        cT = np.stack([_lay_vec(P["c"][b], 8), _lay_vec(P["c_ctx"], 8)], -1).reshape(128, 16)
        m = dict(xT=np.ascontiguousarray(seq.T), cT=np.ascontiguousarray(cT), gfin=_lay_vec(P["final_norm_g"], 8))
        m.update(C)
        for l in range(2):
            W = WL[l, half == 1]
            for n in LAYER_W:
                m["%s_%d" % (n, l)] = W[n]
        in_maps.append(m)
    rF = run_bass_kernel_spmd(ncF, in_maps, core_ids=cores).results
    out = np.zeros_like(P["x"])
    for core in cores:
        b, half = divmod(core, 2)
        xo = rF[core]["xo"].T
        if half == 0:
            out[b, 0:2048] = xo
        else:
            out[b, 2048:4096] = xo[::-1]
    return out.astype(np.float32)
```

```python
from contextlib import ExitStack
import math
import numpy as np
import concourse.bass as bass
import concourse.mybir as mybir
from concourse.bass_utils import run_bass_kernel_spmd

F32 = mybir.dt.float32
I32 = mybir.dt.int32
U32 = mybir.dt.uint32
ALU = mybir.AluOpType
AF = mybir.ActivationFunctionType
AX = mybir.AxisListType


class Sched:
    NDS = 4

    def __init__(self, nc, stack, gate=None, gate_val=0, nds=None):
        self.nc = nc
        self.ndsq = dict(sp=self.NDS, act=self.NDS, pool=self.NDS)
        if nds:
            self.ndsq.update(nds)
        self.gate = gate
        self.gate_val = gate_val
        self.eng = {"pe": nc.tensor, "dve": nc.vector, "act": nc.scalar,
                    "pool": nc.gpsimd, "sp": nc.sync}
        self.sem = {}
        self.cnt = {}
        _UID[0] += 1
        u = "q%d" % _UID[0]
        for e in self.eng:
            self.sem[e] = stack.enter_context(nc.semaphore(u + "s_" + e))
            self.cnt[e] = 0
        self.dq = {}
        for q in ("sp", "act", "pool"):
            sems = [stack.enter_context(nc.semaphore(u + "d_%s%d" % (q, i))) for i in range(self.ndsq[q])]
            self.dq[q] = {"sems": sems, "n": 0}
            for i, s in enumerate(sems):
                self.sem["d_%s%d" % (q, i)] = s
        self.seen = {e: {} for e in self.eng}
        self.prog = {e: [] for e in self.eng}
        self.lastw = {}
        self.reads = {}
        self.ninst = 0
        if gate is not None:
            self.sem["gate"] = gate
        if gate is not None and gate_val > 0:
            for e in self.eng:
                self.prog[e].append(("w", "gate", gate_val))

    def _need(self, need, ev):
        if ev is None:
            return
        s, v = ev
        if need.get(s, 0) < v:
            need[s] = v

    def _waits(self, e, reads, writes):
        need = {}
        for k in reads:
            self._need(need, self.lastw.get(k))
        for k in writes:
            self._need(need, self.lastw.get(k))
            for ev in self.reads.get(k, ()):
                self._need(need, ev)
        eng = self.eng[e]
        seen = self.seen[e]
        for s, v in need.items():
            if seen.get(s, 0) >= v:
                continue
            self.prog[e].append(("w", s, v))
            seen[s] = v
            self.ninst += 1

    def _commit(self, ev, reads, writes):
        for k in writes:
            self.lastw[k] = ev
            self.reads[k] = []
        for k in reads:
            if k in writes:
                continue
            self.reads.setdefault(k, []).append(ev)
            if len(self.reads[k]) > 24:
                d = {}
                for s, v in self.reads[k]:
                    if d.get(s, 0) < v:
                        d[s] = v
                self.reads[k] = list(d.items())

    def op(self, e, fn, reads=(), writes=()):
        reads = tuple(reads)
        writes = tuple(writes)
        self._waits(e, reads, writes)
        self.cnt[e] += 1
        self.prog[e].append(("i", fn, e, 1))
        self.ninst += 1
        self._commit((e, self.cnt[e]), reads, writes)

    def dma(self, q, fn, reads=(), writes=()):
        reads = tuple(reads)
        writes = tuple(writes)
        st = self.dq[q]
        i = st["n"]
        slot = i % self.ndsq[q]
        sname = "d_%s%d" % (q, slot)
        rnd = i // self.ndsq[q]
        eng = self.eng[q]
        if rnd > 0 and self.seen[q].get(sname, 0) < 16 * rnd:
            self.prog[q].append(("w", sname, 16 * rnd))
            self.seen[q][sname] = 16 * rnd
            self.ninst += 1
        self._waits(q, reads, writes)
        self.prog[q].append(("i", fn, sname, 16))
        st["n"] = i + 1
        self.ninst += 1
        self._commit((sname, 16 * (rnd + 1)), reads, writes)

    def finish(self, keys, e="sp"):
        self._waits(e, tuple(keys), ())

    def drain_all(self, e="sp"):
        need = {}
        for en, c in self.cnt.items():
            if c:
                need[en] = c
        for q, stq in self.dq.items():
            n = stq["n"]
            nq = self.ndsq[q]
            for slot in range(nq):
                uses = (n - slot + nq - 1) // nq if n > slot else 0
                if uses:
                    need["d_%s%d" % (q, slot)] = 16 * uses
        eng = self.eng[e]
        for s, v in need.items():
            if self.seen[e].get(s, 0) >= v:
                continue
            self.prog[e].append(("w", s, v))
            self.seen[e][s] = v

    def emit(self):
        nc = self.nc
        with nc.Block() as block:
            def mk(e):
                def body(engine):
                    for it in self.prog[e]:
                        if it[0] == "w":
                            engine.wait_ge(self.sem[it[1]], it[2])
                        else:
                            it[1](engine).then_inc(self.sem[it[2]], it[3])
                return body
            block.sync(mk("sp"))
            block.scalar(mk("act"))
            block.vector(mk("dve"))
            block.gpsimd(mk("pool"))
            block.tensor(mk("pe"))
        self.prog = {e: [] for e in self.eng}

    def sync_all(self):
        for e in self.eng:
            self.drain_all(e)
        self.lastw = {}
        self.reads = {}

D = 1024
NMOD = 6
INW = 2336
INW_T = 19
EPS = 1e-6
NTOK = 2176


_UID = [0]


def _mk(nc, st):
    _UID[0] += 1
    u = "u%d_" % _UID[0]

    def sb(name, shape, dt=F32):
        return st.enter_context(nc.sbuf_tensor(u + name, shape, dt))

    def ps(name, shape, dt=F32):
        return st.enter_context(nc.psum_tensor(u + name, shape, dt))
    return sb, ps


def _groups(n, g=512):
    out = []
    t = 0
    while t < n:
        out.append((t, min(g, n - t)))
        t += g
    return out


def build_PA(ntok=NTOK, nctx=128):
    nc = bass.Bass("TRN2", target_bir_lowering=False)
    xT = nc.dram_tensor("xT", [D, ntok], F32, kind="ExternalInput").ap()
    cT = nc.dram_tensor("cT", [128, 16], F32, kind="ExternalInput").ap()
    w_mod = nc.dram_tensor("w_mod", [D, NMOD * D], F32, kind="ExternalInput").ap()
    b_mod = nc.dram_tensor("b_mod", [128, 48], F32, kind="ExternalInput").ap()
    g1 = nc.dram_tensor("g1", [128, 8], F32, kind="ExternalInput").ap()
    w_in = nc.dram_tensor("w_in", [D, INW], F32, kind="ExternalInput").ap()
    ones = nc.dram_tensor("ones", [128, 128], F32, kind="ExternalInput").ap()
    colsT = nc.dram_tensor("colsT", [INW_T * 128, ntok], F32, kind="ExternalOutput").ap()
    modT = nc.dram_tensor("modT", [128, 96], F32, kind="ExternalOutput").ap()
    with ExitStack() as st:
        S = Sched(nc, st)
        sb, ps = _mk(nc, st)
        CT = sb("CT", [128, 16]); SC = sb("SC", [128, 16]); BM = sb("BM", [128, 48]); G1 = sb("G1", [128, 8])
        ONES = sb("ONES", [128, 128]); MOD = sb("MOD", [128, 96])
        A1 = sb("A1", [128, 16]); TMPA = sb("TMPA", [128, 16])
        WM = [sb("WM%d" % i, [128, 8, 512]) for i in range(2)]
        WIN = sb("WIN", [128, 8, INW])
        XT = [sb("XT%d" % i, [128, 8, 512]) for i in range(2)]
        XSQ = sb("XSQ", [128, 512]); RSTD = sb("RSTD", [128, 512]); TMP = sb("TMP", [128, 512])
        HT = [sb("HT%d" % i, [128, 8, 512]) for i in range(2)]
        OUTB = [sb("OUTB%d" % i, [128, 512]) for i in range(4)]
        pmod = ps("pmod", [128, 96]); pss = ps("pss", [128, 512])
        pout = [ps("pout%d" % i, [128, 512]) for i in range(3)]

        S.dma("sp", lambda e: e.dma_start(out=CT[:], in_=cT), writes=["CT"])
        S.dma("sp", lambda e: e.dma_start(out=BM[:], in_=b_mod), writes=["BM"])
        S.dma("sp", lambda e: e.dma_start(out=G1[:], in_=g1), writes=["G1"])
        S.dma("sp", lambda e: e.dma_start(out=ONES[:], in_=ones), writes=["ONES"])
        S.op("act", lambda e: e.activation(SC[:], CT[:], AF.Silu), reads=["CT"], writes=["SC"])
        SC3 = SC[:].rearrange("p (k c) -> p k c", c=2)
        wm_v = w_mod.rearrange("(k p) f -> p k f", p=128)
        for jg in range(12):
            b = jg % 2
            S.dma("act" if jg % 2 else "sp",
                  lambda e, jg=jg, b=b: e.dma_start(out=WM[b][:], in_=wm_v[:, :, jg * 512:(jg + 1) * 512]),
                  writes=["WM%d" % b])
            for j8 in range(4):
                j = jg * 4 + j8
                for k in range(8):
                    S.op("pe", lambda e, j=j, j8=j8, k=k, b=b: e.matmul(
                        pmod[:, 2 * j:2 * j + 2], WM[b][:, k, j8 * 128:(j8 + 1) * 128], SC3[:, k, :],
                        start=(k == 0), stop=(k == 7)), reads=["WM%d" % b, "SC"], writes=["pmod"])
        S.op("dve", lambda e: e.tensor_tensor(MOD[:].rearrange("p (j c) -> p j c", c=2),
                                              pmod[:].rearrange("p (j c) -> p j c", c=2),
                                              BM[:].unsqueeze(2).to_broadcast([128, 48, 2]), ALU.add),
             reads=["pmod", "BM"], writes=["MOD"])
        S.dma("sp", lambda e: e.dma_start(out=modT, in_=MOD[:]), reads=["MOD"], writes=["modT"])
        MOD3 = MOD[:].rearrange("p (j c) -> p j c", c=2)
        S.op("dve", lambda e: e.tensor_scalar(TMPA[:].rearrange("p (j c) -> p j c", c=2), MOD3[:, 8:16, :], 1.0, None, ALU.add),
             reads=["MOD"], writes=["TMPA"])
        S.op("dve", lambda e: e.tensor_tensor(A1[:].rearrange("p (j c) -> p j c", c=2),
                                              TMPA[:].rearrange("p (j c) -> p j c", c=2),
                                              G1[:].unsqueeze(2).to_broadcast([128, 8, 2]), ALU.mult),
             reads=["TMPA", "G1"], writes=["A1"])
        A13 = A1[:].rearrange("p (j c) -> p j c", c=2)
        win_v = w_in.rearrange("(k p) f -> p k f", p=128)
        for k in range(8):
            S.dma("pool", lambda e, k=k: e.dma_start(out=WIN[:, k, :], in_=win_v[:, k, :]), writes=["WIN%d" % k])
        xT_v = xT.rearrange("(k p) t -> p k t", p=128)
        grp = [(0, nctx, 1)] if nctx else []
        grp += [(nctx + t0, tn, 0) for (t0, tn) in _groups(ntok - nctx)]
        oi = 0
        for gi, (t0, tn, col) in enumerate(grp):
            b = gi % 2
            xk, hk = "XT%d" % b, "HT%d" % b
            S.dma("sp", lambda e, b=b, t0=t0, tn=tn: e.dma_start(out=XT[b][:, :, :tn], in_=xT_v[:, :, t0:t0 + tn]), writes=[xk])
            for k in range(8):
                S.op("act", lambda e, b=b, k=k, tn=tn: e.activation(XSQ[:, :tn], XT[b][:, k, :tn], AF.Square), reads=[xk], writes=["XSQ"])
                S.op("pe", lambda e, k=k, tn=tn: e.matmul(pss[:, :tn], ONES[:], XSQ[:, :tn], start=(k == 0), stop=(k == 7)),
                     reads=["ONES", "XSQ"], writes=["pss"])
            S.op("dve", lambda e, tn=tn: e.tensor_scalar(RSTD[:, :tn], pss[:, :tn], 1.0 / D, EPS, ALU.mult, ALU.add), reads=["pss"], writes=["RSTD"])
            S.op("act", lambda e, tn=tn: e.sqrt(RSTD[:, :tn], RSTD[:, :tn]), reads=["RSTD"], writes=["RSTD"])
            S.op("dve", lambda e, tn=tn: e.reciprocal(RSTD[:, :tn], RSTD[:, :tn]), reads=["RSTD"], writes=["RSTD"])
            for k in range(8):
                S.op("dve", lambda e, b=b, k=k, tn=tn: e.tensor_tensor(TMP[:, :tn], XT[b][:, k, :tn], RSTD[:, :tn], ALU.mult),
                     reads=[xk, "RSTD"], writes=["TMP"])
                S.op("act", lambda e, b=b, k=k, tn=tn, col=col: e.activation(
                    HT[b][:, k, :tn], TMP[:, :tn], AF.Identity, bias=MOD3[:, k, col:col + 1], scale=A13[:, k, col:col + 1]),
                    reads=["TMP", "MOD", "A1"], writes=[hk])
            for ot in range(INW_T):
                m = 128 if ot < INW_T - 1 else INW - 128 * (INW_T - 1)
                pb = oi % 3
                ob = oi % 4
                oi += 1
                for k in range(8):
                    S.op("pe", lambda e, b=b, k=k, tn=tn, ot=ot, m=m, pb=pb: e.matmul(
                        pout[pb][:m, :tn], WIN[:, k, ot * 128:ot * 128 + m], HT[b][:, k, :tn], start=(k == 0), stop=(k == 7)),
                        reads=["WIN%d" % k, hk], writes=["pout%d" % pb])
                if m < 128:
                    S.op("dve", lambda e, ob=ob: e.memset(OUTB[ob][:], 0.0), writes=["OUTB%d" % ob])
                eng = "act" if oi % 2 else "dve"
                if eng == "act":
                    S.op("act", lambda e, ob=ob, pb=pb, m=m, tn=tn: e.copy(OUTB[ob][:m, :tn], pout[pb][:m, :tn]),
                         reads=["pout%d" % pb], writes=["OUTB%d" % ob])
                else:
                    S.op("dve", lambda e, ob=ob, pb=pb, m=m, tn=tn: e.tensor_copy(OUTB[ob][:m, :tn], pout[pb][:m, :tn]),
                         reads=["pout%d" % pb], writes=["OUTB%d" % ob])
                S.dma("sp" if oi % 2 else "act", lambda e, ob=ob, ot=ot, t0=t0, tn=tn: e.dma_start(
                    out=colsT[ot * 128:(ot + 1) * 128, t0:t0 + tn], in_=OUTB[ob][:, :tn]),
                    reads=["OUTB%d" % ob], writes=["colsT_%d" % oi])
        S.drain_all("sp")
        S.emit()
    return nc


SEQT = 4352
S5_CH = [(0, 256)] + [(256 + 512 * i, 512) for i in range(8)]


def build_PB():
    nc = bass.Bass("TRN2", target_bir_lowering=False)
    uin = [nc.dram_tensor(n, [128, SEQT], F32, kind="ExternalInput").ap() for n in ("uf", "ub")]
    prm = nc.dram_tensor("prm", [128, 24], F32, kind="ExternalInput").ap()
    bre = nc.dram_tensor("bre", [128, 128], F32, kind="ExternalInput").ap()
    bim = nc.dram_tensor("bim", [128, 128], F32, kind="ExternalInput").ap()
    cre = nc.dram_tensor("cre", [128, 128], F32, kind="ExternalInput").ap()
    cim = nc.dram_tensor("cim", [128, 128], F32, kind="ExternalInput").ap()
    tau = nc.dram_tensor("tau", [128, 512], F32, kind="ExternalInput").ap()
    ident = nc.dram_tensor("ident", [128, 128], F32, kind="ExternalInput").ap()
    yout = [nc.dram_tensor(n, [128, SEQT], F32, kind="ExternalOutput").ap() for n in ("yf", "yb")]
    TWO_PI = 2.0 * math.pi
    with ExitStack() as st:
        S = Sched(nc, st)
        sb, ps = _mk(nc, st)
        PRM = sb("PRM", [128, 24]); BRE = sb("BRE", [128, 128]); BIM = sb("BIM", [128, 128])
        CRE = sb("CRE", [128, 128]); CIM = sb("CIM", [128, 128]); TAU = sb("TAU", [128, 512]); ID = sb("ID", [128, 128])
        names = ["DT", "LR", "MAG", "TH", "R", "R2", "RF", "FR", "SIN", "COS", "ARE", "AIM", "DEN", "AM1", "FRE", "FIM", "T0", "T1"]
        P = {n: sb("p_" + n, [128, 8]) for n in names}
        RI = sb("p_RI", [128, 8], I32)
        BBR = sb("BBR", [128, 128]); BBI = sb("BBI", [128, 128]); TB = sb("TB", [128, 128])
        PAD = sb("PAD", [128, 128])
        WBR = sb("WBR", [128, 8, 128]); WBI = sb("WBI", [128, 8, 128]); CR = sb("CR", [128, 8, 128]); CIN = sb("CIN", [128, 8, 128])
        TC = sb("TC", [128, 8, 512]); TS = sb("TS", [128, 8, 512]); RHO = sb("RHO", [128, 8, 512])
        RR = sb("RR", [128, 512]); RRF = sb("RRF", [128, 512]); RRI = sb("RRI", [128, 512], I32)
        UC = [sb("UC%d" % i, [128, 512]) for i in range(2)]
        W = {}
        for n in ("BR", "BI", "T1", "T2", "T3", "T4", "XR", "XI", "QR", "QI", "HR", "HI"):
            for i in range(2):
                W[n, i] = sb("w_%s%d" % (n, i), [128, 512])
        HP = sb("HP", [128, 8])
        YO = [sb("YO%d" % i, [128, 512]) for i in range(2)]
        pbr = [ps("pbr%d" % i, [128, 512]) for i in range(2)]
        pbi = [ps("pbi%d" % i, [128, 512]) for i in range(2)]
        py = [ps("py%d" % i, [128, 512]) for i in range(2)]
        ptr = ps("ptr", [128, 128])

        for (t, src, k) in ((PRM, prm, "PRM"), (BRE, bre, "BRE"), (BIM, bim, "BIM"), (CRE, cre, "CRE"), (CIM, cim, "CIM"),
                            (TAU, tau, "TAU"), (ID, ident, "ID")):
            S.dma("sp", lambda e, t=t, src=src: e.dma_start(out=t[:], in_=src), writes=[k])
        PR3 = PRM[:].rearrange("p (a c) -> p a c", c=3)
        K = ["PP"]

        def V(fn, reads=(), writes=()):
            S.op("dve", fn, reads=list(reads) + K, writes=list(writes) + K)

        def A(fn, reads=(), writes=()):
            S.op("act", fn, reads=list(reads) + K, writes=list(writes) + K)

        A(lambda e: e.activation(P["DT"][:], PR3[:, :, 2], AF.Exp), reads=["PRM"])
        V(lambda e: e.tensor_scalar(P["LR"][:], PR3[:, :, 0], -1e-4, None, ALU.min), reads=["PRM"])
        V(lambda e: e.tensor_tensor(P["T0"][:], P["LR"][:], P["DT"][:], ALU.mult))
        A(lambda e: e.activation(P["MAG"][:], P["T0"][:], AF.Exp))
        V(lambda e: e.tensor_tensor(P["TH"][:], PR3[:, :, 1], P["DT"][:], ALU.mult), reads=["PRM"])
        V(lambda e: e.tensor_scalar(P["R"][:], P["TH"][:], 1.0 / TWO_PI, None, ALU.mult))
        V(lambda e: e.tensor_scalar(P["R2"][:], P["R"][:], 0.25, None, ALU.add))
        for (src, dst) in (("R", "SIN"), ("R2", "COS")):
            V(lambda e, src=src: e.tensor_copy(RI[:], P[src][:]))
            V(lambda e: e.tensor_copy(P["RF"][:], RI[:]))
            V(lambda e, src=src: e.tensor_tensor(P["FR"][:], P[src][:], P["RF"][:], ALU.subtract))
            A(lambda e, dst=dst: e.activation(P[dst][:], P["FR"][:], AF.Sin, scale=TWO_PI))
        V(lambda e: e.tensor_tensor(P["ARE"][:], P["MAG"][:], P["COS"][:], ALU.mult))
        V(lambda e: e.tensor_tensor(P["AIM"][:], P["MAG"][:], P["SIN"][:], ALU.mult))
        V(lambda e: e.tensor_tensor(P["T0"][:], P["LR"][:], P["LR"][:], ALU.mult))
        V(lambda e: e.tensor_tensor(P["T1"][:], PR3[:, :, 1], PR3[:, :, 1], ALU.mult), reads=["PRM"])
        V(lambda e: e.tensor_tensor(P["DEN"][:], P["T0"][:], P["T1"][:], ALU.add))
        V(lambda e: e.reciprocal(P["DEN"][:], P["DEN"][:]))
        V(lambda e: e.tensor_scalar(P["AM1"][:], P["ARE"][:], -1.0, None, ALU.add))
        V(lambda e: e.tensor_tensor(P["T0"][:], P["AM1"][:], P["LR"][:], ALU.mult))
        V(lambda e: e.tensor_tensor(P["T1"][:], P["AIM"][:], PR3[:, :, 1], ALU.mult), reads=["PRM"])
        V(lambda e: e.tensor_tensor(P["T0"][:], P["T0"][:], P["T1"][:], ALU.add))
        V(lambda e: e.tensor_tensor(P["FRE"][:], P["T0"][:], P["DEN"][:], ALU.mult))
        V(lambda e: e.tensor_tensor(P["T0"][:], P["AIM"][:], P["LR"][:], ALU.mult))
        V(lambda e: e.tensor_tensor(P["T1"][:], P["AM1"][:], PR3[:, :, 1], ALU.mult), reads=["PRM"])
        V(lambda e: e.tensor_tensor(P["T0"][:], P["T0"][:], P["T1"][:], ALU.subtract))
        V(lambda e: e.tensor_tensor(P["FIM"][:], P["T0"][:], P["DEN"][:], ALU.mult))

        def v3(t):
            return t[:].rearrange("p (a h) -> p a h", h=16)

        def bc(n):
            return P[n][:].unsqueeze(2).to_broadcast([128, 8, 16])
        V(lambda e: e.tensor_tensor(v3(BBR), v3(BRE), bc("FRE"), ALU.mult), reads=["BRE"])
        V(lambda e: e.tensor_tensor(v3(TB), v3(BIM), bc("FIM"), ALU.mult), reads=["BIM"])
        V(lambda e: e.tensor_tensor(BBR[:], BBR[:], TB[:], ALU.subtract))
        V(lambda e: e.tensor_tensor(v3(BBI), v3(BIM), bc("FRE"), ALU.mult), reads=["BIM"])
        V(lambda e: e.tensor_tensor(v3(TB), v3(BRE), bc("FIM"), ALU.mult), reads=["BRE"])
        V(lambda e: e.tensor_tensor(BBI[:], BBI[:], TB[:], ALU.add))
        V(lambda e: e.tensor_scalar(CIM[:], CIM[:], -1.0, None, ALU.mult), reads=["CIM"], writes=["CIM"])
        V(lambda e: e.memset(CR[:], 0.0)); V(lambda e: e.memset(CIN[:], 0.0))
        for dj in range(8):
            j = dj % 4
            for (src, dst) in ((BBR, WBR), (BBI, WBI)):
                V(lambda e: e.memset(PAD[:], 0.0), writes=["PAD"])
                V(lambda e, src=src, dj=dj, j=j: e.tensor_copy(PAD[0:64, 32 * j:32 * j + 16], src[0:64, dj * 16:dj * 16 + 16]), writes=["PAD"])
                V(lambda e, src=src, dj=dj, j=j: e.tensor_copy(PAD[64:128, 32 * j + 16:32 * j + 32], src[64:128, dj * 16:dj * 16 + 16]), writes=["PAD"])
                S.op("pe", lambda e: e.transpose(ptr[:], PAD[:], ID[:]), reads=["PAD", "ID"], writes=["ptr"])
                S.op("act", lambda e, dst=dst, dj=dj: e.copy(dst[:, dj, :], ptr[:]), reads=["ptr"], writes=["WB"])
            for (src, dst) in ((CRE, CR), (CIM, CIN)):
                V(lambda e, src=src, dst=dst, dj=dj, j=j: e.tensor_copy(dst[0:64, dj, 32 * j:32 * j + 16], src[0:64, dj * 16:dj * 16 + 16]), reads=["CRE", "CIM"], writes=["CC"])
                V(lambda e, src=src, dst=dst, dj=dj, j=j: e.tensor_copy(dst[64:128, dj, 32 * j + 16:32 * j + 32], src[64:128, dj * 16:dj * 16 + 16]), reads=["CRE", "CIM"], writes=["CC"])
            for (off, dst) in ((0.0, TS), (0.25, TC)):
                V(lambda e, dj=dj, off=off: e.tensor_scalar(RR[:], TAU[:], P["R"][:, dj:dj + 1], off, ALU.mult, ALU.add), reads=["TAU"], writes=["RR"])
                V(lambda e: e.tensor_copy(RRI[:], RR[:]), reads=["RR"], writes=["RRI"])
                V(lambda e: e.tensor_copy(RRF[:], RRI[:]), reads=["RRI"], writes=["RRF"])
                V(lambda e: e.tensor_tensor(RRF[:], RR[:], RRF[:], ALU.subtract), reads=["RR"], writes=["RRF"])
                S.op("act", lambda e, dst=dst, dj=dj: e.activation(dst[:, dj, :], RRF[:], AF.Sin, scale=TWO_PI), reads=["RRF"], writes=["TAB"])
            V(lambda e, dj=dj: e.tensor_copy(RHO[:, dj, :], P["MAG"][:, dj:dj + 1].to_broadcast([128, 512])), writes=["TAB"])

        G = "pool"
        oi = 0
        for d in range(2):
            V(lambda e: e.memset(HP[:], 0.0), writes=["HP"])
            for ci, (t0, T) in enumerate(S5_CH):
                ub_ = (d * 9 + ci) % 2
                uk = "UC%d" % ub_
                S.dma("sp", lambda e, d=d, t0=t0, T=T, ub_=ub_: e.dma_start(out=UC[ub_][:, :T], in_=uin[d][:, t0:t0 + T]), writes=[uk])
                yb_ = (d * 9 + ci) % 2
                for j in range(4):
                    dj = d * 4 + j
                    b = j % 2
                    w = lambda n, b=b, T=T: W[n, b][:, :T]
                    k = lambda n, b=b: "w_%s%d" % (n, b)
                    S.op("pe", lambda e, dj=dj, b=b, T=T, ub_=ub_: e.matmul(pbr[b][:, :T], WBR[:, dj, :], UC[ub_][:, :T], start=True, stop=True),
                         reads=["WB", uk], writes=["pbr%d" % b])
                    S.op("pe", lambda e, dj=dj, b=b, T=T, ub_=ub_: e.matmul(pbi[b][:, :T], WBI[:, dj, :], UC[ub_][:, :T], start=True, stop=True),
                         reads=["WB", uk], writes=["pbi%d" % b])
                    S.op("act", lambda e, w=w, b=b, T=T: e.copy(w("BR"), pbr[b][:, :T]), reads=["pbr%d" % b], writes=[k("BR")])
                    S.op("act", lambda e, w=w, b=b, T=T: e.copy(w("BI"), pbi[b][:, :T]), reads=["pbi%d" % b], writes=[k("BI")])
                    cs = lambda dj=dj, T=T: TC[:, dj, :T]
                    sn = lambda dj=dj, T=T: TS[:, dj, :T]
                    S.op("dve", lambda e, w=w, cs=cs: e.tensor_tensor(w("T1"), cs(), w("BR"), ALU.mult), reads=["TAB", k("BR")], writes=[k("T1")])
                    S.op("dve", lambda e, w=w, sn=sn: e.tensor_tensor(w("T2"), sn(), w("BI"), ALU.mult), reads=["TAB", k("BI")], writes=[k("T2")])
                    S.op("dve", lambda e, w=w: e.tensor_tensor(w("XR"), w("T1"), w("T2"), ALU.add), reads=[k("T1"), k("T2")], writes=[k("XR")])
                    S.op(G, lambda e, w=w, cs=cs: e.tensor_tensor(w("T3"), cs(), w("BI"), ALU.mult), reads=["TAB", k("BI")], writes=[k("T3")])
                    S.op(G, lambda e, w=w, sn=sn: e.tensor_tensor(w("T4"), sn(), w("BR"), ALU.mult), reads=["TAB", k("BR")], writes=[k("T4")])
                    S.op(G, lambda e, w=w: e.tensor_tensor(w("XI"), w("T3"), w("T4"), ALU.subtract), reads=[k("T3"), k("T4")], writes=[k("XI")])
                    S.op("dve", lambda e, w=w, dj=dj, j=j, T=T: e.tensor_tensor_scan(w("QR"), RHO[:, dj, :T], w("XR"), HP[:, 2 * j:2 * j + 1], ALU.mult, ALU.add),
                         reads=["TAB", k("XR"), "HP"], writes=[k("QR")])
                    S.op("dve", lambda e, w=w, dj=dj, j=j, T=T: e.tensor_tensor_scan(w("QI"), RHO[:, dj, :T], w("XI"), HP[:, 2 * j + 1:2 * j + 2], ALU.mult, ALU.add),
                         reads=["TAB", k("XI"), "HP"], writes=[k("QI")])
                    S.op("dve", lambda e, w=w, cs=cs: e.tensor_tensor(w("T1"), cs(), w("QR"), ALU.mult), reads=["TAB", k("QR")], writes=[k("T1")])
                    S.op("dve", lambda e, w=w, sn=sn: e.tensor_tensor(w("T2"), sn(), w("QI"), ALU.mult), reads=["TAB", k("QI")], writes=[k("T2")])
                    S.op("dve", lambda e, w=w: e.tensor_tensor(w("HR"), w("T1"), w("T2"), ALU.subtract), reads=[k("T1"), k("T2")], writes=[k("HR")])
                    S.op(G, lambda e, w=w, sn=sn: e.tensor_tensor(w("T3"), sn(), w("QR"), ALU.mult), reads=["TAB", k("QR")], writes=[k("T3")])
                    S.op(G, lambda e, w=w, cs=cs: e.tensor_tensor(w("T4"), cs(), w("QI"), ALU.mult), reads=["TAB", k("QI")], writes=[k("T4")])
                    S.op(G, lambda e, w=w: e.tensor_tensor(w("HI"), w("T3"), w("T4"), ALU.add), reads=[k("T3"), k("T4")], writes=[k("HI")])
                    S.op("act", lambda e, b=b, j=j, T=T: e.copy(HP[:, 2 * j:2 * j + 1], W["HR", b][:, T - 1:T]), reads=[k("HR")], writes=["HP"])
                    S.op("act", lambda e, b=b, j=j, T=T: e.copy(HP[:, 2 * j + 1:2 * j + 2], W["HI", b][:, T - 1:T]), reads=[k("HI")], writes=["HP"])
                    S.op("pe", lambda e, dj=dj, w=w, j=j, yb_=yb_, T=T: e.matmul(py[yb_][:, :T], CR[:, dj, :], w("HR"), start=(j == 0), stop=False),
                         reads=["CC", k("HR")], writes=["py%d" % yb_])
                    S.op("pe", lambda e, dj=dj, w=w, j=j, yb_=yb_, T=T: e.matmul(py[yb_][:, :T], CIN[:, dj, :], w("HI"), start=False, stop=(j == 3)),
                         reads=["CC", k("HI")], writes=["py%d" % yb_])
                S.op("act", lambda e, yb_=yb_, T=T: e.copy(YO[yb_][:, :T], py[yb_][:, :T]), reads=["py%d" % yb_], writes=["YO%d" % yb_])
                oi += 1
                S.dma("act", lambda e, d=d, yb_=yb_, t0=t0, T=T: e.dma_start(out=yout[d][:, t0:t0 + T], in_=YO[yb_][:, :T]),
                      reads=["YO%d" % yb_], writes=["yout_%d" % oi])
        S.drain_all("sp")
        S.emit()
    return nc


NCH = 68


def build_PC():
    nc = bass.Bass("TRN2", target_bir_lowering=False)
    I = {}
    for d in range(2):
        I["qT", d] = nc.dram_tensor("qT%d" % d, [128, SEQT], F32, kind="ExternalInput").ap()
        I["kT", d] = nc.dram_tensor("kT%d" % d, [128, SEQT], F32, kind="ExternalInput").ap()
        I["v", d] = nc.dram_tensor("v%d" % d, [SEQT, 256], F32, kind="ExternalInput").ap()
        I["zT", d] = nc.dram_tensor("zT%d" % d, [16, SEQT], F32, kind="ExternalInput").ap()
        I["wg", d] = nc.dram_tensor("wg%d" % d, [16, 128], F32, kind="ExternalInput").ap()
        I["bg", d] = nc.dram_tensor("bg%d" % d, [128, 1], F32, kind="ExternalInput").ap()
        I["o", d] = nc.dram_tensor("o%d" % d, [SEQT, 256], F32, kind="ExternalOutput").ap()
    rst = nc.dram_tensor("rst", [128, 512], F32, kind="ExternalInput").ap()
    tmask = nc.dram_tensor("tmask", [64, 256], F32, kind="ExternalInput").ap()
    blk = nc.dram_tensor("blk", [128, 256], F32, kind="ExternalInput").ap()
    hmask = nc.dram_tensor("hmask", [128, 4], F32, kind="ExternalInput").ap()
    ident = nc.dram_tensor("ident", [128, 128], F32, kind="ExternalInput").ap()
    QSC = 32 ** -0.5
    with ExitStack() as st:
        S = Sched(nc, st)
        sb, ps = _mk(nc, st)
        RST = sb("RST", [128, 512]); TM = sb("TM", [64, 256]); BLK = sb("BLK", [128, 256]); HM = sb("HM", [128, 4]); ID = sb("ID", [128, 128])
        WG = sb("WG", [16, 128]); BG = sb("BG", [128, 1]); NBG = sb("NBG", [128, 1])
        Wb = {}
        for n in ("Q", "K", "LA", "B", "E", "D", "QE", "QS", "KD", "KS0", "KS1", "KS2", "KS3"):
            for i in range(2):
                Wb[n, i] = sb("g_%s%d" % (n, i), [128, 512])
        Z = [sb("Z%d" % i, [16, 512]) for i in range(2)]
        VV = [sb("VV%d" % i, [64, 8, 256]) for i in range(2)]
        DEC = [sb("DEC%d" % i, [128, 8]) for i in range(2)]
        KDT = [sb("KDT%d" % i, [64, 128]) for i in range(2)]
        STt = [sb("ST%d" % i, [64, 256]) for i in range(2)]
        OB = [sb("OB%d" % i, [64, 256]) for i in range(3)]
        KVM = sb("KVM", [128, 256])
        SS = [sb("SS%d" % i, [128, 256]) for i in range(2)]
        pza = ps("pza", [128, 512])
        pt0 = ps("pt0", [64, 128])
        pt = [pt0, pt0]
        pkv = [ps("pkv%d" % i, [128, 256]) for i in range(2)]
        psc = [ps("psc%d" % i, [64, 256]) for i in range(2)]
        po = [ps("po%d" % i, [64, 256]) for i in range(2)]
        for (t, src, k) in ((RST, rst, "RST"), (TM, tmask, "TM"), (BLK, blk, "BLK"), (HM, hmask, "HM"), (ID, ident, "ID")):
            S.dma("sp", lambda e, t=t, src=src: e.dma_start(out=t[:], in_=src), writes=[k])
        gc = 0
        oi = 0
        for d in range(2):
            S.dma("sp", lambda e, d=d: e.dma_start(out=WG[:], in_=I["wg", d]), writes=["WG"])
            S.dma("sp", lambda e, d=d: e.dma_start(out=BG[:], in_=I["bg", d]), writes=["BG"])
            S.op("dve", lambda e: e.tensor_scalar(NBG[:], BG[:], -1.0, None, ALU.mult), reads=["BG"], writes=["NBG"])
            S.op("dve", lambda e: e.memset(SS[0][:], 0.0), writes=["SS0"])
            scur = 0
            for bi, (t0, T) in enumerate(S5_CH):
                nchk = T // 64
                b = (d * 9 + bi) % 2
                w = lambda n, b=b, T=T: Wb[n, b][:, :T]
                k = lambda n, b=b: "g_%s%d" % (n, b)
                w3 = lambda n, b=b, T=T: Wb[n, b][:, :T].rearrange("p (c s) -> p c s", s=64)
                S.dma("sp", lambda e, d=d, b=b, t0=t0, T=T: e.dma_start(out=Wb["Q", b][:, :T], in_=I["qT", d][:, t0:t0 + T]), writes=[k("Q")])
                S.dma("act", lambda e, d=d, b=b, t0=t0, T=T: e.dma_start(out=Wb["K", b][:, :T], in_=I["kT", d][:, t0:t0 + T]), writes=[k("K")])
                S.dma("sp", lambda e, d=d, b=b, t0=t0, T=T: e.dma_start(out=Z[b][:, :T], in_=I["zT", d][:, t0:t0 + T]), writes=["Z%d" % b])
                S.dma("act", lambda e, d=d, b=b, t0=t0, T=T, nchk=nchk: e.dma_start(
                    out=VV[b][:, :nchk, :], in_=I["v", d][t0:t0 + T, :].rearrange("(c s) f -> s c f", s=64)), writes=["VV%d" % b])
                S.op("pe", lambda e, b=b, T=T: e.matmul(pza[:, :T], WG[:], Z[b][:, :T], start=True, stop=True), reads=["WG", "Z%d" % b], writes=["pza"])
                S.op("act", lambda e, w=w, T=T: e.activation(w("E"), pza[:, :T], AF.Exp, bias=NBG[:], scale=-1.0), reads=["pza", "NBG"], writes=[k("E")])
                S.op("act", lambda e, w=w: e.activation(w("E"), w("E"), AF.Ln, bias=1.0), reads=[k("E")], writes=[k("E")])
                S.op("dve", lambda e, w=w: e.tensor_scalar(w("LA"), w("E"), -1.0 / 16.0, None, ALU.mult), reads=[k("E")], writes=[k("LA")])
                S.op("dve", lambda e, w=w, T=T: e.tensor_tensor_scan(w("B"), RST[:, :T], w("LA"), 0.0, ALU.mult, ALU.add), reads=["RST", k("LA")], writes=[k("B")])
                S.op("act", lambda e, b=b, w3=w3, nchk=nchk: e.activation(DEC[b][:, :nchk], w3("B")[:, :, 63], AF.Exp), reads=[k("B")], writes=["DEC%d" % b])
                S.op("act", lambda e, w=w: e.activation(w("E"), w("B"), AF.Exp), reads=[k("B")], writes=[k("E")])
                S.op("dve", lambda e, w=w: e.scalar_tensor_tensor(w("QE"), w("Q"), QSC, w("E"), ALU.mult, ALU.mult), reads=[k("Q"), k("E")], writes=[k("QE")])
                S.op("dve", lambda e, w3=w3, nchk=nchk: e.tensor_tensor(w3("D"), w3("B"), w3("B")[:, :, 32:33].to_broadcast([128, nchk, 64]), ALU.subtract),
                     reads=[k("B")], writes=[k("D")])
                S.op("act", lambda e, w=w: e.activation(w("E"), w("D"), AF.Exp), reads=[k("D"), k("QE")], writes=[k("E")])
                S.op("dve", lambda e, w=w: e.scalar_tensor_tensor(w("QS"), w("Q"), QSC, w("E"), ALU.mult, ALU.mult), reads=[k("Q"), k("E")], writes=[k("QS")])
                S.op("act", lambda e, w=w: e.activation(w("E"), w("D"), AF.Exp, scale=-1.0), reads=[k("D"), k("QS")], writes=[k("E")])
                S.op("dve", lambda e, w=w: e.tensor_tensor(w("LA"), w("K"), w("E"), ALU.mult), reads=[k("K"), k("E"), k("B")], writes=[k("LA")])
                for h in range(4):
                    S.op("pool", lambda e, w=w, h=h: e.tensor_scalar(w("KS%d" % h), w("LA"), HM[:, h:h + 1], None, ALU.mult),
                         reads=[k("LA"), "HM"], writes=[k("KS%d" % h)])
                S.op("dve", lambda e, w3=w3, nchk=nchk: e.tensor_tensor(w3("D"), w3("B")[:, :, 63:64].to_broadcast([128, nchk, 64]), w3("B"), ALU.subtract),
                     reads=[k("B"), k("E"), k("LA")], writes=[k("D")])
                S.op("act", lambda e, w=w: e.activation(w("D"), w("D"), AF.Exp), reads=[k("D")], writes=[k("D")])
                S.op("dve", lambda e, w=w: e.tensor_tensor(w("KD"), w("K"), w("D"), ALU.mult), reads=[k("K"), k("D")], writes=[k("KD")])
                for c in range(nchk):
                    p2 = gc % 2
                    gc += 1
                    cs = slice(c * 64, (c + 1) * 64)
                    S.op("pe", lambda e, b=b, cs=cs, p2=p2: e.transpose(pt[p2][:], Wb["KD", b][:, cs], ID[:]), reads=[k("KD"), "ID"], writes=["pt"])
                    S.op("act", lambda e, p2=p2: e.copy(KDT[p2][:], pt[p2][:]), reads=["pt"], writes=["KDT%d" % p2])
                    S.op("pe", lambda e, b=b, c=c, p2=p2: e.matmul(pkv[p2][:], KDT[p2][:], VV[b][:, c, :], start=True, stop=True),
                         reads=["KDT%d" % p2, "VV%d" % b], writes=["pkv%d" % p2])
                    for h in range(4):
                        S.op("pe", lambda e, b=b, cs=cs, p2=p2, h=h: e.matmul(psc[p2][:, h * 64:(h + 1) * 64], Wb["KS%d" % h, b][:, cs], Wb["QS", b][:, cs],
                                                                            start=True, stop=True),
                             reads=[k("KS%d" % h), k("QS")], writes=["psc%d" % p2])
                    S.op("dve", lambda e, p2=p2: e.tensor_tensor(STt[p2][:], psc[p2][:], TM[:], ALU.mult), reads=["psc%d" % p2, "TM"], writes=["ST%d" % p2])
                    for h in range(4):
                        hs = slice(h * 64, (h + 1) * 64)
                        S.op("pe", lambda e, b=b, c=c, p2=p2, hs=hs: e.matmul(po[p2][:, hs], STt[p2][:, hs], VV[b][:, c, hs], start=True, stop=False),
                             reads=["ST%d" % p2, "VV%d" % b], writes=["po%d" % p2])
                        S.op("pe", lambda e, b=b, cs=cs, p2=p2, hs=hs, scur=scur: e.matmul(po[p2][:, hs], Wb["QE", b][:, cs], SS[scur][:, hs], start=False, stop=True),
                             reads=[k("QE"), "SS%d" % scur], writes=["po%d" % p2])
                    ob = oi % 3
                    oi += 1
                    S.op("act", lambda e, ob=ob, p2=p2: e.copy(OB[ob][:], po[p2][:]), reads=["po%d" % p2], writes=["OB%d" % ob])
                    S.dma("sp" if oi % 2 else "act", lambda e, d=d, ob=ob, t0=t0, c=c: e.dma_start(out=I["o", d][t0 + c * 64:t0 + (c + 1) * 64, :], in_=OB[ob][:]),
                          reads=["OB%d" % ob], writes=["o_%d" % oi])
                    S.op("dve", lambda e, p2=p2: e.tensor_tensor(KVM[:], pkv[p2][:], BLK[:], ALU.mult), reads=["pkv%d" % p2, "BLK"], writes=["KVM"])
                    S.op("dve", lambda e, b=b, c=c, scur=scur: e.scalar_tensor_tensor(SS[1 - scur][:], SS[scur][:], DEC[b][:, c:c + 1], KVM[:], ALU.mult, ALU.add),
                         reads=["SS%d" % scur, "DEC%d" % b, "KVM"], writes=["SS%d" % (1 - scur)])
                    scur = 1 - scur
        S.drain_all("sp")
        S.emit()
    return nc


NEXP = 16384


def build_PD(ntok, nctx, last):
    nc = bass.Bass("TRN2", target_bir_lowering=False)
    def din(name, shape, dt=F32):
        return nc.dram_tensor(name, shape, dt, kind="ExternalInput").ap()
    xT = din("xT", [D, ntok]); modT = din("modT", [128, 96])
    su = din("su", [ntok, 256]); sv = din("sv", [ntok, 256]); gg = din("gg", [ntok, 512])
    s5u = din("s5u", [256, ntok]); yf = din("yf", [256, ntok]); yb = din("yb", [256, ntok])
    of_ = din("of", [ntok, 512]); ob_ = din("ob", [ntok, 512])
    w_out = din("w_out", [D, D]); wsT = din("wsT", [128, 512]); sgub = din("sgub", [128, 4]); s5d = din("s5d", [128, 2])
    wglu = din("wglu", [256, 512]); ng = din("ng", [128, 512]); g2 = din("g2", [128, 8]); wq = din("wq", [D, 2048])
    keysT = din("keysT", [128, 2048]); eu = din("eu", [NEXP, D]); ev = din("ev", [NEXP, D]); gfin = din("gfin", [128, 8])
    ones = din("ones", [128, 128]); ident = din("ident", [128, 128]); iota16 = din("iota16", [128, 16])
    xo = nc.dram_tensor("xo", [D, ntok], F32, kind="ExternalOutput").ap()
    nb = ntok // 128
    with ExitStack() as st:
        S = Sched(nc, st)
        sb, ps = _mk(nc, st)
        MOD = sb("MOD", [128, 96]); WOUT = sb("WOUT", [128, 8, D]); WS = sb("WS", [128, 512]); SGUB = sb("SGUB", [128, 4]); S5D = sb("S5D", [128, 2])
        WGLU = sb("WGLU", [128, 2, 512]); NG = sb("NG", [128, 512]); G2 = sb("G2", [128, 8]); WQ = sb("WQ", [128, 8, 2048]); KEYS = sb("KEYS", [128, 2048])
        GF = sb("GF", [128, 8]); ONES = sb("ONES", [128, 128]); ID = sb("ID", [128, 128]); IOTA = sb("IOTA", [128, 16])
        A2 = sb("A2", [128, 16]); TA = sb("TA", [128, 16])
        U_ = sb("U_", [128, 256]); V_ = sb("V_", [128, 256]); GU = sb("GU", [128, 256]); GV = sb("GV", [128, 256]); SQ = sb("SQ", [128, 512])
        SS = sb("SS", [128, 8]); VN = sb("VN", [128, 256]); MIX = sb("MIX", [128, D])
        S5U = sb("S5U", [128, 2, 128]); YF = sb("YF", [128, 2, 128]); YB = sb("YB", [128, 2, 128]); GE = sb("GE", [128, 2, 128]); SG = sb("SG", [128, 256])
        OF = sb("OF", [128, 512]); OB = sb("OB", [128, 512]); GG = sb("GG", [128, 512]); SL = sb("SL", [128, 512])
        XT = sb("XT", [128, 8, 128]); X1 = sb("X1", [128, 8, 128]); XO = XT; HT = sb("HT", [128, 8, 128]); MIXT = HT; HTOK = sb("HTOK", [128, D])
        RSTD = sb("RSTD", [128, 128]); TMPB = sb("TMPB", [128, 128])
        SC = sb("SC", [128, 2048])
        M16 = sb("M16", [128, 256]); I16 = sb("I16", [128, 256], U32); IF16 = sb("IF16", [128, 256]); I1S = sb("I1S", [128, 128])
        CS = sb("CS", [128, 2048]); SC2 = CS; QT = CS[:].rearrange("p (q t) -> p q t", t=128); CS2 = sb("CS2", [128, 256])
        T16 = sb("T16", [128, 128]); P16 = sb("P16", [128, 128], U32); PF = sb("PF", [128, 128]); AI = sb("AI", [128, 128], I32)
        AFL = sb("AFL", [128, 128]); BFL = sb("BFL", [128, 128]); E1 = sb("E1", [128, 128]); E2 = sb("E2", [128, 128])
        EG = sb("EG", [128, 128]); GATE = sb("GATE", [128, 128]); IDXTI = sb("IDXTI", [128, 128], I32); GATET = sb("GATET", [128, 128])
        ACTT = sb("ACTT", [128, 128]); WT = sb("WT", [128, 128])
        UG = [sb("UG%d" % i, [128, D]) for i in range(2)]; VG = UG
        HB = [sb("HB%d" % i, [128, D]) for i in range(2)]
        P0 = ps("P0", [128, 2048]); P1 = ps("P1", [128, 1024]); P2 = ps("P2", [128, 512]); P3 = ps("P3", [128, 512])

        def V(fn, r=(), w=()):
            S.op("dve", fn, reads=r, writes=w)

        def A(fn, r=(), w=()):
            S.op("act", fn, reads=r, writes=w)

        def PE(fn, r=(), w=()):
            S.op("pe", fn, reads=r, writes=w)

        def LD(q, t, src, key):
            S.dma(q, lambda e: e.dma_start(out=t, in_=src), writes=[key])

        LD("sp", MOD[:], modT, "MOD"); LD("sp", WS[:], wsT, "WS"); LD("sp", SGUB[:], sgub, "SGUB"); LD("sp", S5D[:], s5d, "S5D")
        LD("sp", WGLU[:], wglu.rearrange("(c p) f -> p c f", p=128), "WGLU"); LD("sp", NG[:], ng, "NG"); LD("sp", G2[:], g2, "G2")
        LD("sp", KEYS[:], keysT, "KEYS"); LD("sp", GF[:], gfin, "GF"); LD("sp", ONES[:], ones, "ONES"); LD("sp", ID[:], ident, "ID"); LD("sp", IOTA[:], iota16, "IOTA")
        wo_v = w_out.rearrange("(k p) f -> p k f", p=128)
        wq_v = wq.rearrange("(k p) f -> p k f", p=128)
        for k in range(8):
            LD("act", WOUT[:, k, :], wo_v[:, k, :], "WOUT")
            LD("act", WQ[:, k, :], wq_v[:, k, :], "WQ")
        MOD3 = MOD[:].rearrange("p (j c) -> p j c", c=2)
        V(lambda e: e.tensor_scalar(TA[:].rearrange("p (j c) -> p j c", c=2), MOD3[:, 32:40, :], 1.0, None, ALU.add), ["MOD"], ["TA"])
        V(lambda e: e.tensor_tensor(A2[:].rearrange("p (j c) -> p j c", c=2), TA[:].rearrange("p (j c) -> p j c", c=2),
                                    G2[:].unsqueeze(2).to_broadcast([128, 8, 2]), ALU.mult), ["TA", "G2"], ["A2"])
        A23 = A2[:].rearrange("p (j c) -> p j c", c=2)

        def rs_from_ss(ss, n, scale):
            V(lambda e: e.tensor_scalar(ss, ss, scale, EPS, ALU.mult, ALU.add), ["SS"], ["SS"])
            A(lambda e: e.sqrt(ss, ss), ["SS"], ["SS"])
            V(lambda e: e.reciprocal(ss, ss), ["SS"], ["SS"])

        def top16(src, scratch, mout, iout, n):
            V(lambda e: e.max(mout[:, 0:8], src), ["TK"], ["TK"])
            V(lambda e: e.max_index(iout[:, 0:8], mout[:, 0:8], src), ["TK"], ["TK"])
            V(lambda e: e.match_replace(scratch, mout[:, 0:8], src, -1e30), ["TK"], ["TK"])
            V(lambda e: e.max(mout[:, 8:16], scratch), ["TK"], ["TK"])
            V(lambda e: e.max_index(iout[:, 8:16], mout[:, 8:16], scratch), ["TK"], ["TK"])

        xT_v = xT.rearrange("(k p) t -> p k t", p=128)
        xo_v = xo.rearrange("(k p) t -> p k t", p=128)
        s5u_v = s5u.rearrange("(c p) t -> p c t", p=128)
        yf_v = yf.rearrange("(c p) t -> p c t", p=128)
        yb_v = yb.rearrange("(c p) t -> p c t", p=128)
        for bi in range(nb):
            tk = slice(bi * 128, (bi + 1) * 128)
            col = 1 if bi * 128 < nctx else 0
            LD("sp", U_[:], su[tk, :], "U_"); LD("sp", V_[:], sv[tk, :], "V_"); LD("sp", GG[:], gg[tk, :], "GG")
            LD("act", S5U[:], s5u_v[:, :, tk], "S5U"); LD("act", YF[:], yf_v[:, :, tk], "YF"); LD("act", YB[:], yb_v[:, :, tk], "YB")
            LD("sp", OF[:], of_[tk, :], "OF"); LD("sp", OB[:], ob_[tk, :], "OB"); LD("act", XT[:], xT_v[:, :, tk], "XT")
            A(lambda e: e.activation(GU[:], U_[:], AF.Gelu), ["U_"], ["GU"])
            A(lambda e: e.activation(GV[:], V_[:], AF.Gelu), ["V_"], ["GV"])
            V(lambda e: e.tensor_tensor(SQ[:, 0:256], GV[:], GV[:], ALU.mult), ["GV"], ["SQ"])
            V(lambda e: e.tensor_reduce(SS[:, 0:4], SQ[:, 0:256].rearrange("p (h d) -> p h d", d=64), AX.X, ALU.add), ["SQ"], ["SS"])
            rs_from_ss(SS[:, 0:4], 4, 1.0 / 64)
            V(lambda e: e.tensor_tensor(VN[:].rearrange("p (h d) -> p h d", d=64), GV[:].rearrange("p (h d) -> p h d", d=64),
                                        SS[:, 0:4].unsqueeze(2).to_broadcast([128, 4, 64]), ALU.mult), ["GV", "SS"], ["VN"])
            for h in range(4):
                PE(lambda e, h=h: e.matmul(P2[:, h * 64:(h + 1) * 64], WS[:, h * 128:(h + 1) * 128], VN[:, h * 64:(h + 1) * 64], start=True, stop=True),
                   ["WS", "VN"], ["P2"])
            V(lambda e: e.tensor_tensor(MIX[:, 0:256].rearrange("p (h d) -> p h d", d=64), P2[:, 0:256].rearrange("p (h d) -> p h d", d=64),
                                        SGUB[:].unsqueeze(2).to_broadcast([128, 4, 64]), ALU.add), ["P2", "SGUB"], ["MIXa"])
            V(lambda e: e.tensor_tensor(MIX[:, 0:256], MIX[:, 0:256], GU[:], ALU.mult), ["GU"], ["MIXa"])
            V(lambda e: e.tensor_tensor(YF[:], YF[:], YB[:], ALU.add), ["YB"], ["YF"])
            for ct in range(2):
                V(lambda e, ct=ct: e.scalar_tensor_tensor(YF[:, ct, :], S5U[:, ct, :], S5D[:, ct:ct + 1], YF[:, ct, :], ALU.mult, ALU.add),
                  ["S5U", "S5D"], ["YF"])
            A(lambda e: e.activation(GE[:], YF[:], AF.Gelu), ["YF"], ["GE"])
            for ct in range(2):
                PE(lambda e, ct=ct: e.matmul(P3[:, 0:512], GE[:, ct, :], WGLU[:, ct, :], start=(ct == 0), stop=(ct == 1)), ["GE", "WGLU"], ["P3"])
            A(lambda e: e.activation(SG[:], P3[:, 256:512], AF.Sigmoid), ["P3"], ["SG"])
            V(lambda e: e.tensor_tensor(MIX[:, 256:512], P3[:, 0:256], SG[:], ALU.mult), ["P3", "SG"], ["MIXb"])
            V(lambda e: e.tensor_tensor(OF[:], OF[:], OB[:], ALU.add), ["OB"], ["OF"])
            V(lambda e: e.tensor_tensor(SQ[:], OF[:], OF[:], ALU.mult), ["OF"], ["SQ"])
            V(lambda e: e.tensor_reduce(SS[:, 0:8], SQ[:].rearrange("p (h d) -> p h d", d=64), AX.X, ALU.add), ["SQ"], ["SS"])
            rs_from_ss(SS[:, 0:8], 8, 1.0 / 64)
            V(lambda e: e.tensor_tensor(OF[:].rearrange("p (h d) -> p h d", d=64), OF[:].rearrange("p (h d) -> p h d", d=64),
                                        SS[:, 0:8].unsqueeze(2).to_broadcast([128, 8, 64]), ALU.mult), ["SS"], ["OF"])
            V(lambda e: e.tensor_tensor(OF[:], OF[:], NG[:], ALU.mult), ["NG"], ["OF"])
            A(lambda e: e.activation(SL[:], GG[:], AF.Silu), ["GG"], ["SL"])
            V(lambda e: e.tensor_tensor(MIX[:, 512:1024], OF[:], SL[:], ALU.mult), ["OF", "SL"], ["MIXc"])
            for f in range(8):
                pp, pk = (P2, "P2") if f % 2 == 0 else (P3, "P3")
                PE(lambda e, f=f, pp=pp: e.transpose(pp[:, 0:128], MIX[:, f * 128:(f + 1) * 128], ID[:]), ["MIXa", "MIXb", "MIXc", "ID"], [pk])
                A(lambda e, f=f, pp=pp: e.copy(MIXT[:, f, :], pp[:, 0:128]), [pk], ["HT"])
            for ot in range(8):
                pp, pk = (P2, "P2") if ot % 2 == 0 else (P3, "P3")
                for k in range(8):
                    PE(lambda e, ot=ot, k=k, pp=pp: e.matmul(pp[:, 0:128], WOUT[:, k, ot * 128:(ot + 1) * 128], MIXT[:, k, :], start=(k == 0), stop=(k == 7)),
                       ["WOUT", "HT"], [pk])
                V(lambda e, ot=ot, pp=pp, col=col: e.scalar_tensor_tensor(X1[:, ot, :], pp[:, 0:128], MOD3[:, 16 + ot, col:col + 1], XT[:, ot, :], ALU.mult, ALU.add),
                  [pk, "MOD", "XT"], ["X1"])
            for k in range(8):
                A(lambda e, k=k: e.activation(TMPB[:], X1[:, k, :], AF.Square), ["X1"], ["TMPB"])
                PE(lambda e, k=k: e.matmul(P2[:, 0:128], ONES[:], TMPB[:], start=(k == 0), stop=(k == 7)), ["ONES", "TMPB"], ["P2"])
            V(lambda e: e.tensor_scalar(RSTD[:], P2[:, 0:128], 1.0 / D, EPS, ALU.mult, ALU.add), ["P2"], ["RSTD"])
            A(lambda e: e.sqrt(RSTD[:], RSTD[:]), ["RSTD"], ["RSTD"])
            V(lambda e: e.reciprocal(RSTD[:], RSTD[:]), ["RSTD"], ["RSTD"])
            for k in range(8):
                V(lambda e, k=k: e.tensor_tensor(TMPB[:], X1[:, k, :], RSTD[:], ALU.mult), ["X1", "RSTD"], ["TMPB"])
                A(lambda e, k=k, col=col: e.activation(HT[:, k, :], TMPB[:], AF.Identity, bias=MOD3[:, 24 + k, col:col + 1], scale=A23[:, k, col:col + 1]),
                  ["TMPB", "MOD", "A2"], ["HT"])
            for k in range(8):
                pp, pk = (P2, "P2") if k % 2 == 0 else (P3, "P3")
                PE(lambda e, k=k, pp=pp: e.transpose(pp[:, 0:128], HT[:, k, :], ID[:]), ["HT", "ID"], [pk])
                A(lambda e, k=k, pp=pp: e.copy(HTOK[:, k * 128:(k + 1) * 128], pp[:, 0:128]), [pk], ["HTOK"])
            for qt in range(16):
                pp, pk = (P2, "P2") if qt % 2 == 0 else (P3, "P3")
                for k in range(8):
                    PE(lambda e, qt=qt, k=k, pp=pp: e.matmul(pp[:, 0:128], WQ[:, k, qt * 128:(qt + 1) * 128], HT[:, k, :], start=(k == 0), stop=(k == 7)),
                       ["WQ", "HT"], [pk])
                if qt % 2 == 0:
                    A(lambda e, qt=qt, pp=pp: e.copy(QT[:, qt, :], pp[:, 0:128]), [pk], ["TK"])
                else:
                    V(lambda e, qt=qt, pp=pp: e.tensor_copy(QT[:, qt, :], pp[:, 0:128]), [pk], ["TK"])
            for qt in range(16):
                PE(lambda e, qt=qt: e.matmul(P0[:, qt * 128:(qt + 1) * 128], QT[:, qt, :], KEYS[:, qt * 128:(qt + 1) * 128], start=True, stop=True),
                   ["TK", "KEYS"], ["P0a", "P0b"])
            for q4 in range(4):
                A(lambda e, q4=q4: e.copy(SC[:, q4 * 512:(q4 + 1) * 512], P0[:, q4 * 512:(q4 + 1) * 512]), ["P0a", "P0b"], ["TK"])
            for qt in range(16):
                top16(SC[:, qt * 128:(qt + 1) * 128], SC2[:, qt * 128:(qt + 1) * 128], M16[:, qt * 16:(qt + 1) * 16], I16[:, qt * 16:(qt + 1) * 16], 128)
            V(lambda e: e.tensor_copy(IF16[:], I16[:]), ["TK"], ["TK"])
            M4 = M16[:].rearrange("p (h q k) -> p h q k", q=2, k=16)
            IF4 = IF16[:].rearrange("p (h q k) -> p h q k", q=2, k=16)
            I1S3 = I1S[:].rearrange("p (h k) -> p h k", k=16)
            V(lambda e: e.tensor_scalar(I1S3, IF4[:, :, 0, :], 128.0, None, ALU.mult), ["TK"], ["TK"])
            CS4 = CS[:].rearrange("p (h a b) -> p h a b", a=16, b=16)
            V(lambda e: e.tensor_tensor(CS4, M4[:, :, 0, :].unsqueeze(3).to_broadcast([128, 8, 16, 16]),
                                        M4[:, :, 1, :].unsqueeze(2).to_broadcast([128, 8, 16, 16]), ALU.add), ["TK"], ["TK"])
            for h in range(8):
                top16(CS[:, h * 256:(h + 1) * 256], CS2[:], T16[:, h * 16:(h + 1) * 16], P16[:, h * 16:(h + 1) * 16], 256)
            V(lambda e: e.tensor_copy(PF[:], P16[:]), ["TK"], ["TK"])
            V(lambda e: e.tensor_scalar(AFL[:], PF[:], -7.5, 1.0 / 16, ALU.add, ALU.mult), ["TK"], ["TK"])
            V(lambda e: e.tensor_copy(AI[:], AFL[:]), ["TK"], ["TK"])
            V(lambda e: e.tensor_copy(AFL[:], AI[:]), ["TK"], ["TK"])
            V(lambda e: e.scalar_tensor_tensor(BFL[:], AFL[:], -16.0, PF[:], ALU.mult, ALU.add), ["TK"], ["TK"])
            EQ4 = CS[:].rearrange("p (h k a) -> p h k a", k=16, a=16)
            io4 = IOTA[:].unsqueeze(1).unsqueeze(1).to_broadcast([128, 8, 16, 16])
            for (sel, src, dst) in ((AFL, I1S3, E1), (BFL, IF4[:, :, 1, :], E2)):
                V(lambda e, sel=sel: e.tensor_tensor(EQ4, io4, sel[:].rearrange("p (h k) -> p h k", k=16).unsqueeze(3).to_broadcast([128, 8, 16, 16]), ALU.is_equal),
                  ["TK", "IOTA"], ["TK"])
                V(lambda e, src=src: e.tensor_tensor(EQ4, EQ4, src.unsqueeze(2).to_broadcast([128, 8, 16, 16]), ALU.mult), ["TK"], ["TK"])
                V(lambda e, dst=dst: e.tensor_reduce(dst[:], CS[:].rearrange("p (m a) -> p m a", a=16), AX.X, ALU.add), ["TK"], ["TK"])
            V(lambda e: e.tensor_tensor(E1[:], E1[:], E2[:], ALU.add), ["TK"], ["TK"])
            T3 = T16[:].rearrange("p (h k) -> p h k", k=16)
            V(lambda e: e.tensor_tensor(EG[:].rearrange("p (h k) -> p h k", k=16), T3, T3[:, :, 0:1].to_broadcast([128, 8, 16]), ALU.subtract), ["TK"], ["TK"])
            A(lambda e: e.activation(EG[:], EG[:], AF.Exp), ["TK"], ["TK"])
            V(lambda e: e.tensor_reduce(SS[:, 0:8], EG[:].rearrange("p (h k) -> p h k", k=16), AX.X, ALU.add), ["TK"], ["SS"])
            V(lambda e: e.reciprocal(SS[:, 0:8], SS[:, 0:8]), ["SS"], ["SS"])
            V(lambda e: e.tensor_tensor(GATE[:].rearrange("p (h k) -> p h k", k=16), EG[:].rearrange("p (h k) -> p h k", k=16),
                                        SS[:, 0:8].unsqueeze(2).to_broadcast([128, 8, 16]), ALU.mult), ["TK", "SS"], ["GATE"])
            PE(lambda e: e.transpose(P2[:, 0:128], E1[:], ID[:]), ["TK", "ID"], ["P2"])
            V(lambda e: e.tensor_copy(IDXTI[:], P2[:, 0:128]), ["P2"], ["IDXTI"])
            PE(lambda e: e.transpose(P3[:, 0:128], GATE[:], ID[:]), ["GATE", "ID"], ["P3"])
            A(lambda e: e.copy(GATET[:], P3[:, 0:128]), ["P3"], ["GATET"])
            for t in range(128):
                b = t % 2
                S.dma("pool", lambda e, t=t, b=b: e.indirect_dma_start(out=UG[b][:], out_offset=None, in_=eu,
                                                                       in_offset=bass.IndirectOffsetOnAxis(ap=IDXTI[:, t:t + 1], axis=0)),
                      reads=["IDXTI"], writes=["UG%d" % b])
                pk = "P0a" if b == 0 else "P0b"
                for hf in range(2):
                    PE(lambda e, t=t, b=b, hf=hf: e.matmul(P0[:, b * 1024 + hf * 512:b * 1024 + (hf + 1) * 512], ID[:, t:t + 1].to_broadcast([128, 128]),
                                                           HTOK[:, hf * 512:(hf + 1) * 512], start=True, stop=True), ["ID", "HTOK"], [pk])
                    A(lambda e, b=b, hf=hf: e.copy(HB[b][:, hf * 512:(hf + 1) * 512], P0[:, b * 1024 + hf * 512:b * 1024 + (hf + 1) * 512]), [pk], ["HB%d" % b])
                V(lambda e, t=t, b=b: e.scalar_tensor_tensor(UG[b][:], UG[b][:], 1.0, HB[b][:], ALU.mult, ALU.mult, accum_out=ACTT[:, t:t + 1]),
                  ["HB%d" % b], ["UG%d" % b, "ACTT"])
            A(lambda e: e.activation(WT[:], ACTT[:], AF.Gelu), ["ACTT"], ["WT"])
            V(lambda e: e.tensor_tensor(WT[:], WT[:], GATET[:], ALU.mult), ["GATET"], ["WT"])
            for t in range(128):
                b = t % 2
                S.dma("pool", lambda e, t=t, b=b: e.indirect_dma_start(out=VG[b][:], out_offset=None, in_=ev,
                                                                       in_offset=bass.IndirectOffsetOnAxis(ap=IDXTI[:, t:t + 1], axis=0)),
                      reads=["IDXTI"], writes=["UG%d" % b])
                for ot in range(8):
                    PE(lambda e, t=t, b=b, ot=ot: e.matmul(P1[:, ot * 128 + t:ot * 128 + t + 1], VG[b][:, ot * 128:(ot + 1) * 128], WT[:, t:t + 1],
                                                           start=True, stop=True), ["UG%d" % b, "WT"], ["P1"])
            for ot in range(8):
                V(lambda e, ot=ot, col=col: e.scalar_tensor_tensor(XO[:, ot, :], P1[:, ot * 128:(ot + 1) * 128], MOD3[:, 40 + ot, col:col + 1], X1[:, ot, :],
                                                                   ALU.mult, ALU.add), ["P1", "MOD", "X1"], ["XT"])
            if last:
                for k in range(8):
                    A(lambda e, k=k: e.activation(TMPB[:], XO[:, k, :], AF.Square), ["XT"], ["TMPB"])
                    PE(lambda e, k=k: e.matmul(P2[:, 0:128], ONES[:], TMPB[:], start=(k == 0), stop=(k == 7)), ["ONES", "TMPB"], ["P2"])
                V(lambda e: e.tensor_scalar(RSTD[:], P2[:, 0:128], 1.0 / D, EPS, ALU.mult, ALU.add), ["P2"], ["RSTD"])
                A(lambda e: e.sqrt(RSTD[:], RSTD[:]), ["RSTD"], ["RSTD"])
                V(lambda e: e.reciprocal(RSTD[:], RSTD[:]), ["RSTD"], ["RSTD"])
                for k in range(8):
                    V(lambda e, k=k: e.scalar_tensor_tensor(XO[:, k, :], XO[:, k, :], GF[:, k:k + 1], RSTD[:], ALU.mult, ALU.mult), ["RSTD", "GF"], ["XT"])
            S.dma("sp", lambda e, tk=tk: e.dma_start(out=xo_v[:, :, tk], in_=XO[:]), reads=["XT"], writes=["xo_%d" % bi])
        S.drain_all("sp")
        S.emit()
    return nc


SEQC = 256
SEQL = 4096
OWN = 2176


def _mirror(t0, T):
    if t0 < SEQC:
        return 0, SEQC
    i = (t0 - SEQC) // 512
    return SEQC + SEQL - 512 * (i + 1), 512


def emit_A(nc, S, sb, ps, io, tl_all=False):
    xT, cT, w_mod, b_mod, g1, w_in, ones = io["xT"], io["cT"], io["w_mod"], io["b_mod"], io["g1"], io["w_in"], io["ones"]
    FMU, FMQ, FMK, FMZ, VT, TL, MODS = io["FMU"], io["FMQ"], io["FMK"], io["FMZ"], io["VT"], io["TL"], io["MODS"]
    CT = sb("CT", [128, 16]); SC = sb("SC", [128, 16]); BM = sb("BM", [128, 48]); G1 = sb("G1", [128, 8])
    ONES = sb("ONES", [128, 128]); MOD = sb("MOD", [128, 96])
    A1 = sb("A1", [128, 16]); TMPA = sb("TMPA", [128, 16])
    WM = [sb("WM%d" % i, [128, 8, 512]) for i in range(2)]
    WIN = sb("WIN", [128, 8, INW])
    XT = [sb("XT%d" % i, [128, 8, 512]) for i in range(2)]
    XSQ = sb("XSQ", [128, 512]); RSTD = sb("RSTD", [128, 512]); TMP = sb("TMP", [128, 512])
    HT = [sb("HT%d" % i, [128, 8, 512]) for i in range(2)]
    OUTB = [sb("OUTB%d" % i, [128, 512]) for i in range(4)]
    pmod = ps("pmod", [128, 96]); pss = ps("pss", [128, 512])
    pout = [ps("pout%d" % i, [128, 512]) for i in range(3)]
    S.dma("sp", lambda e: e.dma_start(out=CT[:], in_=cT), writes=["CT"])
    S.dma("sp", lambda e: e.dma_start(out=BM[:], in_=b_mod), writes=["BM"])
    S.dma("sp", lambda e: e.dma_start(out=G1[:], in_=g1), writes=["G1"])
    S.dma("sp", lambda e: e.dma_start(out=ONES[:], in_=ones), writes=["ONES"])
    S.op("act", lambda e: e.activation(SC[:], CT[:], AF.Silu), reads=["CT"], writes=["SC"])
    SC3 = SC[:].rearrange("p (k c) -> p k c", c=2)
    wm_v = w_mod.rearrange("(k p) f -> p k f", p=128)
    for jg in range(12):
        b = jg % 2
        S.dma("act" if jg % 2 else "sp",
              lambda e, jg=jg, b=b: e.dma_start(out=WM[b][:], in_=wm_v[:, :, jg * 512:(jg + 1) * 512]), writes=["WM%d" % b])
        for j8 in range(4):
            j = jg * 4 + j8
            for k in range(8):
                S.op("pe", lambda e, j=j, j8=j8, k=k, b=b: e.matmul(
                    pmod[:, 2 * j:2 * j + 2], WM[b][:, k, j8 * 128:(j8 + 1) * 128], SC3[:, k, :],
                    start=(k == 0), stop=(k == 7)), reads=["WM%d" % b, "SC"], writes=["pmod"])
    S.op("dve", lambda e: e.tensor_tensor(MOD[:].rearrange("p (j c) -> p j c", c=2), pmod[:].rearrange("p (j c) -> p j c", c=2),
                                          BM[:].unsqueeze(2).to_broadcast([128, 48, 2]), ALU.add), reads=["pmod", "BM"], writes=["MOD"])
    S.dma("sp", lambda e: e.dma_start(out=MODS, in_=MOD[:]), reads=["MOD"], writes=["MODS"])
    MOD3 = MOD[:].rearrange("p (j c) -> p j c", c=2)
    S.op("dve", lambda e: e.tensor_scalar(TMPA[:].rearrange("p (j c) -> p j c", c=2), MOD3[:, 8:16, :], 1.0, None, ALU.add), reads=["MOD"], writes=["TMPA"])
    S.op("dve", lambda e: e.tensor_tensor(A1[:].rearrange("p (j c) -> p j c", c=2), TMPA[:].rearrange("p (j c) -> p j c", c=2),
                                          G1[:].unsqueeze(2).to_broadcast([128, 8, 2]), ALU.mult), reads=["TMPA", "G1"], writes=["A1"])
    A13 = A1[:].rearrange("p (j c) -> p j c", c=2)
    win_v = w_in.rearrange("(k p) f -> p k f", p=128)
    for k in range(8):
        S.dma("pool", lambda e, k=k: e.dma_start(out=WIN[:, k, :], in_=win_v[:, k, :]), writes=["WIN%d" % k])
    WK = ["WIN%d" % k for k in range(8)]
    xT_v = xT.rearrange("(k p) t -> p k t", p=128)
    grp = [(0, SEQC, 1, -1)] + [(SEQC + 512 * g, 512, 0, g) for g in range(8)]
    cnt = {"o": 0}

    def evac(dst_ap_fn, pb, m, tn, key, cm=False):
        ob = cnt["o"] % 4
        cnt["o"] += 1
        if cm:
            o_ap = lambda: OUTB[ob][:m, :512].rearrange("p (w r) -> p w r", r=8)
            i_ap = lambda: pout[pb][:m, :512].rearrange("p (r w) -> p w r", w=64)
        else:
            o_ap = lambda: OUTB[ob][:m, :tn]
            i_ap = lambda: pout[pb][:m, :tn]
        if cnt["o"] % 2:
            S.op("act", lambda e: e.copy(o_ap(), i_ap()), reads=["pout%d" % pb], writes=["OUTB%d" % ob])
        else:
            S.op("dve", lambda e: e.tensor_copy(o_ap(), i_ap()), reads=["pout%d" % pb], writes=["OUTB%d" % ob])
        S.dma("sp" if cnt["o"] % 2 else "act", lambda e: dst_ap_fn(e, OUTB[ob]), reads=["OUTB%d" % ob], writes=["%s_%d" % (key, cnt["o"])])

    pi = 0
    for gi, (t0, tn, col, g) in enumerate(grp):
        b = gi % 2
        xk, hk = "XT%d" % b, "HT%d" % b
        S.dma("sp", lambda e, b=b, t0=t0, tn=tn: e.dma_start(out=XT[b][:, :, :tn], in_=xT_v[:, :, t0:t0 + tn]), writes=[xk])
        for k in range(8):
            S.op("act", lambda e, b=b, k=k, tn=tn: e.activation(XSQ[:, :tn], XT[b][:, k, :tn], AF.Square), reads=[xk], writes=["XSQ"])
            S.op("pe", lambda e, k=k, tn=tn: e.matmul(pss[:, :tn], ONES[:], XSQ[:, :tn], start=(k == 0), stop=(k == 7)), reads=["ONES", "XSQ"], writes=["pss"])
        S.op("dve", lambda e, tn=tn: e.tensor_scalar(RSTD[:, :tn], pss[:, :tn], 1.0 / D, EPS, ALU.mult, ALU.add), reads=["pss"], writes=["RSTD"])
        S.op("act", lambda e, tn=tn: e.sqrt(RSTD[:, :tn], RSTD[:, :tn]), reads=["RSTD"], writes=["RSTD"])
        S.op("dve", lambda e, tn=tn: e.reciprocal(RSTD[:, :tn], RSTD[:, :tn]), reads=["RSTD"], writes=["RSTD"])
        for k in range(8):
            S.op("dve", lambda e, b=b, k=k, tn=tn: e.tensor_tensor(TMP[:, :tn], XT[b][:, k, :tn], RSTD[:, :tn], ALU.mult), reads=[xk, "RSTD"], writes=["TMP"])
            S.op("act", lambda e, b=b, k=k, tn=tn, col=col: e.activation(HT[b][:, k, :tn], TMP[:, :tn], AF.Identity, bias=MOD3[:, k, col:col + 1],
                                                                         scale=A13[:, k, col:col + 1]), reads=["TMP", "MOD", "A1"], writes=[hk])
        fm = [(FMU, 0, 512, 128), (FMU, 128, 640, 128), (FMQ, 0, 768, 128), (FMQ, 128, 896, 128), (FMK, 0, 1024, 128), (FMK, 128, 1152, 128), (FMZ, 0, 2304, 32)]
        for (dst, r0, c0, m) in fm:
            pb = pi % 3
            pi += 1
            for k in range(8):
                S.op("pe", lambda e, b=b, k=k, tn=tn, c0=c0, m=m, pb=pb: e.matmul(pout[pb][:m, :tn], WIN[:, k, c0:c0 + m], HT[b][:, k, :tn], start=(k == 0), stop=(k == 7)),
                     reads=WK + [hk], writes=["pout%d" % pb])
            if dst is FMU or g < 0:
                evac(lambda e, ob, dst=dst, r0=r0, m=m, t0=t0, tn=tn: e.dma_start(out=dst[r0:r0 + m, t0:t0 + tn], in_=ob[:m, :tn]), pb, m, tn, "fm")
            else:
                evac(lambda e, ob, dst=dst, r0=r0, m=m, g=g: e.dma_start(
                    out=dst[r0:r0 + m, SEQC:].rearrange("p (w r) -> p w r", r=64)[:, :, 8 * g:8 * g + 8],
                    in_=ob[:m, :512].rearrange("p (w r) -> p w r", r=8)), pb, m, 512, "fm", cm=True)
        for ti in range(tn // 128):
            tl = [(VT, t0 + ti * 128, 0, 1280)]
            own_row = None
            if tl_all:
                own_row = t0 + ti * 128
            elif g < 0 and ti == 0:
                own_row = 0
            elif 0 <= g < 4:
                own_row = 128 + g * 512 + ti * 128
            if own_row is not None:
                tl += [(TL, own_row, 0, 0), (TL, own_row, 512, 1792)]
            for (dst, row, dc, c0) in tl:
                pb = pi % 3
                pi += 1
                for k in range(8):
                    S.op("pe", lambda e, b=b, k=k, ti=ti, c0=c0, pb=pb: e.matmul(pout[pb][:, :], HT[b][:, k, ti * 128:(ti + 1) * 128], WIN[:, k, c0:c0 + 512],
                                                                               start=(k == 0), stop=(k == 7)), reads=WK + [hk], writes=["pout%d" % pb])
                evac(lambda e, ob, dst=dst, row=row, dc=dc: e.dma_start(out=dst[row:row + 128, dc:dc + 512], in_=ob[:, :]), pb, 128, 512, "tm")


def emit_B(nc, S, sb, ps, io):
    FMU, YFs, YBs = io["FMU"], io["YF"], io["YB"]
    tau, ident = io["tau"], io["ident"]
    for ct in range(2):
        prm, bre, bim, cre, cim = io["prm"][ct], io["bre"][ct], io["bim"][ct], io["cre"][ct], io["cim"][ct]
        uin = [FMU, FMU]
        yout = [YFs, YBs]
        X = "c%d_" % ct
        sub = ExitStack()
        sb, ps = _mk(nc, sub)
        TWO_PI = 2.0 * math.pi
        PRM = sb(X + "PRM", [128, 24]); BRE = sb(X + "BRE", [128, 128]); BIM = sb(X + "BIM", [128, 128])
        CRE = sb(X + "CRE", [128, 128]); CIM = sb(X + "CIM", [128, 128]); TAU = sb(X + "TAU", [128, 512]); ID = sb(X + "ID", [128, 128])
        names = ["DT", "LR", "MAG", "TH", "R", "R2", "RF", "FR", "SIN", "COS", "ARE", "AIM", "DEN", "AM1", "FRE", "FIM", "T0", "T1"]
        P = {n: sb(X + "p_" + n, [128, 8]) for n in names}
        RI = sb(X + "p_RI", [128, 8], I32)
        BBR = sb(X + "BBR", [128, 128]); BBI = sb(X + "BBI", [128, 128]); TB = sb(X + "TB", [128, 128])
        PAD = sb(X + "PAD", [128, 128])
        WBR = sb(X + "WBR", [128, 8, 128]); WBI = sb(X + "WBI", [128, 8, 128]); CR = sb(X + "CR", [128, 8, 128]); CIN = sb(X + "CIN", [128, 8, 128])
        TC = sb(X + "TC", [128, 8, 512]); TS = sb(X + "TS", [128, 8, 512]); RHO = sb(X + "RHO", [128, 8, 512])
        RR = sb(X + "RR", [128, 512]); RRF = sb(X + "RRF", [128, 512]); RRI = sb(X + "RRI", [128, 512], I32)
        UC = [sb(X + "UC%d" % i, [128, 512]) for i in range(2)]
        W = {}
        for n in ("BR", "BI", "T1", "T2", "T3", "T4", "XR", "XI", "QR", "QI", "HR", "HI"):
            for i in range(2):
                W[n, i] = sb(X + "w_%s%d" % (n, i), [128, 512])
        HP = sb(X + "HP", [128, 8])
        YO = [sb(X + "YO%d" % i, [128, 512]) for i in range(2)]
        pbr = [ps(X + "pbr%d" % i, [128, 512]) for i in range(2)]
        pbi = [ps(X + "pbi%d" % i, [128, 512]) for i in range(2)]
        py = [ps(X + "py%d" % i, [128, 512]) for i in range(2)]
        ptr = ps(X + "ptr", [128, 128])

        for (t, src, k) in ((PRM, prm, "PRM"), (BRE, bre, "BRE"), (BIM, bim, "BIM"), (CRE, cre, "CRE"), (CIM, cim, "CIM"),
                            (TAU, tau, "TAU"), (ID, ident, "ID")):
            S.dma("sp", lambda e, t=t, src=src: e.dma_start(out=t[:], in_=src), writes=[k])
        PR3 = PRM[:].rearrange("p (a c) -> p a c", c=3)
        K = ["PP"]

        def V(fn, reads=(), writes=()):
            S.op("dve", fn, reads=list(reads) + K, writes=list(writes) + K)

        def A(fn, reads=(), writes=()):
            S.op("act", fn, reads=list(reads) + K, writes=list(writes) + K)

        A(lambda e: e.activation(P["DT"][:], PR3[:, :, 2], AF.Exp), reads=["PRM"])
        V(lambda e: e.tensor_scalar(P["LR"][:], PR3[:, :, 0], -1e-4, None, ALU.min), reads=["PRM"])
        V(lambda e: e.tensor_tensor(P["T0"][:], P["LR"][:], P["DT"][:], ALU.mult))
        A(lambda e: e.activation(P["MAG"][:], P["T0"][:], AF.Exp))
        V(lambda e: e.tensor_tensor(P["TH"][:], PR3[:, :, 1], P["DT"][:], ALU.mult), reads=["PRM"])
        V(lambda e: e.tensor_scalar(P["R"][:], P["TH"][:], 1.0 / TWO_PI, None, ALU.mult))
        V(lambda e: e.tensor_scalar(P["R2"][:], P["R"][:], 0.25, None, ALU.add))
        for (src, dst) in (("R", "SIN"), ("R2", "COS")):
            V(lambda e, src=src: e.tensor_copy(RI[:], P[src][:]))
            V(lambda e: e.tensor_copy(P["RF"][:], RI[:]))
            V(lambda e, src=src: e.tensor_tensor(P["FR"][:], P[src][:], P["RF"][:], ALU.subtract))
            A(lambda e, dst=dst: e.activation(P[dst][:], P["FR"][:], AF.Sin, scale=TWO_PI))
        V(lambda e: e.tensor_tensor(P["ARE"][:], P["MAG"][:], P["COS"][:], ALU.mult))
        V(lambda e: e.tensor_tensor(P["AIM"][:], P["MAG"][:], P["SIN"][:], ALU.mult))
        V(lambda e: e.tensor_tensor(P["T0"][:], P["LR"][:], P["LR"][:], ALU.mult))
        V(lambda e: e.tensor_tensor(P["T1"][:], PR3[:, :, 1], PR3[:, :, 1], ALU.mult), reads=["PRM"])
        V(lambda e: e.tensor_tensor(P["DEN"][:], P["T0"][:], P["T1"][:], ALU.add))
        V(lambda e: e.reciprocal(P["DEN"][:], P["DEN"][:]))
        V(lambda e: e.tensor_scalar(P["AM1"][:], P["ARE"][:], -1.0, None, ALU.add))
        V(lambda e: e.tensor_tensor(P["T0"][:], P["AM1"][:], P["LR"][:], ALU.mult))
        V(lambda e: e.tensor_tensor(P["T1"][:], P["AIM"][:], PR3[:, :, 1], ALU.mult), reads=["PRM"])
        V(lambda e: e.tensor_tensor(P["T0"][:], P["T0"][:], P["T1"][:], ALU.add))
        V(lambda e: e.tensor_tensor(P["FRE"][:], P["T0"][:], P["DEN"][:], ALU.mult))
        V(lambda e: e.tensor_tensor(P["T0"][:], P["AIM"][:], P["LR"][:], ALU.mult))
        V(lambda e: e.tensor_tensor(P["T1"][:], P["AM1"][:], PR3[:, :, 1], ALU.mult), reads=["PRM"])
        V(lambda e: e.tensor_tensor(P["T0"][:], P["T0"][:], P["T1"][:], ALU.subtract))
        V(lambda e: e.tensor_tensor(P["FIM"][:], P["T0"][:], P["DEN"][:], ALU.mult))

        def v3(t):
            return t[:].rearrange("p (a h) -> p a h", h=16)

        def bc(n):
            return P[n][:].unsqueeze(2).to_broadcast([128, 8, 16])
        V(lambda e: e.tensor_tensor(v3(BBR), v3(BRE), bc("FRE"), ALU.mult), reads=["BRE"])
        V(lambda e: e.tensor_tensor(v3(TB), v3(BIM), bc("FIM"), ALU.mult), reads=["BIM"])
        V(lambda e: e.tensor_tensor(BBR[:], BBR[:], TB[:], ALU.subtract))
        V(lambda e: e.tensor_tensor(v3(BBI), v3(BIM), bc("FRE"), ALU.mult), reads=["BIM"])
        V(lambda e: e.tensor_tensor(v3(TB), v3(BRE), bc("FIM"), ALU.mult), reads=["BRE"])
        V(lambda e: e.tensor_tensor(BBI[:], BBI[:], TB[:], ALU.add))
        V(lambda e: e.tensor_scalar(CIM[:], CIM[:], -1.0, None, ALU.mult), reads=["CIM"], writes=["CIM"])
        V(lambda e: e.memset(CR[:], 0.0)); V(lambda e: e.memset(CIN[:], 0.0))
        for dj in range(8):
            j = dj % 4
            for (src, dst) in ((BBR, WBR), (BBI, WBI)):
                V(lambda e: e.memset(PAD[:], 0.0), writes=["PAD"])
                V(lambda e, src=src, dj=dj, j=j: e.tensor_copy(PAD[0:64, 32 * j:32 * j + 16], src[0:64, dj * 16:dj * 16 + 16]), writes=["PAD"])
                V(lambda e, src=src, dj=dj, j=j: e.tensor_copy(PAD[64:128, 32 * j + 16:32 * j + 32], src[64:128, dj * 16:dj * 16 + 16]), writes=["PAD"])
                S.op("pe", lambda e: e.transpose(ptr[:], PAD[:], ID[:]), reads=["PAD", "ID"], writes=["ptr"])
                S.op("act", lambda e, dst=dst, dj=dj: e.copy(dst[:, dj, :], ptr[:]), reads=["ptr"], writes=["WB"])
            for (src, dst) in ((CRE, CR), (CIM, CIN)):
                V(lambda e, src=src, dst=dst, dj=dj, j=j: e.tensor_copy(dst[0:64, dj, 32 * j:32 * j + 16], src[0:64, dj * 16:dj * 16 + 16]), reads=["CRE", "CIM"], writes=["CC"])
                V(lambda e, src=src, dst=dst, dj=dj, j=j: e.tensor_copy(dst[64:128, dj, 32 * j + 16:32 * j + 32], src[64:128, dj * 16:dj * 16 + 16]), reads=["CRE", "CIM"], writes=["CC"])
            for (off, dst) in ((0.0, TS), (0.25, TC)):
                V(lambda e, dj=dj, off=off: e.tensor_scalar(RR[:], TAU[:], P["R"][:, dj:dj + 1], off, ALU.mult, ALU.add), reads=["TAU"], writes=["RR"])
                V(lambda e: e.tensor_copy(RRI[:], RR[:]), reads=["RR"], writes=["RRI"])
                V(lambda e: e.tensor_copy(RRF[:], RRI[:]), reads=["RRI"], writes=["RRF"])
                V(lambda e: e.tensor_tensor(RRF[:], RR[:], RRF[:], ALU.subtract), reads=["RR"], writes=["RRF"])
                S.op("act", lambda e, dst=dst, dj=dj: e.activation(dst[:, dj, :], RRF[:], AF.Sin, scale=TWO_PI), reads=["RRF"], writes=["TAB"])
            V(lambda e, dj=dj: e.tensor_copy(RHO[:, dj, :], P["MAG"][:, dj:dj + 1].to_broadcast([128, 512])), writes=["TAB"])

        G = "pool"
        oi = 0
        for d in range(2):
            V(lambda e: e.memset(HP[:], 0.0), writes=["HP"])
            for ci, (t0, T) in enumerate(S5_CH):
                ub_ = (d * 9 + ci) % 2
                uk = "UC%d" % ub_
                n0 = t0 if d == 0 else _mirror(t0, T)[0]
                S.dma("sp", lambda e, d=d, n0=n0, T=T, ub_=ub_, ct=ct: e.dma_start(out=UC[ub_][:, :T], in_=uin[d][ct * 128:(ct + 1) * 128, n0:n0 + T]), writes=[uk])
                ucv = (lambda ub_=ub_, T=T: UC[ub_][:, :T]) if d == 0 else (lambda ub_=ub_, T=T: UC[ub_][:, :T][:, ::-1])
                yb_ = (d * 9 + ci) % 2
                for j in range(4):
                    dj = d * 4 + j
                    b = j % 2
                    w = lambda n, b=b, T=T: W[n, b][:, :T]
                    k = lambda n, b=b: "w_%s%d" % (n, b)
                    S.op("pe", lambda e, dj=dj, b=b, T=T, ucv=ucv: e.matmul(pbr[b][:, :T], WBR[:, dj, :], ucv(), start=True, stop=True),
                         reads=["WB", uk], writes=["pbr%d" % b])
                    S.op("pe", lambda e, dj=dj, b=b, T=T, ucv=ucv: e.matmul(pbi[b][:, :T], WBI[:, dj, :], ucv(), start=True, stop=True),
                         reads=["WB", uk], writes=["pbi%d" % b])
                    S.op("act", lambda e, w=w, b=b, T=T: e.copy(w("BR"), pbr[b][:, :T]), reads=["pbr%d" % b], writes=[k("BR")])
                    S.op("act", lambda e, w=w, b=b, T=T: e.copy(w("BI"), pbi[b][:, :T]), reads=["pbi%d" % b], writes=[k("BI")])
                    cs = lambda dj=dj, T=T: TC[:, dj, :T]
                    sn = lambda dj=dj, T=T: TS[:, dj, :T]
                    S.op("dve", lambda e, w=w, cs=cs: e.tensor_tensor(w("T1"), cs(), w("BR"), ALU.mult), reads=["TAB", k("BR")], writes=[k("T1")])
                    S.op("dve", lambda e, w=w, sn=sn: e.tensor_tensor(w("T2"), sn(), w("BI"), ALU.mult), reads=["TAB", k("BI")], writes=[k("T2")])
                    S.op("dve", lambda e, w=w: e.tensor_tensor(w("XR"), w("T1"), w("T2"), ALU.add), reads=[k("T1"), k("T2")], writes=[k("XR")])
                    S.op(G, lambda e, w=w, cs=cs: e.tensor_tensor(w("T3"), cs(), w("BI"), ALU.mult), reads=["TAB", k("BI")], writes=[k("T3")])
                    S.op(G, lambda e, w=w, sn=sn: e.tensor_tensor(w("T4"), sn(), w("BR"), ALU.mult), reads=["TAB", k("BR")], writes=[k("T4")])
                    S.op(G, lambda e, w=w: e.tensor_tensor(w("XI"), w("T3"), w("T4"), ALU.subtract), reads=[k("T3"), k("T4")], writes=[k("XI")])
                    S.op("dve", lambda e, w=w, dj=dj, j=j, T=T: e.tensor_tensor_scan(w("QR"), RHO[:, dj, :T], w("XR"), HP[:, 2 * j:2 * j + 1], ALU.mult, ALU.add),
                         reads=["TAB", k("XR"), "HP"], writes=[k("QR")])
                    S.op("dve", lambda e, w=w, dj=dj, j=j, T=T: e.tensor_tensor_scan(w("QI"), RHO[:, dj, :T], w("XI"), HP[:, 2 * j + 1:2 * j + 2], ALU.mult, ALU.add),
                         reads=["TAB", k("XI"), "HP"], writes=[k("QI")])
                    S.op("dve", lambda e, w=w, cs=cs: e.tensor_tensor(w("T1"), cs(), w("QR"), ALU.mult), reads=["TAB", k("QR")], writes=[k("T1")])
                    S.op("dve", lambda e, w=w, sn=sn: e.tensor_tensor(w("T2"), sn(), w("QI"), ALU.mult), reads=["TAB", k("QI")], writes=[k("T2")])
                    S.op("dve", lambda e, w=w: e.tensor_tensor(w("HR"), w("T1"), w("T2"), ALU.subtract), reads=[k("T1"), k("T2")], writes=[k("HR")])
                    S.op(G, lambda e, w=w, sn=sn: e.tensor_tensor(w("T3"), sn(), w("QR"), ALU.mult), reads=["TAB", k("QR")], writes=[k("T3")])
                    S.op(G, lambda e, w=w, cs=cs: e.tensor_tensor(w("T4"), cs(), w("QI"), ALU.mult), reads=["TAB", k("QI")], writes=[k("T4")])
                    S.op(G, lambda e, w=w: e.tensor_tensor(w("HI"), w("T3"), w("T4"), ALU.add), reads=[k("T3"), k("T4")], writes=[k("HI")])
                    S.op("act", lambda e, b=b, j=j, T=T: e.copy(HP[:, 2 * j:2 * j + 1], W["HR", b][:, T - 1:T]), reads=[k("HR")], writes=["HP"])
                    S.op("act", lambda e, b=b, j=j, T=T: e.copy(HP[:, 2 * j + 1:2 * j + 2], W["HI", b][:, T - 1:T]), reads=[k("HI")], writes=["HP"])
                    S.op("pe", lambda e, dj=dj, w=w, j=j, yb_=yb_, T=T: e.matmul(py[yb_][:, :T], CR[:, dj, :], w("HR"), start=(j == 0), stop=False),
                         reads=["CC", k("HR")], writes=["py%d" % yb_])
                    S.op("pe", lambda e, dj=dj, w=w, j=j, yb_=yb_, T=T: e.matmul(py[yb_][:, :T], CIN[:, dj, :], w("HI"), start=False, stop=(j == 3)),
                         reads=["CC", k("HI")], writes=["py%d" % yb_])
                if d == 0:
                    S.op("act", lambda e, yb_=yb_, T=T: e.copy(YO[yb_][:, :T], py[yb_][:, :T]), reads=["py%d" % yb_], writes=["YO%d" % yb_])
                else:
                    S.op("act", lambda e, yb_=yb_, T=T: e.copy(YO[yb_][:, :T][:, ::-1], py[yb_][:, :T]), reads=["py%d" % yb_], writes=["YO%d" % yb_])
                oi += 1
                S.dma("act", lambda e, d=d, yb_=yb_, n0=n0, T=T, ct=ct: e.dma_start(out=yout[d][ct * 128:(ct + 1) * 128, n0:n0 + T], in_=YO[yb_][:, :T]),
                      reads=["YO%d" % yb_], writes=["yout_%d" % oi])
        S.sync_all()
        S.emit()
        sub.close()


def emit_C(nc, S, sb_unused, ps_unused, io):
    FMQ, FMK, FMZ, VT = io["FMQ"], io["FMK"], io["FMZ"], io["VT"]
    OS = [io["OF"], io["OB"]]
    rst, tmask, tmask2, blk, hmask, ident = io["rst"], io["tmask"], io["tmask2"], io["blk"], io["hmask"], io["ident"]
    QSC = 32 ** -0.5
    for hh in range(2):
        X = "h%d_" % hh
        sub = ExitStack()
        sb, ps = _mk(nc, sub)
        cols = slice(hh * 256, hh * 256 + 256)
        TM2 = sb(X + "TM2", [64, 256])
        S.dma("sp", lambda e: e.dma_start(out=TM2[:], in_=tmask2), writes=["TM2"])
        RST = sb(X + "RST", [128, 512]); TM = sb(X + "TM", [64, 256]); BLK = sb(X + "BLK", [128, 256]); HM = sb(X + "HM", [128, 4]); ID = sb(X + "ID", [128, 128])
        WG = sb(X + "WG", [16, 128]); BG = sb(X + "BG", [128, 1]); NBG = sb(X + "NBG", [128, 1])
        Wb = {}
        for n in ("Q", "K", "LA", "B", "E", "D", "QE", "QS", "KD", "KS0", "KS1", "KS2", "KS3"):
            for i in range(2):
                Wb[n, i] = sb(X + "g_%s%d" % (n, i), [128, 512])
        Z = [sb(X + "Z%d" % i, [16, 512]) for i in range(2)]
        VV = [sb(X + "VV%d" % i, [64, 8, 256]) for i in range(2)]
        DEC = [sb(X + "DEC%d" % i, [128, 8]) for i in range(2)]
        KDT = [sb(X + "KDT%d" % i, [64, 128]) for i in range(2)]
        STt = [sb(X + "ST%d" % i, [64, 256]) for i in range(2)]
        OB = [sb(X + "OB%d" % i, [64, 256]) for i in range(3)]
        KVM = sb(X + "KVM", [128, 256])
        SS = [sb(X + "SS%d" % i, [128, 256]) for i in range(2)]
        pza = ps(X + "pza", [128, 512])
        pt0 = ps(X + "pt0", [64, 128])
        pt = [pt0, pt0]
        pkv = [ps(X + "pkv%d" % i, [128, 256]) for i in range(2)]
        psc = [ps(X + "psc%d" % i, [64, 256]) for i in range(2)]
        po = [ps(X + "po%d" % i, [64, 256]) for i in range(2)]
        for (t, src, k) in ((RST, rst, "RST"), (TM, tmask, "TM"), (BLK, blk, "BLK"), (HM, hmask, "HM"), (ID, ident, "ID")):
            S.dma("sp", lambda e, t=t, src=src: e.dma_start(out=t[:], in_=src), writes=[k])
        gc = 0
        oi = 0
        for d in range(2):
            S.dma("sp", lambda e, d=d, hh=hh: e.dma_start(out=WG[:], in_=io["wg"][hh][d]), writes=["WG"])
            S.dma("sp", lambda e, d=d, hh=hh: e.dma_start(out=BG[:], in_=io["bg"][hh][d]), writes=["BG"])
            S.op("dve", lambda e: e.tensor_scalar(NBG[:], BG[:], -1.0, None, ALU.mult), reads=["BG"], writes=["NBG"])
            S.op("dve", lambda e: e.memset(SS[0][:], 0.0), writes=["SS0"])
            scur = 0
            for bi, (t0, T) in enumerate(S5_CH):
                nchk = T // 64
                n0 = t0 if d == 0 else _mirror(t0, T)[0]
                w0 = (n0 - SEQC) // 64
                b = (d * 9 + bi) % 2
                w = lambda n, b=b, T=T: Wb[n, b][:, :T]
                k = lambda n, b=b: "g_%s%d" % (n, b)
                w3 = lambda n, b=b, T=T: Wb[n, b][:, :T].rearrange("p (c s) -> p c s", s=64)
                S.dma("sp", lambda e, d=d, b=b, n0=n0, T=T, hh=hh: e.dma_start(out=Wb["Q", b][:, :T], in_=FMQ[hh * 128:(hh + 1) * 128, n0:n0 + T]), writes=[k("Q")])
                S.dma("act", lambda e, d=d, b=b, n0=n0, T=T, hh=hh: e.dma_start(out=Wb["K", b][:, :T], in_=FMK[hh * 128:(hh + 1) * 128, n0:n0 + T]), writes=[k("K")])
                S.dma("sp", lambda e, d=d, b=b, n0=n0, T=T: e.dma_start(out=Z[b][:, :T], in_=FMZ[d * 16:(d + 1) * 16, n0:n0 + T]), writes=["Z%d" % b])
                if t0 < SEQC:
                    S.dma("act", lambda e, b=b, nchk=nchk, cols=cols: e.dma_start(
                        out=VV[b][:, :nchk, :], in_=VT[0:SEQC, cols].rearrange("(c s) f -> s c f", s=64)), writes=["VV%d" % b])
                else:
                    S.dma("act", lambda e, b=b, w0=w0, cols=cols: e.dma_start(
                        out=VV[b][:, :, :], in_=VT[SEQC:, cols].rearrange("(r w) f -> r w f", w=64)[:, w0:w0 + 8, :]), writes=["VV%d" % b])
                S.op("pe", lambda e, b=b, T=T: e.matmul(pza[:, :T], WG[:], Z[b][:, :T], start=True, stop=True), reads=["WG", "Z%d" % b], writes=["pza"])
                S.op("act", lambda e, w=w, T=T: e.activation(w("E"), pza[:, :T], AF.Exp, bias=NBG[:], scale=-1.0), reads=["pza", "NBG"], writes=[k("E")])
                S.op("act", lambda e, w=w: e.activation(w("E"), w("E"), AF.Ln, bias=1.0), reads=[k("E")], writes=[k("E")])
                S.op("dve", lambda e, w=w: e.tensor_scalar(w("LA"), w("E"), -1.0 / 16.0, None, ALU.mult), reads=[k("E")], writes=[k("LA")])
                S.op("dve", lambda e, w=w, T=T: e.tensor_tensor_scan(w("B"), RST[:, :T], w("LA"), 0.0, ALU.mult, ALU.add), reads=["RST", k("LA")], writes=[k("B")])
                iref, ilast = (32, 63) if d == 0 else (31, 0)
                if d == 1:
                    S.op("dve", lambda e, w3=w3, nchk=nchk: e.tensor_tensor(w3("D"), w3("B")[:, :, 63:64].to_broadcast([128, nchk, 64]), w3("B"), ALU.subtract),
                         reads=[k("B")], writes=[k("D")])
                    S.op("dve", lambda e, w=w: e.tensor_tensor(w("B"), w("D"), w("LA"), ALU.add), reads=[k("D"), k("LA")], writes=[k("B")])
                S.op("act", lambda e, b=b, w3=w3, nchk=nchk, ilast=ilast: e.activation(DEC[b][:, :nchk], w3("B")[:, :, ilast], AF.Exp), reads=[k("B")], writes=["DEC%d" % b])
                S.op("act", lambda e, w=w: e.activation(w("E"), w("B"), AF.Exp), reads=[k("B")], writes=[k("E")])
                S.op("dve", lambda e, w=w: e.scalar_tensor_tensor(w("QE"), w("Q"), QSC, w("E"), ALU.mult, ALU.mult), reads=[k("Q"), k("E")], writes=[k("QE")])
                S.op("dve", lambda e, w3=w3, nchk=nchk, iref=iref: e.tensor_tensor(w3("D"), w3("B"), w3("B")[:, :, iref:iref + 1].to_broadcast([128, nchk, 64]), ALU.subtract),
                     reads=[k("B")], writes=[k("D")])
                S.op("act", lambda e, w=w: e.activation(w("E"), w("D"), AF.Exp), reads=[k("D"), k("QE")], writes=[k("E")])
                S.op("dve", lambda e, w=w: e.scalar_tensor_tensor(w("QS"), w("Q"), QSC, w("E"), ALU.mult, ALU.mult), reads=[k("Q"), k("E")], writes=[k("QS")])
                S.op("act", lambda e, w=w: e.activation(w("E"), w("D"), AF.Exp, scale=-1.0), reads=[k("D"), k("QS")], writes=[k("E")])
                S.op("dve", lambda e, w=w: e.tensor_tensor(w("LA"), w("K"), w("E"), ALU.mult), reads=[k("K"), k("E"), k("B")], writes=[k("LA")])
                for h in range(4):
                    S.op("pool", lambda e, w=w, h=h: e.tensor_scalar(w("KS%d" % h), w("LA"), HM[:, h:h + 1], None, ALU.mult),
                         reads=[k("LA"), "HM"], writes=[k("KS%d" % h)])
                S.op("dve", lambda e, w3=w3, nchk=nchk, ilast=ilast: e.tensor_tensor(w3("D"), w3("B")[:, :, ilast:ilast + 1].to_broadcast([128, nchk, 64]), w3("B"), ALU.subtract),
                     reads=[k("B"), k("E"), k("LA")], writes=[k("D")])
                S.op("act", lambda e, w=w: e.activation(w("D"), w("D"), AF.Exp), reads=[k("D")], writes=[k("D")])
                S.op("dve", lambda e, w=w: e.tensor_tensor(w("KD"), w("K"), w("D"), ALU.mult), reads=[k("K"), k("D")], writes=[k("KD")])
                for c in (range(nchk) if d == 0 else range(nchk - 1, -1, -1)):
                    p2 = gc % 2
                    gc += 1
                    cs = slice(c * 64, (c + 1) * 64)
                    S.op("pe", lambda e, b=b, cs=cs, p2=p2: e.transpose(pt[p2][:], Wb["KD", b][:, cs], ID[:]), reads=[k("KD"), "ID"], writes=["pt"])
                    S.op("act", lambda e, p2=p2: e.copy(KDT[p2][:], pt[p2][:]), reads=["pt"], writes=["KDT%d" % p2])
                    S.op("pe", lambda e, b=b, c=c, p2=p2: e.matmul(pkv[p2][:], KDT[p2][:], VV[b][:, c, :], start=True, stop=True),
                         reads=["KDT%d" % p2, "VV%d" % b], writes=["pkv%d" % p2])
                    for h in range(4):
                        S.op("pe", lambda e, b=b, cs=cs, p2=p2, h=h: e.matmul(psc[p2][:, h * 64:(h + 1) * 64], Wb["KS%d" % h, b][:, cs], Wb["QS", b][:, cs],
                                                                            start=True, stop=True),
                             reads=[k("KS%d" % h), k("QS")], writes=["psc%d" % p2])
                    S.op("dve", lambda e, p2=p2, d=d: e.tensor_tensor(STt[p2][:], psc[p2][:], (TM if d == 0 else TM2)[:], ALU.mult), reads=["psc%d" % p2, "TM", "TM2"], writes=["ST%d" % p2])
                    for h in range(4):
                        hs = slice(h * 64, (h + 1) * 64)
                        S.op("pe", lambda e, b=b, c=c, p2=p2, hs=hs: e.matmul(po[p2][:, hs], STt[p2][:, hs], VV[b][:, c, hs], start=True, stop=False),
                             reads=["ST%d" % p2, "VV%d" % b], writes=["po%d" % p2])
                        S.op("pe", lambda e, b=b, cs=cs, p2=p2, hs=hs, scur=scur: e.matmul(po[p2][:, hs], Wb["QE", b][:, cs], SS[scur][:, hs], start=False, stop=True),
                             reads=[k("QE"), "SS%d" % scur], writes=["po%d" % p2])
                    ob = oi % 3
                    oi += 1
                    S.op("act", lambda e, ob=ob, p2=p2: e.copy(OB[ob][:], po[p2][:]), reads=["po%d" % p2], writes=["OB%d" % ob])
                    if t0 < SEQC:
                        S.dma("sp" if oi % 2 else "act", lambda e, d=d, ob=ob, c=c, cols=cols: e.dma_start(out=OS[d][c * 64:(c + 1) * 64, cols], in_=OB[ob][:]),
                              reads=["OB%d" % ob], writes=["o_%d" % oi])
                    else:
                        S.dma("sp" if oi % 2 else "act", lambda e, d=d, ob=ob, c=c, w0=w0, cols=cols: e.dma_start(
                            out=OS[d][SEQC:, cols].rearrange("(r w) f -> r w f", w=64)[:, w0 + c, :], in_=OB[ob][:]),
                              reads=["OB%d" % ob], writes=["o_%d" % oi])
                    S.op("dve", lambda e, p2=p2: e.tensor_tensor(KVM[:], pkv[p2][:], BLK[:], ALU.mult), reads=["pkv%d" % p2, "BLK"], writes=["KVM"])
                    S.op("dve", lambda e, b=b, c=c, scur=scur: e.scalar_tensor_tensor(SS[1 - scur][:], SS[scur][:], DEC[b][:, c:c + 1], KVM[:], ALU.mult, ALU.add),
                         reads=["SS%d" % scur, "DEC%d" % b, "KVM"], writes=["SS%d" % (1 - scur)])
                    scur = 1 - scur
        S.sync_all()
        S.emit()
        sub.close()


def emit_D(nc, S, sb, ps, io, blocks, last):
    xT = io["xT"]; modT = io["MODS"]; TLs = io["TL"]
    su = TLs[:, 0:256]; sv = TLs[:, 256:512]; gg = TLs[:, 512:1024]
    s5u = io["FMU"]; yf = io["YF"]; yb = io["YB"]; of_ = io["OF"]; ob_ = io["OB"]
    w_out, wsT, sgub, s5d, wglu, ng, g2, wq = io["w_out"], io["wsT"], io["sgub"], io["s5d"], io["wglu"], io["ng"], io["g2"], io["wq"]
    keysT, eu, ev, gfin, ones, ident, iota16 = io["keysT"], io["eu"], io["ev"], io["gfin"], io["ones"], io["ident"], io["iota16"]
    xo = io["xo"]
    nb = len(blocks)
    MOD = sb("MOD", [128, 96]); WOUT = sb("WOUT", [128, 8, D]); WS = sb("WS", [128, 512]); SGUB = sb("SGUB", [128, 4]); S5D = sb("S5D", [128, 2])
    WGLU = sb("WGLU", [128, 2, 512]); NG = sb("NG", [128, 512]); G2 = sb("G2", [128, 8]); WQt = sb("WQt", [128, 8, 1024]); GB = sb("GB", [128, 8 * 1024]); KEYS = sb("KEYS", [128, 2048])
    GF = sb("GF", [128, 8]); ONES = sb("ONES", [128, 128]); ID = sb("ID", [128, 128]); IOTA = sb("IOTA", [128, 16]); IO128 = sb("IO128", [128, 128])
    A2 = sb("A2", [128, 16]); TA = sb("TA", [128, 16])
    U_ = sb("U_", [128, 256]); V_ = sb("V_", [128, 256]); GU = U_; GV = V_; SQ = sb("SQ", [128, 512])
    SS = sb("SS", [128, 8]); VN = sb("VN", [128, 256]); MIX = None
    S5U = sb("S5U", [128, 2, 128]); YF = sb("YF", [128, 2, 128]); YB = sb("YB", [128, 2, 128]); GE = S5U; SG = sb("SG", [128, 256])
    OF = sb("OF", [128, 512]); OB = sb("OB", [128, 512]); GG = sb("GG", [128, 512]); SL = GG
    XT = sb("XT", [128, 8, 128]); X1S_ = [sb("X1_%d" % i, [128, 8, 128]) for i in range(2)]; HT = sb("HT", [128, 8, 128]); MIXT = HT; HTOKS = [sb("HTOK%d" % i, [128, D]) for i in range(2)]
    RSTD = sb("RSTD", [128, 128]); TMPB = sb("TMPB", [128, 128]); RSTD2 = sb("RSTD2", [128, 128]); TMPB2 = sb("TMPB2", [128, 128])
    SC = sb("SC", [128, 2048]); MIX = SC[:, 0:1024]
    M16 = sb("M16", [128, 256]); I16 = sb("I16", [128, 256], U32); IF16 = sb("IF16", [128, 256]); I1S = sb("I1S", [128, 128])
    CS = sb("CS", [128, 2048]); SC2 = CS; QT = CS[:].rearrange("p (q t) -> p q t", t=128); CS2 = sb("CS2", [128, 256])
    T16 = sb("T16", [128, 128]); P16 = sb("P16", [128, 128], U32); PF = sb("PF", [128, 128]); AI = sb("AI", [128, 128], I32)
    AFL = sb("AFL", [128, 128]); BFL = sb("BFL", [128, 128]); E1 = sb("E1", [128, 128]); E2 = sb("E2", [128, 128])
    EG = sb("EG", [128, 128]); GATES = [sb("GATE%d" % i, [128, 128]) for i in range(2)]; IDXS = [sb("IDXTI%d" % i, [128, 128], I32) for i in range(2)]
    ACTT = sb("ACTT", [128, 128]); WT = sb("WT", [128, 128])
    NBUF = 8
    WQ = WQt
    UG = [GB[:, i * 1024:(i + 1) * 1024] for i in range(NBUF)]; VG = UG
    HB = [sb("HB%d" % i, [128, D]) for i in range(2)]
    P0 = ps("P0", [128, 2048]); P1 = ps("P1", [128, 1024]); P2 = ps("P2", [128, 512]); P3 = ps("P3", [128, 512])

    def V(fn, r=(), w=()):
        S.op("dve", fn, reads=r, writes=w)

    def A(fn, r=(), w=()):
        S.op("act", fn, reads=r, writes=w)

    def PE(fn, r=(), w=()):
        S.op("pe", fn, reads=r, writes=w)

    def LD(q, t, src, key):
        S.dma(q, lambda e: e.dma_start(out=t, in_=src), writes=(key if isinstance(key, list) else [key]))

    LD("sp", MOD[:], modT, "MOD"); LD("sp", WS[:], wsT, "WS"); LD("sp", SGUB[:], sgub, "SGUB"); LD("sp", S5D[:], s5d, "S5D")
    LD("sp", WGLU[:], wglu.rearrange("(c p) f -> p c f", p=128), "WGLU"); LD("sp", NG[:], ng, "NG"); LD("sp", G2[:], g2, "G2")
    LD("sp", KEYS[:], keysT, "KEYS"); LD("sp", GF[:], gfin, "GF"); LD("sp", ONES[:], ones, "ONES"); LD("sp", ID[:], ident, "ID"); LD("sp", IOTA[:], iota16, "IOTA"); LD("sp", IO128[:], io["iota128"], "IO128")
    wo_v = w_out.rearrange("(k p) f -> p k f", p=128)
    wq_v = wq.rearrange("(k p) f -> p k f", p=128)
    for k in range(8):
        LD("act", WOUT[:, k, :], wo_v[:, k, :], "WOUT")
    MOD3 = MOD[:].rearrange("p (j c) -> p j c", c=2)
    V(lambda e: e.tensor_scalar(TA[:].rearrange("p (j c) -> p j c", c=2), MOD3[:, 32:40, :], 1.0, None, ALU.add), ["MOD"], ["TA"])
    V(lambda e: e.tensor_tensor(A2[:].rearrange("p (j c) -> p j c", c=2), TA[:].rearrange("p (j c) -> p j c", c=2),
                                G2[:].unsqueeze(2).to_broadcast([128, 8, 2]), ALU.mult), ["TA", "G2"], ["A2"])
    A23 = A2[:].rearrange("p (j c) -> p j c", c=2)

    def rs_from_ss(ss, n, scale):
        V(lambda e: e.tensor_scalar(ss, ss, scale, EPS, ALU.mult, ALU.add), ["SS"], ["SS"])
        A(lambda e: e.sqrt(ss, ss), ["SS"], ["SS"])
        V(lambda e: e.reciprocal(ss, ss), ["SS"], ["SS"])

    def top16(src, scratch, mout, iout, n):
        V(lambda e: e.max(mout[:, 0:8], src), ["TK"], ["TK"])
        V(lambda e: e.max_index(iout[:, 0:8], mout[:, 0:8], src), ["TK"], ["TK"])
        V(lambda e: e.match_replace(scratch, mout[:, 0:8], src, -1e30), ["TK"], ["TK"])
        V(lambda e: e.max(mout[:, 8:16], scratch), ["TK"], ["TK"])
        V(lambda e: e.max_index(iout[:, 8:16], mout[:, 8:16], scratch), ["TK"], ["TK"])

    xT_v = xT.rearrange("(k p) t -> p k t", p=128)
    xo_v = xo.rearrange("(k p) t -> p k t", p=128)
    s5u_v = s5u.rearrange("(c p) t -> p c t", p=128)
    yf_v = yf.rearrange("(c p) t -> p c t", p=128)
    yb_v = yb.rearrange("(c p) t -> p c t", p=128)
    def front(bi):
        pb_ = bi % 2
        HTOK = HTOKS[pb_]; X1 = X1S_[pb_]; IDXTI = IDXS[pb_]; GATE = GATES[pb_]
        KH = "HTOK%d" % pb_; KX = "X1_%d" % pb_; KI = "IDXTI%d" % pb_; KG = "GATE%d" % pb_
        sq0, r0_, oc0, isctx = blocks[bi]
        col = 1 if isctx else 0
        tk = slice(sq0, sq0 + 128)
        tr = slice(r0_, r0_ + 128)
        to = slice(oc0, oc0 + 128)
        XO = HB[1][:].rearrange("p (k t) -> p k t", t=128)
        yield
        LD("sp", U_[:], su[tr, :], "U_"); LD("sp", V_[:], sv[tr, :], "V_"); LD("sp", GG[:], gg[tr, :], "GG")
        yield
        LD("act", S5U[:], s5u_v[:, :, tk], "S5U"); LD("act", YF[:], yf_v[:, :, tk], "YF"); LD("act", YB[:], yb_v[:, :, tk], "YB")
        yield
        LD("sp", OF[:], of_[tk, :], "OF"); LD("sp", OB[:], ob_[tk, :], "OB"); LD("act", XT[:], xT_v[:, :, tk], "XT")
        yield
        A(lambda e: e.activation(GU[:], U_[:], AF.Gelu), ["U_"], ["U_"])
        yield
        A(lambda e: e.activation(GV[:], V_[:], AF.Gelu), ["V_"], ["V_"])
        yield
        V(lambda e: e.tensor_tensor(SQ[:, 0:256], GV[:], GV[:], ALU.mult), ["V_"], ["SQ"])
        yield
        V(lambda e: e.tensor_reduce(SS[:, 0:4], SQ[:, 0:256].rearrange("p (h d) -> p h d", d=64), AX.X, ALU.add), ["SQ"], ["SS"])
        yield
        rs_from_ss(SS[:, 0:4], 4, 1.0 / 64)
        yield
        V(lambda e: e.tensor_tensor(VN[:].rearrange("p (h d) -> p h d", d=64), GV[:].rearrange("p (h d) -> p h d", d=64),
                                    SS[:, 0:4].unsqueeze(2).to_broadcast([128, 4, 64]), ALU.mult), ["V_", "SS"], ["VN"])
        yield
        for h in range(4):
            PE(lambda e, h=h: e.matmul(P2[:, h * 64:(h + 1) * 64], WS[:, h * 128:(h + 1) * 128], VN[:, h * 64:(h + 1) * 64], start=True, stop=True),
               ["WS", "VN"], ["P2"])
            yield
        yield
        V(lambda e: e.tensor_tensor(MIX[:, 0:256].rearrange("p (h d) -> p h d", d=64), P2[:, 0:256].rearrange("p (h d) -> p h d", d=64),
                                    SGUB[:].unsqueeze(2).to_broadcast([128, 4, 64]), ALU.add), ["P2", "SGUB"], ["MIXa"])
        yield
        V(lambda e: e.tensor_tensor(MIX[:, 0:256], MIX[:, 0:256], GU[:], ALU.mult), ["U_"], ["MIXa"])
        yield
        V(lambda e: e.tensor_tensor(YF[:], YF[:], YB[:], ALU.add), ["YB"], ["YF"])
        yield
        for ct in range(2):
            V(lambda e, ct=ct: e.scalar_tensor_tensor(YF[:, ct, :], S5U[:, ct, :], S5D[:, ct:ct + 1], YF[:, ct, :], ALU.mult, ALU.add),
              ["S5U", "S5D"], ["YF"])
            yield
        yield
        A(lambda e: e.activation(GE[:], YF[:], AF.Gelu), ["YF"], ["S5U"])
        yield
        for ct in range(2):
            PE(lambda e, ct=ct: e.matmul(P3[:, 0:512], GE[:, ct, :], WGLU[:, ct, :], start=(ct == 0), stop=(ct == 1)), ["S5U", "WGLU"], ["P3"])
            yield
        yield
        A(lambda e: e.activation(SG[:], P3[:, 256:512], AF.Sigmoid), ["P3"], ["SG"])
        yield
        V(lambda e: e.tensor_tensor(MIX[:, 256:512], P3[:, 0:256], SG[:], ALU.mult), ["P3", "SG"], ["MIXb"])
        yield
        V(lambda e: e.tensor_tensor(OF[:], OF[:], OB[:], ALU.add), ["OB"], ["OF"])
        yield
        V(lambda e: e.tensor_tensor(SQ[:], OF[:], OF[:], ALU.mult), ["OF"], ["SQ"])
        yield
        V(lambda e: e.tensor_reduce(SS[:, 0:8], SQ[:].rearrange("p (h d) -> p h d", d=64), AX.X, ALU.add), ["SQ"], ["SS"])
        yield
        rs_from_ss(SS[:, 0:8], 8, 1.0 / 64)
        yield
        V(lambda e: e.tensor_tensor(OF[:].rearrange("p (h d) -> p h d", d=64), OF[:].rearrange("p (h d) -> p h d", d=64),
                                    SS[:, 0:8].unsqueeze(2).to_broadcast([128, 8, 64]), ALU.mult), ["SS"], ["OF"])
        yield
        V(lambda e: e.tensor_tensor(OF[:], OF[:], NG[:], ALU.mult), ["NG"], ["OF"])
        yield
        A(lambda e: e.activation(SL[:], GG[:], AF.Silu), ["GG"], ["GG"])
        yield
        V(lambda e: e.tensor_tensor(MIX[:, 512:1024], OF[:], SL[:], ALU.mult), ["OF", "GG"], ["MIXc"])
        yield
        for f in range(8):
            pp, pk = (P2, "P2") if f % 2 == 0 else (P3, "P3")
            PE(lambda e, f=f, pp=pp: e.transpose(pp[:, 0:128], MIX[:, f * 128:(f + 1) * 128], ID[:]), ["MIXa", "MIXb", "MIXc", "ID"], [pk])
            A(lambda e, f=f, pp=pp: e.copy(MIXT[:, f, :], pp[:, 0:128]), [pk], ["HT"])
            yield
        yield
        for ot in range(8):
            pp, pk = (P2, "P2") if ot % 2 == 0 else (P3, "P3")
            for k in range(8):
                PE(lambda e, ot=ot, k=k, pp=pp: e.matmul(pp[:, 0:128], WOUT[:, k, ot * 128:(ot + 1) * 128], MIXT[:, k, :], start=(k == 0), stop=(k == 7)),
                   ["WOUT", "HT"], [pk])
            V(lambda e, ot=ot, pp=pp, col=col: e.scalar_tensor_tensor(X1[:, ot, :], pp[:, 0:128], MOD3[:, 16 + ot, col:col + 1], XT[:, ot, :], ALU.mult, ALU.add),
              [pk, "MOD", "XT"], [KX])
            yield
        yield
        for k in range(8):
            A(lambda e, k=k: e.activation(TMPB[:], X1[:, k, :], AF.Square), [KX], ["TMPB"])
            PE(lambda e, k=k: e.matmul(P2[:, 0:128], ONES[:], TMPB[:], start=(k == 0), stop=(k == 7)), ["ONES", "TMPB"], ["P2"])
            yield
        yield
        V(lambda e: e.tensor_scalar(RSTD[:], P2[:, 0:128], 1.0 / D, EPS, ALU.mult, ALU.add), ["P2"], ["RSTD"])
        yield
        A(lambda e: e.sqrt(RSTD[:], RSTD[:]), ["RSTD"], ["RSTD"])
        yield
        V(lambda e: e.reciprocal(RSTD[:], RSTD[:]), ["RSTD"], ["RSTD"])
        yield
        for k in range(8):
            V(lambda e, k=k: e.tensor_tensor(TMPB[:], X1[:, k, :], RSTD[:], ALU.mult), [KX, "RSTD"], ["TMPB"])
            A(lambda e, k=k, col=col: e.activation(HT[:, k, :], TMPB[:], AF.Identity, bias=MOD3[:, 24 + k, col:col + 1], scale=A23[:, k, col:col + 1]),
              ["TMPB", "MOD", "A2"], ["HT"])
            yield
        yield
        for k in range(8):
            pp, pk = (P2, "P2") if k % 2 == 0 else (P3, "P3")
            PE(lambda e, k=k, pp=pp: e.transpose(pp[:, 0:128], HT[:, k, :], ID[:]), ["HT", "ID"], [pk])
            A(lambda e, k=k, pp=pp: e.copy(HTOK[:, k * 128:(k + 1) * 128], pp[:, 0:128]), [pk], [KH])
            yield
        yield
        for qt in range(16):
            pp, pk = (P2, "P2") if qt % 2 == 0 else (P3, "P3")
            if qt % 8 == 0:
                for k in range(8):
                    LD("act" if k % 2 else "sp", WQ[:, k, :], wq_v[:, k, (qt // 8) * 1024:(qt // 8 + 1) * 1024], "WQ")
            for k in range(8):
                PE(lambda e, qt=qt, k=k, pp=pp: e.matmul(pp[:, 0:128], WQ[:, k, (qt % 8) * 128:(qt % 8 + 1) * 128], HT[:, k, :], start=(k == 0), stop=(k == 7)),
                   ["WQ", "HT"], [pk])
            if qt % 2 == 0:
                A(lambda e, qt=qt, pp=pp: e.copy(QT[:, qt, :], pp[:, 0:128]), [pk], ["TK"])
            else:
                V(lambda e, qt=qt, pp=pp: e.tensor_copy(QT[:, qt, :], pp[:, 0:128]), [pk], ["TK"])
            yield
        yield
        for qt in range(16):
            PE(lambda e, qt=qt: e.matmul(P0[:, qt * 128:(qt + 1) * 128], QT[:, qt, :], KEYS[:, qt * 128:(qt + 1) * 128], start=True, stop=True),
               ["TK", "KEYS"], ["P0a", "P0b"])
            yield
        yield
        for q4 in range(4):
            A(lambda e, q4=q4: e.copy(SC[:, q4 * 512:(q4 + 1) * 512], P0[:, q4 * 512:(q4 + 1) * 512]), ["P0a", "P0b"], ["TK"])
            yield
        yield
        for qt in range(16):
            top16(SC[:, qt * 128:(qt + 1) * 128], SC2[:, qt * 128:(qt + 1) * 128], M16[:, qt * 16:(qt + 1) * 16], I16[:, qt * 16:(qt + 1) * 16], 128)
            yield
        yield
        V(lambda e: e.tensor_copy(IF16[:], I16[:]), ["TK"], ["TK"])
        yield
        M4 = M16[:].rearrange("p (h q k) -> p h q k", q=2, k=16)
        yield
        IF4 = IF16[:].rearrange("p (h q k) -> p h q k", q=2, k=16)
        yield
        I1S3 = I1S[:].rearrange("p (h k) -> p h k", k=16)
        yield
        V(lambda e: e.tensor_scalar(I1S3, IF4[:, :, 0, :], 128.0, None, ALU.mult), ["TK"], ["TK"])
        yield
        CS4 = CS[:].rearrange("p (h a b) -> p h a b", a=16, b=16)
        yield
        V(lambda e: e.tensor_tensor(CS4, M4[:, :, 0, :].unsqueeze(3).to_broadcast([128, 8, 16, 16]),
                                    M4[:, :, 1, :].unsqueeze(2).to_broadcast([128, 8, 16, 16]), ALU.add), ["TK"], ["TK"])
        yield
        for h in range(8):
            top16(CS[:, h * 256:(h + 1) * 256], CS2[:], T16[:, h * 16:(h + 1) * 16], P16[:, h * 16:(h + 1) * 16], 256)
            yield
        yield
        V(lambda e: e.tensor_copy(PF[:], P16[:]), ["TK"], ["TK"])
        yield
        V(lambda e: e.tensor_scalar(AFL[:], PF[:], -7.5, 1.0 / 16, ALU.add, ALU.mult), ["TK"], ["TK"])
        yield
        V(lambda e: e.tensor_copy(AI[:], AFL[:]), ["TK"], ["TK"])
        yield
        V(lambda e: e.tensor_copy(AFL[:], AI[:]), ["TK"], ["TK"])
        yield
        V(lambda e: e.scalar_tensor_tensor(BFL[:], AFL[:], -16.0, PF[:], ALU.mult, ALU.add), ["TK"], ["TK"])
        yield
        EQ4 = CS[:].rearrange("p (h k a) -> p h k a", k=16, a=16)
        yield
        io4 = IOTA[:].unsqueeze(1).unsqueeze(1).to_broadcast([128, 8, 16, 16])
        yield
        for (sel, src, dst) in ((AFL, I1S3, E1), (BFL, IF4[:, :, 1, :], E2)):
            V(lambda e, sel=sel: e.tensor_tensor(EQ4, io4, sel[:].rearrange("p (h k) -> p h k", k=16).unsqueeze(3).to_broadcast([128, 8, 16, 16]), ALU.is_equal),
              ["TK", "IOTA"], ["TK"])
            V(lambda e, src=src: e.tensor_tensor(EQ4, EQ4, src.unsqueeze(2).to_broadcast([128, 8, 16, 16]), ALU.mult), ["TK"], ["TK"])
            V(lambda e, dst=dst: e.tensor_reduce(dst[:], CS[:].rearrange("p (m a) -> p m a", a=16), AX.X, ALU.add), ["TK"], ["TK"])
            yield
        yield
        V(lambda e: e.tensor_tensor(E1[:], E1[:], E2[:], ALU.add), ["TK"], ["TK"])
        yield
        T3 = T16[:].rearrange("p (h k) -> p h k", k=16)
        yield
        V(lambda e: e.tensor_tensor(EG[:].rearrange("p (h k) -> p h k", k=16), T3, T3[:, :, 0:1].to_broadcast([128, 8, 16]), ALU.subtract), ["TK"], ["TK"])
        yield
        A(lambda e: e.activation(EG[:], EG[:], AF.Exp), ["TK"], ["TK"])
        yield
        V(lambda e: e.tensor_reduce(SS[:, 0:8], EG[:].rearrange("p (h k) -> p h k", k=16), AX.X, ALU.add), ["TK"], ["SS"])
        yield
        V(lambda e: e.reciprocal(SS[:, 0:8], SS[:, 0:8]), ["SS"], ["SS"])
        yield
        V(lambda e: e.tensor_tensor(GATE[:].rearrange("p (h k) -> p h k", k=16), EG[:].rearrange("p (h k) -> p h k", k=16),
                                    SS[:, 0:8].unsqueeze(2).to_broadcast([128, 8, 16]), ALU.mult), ["TK", "SS"], [KG])
        yield
        V(lambda e: e.tensor_copy(IDXTI[:], E1[:]), ["TK"], [KI])


    def loops(bi):
        pb_ = bi % 2
        HTOK = HTOKS[pb_]; X1 = X1S_[pb_]; IDXTI = IDXS[pb_]; GATE = GATES[pb_]
        KH = "HTOK%d" % pb_; KX = "X1_%d" % pb_; KI = "IDXTI%d" % pb_; KG = "GATE%d" % pb_
        sq0, r0_, oc0, isctx = blocks[bi]
        col = 1 if isctx else 0
        tk = slice(sq0, sq0 + 128)
        tr = slice(r0_, r0_ + 128)
        to = slice(oc0, oc0 + 128)
        XO = HB[1][:].rearrange("p (k t) -> p k t", t=128)
        yield
        for j in range(128):
            g = j % NBUF
            S.dma("pool", lambda e, j=j, g=g: e.indirect_dma_start(out=UG[g], out_offset=None, in_=eu,
                                                                   in_offset=bass.IndirectOffsetOnAxis(ap=IDXTI[:, j:j + 1], axis=0)),
                  reads=[KI], writes=["R%d" % g])
            V(lambda e, j=j, g=g: e.scalar_tensor_tensor(UG[g], UG[g], 1.0, HTOK[:], ALU.mult, ALU.mult, accum_out=ACTT[:, j:j + 1]),
              [KH], ["R%d" % g, "ACTT"])
            yield
        yield
        A(lambda e: e.activation(WT[:], ACTT[:], AF.Gelu), ["ACTT"], ["WT"])
        yield
        V(lambda e: e.tensor_tensor(WT[:], WT[:], GATE[:], ALU.mult), [KG], ["WT"])
        yield
        ACC = HB[0]
        yield
        for j in range(128):
            g = j % NBUF
            S.dma("pool", lambda e, j=j, g=g: e.indirect_dma_start(out=VG[g], out_offset=None, in_=ev,
                                                                   in_offset=bass.IndirectOffsetOnAxis(ap=IDXTI[:, j:j + 1], axis=0)),
                  reads=[KI], writes=["R%d" % g])
            if j == 0:
                V(lambda e, g=g: e.tensor_scalar(ACC[:], VG[g], WT[:, 0:1], None, ALU.mult), ["R%d" % g, "WT"], ["HB0"])
            else:
                V(lambda e, j=j, g=g: e.scalar_tensor_tensor(ACC[:], VG[g], WT[:, j:j + 1], ACC[:], ALU.mult, ALU.add), ["R%d" % g, "WT"], ["HB0"])
            yield
        yield
        for ot in range(8):
            pp, pk = (P1[:, 0:512], "P1a") if ot % 2 == 0 else (P1[:, 512:1024], "P1b")
            PE(lambda e, ot=ot, pp=pp: e.transpose(pp[:, 0:128], ACC[:, ot * 128:(ot + 1) * 128], ID[:]), ["HB0", "ID"], [pk])
            V(lambda e, ot=ot, col=col, pp=pp: e.scalar_tensor_tensor(XO[:, ot, :], pp[:, 0:128], MOD3[:, 40 + ot, col:col + 1], X1[:, ot, :],
                                                                      ALU.mult, ALU.add), [pk, "MOD", KX], ["HB1"])
            yield
        yield
        if last:
            for k in range(8):
                A(lambda e, k=k: e.activation(TMPB2[:], XO[:, k, :], AF.Square), ["HB1"], ["TMPB2"])
                PE(lambda e, k=k: e.matmul(P1[:, 0:128], ONES[:], TMPB2[:], start=(k == 0), stop=(k == 7)), ["ONES", "TMPB2"], ["P1a"])
            V(lambda e: e.tensor_scalar(RSTD2[:], P1[:, 0:128], 1.0 / D, EPS, ALU.mult, ALU.add), ["P1a"], ["RSTD2"])
            A(lambda e: e.sqrt(RSTD2[:], RSTD2[:]), ["RSTD2"], ["RSTD2"])
            V(lambda e: e.reciprocal(RSTD2[:], RSTD2[:]), ["RSTD2"], ["RSTD2"])
            for k in range(8):
                V(lambda e, k=k: e.scalar_tensor_tensor(XO[:, k, :], XO[:, k, :], GF[:, k:k + 1], RSTD2[:], ALU.mult, ALU.mult), ["RSTD2", "GF"], ["HB1"])
        yield
        S.dma("sp", lambda e, to=to: e.dma_start(out=xo_v[:, :, to], in_=XO[:]), reads=["HB1"], writes=["xo_%d" % bi])


    def run_all(g):
        for _ in g:
            pass
    run_all(front(0))
    for bi in range(nb):
        nxt = front(bi + 1) if bi + 1 < nb else None
        for step, _ in enumerate(loops(bi)):
            if nxt is not None:
                if next(nxt, "END") == "END":
                    nxt = None
        if nxt is not None:
            run_all(nxt)


def build_layer(last, dbg=False):
    nc = bass.Bass("TRN2", target_bir_lowering=False)
    io = {}

    def din(name, shape, dt=F32):
        io[name] = nc.dram_tensor(name, shape, dt, kind="ExternalInput").ap()

    def scr(name, shape):
        io[name] = nc.dram_tensor(name, shape, F32, kind=("ExternalOutput" if dbg else "Internal")).ap()
    nctx = 0 if last else 128
    ntok = 2048 + nctx
    din("xT", [D, SEQT]); din("cT", [128, 16]); din("w_mod", [D, NMOD * D]); din("b_mod", [128, 48]); din("g1", [128, 8]); din("w_in", [D, INW])
    din("ones", [128, 128]); din("ident", [128, 128]); din("tau", [128, 512]); din("iota16", [128, 16]); din("iota128", [128, 128])
    din("prm", [2, 128, 24]); din("bre", [2, 128, 128]); din("bim", [2, 128, 128]); din("cre", [2, 128, 128]); din("cim", [2, 128, 128])
    din("wg", [2, 2, 16, 128]); din("bg", [2, 2, 128, 1])
    din("rst", [128, 512]); din("tmask", [64, 256]); din("tmask2", [64, 256]); din("blk", [128, 256]); din("hmask", [128, 4])
    din("w_out", [D, D]); din("wsT", [128, 512]); din("sgub", [128, 4]); din("s5d", [128, 2]); din("wglu", [256, 512]); din("ng", [128, 512])
    din("g2", [128, 8]); din("wq", [D, 2048]); din("keysT", [128, 2048]); din("eu", [NEXP, D]); din("ev", [NEXP, D]); din("gfin", [128, 8])
    scr("FMU", [256, SEQT]); scr("FMQ", [256, SEQT]); scr("FMK", [256, SEQT]); scr("FMZ", [32, SEQT]); scr("VT", [SEQT, 512]); scr("TL", [OWN, 1024])
    scr("MODS", [128, 96]); scr("YF", [256, SEQT]); scr("YB", [256, SEQT]); scr("OF", [SEQT, 512]); scr("OB", [SEQT, 512]); scr("GS", [2, 16])
    io["xo"] = nc.dram_tensor("xo", [D, ntok], F32, kind="ExternalOutput").ap()
    with ExitStack() as top:
        gate = top.enter_context(nc.semaphore("gate"))
        ph = [0]

        def phase(fn):
            with ExitStack() as st:
                S = Sched(nc, top, gate, 16 * ph[0])
                sb, ps = _mk(nc, st)
                fn(S, sb, ps)
                S.drain_all("sp")
                GS = io["GS"]
                S.prog["sp"].append(("i", lambda e: e.dma_start(out=GS[0:1, :], in_=GS[1:2, :]), "gate", 16))
                S.emit()
            ph[0] += 1
        phase(lambda S, sb, ps: emit_A(nc, S, sb, ps, io))
        phase(lambda S, sb, ps: emit_B(nc, S, sb, ps, io))
        phase(lambda S, sb, ps: emit_C(nc, S, sb, ps, io))
        if last:
            blocks = [(SEQC + i * 128, 128 + i * 128, i * 128, False) for i in range(16)]
        else:
            blocks = [(0, 0, 0, True)] + [(SEQC + i * 128, 128 + i * 128, 128 + i * 128, False) for i in range(16)]
        phase(lambda S, sb, ps: emit_D(nc, S, sb, ps, io, blocks, last))
    return nc


def _lay_vec(v, n):
    return np.ascontiguousarray(np.asarray(v, np.float32).reshape(n, 128).T)


_CONST = {}


def _consts():
    if not _CONST:
        rst = np.ones((128, 512), np.float32)
        rst[:, ::64] = 0
        tm = (np.arange(64)[None, :] >= np.arange(64)[:, None]).astype(np.float32)
        _CONST.update(
            ones=np.ones((128, 128), np.float32), ident=np.eye(128, dtype=np.float32),
            iota16=np.ascontiguousarray(np.broadcast_to(np.arange(16, dtype=np.float32), (128, 16))),
            iota128=np.ascontiguousarray(np.broadcast_to(np.arange(128, dtype=np.float32), (128, 128))),
            tau=np.ascontiguousarray(np.broadcast_to(np.arange(1, 513, dtype=np.float32), (128, 512))),
            rst=rst, tmask=np.ascontiguousarray(np.tile(tm, (1, 4))), tmask2=np.ascontiguousarray(np.tile(tm.T, (1, 4))),
            blk=np.kron(np.eye(4, dtype=np.float32), np.ones((32, 64), np.float32)),
            hmask=np.kron(np.eye(4, dtype=np.float32), np.ones((32, 1), np.float32)))
    return _CONST


def _pb_params(P, l, gh, swap):
    def stt(a):
        if swap:
            a = a[::-1]
        g = a[:, gh * 8:gh * 8 + 8]
        g = g.reshape((2, 4, 2, 64) + a.shape[3:])
        g = np.moveaxis(g, (2, 3), (0, 1))
        return np.ascontiguousarray(g.reshape((128, 2, 4) + a.shape[3:]))
    lre = stt(P["s5_lambda_re"][l]); lim = stt(P["s5_lambda_im"][l])
    ls = stt(np.broadcast_to(P["s5_log_step"][l][:, :, None], (2, 16, 64)))
    prm = np.ascontiguousarray(np.stack([lre, lim, ls], -1).reshape(128, 24).astype(np.float32))
    return dict(prm=prm, bre=stt(P["s5_b_re"][l]).reshape(128, 128), bim=stt(P["s5_b_im"][l]).reshape(128, 128),
                cre=stt(np.swapaxes(P["s5_c_re"][l], 2, 3)).reshape(128, 128), cim=stt(np.swapaxes(P["s5_c_im"][l], 2, 3)).reshape(128, 128))


def _layer_weights(P, l, swap):
    C = _consts()
    sk = P["peer_sub_keys"][l]
    keysT = np.zeros((128, 16, 128), np.float32)
    for h in range(8):
        for p in range(2):
            keysT[:, h * 2 + p, :] = sk[p, h].T
    sw = P["sgu_w"][l]
    sbb = P["sgu_b"][l]
    wgate = P["gla_w_gate"][l]
    bgate = P["gla_b_gate"][l]
    if swap:
        sw = sw[:, ::-1, ::-1]
        sbb = sbb[:, ::-1]
        wgate = wgate[::-1]
        bgate = bgate[::-1]
    pb = [_pb_params(P, l, ct, swap) for ct in range(2)]
    w_in = P["w_in"][l]
    if swap:
        w_in = np.ascontiguousarray(np.concatenate([w_in[:, :2304], w_in[:, 2320:2336], w_in[:, 2304:2320]], 1))
    W = dict(w_mod=P["w_mod"][l], b_mod=_lay_vec(P["b_mod"][l], 48), g1=_lay_vec(P["norm1_g"][l], 8), w_in=w_in,
             w_out=P["w_out"][l], wsT=np.ascontiguousarray(np.transpose(sw, (2, 0, 1)).reshape(128, 512)),
             sgub=np.ascontiguousarray(sbb.T), s5d=_lay_vec(P["s5_d"][l], 2), wglu=P["s5_w_glu"][l],
             ng=np.ascontiguousarray(np.broadcast_to(P["gla_norm_g"][l], (128, 512))), g2=_lay_vec(P["norm2_g"][l], 8),
             wq=P["peer_w_query"][l], keysT=keysT.reshape(128, 2048), eu=P["peer_expert_u"][l], ev=P["peer_expert_v"][l],
             gfin=_lay_vec(P["final_norm_g"], 8),
             wg=np.ascontiguousarray(np.stack([[wgate[d][:, hh * 128:hh * 128 + 128] for d in range(2)] for hh in range(2)])),
             bg=np.ascontiguousarray(np.stack([[bgate[d][hh * 128:hh * 128 + 128][:, None] for d in range(2)] for hh in range(2)])))
    for k in ("prm", "bre", "bim", "cre", "cim"):
        W[k] = np.ascontiguousarray(np.stack([pb[0][k], pb[1][k]]))
    W.update(C)
    return W


LAYER_W = ["w_mod", "b_mod", "g1", "w_in", "prm", "bre", "bim", "cre", "cim", "wg", "bg", "w_out", "wsT", "sgub", "s5d", "wglu", "ng", "g2",
           "wq", "keysT", "eu", "ev"]
LAYER_W_SHAPES = dict(w_mod=[D, NMOD * D], b_mod=[128, 48], g1=[128, 8], w_in=[D, INW], prm=[2, 128, 24], bre=[2, 128, 128], bim=[2, 128, 128],
                      cre=[2, 128, 128], cim=[2, 128, 128], wg=[2, 2, 16, 128], bg=[2, 2, 128, 1], w_out=[D, D], wsT=[128, 512], sgub=[128, 4],
                      s5d=[128, 2], wglu=[256, 512], ng=[128, 512], g2=[128, 8], wq=[D, 2048], keysT=[128, 2048], eu=[NEXP, D], ev=[NEXP, D])


def build_full():
    nc = bass.Bass("TRN2", target_bir_lowering=False)
    io = {}

    def din(name, shape, dt=F32):
        io[name] = nc.dram_tensor(name, shape, dt, kind="ExternalInput").ap()

    def scr(name, shape):
        io[name] = nc.dram_tensor(name, shape, F32, kind="Internal").ap()
    din("xT", [D, SEQT]); din("cT", [128, 16]); din("gfin", [128, 8])
    din("ones", [128, 128]); din("ident", [128, 128]); din("tau", [128, 512]); din("iota16", [128, 16]); din("iota128", [128, 128])
    din("rst", [128, 512]); din("tmask", [64, 256]); din("tmask2", [64, 256]); din("blk", [128, 256]); din("hmask", [128, 4])
    for l in range(2):
        for n in LAYER_W:
            din("%s_%d" % (n, l), LAYER_W_SHAPES[n])
    scr("FMU", [256, SEQT]); scr("FMQ", [256, SEQT]); scr("FMK", [256, SEQT]); scr("FMZ", [32, SEQT]); scr("VT", [SEQT, 512]); scr("TL", [SEQT, 1024])
    scr("MODS", [128, 96]); scr("YF", [256, SEQT]); scr("YB", [256, SEQT]); scr("OF", [SEQT, 512]); scr("OB", [SEQT, 512]); scr("GS", [2, 16])
    scr("X1S", [D, SEQT])
    io["xo"] = nc.dram_tensor("xo", [D, 2048], F32, kind="ExternalOutput").ap()
    with ExitStack() as top:
        gate = top.enter_context(nc.semaphore("gate"))
        ph = [0]

        def phase(fn, nds=None):
            with ExitStack() as st:
                S = Sched(nc, top, gate, 16 * ph[0], nds=nds)
                sb, ps = _mk(nc, st)
                fn(S, sb, ps)
                S.drain_all("sp")
                GS = io["GS"]
                S.prog["sp"].append(("i", lambda e: e.dma_start(out=GS[0:1, :], in_=GS[1:2, :]), "gate", 16))
                S.emit()
            ph[0] += 1
        for l in range(2):
            last = l == 1
            iol = dict(io)
            for n in LAYER_W:
                iol[n] = io["%s_%d" % (n, l)]
            if last:
                iol["xT"] = io["X1S"]
                blocks = [(SEQC + i * 128, 128 + i * 128, i * 128, False) for i in range(16)]
            else:
                iol["xo"] = io["X1S"]
                blocks = [(0, 0, 0, True), (128, 128, 128, True)] + [(SEQC + i * 128, SEQC + i * 128, SEQC + i * 128, False) for i in range(32)]
            n2 = dict(sp=2, act=2, pool=2)
            phase(lambda S, sb, ps: emit_A(nc, S, sb, ps, iol, tl_all=not last), nds=n2)
            phase(lambda S, sb, ps: emit_B(nc, S, sb, ps, iol), nds=n2)
            phase(lambda S, sb, ps: emit_C(nc, S, sb, ps, iol), nds=n2)
            phase(lambda S, sb, ps: emit_D(nc, S, sb, ps, iol, blocks, last), nds=dict(sp=2, act=2, pool=7))
    return nc


_PROG = {}


def _prog(name, fn):
    if name not in _PROG:
        _PROG[name] = fn()
    return _PROG[name]


def kernel(**inputs):
    P = {k: np.ascontiguousarray(np.asarray(v)) for k, v in inputs.items()}
    cores = list(range(8))
    C = _consts()
    ncF = _prog("F", build_full)
    WL = {(l, sw): _layer_weights(P, l, sw) for l in range(2) for sw in (False, True)}
    in_maps = []
    for core in cores:
        b, half = divmod(core, 2)
        xc, xl = P["ctx"][b], P["x"][b]
        seq = np.concatenate([xc, xl], 0) if half == 0 else np.concatenate([xc[::-1], xl[::-1]], 0)
        cT = np.stack([_lay_vec(P["c"][b], 8), _lay_vec(P["c_ctx"], 8)], -1).reshape(128, 16)
        m = dict(xT=np.ascontiguousarray(seq.T), cT=np.ascontiguousarray(cT), gfin=_lay_vec(P["final_norm_g"], 8))
        m.update(C)
        for l in range(2):
            W = WL[l, half == 1]
            for n in LAYER_W:
                m["%s_%d" % (n, l)] = W[n]
        in_maps.append(m)
    rF = run_bass_kernel_spmd(ncF, in_maps, core_ids=cores).results
    out = np.zeros_like(P["x"])
    for core in cores:
        b, half = divmod(core, 2)
        xo = rF[core]["xo"].T
        if half == 0:
            out[b, 0:2048] = xo
        else:
            out[b, 2048:4096] = xo[::-1]
    return out.astype(np.float32)
```

```python
from contextlib import ExitStack
import math
import numpy as np
import concourse.bass as bass
import concourse.mybir as mybir
from concourse.bass_utils import run_bass_kernel_spmd

F32 = mybir.dt.float32
I32 = mybir.dt.int32
U32 = mybir.dt.uint32
ALU = mybir.AluOpType
AF = mybir.ActivationFunctionType
AX = mybir.AxisListType


class Sched:
    NDS = 4

    def __init__(self, nc, stack, gate=None, gate_val=0, nds=None):
        self.nc = nc
        self.ndsq = dict(sp=self.NDS, act=self.NDS, pool=self.NDS)
        if nds:
            self.ndsq.update(nds)
        self.gate = gate
        self.gate_val = gate_val
        self.eng = {"pe": nc.tensor, "dve": nc.vector, "act": nc.scalar,
                    "pool": nc.gpsimd, "sp": nc.sync}
        self.sem = {}
        self.cnt = {}
        _UID[0] += 1
        u = "q%d" % _UID[0]
        for e in self.eng:
            self.sem[e] = stack.enter_context(nc.semaphore(u + "s_" + e))
            self.cnt[e] = 0
        self.dq = {}
        for q in ("sp", "act", "pool"):
            sems = [stack.enter_context(nc.semaphore(u + "d_%s%d" % (q, i))) for i in range(self.ndsq[q])]
            self.dq[q] = {"sems": sems, "n": 0}
            for i, s in enumerate(sems):
                self.sem["d_%s%d" % (q, i)] = s
        self.seen = {e: {} for e in self.eng}
        self.prog = {e: [] for e in self.eng}
        self.lastw = {}
        self.reads = {}
        self.ninst = 0
        if gate is not None:
            self.sem["gate"] = gate
        if gate is not None and gate_val > 0:
            for e in self.eng:
                self.prog[e].append(("w", "gate", gate_val))

    def _need(self, need, ev):
        if ev is None:
            return
        s, v = ev
        if need.get(s, 0) < v:
            need[s] = v

    def _waits(self, e, reads, writes):
        need = {}
        for k in reads:
            self._need(need, self.lastw.get(k))
        for k in writes:
            self._need(need, self.lastw.get(k))
            for ev in self.reads.get(k, ()):
                self._need(need, ev)
        eng = self.eng[e]
        seen = self.seen[e]
        for s, v in need.items():
            if seen.get(s, 0) >= v:
                continue
            self.prog[e].append(("w", s, v))
            seen[s] = v
            self.ninst += 1

    def _commit(self, ev, reads, writes):
        for k in writes:
            self.lastw[k] = ev
            self.reads[k] = []
        for k in reads:
            if k in writes:
                continue
            self.reads.setdefault(k, []).append(ev)
            if len(self.reads[k]) > 24:
                d = {}
                for s, v in self.reads[k]:
                    if d.get(s, 0) < v:
                        d[s] = v
                self.reads[k] = list(d.items())

    def op(self, e, fn, reads=(), writes=()):
        reads = tuple(reads)
        writes = tuple(writes)
        self._waits(e, reads, writes)
        self.cnt[e] += 1
        self.prog[e].append(("i", fn, e, 1))
        self.ninst += 1
        self._commit((e, self.cnt[e]), reads, writes)

    def dma(self, q, fn, reads=(), writes=()):
        reads = tuple(reads)
        writes = tuple(writes)
        st = self.dq[q]
        i = st["n"]
        slot = i % self.ndsq[q]
        sname = "d_%s%d" % (q, slot)
        rnd = i // self.ndsq[q]
        eng = self.eng[q]
        if rnd > 0 and self.seen[q].get(sname, 0) < 16 * rnd:
            self.prog[q].append(("w", sname, 16 * rnd))
            self.seen[q][sname] = 16 * rnd
            self.ninst += 1
        self._waits(q, reads, writes)
        self.prog[q].append(("i", fn, sname, 16))
        st["n"] = i + 1
        self.ninst += 1
        self._commit((sname, 16 * (rnd + 1)), reads, writes)

    def finish(self, keys, e="sp"):
        self._waits(e, tuple(keys), ())

    def drain_all(self, e="sp"):
        need = {}
        for en, c in self.cnt.items():
            if c:
                need[en] = c
        for q, stq in self.dq.items():
            n = stq["n"]
            nq = self.ndsq[q]
            for slot in range(nq):
                uses = (n - slot + nq - 1) // nq if n > slot else 0
                if uses:
                    need["d_%s%d" % (q, slot)] = 16 * uses
        eng = self.eng[e]
        for s, v in need.items():
            if self.seen[e].get(s, 0) >= v:
                continue
            self.prog[e].append(("w", s, v))
            self.seen[e][s] = v

    def emit(self):
        nc = self.nc
        with nc.Block() as block:
            def mk(e):
                def body(engine):
                    for it in self.prog[e]:
                        if it[0] == "w":
                            engine.wait_ge(self.sem[it[1]], it[2])
                        else:
                            it[1](engine).then_inc(self.sem[it[2]], it[3])
                return body
            block.sync(mk("sp"))
            block.scalar(mk("act"))
            block.vector(mk("dve"))
            block.gpsimd(mk("pool"))
            block.tensor(mk("pe"))
        self.prog = {e: [] for e in self.eng}

    def sync_all(self):
        for e in self.eng:
            self.drain_all(e)
        self.lastw = {}
        self.reads = {}

D = 1024
NMOD = 6
INW = 2336
INW_T = 19
EPS = 1e-6
NTOK = 2176


_UID = [0]


def _mk(nc, st):
    _UID[0] += 1
    u = "u%d_" % _UID[0]

    def sb(name, shape, dt=F32):
        return st.enter_context(nc.sbuf_tensor(u + name, shape, dt))

    def ps(name, shape, dt=F32):
        return st.enter_context(nc.psum_tensor(u + name, shape, dt))
    return sb, ps


def _groups(n, g=512):
    out = []
    t = 0
    while t < n:
        out.append((t, min(g, n - t)))
        t += g
    return out


def build_PA(ntok=NTOK, nctx=128):
    nc = bass.Bass("TRN2", target_bir_lowering=False)
    xT = nc.dram_tensor("xT", [D, ntok], F32, kind="ExternalInput").ap()
    cT = nc.dram_tensor("cT", [128, 16], F32, kind="ExternalInput").ap()
    w_mod = nc.dram_tensor("w_mod", [D, NMOD * D], F32, kind="ExternalInput").ap()
    b_mod = nc.dram_tensor("b_mod", [128, 48], F32, kind="ExternalInput").ap()
    g1 = nc.dram_tensor("g1", [128, 8], F32, kind="ExternalInput").ap()
    w_in = nc.dram_tensor("w_in", [D, INW], F32, kind="ExternalInput").ap()
    ones = nc.dram_tensor("ones", [128, 128], F32, kind="ExternalInput").ap()
    colsT = nc.dram_tensor("colsT", [INW_T * 128, ntok], F32, kind="ExternalOutput").ap()
    modT = nc.dram_tensor("modT", [128, 96], F32, kind="ExternalOutput").ap()
    with ExitStack() as st:
        S = Sched(nc, st)
        sb, ps = _mk(nc, st)
        CT = sb("CT", [128, 16]); SC = sb("SC", [128, 16]); BM = sb("BM", [128, 48]); G1 = sb("G1", [128, 8])
        ONES = sb("ONES", [128, 128]); MOD = sb("MOD", [128, 96])
        A1 = sb("A1", [128, 16]); TMPA = sb("TMPA", [128, 16])
        WM = [sb("WM%d" % i, [128, 8, 512]) for i in range(2)]
        WIN = sb("WIN", [128, 8, INW])
        XT = [sb("XT%d" % i, [128, 8, 512]) for i in range(2)]
        XSQ = sb("XSQ", [128, 512]); RSTD = sb("RSTD", [128, 512]); TMP = sb("TMP", [128, 512])
        HT = [sb("HT%d" % i, [128, 8, 512]) for i in range(2)]
        OUTB = [sb("OUTB%d" % i, [128, 512]) for i in range(4)]
        pmod = ps("pmod", [128, 96]); pss = ps("pss", [128, 512])
        pout = [ps("pout%d" % i, [128, 512]) for i in range(3)]

        S.dma("sp", lambda e: e.dma_start(out=CT[:], in_=cT), writes=["CT"])
        S.dma("sp", lambda e: e.dma_start(out=BM[:], in_=b_mod), writes=["BM"])
        S.dma("sp", lambda e: e.dma_start(out=G1[:], in_=g1), writes=["G1"])
        S.dma("sp", lambda e: e.dma_start(out=ONES[:], in_=ones), writes=["ONES"])
        S.op("act", lambda e: e.activation(SC[:], CT[:], AF.Silu), reads=["CT"], writes=["SC"])
        SC3 = SC[:].rearrange("p (k c) -> p k c", c=2)
        wm_v = w_mod.rearrange("(k p) f -> p k f", p=128)
        for jg in range(12):
            b = jg % 2
            S.dma("act" if jg % 2 else "sp",
                  lambda e, jg=jg, b=b: e.dma_start(out=WM[b][:], in_=wm_v[:, :, jg * 512:(jg + 1) * 512]),
                  writes=["WM%d" % b])
            for j8 in range(4):
                j = jg * 4 + j8
                for k in range(8):
                    S.op("pe", lambda e, j=j, j8=j8, k=k, b=b: e.matmul(
                        pmod[:, 2 * j:2 * j + 2], WM[b][:, k, j8 * 128:(j8 + 1) * 128], SC3[:, k, :],
                        start=(k == 0), stop=(k == 7)), reads=["WM%d" % b, "SC"], writes=["pmod"])
        S.op("dve", lambda e: e.tensor_tensor(MOD[:].rearrange("p (j c) -> p j c", c=2),
                                              pmod[:].rearrange("p (j c) -> p j c", c=2),
                                              BM[:].unsqueeze(2).to_broadcast([128, 48, 2]), ALU.add),
             reads=["pmod", "BM"], writes=["MOD"])
        S.dma("sp", lambda e: e.dma_start(out=modT, in_=MOD[:]), reads=["MOD"], writes=["modT"])
        MOD3 = MOD[:].rearrange("p (j c) -> p j c", c=2)
        S.op("dve", lambda e: e.tensor_scalar(TMPA[:].rearrange("p (j c) -> p j c", c=2), MOD3[:, 8:16, :], 1.0, None, ALU.add),
             reads=["MOD"], writes=["TMPA"])
        S.op("dve", lambda e: e.tensor_tensor(A1[:].rearrange("p (j c) -> p j c", c=2),
                                              TMPA[:].rearrange("p (j c) -> p j c", c=2),
                                              G1[:].unsqueeze(2).to_broadcast([128, 8, 2]), ALU.mult),
             reads=["TMPA", "G1"], writes=["A1"])
        A13 = A1[:].rearrange("p (j c) -> p j c", c=2)
        win_v = w_in.rearrange("(k p) f -> p k f", p=128)
        for k in range(8):
            S.dma("pool", lambda e, k=k: e.dma_start(out=WIN[:, k, :], in_=win_v[:, k, :]), writes=["WIN%d" % k])
        xT_v = xT.rearrange("(k p) t -> p k t", p=128)
        grp = [(0, nctx, 1)] if nctx else []
        grp += [(nctx + t0, tn, 0) for (t0, tn) in _groups(ntok - nctx)]
        oi = 0
        for gi, (t0, tn, col) in enumerate(grp):
            b = gi % 2
            xk, hk = "XT%d" % b, "HT%d" % b
            S.dma("sp", lambda e, b=b, t0=t0, tn=tn: e.dma_start(out=XT[b][:, :, :tn], in_=xT_v[:, :, t0:t0 + tn]), writes=[xk])
            for k in range(8):
                S.op("act", lambda e, b=b, k=k, tn=tn: e.activation(XSQ[:, :tn], XT[b][:, k, :tn], AF.Square), reads=[xk], writes=["XSQ"])
                S.op("pe", lambda e, k=k, tn=tn: e.matmul(pss[:, :tn], ONES[:], XSQ[:, :tn], start=(k == 0), stop=(k == 7)),
                     reads=["ONES", "XSQ"], writes=["pss"])
            S.op("dve", lambda e, tn=tn: e.tensor_scalar(RSTD[:, :tn], pss[:, :tn], 1.0 / D, EPS, ALU.mult, ALU.add), reads=["pss"], writes=["RSTD"])
            S.op("act", lambda e, tn=tn: e.sqrt(RSTD[:, :tn], RSTD[:, :tn]), reads=["RSTD"], writes=["RSTD"])
            S.op("dve", lambda e, tn=tn: e.reciprocal(RSTD[:, :tn], RSTD[:, :tn]), reads=["RSTD"], writes=["RSTD"])
            for k in range(8):
                S.op("dve", lambda e, b=b, k=k, tn=tn: e.tensor_tensor(TMP[:, :tn], XT[b][:, k, :tn], RSTD[:, :tn], ALU.mult),
                     reads=[xk, "RSTD"], writes=["TMP"])
                S.op("act", lambda e, b=b, k=k, tn=tn, col=col: e.activation(
                    HT[b][:, k, :tn], TMP[:, :tn], AF.Identity, bias=MOD3[:, k, col:col + 1], scale=A13[:, k, col:col + 1]),
                    reads=["TMP", "MOD", "A1"], writes=[hk])
            for ot in range(INW_T):
                m = 128 if ot < INW_T - 1 else INW - 128 * (INW_T - 1)
                pb = oi % 3
                ob = oi % 4
                oi += 1
                for k in range(8):
                    S.op("pe", lambda e, b=b, k=k, tn=tn, ot=ot, m=m, pb=pb: e.matmul(
                        pout[pb][:m, :tn], WIN[:, k, ot * 128:ot * 128 + m], HT[b][:, k, :tn], start=(k == 0), stop=(k == 7)),
                        reads=["WIN%d" % k, hk], writes=["pout%d" % pb])
                if m < 128:
                    S.op("dve", lambda e, ob=ob: e.memset(OUTB[ob][:], 0.0), writes=["OUTB%d" % ob])
                eng = "act" if oi % 2 else "dve"
                if eng == "act":
                    S.op("act", lambda e, ob=ob, pb=pb, m=m, tn=tn: e.copy(OUTB[ob][:m, :tn], pout[pb][:m, :tn]),
                         reads=["pout%d" % pb], writes=["OUTB%d" % ob])
                else:
                    S.op("dve", lambda e, ob=ob, pb=pb, m=m, tn=tn: e.tensor_copy(OUTB[ob][:m, :tn], pout[pb][:m, :tn]),
                         reads=["pout%d" % pb], writes=["OUTB%d" % ob])
                S.dma("sp" if oi % 2 else "act", lambda e, ob=ob, ot=ot, t0=t0, tn=tn: e.dma_start(
                    out=colsT[ot * 128:(ot + 1) * 128, t0:t0 + tn], in_=OUTB[ob][:, :tn]),
                    reads=["OUTB%d" % ob], writes=["colsT_%d" % oi])
        S.drain_all("sp")
        S.emit()
    return nc


SEQT = 4352
S5_CH = [(0, 256)] + [(256 + 512 * i, 512) for i in range(8)]


def build_PB():
    nc = bass.Bass("TRN2", target_bir_lowering=False)
    uin = [nc.dram_tensor(n, [128, SEQT], F32, kind="ExternalInput").ap() for n in ("uf", "ub")]
    prm = nc.dram_tensor("prm", [128, 24], F32, kind="ExternalInput").ap()
    bre = nc.dram_tensor("bre", [128, 128], F32, kind="ExternalInput").ap()
    bim = nc.dram_tensor("bim", [128, 128], F32, kind="ExternalInput").ap()
    cre = nc.dram_tensor("cre", [128, 128], F32, kind="ExternalInput").ap()
    cim = nc.dram_tensor("cim", [128, 128], F32, kind="ExternalInput").ap()
    tau = nc.dram_tensor("tau", [128, 512], F32, kind="ExternalInput").ap()
    ident = nc.dram_tensor("ident", [128, 128], F32, kind="ExternalInput").ap()
    yout = [nc.dram_tensor(n, [128, SEQT], F32, kind="ExternalOutput").ap() for n in ("yf", "yb")]
    TWO_PI = 2.0 * math.pi
    with ExitStack() as st:
        S = Sched(nc, st)
        sb, ps = _mk(nc, st)
        PRM = sb("PRM", [128, 24]); BRE = sb("BRE", [128, 128]); BIM = sb("BIM", [128, 128])
        CRE = sb("CRE", [128, 128]); CIM = sb("CIM", [128, 128]); TAU = sb("TAU", [128, 512]); ID = sb("ID", [128, 128])
        names = ["DT", "LR", "MAG", "TH", "R", "R2", "RF", "FR", "SIN", "COS", "ARE", "AIM", "DEN", "AM1", "FRE", "FIM", "T0", "T1"]
        P = {n: sb("p_" + n, [128, 8]) for n in names}
        RI = sb("p_RI", [128, 8], I32)
        BBR = sb("BBR", [128, 128]); BBI = sb("BBI", [128, 128]); TB = sb("TB", [128, 128])
        PAD = sb("PAD", [128, 128])
        WBR = sb("WBR", [128, 8, 128]); WBI = sb("WBI", [128, 8, 128]); CR = sb("CR", [128, 8, 128]); CIN = sb("CIN", [128, 8, 128])
        TC = sb("TC", [128, 8, 512]); TS = sb("TS", [128, 8, 512]); RHO = sb("RHO", [128, 8, 512])
        RR = sb("RR", [128, 512]); RRF = sb("RRF", [128, 512]); RRI = sb("RRI", [128, 512], I32)
        UC = [sb("UC%d" % i, [128, 512]) for i in range(2)]
        W = {}
        for n in ("BR", "BI", "T1", "T2", "T3", "T4", "XR", "XI", "QR", "QI", "HR", "HI"):
            for i in range(2):
                W[n, i] = sb("w_%s%d" % (n, i), [128, 512])
        HP = sb("HP", [128, 8])
        YO = [sb("YO%d" % i, [128, 512]) for i in range(2)]
        pbr = [ps("pbr%d" % i, [128, 512]) for i in range(2)]
        pbi = [ps("pbi%d" % i, [128, 512]) for i in range(2)]
        py = [ps("py%d" % i, [128, 512]) for i in range(2)]
        ptr = ps("ptr", [128, 128])

        for (t, src, k) in ((PRM, prm, "PRM"), (BRE, bre, "BRE"), (BIM, bim, "BIM"), (CRE, cre, "CRE"), (CIM, cim, "CIM"),
                            (TAU, tau, "TAU"), (ID, ident, "ID")):
            S.dma("sp", lambda e, t=t, src=src: e.dma_start(out=t[:], in_=src), writes=[k])
        PR3 = PRM[:].rearrange("p (a c) -> p a c", c=3)
        K = ["PP"]

        def V(fn, reads=(), writes=()):
            S.op("dve", fn, reads=list(reads) + K, writes=list(writes) + K)

        def A(fn, reads=(), writes=()):
            S.op("act", fn, reads=list(reads) + K, writes=list(writes) + K)

        A(lambda e: e.activation(P["DT"][:], PR3[:, :, 2], AF.Exp), reads=["PRM"])
        V(lambda e: e.tensor_scalar(P["LR"][:], PR3[:, :, 0], -1e-4, None, ALU.min), reads=["PRM"])
        V(lambda e: e.tensor_tensor(P["T0"][:], P["LR"][:], P["DT"][:], ALU.mult))
        A(lambda e: e.activation(P["MAG"][:], P["T0"][:], AF.Exp))
        V(lambda e: e.tensor_tensor(P["TH"][:], PR3[:, :, 1], P["DT"][:], ALU.mult), reads=["PRM"])
        V(lambda e: e.tensor_scalar(P["R"][:], P["TH"][:], 1.0 / TWO_PI, None, ALU.mult))
        V(lambda e: e.tensor_scalar(P["R2"][:], P["R"][:], 0.25, None, ALU.add))
        for (src, dst) in (("R", "SIN"), ("R2", "COS")):
            V(lambda e, src=src: e.tensor_copy(RI[:], P[src][:]))
            V(lambda e: e.tensor_copy(P["RF"][:], RI[:]))
            V(lambda e, src=src: e.tensor_tensor(P["FR"][:], P[src][:], P["RF"][:], ALU.subtract))
            A(lambda e, dst=dst: e.activation(P[dst][:], P["FR"][:], AF.Sin, scale=TWO_PI))
        V(lambda e: e.tensor_tensor(P["ARE"][:], P["MAG"][:], P["COS"][:], ALU.mult))
        V(lambda e: e.tensor_tensor(P["AIM"][:], P["MAG"][:], P["SIN"][:], ALU.mult))
        V(lambda e: e.tensor_tensor(P["T0"][:], P["LR"][:], P["LR"][:], ALU.mult))
        V(lambda e: e.tensor_tensor(P["T1"][:], PR3[:, :, 1], PR3[:, :, 1], ALU.mult), reads=["PRM"])
        V(lambda e: e.tensor_tensor(P["DEN"][:], P["T0"][:], P["T1"][:], ALU.add))
        V(lambda e: e.reciprocal(P["DEN"][:], P["DEN"][:]))
        V(lambda e: e.tensor_scalar(P["AM1"][:], P["ARE"][:], -1.0, None, ALU.add))
        V(lambda e: e.tensor_tensor(P["T0"][:], P["AM1"][:], P["LR"][:], ALU.mult))
        V(lambda e: e.tensor_tensor(P["T1"][:], P["AIM"][:], PR3[:, :, 1], ALU.mult), reads=["PRM"])
        V(lambda e: e.tensor_tensor(P["T0"][:], P["T0"][:], P["T1"][:], ALU.add))
        V(lambda e: e.tensor_tensor(P["FRE"][:], P["T0"][:], P["DEN"][:], ALU.mult))
        V(lambda e: e.tensor_tensor(P["T0"][:], P["AIM"][:], P["LR"][:], ALU.mult))
        V(lambda e: e.tensor_tensor(P["T1"][:], P["AM1"][:], PR3[:, :, 1], ALU.mult), reads=["PRM"])
        V(lambda e: e.tensor_tensor(P["T0"][:], P["T0"][:], P["T1"][:], ALU.subtract))
        V(lambda e: e.tensor_tensor(P["FIM"][:], P["T0"][:], P["DEN"][:], ALU.mult))

        def v3(t):
            return t[:].rearrange("p (a h) -> p a h", h=16)

        def bc(n):
            return P[n][:].unsqueeze(2).to_broadcast([128, 8, 16])
        V(lambda e: e.tensor_tensor(v3(BBR), v3(BRE), bc("FRE"), ALU.mult), reads=["BRE"])
        V(lambda e: e.tensor_tensor(v3(TB), v3(BIM), bc("FIM"), ALU.mult), reads=["BIM"])
        V(lambda e: e.tensor_tensor(BBR[:], BBR[:], TB[:], ALU.subtract))
        V(lambda e: e.tensor_tensor(v3(BBI), v3(BIM), bc("FRE"), ALU.mult), reads=["BIM"])
        V(lambda e: e.tensor_tensor(v3(TB), v3(BRE), bc("FIM"), ALU.mult), reads=["BRE"])
        V(lambda e: e.tensor_tensor(BBI[:], BBI[:], TB[:], ALU.add))
        V(lambda e: e.tensor_scalar(CIM[:], CIM[:], -1.0, None, ALU.mult), reads=["CIM"], writes=["CIM"])
        V(lambda e: e.memset(CR[:], 0.0)); V(lambda e: e.memset(CIN[:], 0.0))
        for dj in range(8):
            j = dj % 4
            for (src, dst) in ((BBR, WBR), (BBI, WBI)):
                V(lambda e: e.memset(PAD[:], 0.0), writes=["PAD"])
                V(lambda e, src=src, dj=dj, j=j: e.tensor_copy(PAD[0:64, 32 * j:32 * j + 16], src[0:64, dj * 16:dj * 16 + 16]), writes=["PAD"])
                V(lambda e, src=src, dj=dj, j=j: e.tensor_copy(PAD[64:128, 32 * j + 16:32 * j + 32], src[64:128, dj * 16:dj * 16 + 16]), writes=["PAD"])
                S.op("pe", lambda e: e.transpose(ptr[:], PAD[:], ID[:]), reads=["PAD", "ID"], writes=["ptr"])
                S.op("act", lambda e, dst=dst, dj=dj: e.copy(dst[:, dj, :], ptr[:]), reads=["ptr"], writes=["WB"])
            for (src, dst) in ((CRE, CR), (CIM, CIN)):
                V(lambda e, src=src, dst=dst, dj=dj, j=j: e.tensor_copy(dst[0:64, dj, 32 * j:32 * j + 16], src[0:64, dj * 16:dj * 16 + 16]), reads=["CRE", "CIM"], writes=["CC"])
                V(lambda e, src=src, dst=dst, dj=dj, j=j: e.tensor_copy(dst[64:128, dj, 32 * j + 16:32 * j + 32], src[64:128, dj * 16:dj * 16 + 16]), reads=["CRE", "CIM"], writes=["CC"])
            for (off, dst) in ((0.0, TS), (0.25, TC)):
                V(lambda e, dj=dj, off=off: e.tensor_scalar(RR[:], TAU[:], P["R"][:, dj:dj + 1], off, ALU.mult, ALU.add), reads=["TAU"], writes=["RR"])
                V(lambda e: e.tensor_copy(RRI[:], RR[:]), reads=["RR"], writes=["RRI"])
                V(lambda e: e.tensor_copy(RRF[:], RRI[:]), reads=["RRI"], writes=["RRF"])
                V(lambda e: e.tensor_tensor(RRF[:], RR[:], RRF[:], ALU.subtract), reads=["RR"], writes=["RRF"])
                S.op("act", lambda e, dst=dst, dj=dj: e.activation(dst[:, dj, :], RRF[:], AF.Sin, scale=TWO_PI), reads=["RRF"], writes=["TAB"])
            V(lambda e, dj=dj: e.tensor_copy(RHO[:, dj, :], P["MAG"][:, dj:dj + 1].to_broadcast([128, 512])), writes=["TAB"])

        G = "pool"
        oi = 0
        for d in range(2):
            V(lambda e: e.memset(HP[:], 0.0), writes=["HP"])
            for ci, (t0, T) in enumerate(S5_CH):
                ub_ = (d * 9 + ci) % 2
                uk = "UC%d" % ub_
                S.dma("sp", lambda e, d=d, t0=t0, T=T, ub_=ub_: e.dma_start(out=UC[ub_][:, :T], in_=uin[d][:, t0:t0 + T]), writes=[uk])
                yb_ = (d * 9 + ci) % 2
                for j in range(4):
                    dj = d * 4 + j
                    b = j % 2
                    w = lambda n, b=b, T=T: W[n, b][:, :T]
                    k = lambda n, b=b: "w_%s%d" % (n, b)
                    S.op("pe", lambda e, dj=dj, b=b, T=T, ub_=ub_: e.matmul(pbr[b][:, :T], WBR[:, dj, :], UC[ub_][:, :T], start=True, stop=True),
                         reads=["WB", uk], writes=["pbr%d" % b])
                    S.op("pe", lambda e, dj=dj, b=b, T=T, ub_=ub_: e.matmul(pbi[b][:, :T], WBI[:, dj, :], UC[ub_][:, :T], start=True, stop=True),
                         reads=["WB", uk], writes=["pbi%d" % b])
                    S.op("act", lambda e, w=w, b=b, T=T: e.copy(w("BR"), pbr[b][:, :T]), reads=["pbr%d" % b], writes=[k("BR")])
                    S.op("act", lambda e, w=w, b=b, T=T: e.copy(w("BI"), pbi[b][:, :T]), reads=["pbi%d" % b], writes=[k("BI")])
                    cs = lambda dj=dj, T=T: TC[:, dj, :T]
                    sn = lambda dj=dj, T=T: TS[:, dj, :T]
                    S.op("dve", lambda e, w=w, cs=cs: e.tensor_tensor(w("T1"), cs(), w("BR"), ALU.mult), reads=["TAB", k("BR")], writes=[k("T1")])
                    S.op("dve", lambda e, w=w, sn=sn: e.tensor_tensor(w("T2"), sn(), w("BI"), ALU.mult), reads=["TAB", k("BI")], writes=[k("T2")])
                    S.op("dve", lambda e, w=w: e.tensor_tensor(w("XR"), w("T1"), w("T2"), ALU.add), reads=[k("T1"), k("T2")], writes=[k("XR")])
                    S.op(G, lambda e, w=w, cs=cs: e.tensor_tensor(w("T3"), cs(), w("BI"), ALU.mult), reads=["TAB", k("BI")], writes=[k("T3")])
                    S.op(G, lambda e, w=w, sn=sn: e.tensor_tensor(w("T4"), sn(), w("BR"), ALU.mult), reads=["TAB", k("BR")], writes=[k("T4")])
                    S.op(G, lambda e, w=w: e.tensor_tensor(w("XI"), w("T3"), w("T4"), ALU.subtract), reads=[k("T3"), k("T4")], writes=[k("XI")])
                    S.op("dve", lambda e, w=w, dj=dj, j=j, T=T: e.tensor_tensor_scan(w("QR"), RHO[:, dj, :T], w("XR"), HP[:, 2 * j:2 * j + 1], ALU.mult, ALU.add),
                         reads=["TAB", k("XR"), "HP"], writes=[k("QR")])
                    S.op("dve", lambda e, w=w, dj=dj, j=j, T=T: e.tensor_tensor_scan(w("QI"), RHO[:, dj, :T], w("XI"), HP[:, 2 * j + 1:2 * j + 2], ALU.mult, ALU.add),
                         reads=["TAB", k("XI"), "HP"], writes=[k("QI")])
                    S.op("dve", lambda e, w=w, cs=cs: e.tensor_tensor(w("T1"), cs(), w("QR"), ALU.mult), reads=["TAB", k("QR")], writes=[k("T1")])
                    S.op("dve", lambda e, w=w, sn=sn: e.tensor_tensor(w("T2"), sn(), w("QI"), ALU.mult), reads=["TAB", k("QI")], writes=[k("T2")])
                    S.op("dve", lambda e, w=w: e.tensor_tensor(w("HR"), w("T1"), w("T2"), ALU.subtract), reads=[k("T1"), k("T2")], writes=[k("HR")])
                    S.op(G, lambda e, w=w, sn=sn: e.tensor_tensor(w("T3"), sn(), w("QR"), ALU.mult), reads=["TAB", k("QR")], writes=[k("T3")])
                    S.op(G, lambda e, w=w, cs=cs: e.tensor_tensor(w("T4"), cs(), w("QI"), ALU.mult), reads=["TAB", k("QI")], writes=[k("T4")])
                    S.op(G, lambda e, w=w: e.tensor_tensor(w("HI"), w("T3"), w("T4"), ALU.add), reads=[k("T3"), k("T4")], writes=[k("HI")])
                    S.op("act", lambda e, b=b, j=j, T=T: e.copy(HP[:, 2 * j:2 * j + 1], W["HR", b][:, T - 1:T]), reads=[k("HR")], writes=["HP"])
                    S.op("act", lambda e, b=b, j=j, T=T: e.copy(HP[:, 2 * j + 1:2 * j + 2], W["HI", b][:, T - 1:T]), reads=[k("HI")], writes=["HP"])
                    if own_only and not own_chunk:
                        continue
                    S.op("pe", lambda e, dj=dj, w=w, j=j, yb_=yb_, T=T: e.matmul(py[yb_][:, :T], CR[:, dj, :], w("HR"), start=(j == 0), stop=False),
                         reads=["CC", k("HR")], writes=["py%d" % yb_])
                    S.op("pe", lambda e, dj=dj, w=w, j=j, yb_=yb_, T=T: e.matmul(py[yb_][:, :T], CIN[:, dj, :], w("HI"), start=False, stop=(j == 3)),
                         reads=["CC", k("HI")], writes=["py%d" % yb_])
                if own_only and not own_chunk:
                    continue
                S.op("act", lambda e, yb_=yb_, T=T: e.copy(YO[yb_][:, :T], py[yb_][:, :T]), reads=["py%d" % yb_], writes=["YO%d" % yb_])
                oi += 1
                S.dma("act", lambda e, d=d, yb_=yb_, t0=t0, T=T: e.dma_start(out=yout[d][:, t0:t0 + T], in_=YO[yb_][:, :T]),
                      reads=["YO%d" % yb_], writes=["yout_%d" % oi])
        S.drain_all("sp")
        S.emit()
    return nc


NCH = 68


def build_PC():
    nc = bass.Bass("TRN2", target_bir_lowering=False)
    I = {}
    for d in range(2):
        I["qT", d] = nc.dram_tensor("qT%d" % d, [128, SEQT], F32, kind="ExternalInput").ap()
        I["kT", d] = nc.dram_tensor("kT%d" % d, [128, SEQT], F32, kind="ExternalInput").ap()
        I["v", d] = nc.dram_tensor("v%d" % d, [SEQT, 256], F32, kind="ExternalInput").ap()
        I["zT", d] = nc.dram_tensor("zT%d" % d, [16, SEQT], F32, kind="ExternalInput").ap()
        I["wg", d] = nc.dram_tensor("wg%d" % d, [16, 128], F32, kind="ExternalInput").ap()
        I["bg", d] = nc.dram_tensor("bg%d" % d, [128, 1], F32, kind="ExternalInput").ap()
        I["o", d] = nc.dram_tensor("o%d" % d, [SEQT, 256], F32, kind="ExternalOutput").ap()
    rst = nc.dram_tensor("rst", [128, 512], F32, kind="ExternalInput").ap()
    tmask = nc.dram_tensor("tmask", [64, 256], F32, kind="ExternalInput").ap()
    blk = nc.dram_tensor("blk", [128, 256], F32, kind="ExternalInput").ap()
    hmask = nc.dram_tensor("hmask", [128, 4], F32, kind="ExternalInput").ap()
    ident = nc.dram_tensor("ident", [128, 128], F32, kind="ExternalInput").ap()
    QSC = 32 ** -0.5
    with ExitStack() as st:
        S = Sched(nc, st)
        sb, ps = _mk(nc, st)
        RST = sb("RST", [128, 512]); TM = sb("TM", [64, 256]); BLK = sb("BLK", [128, 256]); HM = sb("HM", [128, 4]); ID = sb("ID", [128, 128])
        WG = sb("WG", [16, 128]); BG = sb("BG", [128, 1]); NBG = sb("NBG", [128, 1])
        Wb = {}
        for n in ("Q", "K", "LA", "B", "E", "D", "QE", "QS", "KD", "KS0", "KS1", "KS2", "KS3"):
            for i in range(2):
                Wb[n, i] = sb("g_%s%d" % (n, i), [128, 512])
        Z = [sb("Z%d" % i, [16, 512]) for i in range(2)]
        VV = [sb("VV%d" % i, [64, 8, 256]) for i in range(2)]
        DEC = [sb("DEC%d" % i, [128, 8]) for i in range(2)]
        KDT = [sb("KDT%d" % i, [64, 128]) for i in range(2)]
        STt = [sb("ST%d" % i, [64, 256]) for i in range(2)]
        OB = [sb("OB%d" % i, [64, 256]) for i in range(3)]
        KVM = sb("KVM", [128, 256])
        SS = [sb("SS%d" % i, [128, 256]) for i in range(2)]
        pza = ps("pza", [128, 512])
        pt0 = ps("pt0", [64, 128])
        pt = [pt0, pt0]
        pkv = [ps("pkv%d" % i, [128, 256]) for i in range(2)]
        psc = [ps("psc%d" % i, [64, 256]) for i in range(2)]
        po = [ps("po%d" % i, [64, 256]) for i in range(2)]
        for (t, src, k) in ((RST, rst, "RST"), (TM, tmask, "TM"), (BLK, blk, "BLK"), (HM, hmask, "HM"), (ID, ident, "ID")):
            S.dma("sp", lambda e, t=t, src=src: e.dma_start(out=t[:], in_=src), writes=[k])
        gc = 0
        oi = 0
        for d in range(2):
            S.dma("sp", lambda e, d=d: e.dma_start(out=WG[:], in_=I["wg", d]), writes=["WG"])
            S.dma("sp", lambda e, d=d: e.dma_start(out=BG[:], in_=I["bg", d]), writes=["BG"])
            S.op("dve", lambda e: e.tensor_scalar(NBG[:], BG[:], -1.0, None, ALU.mult), reads=["BG"], writes=["NBG"])
            S.op("dve", lambda e: e.memset(SS[0][:], 0.0), writes=["SS0"])
            scur = 0
            for bi, (t0, T) in enumerate(S5_CH):
                nchk = T // 64
                b = (d * 9 + bi) % 2
                w = lambda n, b=b, T=T: Wb[n, b][:, :T]
                k = lambda n, b=b: "g_%s%d" % (n, b)
                w3 = lambda n, b=b, T=T: Wb[n, b][:, :T].rearrange("p (c s) -> p c s", s=64)
                S.dma("sp", lambda e, d=d, b=b, t0=t0, T=T: e.dma_start(out=Wb["Q", b][:, :T], in_=I["qT", d][:, t0:t0 + T]), writes=[k("Q")])
                S.dma("act", lambda e, d=d, b=b, t0=t0, T=T: e.dma_start(out=Wb["K", b][:, :T], in_=I["kT", d][:, t0:t0 + T]), writes=[k("K")])
                S.dma("sp", lambda e, d=d, b=b, t0=t0, T=T: e.dma_start(out=Z[b][:, :T], in_=I["zT", d][:, t0:t0 + T]), writes=["Z%d" % b])
                S.dma("act", lambda e, d=d, b=b, t0=t0, T=T, nchk=nchk: e.dma_start(
                    out=VV[b][:, :nchk, :], in_=I["v", d][t0:t0 + T, :].rearrange("(c s) f -> s c f", s=64)), writes=["VV%d" % b])
                S.op("pe", lambda e, b=b, T=T: e.matmul(pza[:, :T], WG[:], Z[b][:, :T], start=True, stop=True), reads=["WG", "Z%d" % b], writes=["pza"])
                S.op("act", lambda e, w=w, T=T: e.activation(w("E"), pza[:, :T], AF.Exp, bias=NBG[:], scale=-1.0), reads=["pza", "NBG"], writes=[k("E")])
                S.op("act", lambda e, w=w: e.activation(w("E"), w("E"), AF.Ln, bias=1.0), reads=[k("E")], writes=[k("E")])
                S.op("dve", lambda e, w=w: e.tensor_scalar(w("LA"), w("E"), -1.0 / 16.0, None, ALU.mult), reads=[k("E")], writes=[k("LA")])
                S.op("dve", lambda e, w=w, T=T: e.tensor_tensor_scan(w("B"), RST[:, :T], w("LA"), 0.0, ALU.mult, ALU.add), reads=["RST", k("LA")], writes=[k("B")])
                S.op("act", lambda e, b=b, w3=w3, nchk=nchk: e.activation(DEC[b][:, :nchk], w3("B")[:, :, 63], AF.Exp), reads=[k("B")], writes=["DEC%d" % b])
                S.op("act", lambda e, w=w: e.activation(w("E"), w("B"), AF.Exp), reads=[k("B")], writes=[k("E")])
                S.op("dve", lambda e, w=w: e.scalar_tensor_tensor(w("QE"), w("Q"), QSC, w("E"), ALU.mult, ALU.mult), reads=[k("Q"), k("E")], writes=[k("QE")])
                S.op("dve", lambda e, w3=w3, nchk=nchk: e.tensor_tensor(w3("D"), w3("B"), w3("B")[:, :, 32:33].to_broadcast([128, nchk, 64]), ALU.subtract),
                     reads=[k("B")], writes=[k("D")])
                S.op("act", lambda e, w=w: e.activation(w("E"), w("D"), AF.Exp), reads=[k("D"), k("QE")], writes=[k("E")])
                S.op("dve", lambda e, w=w: e.scalar_tensor_tensor(w("QS"), w("Q"), QSC, w("E"), ALU.mult, ALU.mult), reads=[k("Q"), k("E")], writes=[k("QS")])
                S.op("act", lambda e, w=w: e.activation(w("E"), w("D"), AF.Exp, scale=-1.0), reads=[k("D"), k("QS")], writes=[k("E")])
                S.op("dve", lambda e, w=w: e.tensor_tensor(w("LA"), w("K"), w("E"), ALU.mult), reads=[k("K"), k("E"), k("B")], writes=[k("LA")])
                for h in range(4):
                    S.op("pool", lambda e, w=w, h=h: e.tensor_scalar(w("KS%d" % h), w("LA"), HM[:, h:h + 1], None, ALU.mult),
                         reads=[k("LA"), "HM"], writes=[k("KS%d" % h)])
                S.op("dve", lambda e, w3=w3, nchk=nchk: e.tensor_tensor(w3("D"), w3("B")[:, :, 63:64].to_broadcast([128, nchk, 64]), w3("B"), ALU.subtract),
                     reads=[k("B"), k("E"), k("LA")], writes=[k("D")])
                S.op("act", lambda e, w=w: e.activation(w("D"), w("D"), AF.Exp), reads=[k("D")], writes=[k("D")])
                S.op("dve", lambda e, w=w: e.tensor_tensor(w("KD"), w("K"), w("D"), ALU.mult), reads=[k("K"), k("D")], writes=[k("KD")])
                for c in range(nchk):
                    p2 = gc % 2
                    gc += 1
                    cs = slice(c * 64, (c + 1) * 64)
                    S.op("pe", lambda e, b=b, cs=cs, p2=p2: e.transpose(pt[p2][:], Wb["KD", b][:, cs], ID[:]), reads=[k("KD"), "ID"], writes=["pt"])
                    S.op("act", lambda e, p2=p2: e.copy(KDT[p2][:], pt[p2][:]), reads=["pt"], writes=["KDT%d" % p2])
                    S.op("pe", lambda e, b=b, c=c, p2=p2: e.matmul(pkv[p2][:], KDT[p2][:], VV[b][:, c, :], start=True, stop=True),
                         reads=["KDT%d" % p2, "VV%d" % b], writes=["pkv%d" % p2])
                    for h in range(4 if need_o else 0):
                        S.op("pe", lambda e, b=b, cs=cs, p2=p2, h=h: e.matmul(psc[p2][:, h * 64:(h + 1) * 64], Wb["KS%d" % h, b][:, cs], Wb["QS", b][:, cs],
                                                                            start=True, stop=True),
                             reads=[k("KS%d" % h), k("QS")], writes=["psc%d" % p2])
                    S.op("dve", lambda e, p2=p2: e.tensor_tensor(STt[p2][:], psc[p2][:], TM[:], ALU.mult), reads=["psc%d" % p2, "TM"], writes=["ST%d" % p2])
                    for h in range(4 if need_o else 0):
                        hs = slice(h * 64, (h + 1) * 64)
                        S.op("pe", lambda e, b=b, c=c, p2=p2, hs=hs: e.matmul(po[p2][:, hs], STt[p2][:, hs], VV[b][:, c, hs], start=True, stop=False),
                             reads=["ST%d" % p2, "VV%d" % b], writes=["po%d" % p2])
                        S.op("pe", lambda e, b=b, cs=cs, p2=p2, hs=hs, scur=scur: e.matmul(po[p2][:, hs], Wb["QE", b][:, cs], SS[scur][:, hs], start=False, stop=True),
                             reads=[k("QE"), "SS%d" % scur], writes=["po%d" % p2])
                    ob = oi % 3
                    oi += 1
                    S.op("act", lambda e, ob=ob, p2=p2: e.copy(OB[ob][:], po[p2][:]), reads=["po%d" % p2], writes=["OB%d" % ob])
                    S.dma("sp" if oi % 2 else "act", lambda e, d=d, ob=ob, t0=t0, c=c: e.dma_start(out=I["o", d][t0 + c * 64:t0 + (c + 1) * 64, :], in_=OB[ob][:]),
                          reads=["OB%d" % ob], writes=["o_%d" % oi])
                    S.op("dve", lambda e, p2=p2: e.tensor_tensor(KVM[:], pkv[p2][:], BLK[:], ALU.mult), reads=["pkv%d" % p2, "BLK"], writes=["KVM"])
                    S.op("dve", lambda e, b=b, c=c, scur=scur: e.scalar_tensor_tensor(SS[1 - scur][:], SS[scur][:], DEC[b][:, c:c + 1], KVM[:], ALU.mult, ALU.add),
                         reads=["SS%d" % scur, "DEC%d" % b, "KVM"], writes=["SS%d" % (1 - scur)])
                    scur = 1 - scur
        S.drain_all("sp")
        S.emit()
    return nc


NEXP = 16384


def build_PD(ntok, nctx, last):
    nc = bass.Bass("TRN2", target_bir_lowering=False)
    def din(name, shape, dt=F32):
        return nc.dram_tensor(name, shape, dt, kind="ExternalInput").ap()
    xT = din("xT", [D, ntok]); modT = din("modT", [128, 96])
    su = din("su", [ntok, 256]); sv = din("sv", [ntok, 256]); gg = din("gg", [ntok, 512])
    s5u = din("s5u", [256, ntok]); yf = din("yf", [256, ntok]); yb = din("yb", [256, ntok])
    of_ = din("of", [ntok, 512]); ob_ = din("ob", [ntok, 512])
    w_out = din("w_out", [D, D]); wsT = din("wsT", [128, 512]); sgub = din("sgub", [128, 4]); s5d = din("s5d", [128, 2])
    wglu = din("wglu", [256, 512]); ng = din("ng", [128, 512]); g2 = din("g2", [128, 8]); wq = din("wq", [D, 2048])
    keysT = din("keysT", [128, 2048]); eu = din("eu", [NEXP, D]); ev = din("ev", [NEXP, D]); gfin = din("gfin", [128, 8])
    ones = din("ones", [128, 128]); ident = din("ident", [128, 128]); iota16 = din("iota16", [128, 16])
    xo = nc.dram_tensor("xo", [D, ntok], F32, kind="ExternalOutput").ap()
    nb = ntok // 128
    with ExitStack() as st:
        S = Sched(nc, st)
        sb, ps = _mk(nc, st)
        MOD = sb("MOD", [128, 96]); WOUT = sb("WOUT", [128, 8, D]); WS = sb("WS", [128, 512]); SGUB = sb("SGUB", [128, 4]); S5D = sb("S5D", [128, 2])
        WGLU = sb("WGLU", [128, 2, 512]); NG = sb("NG", [128, 512]); G2 = sb("G2", [128, 8]); WQ = sb("WQ", [128, 8, 2048]); KEYS = sb("KEYS", [128, 2048])
        GF = sb("GF", [128, 8]); ONES = sb("ONES", [128, 128]); ID = sb("ID", [128, 128]); IOTA = sb("IOTA", [128, 16])
        A2 = sb("A2", [128, 16]); TA = sb("TA", [128, 16])
        U_ = sb("U_", [128, 256]); V_ = sb("V_", [128, 256]); GU = sb("GU", [128, 256]); GV = sb("GV", [128, 256]); SQ = sb("SQ", [128, 512])
        SS = sb("SS", [128, 8]); VN = sb("VN", [128, 256]); MIX = sb("MIX", [128, D])
        S5U = sb("S5U", [128, 2, 128]); YF = sb("YF", [128, 2, 128]); YB = sb("YB", [128, 2, 128]); GE = sb("GE", [128, 2, 128]); SG = sb("SG", [128, 256])
        OF = sb("OF", [128, 512]); OB = sb("OB", [128, 512]); GG = sb("GG", [128, 512]); SL = sb("SL", [128, 512])
        XT = sb("XT", [128, 8, 128]); X1 = sb("X1", [128, 8, 128]); XO = XT; HT = sb("HT", [128, 8, 128]); MIXT = HT; HTOK = sb("HTOK", [128, D])
        RSTD = sb("RSTD", [128, 128]); TMPB = sb("TMPB", [128, 128])
        SC = sb("SC", [128, 2048])
        M16 = sb("M16", [128, 256]); I16 = sb("I16", [128, 256], U32); IF16 = sb("IF16", [128, 256]); I1S = sb("I1S", [128, 128])
        CS = sb("CS", [128, 2048]); SC2 = CS; QT = CS[:].rearrange("p (q t) -> p q t", t=128); CS2 = sb("CS2", [128, 256])
        T16 = sb("T16", [128, 128]); P16 = sb("P16", [128, 128], U32); PF = sb("PF", [128, 128]); AI = sb("AI", [128, 128], I32)
        AFL = sb("AFL", [128, 128]); BFL = sb("BFL", [128, 128]); E1 = sb("E1", [128, 128]); E2 = sb("E2", [128, 128])
        EG = sb("EG", [128, 128]); GATE = sb("GATE", [128, 128]); IDXTI = sb("IDXTI", [128, 128], I32); GATET = sb("GATET", [128, 128])
        ACTT = sb("ACTT", [128, 128]); WT = sb("WT", [128, 128])
        UG = [sb("UG%d" % i, [128, D]) for i in range(2)]; VG = UG
        HB = [sb("HB%d" % i, [128, D]) for i in range(2)]
        P0 = ps("P0", [128, 2048]); P1 = ps("P1", [128, 1024]); P2 = ps("P2", [128, 512]); P3 = ps("P3", [128, 512])

        def V(fn, r=(), w=()):
            S.op("dve", fn, reads=r, writes=w)

        def A(fn, r=(), w=()):
            S.op("act", fn, reads=r, writes=w)

        def PE(fn, r=(), w=()):
            S.op("pe", fn, reads=r, writes=w)

        def LD(q, t, src, key):
            S.dma(q, lambda e: e.dma_start(out=t, in_=src), writes=[key])

        LD("sp", MOD[:], modT, "MOD"); LD("sp", WS[:], wsT, "WS"); LD("sp", SGUB[:], sgub, "SGUB"); LD("sp", S5D[:], s5d, "S5D")
        LD("sp", WGLU[:], wglu.rearrange("(c p) f -> p c f", p=128), "WGLU"); LD("sp", NG[:], ng, "NG"); LD("sp", G2[:], g2, "G2")
        LD("sp", KEYS[:], keysT, "KEYS"); LD("sp", GF[:], gfin, "GF"); LD("sp", ONES[:], ones, "ONES"); LD("sp", ID[:], ident, "ID"); LD("sp", IOTA[:], iota16, "IOTA")
        wo_v = w_out.rearrange("(k p) f -> p k f", p=128)
        wq_v = wq.rearrange("(k p) f -> p k f", p=128)
        for k in range(8):
            LD("act", WOUT[:, k, :], wo_v[:, k, :], "WOUT")
            LD("act", WQ[:, k, :], wq_v[:, k, :], "WQ")
        MOD3 = MOD[:].rearrange("p (j c) -> p j c", c=2)
        V(lambda e: e.tensor_scalar(TA[:].rearrange("p (j c) -> p j c", c=2), MOD3[:, 32:40, :], 1.0, None, ALU.add), ["MOD"], ["TA"])
        V(lambda e: e.tensor_tensor(A2[:].rearrange("p (j c) -> p j c", c=2), TA[:].rearrange("p (j c) -> p j c", c=2),
                                    G2[:].unsqueeze(2).to_broadcast([128, 8, 2]), ALU.mult), ["TA", "G2"], ["A2"])
        A23 = A2[:].rearrange("p (j c) -> p j c", c=2)

        def rs_from_ss(ss, n, scale):
            V(lambda e: e.tensor_scalar(ss, ss, scale, EPS, ALU.mult, ALU.add), ["SS"], ["SS"])
            A(lambda e: e.sqrt(ss, ss), ["SS"], ["SS"])
            V(lambda e: e.reciprocal(ss, ss), ["SS"], ["SS"])

        def top16(src, scratch, mout, iout, n):
            V(lambda e: e.max(mout[:, 0:8], src), ["TK"], ["TK"])
            V(lambda e: e.max_index(iout[:, 0:8], mout[:, 0:8], src), ["TK"], ["TK"])
            V(lambda e: e.match_replace(scratch, mout[:, 0:8], src, -1e30), ["TK"], ["TK"])
            V(lambda e: e.max(mout[:, 8:16], scratch), ["TK"], ["TK"])
            V(lambda e: e.max_index(iout[:, 8:16], mout[:, 8:16], scratch), ["TK"], ["TK"])

        xT_v = xT.rearrange("(k p) t -> p k t", p=128)
        xo_v = xo.rearrange("(k p) t -> p k t", p=128)
        s5u_v = s5u.rearrange("(c p) t -> p c t", p=128)
        yf_v = yf.rearrange("(c p) t -> p c t", p=128)
        yb_v = yb.rearrange("(c p) t -> p c t", p=128)
        for bi in range(nb):
            tk = slice(bi * 128, (bi + 1) * 128)
            col = 1 if bi * 128 < nctx else 0
            LD("sp", U_[:], su[tk, :], "U_"); LD("sp", V_[:], sv[tk, :], "V_"); LD("sp", GG[:], gg[tk, :], "GG")
            LD("act", S5U[:], s5u_v[:, :, tk], "S5U"); LD("act", YF[:], yf_v[:, :, tk], "YF"); LD("act", YB[:], yb_v[:, :, tk], "YB")
            LD("sp", OF[:], of_[tk, :], "OF"); LD("sp", OB[:], ob_[tk, :], "OB"); LD("act", XT[:], xT_v[:, :, tk], "XT")
            A(lambda e: e.activation(GU[:], U_[:], AF.Gelu), ["U_"], ["GU"])
            A(lambda e: e.activation(GV[:], V_[:], AF.Gelu), ["V_"], ["GV"])
            V(lambda e: e.tensor_tensor(SQ[:, 0:256], GV[:], GV[:], ALU.mult), ["GV"], ["SQ"])
            V(lambda e: e.tensor_reduce(SS[:, 0:4], SQ[:, 0:256].rearrange("p (h d) -> p h d", d=64), AX.X, ALU.add), ["SQ"], ["SS"])
            rs_from_ss(SS[:, 0:4], 4, 1.0 / 64)
            V(lambda e: e.tensor_tensor(VN[:].rearrange("p (h d) -> p h d", d=64), GV[:].rearrange("p (h d) -> p h d", d=64),
                                        SS[:, 0:4].unsqueeze(2).to_broadcast([128, 4, 64]), ALU.mult), ["GV", "SS"], ["VN"])
            for h in range(4):
                PE(lambda e, h=h: e.matmul(P2[:, h * 64:(h + 1) * 64], WS[:, h * 128:(h + 1) * 128], VN[:, h * 64:(h + 1) * 64], start=True, stop=True),
                   ["WS", "VN"], ["P2"])
            V(lambda e: e.tensor_tensor(MIX[:, 0:256].rearrange("p (h d) -> p h d", d=64), P2[:, 0:256].rearrange("p (h d) -> p h d", d=64),
                                        SGUB[:].unsqueeze(2).to_broadcast([128, 4, 64]), ALU.add), ["P2", "SGUB"], ["MIXa"])
            V(lambda e: e.tensor_tensor(MIX[:, 0:256], MIX[:, 0:256], GU[:], ALU.mult), ["GU"], ["MIXa"])
            V(lambda e: e.tensor_tensor(YF[:], YF[:], YB[:], ALU.add), ["YB"], ["YF"])
            for ct in range(2):
                V(lambda e, ct=ct: e.scalar_tensor_tensor(YF[:, ct, :], S5U[:, ct, :], S5D[:, ct:ct + 1], YF[:, ct, :], ALU.mult, ALU.add),
                  ["S5U", "S5D"], ["YF"])
            A(lambda e: e.activation(GE[:], YF[:], AF.Gelu), ["YF"], ["GE"])
            for ct in range(2):
                PE(lambda e, ct=ct: e.matmul(P3[:, 0:512], GE[:, ct, :], WGLU[:, ct, :], start=(ct == 0), stop=(ct == 1)), ["GE", "WGLU"], ["P3"])
            A(lambda e: e.activation(SG[:], P3[:, 256:512], AF.Sigmoid), ["P3"], ["SG"])
            V(lambda e: e.tensor_tensor(MIX[:, 256:512], P3[:, 0:256], SG[:], ALU.mult), ["P3", "SG"], ["MIXb"])
            V(lambda e: e.tensor_tensor(OF[:], OF[:], OB[:], ALU.add), ["OB"], ["OF"])
            V(lambda e: e.tensor_tensor(SQ[:], OF[:], OF[:], ALU.mult), ["OF"], ["SQ"])
            V(lambda e: e.tensor_reduce(SS[:, 0:8], SQ[:].rearrange("p (h d) -> p h d", d=64), AX.X, ALU.add), ["SQ"], ["SS"])
            rs_from_ss(SS[:, 0:8], 8, 1.0 / 64)
            V(lambda e: e.tensor_tensor(OF[:].rearrange("p (h d) -> p h d", d=64), OF[:].rearrange("p (h d) -> p h d", d=64),
                                        SS[:, 0:8].unsqueeze(2).to_broadcast([128, 8, 64]), ALU.mult), ["SS"], ["OF"])
            V(lambda e: e.tensor_tensor(OF[:], OF[:], NG[:], ALU.mult), ["NG"], ["OF"])
            A(lambda e: e.activation(SL[:], GG[:], AF.Silu), ["GG"], ["SL"])
            V(lambda e: e.tensor_tensor(MIX[:, 512:1024], OF[:], SL[:], ALU.mult), ["OF", "SL"], ["MIXc"])
            for f in range(8):
                pp, pk = (P2, "P2") if f % 2 == 0 else (P3, "P3")
                PE(lambda e, f=f, pp=pp: e.transpose(pp[:, 0:128], MIX[:, f * 128:(f + 1) * 128], ID[:]), ["MIXa", "MIXb", "MIXc", "ID"], [pk])
                A(lambda e, f=f, pp=pp: e.copy(MIXT[:, f, :], pp[:, 0:128]), [pk], ["HT"])
            for ot in range(8):
                pp, pk = (P2, "P2") if ot % 2 == 0 else (P3, "P3")
                for k in range(8):
                    PE(lambda e, ot=ot, k=k, pp=pp: e.matmul(pp[:, 0:128], WOUT[:, k, ot * 128:(ot + 1) * 128], MIXT[:, k, :], start=(k == 0), stop=(k == 7)),
                       ["WOUT", "HT"], [pk])
                V(lambda e, ot=ot, pp=pp, col=col: e.scalar_tensor_tensor(X1[:, ot, :], pp[:, 0:128], MOD3[:, 16 + ot, col:col + 1], XT[:, ot, :], ALU.mult, ALU.add),
                  [pk, "MOD", "XT"], ["X1"])
            for k in range(8):
                A(lambda e, k=k: e.activation(TMPB[:], X1[:, k, :], AF.Square), ["X1"], ["TMPB"])
                PE(lambda e, k=k: e.matmul(P2[:, 0:128], ONES[:], TMPB[:], start=(k == 0), stop=(k == 7)), ["ONES", "TMPB"], ["P2"])
            V(lambda e: e.tensor_scalar(RSTD[:], P2[:, 0:128], 1.0 / D, EPS, ALU.mult, ALU.add), ["P2"], ["RSTD"])
            A(lambda e: e.sqrt(RSTD[:], RSTD[:]), ["RSTD"], ["RSTD"])
            V(lambda e: e.reciprocal(RSTD[:], RSTD[:]), ["RSTD"], ["RSTD"])
            for k in range(8):
                V(lambda e, k=k: e.tensor_tensor(TMPB[:], X1[:, k, :], RSTD[:], ALU.mult), ["X1", "RSTD"], ["TMPB"])
                A(lambda e, k=k, col=col: e.activation(HT[:, k, :], TMPB[:], AF.Identity, bias=MOD3[:, 24 + k, col:col + 1], scale=A23[:, k, col:col + 1]),
                  ["TMPB", "MOD", "A2"], ["HT"])
            for k in range(8):
                pp, pk = (P2, "P2") if k % 2 == 0 else (P3, "P3")
                PE(lambda e, k=k, pp=pp: e.transpose(pp[:, 0:128], HT[:, k, :], ID[:]), ["HT", "ID"], [pk])
                A(lambda e, k=k, pp=pp: e.copy(HTOK[:, k * 128:(k + 1) * 128], pp[:, 0:128]), [pk], ["HTOK"])
            for qt in range(16):
                pp, pk = (P2, "P2") if qt % 2 == 0 else (P3, "P3")
                for k in range(8):
                    PE(lambda e, qt=qt, k=k, pp=pp: e.matmul(pp[:, 0:128], WQ[:, k, qt * 128:(qt + 1) * 128], HT[:, k, :], start=(k == 0), stop=(k == 7)),
                       ["WQ", "HT"], [pk])
                if qt % 2 == 0:
                    A(lambda e, qt=qt, pp=pp: e.copy(QT[:, qt, :], pp[:, 0:128]), [pk], ["TK"])
                else:
                    V(lambda e, qt=qt, pp=pp: e.tensor_copy(QT[:, qt, :], pp[:, 0:128]), [pk], ["TK"])
            for qt in range(16):
                PE(lambda e, qt=qt: e.matmul(P0[:, qt * 128:(qt + 1) * 128], QT[:, qt, :], KEYS[:, qt * 128:(qt + 1) * 128], start=True, stop=True),
                   ["TK", "KEYS"], ["P0a", "P0b"])
            for q4 in range(4):
                A(lambda e, q4=q4: e.copy(SC[:, q4 * 512:(q4 + 1) * 512], P0[:, q4 * 512:(q4 + 1) * 512]), ["P0a", "P0b"], ["TK"])
            for qt in range(16):
                top16(SC[:, qt * 128:(qt + 1) * 128], SC2[:, qt * 128:(qt + 1) * 128], M16[:, qt * 16:(qt + 1) * 16], I16[:, qt * 16:(qt + 1) * 16], 128)
            V(lambda e: e.tensor_copy(IF16[:], I16[:]), ["TK"], ["TK"])
            M4 = M16[:].rearrange("p (h q k) -> p h q k", q=2, k=16)
            IF4 = IF16[:].rearrange("p (h q k) -> p h q k", q=2, k=16)
            I1S3 = I1S[:].rearrange("p (h k) -> p h k", k=16)
            V(lambda e: e.tensor_scalar(I1S3, IF4[:, :, 0, :], 128.0, None, ALU.mult), ["TK"], ["TK"])
            CS4 = CS[:].rearrange("p (h a b) -> p h a b", a=16, b=16)
            V(lambda e: e.tensor_tensor(CS4, M4[:, :, 0, :].unsqueeze(3).to_broadcast([128, 8, 16, 16]),
                                        M4[:, :, 1, :].unsqueeze(2).to_broadcast([128, 8, 16, 16]), ALU.add), ["TK"], ["TK"])
            for h in range(8):
                top16(CS[:, h * 256:(h + 1) * 256], CS2[:], T16[:, h * 16:(h + 1) * 16], P16[:, h * 16:(h + 1) * 16], 256)
            V(lambda e: e.tensor_copy(PF[:], P16[:]), ["TK"], ["TK"])
            V(lambda e: e.tensor_scalar(AFL[:], PF[:], -7.5, 1.0 / 16, ALU.add, ALU.mult), ["TK"], ["TK"])
            V(lambda e: e.tensor_copy(AI[:], AFL[:]), ["TK"], ["TK"])
            V(lambda e: e.tensor_copy(AFL[:], AI[:]), ["TK"], ["TK"])
            V(lambda e: e.scalar_tensor_tensor(BFL[:], AFL[:], -16.0, PF[:], ALU.mult, ALU.add), ["TK"], ["TK"])
            EQ4 = CS[:].rearrange("p (h k a) -> p h k a", k=16, a=16)
            io4 = IOTA[:].unsqueeze(1).unsqueeze(1).to_broadcast([128, 8, 16, 16])
            for (sel, src, dst) in ((AFL, I1S3, E1), (BFL, IF4[:, :, 1, :], E2)):
                V(lambda e, sel=sel: e.tensor_tensor(EQ4, io4, sel[:].rearrange("p (h k) -> p h k", k=16).unsqueeze(3).to_broadcast([128, 8, 16, 16]), ALU.is_equal),
                  ["TK", "IOTA"], ["TK"])
                V(lambda e, src=src: e.tensor_tensor(EQ4, EQ4, src.unsqueeze(2).to_broadcast([128, 8, 16, 16]), ALU.mult), ["TK"], ["TK"])
                V(lambda e, dst=dst: e.tensor_reduce(dst[:], CS[:].rearrange("p (m a) -> p m a", a=16), AX.X, ALU.add), ["TK"], ["TK"])
            V(lambda e: e.tensor_tensor(E1[:], E1[:], E2[:], ALU.add), ["TK"], ["TK"])
            T3 = T16[:].rearrange("p (h k) -> p h k", k=16)
            V(lambda e: e.tensor_tensor(EG[:].rearrange("p (h k) -> p h k", k=16), T3, T3[:, :, 0:1].to_broadcast([128, 8, 16]), ALU.subtract), ["TK"], ["TK"])
            A(lambda e: e.activation(EG[:], EG[:], AF.Exp), ["TK"], ["TK"])
            V(lambda e: e.tensor_reduce(SS[:, 0:8], EG[:].rearrange("p (h k) -> p h k", k=16), AX.X, ALU.add), ["TK"], ["SS"])
            V(lambda e: e.reciprocal(SS[:, 0:8], SS[:, 0:8]), ["SS"], ["SS"])
            V(lambda e: e.tensor_tensor(GATE[:].rearrange("p (h k) -> p h k", k=16), EG[:].rearrange("p (h k) -> p h k", k=16),
                                        SS[:, 0:8].unsqueeze(2).to_broadcast([128, 8, 16]), ALU.mult), ["TK", "SS"], ["GATE"])
            PE(lambda e: e.transpose(P2[:, 0:128], E1[:], ID[:]), ["TK", "ID"], ["P2"])
            V(lambda e: e.tensor_copy(IDXTI[:], P2[:, 0:128]), ["P2"], ["IDXTI"])
            PE(lambda e: e.transpose(P3[:, 0:128], GATE[:], ID[:]), ["GATE", "ID"], ["P3"])
            A(lambda e: e.copy(GATET[:], P3[:, 0:128]), ["P3"], ["GATET"])
            for t in range(128):
                b = t % 2
                S.dma("pool", lambda e, t=t, b=b: e.indirect_dma_start(out=UG[b][:], out_offset=None, in_=eu,
                                                                       in_offset=bass.IndirectOffsetOnAxis(ap=IDXTI[:, t:t + 1], axis=0)),
                      reads=["IDXTI"], writes=["UG%d" % b])
                pk = "P0a" if b == 0 else "P0b"
                for hf in range(2):
                    PE(lambda e, t=t, b=b, hf=hf: e.matmul(P0[:, b * 1024 + hf * 512:b * 1024 + (hf + 1) * 512], ID[:, t:t + 1].to_broadcast([128, 128]),
                                                           HTOK[:, hf * 512:(hf + 1) * 512], start=True, stop=True), ["ID", "HTOK"], [pk])
                    A(lambda e, b=b, hf=hf: e.copy(HB[b][:, hf * 512:(hf + 1) * 512], P0[:, b * 1024 + hf * 512:b * 1024 + (hf + 1) * 512]), [pk], ["HB%d" % b])
                V(lambda e, t=t, b=b: e.scalar_tensor_tensor(UG[b][:], UG[b][:], 1.0, HB[b][:], ALU.mult, ALU.mult, accum_out=ACTT[:, t:t + 1]),
                  ["HB%d" % b], ["UG%d" % b, "ACTT"])
            A(lambda e: e.activation(WT[:], ACTT[:], AF.Gelu), ["ACTT"], ["WT"])
            V(lambda e: e.tensor_tensor(WT[:], WT[:], GATET[:], ALU.mult), ["GATET"], ["WT"])
            for t in range(128):
                b = t % 2
                S.dma("pool", lambda e, t=t, b=b: e.indirect_dma_start(out=VG[b][:], out_offset=None, in_=ev,
                                                                       in_offset=bass.IndirectOffsetOnAxis(ap=IDXTI[:, t:t + 1], axis=0)),
                      reads=["IDXTI"], writes=["UG%d" % b])
                for ot in range(8):
                    PE(lambda e, t=t, b=b, ot=ot: e.matmul(P1[:, ot * 128 + t:ot * 128 + t + 1], VG[b][:, ot * 128:(ot + 1) * 128], WT[:, t:t + 1],
                                                           start=True, stop=True), ["UG%d" % b, "WT"], ["P1"])
            for ot in range(8):
                V(lambda e, ot=ot, col=col: e.scalar_tensor_tensor(XO[:, ot, :], P1[:, ot * 128:(ot + 1) * 128], MOD3[:, 40 + ot, col:col + 1], X1[:, ot, :],
                                                                   ALU.mult, ALU.add), ["P1", "MOD", "X1"], ["XT"])
            if last:
                for k in range(8):
                    A(lambda e, k=k: e.activation(TMPB[:], XO[:, k, :], AF.Square), ["XT"], ["TMPB"])
                    PE(lambda e, k=k: e.matmul(P2[:, 0:128], ONES[:], TMPB[:], start=(k == 0), stop=(k == 7)), ["ONES", "TMPB"], ["P2"])
                V(lambda e: e.tensor_scalar(RSTD[:], P2[:, 0:128], 1.0 / D, EPS, ALU.mult, ALU.add), ["P2"], ["RSTD"])
                A(lambda e: e.sqrt(RSTD[:], RSTD[:]), ["RSTD"], ["RSTD"])
                V(lambda e: e.reciprocal(RSTD[:], RSTD[:]), ["RSTD"], ["RSTD"])
                for k in range(8):
                    V(lambda e, k=k: e.scalar_tensor_tensor(XO[:, k, :], XO[:, k, :], GF[:, k:k + 1], RSTD[:], ALU.mult, ALU.mult), ["RSTD", "GF"], ["XT"])
            S.dma("sp", lambda e, tk=tk: e.dma_start(out=xo_v[:, :, tk], in_=XO[:]), reads=["XT"], writes=["xo_%d" % bi])
        S.drain_all("sp")
        S.emit()
    return nc


SEQC = 256
SEQL = 4096
OWN = 2176


def _mirror(t0, T):
    if t0 < SEQC:
        return 0, SEQC
    i = (t0 - SEQC) // 512
    return SEQC + SEQL - 512 * (i + 1), 512


def emit_A(nc, S, sb, ps, io, tl_all=False):
    xT, cT, w_mod, b_mod, g1, w_in, ones = io["xT"], io["cT"], io["w_mod"], io["b_mod"], io["g1"], io["w_in"], io["ones"]
    FMU, FMQ, FMK, FMZ, VT, TL, MODS = io["FMU"], io["FMQ"], io["FMK"], io["FMZ"], io["VT"], io["TL"], io["MODS"]
    CT = sb("CT", [128, 16]); SC = sb("SC", [128, 16]); BM = sb("BM", [128, 48]); G1 = sb("G1", [128, 8])
    ONES = sb("ONES", [128, 128]); MOD = sb("MOD", [128, 96])
    A1 = sb("A1", [128, 16]); TMPA = sb("TMPA", [128, 16])
    WM = [sb("WM%d" % i, [128, 8, 512]) for i in range(2)]
    WIN = sb("WIN", [128, 8, INW])
    XT = [sb("XT%d" % i, [128, 8, 512]) for i in range(2)]
    XSQ = sb("XSQ", [128, 512]); RSTD = sb("RSTD", [128, 512]); TMP = sb("TMP", [128, 512])
    HT = [sb("HT%d" % i, [128, 8, 512]) for i in range(2)]
    OUTB = [sb("OUTB%d" % i, [128, 512]) for i in range(4)]
    pmod = ps("pmod", [128, 96]); pss = ps("pss", [128, 512])
    pout = [ps("pout%d" % i, [128, 512]) for i in range(3)]
    S.dma("sp", lambda e: e.dma_start(out=CT[:], in_=cT), writes=["CT"])
    S.dma("sp", lambda e: e.dma_start(out=BM[:], in_=b_mod), writes=["BM"])
    S.dma("sp", lambda e: e.dma_start(out=G1[:], in_=g1), writes=["G1"])
    S.dma("sp", lambda e: e.dma_start(out=ONES[:], in_=ones), writes=["ONES"])
    S.op("act", lambda e: e.activation(SC[:], CT[:], AF.Silu), reads=["CT"], writes=["SC"])
    SC3 = SC[:].rearrange("p (k c) -> p k c", c=2)
    wm_v = w_mod.rearrange("(k p) f -> p k f", p=128)
    for jg in range(12):
        b = jg % 2
        S.dma("act" if jg % 2 else "sp",
              lambda e, jg=jg, b=b: e.dma_start(out=WM[b][:], in_=wm_v[:, :, jg * 512:(jg + 1) * 512]), writes=["WM%d" % b])
        for j8 in range(4):
            j = jg * 4 + j8
            for k in range(8):
                S.op("pe", lambda e, j=j, j8=j8, k=k, b=b: e.matmul(
                    pmod[:, 2 * j:2 * j + 2], WM[b][:, k, j8 * 128:(j8 + 1) * 128], SC3[:, k, :],
                    start=(k == 0), stop=(k == 7)), reads=["WM%d" % b, "SC"], writes=["pmod"])
    S.op("dve", lambda e: e.tensor_tensor(MOD[:].rearrange("p (j c) -> p j c", c=2), pmod[:].rearrange("p (j c) -> p j c", c=2),
                                          BM[:].unsqueeze(2).to_broadcast([128, 48, 2]), ALU.add), reads=["pmod", "BM"], writes=["MOD"])
    S.dma("sp", lambda e: e.dma_start(out=MODS, in_=MOD[:]), reads=["MOD"], writes=["MODS"])
    MOD3 = MOD[:].rearrange("p (j c) -> p j c", c=2)
    S.op("dve", lambda e: e.tensor_scalar(TMPA[:].rearrange("p (j c) -> p j c", c=2), MOD3[:, 8:16, :], 1.0, None, ALU.add), reads=["MOD"], writes=["TMPA"])
    S.op("dve", lambda e: e.tensor_tensor(A1[:].rearrange("p (j c) -> p j c", c=2), TMPA[:].rearrange("p (j c) -> p j c", c=2),
                                          G1[:].unsqueeze(2).to_broadcast([128, 8, 2]), ALU.mult), reads=["TMPA", "G1"], writes=["A1"])
    A13 = A1[:].rearrange("p (j c) -> p j c", c=2)
    win_v = w_in.rearrange("(k p) f -> p k f", p=128)
    for k in range(8):
        S.dma("pool", lambda e, k=k: e.dma_start(out=WIN[:, k, :], in_=win_v[:, k, :]), writes=["WIN%d" % k])
    WK = ["WIN%d" % k for k in range(8)]
    xT_v = xT.rearrange("(k p) t -> p k t", p=128)
    grp = [(0, SEQC, 1, -1)] + [(SEQC + 512 * g, 512, 0, g) for g in range(8)]
    cnt = {"o": 0}

    def evac(dst_ap_fn, pb, m, tn, key, cm=False):
        ob = cnt["o"] % 4
        cnt["o"] += 1
        if cm:
            o_ap = lambda: OUTB[ob][:m, :512].rearrange("p (w r) -> p w r", r=8)
            i_ap = lambda: pout[pb][:m, :512].rearrange("p (r w) -> p w r", w=64)
        else:
            o_ap = lambda: OUTB[ob][:m, :tn]
            i_ap = lambda: pout[pb][:m, :tn]
        if cnt["o"] % 2:
            S.op("act", lambda e: e.copy(o_ap(), i_ap()), reads=["pout%d" % pb], writes=["OUTB%d" % ob])
        else:
            S.op("dve", lambda e: e.tensor_copy(o_ap(), i_ap()), reads=["pout%d" % pb], writes=["OUTB%d" % ob])
        S.dma("sp" if cnt["o"] % 2 else "act", lambda e: dst_ap_fn(e, OUTB[ob]), reads=["OUTB%d" % ob], writes=["%s_%d" % (key, cnt["o"])])

    pi = 0
    for gi, (t0, tn, col, g) in enumerate(grp):
        b = gi % 2
        xk, hk = "XT%d" % b, "HT%d" % b
        S.dma("sp", lambda e, b=b, t0=t0, tn=tn: e.dma_start(out=XT[b][:, :, :tn], in_=xT_v[:, :, t0:t0 + tn]), writes=[xk])
        for k in range(8):
            S.op("act", lambda e, b=b, k=k, tn=tn: e.activation(XSQ[:, :tn], XT[b][:, k, :tn], AF.Square), reads=[xk], writes=["XSQ"])
            S.op("pe", lambda e, k=k, tn=tn: e.matmul(pss[:, :tn], ONES[:], XSQ[:, :tn], start=(k == 0), stop=(k == 7)), reads=["ONES", "XSQ"], writes=["pss"])
        S.op("dve", lambda e, tn=tn: e.tensor_scalar(RSTD[:, :tn], pss[:, :tn], 1.0 / D, EPS, ALU.mult, ALU.add), reads=["pss"], writes=["RSTD"])
        S.op("act", lambda e, tn=tn: e.sqrt(RSTD[:, :tn], RSTD[:, :tn]), reads=["RSTD"], writes=["RSTD"])
        S.op("dve", lambda e, tn=tn: e.reciprocal(RSTD[:, :tn], RSTD[:, :tn]), reads=["RSTD"], writes=["RSTD"])
        for k in range(8):
            S.op("dve", lambda e, b=b, k=k, tn=tn: e.tensor_tensor(TMP[:, :tn], XT[b][:, k, :tn], RSTD[:, :tn], ALU.mult), reads=[xk, "RSTD"], writes=["TMP"])
            S.op("act", lambda e, b=b, k=k, tn=tn, col=col: e.activation(HT[b][:, k, :tn], TMP[:, :tn], AF.Identity, bias=MOD3[:, k, col:col + 1],
                                                                         scale=A13[:, k, col:col + 1]), reads=["TMP", "MOD", "A1"], writes=[hk])
        fm = [(FMU, 0, 512, 128), (FMU, 128, 640, 128), (FMQ, 0, 768, 128), (FMQ, 128, 896, 128), (FMK, 0, 1024, 128), (FMK, 128, 1152, 128), (FMZ, 0, 2304, 32)]
        for (dst, r0, c0, m) in fm:
            pb = pi % 3
            pi += 1
            for k in range(8):
                S.op("pe", lambda e, b=b, k=k, tn=tn, c0=c0, m=m, pb=pb: e.matmul(pout[pb][:m, :tn], WIN[:, k, c0:c0 + m], HT[b][:, k, :tn], start=(k == 0), stop=(k == 7)),
                     reads=WK + [hk], writes=["pout%d" % pb])
            if dst is FMU or g < 0:
                evac(lambda e, ob, dst=dst, r0=r0, m=m, t0=t0, tn=tn: e.dma_start(out=dst[r0:r0 + m, t0:t0 + tn], in_=ob[:m, :tn]), pb, m, tn, "fm")
            else:
                evac(lambda e, ob, dst=dst, r0=r0, m=m, g=g: e.dma_start(
                    out=dst[r0:r0 + m, SEQC:].rearrange("p (w r) -> p w r", r=64)[:, :, 8 * g:8 * g + 8],
                    in_=ob[:m, :512].rearrange("p (w r) -> p w r", r=8)), pb, m, 512, "fm", cm=True)
        for ti in range(tn // 128):
            tl = [(VT, t0 + ti * 128, 0, 1280)]
            own_row = None
            if tl_all:
                own_row = t0 + ti * 128
            elif g < 0 and ti == 0:
                own_row = 0
            elif 0 <= g < 4:
                own_row = 128 + g * 512 + ti * 128
            if own_row is not None:
                tl += [(TL, own_row, 0, 0), (TL, own_row, 512, 1792)]
            for (dst, row, dc, c0) in tl:
                pb = pi % 3
                pi += 1
                for k in range(8):
                    S.op("pe", lambda e, b=b, k=k, ti=ti, c0=c0, pb=pb: e.matmul(pout[pb][:, :], HT[b][:, k, ti * 128:(ti + 1) * 128], WIN[:, k, c0:c0 + 512],
                                                                               start=(k == 0), stop=(k == 7)), reads=WK + [hk], writes=["pout%d" % pb])
                evac(lambda e, ob, dst=dst, row=row, dc=dc: e.dma_start(out=dst[row:row + 128, dc:dc + 512], in_=ob[:, :]), pb, 128, 512, "tm")


def emit_B(nc, S, sb, ps, io, own_only=False):
    FMU, YFs, YBs = io["FMU"], io["YF"], io["YB"]
    tau, ident = io["tau"], io["ident"]
    for ct in range(2):
        prm, bre, bim, cre, cim = io["prm"][ct], io["bre"][ct], io["bim"][ct], io["cre"][ct], io["cim"][ct]
        uin = [FMU, FMU]
        yout = [YFs, YBs]
        X = "c%d_" % ct
        sub = ExitStack()
        sb, ps = _mk(nc, sub)
        TWO_PI = 2.0 * math.pi
        PRM = sb(X + "PRM", [128, 24]); BRE = sb(X + "BRE", [128, 128]); BIM = sb(X + "BIM", [128, 128])
        CRE = sb(X + "CRE", [128, 128]); CIM = sb(X + "CIM", [128, 128]); TAU = sb(X + "TAU", [128, 512]); ID = sb(X + "ID", [128, 128])
        names = ["DT", "LR", "MAG", "TH", "R", "R2", "RF", "FR", "SIN", "COS", "ARE", "AIM", "DEN", "AM1", "FRE", "FIM", "T0", "T1"]
        P = {n: sb(X + "p_" + n, [128, 8]) for n in names}
        RI = sb(X + "p_RI", [128, 8], I32)
        BBR = sb(X + "BBR", [128, 128]); BBI = sb(X + "BBI", [128, 128]); TB = sb(X + "TB", [128, 128])
        PAD = sb(X + "PAD", [128, 128])
        WBR = sb(X + "WBR", [128, 8, 128]); WBI = sb(X + "WBI", [128, 8, 128]); CR = sb(X + "CR", [128, 8, 128]); CIN = sb(X + "CIN", [128, 8, 128])
        TC = sb(X + "TC", [128, 8, 512]); TS = sb(X + "TS", [128, 8, 512]); RHO = sb(X + "RHO", [128, 8, 512])
        RR = sb(X + "RR", [128, 512]); RRF = sb(X + "RRF", [128, 512]); RRI = sb(X + "RRI", [128, 512], I32)
        UC = [sb(X + "UC%d" % i, [128, 512]) for i in range(2)]
        W = {}
        for n in ("BR", "BI", "T1", "T2", "T3", "T4", "XR", "XI", "QR", "QI", "HR", "HI"):
            for i in range(2):
                W[n, i] = sb(X + "w_%s%d" % (n, i), [128, 512])
        HP = sb(X + "HP", [128, 8])
        YO = [sb(X + "YO%d" % i, [128, 512]) for i in range(2)]
        pbr = [ps(X + "pbr%d" % i, [128, 512]) for i in range(2)]
        pbi = [ps(X + "pbi%d" % i, [128, 512]) for i in range(2)]
        py = [ps(X + "py%d" % i, [128, 512]) for i in range(2)]
        ptr = ps(X + "ptr", [128, 128])

        for (t, src, k) in ((PRM, prm, "PRM"), (BRE, bre, "BRE"), (BIM, bim, "BIM"), (CRE, cre, "CRE"), (CIM, cim, "CIM"),
                            (TAU, tau, "TAU"), (ID, ident, "ID")):
            S.dma("sp", lambda e, t=t, src=src: e.dma_start(out=t[:], in_=src), writes=[k])
        PR3 = PRM[:].rearrange("p (a c) -> p a c", c=3)
        K = ["PP"]

        def V(fn, reads=(), writes=()):
            S.op("dve", fn, reads=list(reads) + K, writes=list(writes) + K)

        def A(fn, reads=(), writes=()):
            S.op("act", fn, reads=list(reads) + K, writes=list(writes) + K)

        A(lambda e: e.activation(P["DT"][:], PR3[:, :, 2], AF.Exp), reads=["PRM"])
        V(lambda e: e.tensor_scalar(P["LR"][:], PR3[:, :, 0], -1e-4, None, ALU.min), reads=["PRM"])
        V(lambda e: e.tensor_tensor(P["T0"][:], P["LR"][:], P["DT"][:], ALU.mult))
        A(lambda e: e.activation(P["MAG"][:], P["T0"][:], AF.Exp))
        V(lambda e: e.tensor_tensor(P["TH"][:], PR3[:, :, 1], P["DT"][:], ALU.mult), reads=["PRM"])
        V(lambda e: e.tensor_scalar(P["R"][:], P["TH"][:], 1.0 / TWO_PI, None, ALU.mult))
        V(lambda e: e.tensor_scalar(P["R2"][:], P["R"][:], 0.25, None, ALU.add))
        for (src, dst) in (("R", "SIN"), ("R2", "COS")):
            V(lambda e, src=src: e.tensor_copy(RI[:], P[src][:]))
            V(lambda e: e.tensor_copy(P["RF"][:], RI[:]))
            V(lambda e, src=src: e.tensor_tensor(P["FR"][:], P[src][:], P["RF"][:], ALU.subtract))
            A(lambda e, dst=dst: e.activation(P[dst][:], P["FR"][:], AF.Sin, scale=TWO_PI))
        V(lambda e: e.tensor_tensor(P["ARE"][:], P["MAG"][:], P["COS"][:], ALU.mult))
        V(lambda e: e.tensor_tensor(P["AIM"][:], P["MAG"][:], P["SIN"][:], ALU.mult))
        V(lambda e: e.tensor_tensor(P["T0"][:], P["LR"][:], P["LR"][:], ALU.mult))
        V(lambda e: e.tensor_tensor(P["T1"][:], PR3[:, :, 1], PR3[:, :, 1], ALU.mult), reads=["PRM"])
        V(lambda e: e.tensor_tensor(P["DEN"][:], P["T0"][:], P["T1"][:], ALU.add))
        V(lambda e: e.reciprocal(P["DEN"][:], P["DEN"][:]))
        V(lambda e: e.tensor_scalar(P["AM1"][:], P["ARE"][:], -1.0, None, ALU.add))
        V(lambda e: e.tensor_tensor(P["T0"][:], P["AM1"][:], P["LR"][:], ALU.mult))
        V(lambda e: e.tensor_tensor(P["T1"][:], P["AIM"][:], PR3[:, :, 1], ALU.mult), reads=["PRM"])
        V(lambda e: e.tensor_tensor(P["T0"][:], P["T0"][:], P["T1"][:], ALU.add))
        V(lambda e: e.tensor_tensor(P["FRE"][:], P["T0"][:], P["DEN"][:], ALU.mult))
        V(lambda e: e.tensor_tensor(P["T0"][:], P["AIM"][:], P["LR"][:], ALU.mult))
        V(lambda e: e.tensor_tensor(P["T1"][:], P["AM1"][:], PR3[:, :, 1], ALU.mult), reads=["PRM"])
        V(lambda e: e.tensor_tensor(P["T0"][:], P["T0"][:], P["T1"][:], ALU.subtract))
        V(lambda e: e.tensor_tensor(P["FIM"][:], P["T0"][:], P["DEN"][:], ALU.mult))

        def v3(t):
            return t[:].rearrange("p (a h) -> p a h", h=16)

        def bc(n):
            return P[n][:].unsqueeze(2).to_broadcast([128, 8, 16])
        V(lambda e: e.tensor_tensor(v3(BBR), v3(BRE), bc("FRE"), ALU.mult), reads=["BRE"])
        V(lambda e: e.tensor_tensor(v3(TB), v3(BIM), bc("FIM"), ALU.mult), reads=["BIM"])
        V(lambda e: e.tensor_tensor(BBR[:], BBR[:], TB[:], ALU.subtract))
        V(lambda e: e.tensor_tensor(v3(BBI), v3(BIM), bc("FRE"), ALU.mult), reads=["BIM"])
        V(lambda e: e.tensor_tensor(v3(TB), v3(BRE), bc("FIM"), ALU.mult), reads=["BRE"])
        V(lambda e: e.tensor_tensor(BBI[:], BBI[:], TB[:], ALU.add))
        V(lambda e: e.tensor_scalar(CIM[:], CIM[:], -1.0, None, ALU.mult), reads=["CIM"], writes=["CIM"])
        V(lambda e: e.memset(CR[:], 0.0)); V(lambda e: e.memset(CIN[:], 0.0))
        for dj in range(8):
            j = dj % 4
            for (src, dst) in ((BBR, WBR), (BBI, WBI)):
                V(lambda e: e.memset(PAD[:], 0.0), writes=["PAD"])
                V(lambda e, src=src, dj=dj, j=j: e.tensor_copy(PAD[0:64, 32 * j:32 * j + 16], src[0:64, dj * 16:dj * 16 + 16]), writes=["PAD"])
                V(lambda e, src=src, dj=dj, j=j: e.tensor_copy(PAD[64:128, 32 * j + 16:32 * j + 32], src[64:128, dj * 16:dj * 16 + 16]), writes=["PAD"])
                S.op("pe", lambda e: e.transpose(ptr[:], PAD[:], ID[:]), reads=["PAD", "ID"], writes=["ptr"])
                S.op("act", lambda e, dst=dst, dj=dj: e.copy(dst[:, dj, :], ptr[:]), reads=["ptr"], writes=["WB"])
            for (src, dst) in ((CRE, CR), (CIM, CIN)):
                V(lambda e, src=src, dst=dst, dj=dj, j=j: e.tensor_copy(dst[0:64, dj, 32 * j:32 * j + 16], src[0:64, dj * 16:dj * 16 + 16]), reads=["CRE", "CIM"], writes=["CC"])
                V(lambda e, src=src, dst=dst, dj=dj, j=j: e.tensor_copy(dst[64:128, dj, 32 * j + 16:32 * j + 32], src[64:128, dj * 16:dj * 16 + 16]), reads=["CRE", "CIM"], writes=["CC"])
            for (off, dst) in ((0.0, TS), (0.25, TC)):
                V(lambda e, dj=dj, off=off: e.tensor_scalar(RR[:], TAU[:], P["R"][:, dj:dj + 1], off, ALU.mult, ALU.add), reads=["TAU"], writes=["RR"])
                V(lambda e: e.tensor_copy(RRI[:], RR[:]), reads=["RR"], writes=["RRI"])
                V(lambda e: e.tensor_copy(RRF[:], RRI[:]), reads=["RRI"], writes=["RRF"])
                V(lambda e: e.tensor_tensor(RRF[:], RR[:], RRF[:], ALU.subtract), reads=["RR"], writes=["RRF"])
                S.op("act", lambda e, dst=dst, dj=dj: e.activation(dst[:, dj, :], RRF[:], AF.Sin, scale=TWO_PI), reads=["RRF"], writes=["TAB"])
            V(lambda e, dj=dj: e.tensor_copy(RHO[:, dj, :], P["MAG"][:, dj:dj + 1].to_broadcast([128, 512])), writes=["TAB"])

        G = "pool"
        oi = 0
        for d in range(2):
            V(lambda e: e.memset(HP[:], 0.0), writes=["HP"])
            for ci, (t0, T) in enumerate(S5_CH):
                ub_ = (d * 9 + ci) % 2
                uk = "UC%d" % ub_
                n0 = t0 if d == 0 else _mirror(t0, T)[0]
                own_chunk = n0 < SEQC + SEQL // 2
                if own_only and d == 0 and not own_chunk:
                    break
                S.dma("sp", lambda e, d=d, n0=n0, T=T, ub_=ub_, ct=ct: e.dma_start(out=UC[ub_][:, :T], in_=uin[d][ct * 128:(ct + 1) * 128, n0:n0 + T]), writes=[uk])
                ucv = (lambda ub_=ub_, T=T: UC[ub_][:, :T]) if d == 0 else (lambda ub_=ub_, T=T: UC[ub_][:, :T][:, ::-1])
                yb_ = (d * 9 + ci) % 2
                for j in range(4):
                    dj = d * 4 + j
                    b = j % 2
                    w = lambda n, b=b, T=T: W[n, b][:, :T]
                    k = lambda n, b=b: "w_%s%d" % (n, b)
                    S.op("pe", lambda e, dj=dj, b=b, T=T, ucv=ucv: e.matmul(pbr[b][:, :T], WBR[:, dj, :], ucv(), start=True, stop=True),
                         reads=["WB", uk], writes=["pbr%d" % b])
                    S.op("pe", lambda e, dj=dj, b=b, T=T, ucv=ucv: e.matmul(pbi[b][:, :T], WBI[:, dj, :], ucv(), start=True, stop=True),
                         reads=["WB", uk], writes=["pbi%d" % b])
                    S.op("act", lambda e, w=w, b=b, T=T: e.copy(w("BR"), pbr[b][:, :T]), reads=["pbr%d" % b], writes=[k("BR")])
                    S.op("act", lambda e, w=w, b=b, T=T: e.copy(w("BI"), pbi[b][:, :T]), reads=["pbi%d" % b], writes=[k("BI")])
                    cs = lambda dj=dj, T=T: TC[:, dj, :T]
                    sn = lambda dj=dj, T=T: TS[:, dj, :T]
                    S.op("dve", lambda e, w=w, cs=cs: e.tensor_tensor(w("T1"), cs(), w("BR"), ALU.mult), reads=["TAB", k("BR")], writes=[k("T1")])
                    S.op("dve", lambda e, w=w, sn=sn: e.tensor_tensor(w("T2"), sn(), w("BI"), ALU.mult), reads=["TAB", k("BI")], writes=[k("T2")])
                    S.op("dve", lambda e, w=w: e.tensor_tensor(w("XR"), w("T1"), w("T2"), ALU.add), reads=[k("T1"), k("T2")], writes=[k("XR")])
                    S.op(G, lambda e, w=w, cs=cs: e.tensor_tensor(w("T3"), cs(), w("BI"), ALU.mult), reads=["TAB", k("BI")], writes=[k("T3")])
                    S.op(G, lambda e, w=w, sn=sn: e.tensor_tensor(w("T4"), sn(), w("BR"), ALU.mult), reads=["TAB", k("BR")], writes=[k("T4")])
                    S.op(G, lambda e, w=w: e.tensor_tensor(w("XI"), w("T3"), w("T4"), ALU.subtract), reads=[k("T3"), k("T4")], writes=[k("XI")])
                    S.op("dve", lambda e, w=w, dj=dj, j=j, T=T: e.tensor_tensor_scan(w("QR"), RHO[:, dj, :T], w("XR"), HP[:, 2 * j:2 * j + 1], ALU.mult, ALU.add),
                         reads=["TAB", k("XR"), "HP"], writes=[k("QR")])
                    S.op("dve", lambda e, w=w, dj=dj, j=j, T=T: e.tensor_tensor_scan(w("QI"), RHO[:, dj, :T], w("XI"), HP[:, 2 * j + 1:2 * j + 2], ALU.mult, ALU.add),
                         reads=["TAB", k("XI"), "HP"], writes=[k("QI")])
                    S.op("dve", lambda e, w=w, cs=cs: e.tensor_tensor(w("T1"), cs(), w("QR"), ALU.mult), reads=["TAB", k("QR")], writes=[k("T1")])
                    S.op("dve", lambda e, w=w, sn=sn: e.tensor_tensor(w("T2"), sn(), w("QI"), ALU.mult), reads=["TAB", k("QI")], writes=[k("T2")])
                    S.op("dve", lambda e, w=w: e.tensor_tensor(w("HR"), w("T1"), w("T2"), ALU.subtract), reads=[k("T1"), k("T2")], writes=[k("HR")])
                    S.op(G, lambda e, w=w, sn=sn: e.tensor_tensor(w("T3"), sn(), w("QR"), ALU.mult), reads=["TAB", k("QR")], writes=[k("T3")])
                    S.op(G, lambda e, w=w, cs=cs: e.tensor_tensor(w("T4"), cs(), w("QI"), ALU.mult), reads=["TAB", k("QI")], writes=[k("T4")])
                    S.op(G, lambda e, w=w: e.tensor_tensor(w("HI"), w("T3"), w("T4"), ALU.add), reads=[k("T3"), k("T4")], writes=[k("HI")])
                    S.op("act", lambda e, b=b, j=j, T=T: e.copy(HP[:, 2 * j:2 * j + 1], W["HR", b][:, T - 1:T]), reads=[k("HR")], writes=["HP"])
                    S.op("act", lambda e, b=b, j=j, T=T: e.copy(HP[:, 2 * j + 1:2 * j + 2], W["HI", b][:, T - 1:T]), reads=[k("HI")], writes=["HP"])
                    if own_only and not own_chunk:
                        continue
                    S.op("pe", lambda e, dj=dj, w=w, j=j, yb_=yb_, T=T: e.matmul(py[yb_][:, :T], CR[:, dj, :], w("HR"), start=(j == 0), stop=False),
                         reads=["CC", k("HR")], writes=["py%d" % yb_])
                    S.op("pe", lambda e, dj=dj, w=w, j=j, yb_=yb_, T=T: e.matmul(py[yb_][:, :T], CIN[:, dj, :], w("HI"), start=False, stop=(j == 3)),
                         reads=["CC", k("HI")], writes=["py%d" % yb_])
                if own_only and not own_chunk:
                    continue
                if d == 0:
                    S.op("act", lambda e, yb_=yb_, T=T: e.copy(YO[yb_][:, :T], py[yb_][:, :T]), reads=["py%d" % yb_], writes=["YO%d" % yb_])
                else:
                    S.op("act", lambda e, yb_=yb_, T=T: e.copy(YO[yb_][:, :T][:, ::-1], py[yb_][:, :T]), reads=["py%d" % yb_], writes=["YO%d" % yb_])
                oi += 1
                S.dma("act", lambda e, d=d, yb_=yb_, n0=n0, T=T, ct=ct: e.dma_start(out=yout[d][ct * 128:(ct + 1) * 128, n0:n0 + T], in_=YO[yb_][:, :T]),
                      reads=["YO%d" % yb_], writes=["yout_%d" % oi])
        S.sync_all()
        S.emit()
        sub.close()


def emit_C(nc, S, sb_unused, ps_unused, io, own_only=False):
    FMQ, FMK, FMZ, VT = io["FMQ"], io["FMK"], io["FMZ"], io["VT"]
    OS = [io["OF"], io["OB"]]
    rst, tmask, tmask2, blk, hmask, ident = io["rst"], io["tmask"], io["tmask2"], io["blk"], io["hmask"], io["ident"]
    QSC = 32 ** -0.5
    for hh in range(2):
        X = "h%d_" % hh
        sub = ExitStack()
        sb, ps = _mk(nc, sub)
        cols = slice(hh * 256, hh * 256 + 256)
        TM2 = sb(X + "TM2", [64, 256])
        S.dma("sp", lambda e: e.dma_start(out=TM2[:], in_=tmask2), writes=["TM2"])
        RST = sb(X + "RST", [128, 512]); TM = sb(X + "TM", [64, 256]); BLK = sb(X + "BLK", [128, 256]); HM = sb(X + "HM", [128, 4]); ID = sb(X + "ID", [128, 128])
        WG = sb(X + "WG", [16, 128]); BG = sb(X + "BG", [128, 1]); NBG = sb(X + "NBG", [128, 1])
        Wb = {}
        for n in ("Q", "K", "LA", "B", "E", "D", "QE", "QS", "KD", "KS0", "KS1", "KS2", "KS3"):
            for i in range(2):
                Wb[n, i] = sb(X + "g_%s%d" % (n, i), [128, 512])
        Z = [sb(X + "Z%d" % i, [16, 512]) for i in range(2)]
        VV = [sb(X + "VV%d" % i, [64, 8, 256]) for i in range(2)]
        DEC = [sb(X + "DEC%d" % i, [128, 8]) for i in range(2)]
        KDT = [sb(X + "KDT%d" % i, [64, 128]) for i in range(2)]
        STt = [sb(X + "ST%d" % i, [64, 256]) for i in range(2)]
        OB = [sb(X + "OB%d" % i, [64, 256]) for i in range(3)]
        KVM = sb(X + "KVM", [128, 256])
        SS = [sb(X + "SS%d" % i, [128, 256]) for i in range(2)]
        pza = ps(X + "pza", [128, 512])
        pt0 = ps(X + "pt0", [64, 128])
        pt = [pt0, pt0]
        pkv = [ps(X + "pkv%d" % i, [128, 256]) for i in range(2)]
        psc = [ps(X + "psc%d" % i, [64, 256]) for i in range(2)]
        po = [ps(X + "po%d" % i, [64, 256]) for i in range(2)]
        for (t, src, k) in ((RST, rst, "RST"), (TM, tmask, "TM"), (BLK, blk, "BLK"), (HM, hmask, "HM"), (ID, ident, "ID")):
            S.dma("sp", lambda e, t=t, src=src: e.dma_start(out=t[:], in_=src), writes=[k])
        gc = 0
        oi = 0
        for d in range(2):
            S.dma("sp", lambda e, d=d, hh=hh: e.dma_start(out=WG[:], in_=io["wg"][hh][d]), writes=["WG"])
            S.dma("sp", lambda e, d=d, hh=hh: e.dma_start(out=BG[:], in_=io["bg"][hh][d]), writes=["BG"])
            S.op("dve", lambda e: e.tensor_scalar(NBG[:], BG[:], -1.0, None, ALU.mult), reads=["BG"], writes=["NBG"])
            S.op("dve", lambda e: e.memset(SS[0][:], 0.0), writes=["SS0"])
            scur = 0
            for bi, (t0, T) in enumerate(S5_CH):
                nchk = T // 64
                n0 = t0 if d == 0 else _mirror(t0, T)[0]
                w0 = (n0 - SEQC) // 64
                own_blk = n0 < SEQC + SEQL // 2
                if own_only and d == 0 and not own_blk:
                    break
                need_o = own_blk or not own_only
                b = (d * 9 + bi) % 2
                w = lambda n, b=b, T=T: Wb[n, b][:, :T]
                k = lambda n, b=b: "g_%s%d" % (n, b)
                w3 = lambda n, b=b, T=T: Wb[n, b][:, :T].rearrange("p (c s) -> p c s", s=64)
                S.dma("sp", lambda e, d=d, b=b, n0=n0, T=T, hh=hh: e.dma_start(out=Wb["Q", b][:, :T], in_=FMQ[hh * 128:(hh + 1) * 128, n0:n0 + T]), writes=[k("Q")])
                S.dma("act", lambda e, d=d, b=b, n0=n0, T=T, hh=hh: e.dma_start(out=Wb["K", b][:, :T], in_=FMK[hh * 128:(hh + 1) * 128, n0:n0 + T]), writes=[k("K")])
                S.dma("sp", lambda e, d=d, b=b, n0=n0, T=T: e.dma_start(out=Z[b][:, :T], in_=FMZ[d * 16:(d + 1) * 16, n0:n0 + T]), writes=["Z%d" % b])
                if t0 < SEQC:
                    S.dma("act", lambda e, b=b, nchk=nchk, cols=cols: e.dma_start(
                        out=VV[b][:, :nchk, :], in_=VT[0:SEQC, cols].rearrange("(c s) f -> s c f", s=64)), writes=["VV%d" % b])
                else:
                    S.dma("act", lambda e, b=b, w0=w0, cols=cols: e.dma_start(
                        out=VV[b][:, :, :], in_=VT[SEQC:, cols].rearrange("(r w) f -> r w f", w=64)[:, w0:w0 + 8, :]), writes=["VV%d" % b])
                S.op("pe", lambda e, b=b, T=T: e.matmul(pza[:, :T], WG[:], Z[b][:, :T], start=True, stop=True), reads=["WG", "Z%d" % b], writes=["pza"])
                S.op("act", lambda e, w=w, T=T: e.activation(w("E"), pza[:, :T], AF.Exp, bias=NBG[:], scale=-1.0), reads=["pza", "NBG"], writes=[k("E")])
                S.op("act", lambda e, w=w: e.activation(w("E"), w("E"), AF.Ln, bias=1.0), reads=[k("E")], writes=[k("E")])
                S.op("dve", lambda e, w=w: e.tensor_scalar(w("LA"), w("E"), -1.0 / 16.0, None, ALU.mult), reads=[k("E")], writes=[k("LA")])
                S.op("dve", lambda e, w=w, T=T: e.tensor_tensor_scan(w("B"), RST[:, :T], w("LA"), 0.0, ALU.mult, ALU.add), reads=["RST", k("LA")], writes=[k("B")])
                iref, ilast = (32, 63) if d == 0 else (31, 0)
                if d == 1:
                    S.op("dve", lambda e, w3=w3, nchk=nchk: e.tensor_tensor(w3("D"), w3("B")[:, :, 63:64].to_broadcast([128, nchk, 64]), w3("B"), ALU.subtract),
                         reads=[k("B")], writes=[k("D")])
                    S.op("dve", lambda e, w=w: e.tensor_tensor(w("B"), w("D"), w("LA"), ALU.add), reads=[k("D"), k("LA")], writes=[k("B")])
                S.op("act", lambda e, b=b, w3=w3, nchk=nchk, ilast=ilast: e.activation(DEC[b][:, :nchk], w3("B")[:, :, ilast], AF.Exp), reads=[k("B")], writes=["DEC%d" % b])
                S.op("act", lambda e, w=w: e.activation(w("E"), w("B"), AF.Exp), reads=[k("B")], writes=[k("E")])
                S.op("dve", lambda e, w=w: e.scalar_tensor_tensor(w("QE"), w("Q"), QSC, w("E"), ALU.mult, ALU.mult), reads=[k("Q"), k("E")], writes=[k("QE")])
                S.op("dve", lambda e, w3=w3, nchk=nchk, iref=iref: e.tensor_tensor(w3("D"), w3("B"), w3("B")[:, :, iref:iref + 1].to_broadcast([128, nchk, 64]), ALU.subtract),
                     reads=[k("B")], writes=[k("D")])
                S.op("act", lambda e, w=w: e.activation(w("E"), w("D"), AF.Exp), reads=[k("D"), k("QE")], writes=[k("E")])
                S.op("dve", lambda e, w=w: e.scalar_tensor_tensor(w("QS"), w("Q"), QSC, w("E"), ALU.mult, ALU.mult), reads=[k("Q"), k("E")], writes=[k("QS")])
                S.op("act", lambda e, w=w: e.activation(w("E"), w("D"), AF.Exp, scale=-1.0), reads=[k("D"), k("QS")], writes=[k("E")])
                S.op("dve", lambda e, w=w: e.tensor_tensor(w("LA"), w("K"), w("E"), ALU.mult), reads=[k("K"), k("E"), k("B")], writes=[k("LA")])
                for h in range(4):
                    S.op("pool", lambda e, w=w, h=h: e.tensor_scalar(w("KS%d" % h), w("LA"), HM[:, h:h + 1], None, ALU.mult),
                         reads=[k("LA"), "HM"], writes=[k("KS%d" % h)])
                S.op("dve", lambda e, w3=w3, nchk=nchk, ilast=ilast: e.tensor_tensor(w3("D"), w3("B")[:, :, ilast:ilast + 1].to_broadcast([128, nchk, 64]), w3("B"), ALU.subtract),
                     reads=[k("B"), k("E"), k("LA")], writes=[k("D")])
                S.op("act", lambda e, w=w: e.activation(w("D"), w("D"), AF.Exp), reads=[k("D")], writes=[k("D")])
                S.op("dve", lambda e, w=w: e.tensor_tensor(w("KD"), w("K"), w("D"), ALU.mult), reads=[k("K"), k("D")], writes=[k("KD")])
                for c in (range(nchk) if d == 0 else range(nchk - 1, -1, -1)):
                    p2 = gc % 2
                    gc += 1
                    cs = slice(c * 64, (c + 1) * 64)
                    S.op("pe", lambda e, b=b, cs=cs, p2=p2: e.transpose(pt[p2][:], Wb["KD", b][:, cs], ID[:]), reads=[k("KD"), "ID"], writes=["pt"])
                    S.op("act", lambda e, p2=p2: e.copy(KDT[p2][:], pt[p2][:]), reads=["pt"], writes=["KDT%d" % p2])
                    S.op("pe", lambda e, b=b, c=c, p2=p2: e.matmul(pkv[p2][:], KDT[p2][:], VV[b][:, c, :], start=True, stop=True),
                         reads=["KDT%d" % p2, "VV%d" % b], writes=["pkv%d" % p2])
                    for h in range(4 if need_o else 0):
                        S.op("pe", lambda e, b=b, cs=cs, p2=p2, h=h: e.matmul(psc[p2][:, h * 64:(h + 1) * 64], Wb["KS%d" % h, b][:, cs], Wb["QS", b][:, cs],
                                                                            start=True, stop=True),
                             reads=[k("KS%d" % h), k("QS")], writes=["psc%d" % p2])
                    if need_o:
                      S.op("dve", lambda e, p2=p2, d=d: e.tensor_tensor(STt[p2][:], psc[p2][:], (TM if d == 0 else TM2)[:], ALU.mult), reads=["psc%d" % p2, "TM", "TM2"], writes=["ST%d" % p2])
                    for h in range(4 if need_o else 0):
                        hs = slice(h * 64, (h + 1) * 64)
                        S.op("pe", lambda e, b=b, c=c, p2=p2, hs=hs: e.matmul(po[p2][:, hs], STt[p2][:, hs], VV[b][:, c, hs], start=True, stop=False),
                             reads=["ST%d" % p2, "VV%d" % b], writes=["po%d" % p2])
                        S.op("pe", lambda e, b=b, cs=cs, p2=p2, hs=hs, scur=scur: e.matmul(po[p2][:, hs], Wb["QE", b][:, cs], SS[scur][:, hs], start=False, stop=True),
                             reads=[k("QE"), "SS%d" % scur], writes=["po%d" % p2])
                    ob = oi % 3
                    oi += 1
                    if need_o:
                        S.op("act", lambda e, ob=ob, p2=p2: e.copy(OB[ob][:], po[p2][:]), reads=["po%d" % p2], writes=["OB%d" % ob])
                    if not need_o:
                        pass
                    elif t0 < SEQC:
                        S.dma("sp" if oi % 2 else "act", lambda e, d=d, ob=ob, c=c, cols=cols: e.dma_start(out=OS[d][c * 64:(c + 1) * 64, cols], in_=OB[ob][:]),
                              reads=["OB%d" % ob], writes=["o_%d" % oi])
                    else:
                        S.dma("sp" if oi % 2 else "act", lambda e, d=d, ob=ob, c=c, w0=w0, cols=cols: e.dma_start(
                            out=OS[d][SEQC:, cols].rearrange("(r w) f -> r w f", w=64)[:, w0 + c, :], in_=OB[ob][:]),
                              reads=["OB%d" % ob], writes=["o_%d" % oi])
                    S.op("dve", lambda e, p2=p2: e.tensor_tensor(KVM[:], pkv[p2][:], BLK[:], ALU.mult), reads=["pkv%d" % p2, "BLK"], writes=["KVM"])
                    S.op("dve", lambda e, b=b, c=c, scur=scur: e.scalar_tensor_tensor(SS[1 - scur][:], SS[scur][:], DEC[b][:, c:c + 1], KVM[:], ALU.mult, ALU.add),
                         reads=["SS%d" % scur, "DEC%d" % b, "KVM"], writes=["SS%d" % (1 - scur)])
                    scur = 1 - scur
        S.sync_all()
        S.emit()
        sub.close()


def emit_D(nc, S, sb, ps, io, blocks, last):
    xT = io["xT"]; modT = io["MODS"]; TLs = io["TL"]
    su = TLs[:, 0:256]; sv = TLs[:, 256:512]; gg = TLs[:, 512:1024]
    s5u = io["FMU"]; yf = io["YF"]; yb = io["YB"]; of_ = io["OF"]; ob_ = io["OB"]
    w_out, wsT, sgub, s5d, wglu, ng, g2, wq = io["w_out"], io["wsT"], io["sgub"], io["s5d"], io["wglu"], io["ng"], io["g2"], io["wq"]
    keysT, eu, ev, gfin, ones, ident, iota16 = io["keysT"], io["eu"], io["ev"], io["gfin"], io["ones"], io["ident"], io["iota16"]
    xo = io["xo"]
    nb = len(blocks)
    MOD = sb("MOD", [128, 96]); WOUT = sb("WOUT", [128, 8, D]); WS = sb("WS", [128, 512]); SGUB = sb("SGUB", [128, 4]); S5D = sb("S5D", [128, 2])
    WGLU = sb("WGLU", [128, 2, 512]); NG = sb("NG", [128, 512]); G2 = sb("G2", [128, 8]); WQt = sb("WQt", [128, 8, 1024]); GB = sb("GB", [128, 8 * 1024]); KEYS = sb("KEYS", [128, 2048])
    GF = sb("GF", [128, 8]); ONES = sb("ONES", [128, 128]); ID = sb("ID", [128, 128]); IOTA = sb("IOTA", [128, 16]); IO128 = sb("IO128", [128, 128])
    A2 = sb("A2", [128, 16]); TA = sb("TA", [128, 16])
    U_ = sb("U_", [128, 256]); V_ = sb("V_", [128, 256]); GU = U_; GV = V_; SQ = sb("SQ", [128, 512])
    SS = sb("SS", [128, 8]); VN = sb("VN", [128, 256]); MIX = None
    S5U = sb("S5U", [128, 2, 128]); YF = sb("YF", [128, 2, 128]); YB = sb("YB", [128, 2, 128]); GE = S5U; SG = sb("SG", [128, 256])
    OF = sb("OF", [128, 512]); OB = sb("OB", [128, 512]); GG = sb("GG", [128, 512]); SL = GG
    XT = sb("XT", [128, 8, 128]); X1S_ = [sb("X1_%d" % i, [128, 8, 128]) for i in range(2)]; HT = sb("HT", [128, 8, 128]); MIXT = HT; HTOKS = [sb("HTOK%d" % i, [128, D]) for i in range(2)]
    RSTD = sb("RSTD", [128, 128]); TMPB = sb("TMPB", [128, 128]); RSTD2 = sb("RSTD2", [128, 128]); TMPB2 = sb("TMPB2", [128, 128])
    SC = sb("SC", [128, 2048]); MIX = SC[:, 0:1024]
    M16 = sb("M16", [128, 256]); I16 = sb("I16", [128, 256], U32); IF16 = sb("IF16", [128, 256]); I1S = sb("I1S", [128, 128])
    CS = sb("CS", [128, 2048]); SC2 = CS; QT = CS[:].rearrange("p (q t) -> p q t", t=128); CS2 = sb("CS2", [128, 256])
    T16 = sb("T16", [128, 128]); P16 = sb("P16", [128, 128], U32); PF = sb("PF", [128, 128]); AI = sb("AI", [128, 128], I32)
    AFL = sb("AFL", [128, 128]); BFL = sb("BFL", [128, 128]); E1 = sb("E1", [128, 128]); E2 = sb("E2", [128, 128])
    EG = sb("EG", [128, 128]); GATES = [sb("GATE%d" % i, [128, 128]) for i in range(2)]; IDXS = [sb("IDXTI%d" % i, [128, 128], I32) for i in range(2)]
    ACTT = sb("ACTT", [128, 128]); WT = sb("WT", [128, 128])
    NBUF = 8
    WQ = WQt
    UG = [GB[:, i * 1024:(i + 1) * 1024] for i in range(NBUF)]; VG = UG
    HB = [sb("HB%d" % i, [128, D]) for i in range(2)]
    P0 = ps("P0", [128, 2048]); P1 = ps("P1", [128, 1024]); P2 = ps("P2", [128, 512]); P3 = ps("P3", [128, 512])

    def V(fn, r=(), w=()):
        S.op("dve", fn, reads=r, writes=w)

    def A(fn, r=(), w=()):
        S.op("act", fn, reads=r, writes=w)

    def PE(fn, r=(), w=()):
        S.op("pe", fn, reads=r, writes=w)

    def LD(q, t, src, key):
        S.dma(q, lambda e: e.dma_start(out=t, in_=src), writes=(key if isinstance(key, list) else [key]))

    LD("sp", MOD[:], modT, "MOD"); LD("sp", WS[:], wsT, "WS"); LD("sp", SGUB[:], sgub, "SGUB"); LD("sp", S5D[:], s5d, "S5D")
    LD("sp", WGLU[:], wglu.rearrange("(c p) f -> p c f", p=128), "WGLU"); LD("sp", NG[:], ng, "NG"); LD("sp", G2[:], g2, "G2")
    LD("sp", KEYS[:], keysT, "KEYS"); LD("sp", GF[:], gfin, "GF"); LD("sp", ONES[:], ones, "ONES"); LD("sp", ID[:], ident, "ID"); LD("sp", IOTA[:], iota16, "IOTA"); LD("sp", IO128[:], io["iota128"], "IO128")
    wo_v = w_out.rearrange("(k p) f -> p k f", p=128)
    wq_v = wq.rearrange("(k p) f -> p k f", p=128)
    for k in range(8):
        LD("act", WOUT[:, k, :], wo_v[:, k, :], "WOUT")
    MOD3 = MOD[:].rearrange("p (j c) -> p j c", c=2)
    V(lambda e: e.tensor_scalar(TA[:].rearrange("p (j c) -> p j c", c=2), MOD3[:, 32:40, :], 1.0, None, ALU.add), ["MOD"], ["TA"])
    V(lambda e: e.tensor_tensor(A2[:].rearrange("p (j c) -> p j c", c=2), TA[:].rearrange("p (j c) -> p j c", c=2),
                                G2[:].unsqueeze(2).to_broadcast([128, 8, 2]), ALU.mult), ["TA", "G2"], ["A2"])
    A23 = A2[:].rearrange("p (j c) -> p j c", c=2)

    def rs_from_ss(ss, n, scale):
        V(lambda e: e.tensor_scalar(ss, ss, scale, EPS, ALU.mult, ALU.add), ["SS"], ["SS"])
        A(lambda e: e.sqrt(ss, ss), ["SS"], ["SS"])
        V(lambda e: e.reciprocal(ss, ss), ["SS"], ["SS"])

    def top16(src, scratch, mout, iout, n):
        V(lambda e: e.max(mout[:, 0:8], src), ["TK"], ["TK"])
        V(lambda e: e.max_index(iout[:, 0:8], mout[:, 0:8], src), ["TK"], ["TK"])
        V(lambda e: e.match_replace(scratch, mout[:, 0:8], src, -1e30), ["TK"], ["TK"])
        V(lambda e: e.max(mout[:, 8:16], scratch), ["TK"], ["TK"])
        V(lambda e: e.max_index(iout[:, 8:16], mout[:, 8:16], scratch), ["TK"], ["TK"])

    xT_v = xT.rearrange("(k p) t -> p k t", p=128)
    xo_v = xo.rearrange("(k p) t -> p k t", p=128)
    s5u_v = s5u.rearrange("(c p) t -> p c t", p=128)
    yf_v = yf.rearrange("(c p) t -> p c t", p=128)
    yb_v = yb.rearrange("(c p) t -> p c t", p=128)
    def front(bi):
        pb_ = bi % 2
        HTOK = HTOKS[pb_]; X1 = X1S_[pb_]; IDXTI = IDXS[pb_]; GATE = GATES[pb_]
        KH = "HTOK%d" % pb_; KX = "X1_%d" % pb_; KI = "IDXTI%d" % pb_; KG = "GATE%d" % pb_
        sq0, r0_, oc0, isctx = blocks[bi]
        col = 1 if isctx else 0
        tk = slice(sq0, sq0 + 128)
        tr = slice(r0_, r0_ + 128)
        to = slice(oc0, oc0 + 128)
        XO = HB[1][:].rearrange("p (k t) -> p k t", t=128)
        yield
        LD("sp", U_[:], su[tr, :], "U_"); LD("sp", V_[:], sv[tr, :], "V_"); LD("sp", GG[:], gg[tr, :], "GG")
        yield
        LD("act", S5U[:], s5u_v[:, :, tk], "S5U"); LD("act", YF[:], yf_v[:, :, tk], "YF"); LD("act", YB[:], yb_v[:, :, tk], "YB")
        yield
        LD("sp", OF[:], of_[tk, :], "OF"); LD("sp", OB[:], ob_[tk, :], "OB"); LD("act", XT[:], xT_v[:, :, tk], "XT")
        yield
        A(lambda e: e.activation(GU[:], U_[:], AF.Gelu), ["U_"], ["U_"])
        yield
        A(lambda e: e.activation(GV[:], V_[:], AF.Gelu), ["V_"], ["V_"])
        yield
        V(lambda e: e.tensor_tensor(SQ[:, 0:256], GV[:], GV[:], ALU.mult), ["V_"], ["SQ"])
        yield
        V(lambda e: e.tensor_reduce(SS[:, 0:4], SQ[:, 0:256].rearrange("p (h d) -> p h d", d=64), AX.X, ALU.add), ["SQ"], ["SS"])
        yield
        rs_from_ss(SS[:, 0:4], 4, 1.0 / 64)
        yield
        V(lambda e: e.tensor_tensor(VN[:].rearrange("p (h d) -> p h d", d=64), GV[:].rearrange("p (h d) -> p h d", d=64),
                                    SS[:, 0:4].unsqueeze(2).to_broadcast([128, 4, 64]), ALU.mult), ["V_", "SS"], ["VN"])
        yield
        for h in range(4):
            PE(lambda e, h=h: e.matmul(P2[:, h * 64:(h + 1) * 64], WS[:, h * 128:(h + 1) * 128], VN[:, h * 64:(h + 1) * 64], start=True, stop=True),
               ["WS", "VN"], ["P2"])
            yield
        yield
        V(lambda e: e.tensor_tensor(MIX[:, 0:256].rearrange("p (h d) -> p h d", d=64), P2[:, 0:256].rearrange("p (h d) -> p h d", d=64),
                                    SGUB[:].unsqueeze(2).to_broadcast([128, 4, 64]), ALU.add), ["P2", "SGUB"], ["MIXa"])
        yield
        V(lambda e: e.tensor_tensor(MIX[:, 0:256], MIX[:, 0:256], GU[:], ALU.mult), ["U_"], ["MIXa"])
        yield
        V(lambda e: e.tensor_tensor(YF[:], YF[:], YB[:], ALU.add), ["YB"], ["YF"])
        yield
        for ct in range(2):
            V(lambda e, ct=ct: e.scalar_tensor_tensor(YF[:, ct, :], S5U[:, ct, :], S5D[:, ct:ct + 1], YF[:, ct, :], ALU.mult, ALU.add),
              ["S5U", "S5D"], ["YF"])
            yield
        yield
        A(lambda e: e.activation(GE[:], YF[:], AF.Gelu), ["YF"], ["S5U"])
        yield
        for ct in range(2):
            PE(lambda e, ct=ct: e.matmul(P3[:, 0:512], GE[:, ct, :], WGLU[:, ct, :], start=(ct == 0), stop=(ct == 1)), ["S5U", "WGLU"], ["P3"])
            yield
        yield
        A(lambda e: e.activation(SG[:], P3[:, 256:512], AF.Sigmoid), ["P3"], ["SG"])
        yield
        V(lambda e: e.tensor_tensor(MIX[:, 256:512], P3[:, 0:256], SG[:], ALU.mult), ["P3", "SG"], ["MIXb"])
        yield
        V(lambda e: e.tensor_tensor(OF[:], OF[:], OB[:], ALU.add), ["OB"], ["OF"])
        yield
        V(lambda e: e.tensor_tensor(SQ[:], OF[:], OF[:], ALU.mult), ["OF"], ["SQ"])
        yield
        V(lambda e: e.tensor_reduce(SS[:, 0:8], SQ[:].rearrange("p (h d) -> p h d", d=64), AX.X, ALU.add), ["SQ"], ["SS"])
        yield
        rs_from_ss(SS[:, 0:8], 8, 1.0 / 64)
        yield
        V(lambda e: e.tensor_tensor(OF[:].rearrange("p (h d) -> p h d", d=64), OF[:].rearrange("p (h d) -> p h d", d=64),
                                    SS[:, 0:8].unsqueeze(2).to_broadcast([128, 8, 64]), ALU.mult), ["SS"], ["OF"])
        yield
        V(lambda e: e.tensor_tensor(OF[:], OF[:], NG[:], ALU.mult), ["NG"], ["OF"])
        yield
        A(lambda e: e.activation(SL[:], GG[:], AF.Silu), ["GG"], ["GG"])
        yield
        V(lambda e: e.tensor_tensor(MIX[:, 512:1024], OF[:], SL[:], ALU.mult), ["OF", "GG"], ["MIXc"])
        yield
        for f in range(8):
            pp, pk = (P2, "P2") if f % 2 == 0 else (P3, "P3")
            PE(lambda e, f=f, pp=pp: e.transpose(pp[:, 0:128], MIX[:, f * 128:(f + 1) * 128], ID[:]), ["MIXa", "MIXb", "MIXc", "ID"], [pk])
            A(lambda e, f=f, pp=pp: e.copy(MIXT[:, f, :], pp[:, 0:128]), [pk], ["HT"])
            yield
        yield
        for ot in range(8):
            pp, pk = (P2, "P2") if ot % 2 == 0 else (P3, "P3")
            for k in range(8):
                PE(lambda e, ot=ot, k=k, pp=pp: e.matmul(pp[:, 0:128], WOUT[:, k, ot * 128:(ot + 1) * 128], MIXT[:, k, :], start=(k == 0), stop=(k == 7)),
                   ["WOUT", "HT"], [pk])
            V(lambda e, ot=ot, pp=pp, col=col: e.scalar_tensor_tensor(X1[:, ot, :], pp[:, 0:128], MOD3[:, 16 + ot, col:col + 1], XT[:, ot, :], ALU.mult, ALU.add),
              [pk, "MOD", "XT"], [KX])
            yield
        yield
        for k in range(8):
            A(lambda e, k=k: e.activation(TMPB[:], X1[:, k, :], AF.Square), [KX], ["TMPB"])
            PE(lambda e, k=k: e.matmul(P2[:, 0:128], ONES[:], TMPB[:], start=(k == 0), stop=(k == 7)), ["ONES", "TMPB"], ["P2"])
            yield
        yield
        V(lambda e: e.tensor_scalar(RSTD[:], P2[:, 0:128], 1.0 / D, EPS, ALU.mult, ALU.add), ["P2"], ["RSTD"])
        yield
        A(lambda e: e.sqrt(RSTD[:], RSTD[:]), ["RSTD"], ["RSTD"])
        yield
        V(lambda e: e.reciprocal(RSTD[:], RSTD[:]), ["RSTD"], ["RSTD"])
        yield
        for k in range(8):
            V(lambda e, k=k: e.tensor_tensor(TMPB[:], X1[:, k, :], RSTD[:], ALU.mult), [KX, "RSTD"], ["TMPB"])
            A(lambda e, k=k, col=col: e.activation(HT[:, k, :], TMPB[:], AF.Identity, bias=MOD3[:, 24 + k, col:col + 1], scale=A23[:, k, col:col + 1]),
              ["TMPB", "MOD", "A2"], ["HT"])
            yield
        yield
        for k in range(8):
            pp, pk = (P2, "P2") if k % 2 == 0 else (P3, "P3")
            PE(lambda e, k=k, pp=pp: e.transpose(pp[:, 0:128], HT[:, k, :], ID[:]), ["HT", "ID"], [pk])
            A(lambda e, k=k, pp=pp: e.copy(HTOK[:, k * 128:(k + 1) * 128], pp[:, 0:128]), [pk], [KH])
            yield
        yield
        for qt in range(16):
            pp, pk = (P2, "P2") if qt % 2 == 0 else (P3, "P3")
            if qt % 8 == 0:
                for k in range(8):
                    LD("act" if k % 2 else "sp", WQ[:, k, :], wq_v[:, k, (qt // 8) * 1024:(qt // 8 + 1) * 1024], "WQ")
            for k in range(8):
                PE(lambda e, qt=qt, k=k, pp=pp: e.matmul(pp[:, 0:128], WQ[:, k, (qt % 8) * 128:(qt % 8 + 1) * 128], HT[:, k, :], start=(k == 0), stop=(k == 7)),
                   ["WQ", "HT"], [pk])
            if qt % 2 == 0:
                A(lambda e, qt=qt, pp=pp: e.copy(QT[:, qt, :], pp[:, 0:128]), [pk], ["TK"])
            else:
                V(lambda e, qt=qt, pp=pp: e.tensor_copy(QT[:, qt, :], pp[:, 0:128]), [pk], ["TK"])
            yield
        yield
        for qt in range(16):
            PE(lambda e, qt=qt: e.matmul(P0[:, qt * 128:(qt + 1) * 128], QT[:, qt, :], KEYS[:, qt * 128:(qt + 1) * 128], start=True, stop=True),
               ["TK", "KEYS"], ["P0a", "P0b"])
            yield
        yield
        for q4 in range(4):
            A(lambda e, q4=q4: e.copy(SC[:, q4 * 512:(q4 + 1) * 512], P0[:, q4 * 512:(q4 + 1) * 512]), ["P0a", "P0b"], ["TK"])
            yield
        yield
        for qt in range(16):
            top16(SC[:, qt * 128:(qt + 1) * 128], SC2[:, qt * 128:(qt + 1) * 128], M16[:, qt * 16:(qt + 1) * 16], I16[:, qt * 16:(qt + 1) * 16], 128)
            yield
        yield
        V(lambda e: e.tensor_copy(IF16[:], I16[:]), ["TK"], ["TK"])
        yield
        M4 = M16[:].rearrange("p (h q k) -> p h q k", q=2, k=16)
        yield
        IF4 = IF16[:].rearrange("p (h q k) -> p h q k", q=2, k=16)
        yield
        I1S3 = I1S[:].rearrange("p (h k) -> p h k", k=16)
        yield
        V(lambda e: e.tensor_scalar(I1S3, IF4[:, :, 0, :], 128.0, None, ALU.mult), ["TK"], ["TK"])
        yield
        CS4 = CS[:].rearrange("p (h a b) -> p h a b", a=16, b=16)
        yield
        V(lambda e: e.tensor_tensor(CS4, M4[:, :, 0, :].unsqueeze(3).to_broadcast([128, 8, 16, 16]),
                                    M4[:, :, 1, :].unsqueeze(2).to_broadcast([128, 8, 16, 16]), ALU.add), ["TK"], ["TK"])
        yield
        for h in range(8):
            top16(CS[:, h * 256:(h + 1) * 256], CS2[:], T16[:, h * 16:(h + 1) * 16], P16[:, h * 16:(h + 1) * 16], 256)
            yield
        yield
        V(lambda e: e.tensor_copy(PF[:], P16[:]), ["TK"], ["TK"])
        yield
        V(lambda e: e.tensor_scalar(AFL[:], PF[:], -7.5, 1.0 / 16, ALU.add, ALU.mult), ["TK"], ["TK"])
        yield
        V(lambda e: e.tensor_copy(AI[:], AFL[:]), ["TK"], ["TK"])
        yield
        V(lambda e: e.tensor_copy(AFL[:], AI[:]), ["TK"], ["TK"])
        yield
        V(lambda e: e.scalar_tensor_tensor(BFL[:], AFL[:], -16.0, PF[:], ALU.mult, ALU.add), ["TK"], ["TK"])
        yield
        EQ4 = CS[:].rearrange("p (h k a) -> p h k a", k=16, a=16)
        yield
        io4 = IOTA[:].unsqueeze(1).unsqueeze(1).to_broadcast([128, 8, 16, 16])
        yield
        for (sel, src, dst) in ((AFL, I1S3, E1), (BFL, IF4[:, :, 1, :], E2)):
            V(lambda e, sel=sel: e.tensor_tensor(EQ4, io4, sel[:].rearrange("p (h k) -> p h k", k=16).unsqueeze(3).to_broadcast([128, 8, 16, 16]), ALU.is_equal),
              ["TK", "IOTA"], ["TK"])
            V(lambda e, src=src: e.tensor_tensor(EQ4, EQ4, src.unsqueeze(2).to_broadcast([128, 8, 16, 16]), ALU.mult), ["TK"], ["TK"])
            V(lambda e, dst=dst: e.tensor_reduce(dst[:], CS[:].rearrange("p (m a) -> p m a", a=16), AX.X, ALU.add), ["TK"], ["TK"])
            yield
        yield
        V(lambda e: e.tensor_tensor(E1[:], E1[:], E2[:], ALU.add), ["TK"], ["TK"])
        yield
        T3 = T16[:].rearrange("p (h k) -> p h k", k=16)
        yield
        V(lambda e: e.tensor_tensor(EG[:].rearrange("p (h k) -> p h k", k=16), T3, T3[:, :, 0:1].to_broadcast([128, 8, 16]), ALU.subtract), ["TK"], ["TK"])
        yield
        A(lambda e: e.activation(EG[:], EG[:], AF.Exp), ["TK"], ["TK"])
        yield
        V(lambda e: e.tensor_reduce(SS[:, 0:8], EG[:].rearrange("p (h k) -> p h k", k=16), AX.X, ALU.add), ["TK"], ["SS"])
        yield
        V(lambda e: e.reciprocal(SS[:, 0:8], SS[:, 0:8]), ["SS"], ["SS"])
        yield
        V(lambda e: e.tensor_tensor(GATE[:].rearrange("p (h k) -> p h k", k=16), EG[:].rearrange("p (h k) -> p h k", k=16),
                                    SS[:, 0:8].unsqueeze(2).to_broadcast([128, 8, 16]), ALU.mult), ["TK", "SS"], [KG])
        yield
        V(lambda e: e.tensor_copy(IDXTI[:], E1[:]), ["TK"], [KI])


    def loops(bi):
        pb_ = bi % 2
        HTOK = HTOKS[pb_]; X1 = X1S_[pb_]; IDXTI = IDXS[pb_]; GATE = GATES[pb_]
        KH = "HTOK%d" % pb_; KX = "X1_%d" % pb_; KI = "IDXTI%d" % pb_; KG = "GATE%d" % pb_
        sq0, r0_, oc0, isctx = blocks[bi]
        col = 1 if isctx else 0
        tk = slice(sq0, sq0 + 128)
        tr = slice(r0_, r0_ + 128)
        to = slice(oc0, oc0 + 128)
        XO = HB[1][:].rearrange("p (k t) -> p k t", t=128)
        yield
        for j in range(128):
            g = j % NBUF
            S.dma("pool", lambda e, j=j, g=g: e.indirect_dma_start(out=UG[g], out_offset=None, in_=eu,
                                                                   in_offset=bass.IndirectOffsetOnAxis(ap=IDXTI[:, j:j + 1], axis=0)),
                  reads=[KI], writes=["R%d" % g])
            V(lambda e, j=j, g=g: e.scalar_tensor_tensor(UG[g], UG[g], 1.0, HTOK[:], ALU.mult, ALU.mult, accum_out=ACTT[:, j:j + 1]),
              [KH], ["R%d" % g, "ACTT"])
            yield
        yield
        A(lambda e: e.activation(WT[:], ACTT[:], AF.Gelu), ["ACTT"], ["WT"])
        yield
        V(lambda e: e.tensor_tensor(WT[:], WT[:], GATE[:], ALU.mult), [KG], ["WT"])
        yield
        ACC = HB[0]
        yield
        for j in range(128):
            g = j % NBUF
            S.dma("pool", lambda e, j=j, g=g: e.indirect_dma_start(out=VG[g], out_offset=None, in_=ev,
                                                                   in_offset=bass.IndirectOffsetOnAxis(ap=IDXTI[:, j:j + 1], axis=0)),
                  reads=[KI], writes=["R%d" % g])
            if j == 0:
                V(lambda e, g=g: e.tensor_scalar(ACC[:], VG[g], WT[:, 0:1], None, ALU.mult), ["R%d" % g, "WT"], ["HB0"])
            else:
                V(lambda e, j=j, g=g: e.scalar_tensor_tensor(ACC[:], VG[g], WT[:, j:j + 1], ACC[:], ALU.mult, ALU.add), ["R%d" % g, "WT"], ["HB0"])
            yield
        yield
        for ot in range(8):
            pp, pk = (P1[:, 0:512], "P1a") if ot % 2 == 0 else (P1[:, 512:1024], "P1b")
            PE(lambda e, ot=ot, pp=pp: e.transpose(pp[:, 0:128], ACC[:, ot * 128:(ot + 1) * 128], ID[:]), ["HB0", "ID"], [pk])
            V(lambda e, ot=ot, col=col, pp=pp: e.scalar_tensor_tensor(XO[:, ot, :], pp[:, 0:128], MOD3[:, 40 + ot, col:col + 1], X1[:, ot, :],
                                                                      ALU.mult, ALU.add), [pk, "MOD", KX], ["HB1"])
            yield
        yield
        if last:
            for k in range(8):
                A(lambda e, k=k: e.activation(TMPB2[:], XO[:, k, :], AF.Square), ["HB1"], ["TMPB2"])
                PE(lambda e, k=k: e.matmul(P1[:, 0:128], ONES[:], TMPB2[:], start=(k == 0), stop=(k == 7)), ["ONES", "TMPB2"], ["P1a"])
            V(lambda e: e.tensor_scalar(RSTD2[:], P1[:, 0:128], 1.0 / D, EPS, ALU.mult, ALU.add), ["P1a"], ["RSTD2"])
            A(lambda e: e.sqrt(RSTD2[:], RSTD2[:]), ["RSTD2"], ["RSTD2"])
            V(lambda e: e.reciprocal(RSTD2[:], RSTD2[:]), ["RSTD2"], ["RSTD2"])
            for k in range(8):
                V(lambda e, k=k: e.scalar_tensor_tensor(XO[:, k, :], XO[:, k, :], GF[:, k:k + 1], RSTD2[:], ALU.mult, ALU.mult), ["RSTD2", "GF"], ["HB1"])
        yield
        S.dma("sp", lambda e, to=to: e.dma_start(out=xo_v[:, :, to], in_=XO[:]), reads=["HB1"], writes=["xo_%d" % bi])


    def run_all(g):
        for _ in g:
            pass
    run_all(front(0))
    for bi in range(nb):
        nxt = front(bi + 1) if bi + 1 < nb else None
        for step, _ in enumerate(loops(bi)):
            if nxt is not None:
                if next(nxt, "END") == "END":
                    nxt = None
        if nxt is not None:
            run_all(nxt)


def build_layer(last, dbg=False):
    nc = bass.Bass("TRN2", target_bir_lowering=False)
    io = {}

    def din(name, shape, dt=F32):
        io[name] = nc.dram_tensor(name, shape, dt, kind="ExternalInput").ap()

    def scr(name, shape):
        io[name] = nc.dram_tensor(name, shape, F32, kind=("ExternalOutput" if dbg else "Internal")).ap()
    nctx = 0 if last else 128
    ntok = 2048 + nctx
    din("xT", [D, SEQT]); din("cT", [128, 16]); din("w_mod", [D, NMOD * D]); din("b_mod", [128, 48]); din("g1", [128, 8]); din("w_in", [D, INW])
    din("ones", [128, 128]); din("ident", [128, 128]); din("tau", [128, 512]); din("iota16", [128, 16]); din("iota128", [128, 128])
    din("prm", [2, 128, 24]); din("bre", [2, 128, 128]); din("bim", [2, 128, 128]); din("cre", [2, 128, 128]); din("cim", [2, 128, 128])
    din("wg", [2, 2, 16, 128]); din("bg", [2, 2, 128, 1])
    din("rst", [128, 512]); din("tmask", [64, 256]); din("tmask2", [64, 256]); din("blk", [128, 256]); din("hmask", [128, 4])
    din("w_out", [D, D]); din("wsT", [128, 512]); din("sgub", [128, 4]); din("s5d", [128, 2]); din("wglu", [256, 512]); din("ng", [128, 512])
    din("g2", [128, 8]); din("wq", [D, 2048]); din("keysT", [128, 2048]); din("eu", [NEXP, D]); din("ev", [NEXP, D]); din("gfin", [128, 8])
    scr("FMU", [256, SEQT]); scr("FMQ", [256, SEQT]); scr("FMK", [256, SEQT]); scr("FMZ", [32, SEQT]); scr("VT", [SEQT, 512]); scr("TL", [OWN, 1024])
    scr("MODS", [128, 96]); scr("YF", [256, SEQT]); scr("YB", [256, SEQT]); scr("OF", [SEQT, 512]); scr("OB", [SEQT, 512]); scr("GS", [2, 16])
    io["xo"] = nc.dram_tensor("xo", [D, ntok], F32, kind="ExternalOutput").ap()
    with ExitStack() as top:
        gate = top.enter_context(nc.semaphore("gate"))
        ph = [0]

        def phase(fn):
            with ExitStack() as st:
                S = Sched(nc, top, gate, 16 * ph[0])
                sb, ps = _mk(nc, st)
                fn(S, sb, ps)
                S.drain_all("sp")
                GS = io["GS"]
                S.prog["sp"].append(("i", lambda e: e.dma_start(out=GS[0:1, :], in_=GS[1:2, :]), "gate", 16))
                S.emit()
            ph[0] += 1
        phase(lambda S, sb, ps: emit_A(nc, S, sb, ps, io))
        phase(lambda S, sb, ps: emit_B(nc, S, sb, ps, io))
        phase(lambda S, sb, ps: emit_C(nc, S, sb, ps, io))
        if last:
            blocks = [(SEQC + i * 128, 128 + i * 128, i * 128, False) for i in range(16)]
        else:
            blocks = [(0, 0, 0, True)] + [(SEQC + i * 128, 128 + i * 128, 128 + i * 128, False) for i in range(16)]
        phase(lambda S, sb, ps: emit_D(nc, S, sb, ps, io, blocks, last))
    return nc


def _lay_vec(v, n):
    return np.ascontiguousarray(np.asarray(v, np.float32).reshape(n, 128).T)


_CONST = {}


def _consts():
    if not _CONST:
        rst = np.ones((128, 512), np.float32)
        rst[:, ::64] = 0
        tm = (np.arange(64)[None, :] >= np.arange(64)[:, None]).astype(np.float32)
        _CONST.update(
            ones=np.ones((128, 128), np.float32), ident=np.eye(128, dtype=np.float32),
            iota16=np.ascontiguousarray(np.broadcast_to(np.arange(16, dtype=np.float32), (128, 16))),
            iota128=np.ascontiguousarray(np.broadcast_to(np.arange(128, dtype=np.float32), (128, 128))),
            tau=np.ascontiguousarray(np.broadcast_to(np.arange(1, 513, dtype=np.float32), (128, 512))),
            rst=rst, tmask=np.ascontiguousarray(np.tile(tm, (1, 4))), tmask2=np.ascontiguousarray(np.tile(tm.T, (1, 4))),
            blk=np.kron(np.eye(4, dtype=np.float32), np.ones((32, 64), np.float32)),
            hmask=np.kron(np.eye(4, dtype=np.float32), np.ones((32, 1), np.float32)))
    return _CONST


def _pb_params(P, l, gh, swap):
    def stt(a):
        if swap:
            a = a[::-1]
        g = a[:, gh * 8:gh * 8 + 8]
        g = g.reshape((2, 4, 2, 64) + a.shape[3:])
        g = np.moveaxis(g, (2, 3), (0, 1))
        return np.ascontiguousarray(g.reshape((128, 2, 4) + a.shape[3:]))
    lre = stt(P["s5_lambda_re"][l]); lim = stt(P["s5_lambda_im"][l])
    ls = stt(np.broadcast_to(P["s5_log_step"][l][:, :, None], (2, 16, 64)))
    prm = np.ascontiguousarray(np.stack([lre, lim, ls], -1).reshape(128, 24).astype(np.float32))
    return dict(prm=prm, bre=stt(P["s5_b_re"][l]).reshape(128, 128), bim=stt(P["s5_b_im"][l]).reshape(128, 128),
                cre=stt(np.swapaxes(P["s5_c_re"][l], 2, 3)).reshape(128, 128), cim=stt(np.swapaxes(P["s5_c_im"][l], 2, 3)).reshape(128, 128))


def _layer_weights(P, l, swap):
    C = _consts()
    sk = P["peer_sub_keys"][l]
    keysT = np.zeros((128, 16, 128), np.float32)
    for h in range(8):
        for p in range(2):
            keysT[:, h * 2 + p, :] = sk[p, h].T
    sw = P["sgu_w"][l]
    sbb = P["sgu_b"][l]
    wgate = P["gla_w_gate"][l]
    bgate = P["gla_b_gate"][l]
    if swap:
        sw = sw[:, ::-1, ::-1]
        sbb = sbb[:, ::-1]
        wgate = wgate[::-1]
        bgate = bgate[::-1]
    pb = [_pb_params(P, l, ct, swap) for ct in range(2)]
    w_in = P["w_in"][l]
    if swap:
        w_in = np.ascontiguousarray(np.concatenate([w_in[:, :2304], w_in[:, 2320:2336], w_in[:, 2304:2320]], 1))
    W = dict(w_mod=P["w_mod"][l], b_mod=_lay_vec(P["b_mod"][l], 48), g1=_lay_vec(P["norm1_g"][l], 8), w_in=w_in,
             w_out=P["w_out"][l], wsT=np.ascontiguousarray(np.transpose(sw, (2, 0, 1)).reshape(128, 512)),
             sgub=np.ascontiguousarray(sbb.T), s5d=_lay_vec(P["s5_d"][l], 2), wglu=P["s5_w_glu"][l],
             ng=np.ascontiguousarray(np.broadcast_to(P["gla_norm_g"][l], (128, 512))), g2=_lay_vec(P["norm2_g"][l], 8),
             wq=P["peer_w_query"][l], keysT=keysT.reshape(128, 2048), eu=P["peer_expert_u"][l], ev=P["peer_expert_v"][l],
             gfin=_lay_vec(P["final_norm_g"], 8),
             wg=np.ascontiguousarray(np.stack([[wgate[d][:, hh * 128:hh * 128 + 128] for d in range(2)] for hh in range(2)])),
             bg=np.ascontiguousarray(np.stack([[bgate[d][hh * 128:hh * 128 + 128][:, None] for d in range(2)] for hh in range(2)])))
    for k in ("prm", "bre", "bim", "cre", "cim"):
        W[k] = np.ascontiguousarray(np.stack([pb[0][k], pb[1][k]]))
    W.update(C)
    return W


LAYER_W = ["w_mod", "b_mod", "g1", "w_in", "prm", "bre", "bim", "cre", "cim", "wg", "bg", "w_out", "wsT", "sgub", "s5d", "wglu", "ng", "g2",
           "wq", "keysT", "eu", "ev"]
LAYER_W_SHAPES = dict(w_mod=[D, NMOD * D], b_mod=[128, 48], g1=[128, 8], w_in=[D, INW], prm=[2, 128, 24], bre=[2, 128, 128], bim=[2, 128, 128],
                      cre=[2, 128, 128], cim=[2, 128, 128], wg=[2, 2, 16, 128], bg=[2, 2, 128, 1], w_out=[D, D], wsT=[128, 512], sgub=[128, 4],
                      s5d=[128, 2], wglu=[256, 512], ng=[128, 512], g2=[128, 8], wq=[D, 2048], keysT=[128, 2048], eu=[NEXP, D], ev=[NEXP, D])


def build_full():
    nc = bass.Bass("TRN2", target_bir_lowering=False)
    io = {}

    def din(name, shape, dt=F32):
        io[name] = nc.dram_tensor(name, shape, dt, kind="ExternalInput").ap()

    def scr(name, shape):
        io[name] = nc.dram_tensor(name, shape, F32, kind="Internal").ap()
    din("xT", [D, SEQT]); din("cT", [128, 16]); din("gfin", [128, 8])
    din("ones", [128, 128]); din("ident", [128, 128]); din("tau", [128, 512]); din("iota16", [128, 16]); din("iota128", [128, 128])
    din("rst", [128, 512]); din("tmask", [64, 256]); din("tmask2", [64, 256]); din("blk", [128, 256]); din("hmask", [128, 4])
    for l in range(2):
        for n in LAYER_W:
            din("%s_%d" % (n, l), LAYER_W_SHAPES[n])
    scr("FMU", [256, SEQT]); scr("FMQ", [256, SEQT]); scr("FMK", [256, SEQT]); scr("FMZ", [32, SEQT]); scr("VT", [SEQT, 512]); scr("TL", [SEQT, 1024])
    scr("MODS", [128, 96]); scr("YF", [256, SEQT]); scr("YB", [256, SEQT]); scr("OF", [SEQT, 512]); scr("OB", [SEQT, 512]); scr("GS", [2, 16])
    scr("X1S", [D, SEQT])
    io["xo"] = nc.dram_tensor("xo", [D, 2048], F32, kind="ExternalOutput").ap()
    with ExitStack() as top:
        gate = top.enter_context(nc.semaphore("gate"))
        ph = [0]

        def phase(fn, nds=None):
            with ExitStack() as st:
                S = Sched(nc, top, gate, 16 * ph[0], nds=nds)
                sb, ps = _mk(nc, st)
                fn(S, sb, ps)
                S.drain_all("sp")
                GS = io["GS"]
                S.prog["sp"].append(("i", lambda e: e.dma_start(out=GS[0:1, :], in_=GS[1:2, :]), "gate", 16))
                S.emit()
            ph[0] += 1
        for l in range(2):
            last = l == 1
            iol = dict(io)
            for n in LAYER_W:
                iol[n] = io["%s_%d" % (n, l)]
            if last:
                iol["xT"] = io["X1S"]
                blocks = [(SEQC + i * 128, 128 + i * 128, i * 128, False) for i in range(16)]
            else:
                iol["xo"] = io["X1S"]
                blocks = [(0, 0, 0, True), (128, 128, 128, True)] + [(SEQC + i * 128, SEQC + i * 128, SEQC + i * 128, False) for i in range(32)]
            n2 = dict(sp=2, act=2, pool=2)
            phase(lambda S, sb, ps: emit_A(nc, S, sb, ps, iol, tl_all=not last), nds=n2)
            phase(lambda S, sb, ps: emit_B(nc, S, sb, ps, iol, own_only=last), nds=n2)
            phase(lambda S, sb, ps: emit_C(nc, S, sb, ps, iol), nds=n2)
            phase(lambda S, sb, ps: emit_D(nc, S, sb, ps, iol, blocks, last), nds=dict(sp=2, act=2, pool=7))
    return nc


_PROG = {}


def _prog(name, fn):
    if name not in _PROG:
        _PROG[name] = fn()
    return _PROG[name]


def kernel(**inputs):
    P = {k: np.ascontiguousarray(np.asarray(v)) for k, v in inputs.items()}
    cores = list(range(8))
    C = _consts()
    ncF = _prog("F", build_full)
    WL = {(l, sw): _layer_weights(P, l, sw) for l in range(2) for sw in (False, True)}
    in_maps = []
    for core in cores:
        b, half = divmod(core, 2)
        xc, xl = P["ctx"][b], P["x"][b]
        seq = np.concatenate([xc, xl], 0) if half == 0 else np.concatenate([xc[::-1], xl[::-1]], 0)
        cT = np.stack([_lay_vec(P["c"][b], 8), _lay_vec(P["c_ctx"], 8)], -1).reshape(128, 16)
        m = dict(xT=np.ascontiguousarray(seq.T), cT=np.ascontiguousarray(cT), gfin=_lay_vec(P["final_norm_g"], 8))
        m.update(C)
        for l in range(2):
            W = WL[l, half == 1]
            for n in LAYER_W:
                m["%s_%d" % (n, l)] = W[n]
        in_maps.append(m)
    rF = run_bass_kernel_spmd(ncF, in_maps, core_ids=cores).results
    out = np.zeros_like(P["x"])
    for core in cores:
        b, half = divmod(core, 2)
        xo = rF[core]["xo"].T
        if half == 0:
            out[b, 0:2048] = xo
        else:
            out[b, 2048:4096] = xo[::-1]
    return out.astype(np.float32)
```
